# Optimizing a Trainium2 kernel written in Bass

```python
import math
import jax, jax.numpy as jnp
from jax import lax
import numpy as np

D_MODEL = 1024
BATCH = 4
SEQ = 4096
DEPTH = 1

N_HEADS = 8
HEAD_DIM = 64
N_KV_HEADS = 2
GROUP = N_HEADS // N_KV_HEADS
ATTN_WIDTH = N_HEADS * HEAD_DIM
KV_W = N_KV_HEADS * HEAD_DIM
LRU_WIDTH = D_MODEL - ATTN_WIDTH
LRU_BLOCKS = 8
LRU_BLOCK_DIM = LRU_WIDTH // LRU_BLOCKS
CONV_WIDTH = 4
LRU_C = 8.0
CMP_LEN = 32
CMP_STRIDE = 16
CMP_HIDDEN = 256
SEL_LEN = 64
SEL_TOPK = 16
N_LOCAL_BLOCKS = 2
WINDOW = 512
Q_BLOCK = 128
N_BRANCH = 3
FORCE_LOCAL = 2.0e4
FORCE_INIT = 1.0e4
REL_BUCKETS = 32
REL_MAX_DIST = 128
PEER_HEADS = 8
PEER_N_KEYS = 128
PEER_EXPERTS = PEER_N_KEYS * PEER_N_KEYS
PEER_D_KEY = 256
PEER_TOPK = 16
PEER_CHUNK = 128
PLE_DIM = 256
EPS = 1e-6
NEG = -1e30
IN_SPLITS = [ATTN_WIDTH] + [KV_W] * 6 + [N_HEADS * N_BRANCH, LRU_WIDTH, LRU_WIDTH]
IN_COLS = sum(IN_SPLITS)

kernel_name = 'hymba_nsa_rglru_peer_layer'


def rmsnorm(x, g):
    xf = x.astype(jnp.float32)
    y = xf * lax.rsqrt(jnp.mean(xf * xf, axis=-1, keepdims=True) + EPS)
    return (y * g.astype(jnp.float32)).astype(x.dtype)


def masked_softmax(logits, mask):
    logits = jnp.where(mask, logits.astype(jnp.float32), NEG)
    p = jax.nn.softmax(logits, axis=-1)
    return jnp.where(mask, p, 0.0)


def rel_bucket(dist):
    n = jnp.maximum(dist, 0)
    max_exact = REL_BUCKETS // 2
    nf = jnp.maximum(n, max_exact).astype(jnp.float32)
    large = max_exact + (jnp.log(nf / max_exact) / math.log(REL_MAX_DIST / max_exact)
                         * (REL_BUCKETS - max_exact)).astype(jnp.int32)
    large = jnp.minimum(large, REL_BUCKETS - 1)
    return jnp.where(n < max_exact, n, large)


def nsa_mixer(q, kc, vc, ks, vs, kw, vw, gates, cmp_k_pe, cmp_k_w1, cmp_k_w2,
              cmp_v_pe, cmp_v_w1, cmp_v_w2, rel_table):
    B, T = q.shape[0], q.shape[1]
    dt = q.dtype
    f32 = jnp.float32
    table_kg = rel_table.T.reshape(N_KV_HEADS, GROUP, REL_BUCKETS)

    def heads(t, n):
        return t.reshape(B, T, n, HEAD_DIM).transpose(0, 2, 1, 3)

    qh = heads(q, N_HEADS).reshape(B, N_KV_HEADS, GROUP, T, HEAD_DIM) * (HEAD_DIM ** -0.5)
    kc, vc, ks, vs, kw, vw = [heads(t, N_KV_HEADS) for t in (kc, vc, ks, vs, kw, vw)]
    t_pos = jnp.arange(T, dtype=jnp.int32)

    n_cmp = (T - CMP_LEN) // CMP_STRIDE + 1
    cmp_start = jnp.arange(n_cmp, dtype=jnp.int32) * CMP_STRIDE
    tok_idx = cmp_start[:, None] + jnp.arange(CMP_LEN, dtype=jnp.int32)[None, :]

    def compress(kv, pe, w1, w2):
        blk = kv[:, :, tok_idx] + pe
        blk = blk.reshape(B, N_KV_HEADS, n_cmp, CMP_LEN * HEAD_DIM)
        return jax.nn.gelu(blk @ w1) @ w2

    k_cmp = compress(kc, cmp_k_pe, cmp_k_w1, cmp_k_w2)
    v_cmp = compress(vc, cmp_v_pe, cmp_v_w1, cmp_v_w2)
    dist_c = t_pos[:, None] - (cmp_start + CMP_LEN - 1)[None, :]
    bias_c = table_kg[:, :, rel_bucket(dist_c)]
    logit_c = jnp.einsum('bkgtd,bkcd->bkgtc', qh, k_cmp).astype(f32) + bias_c
    p_c = masked_softmax(logit_c, dist_c >= 0)
    o_c = jnp.einsum('bkgtc,bkcd->bkgtd', p_c.astype(dt), v_cmp)

    n_sel = T // SEL_LEN
    sel_start = jnp.arange(n_sel, dtype=jnp.int32) * SEL_LEN
    overlap = jnp.clip(jnp.minimum(cmp_start[:, None] + CMP_LEN, sel_start[None, :] + SEL_LEN)
                       - jnp.maximum(cmp_start[:, None], sel_start[None, :]), 0, None)
    overlap = overlap.astype(f32) / CMP_LEN
    imp = jnp.einsum('bkgtc,cs->bkts', p_c, overlap)
    d_blk = (t_pos // SEL_LEN)[:, None] - jnp.arange(n_sel, dtype=jnp.int32)[None, :]
    local = (d_blk >= 0) & (d_blk < N_LOCAL_BLOCKS)
    initial = (jnp.arange(n_sel) == 0)[None, :]
    imp = jnp.where(local, FORCE_LOCAL, jnp.where(initial, FORCE_INIT, jnp.where(d_blk >= 0, imp, -1.0)))
    k_top = min(SEL_TOPK, n_sel)
    _, sel_idx = lax.top_k(imp, k_top)

    ks_blk = ks.reshape(B, N_KV_HEADS, n_sel, SEL_LEN, HEAD_DIM)
    vs_blk = vs.reshape(B, N_KV_HEADS, n_sel, SEL_LEN, HEAD_DIM)
    kw_pad = jnp.pad(kw, ((0, 0), (0, 0), (WINDOW, 0), (0, 0)))
    vw_pad = jnp.pad(vw, ((0, 0), (0, 0), (WINDOW, 0), (0, 0)))
    span = WINDOW + Q_BLOCK
    dist_w = (jnp.arange(Q_BLOCK, dtype=jnp.int32)[:, None]
              - jnp.arange(span, dtype=jnp.int32)[None, :] + WINDOW)
    band = (dist_w >= 0) & (dist_w < WINDOW)
    bias_w = table_kg[:, :, rel_bucket(dist_w)]
    bi = jnp.arange(B)[:, None, None, None]
    hi = jnp.arange(N_KV_HEADS)[None, :, None, None]
    hk6 = jnp.arange(N_KV_HEADS)[None, :, None, None, None, None]
    gi6 = jnp.arange(GROUP)[None, None, :, None, None, None]
    n_keys_sel = k_top * SEL_LEN

    def chunk_fn(args):
        q_c, idx_c, q0 = args
        tq = q0 + jnp.arange(Q_BLOCK, dtype=jnp.int32)
        k_g = ks_blk[bi, hi, idx_c]
        v_g = vs_blk[bi, hi, idx_c]
        kpos = idx_c[..., None] * SEL_LEN + jnp.arange(SEL_LEN, dtype=jnp.int32)
        dist_s = tq[None, None, :, None, None] - kpos
        bias_s = table_kg[hk6, gi6, rel_bucket(dist_s)[:, :, None]]
        logit_s = jnp.einsum('bkgqd,bkqnsd->bkgqns', q_c, k_g).astype(f32) + bias_s
        logit_s = logit_s.reshape(B, N_KV_HEADS, GROUP, Q_BLOCK, n_keys_sel)
        mask_s = (dist_s >= 0).reshape(B, N_KV_HEADS, 1, Q_BLOCK, n_keys_sel)
        p_s = masked_softmax(logit_s, mask_s)
        o_s = jnp.einsum('bkgqn,bkqnd->bkgqd', p_s.astype(dt),
                         v_g.reshape(B, N_KV_HEADS, Q_BLOCK, n_keys_sel, HEAD_DIM))
        k_win = lax.dynamic_slice_in_dim(kw_pad, q0, span, axis=2)
        v_win = lax.dynamic_slice_in_dim(vw_pad, q0, span, axis=2)
        kpos_w = q0 - WINDOW + jnp.arange(span, dtype=jnp.int32)
        mask_w = band & (kpos_w >= 0)[None, :]
        logit_w = jnp.einsum('bkgqd,bksd->bkgqs', q_c, k_win).astype(f32) + bias_w
        p_w = masked_softmax(logit_w, mask_w)
        o_w = jnp.einsum('bkgqs,bksd->bkgqd', p_w.astype(dt), v_win)
        return o_s, o_w

    n_chunk = T // Q_BLOCK
    q_chunks = jnp.moveaxis(qh.reshape(B, N_KV_HEADS, GROUP, n_chunk, Q_BLOCK, HEAD_DIM), 3, 0)
    idx_chunks = jnp.moveaxis(sel_idx.reshape(B, N_KV_HEADS, n_chunk, Q_BLOCK, k_top), 2, 0)
    q0s = jnp.arange(n_chunk, dtype=jnp.int32) * Q_BLOCK
    o_s, o_w = lax.map(chunk_fn, (q_chunks, idx_chunks, q0s))
    o_s = jnp.moveaxis(o_s, 0, 3).reshape(B, N_HEADS, T, HEAD_DIM)
    o_w = jnp.moveaxis(o_w, 0, 3).reshape(B, N_HEADS, T, HEAD_DIM)
    o_c = o_c.reshape(B, N_HEADS, T, HEAD_DIM)

    g = jax.nn.sigmoid(gates.reshape(B, T, N_HEADS, N_BRANCH)).transpose(0, 2, 1, 3)
    o = g[..., 0:1] * o_c + g[..., 1:2] * o_s + g[..., 2:3] * o_w
    return o.transpose(0, 2, 1, 3).reshape(B, T, ATTN_WIDTH)


def rglru_mixer(xr, xg, conv_w, conv_b, wa, ba, wx, bx, lam):
    B, T, _ = xr.shape
    dt = xr.dtype
    xc = lax.conv_general_dilated(xr, conv_w, window_strides=(1,), padding=[(CONV_WIDTH - 1, 0)],
                                  dimension_numbers=('NWC', 'WIO', 'NWC'),
                                  feature_group_count=LRU_WIDTH) + conv_b
    xb = xc.reshape(B, T, LRU_BLOCKS, LRU_BLOCK_DIM)
    r = jax.nn.sigmoid(jnp.einsum('btnc,ncd->btnd', xb, wa).reshape(B, T, LRU_WIDTH) + ba)
    i = jax.nn.sigmoid(jnp.einsum('btnc,ncd->btnd', xb, wx).reshape(B, T, LRU_WIDTH) + bx)
    log_a = -LRU_C * r.astype(jnp.float32) * jax.nn.softplus(-lam.astype(jnp.float32))
    a = jnp.exp(log_a)
    b = jnp.sqrt(-jnp.expm1(2.0 * log_a)) * (i * xc).astype(jnp.float32)

    def combine(left, right):
        a1, b1 = left
        a2, b2 = right
        return a1 * a2, a2 * b1 + b2

    _, h = lax.associative_scan(combine, (a, b), axis=1)
    return h.astype(dt) * jax.nn.gelu(xg)


def peer_ffn(xn, wq, sub_keys, u_tab, v_tab):
    B, T, D = xn.shape
    dt = xn.dtype
    xt = xn.reshape(-1, PEER_CHUNK, D)

    def chunk(xc):
        q = (xc @ wq).reshape(PEER_CHUNK, PEER_HEADS, 2, PEER_D_KEY // 2)
        s = jnp.einsum('chpd,pkd->chpk', q, sub_keys).astype(jnp.float32)
        s1, i1 = lax.top_k(s[:, :, 0], PEER_TOPK)
        s2, i2 = lax.top_k(s[:, :, 1], PEER_TOPK)
        cand = (s1[..., :, None] + s2[..., None, :]).reshape(PEER_CHUNK, PEER_HEADS, PEER_TOPK * PEER_TOPK)
        sc, ci = lax.top_k(cand, PEER_TOPK)
        e = (jnp.take_along_axis(i1, ci // PEER_TOPK, axis=-1) * PEER_N_KEYS
             + jnp.take_along_axis(i2, ci % PEER_TOPK, axis=-1))
        g = jax.nn.softmax(sc, axis=-1).astype(dt)
        act = jax.nn.gelu(jnp.einsum('cd,chkd->chk', xc, u_tab[e]))
        return jnp.einsum('chk,chkd->cd', g * act, v_tab[e])

    return lax.map(chunk, xt).reshape(B, T, D)


def setup_inputs(seed: int = 0) -> dict:
    key = jax.random.key(seed)
    ks = jax.random.split(key, 32)
    f32 = jnp.float32

    def nrm(k, shape, scale):
        return jax.random.normal(k, shape, f32) * scale

    def gain(k, shape):
        return 1.0 + 0.02 * jax.random.normal(k, shape, f32)

    a8 = jax.random.uniform(ks[17], (DEPTH, LRU_WIDTH), f32, 0.9, 0.999)
    a = a8 ** (1.0 / LRU_C)
    lam = jnp.log(a) - jnp.log1p(-a)
    return {
        'x': nrm(ks[0], (BATCH, SEQ, D_MODEL), 1.0),
        'p': nrm(ks[1], (DEPTH, BATCH, SEQ, PLE_DIM), 1.0),
        'attn_norm': gain(ks[2], (DEPTH, D_MODEL)),
        'w_in': nrm(ks[3], (DEPTH, D_MODEL, IN_COLS), D_MODEL ** -0.5),
        'cmp_k_pe': nrm(ks[4], (DEPTH, CMP_LEN, HEAD_DIM), 0.3),
        'cmp_k_w1': nrm(ks[5], (DEPTH, CMP_LEN * HEAD_DIM, CMP_HIDDEN), (CMP_LEN * HEAD_DIM) ** -0.5),
        'cmp_k_w2': nrm(ks[6], (DEPTH, CMP_HIDDEN, HEAD_DIM), CMP_HIDDEN ** -0.5),
        'cmp_v_pe': nrm(ks[7], (DEPTH, CMP_LEN, HEAD_DIM), 0.3),
        'cmp_v_w1': nrm(ks[8], (DEPTH, CMP_LEN * HEAD_DIM, CMP_HIDDEN), (CMP_LEN * HEAD_DIM) ** -0.5),
        'cmp_v_w2': nrm(ks[9], (DEPTH, CMP_HIDDEN, HEAD_DIM), CMP_HIDDEN ** -0.5),
        'rel_table': nrm(ks[10], (REL_BUCKETS, N_HEADS), 0.5),
        'conv_w': nrm(ks[11], (DEPTH, CONV_WIDTH, 1, LRU_WIDTH), CONV_WIDTH ** -0.5),
        'conv_b': nrm(ks[12], (DEPTH, LRU_WIDTH), 0.01),
        'lru_wa': nrm(ks[13], (DEPTH, LRU_BLOCKS, LRU_BLOCK_DIM, LRU_BLOCK_DIM), LRU_BLOCK_DIM ** -0.5),
        'lru_ba': nrm(ks[14], (DEPTH, LRU_WIDTH), 0.01),
        'lru_wx': nrm(ks[15], (DEPTH, LRU_BLOCKS, LRU_BLOCK_DIM, LRU_BLOCK_DIM), LRU_BLOCK_DIM ** -0.5),
        'lru_bx': nrm(ks[16], (DEPTH, LRU_WIDTH), 0.01),
        'lru_lambda': lam,
        'grp_norm_attn': gain(ks[18], (DEPTH, ATTN_WIDTH)),
        'grp_norm_lru': gain(ks[19], (DEPTH, LRU_WIDTH)),
        'w_out': nrm(ks[20], (DEPTH, D_MODEL, D_MODEL), D_MODEL ** -0.5),
        'ffn_norm': gain(ks[21], (DEPTH, D_MODEL)),
        'peer_wq': nrm(ks[22], (DEPTH, D_MODEL, PEER_HEADS * PEER_D_KEY), D_MODEL ** -0.5),
        'peer_subkeys': nrm(ks[23], (DEPTH, 2, PEER_N_KEYS, PEER_D_KEY // 2), (PEER_D_KEY // 2) ** -0.5),
        'peer_u': nrm(ks[24], (DEPTH, PEER_EXPERTS, D_MODEL), D_MODEL ** -0.5),
        'peer_v': nrm(ks[25], (DEPTH, PEER_EXPERTS, D_MODEL), 0.3),
        'ple_norm': gain(ks[26], (DEPTH, D_MODEL)),
        'ple_wgate': nrm(ks[27], (DEPTH, D_MODEL, D_MODEL), D_MODEL ** -0.5),
        'ple_bgate': nrm(ks[28], (DEPTH, D_MODEL), 0.01),
        'ple_proj': nrm(ks[29], (DEPTH, PLE_DIM, D_MODEL), PLE_DIM ** -0.5),
        'final_norm': gain(ks[30], (D_MODEL,)),
    }


def reference(x, p, attn_norm, w_in, cmp_k_pe, cmp_k_w1, cmp_k_w2, cmp_v_pe, cmp_v_w1, cmp_v_w2,
              rel_table, conv_w, conv_b, lru_wa, lru_ba, lru_wx, lru_bx, lru_lambda,
              grp_norm_attn, grp_norm_lru, w_out, ffn_norm, peer_wq, peer_subkeys, peer_u, peer_v,
              ple_norm, ple_wgate, ple_bgate, ple_proj, final_norm):
    h = x
    split_at = [int(v) for v in np.cumsum(IN_SPLITS)[:-1]]
    for i in range(DEPTH):
        xn = rmsnorm(h, attn_norm[i])
        proj = xn @ w_in[i]
        q, kc, vc, ks, vs, kw, vw, gates, xr, xg = jnp.split(proj, split_at, axis=-1)
        a_out = nsa_mixer(q, kc, vc, ks, vs, kw, vw, gates, cmp_k_pe[i], cmp_k_w1[i], cmp_k_w2[i],
                          cmp_v_pe[i], cmp_v_w1[i], cmp_v_w2[i], rel_table)
        l_out = rglru_mixer(xr, xg, conv_w[i], conv_b[i], lru_wa[i], lru_ba[i], lru_wx[i], lru_bx[i],
                            lru_lambda[i])
        mixed = jnp.concatenate([rmsnorm(a_out, grp_norm_attn[i]), rmsnorm(l_out, grp_norm_lru[i])], axis=-1)
        h = h + mixed @ w_out[i]
        h = h + peer_ffn(rmsnorm(h, ffn_norm[i]), peer_wq[i], peer_subkeys[i], peer_u[i], peer_v[i])
        gate = jax.nn.sigmoid(rmsnorm(h, ple_norm[i]) @ ple_wgate[i] + ple_bgate[i])
        h = h + gate * (p[i] @ ple_proj[i])
    return rmsnorm(h, final_norm)
```

```python
import numpy as np
from contextlib import ExitStack
import concourse.bass as bass
import concourse.mybir as mybir
from concourse.bass_utils import run_bass_kernel_spmd

F32 = mybir.dt.float32
BF16 = mybir.dt.bfloat16
U32 = mybir.dt.uint32
AF = mybir.ActivationFunctionType
ALU = mybir.AluOpType
AX = mybir.AxisListType

T = 4096
D = 1024
NT = T // 128
TH = 2048
IN_COLS = 2328
EPS = 1e-6
NEGM = -30000.0


class Res:
    __slots__ = ("name", "lw", "rd")

    def __init__(self, name=""):
        self.name = name
        self.lw = None
        self.rd = {}


class KB:
    NDMA = 4

    def __init__(self, nc, stack):
        self.nc = nc
        self.issue = {"pe": nc.tensor, "act": nc.scalar, "dve": nc.vector, "pool": nc.gpsimd,
                      "dsp": nc.sync, "dact": nc.scalar, "dpool": nc.gpsimd}
        self.stream = {"pe": "pe", "act": "act", "dve": "dve", "pool": "pool",
                       "dsp": "sp", "dact": "act", "dpool": "pool"}
        self.sems = {}
        self.cnt = {}
        for q in self.issue:
            n = self.NDMA if self.is_dma(q) else 1
            self.sems[q] = [stack.enter_context(nc.semaphore(f"s_{q}{i}")) for i in range(n)]
            self.cnt[q] = 0
        self.waited = {s: {} for s in ("pe", "act", "dve", "pool", "sp")}
        self.ninst = 0
        self._rr = 0

    @staticmethod
    def is_dma(q):
        return q in ("dsp", "dact", "dpool")

    @staticmethod
    def _need(need, dep):
        if dep is None:
            return
        q, c = dep
        if need.get(q, 0) < c:
            need[q] = c

    def _waits(self, st, eng, need, skip_q=None):
        for dq, c in need.items():
            if dq == "pe" and skip_q == "pe":
                continue
            if self.is_dma(dq):
                n = self.NDMA
                for si in range(n):
                    k = (c - 1 - si) // n + 1 if c - 1 >= si else 0
                    if k <= 0:
                        continue
                    key = (dq, si)
                    if self.waited[st].get(key, 0) >= k:
                        continue
                    eng.wait_ge(self.sems[dq][si], 16 * k)
                    self.waited[st][key] = k
            else:
                key = (dq, 0)
                if self.waited[st].get(key, 0) >= c:
                    continue
                eng.wait_ge(self.sems[dq][0], c)
                self.waited[st][key] = c

    def emit(self, q, fn, reads=(), writes=()):
        need = {}
        for r in reads:
            self._need(need, r.lw)
        for w in writes:
            self._need(need, w.lw)
            for rq, rc in w.rd.items():
                self._need(need, (rq, rc))
        st = self.stream[q]
        self._waits(st, self.issue[q], need, skip_q=q)
        inst = fn()
        self.cnt[q] += 1
        c = self.cnt[q]
        if self.is_dma(q):
            inst.then_inc(self.sems[q][(c - 1) % self.NDMA], 16)
        else:
            inst.then_inc(self.sems[q][0], 1)
        for r in reads:
            if r.rd.get(q, 0) < c:
                r.rd[q] = c
        for w in writes:
            w.lw = (q, c)
            w.rd = {}
        self.ninst += 1
        return inst

    def dmaq(self):
        self._rr ^= 1
        return "dsp" if self._rr else "dact"

    def barrier(self):
        need = {q: c for q, c in self.cnt.items() if c > 0}
        for st, eng in (("pe", self.nc.tensor), ("act", self.nc.scalar), ("dve", self.nc.vector),
                        ("pool", self.nc.gpsimd), ("sp", self.nc.sync)):
            self._waits(st, eng, dict(need))

    def drain_all(self):
        need = {q: c for q, c in self.cnt.items() if c > 0}
        self._waits("sp", self.nc.sync, need)


class Scope:
    def __init__(self, kb):
        self.kb = kb
        self.st = ExitStack()

    def __enter__(self):
        self.st.__enter__()
        return self.st

    def __exit__(self, *a):
        if a[0] is None:
            self.kb.barrier()
        return self.st.__exit__(*a)


class Ring:
    def __init__(self, tiles):
        self.tiles = tiles
        self.res = [Res() for _ in tiles]
        self.i = -1

    def next(self):
        self.i = (self.i + 1) % len(self.tiles)
        return self.tiles[self.i], self.res[self.i]


def build_program(dbg=None, phases=("A", "B", "C", "D")):
    nc = bass.Bass("TRN2", target_bir_lowering=False)

    def din(name, shape, dt=F32):
        return nc.dram_tensor(name, list(shape), dt, kind="ExternalInput").ap()

    dbg = dbg or ()

    def dscr(name, shape, dt):
        kind = "ExternalOutput" if name in dbg else "Internal"
        return nc.dram_tensor(name, list(shape), dt, kind=kind).ap()

    xb = din("xb", [T, D])
    xh = din("xh", [TH, D])
    ph = din("ph", [TH, 256])
    selc = din("selc", [128, 2])
    ident = din("ident", [128, 128])
    w_in = din("w_in", [D, IN_COLS])
    g_attn = din("g_attn", [128, 8])
    out = nc.dram_tensor("out", [TH, D], F32, kind="ExternalOutput").ap()
    lru_cw = din("lru_cw", [128, 4, 4])
    lru_vec = din("lru_vec", [128, 5, 4])
    lru_bda = din("lru_bda", [128, 4, 128])
    lru_bdx = din("lru_bdx", [128, 4, 128])

    qT_s = dscr("qT_s", [512, T], BF16)
    kcT_s = dscr("kcT_s", [128, T], BF16)
    vcT_s = dscr("vcT_s", [128, T], BF16)
    ksT_s = dscr("ksT_s", [128, T], BF16)
    kwT_s = dscr("kwT_s", [128, T], BF16)
    vs_s = dscr("vs_s", [T, 128], BF16)
    vw_s = dscr("vw_s", [T, 128], BF16)
    gates_s = dscr("gates_s", [T, 24], F32)
    xrT_s = dscr("xrT_s", [512, T], F32)
    xgT_s = dscr("xgT_s", [512, T], F32)

    cmp_w1 = din("cmp_w1", [2, 128, 16, 256])
    cmp_pe = din("cmp_pe", [2, 128, 16])
    cmp_w2 = din("cmp_w2", [2, 128, 2, 64])
    ovl_ext = din("ovl_ext", [128, 2, 65])
    bc_g = din("bc_g", [8, 128, 5, 512])
    bc_m = din("bc_m", [128, 5, 512])
    bd_g = din("bd_g", [8, 128, 2, 128])
    bd_m = din("bd_m", [128, 3, 128])
    t31_in = din("t31", [128, 8])
    force_in = din("force_c", [128, 32, 64])
    keep_in = din("keep_c", [128, 32, 64])
    erows = din("erows", [64, T])
    ga_in = din("ga_rep", [128, 512])
    w_out_in = din("w_out", [D, D])
    peer_wq = din("peer_wq", [D, 2048])
    sk_T = din("sk_T", [2, 128, 128])
    peer_u = din("peer_u", [16384, D])
    peer_v = din("peer_v", [16384, D])
    ple_wg = din("ple_wgate", [D, D])
    ple_pj = din("ple_proj", [256, D])
    rep4 = din("rep4", [128, 4, D])
    iota16 = din("iota16", [128, 16])
    mixT_s = dscr("mixT_s", [1024, T], BF16)
    R = {n: Res(n) for n in ("mixT_s", "qT_s", "kcT_s", "vcT_s", "ksT_s", "kwT_s", "vs_s", "vw_s", "gates_s", "xrT_s", "xgT_s")}

    with ExitStack() as top:
        kb = KB(nc, top)
        E = kb.emit

        uniq = [0]

        def sb(st, name, shape, dt):
            uniq[0] += 1
            return st.enter_context(nc.sbuf_tensor(f"sb{uniq[0]}_{name}", list(shape), dt))

        def ps(st, name, shape, dt):
            uniq[0] += 1
            return st.enter_context(nc.psum_tensor(f"ps{uniq[0]}_{name}", list(shape), dt))


        def MM(out_, lhsT, rhs, start, stop, reads, writes):
            return E("pe", lambda: nc.tensor.matmul(out_, lhsT=lhsT, rhs=rhs, start=start, stop=stop), reads, writes)

        def TR(out_, in_, idt, reads, writes):
            return E("pe", lambda: nc.tensor.transpose(out=out_, in_=in_, identity=idt), reads, writes)

        def ACTF(out_, in_, func, reads, writes, **kw):
            return E("act", lambda: nc.scalar.activation(out=out_, in_=in_, func=func, **kw), reads, writes)

        def veng(q):
            return nc.vector if q == "dve" else nc.gpsimd

        def TS(q, out_, in0, s1, s2, op0, op1, reads, writes):
            if op1 is None:
                return E(q, lambda: veng(q).tensor_scalar(out=out_, in0=in0, scalar1=s1, scalar2=None, op0=op0), reads, writes)
            return E(q, lambda: veng(q).tensor_scalar(out=out_, in0=in0, scalar1=s1, scalar2=s2, op0=op0, op1=op1), reads, writes)

        def TT(q, out_, in0, in1, op, reads, writes):
            return E(q, lambda: veng(q).tensor_tensor(out=out_, in0=in0, in1=in1, op=op), reads, writes)

        def STT(out_, in0, scalar, in1, op0, op1, reads, writes, **kw):
            return E("dve", lambda: nc.vector.scalar_tensor_tensor(out=out_, in0=in0, scalar=scalar, in1=in1, op0=op0, op1=op1, **kw), reads, writes)

        def CP(q, out_, in_, reads, writes):
            if q == "act":
                return E("act", lambda: nc.scalar.copy(out=out_, in_=in_), reads, writes)
            return E(q, lambda: veng(q).tensor_copy(out=out_, in_=in_), reads, writes)

        def MSET(q, out_, val, writes):
            return E(q, lambda: veng(q).memset(out_, val), (), writes)

        def DMA(q, out_, in_, reads, writes):
            eng = {"dsp": nc.sync, "dact": nc.scalar, "dpool": nc.gpsimd}[q]
            return E(q, lambda: eng.dma_start(out=out_, in_=in_), reads, writes)

        def dump(name, ap, shape, dt, res):
            if name not in dbg:
                return
            d = nc.dram_tensor(name, list(shape), dt, kind="ExternalOutput").ap()
            DMA("dsp", d, ap, [res] if not isinstance(res, list) else res, [])

        ident_f = sb(top, "ident_f", [128, 128], F32); r_identf = Res()
        ident_b = sb(top, "ident_b", [128, 128], BF16); r_identb = Res()
        E("dsp", lambda: nc.sync.dma_start(out=ident_f[:], in_=ident), writes=[r_identf])
        E("dve", lambda: nc.vector.tensor_copy(out=ident_b[:], in_=ident_f[:]), reads=[r_identf], writes=[r_identb])

        if "A" in phases:
            with Scope(kb) as st:
                Wg = sb(st, "Wg", [128, 8, IN_COLS], BF16); r_Wg = Res()
                gcol = sb(st, "gcol", [128, 8], F32); r_gcol = Res()
                wst = Ring([sb(st, f"wst{i}", [128, IN_COLS], F32) for i in range(2)])
                E("dsp", lambda: nc.sync.dma_start(out=gcol[:], in_=g_attn), writes=[r_gcol])
                for dc in range(8):
                    w_t, w_r = wst.next()
                    E("dsp" if dc % 2 == 0 else "dact",
                      (lambda w_t=w_t, dc=dc: nc.sync.dma_start(out=w_t[:], in_=w_in[dc * 128:(dc + 1) * 128, :])) if dc % 2 == 0 else
                      (lambda w_t=w_t, dc=dc: nc.scalar.dma_start(out=w_t[:], in_=w_in[dc * 128:(dc + 1) * 128, :])),
                      writes=[w_r])
                    eng = "dve" if dc % 2 == 0 else "pool"
                    ve = nc.vector if dc % 2 == 0 else nc.gpsimd
                    E(eng, lambda ve=ve, w_t=w_t, dc=dc: ve.tensor_scalar(out=Wg[:, dc, :], in0=w_t[:], scalar1=gcol[:, dc:dc + 1], scalar2=None, op0=ALU.mult),
                      reads=[w_r, r_gcol], writes=[r_Wg])

                xt_ring = Ring([sb(st, f"xt{i}", [128, 4, D], F32) for i in range(2)])
                xnb_ring = Ring([sb(st, f"xnb{i}", [128, 4, D], BF16) for i in range(2)])
                xnT_ring = Ring([sb(st, f"xnT{i}", [128, 8, 512], BF16) for i in range(2)])
                junk = sb(st, "junkA", [128, D], BF16); r_junk = Res()
                ss_ring = Ring([sb(st, f"ss{i}", [128, 8], F32) for i in range(2)])
                pT_ring = Ring([ps(st, f"pT{i}", [128, 512], BF16) for i in range(2)])
                pacc = Ring([ps(st, f"pacc{i}", [128, 512], F32) for i in range(4)])
                ostf = Ring([sb(st, f"ostf{i}", [128, 512], F32) for i in range(3)])
                ostb = Ring([sb(st, f"ostb{i}", [128, 512], BF16) for i in range(3)])
                osv = Ring([sb(st, f"osv{i}", [128, 256], BF16) for i in range(2)])
                osg = Ring([sb(st, f"osg{i}", [128, 24], F32) for i in range(2)])
                xb_v = xb.rearrange("(n p) d -> p n d", p=128)
                fm = []
                for cc in range(4):
                    fm.append((cc * 128, qT_s[cc * 128:(cc + 1) * 128, :], 0.125, True, R["qT_s"]))
                fm.append((512, kcT_s, 1.0, True, R["kcT_s"]))
                fm.append((640, vcT_s, 1.0, True, R["vcT_s"]))
                fm.append((768, ksT_s, 1.0, True, R["ksT_s"]))
                fm.append((1024, kwT_s, 1.0, True, R["kwT_s"]))
                for cc in range(4):
                    fm.append((1304 + cc * 128, xrT_s[cc * 128:(cc + 1) * 128, :], 1.0, False, R["xrT_s"]))
                for cc in range(4):
                    fm.append((1816 + cc * 128, xgT_s[cc * 128:(cc + 1) * 128, :], 1.0, False, R["xgT_s"]))
                ev = 0
                for tcn in range(8):
                    xt, xt_r = xt_ring.next()
                    E("dsp", lambda xt=xt, tcn=tcn: nc.sync.dma_start(out=xt[:], in_=xb_v[:, tcn * 4:(tcn + 1) * 4, :]), writes=[xt_r])
                    ss, ss_r = ss_ring.next()
                    for n in range(4):
                        E("act", lambda xt=xt, ss=ss, n=n: nc.scalar.activation(out=junk[:], in_=xt[:, n, :], func=AF.Square, accum_out=ss[:, n:n + 1]),
                          reads=[xt_r], writes=[r_junk, ss_r])
                    E("dve", lambda ss=ss: nc.vector.tensor_scalar(out=ss[:, 4:8], in0=ss[:, 0:4], scalar1=1.0 / D, scalar2=EPS, op0=ALU.mult, op1=ALU.add), reads=[ss_r], writes=[ss_r])
                    E("act", lambda ss=ss: nc.scalar.activation(out=ss[:, 4:8], in_=ss[:, 4:8], func=AF.Sqrt), reads=[ss_r], writes=[ss_r])
                    E("dve", lambda ss=ss: nc.vector.reciprocal(out=ss[:, 4:8], in_=ss[:, 4:8]), reads=[ss_r], writes=[ss_r])
                    xnb, xnb_r = xnb_ring.next()
                    for n in range(4):
                        if n % 2 == 0:
                            E("dve", lambda xt=xt, xnb=xnb, ss=ss, n=n: nc.vector.tensor_scalar(out=xnb[:, n, :], in0=xt[:, n, :], scalar1=ss[:, 4 + n:5 + n], scalar2=None, op0=ALU.mult),
                              reads=[xt_r, ss_r], writes=[xnb_r])
                        else:
                            E("pool", lambda xt=xt, xnb=xnb, ss=ss, n=n: nc.gpsimd.tensor_scalar(out=xnb[:, n, :], in0=xt[:, n, :], scalar1=ss[:, 4 + n:5 + n], scalar2=None, op0=ALU.mult),
                              reads=[xt_r, ss_r], writes=[xnb_r])
                    xnT, xnT_r = xnT_ring.next()
                    for dc in range(8):
                        pT, pT_r = pT_ring.next()
                        for n in range(4):
                            E("pe", lambda pT=pT, xnb=xnb, n=n, dc=dc: nc.tensor.transpose(out=pT[:, n * 128:(n + 1) * 128], in_=xnb[:, n, dc * 128:(dc + 1) * 128], identity=ident_b[:]),
                              reads=[xnb_r, r_identb], writes=[pT_r])
                        if dc % 2 == 0:
                            E("act", lambda pT=pT, xnT=xnT, dc=dc: nc.scalar.copy(out=xnT[:, dc, :], in_=pT[:]), reads=[pT_r], writes=[xnT_r])
                        else:
                            E("dve", lambda pT=pT, xnT=xnT, dc=dc: nc.vector.tensor_copy(out=xnT[:, dc, :], in_=pT[:]), reads=[pT_r], writes=[xnT_r])
                    for (c0, dst, scale, isb, dres) in fm:
                        pa, pa_r = pacc.next()
                        for dc in range(8):
                            E("pe", lambda pa=pa, dc=dc, c0=c0, xnT=xnT: nc.tensor.matmul(pa[:], lhsT=Wg[:, dc, c0:c0 + 128], rhs=xnT[:, dc, :], start=(dc == 0), stop=(dc == 7)),
                              reads=[r_Wg, xnT_r], writes=[pa_r])
                        o_t, o_r = (ostb if isb else ostf).next()
                        ev += 1
                        if ev % 2 == 0:
                            E("act", lambda o_t=o_t, pa=pa, scale=scale: nc.scalar.activation(out=o_t[:], in_=pa[:], func=AF.Copy, scale=scale), reads=[pa_r], writes=[o_r])
                        else:
                            E("dve", lambda o_t=o_t, pa=pa, scale=scale: nc.vector.tensor_scalar(out=o_t[:], in0=pa[:], scalar1=scale, scalar2=None, op0=ALU.mult), reads=[pa_r], writes=[o_r])
                        if ev % 2 == 0:
                            E("dsp", lambda o_t=o_t, dst=dst, tcn=tcn: nc.sync.dma_start(out=dst[:, tcn * 512:(tcn + 1) * 512], in_=o_t[:]), reads=[o_r], writes=[dres])
                        else:
                            E("dpool", lambda o_t=o_t, dst=dst, tcn=tcn: nc.gpsimd.dma_start(out=dst[:, tcn * 512:(tcn + 1) * 512], in_=o_t[:]), reads=[o_r], writes=[dres])
                    for n in range(4):
                        t0 = tcn * 512 + n * 128
                        pa, pa_r = pacc.next()
                        for dc in range(8):
                            E("pe", lambda pa=pa, dc=dc, xnT=xnT, n=n: nc.tensor.matmul(pa[:, 0:128], lhsT=xnT[:, dc, n * 128:(n + 1) * 128], rhs=Wg[:, dc, 896:1024], start=(dc == 0), stop=(dc == 7)),
                              reads=[r_Wg, xnT_r], writes=[pa_r])
                        pb, pb_r = pacc.next()
                        for dc in range(8):
                            E("pe", lambda pb=pb, dc=dc, xnT=xnT, n=n: nc.tensor.matmul(pb[:, 0:152], lhsT=xnT[:, dc, n * 128:(n + 1) * 128], rhs=Wg[:, dc, 1152:1304], start=(dc == 0), stop=(dc == 7)),
                              reads=[r_Wg, xnT_r], writes=[pb_r])
                        ov, ov_r = osv.next()
                        og, og_r = osg.next()
                        E("act", lambda ov=ov, pa=pa: nc.scalar.copy(out=ov[:, 0:128], in_=pa[:, 0:128]), reads=[pa_r], writes=[ov_r])
                        E("dve", lambda ov=ov, pb=pb: nc.vector.tensor_copy(out=ov[:, 128:256], in_=pb[:, 0:128]), reads=[pb_r], writes=[ov_r])
                        E("dve", lambda og=og, pb=pb: nc.vector.tensor_copy(out=og[:], in_=pb[:, 128:152]), reads=[pb_r], writes=[og_r])
                        E("dsp", lambda ov=ov, t0=t0: nc.sync.dma_start(out=vs_s[t0:t0 + 128, :], in_=ov[:, 0:128]), reads=[ov_r], writes=[R["vs_s"]])
                        E("dpool", lambda ov=ov, t0=t0: nc.gpsimd.dma_start(out=vw_s[t0:t0 + 128, :], in_=ov[:, 128:256]), reads=[ov_r], writes=[R["vw_s"]])
                        E("dsp", lambda og=og, t0=t0: nc.sync.dma_start(out=gates_s[t0:t0 + 128, :], in_=og[:]), reads=[og_r], writes=[R["gates_s"]])

        if "B" in phases:
            with Scope(kb) as st:
                cw = sb(st, "cw", [128, 4, 4], F32); r_cw = Res()
                lv = sb(st, "lv", [128, 5, 4], F32); r_lv = Res()
                clc = sb(st, "clc", [128, 3, 4], F32); r_clc = Res()
                bdf = sb(st, "bdf", [128, 2, 4, 128], F32); r_bdf = Res()
                bdb = sb(st, "bdb", [128, 2, 4, 128], BF16); r_bdb = Res()
                ones_b = sb(st, "ones_b", [128, 128], BF16); r_ones = Res()
                E("dsp", lambda: nc.sync.dma_start(out=cw[:], in_=lru_cw), writes=[r_cw])
                E("dact", lambda: nc.scalar.dma_start(out=lv[:], in_=lru_vec), writes=[r_lv])
                E("dsp", lambda: nc.sync.dma_start(out=bdf[:, 0], in_=lru_bda), writes=[r_bdf])
                E("dact", lambda: nc.scalar.dma_start(out=bdf[:, 1], in_=lru_bdx), writes=[r_bdf])
                E("dve", lambda: nc.vector.tensor_copy(out=bdb[:], in_=bdf[:]), reads=[r_bdf], writes=[r_bdb])
                E("dve", lambda: nc.vector.memset(ones_b[:], 1.0), writes=[r_ones])
                E("act", lambda: nc.scalar.activation(out=clc[:, 0, :], in_=lv[:, 3, :], func=AF.Exp, scale=-1.0), reads=[r_lv], writes=[r_clc])
                E("act", lambda: nc.scalar.activation(out=clc[:, 0, :], in_=clc[:, 0, :], func=AF.Ln, bias=1.0), reads=[r_clc], writes=[r_clc])
                E("dve", lambda: nc.vector.tensor_scalar(out=clc[:, 1, :], in0=clc[:, 0, :], scalar1=-8.0, scalar2=None, op0=ALU.mult), reads=[r_clc], writes=[r_clc])
                E("dve", lambda: nc.vector.tensor_scalar(out=clc[:, 2, :], in0=clc[:, 0, :], scalar1=-16.0, scalar2=None, op0=ALU.mult), reads=[r_clc], writes=[r_clc])
                L = sb(st, "Lall", [128, 4, T], F32); r_L = Res()
                X = [sb(st, f"lruX{i}", [128, T], F32) for i in range(5)]
                rX = [Res() for _ in range(5)]
                xcb = sb(st, "xcb", [128, T], BF16); r_xcb = Res()
                pg = Ring([ps(st, f"pg{i}", [128, 512], F32) for i in range(4)])
                for cc in range(4):
                    X1, X2, X3, X4, X5 = X
                    r1, r2, r3, r4, r5 = rX
                    for hh in range(2):
                        E("dsp", lambda cc=cc, hh=hh: nc.sync.dma_start(out=X1[:, hh * 2048:(hh + 1) * 2048], in_=xrT_s[cc * 128:(cc + 1) * 128, hh * 2048:(hh + 1) * 2048]), reads=[R["xrT_s"]], writes=[r1])
                        E("dact", lambda cc=cc, hh=hh: nc.scalar.dma_start(out=X3[:, hh * 2048:(hh + 1) * 2048], in_=xgT_s[cc * 128:(cc + 1) * 128, hh * 2048:(hh + 1) * 2048]), reads=[R["xgT_s"]], writes=[r3])
                    E("dve", lambda cc=cc: nc.vector.tensor_scalar(out=X2[:], in0=X1[:], scalar1=cw[:, cc, 3:4], scalar2=lv[:, 0, cc:cc + 1], op0=ALU.mult, op1=ALU.add), reads=[r1, r_cw, r_lv], writes=[r2])
                    for sh in (1, 2, 3):
                        E("dve", lambda cc=cc, sh=sh: nc.vector.scalar_tensor_tensor(out=X2[:, sh:T], in0=X1[:, 0:T - sh], scalar=cw[:, cc, 3 - sh:4 - sh], in1=X2[:, sh:T], op0=ALU.mult, op1=ALU.add), reads=[r1, r2, r_cw], writes=[r2])
                    E("pool", lambda: nc.gpsimd.tensor_copy(out=xcb[:], in_=X2[:]), reads=[r2], writes=[r_xcb])
                    for gi, (Xo, ro, bi) in enumerate(((X4, r4, 1), (X5, r5, 2))):
                        for tcn in range(8):
                            pgt, pg_r = pg.next()
                            E("pe", lambda pgt=pgt, gi=gi, cc=cc, tcn=tcn: nc.tensor.matmul(pgt[:], lhsT=bdb[:, gi, cc, :], rhs=xcb[:, tcn * 512:(tcn + 1) * 512], start=True, stop=True), reads=[r_bdb, r_xcb], writes=[pg_r])
                            E("act", lambda pgt=pgt, Xo=Xo, bi=bi, cc=cc, tcn=tcn: nc.scalar.activation(out=Xo[:, tcn * 512:(tcn + 1) * 512], in_=pgt[:], func=AF.Sigmoid, bias=lv[:, bi, cc:cc + 1]), reads=[pg_r, r_lv], writes=[ro])
                    E("act", lambda cc=cc: nc.scalar.activation(out=X1[:], in_=X4[:], func=AF.Exp, scale=clc[:, 1, cc:cc + 1]), reads=[r4, r_clc], writes=[r1])
                    E("act", lambda cc=cc: nc.scalar.activation(out=X4[:], in_=X4[:], func=AF.Exp, scale=clc[:, 2, cc:cc + 1]), reads=[r4, r_clc], writes=[r4])
                    E("act", lambda: nc.scalar.activation(out=X4[:], in_=X4[:], func=AF.Sqrt, scale=-1.0, bias=1.0), reads=[r4], writes=[r4])
                    E("pool", lambda: nc.gpsimd.tensor_tensor(out=X5[:], in0=X5[:], in1=X2[:], op=ALU.mult), reads=[r5, r2], writes=[r5])
                    E("dve", lambda: nc.vector.tensor_tensor(out=X4[:], in0=X4[:], in1=X5[:], op=ALU.mult), reads=[r4, r5], writes=[r4])
                    E("dve", lambda: nc.vector.tensor_tensor_scan(out=X2[:], data0=X1[:], data1=X4[:], initial=0.0, op0=ALU.mult, op1=ALU.add), reads=[r1, r4], writes=[r2])
                    E("act", lambda: nc.scalar.activation(out=X3[:], in_=X3[:], func=AF.Gelu_apprx_tanh), reads=[r3], writes=[r3])
                    E("pool", lambda cc=cc: nc.gpsimd.tensor_tensor(out=L[:, cc, :], in0=X2[:], in1=X3[:], op=ALU.mult), reads=[r2, r3], writes=[r_L])
                sq = Ring([sb(st, f"lsq{i}", [128, 512], BF16) for i in range(2)])
                rs_ring = Ring([sb(st, f"lrs{i}", [128, 512], F32) for i in range(2)])
                lo = Ring([sb(st, f"lo{i}", [128, 512], BF16) for i in range(3)])
                for tcn in range(8):
                    pgt, pg_r = pg.next()
                    for cc in range(4):
                        sq_t, sq_r = sq.next()
                        E("act", lambda sq_t=sq_t, cc=cc, tcn=tcn: nc.scalar.activation(out=sq_t[:], in_=L[:, cc, tcn * 512:(tcn + 1) * 512], func=AF.Square), reads=[r_L], writes=[sq_r])
                        E("pe", lambda pgt=pgt, sq_t=sq_t, cc=cc: nc.tensor.matmul(pgt[:], lhsT=ones_b[:], rhs=sq_t[:], start=(cc == 0), stop=(cc == 3)), reads=[r_ones, sq_r], writes=[pg_r])
                    rs_t, rs_r = rs_ring.next()
                    E("dve", lambda rs_t=rs_t, pgt=pgt: nc.vector.tensor_scalar(out=rs_t[:], in0=pgt[:], scalar1=1.0 / 512, scalar2=EPS, op0=ALU.mult, op1=ALU.add), reads=[pg_r], writes=[rs_r])
                    E("act", lambda rs_t=rs_t: nc.scalar.activation(out=rs_t[:], in_=rs_t[:], func=AF.Sqrt), reads=[rs_r], writes=[rs_r])
                    E("dve", lambda rs_t=rs_t: nc.vector.reciprocal(out=rs_t[:], in_=rs_t[:]), reads=[rs_r], writes=[rs_r])
                    for cc in range(4):
                        lo_t, lo_r = lo.next()
                        E("dve", lambda lo_t=lo_t, rs_t=rs_t, cc=cc, tcn=tcn: nc.vector.scalar_tensor_tensor(out=lo_t[:], in0=L[:, cc, tcn * 512:(tcn + 1) * 512], scalar=lv[:, 4, cc:cc + 1], in1=rs_t[:], op0=ALU.mult, op1=ALU.mult), reads=[r_L, rs_r, r_lv], writes=[lo_r])
                        E("dsp", lambda lo_t=lo_t, cc=cc, tcn=tcn: nc.sync.dma_start(out=mixT_s[512 + cc * 128:512 + (cc + 1) * 128, tcn * 512:(tcn + 1) * 512], in_=lo_t[:]), reads=[lo_r], writes=[R["mixT_s"]])

        if "C" in phases:
            with Scope(kb) as st:
                Aout = sb(st, "Aout", [128, NT, 512], BF16)
                rA = [Res() for _ in range(NT)]
                sig = sb(st, "sig", [128, NT, 24], F32); r_sig = Res()
                force_t = sb(st, "force_t", [128, NT, 64], F32); r_force = Res()
                keep_t = sb(st, "keep_t", [128, NT, 64], F32); r_keep = Res()
                t31 = sb(st, "t31", [128, 8], F32); r_t31 = Res()
                BD = sb(st, "BD", [128, 8, 3, 128], BF16); r_BD = Res()
                ovl_t = sb(st, "ovl_t", [128, 2, 65], F32); r_ovl = Res()
                ga_t = sb(st, "ga_t", [128, 512], F32); r_ga = Res()
                bcm = sb(st, "bcm", [128, 5, 512], F32); r_bcm = Res()
                DMA("dsp", sig[:], gates_s.rearrange("(n p) c -> p n c", p=128), [R["gates_s"]], [r_sig])
                ACTF(sig[:], sig[:], AF.Sigmoid, [r_sig], [r_sig])
                DMA("dact", force_t[:], force_in, [], [r_force])
                DMA("dsp", keep_t[:], keep_in, [], [r_keep])
                DMA("dact", t31[:], t31_in, [], [r_t31])
                DMA("dsp", ovl_t[:], ovl_ext, [], [r_ovl])
                DMA("dact", ga_t[:], ga_in, [], [r_ga])
                DMA("dsp", bcm[:], bc_m, [], [r_bcm])
                psb = [ps(st, f"pC{i}", [128, 512], F32) for i in range(8)]
                pS = Ring(psb[0:2])
                pO = psb[2:6]; r_pO = [Res() for _ in range(4)]
                pX = Ring(psb[6:8])
                with Scope(kb) as st2:
                    bdg = sb(st2, "bdg", [128, 8, 2, 128], F32); r_bdg = Res()
                    bdm = sb(st2, "bdm", [128, 3, 128], F32); r_bdm = Res()
                    DMA("dsp", bdg[:], bd_g.rearrange("h p j t -> p h j t"), [], [r_bdg])
                    DMA("dact", bdm[:], bd_m, [], [r_bdm])
                    for hg in range(8):
                        for j in range(2):
                            STT(BD[:, hg, j, :], bdg[:, hg, j, :], t31[:, hg:hg + 1], bdm[:, j, :], ALU.subtract, ALU.add, [r_bdg, r_bdm, r_t31], [r_BD])
                        CP("dve", BD[:, hg, 2, :], bdm[:, 2, :], [r_bdm], [r_BD])
                P_ring = Ring([sb(st, f"Pt{i}", [128, 512], BF16) for i in range(3)])
                sm = Ring([sb(st, f"smC{i}", [128, 8], F32) for i in range(8)])

                def finish_tile(po, po_r, ncol, i, hg, br, first, imp=None):
                    s_t, s_r = sm.next()
                    TS("dve", s_t[:, 0:1], po[:, ncol:ncol + 1], 1e-30, None, ALU.max, None, [po_r], [s_r])
                    E("dve", lambda: nc.vector.reciprocal(out=s_t[:, 1:2], in_=s_t[:, 0:1]), [s_r], [s_r])
                    TT("dve", s_t[:, 2:3], s_t[:, 1:2], sig[:, i, hg * 3 + br:hg * 3 + br + 1], ALU.mult, [s_r, r_sig], [s_r])
                    dst = Aout[:, i, hg * 64:(hg + 1) * 64]
                    if first:
                        TS("dve", dst, po[:, 0:64], s_t[:, 2:3], None, ALU.mult, None, [po_r, s_r], [rA[i]])
                    else:
                        STT(dst, po[:, 0:64], s_t[:, 2:3], dst, ALU.mult, ALU.add, [po_r, s_r, rA[i]], [rA[i]])
                    if imp is not None:
                        imp_t, imp_r, imp_first = imp
                        if imp_first:
                            TS("dve", imp_t, po[:, 64:128], s_t[:, 1:2], None, ALU.mult, None, [po_r, s_r], [imp_r])
                        else:
                            STT(imp_t, po[:, 64:128], s_t[:, 1:2], imp_t, ALU.mult, ALU.add, [po_r, s_r, imp_r], [imp_r])

                for k in range(2):
                    with Scope(kb) as stg:
                        KcmpT = sb(stg, "KcmpT", [64, 256], BF16); r_Kc = Res()
                        Vco = sb(stg, "Vco", [128, 2, 129], BF16); r_Vco = Res()
                        with Scope(kb) as stc:
                            w1s = Ring([sb(stc, f"w1s{i}", [128, 8, 256], F32) for i in range(2)])
                            w1b = sb(stc, "w1b", [128, 2, 16, 256], BF16); r_w1b = Res()
                            pes = sb(stc, "pes", [128, 2, 16], F32); r_pes = Res()
                            peb = sb(stc, "peb", [128, 2, 16], BF16); r_peb = Res()
                            w2s = sb(stc, "w2s", [128, 2, 2, 64], F32); r_w2s = Res()
                            w2b = sb(stc, "w2b", [128, 2, 2, 64], BF16); r_w2b = Res()
                            stk = sb(stc, "stk", [128, 2, T], BF16); r_stk = Res()
                            hb = sb(stc, "hb", [128, 4], F32); r_hb = Res()
                            gh = sb(stc, "gh", [128, 2, 2, 256], BF16); r_gh = Res()
                            for kv in range(2):
                                for hh in range(2):
                                    w_t, w_r = w1s.next()
                                    DMA("dsp" if hh == 0 else "dact", w_t[:], cmp_w1[kv, :, hh * 8:(hh + 1) * 8, :], [], [w_r])
                                    CP("pool" if hh == 0 else "dve", w1b[:, kv, hh * 8:(hh + 1) * 8, :], w_t[:], [w_r], [r_w1b])
                                DMA("dsp", pes[:, kv, :], cmp_pe[kv], [], [r_pes])
                                DMA("dact", w2s[:, kv], cmp_w2[kv], [], [r_w2s])
                                src = kcT_s if kv == 0 else vcT_s
                                sres = R["kcT_s"] if kv == 0 else R["vcT_s"]
                                DMA("dsp", stk[0:64, kv, :], src[k * 64:(k + 1) * 64, :], [sres], [r_stk])
                                MSET("pool", stk[64:128, kv, T - 1:T], 0.0, [r_stk])
                                DMA("dact", stk[64:128, kv, 0:T - 1], src[k * 64:(k + 1) * 64, 1:T], [sres], [r_stk])
                            CP("dve", peb[:], pes[:], [r_pes], [r_peb])
                            CP("dve", w2b[:], w2s[:], [r_w2s], [r_w2b])
                            MSET("pool", gh[:], 0.0, [r_gh])
                            for kv in range(2):
                                for hh in range(2):
                                    px, px_r = pX.next()
                                    for m in range(16):
                                        MM(px[:, 0:1], w1b[:, kv, m, hh * 128:(hh + 1) * 128], peb[:, kv, m:m + 1], m == 0, m == 15, [r_w1b, r_peb], [px_r])
                                    CP("dve", hb[:, kv * 2 + hh:kv * 2 + hh + 1], px[:, 0:1], [px_r], [r_hb])
                                    p_s, p_r = pS.next()
                                    for m in range(16):
                                        MM(p_s[:, 0:255], w1b[:, kv, m, hh * 128:(hh + 1) * 128], stk[:, kv, 2 * m:2 * m + 16 * 254 + 1:16], m == 0, m == 15, [r_w1b, r_stk], [p_r])
                                    ACTF(gh[:, kv, hh, 0:255], p_s[:, 0:255], AF.Gelu_apprx_tanh, [p_r, r_hb], [r_gh], bias=hb[:, kv * 2 + hh:kv * 2 + hh + 1])
                            px, px_r = pX.next()
                            for hh in range(2):
                                MM(px[0:64, 0:256], w2b[:, 0, hh, :], gh[:, 0, hh, :], hh == 0, hh == 1, [r_w2b, r_gh], [px_r])
                            CP("dve", KcmpT[:], px[0:64, 0:256], [px_r], [r_Kc])
                            for ct in range(2):
                                px, px_r = pX.next()
                                for hh in range(2):
                                    MM(px[:, 0:64], gh[:, 1, hh, ct * 128:(ct + 1) * 128], w2b[:, 1, hh, :], hh == 0, hh == 1, [r_gh, r_w2b], [px_r])
                                CP("dve", Vco[:, ct, 0:64], px[:, 0:64], [px_r], [r_Vco])
                            CP("pool", Vco[:, :, 64:129], ovl_t[:], [r_ovl], [r_Vco])
                            if k == 0:
                                dump("d_kcmp", KcmpT[:], [64, 256], BF16, r_Kc)
                                dump("d_vco", Vco[:], [128, 2, 129], BF16, r_Vco)
                                dump("d_hb", hb[:], [128, 4], F32, r_hb)
                                dump("d_gh", gh[:], [128, 2, 2, 256], BF16, r_gh)

                        QT = sb(stg, "QT", [128, 4, T], BF16)
                        r_QT = [Res() for _ in range(4)]
                        r_QM = [[Res() for _ in range(NT)] for _ in range(4)]
                        KsT = sb(stg, "KsT", [128, T], BF16); r_KsT = Res()
                        KwT = sb(stg, "KwT", [64, T], BF16); r_KwT = Res()
                        Vs = sb(stg, "Vs", [128, NT, 65], BF16); r_Vs = Res()
                        Vw = sb(stg, "Vw", [128, NT, 65], BF16); r_Vw = Res()
                        imp_acc = sb(stg, "imp_acc", [128, NT, 64], F32)
                        r_imp = [Res() for _ in range(NT)]
                        for g in range(4):
                            hg = 4 * k + g
                            DMA("dsp" if g % 2 == 0 else "dact", QT[0:64, g, :], qT_s[hg * 64:(hg + 1) * 64, :], [R["qT_s"]], [r_QT[g]])
                        DMA("dsp", KsT[0:64, :], ksT_s[k * 64:(k + 1) * 64, :], [R["ksT_s"]], [r_KsT])
                        with Scope(kb) as ste:
                            ers = sb(ste, "ers", [128, T], F32); r_ers = Res()
                            DMA("dact", ers[64:128, :], erows, [], [r_ers])
                            CP("pool", KsT[64:128, :], ers[64:128, :], [r_ers], [r_KsT])
                        DMA("dact", KwT[:], kwT_s[k * 64:(k + 1) * 64, :], [R["kwT_s"]], [r_KwT])
                        DMA("dsp", Vs[:, :, 0:64], vs_s.rearrange("(n p) c -> p n c", p=128)[:, :, k * 64:(k + 1) * 64], [R["vs_s"]], [r_Vs])
                        DMA("dact", Vw[:, :, 0:64], vw_s.rearrange("(n p) c -> p n c", p=128)[:, :, k * 64:(k + 1) * 64], [R["vw_s"]], [r_Vw])
                        MSET("pool", Vs[:, :, 64:65], 1.0, [r_Vs])
                        MSET("pool", Vw[:, :, 64:65], 1.0, [r_Vw])

                        bcs = Ring([sb(stg, f"bcs{i}", [128, 5, 512], F32) for i in range(2)])
                        BC = Ring([sb(stg, f"BCb{i}", [128, 5, 512], BF16) for i in range(2)])
                        for g in range(4):
                            hg = 4 * k + g
                            bs_t, bs_r = bcs.next()
                            DMA("dsp", bs_t[:, 0:3], bc_g[hg, :, 0:3], [], [bs_r])
                            DMA("dact", bs_t[:, 3:5], bc_g[hg, :, 3:5], [], [bs_r])
                            bc_t, bc_r = BC.next()
                            for m in range(5):
                                STT(bc_t[:, m, :], bs_t[:, m, :], t31[:, hg:hg + 1], bcm[:, m, :], ALU.subtract, ALU.add, [bs_r, r_bcm, r_t31], [bc_r])
                            for tcn in range(8):
                                cts = [0] if tcn < 4 else [0, 1]
                                for ct in cts:
                                    mp = tcn - 4 * ct
                                    p_s, p_r = pS.next()
                                    MM(p_s[:], KcmpT[:, ct * 128:(ct + 1) * 128], QT[0:64, g, tcn * 512:(tcn + 1) * 512], True, mp >= 5, [r_Kc, r_QT[g]], [p_r])
                                    if mp < 5:
                                        MM(p_s[:], ident_b[:], bc_t[:, mp, :], False, True, [r_identb, bc_r], [p_r])
                                    P_t, P_r = P_ring.next()
                                    ACTF(P_t[:], p_s[:], AF.Exp, [p_r, r_t31], [P_r], bias=t31[:, hg:hg + 1])
                                    for q in range(4):
                                        MM(pO[q][:, 0:129], P_t[:, q * 128:(q + 1) * 128], Vco[:, ct, :], ct == 0, ct == cts[-1], [P_r, r_Vco], [r_pO[q]])
                                for q in range(4):
                                    i = 4 * tcn + q
                                    finish_tile(pO[q], r_pO[q], 128, i, hg, 0, True, imp=(imp_acc[:, i, :], r_imp[i], g == 0))

                        if k == 0:
                            dump("d_imp", imp_acc[:], [128, NT, 64], F32, r_imp)
                            dump("d_aout_c", Aout[:], [128, NT, 512], BF16, rA)
                        MBr = Ring([sb(stg, f"MB{i}", [128, 128], F32) for i in range(2)])
                        for (mb_t, mb_r) in zip(MBr.tiles, MBr.res):
                            MSET("dve", mb_t[:], 0.0, [mb_r])
                        tk = Ring([sb(stg, f"tk{i}", [128, 2, 64], F32) for i in range(2)])
                        mxr = Ring([sb(stg, f"mx{i}", [128, 16], F32) for i in range(2)])
                        mtr = Ring([sb(stg, f"mtr{i}", [128, 128], BF16) for i in range(2)])
                        for i in range(NT):
                            tk_t, tk_r = tk.next()
                            mx_t, mx_r = mxr.next()
                            TT("dve", tk_t[:, 0, :], imp_acc[:, i, :], keep_t[:, i, :], ALU.mult, [r_imp[i], r_keep], [tk_r])
                            TT("dve", tk_t[:, 0, :], tk_t[:, 0, :], force_t[:, i, :], ALU.add, [tk_r, r_force], [tk_r])
                            E("dve", lambda: nc.vector.max(out=mx_t[:, 0:8], in_=tk_t[:, 0, :]), [tk_r], [mx_r])
                            E("dve", lambda: nc.vector.match_replace(out=tk_t[:, 1, :], in_to_replace=mx_t[:, 0:8], in_values=tk_t[:, 0, :], imm_value=-1e30), [tk_r, mx_r], [tk_r])
                            E("dve", lambda: nc.vector.max(out=mx_t[:, 8:16], in_=tk_t[:, 1, :]), [tk_r], [mx_r])
                            mb_t, mb_r = MBr.next()
                            TS("dve", mb_t[:, 64:128], tk_t[:, 0, :], mx_t[:, 15:16], None, ALU.is_ge, None, [tk_r, mx_r], [mb_r])
                            TS("dve", mb_t[:, 64:128], mb_t[:, 64:128], 1.0, -NEGM, ALU.subtract, ALU.mult, [mb_r], [mb_r])
                            px, px_r = pX.next()
                            TR(px[:, 0:128], mb_t[:], ident_f[:], [mb_r, r_identf], [px_r])
                            mt_t, mt_r = mtr.next()
                            CP("act", mt_t[64:128, :], px[64:128, 0:128], [px_r], [mt_r])
                            for g in range(4):
                                CP("pool" if g % 2 == 0 else "dve", QT[64:128, g, i * 128:(i + 1) * 128], mt_t[64:128, :], [mt_r], [r_QM[g][i]])

                        if k == 0:
                            dump("d_qt0", QT[:, 0, :], [128, T], BF16, r_QT + [x for l in r_QM for x in l])
                        for g in range(4):
                            hg = 4 * k + g
                            for br in (1, 2):
                                for tcn in range(8):
                                    j_lo = 0 if br == 1 else max(0, 4 * tcn - 4)
                                    j_hi = 4 * tcn + 3
                                    for j in range(j_lo, j_hi + 1):
                                        qa = max(0, j - 4 * tcn)
                                        qb = 3 if br == 1 else min(3, j + 4 - 4 * tcn)
                                        c0, c1 = qa * 128, (qb + 1) * 128
                                        t0 = tcn * 512
                                        adds = []
                                        for q in range(qa, qb + 1):
                                            dlt = 4 * tcn + q - j
                                            if dlt == 0:
                                                adds.append((q, 0))
                                            elif dlt == 1:
                                                adds.append((q, 1))
                                            elif dlt == 4 and br == 2:
                                                adds.append((q, 2))
                                        p_s, p_r = pS.next()
                                        if br == 1:
                                            rd = [r_KsT, r_QT[g]] + [r_QM[g][4 * tcn + q] for q in range(qa, qb + 1)]
                                            MM(p_s[:, c0:c1], KsT[:, j * 128:(j + 1) * 128], QT[:, g, t0 + c0:t0 + c1], True, len(adds) == 0, rd, [p_r])
                                        else:
                                            MM(p_s[:, c0:c1], KwT[:, j * 128:(j + 1) * 128], QT[0:64, g, t0 + c0:t0 + c1], True, len(adds) == 0, [r_KwT, r_QT[g]], [p_r])
                                        for ai, (q, ty) in enumerate(adds):
                                            MM(p_s[:, q * 128:(q + 1) * 128], ident_b[:], BD[:, hg, ty, :], False, ai == len(adds) - 1, [r_identb, r_BD], [p_r])
                                        P_t, P_r = P_ring.next()
                                        ACTF(P_t[:, c0:c1], p_s[:, c0:c1], AF.Exp, [p_r, r_t31], [P_r], bias=t31[:, hg:hg + 1])
                                        Vx, r_Vx = (Vs, r_Vs) if br == 1 else (Vw, r_Vw)
                                        for q in range(qa, qb + 1):
                                            i = 4 * tcn + q
                                            first_j = 0 if br == 1 else max(0, i - 4)
                                            MM(pO[q][:, 0:65], P_t[:, q * 128:(q + 1) * 128], Vx[:, j, :], j == first_j, j == i, [P_r, r_Vx], [r_pO[q]])
                                    for q in range(4):
                                        i = 4 * tcn + q
                                        finish_tile(pO[q], r_pO[q], 64, i, hg, br, False)

                dump("d_aout", Aout[:], [128, NT, 512], BF16, rA)
                with Scope(kb) as stn:
                    junkC = sb(stn, "junkC", [128, 512], BF16); r_junkC = Res()
                    an = Ring([sb(stn, f"an{i}", [128, 512], BF16) for i in range(2)])
                    af = Ring([sb(stn, f"af{i}", [128, 512], F32) for i in range(2)])
                    ao = Ring([sb(stn, f"ao{i}", [128, 512], BF16) for i in range(2)])
                    pTb = Ring([ps(stn, f"pTC{i}", [128, 512], BF16) for i in range(2)]) if False else None
                    for i in range(NT):
                        s_t, s_r = sm.next()
                        ACTF(junkC[:], Aout[:, i, :], AF.Square, [rA[i]], [r_junkC, s_r], accum_out=s_t[:, 0:1])
                        TS("dve", s_t[:, 1:2], s_t[:, 0:1], 1.0 / 512, EPS, ALU.mult, ALU.add, [s_r], [s_r])
                        ACTF(s_t[:, 1:2], s_t[:, 1:2], AF.Sqrt, [s_r], [s_r])
                        E("dve", lambda: nc.vector.reciprocal(out=s_t[:, 2:3], in_=s_t[:, 1:2]), [s_r], [s_r])
                        af_t, af_r = af.next()
                        STT(af_t[:], Aout[:, i, :], s_t[:, 2:3], ga_t[:], ALU.mult, ALU.mult, [rA[i], s_r, r_ga], [af_r])
                        px, px_r = pX.next()
                        for fc in range(4):
                            TR(px[:, fc * 128:(fc + 1) * 128], af_t[:, fc * 128:(fc + 1) * 128], ident_f[:], [af_r, r_identf], [px_r])
                        ao_t, ao_r = ao.next()
                        CP("act", ao_t[:], px[:], [px_r], [ao_r])
                        DMA("dsp" if i % 2 == 0 else "dpool", mixT_s[0:512, i * 128:(i + 1) * 128].rearrange("(f p) t -> p f t", p=128),
                            ao_t[:].rearrange("p (f t) -> p f t", f=4), [ao_r], [R["mixT_s"]])

        if "D" in phases:
            with Scope(kb) as st:
                Wo = sb(st, "Wo", [128, 8, D], BF16); r_Wo = Res()
                Wq = sb(st, "Wq", [128, 8, 2048], BF16); r_Wq = Res()
                Wgt = sb(st, "Wgt", [128, 8, D], BF16); r_Wgt = Res()
                Wp = sb(st, "Wp", [128, 2, D], BF16); r_Wp = Res()
                skb = sb(st, "skb", [128, 2, 128], BF16); r_skb = Res()
                rep = sb(st, "rep", [128, 4, D], F32); r_rep = Res()
                io16 = sb(st, "io16", [128, 16], F32); r_io = Res()
                selt = sb(st, "selt", [128, 2], F32); r_sel = Res()
                mst = Ring([sb(st, f"mst{i}", [128, 8, 2, 128], BF16) for i in range(2)])
                mixh_ring = Ring([sb(st, f"mixh{i}", [128, 8, 128], BF16) for i in range(2)])
                DMA("dsp", rep[:], rep4, [], [r_rep])
                DMA("dact", io16[:], iota16, [], [r_io])
                DMA("dact", selt[:], selc, [], [r_sel])
                with Scope(kb) as stw:
                    wst = Ring([sb(stw, f"wstD{i}", [128, 2048], F32) for i in range(3)])
                    n = 0
                    for (src, dstw, dres, ncol, nch) in ((w_out_in, Wo, r_Wo, D, 8), (peer_wq, Wq, r_Wq, 2048, 8), (ple_wg, Wgt, r_Wgt, D, 8), (ple_pj, Wp, r_Wp, D, 2)):
                        for dc in range(nch):
                            w_t, w_r = wst.next()
                            n += 1
                            DMA("dsp" if n % 2 == 0 else "dact", w_t[:, 0:ncol], src[dc * 128:(dc + 1) * 128, :], [], [w_r])
                            CP("dve" if n % 2 == 0 else "pool", dstw[:, dc, :], w_t[:, 0:ncol], [w_r], [dres])
                    w_t, w_r = wst.next()
                    DMA("dsp", w_t[:, 0:256].rearrange("p (a k) -> p a k", a=2), sk_T.rearrange("a p k -> p a k"), [], [w_r])
                    CP("dve", skb[:], w_t[:, 0:256].rearrange("p (a k) -> p a k", a=2), [w_r], [r_skb])

                pacc = Ring([ps(st, f"pD{i}", [128, 512], F32) for i in range(6)])
                ptb = Ring([ps(st, f"pDb{i}", [128, 1024], BF16) for i in range(2)])
                xh_ring = Ring([sb(st, f"xhD{i}", [128, D], F32) for i in range(1)])
                H_ring = Ring([sb(st, f"HD{i}", [128, D], F32) for i in range(1)])
                xng_ring = Ring([sb(st, f"xng{i}", [128, D], F32) for i in range(1)])
                xnb_ring = Ring([sb(st, f"xnbD{i}", [128, D], BF16) for i in range(1)])
                xT_ring = Ring([sb(st, f"xTD{i}", [128, 8, 128], BF16) for i in range(1)])
                qTb = sb(st, "qTb", [128, 16, 128], BF16); r_qTb = Res()
                Ssc = sb(st, "Ssc", [128, 16, 128], F32); r_S = Res()
                Swk = sb(st, "Swk", [128, 128], F32); r_Swk = Res()
                v16 = sb(st, "v16", [128, 16, 16], F32); r_v16 = Res()
                i16 = sb(st, "i16", [128, 16, 16], U32); r_i16 = Res()
                i16f = sb(st, "i16f", [128, 16, 16], F32); r_i16f = Res()
                cand = sb(st, "cand", [128, 8, 256], F32); r_cand = Res()
                cwk = sb(st, "cwk", [128, 256], F32); r_cwk = Res()
                sc16 = sb(st, "sc16", [128, 8, 16], F32); r_sc = Res()
                ci16 = sb(st, "ci16", [128, 8, 16], U32); r_ci = Res()
                ab_u = sb(st, "ab_u", [128, 2, 8, 16], U32); r_abu = Res()
                ab_f = sb(st, "ab_f", [128, 2, 8, 16], F32); r_abf = Res()
                eq = sb(st, "eq", [128, 8, 16, 16], F32); r_eq = Res()
                isel = sb(st, "isel", [128, 2, 8, 16], F32); r_isel = Res()
                ef = sb(st, "ef", [128, 128], F32); r_ef = Res()
                eu = sb(st, "eu", [128, 128], U32); r_eu = Res()
                gw = sb(st, "gw", [128, 8, 16], F32); r_gw = Res()
                gz = sb(st, "gz", [128, 16], F32); r_gz = Res()
                dots = sb(st, "dots", [128, 128], F32); r_dots = Res()
                coef = sb(st, "coef", [128, 128], F32); r_coef = Res()
                junkD = sb(st, "junkD", [128, D], F32); r_junkD = Res()
                junkB = sb(st, "junkDb", [128, D], BF16); r_junkB = Res()
                ug_ring = Ring([sb(st, f"ug{i}", [128, D], F32) for i in range(3)])
                vg_ring = Ring([sb(st, f"vg{i}", [128, D], F32) for i in range(3)])
                smD = Ring([sb(st, f"smD{i}", [128, 8], F32) for i in range(4)])
                pht = Ring([sb(st, f"pht{i}", [128, 256], F32) for i in range(2)])
                phb = Ring([sb(st, f"phb{i}", [128, 256], BF16) for i in range(2)])
                phT = Ring([sb(st, f"phT{i}", [128, 2, 128], BF16) for i in range(2)])
                gt_ring = Ring([sb(st, f"gtD{i}", [128, D], F32) for i in range(1)])
                ot_ring = Ring([sb(st, f"otD{i}", [128, D], F32) for i in range(1)])

                def rms_scaled(src, src_r, gi, dstf, dstf_r):
                    s_t, s_r = smD.next()
                    ACTF(junkB[:], src, AF.Square, [src_r], [r_junkB, s_r], accum_out=s_t[:, 0:1])
                    TS("dve", s_t[:, 1:2], s_t[:, 0:1], 1.0 / D, EPS, ALU.mult, ALU.add, [s_r], [s_r])
                    ACTF(s_t[:, 1:2], s_t[:, 1:2], AF.Sqrt, [s_r], [s_r])
                    E("dve", lambda: nc.vector.reciprocal(out=s_t[:, 2:3], in_=s_t[:, 1:2]), [s_r], [s_r])
                    STT(dstf, src, s_t[:, 2:3], rep[:, gi, :], ALU.mult, ALU.mult, [src_r, s_r, r_rep], [dstf_r])

                def transpose8(srcb, srcb_r, dstT, dstT_r, nblk=8):
                    pt, pt_r = ptb.next()
                    for dc in range(nblk):
                        TR(pt[:, dc * 128:(dc + 1) * 128], srcb[:, dc * 128:(dc + 1) * 128], ident_b[:], [srcb_r, r_identb], [pt_r])
                    CP("act", dstT.rearrange("p a t -> p (a t)"), pt[:, 0:nblk * 128], [pt_r], [dstT_r])

                for it in range(TH // 128):
                    tsl = slice(it * 128, (it + 1) * 128)
                    xh_t, xh_r = xh_ring.next()
                    DMA("dsp", xh_t[:], xh[tsl, :], [], [xh_r])
                    H, H_r = H_ring.next()
                    m_t, m_r = mst.next()
                    for a in range(2):
                        DMA("dsp" if a == 0 else "dact", m_t[:, :, a, :], mixT_s[:, a * TH + it * 128:a * TH + (it + 1) * 128].rearrange("(f p) t -> p f t", p=128), [R["mixT_s"]], [m_r])
                    mixh, r_mixh = mixh_ring.next()
                    TS("pool", mixh[:], m_t[:, :, 0, :], selt[:, 0:1], None, ALU.mult, None, [m_r, r_sel], [r_mixh])
                    STT(mixh[:], m_t[:, :, 1, :], selt[:, 1:2], mixh[:], ALU.mult, ALU.add, [m_r, r_sel, r_mixh], [r_mixh])
                    for ch in range(2):
                        pa, pa_r = pacc.next()
                        for fc in range(8):
                            MM(pa[:], mixh[:, fc, :], Wo[:, fc, ch * 512:(ch + 1) * 512], fc == 0, fc == 7, [r_mixh, r_Wo], [pa_r])
                        TT("dve", H[:, ch * 512:(ch + 1) * 512], pa[:], xh_t[:, ch * 512:(ch + 1) * 512], ALU.add, [pa_r, xh_r], [H_r])
                    xng, xng_r = xng_ring.next()
                    rms_scaled(H[:], H_r, 0, xng[:], xng_r)
                    xnb, xnb_r = xnb_ring.next()
                    CP("pool", xnb[:], xng[:], [xng_r], [xnb_r])
                    xT, xT_r = xT_ring.next()
                    transpose8(xnb, xnb_r, xT[:], xT_r)
                    for grp in range(4):
                        pa, pa_r = pacc.next()
                        for j in range(4):
                            hp = grp * 4 + j
                            for dc in range(8):
                                MM(pa[:, j * 128:(j + 1) * 128], Wq[:, dc, hp * 128:(hp + 1) * 128], xT[:, dc, :], dc == 0, dc == 7, [r_Wq, xT_r], [pa_r])
                        CP("act" if grp % 2 == 0 else "dve", qTb[:, grp * 4:(grp + 1) * 4, :].rearrange("p a t -> p (a t)"), pa[:], [pa_r], [r_qTb])
                    for grp in range(4):
                        pa, pa_r = pacc.next()
                        for j in range(4):
                            hp = grp * 4 + j
                            MM(pa[:, j * 128:(j + 1) * 128], qTb[:, hp, :], skb[:, hp % 2, :], True, True, [r_qTb, r_skb], [pa_r])
                        CP("act" if grp % 2 == 0 else "dve", Ssc[:, grp * 4:(grp + 1) * 4, :].rearrange("p a t -> p (a t)"), pa[:], [pa_r], [r_S])
                    for hp in range(16):
                        E("dve", lambda: nc.vector.max(out=v16[:, hp, 0:8], in_=Ssc[:, hp, :]), [r_S], [r_v16])
                        E("dve", lambda: nc.vector.max_index(out=i16[:, hp, 0:8], in_max=v16[:, hp, 0:8], in_values=Ssc[:, hp, :]), [r_S, r_v16], [r_i16])
                        E("dve", lambda: nc.vector.match_replace(out=Swk[:], in_to_replace=v16[:, hp, 0:8], in_values=Ssc[:, hp, :], imm_value=-1e30), [r_S, r_v16], [r_Swk])
                        E("dve", lambda: nc.vector.max(out=v16[:, hp, 8:16], in_=Swk[:]), [r_Swk], [r_v16])
                        E("dve", lambda: nc.vector.max_index(out=i16[:, hp, 8:16], in_max=v16[:, hp, 8:16], in_values=Swk[:]), [r_Swk, r_v16], [r_i16])
                    CP("dve", i16f[:], i16[:], [r_i16], [r_i16f])
                    v4 = v16[:].rearrange("p (h two) k -> p h two k", two=2)
                    in0 = v4[:, :, 0, :].rearrange("p h (a o) -> p h a o", o=1).to_broadcast([128, 8, 16, 16])
                    in1 = v4[:, :, 1, :].rearrange("p h (o b) -> p h o b", o=1).to_broadcast([128, 8, 16, 16])
                    TT("dve", cand[:].rearrange("p h (a b) -> p h a b", a=16), in0, in1, ALU.add, [r_v16], [r_cand])
                    for h in range(8):
                        E("dve", lambda: nc.vector.max(out=sc16[:, h, 0:8], in_=cand[:, h, :]), [r_cand], [r_sc])
                        E("dve", lambda: nc.vector.max_index(out=ci16[:, h, 0:8], in_max=sc16[:, h, 0:8], in_values=cand[:, h, :]), [r_cand, r_sc], [r_ci])
                        E("dve", lambda: nc.vector.match_replace(out=cwk[:], in_to_replace=sc16[:, h, 0:8], in_values=cand[:, h, :], imm_value=-1e30), [r_cand, r_sc], [r_cwk])
                        E("dve", lambda: nc.vector.max(out=sc16[:, h, 8:16], in_=cwk[:]), [r_cwk], [r_sc])
                        E("dve", lambda: nc.vector.max_index(out=ci16[:, h, 8:16], in_max=sc16[:, h, 8:16], in_values=cwk[:]), [r_cwk, r_sc], [r_ci])
                    E("dve", lambda: nc.vector.tensor_single_scalar(out=ab_u[:, 0], in_=ci16[:], scalar=4, op=ALU.logical_shift_right), [r_ci], [r_abu])
                    E("dve", lambda: nc.vector.tensor_single_scalar(out=ab_u[:, 1], in_=ci16[:], scalar=15, op=ALU.bitwise_and), [r_ci], [r_abu])
                    CP("dve", ab_f[:], ab_u[:], [r_abu], [r_abf])
                    i4 = i16f[:].rearrange("p (h two) k -> p h two k", two=2)
                    for w in range(2):
                        a_b = ab_f[:, w].rearrange("p h (k o) -> p h k o", o=1).to_broadcast([128, 8, 16, 16])
                        io_b = io16[:].rearrange("p (o q a) -> p o q a", o=1, q=1).to_broadcast([128, 8, 16, 16])
                        TT("dve", eq[:], a_b, io_b, ALU.is_equal, [r_abf, r_io], [r_eq])
                        iv_b = i4[:, :, w, :].rearrange("p h (o a) -> p h o a", o=1).to_broadcast([128, 8, 16, 16])
                        TT("dve", eq[:], eq[:], iv_b, ALU.mult, [r_eq, r_i16f], [r_eq])
                        E("dve", lambda: nc.vector.tensor_reduce(out=isel[:, w], in_=eq[:], axis=AX.X, op=ALU.add), [r_eq], [r_isel])
                    STT(ef[:].rearrange("p (h k) -> p h k", h=8), isel[:, 0], 128.0, isel[:, 1], ALU.mult, ALU.add, [r_isel], [r_ef])
                    CP("dve", eu[:], ef[:], [r_ef], [r_eu])
                    TT("dve", gw[:], sc16[:], sc16[:, :, 0:1].to_broadcast([128, 8, 16]), ALU.subtract, [r_sc], [r_gw])
                    ACTF(gw[:], gw[:], AF.Exp, [r_gw], [r_gw])
                    E("dve", lambda: nc.vector.tensor_reduce(out=gz[:, 0:8], in_=gw[:], axis=AX.X, op=ALU.add), [r_gw], [r_gz])
                    E("dve", lambda: nc.vector.reciprocal(out=gz[:, 8:16], in_=gz[:, 0:8]), [r_gz], [r_gz])
                    TT("dve", gw[:], gw[:], gz[:, 8:16].rearrange("p (h o) -> p h o", o=1).to_broadcast([128, 8, 16]), ALU.mult, [r_gw, r_gz], [r_gw])
                    for c in range(128):
                        ug, ug_r = ug_ring.next()
                        E("dpool", lambda: nc.gpsimd.indirect_dma_start(out=ug[:], out_offset=None, in_=peer_u, in_offset=bass.IndirectOffsetOnAxis(ap=eu[:, c:c + 1], axis=0)), [r_eu], [ug_r])
                        STT(junkD[:], ug[:], 1.0, xng[:], ALU.mult, ALU.mult, [ug_r, xng_r], [r_junkD, r_dots], accum_out=dots[:, c:c + 1])
                    ACTF(coef[:], dots[:], AF.Gelu_apprx_tanh, [r_dots], [r_coef])
                    TT("dve", coef[:], coef[:], gw[:].rearrange("p h k -> p (h k)"), ALU.mult, [r_coef, r_gw], [r_coef])
                    for c in range(128):
                        vg, vg_r = vg_ring.next()
                        E("dpool", lambda: nc.gpsimd.indirect_dma_start(out=vg[:], out_offset=None, in_=peer_v, in_offset=bass.IndirectOffsetOnAxis(ap=eu[:, c:c + 1], axis=0)), [r_eu], [vg_r])
                        STT(H[:], vg[:], coef[:, c:c + 1], H[:], ALU.mult, ALU.add, [vg_r, r_coef, H_r], [H_r])
                    x3, x3_r = xng_ring.next()
                    rms_scaled(H[:], H_r, 1, x3[:], x3_r)
                    x3b, x3b_r = xnb_ring.next()
                    CP("pool", x3b[:], x3[:], [x3_r], [x3b_r])
                    x3T, x3T_r = xT_ring.next()
                    transpose8(x3b, x3b_r, x3T[:], x3T_r)
                    ph_t, ph_r = pht.next()
                    DMA("dact", ph_t[:], ph[tsl, :], [], [ph_r])
                    pb_t, pb_r = phb.next()
                    CP("pool", pb_t[:], ph_t[:], [ph_r], [pb_r])
                    pT_t, pT_r = phT.next()
                    transpose8(pb_t, pb_r, pT_t[:], pT_r, nblk=2)
                    gt, gt_r = gt_ring.next()
                    for ch in range(2):
                        csl = slice(ch * 512, (ch + 1) * 512)
                        pa, pa_r = pacc.next()
                        for dc in range(8):
                            MM(pa[:], x3T[:, dc, :], Wgt[:, dc, csl], dc == 0, dc == 7, [x3T_r, r_Wgt], [pa_r])
                        TT("dve", gt[:, csl], pa[:], rep[:, 3, csl], ALU.add, [pa_r, r_rep], [gt_r])
                        ACTF(gt[:, csl], gt[:, csl], AF.Sigmoid, [gt_r], [gt_r])
                        pb2, pb2_r = pacc.next()
                        for dc in range(2):
                            MM(pb2[:], pT_t[:, dc, :], Wp[:, dc, csl], dc == 0, dc == 1, [pT_r, r_Wp], [pb2_r])
                        TT("dve", gt[:, csl], gt[:, csl], pb2[:], ALU.mult, [gt_r, pb2_r], [gt_r])
                        TT("pool", H[:, csl], H[:, csl], gt[:, csl], ALU.add, [H_r, gt_r], [H_r])
                    ot, ot_r = ot_ring.next()
                    rms_scaled(H[:], H_r, 2, ot[:], ot_r)
                    DMA("dsp", out[tsl, :], ot[:], [ot_r], [])

        kb.drain_all()
    return nc


def _blockdiag(w):
    o = np.zeros((128, 4, 128), np.float32)
    for n in range(8):
        cc, j = n // 2, n % 2
        o[j * 64:(j + 1) * 64, cc, j * 64:(j + 1) * 64] = w[n]
    return o


def _rel_bucket(dist):
    n = np.maximum(dist, 0)
    nf = np.maximum(n, 16).astype(np.float32)
    large = 16 + (np.log(nf / np.float32(16)) / np.float32(np.log(8.0)) * np.float32(16)).astype(np.int32)
    large = np.minimum(large, 31)
    return np.where(n < 16, n, large)


def _nsa_consts(rel_table):
    c = {}
    assert (_rel_bucket(np.arange(113, 8192)) == 31).all()
    cl = np.arange(128)[:, None, None]; mp = np.arange(5)[None, :, None]; tt = np.arange(512)[None, None, :]
    dist = 512 * mp + tt - 16 * cl - 31
    c["bc_g"] = np.ascontiguousarray(rel_table[_rel_bucket(dist)].transpose(3, 0, 1, 2))
    c["bc_m"] = np.where(dist >= 0, 0.0, NEGM).astype(np.float32)
    assert (512 * 5 - 16 * 127 - 31) >= 113
    sl = np.arange(128)[:, None]; tl = np.arange(128)[None, :]
    d0 = tl - sl; d1 = 128 + tl - sl
    g0 = rel_table[_rel_bucket(d0)]; g1 = rel_table[_rel_bucket(d1)]
    c["bd_g"] = np.ascontiguousarray(np.stack([g0, g1], 0).transpose(3, 1, 0, 2))
    m0 = np.where(d0 >= 0, 0.0, NEGM); m2 = np.where(tl < sl, 0.0, NEGM)
    c["bd_m"] = np.ascontiguousarray(np.stack([m0, np.zeros_like(m0), m2], 1)).astype(np.float32)
    c["t31"] = np.ascontiguousarray(np.broadcast_to(rel_table[31][None, :], (128, 8))).astype(np.float32)
    t = (np.arange(NT)[None, :, None] * 128 + np.arange(128)[:, None, None])
    blk = np.arange(64)[None, None, :]
    d = t // 64 - blk
    local = (d >= 0) & (d < 2)
    init = (blk == 0) & ~local
    past = (d >= 0) & ~local & ~init
    c["force_c"] = np.where(local, 2.0e4, np.where(init, 1.0e4, np.where(past, 0.0, -1.0))).astype(np.float32)
    c["keep_c"] = past.astype(np.float32)
    cs = np.arange(256)[:, None] * 16; ss = np.arange(64)[None, :] * 64
    ov = np.clip(np.minimum(cs + 32, ss + 64) - np.maximum(cs, ss), 0, None).astype(np.float32) / 32.0
    ove = np.concatenate([ov, np.ones((256, 1), np.float32)], 1)
    ove[255] = 0.0
    c["ovl_ext"] = np.ascontiguousarray(ove.reshape(2, 128, 65).transpose(1, 0, 2))
    c["erows"] = (np.arange(T)[None, :] // 64 == np.arange(64)[:, None]).astype(np.float32)
    return c


def _prep_inputs(inputs):
    x = np.ascontiguousarray(inputs["x"], dtype=np.float32)
    p = np.ascontiguousarray(inputs["p"], dtype=np.float32)
    shared = {
        "ident": np.eye(128, dtype=np.float32),
        "w_in": np.ascontiguousarray(inputs["w_in"][0]),
        "g_attn": np.ascontiguousarray(inputs["attn_norm"][0].reshape(8, 128).T),
        "lru_cw": np.ascontiguousarray(inputs["conv_w"][0][:, 0, :].reshape(4, 4, 128).transpose(2, 1, 0)),
        "lru_vec": np.ascontiguousarray(np.stack([inputs[k][0].reshape(4, 128) for k in
                                                  ("conv_b", "lru_ba", "lru_bx", "lru_lambda", "grp_norm_lru")], 0).transpose(2, 0, 1)),
        "cmp_w1": np.ascontiguousarray(np.stack([inputs[k][0].reshape(16, 128, 256).transpose(1, 0, 2) for k in ("cmp_k_w1", "cmp_v_w1")], 0)),
        "cmp_pe": np.ascontiguousarray(np.stack([inputs[k][0].reshape(16, 128).T for k in ("cmp_k_pe", "cmp_v_pe")], 0)),
        "cmp_w2": np.ascontiguousarray(np.stack([inputs[k][0].reshape(2, 128, 64).transpose(1, 0, 2) for k in ("cmp_k_w2", "cmp_v_w2")], 0)),
        "ga_rep": np.ascontiguousarray(np.broadcast_to(inputs["grp_norm_attn"][0][None, :], (128, 512))).astype(np.float32),
        "w_out": np.ascontiguousarray(inputs["w_out"][0]),
        "peer_wq": np.ascontiguousarray(inputs["peer_wq"][0]),
        "sk_T": np.ascontiguousarray(inputs["peer_subkeys"][0].transpose(0, 2, 1)),
        "peer_u": np.ascontiguousarray(inputs["peer_u"][0]),
        "peer_v": np.ascontiguousarray(inputs["peer_v"][0]),
        "ple_wgate": np.ascontiguousarray(inputs["ple_wgate"][0]),
        "ple_proj": np.ascontiguousarray(inputs["ple_proj"][0]),
        "rep4": np.ascontiguousarray(np.broadcast_to(np.stack([inputs["ffn_norm"][0], inputs["ple_norm"][0], inputs["final_norm"], inputs["ple_bgate"][0]], 0)[None], (128, 4, D))).astype(np.float32),
        "iota16": np.ascontiguousarray(np.broadcast_to(np.arange(16, dtype=np.float32)[None], (128, 16))),
        "lru_bda": _blockdiag(inputs["lru_wa"][0]),
        "lru_bdx": _blockdiag(inputs["lru_wx"][0]),
    }
    shared.update(_nsa_consts(np.asarray(inputs["rel_table"], np.float32)))
    in_maps = []
    for c in range(8):
        b, hf = c // 2, c % 2
        m = dict(shared)
        m["xb"] = x[b]
        m["xh"] = np.ascontiguousarray(x[b, hf * TH:(hf + 1) * TH])
        m["ph"] = np.ascontiguousarray(p[0, b, hf * TH:(hf + 1) * TH])
        sel = np.zeros((128, 2), np.float32); sel[:, hf] = 1.0
        m["selc"] = sel
        in_maps.append(m)
    return in_maps


def kernel(**inputs):
    nc = build_program()
    in_maps = _prep_inputs(inputs)
    res = run_bass_kernel_spmd(nc, in_maps, core_ids=list(range(8)))
    outp = np.zeros((4, T, D), np.float32)
    for c in range(8):
        b, hf = c // 2, c % 2
        outp[b, hf * TH:(hf + 1) * TH] = res.results[c]["out"]
    return outp
```

```python
import numpy as np
from contextlib import ExitStack
import concourse.bass as bass
import concourse.mybir as mybir
from concourse.bass_utils import run_bass_kernel_spmd

F32 = mybir.dt.float32
BF16 = mybir.dt.bfloat16
U32 = mybir.dt.uint32
AF = mybir.ActivationFunctionType
ALU = mybir.AluOpType
AX = mybir.AxisListType

T = 4096
D = 1024
NT = T // 128
TH = 2048
IN_COLS = 2328
EPS = 1e-6
NEGM = -30000.0


class Res:
    __slots__ = ("name", "lw", "rd")

    def __init__(self, name=""):
        self.name = name
        self.lw = None
        self.rd = {}


class KB:
    NDMA = 4

    def __init__(self, nc, stack):
        self.nc = nc
        self.issue = {"pe": nc.tensor, "act": nc.scalar, "dve": nc.vector, "pool": nc.gpsimd,
                      "dsp": nc.sync, "dact": nc.scalar, "dpool": nc.gpsimd}
        self.stream = {"pe": "pe", "act": "act", "dve": "dve", "pool": "pool",
                       "dsp": "sp", "dact": "act", "dpool": "pool"}
        self.sems = {}
        self.cnt = {}
        for q in self.issue:
            n = self.NDMA if self.is_dma(q) else 1
            self.sems[q] = [stack.enter_context(nc.semaphore(f"s_{q}{i}")) for i in range(n)]
            self.cnt[q] = 0
        self.waited = {s: {} for s in ("pe", "act", "dve", "pool", "sp")}
        self.ninst = 0
        self._rr = 0

    @staticmethod
    def is_dma(q):
        return q in ("dsp", "dact", "dpool")

    @staticmethod
    def _need(need, dep):
        if dep is None:
            return
        q, c = dep
        if need.get(q, 0) < c:
            need[q] = c

    def _waits(self, st, eng, need, skip_q=None):
        for dq, c in need.items():
            if dq == "pe" and skip_q == "pe":
                continue
            if self.is_dma(dq):
                n = self.NDMA
                for si in range(n):
                    k = (c - 1 - si) // n + 1 if c - 1 >= si else 0
                    if k <= 0:
                        continue
                    key = (dq, si)
                    if self.waited[st].get(key, 0) >= k:
                        continue
                    eng.wait_ge(self.sems[dq][si], 16 * k)
                    self.waited[st][key] = k
            else:
                key = (dq, 0)
                if self.waited[st].get(key, 0) >= c:
                    continue
                eng.wait_ge(self.sems[dq][0], c)
                self.waited[st][key] = c

    def emit(self, q, fn, reads=(), writes=()):
        need = {}
        for r in reads:
            self._need(need, r.lw)
        for w in writes:
            self._need(need, w.lw)
            for rq, rc in w.rd.items():
                self._need(need, (rq, rc))
        st = self.stream[q]
        self._waits(st, self.issue[q], need, skip_q=q)
        inst = fn()
        self.cnt[q] += 1
        c = self.cnt[q]
        if self.is_dma(q):
            inst.then_inc(self.sems[q][(c - 1) % self.NDMA], 16)
        else:
            inst.then_inc(self.sems[q][0], 1)
        for r in reads:
            if r.rd.get(q, 0) < c:
                r.rd[q] = c
        for w in writes:
            w.lw = (q, c)
            w.rd = {}
        self.ninst += 1
        return inst

    def dmaq(self):
        self._rr ^= 1
        return "dsp" if self._rr else "dact"

    def barrier(self):
        need = {q: c for q, c in self.cnt.items() if c > 0}
        for st, eng in (("pe", self.nc.tensor), ("act", self.nc.scalar), ("dve", self.nc.vector),
                        ("pool", self.nc.gpsimd), ("sp", self.nc.sync)):
            self._waits(st, eng, dict(need))

    def drain_all(self):
        need = {q: c for q, c in self.cnt.items() if c > 0}
        self._waits("sp", self.nc.sync, need)


class Scope:
    def __init__(self, kb):
        self.kb = kb
        self.st = ExitStack()

    def __enter__(self):
        self.st.__enter__()
        return self.st

    def __exit__(self, *a):
        if a[0] is None:
            self.kb.barrier()
        return self.st.__exit__(*a)


class Ring:
    def __init__(self, tiles):
        self.tiles = tiles
        self.res = [Res() for _ in tiles]
        self.i = -1

    def next(self):
        self.i = (self.i + 1) % len(self.tiles)
        return self.tiles[self.i], self.res[self.i]


def build_program(dbg=None, phases=("A", "B", "C", "D")):
    nc = bass.Bass("TRN2", target_bir_lowering=False)

    def din(name, shape, dt=F32):
        return nc.dram_tensor(name, list(shape), dt, kind="ExternalInput").ap()

    dbg = dbg or ()

    def dscr(name, shape, dt):
        kind = "ExternalOutput" if name in dbg else "Internal"
        return nc.dram_tensor(name, list(shape), dt, kind=kind).ap()

    xb = din("xb", [T, D])
    xh = din("xh", [TH, D])
    ph = din("ph", [TH, 256])
    selc = din("selc", [128, 2])
    ident = din("ident", [128, 128])
    w_in = din("w_in", [D, IN_COLS])
    g_attn = din("g_attn", [128, 8])
    out = nc.dram_tensor("out", [TH, D], F32, kind="ExternalOutput").ap()
    lru_cw = din("lru_cw", [128, 4, 4])
    lru_vec = din("lru_vec", [128, 5, 4])
    lru_bda = din("lru_bda", [128, 4, 128])
    lru_bdx = din("lru_bdx", [128, 4, 128])

    qT_s = dscr("qT_s", [512, T], BF16)
    kcT_s = dscr("kcT_s", [128, T], BF16)
    vcT_s = dscr("vcT_s", [128, T], BF16)
    ksT_s = dscr("ksT_s", [128, T], BF16)
    kwT_s = dscr("kwT_s", [128, T], BF16)
    vs_s = dscr("vs_s", [T, 128], BF16)
    vw_s = dscr("vw_s", [T, 128], BF16)
    gates_s = dscr("gates_s", [T, 24], F32)
    xrT_s = dscr("xrT_s", [512, T], F32)
    xgT_s = dscr("xgT_s", [512, T], F32)

    cmp_w1 = din("cmp_w1", [2, 128, 16, 256])
    cmp_pe = din("cmp_pe", [2, 128, 16])
    cmp_w2 = din("cmp_w2", [2, 128, 2, 64])
    ovl_ext = din("ovl_ext", [128, 2, 65])
    bc_g = din("bc_g", [8, 128, 5, 512])
    bc_m = din("bc_m", [128, 5, 512])
    bd_g = din("bd_g", [8, 128, 2, 128])
    bd_m = din("bd_m", [128, 3, 128])
    t31_in = din("t31", [128, 8])
    force_in = din("force_c", [128, 32, 64])
    keep_in = din("keep_c", [128, 32, 64])
    erows = din("erows", [64, T])
    ga_in = din("ga_rep", [128, 512])
    w_out_in = din("w_out", [D, D])
    peer_wq = din("peer_wq", [D, 2048])
    sk_T = din("sk_T", [2, 128, 128])
    peer_u = din("peer_u", [16384, D])
    peer_v = din("peer_v", [16384, D])
    ple_wg = din("ple_wgate", [D, D])
    ple_pj = din("ple_proj", [256, D])
    rep4 = din("rep4", [128, 4, D])
    iota16 = din("iota16", [128, 16])
    iota128 = din("iota128", [128, 128])
    H1_s = dscr("H1_s", [TH, D], F32)
    xnT2_s = dscr("xnT2_s", [128, 8, TH], BF16)
    Wt_s = dscr("Wt_s", [128, 128, TH], BF16)
    mixT_s = dscr("mixT_s", [1024, T], BF16)
    R = {n: Res(n) for n in ("H1_s", "xnT2_s", "Wt_s", "mixT_s", "qT_s", "kcT_s", "vcT_s", "ksT_s", "kwT_s", "vs_s", "vw_s", "gates_s", "xrT_s", "xgT_s")}

    with ExitStack() as top:
        kb = KB(nc, top)
        E = kb.emit

        uniq = [0]

        def sb(st, name, shape, dt):
            uniq[0] += 1
            return st.enter_context(nc.sbuf_tensor(f"sb{uniq[0]}_{name}", list(shape), dt))

        def ps(st, name, shape, dt):
            uniq[0] += 1
            return st.enter_context(nc.psum_tensor(f"ps{uniq[0]}_{name}", list(shape), dt))


        def MM(out_, lhsT, rhs, start, stop, reads, writes):
            return E("pe", lambda: nc.tensor.matmul(out_, lhsT=lhsT, rhs=rhs, start=start, stop=stop), reads, writes)

        def TR(out_, in_, idt, reads, writes):
            return E("pe", lambda: nc.tensor.transpose(out=out_, in_=in_, identity=idt), reads, writes)

        def ACTF(out_, in_, func, reads, writes, **kw):
            return E("act", lambda: nc.scalar.activation(out=out_, in_=in_, func=func, **kw), reads, writes)

        def veng(q):
            return nc.vector if q == "dve" else nc.gpsimd

        def TS(q, out_, in0, s1, s2, op0, op1, reads, writes):
            if op1 is None:
                return E(q, lambda: veng(q).tensor_scalar(out=out_, in0=in0, scalar1=s1, scalar2=None, op0=op0), reads, writes)
            return E(q, lambda: veng(q).tensor_scalar(out=out_, in0=in0, scalar1=s1, scalar2=s2, op0=op0, op1=op1), reads, writes)

        def TT(q, out_, in0, in1, op, reads, writes):
            return E(q, lambda: veng(q).tensor_tensor(out=out_, in0=in0, in1=in1, op=op), reads, writes)

        def STT(out_, in0, scalar, in1, op0, op1, reads, writes, **kw):
            return E("dve", lambda: nc.vector.scalar_tensor_tensor(out=out_, in0=in0, scalar=scalar, in1=in1, op0=op0, op1=op1, **kw), reads, writes)

        def CP(q, out_, in_, reads, writes):
            if q == "act":
                return E("act", lambda: nc.scalar.copy(out=out_, in_=in_), reads, writes)
            return E(q, lambda: veng(q).tensor_copy(out=out_, in_=in_), reads, writes)

        def MSET(q, out_, val, writes):
            return E(q, lambda: veng(q).memset(out_, val), (), writes)

        def DMA(q, out_, in_, reads, writes):
            eng = {"dsp": nc.sync, "dact": nc.scalar, "dpool": nc.gpsimd}[q]
            return E(q, lambda: eng.dma_start(out=out_, in_=in_), reads, writes)

        def dump(name, ap, shape, dt, res):
            if name not in dbg:
                return
            d = nc.dram_tensor(name, list(shape), dt, kind="ExternalOutput").ap()
            DMA("dsp", d, ap, [res] if not isinstance(res, list) else res, [])

        ident_f = sb(top, "ident_f", [128, 128], F32); r_identf = Res()
        ident_b = sb(top, "ident_b", [128, 128], BF16); r_identb = Res()
        E("dsp", lambda: nc.sync.dma_start(out=ident_f[:], in_=ident), writes=[r_identf])
        E("dve", lambda: nc.vector.tensor_copy(out=ident_b[:], in_=ident_f[:]), reads=[r_identf], writes=[r_identb])

        if "A" in phases:
            with Scope(kb) as st:
                Wg = sb(st, "Wg", [128, 8, IN_COLS], BF16); r_Wg = Res()
                gcol = sb(st, "gcol", [128, 8], F32); r_gcol = Res()
                wst = Ring([sb(st, f"wst{i}", [128, IN_COLS], F32) for i in range(2)])
                E("dsp", lambda: nc.sync.dma_start(out=gcol[:], in_=g_attn), writes=[r_gcol])
                for dc in range(8):
                    w_t, w_r = wst.next()
                    E("dsp" if dc % 2 == 0 else "dact",
                      (lambda w_t=w_t, dc=dc: nc.sync.dma_start(out=w_t[:], in_=w_in[dc * 128:(dc + 1) * 128, :])) if dc % 2 == 0 else
                      (lambda w_t=w_t, dc=dc: nc.scalar.dma_start(out=w_t[:], in_=w_in[dc * 128:(dc + 1) * 128, :])),
                      writes=[w_r])
                    eng = "dve" if dc % 2 == 0 else "pool"
                    ve = nc.vector if dc % 2 == 0 else nc.gpsimd
                    E(eng, lambda ve=ve, w_t=w_t, dc=dc: ve.tensor_scalar(out=Wg[:, dc, :], in0=w_t[:], scalar1=gcol[:, dc:dc + 1], scalar2=None, op0=ALU.mult),
                      reads=[w_r, r_gcol], writes=[r_Wg])

                xt_ring = Ring([sb(st, f"xt{i}", [128, 4, D], F32) for i in range(2)])
                xnb_ring = Ring([sb(st, f"xnb{i}", [128, 4, D], BF16) for i in range(2)])
                xnT_ring = Ring([sb(st, f"xnT{i}", [128, 8, 512], BF16) for i in range(2)])
                junk = sb(st, "junkA", [128, D], BF16); r_junk = Res()
                ss_ring = Ring([sb(st, f"ss{i}", [128, 8], F32) for i in range(2)])
                pT_ring = Ring([ps(st, f"pT{i}", [128, 512], BF16) for i in range(2)])
                pacc = Ring([ps(st, f"pacc{i}", [128, 512], F32) for i in range(4)])
                ostf = Ring([sb(st, f"ostf{i}", [128, 512], F32) for i in range(3)])
                ostb = Ring([sb(st, f"ostb{i}", [128, 512], BF16) for i in range(3)])
                osv = Ring([sb(st, f"osv{i}", [128, 256], BF16) for i in range(2)])
                osg = Ring([sb(st, f"osg{i}", [128, 24], F32) for i in range(2)])
                xb_v = xb.rearrange("(n p) d -> p n d", p=128)
                fm = []
                for cc in range(4):
                    fm.append((cc * 128, qT_s[cc * 128:(cc + 1) * 128, :], 0.125, True, R["qT_s"]))
                fm.append((512, kcT_s, 1.0, True, R["kcT_s"]))
                fm.append((640, vcT_s, 1.0, True, R["vcT_s"]))
                fm.append((768, ksT_s, 1.0, True, R["ksT_s"]))
                fm.append((1024, kwT_s, 1.0, True, R["kwT_s"]))
                for cc in range(4):
                    fm.append((1304 + cc * 128, xrT_s[cc * 128:(cc + 1) * 128, :], 1.0, False, R["xrT_s"]))
                for cc in range(4):
                    fm.append((1816 + cc * 128, xgT_s[cc * 128:(cc + 1) * 128, :], 1.0, False, R["xgT_s"]))
                ev = 0
                for tcn in range(8):
                    xt, xt_r = xt_ring.next()
                    E("dsp", lambda xt=xt, tcn=tcn: nc.sync.dma_start(out=xt[:], in_=xb_v[:, tcn * 4:(tcn + 1) * 4, :]), writes=[xt_r])
                    ss, ss_r = ss_ring.next()
                    for n in range(4):
                        E("act", lambda xt=xt, ss=ss, n=n: nc.scalar.activation(out=junk[:], in_=xt[:, n, :], func=AF.Square, accum_out=ss[:, n:n + 1]),
                          reads=[xt_r], writes=[r_junk, ss_r])
                    E("dve", lambda ss=ss: nc.vector.tensor_scalar(out=ss[:, 4:8], in0=ss[:, 0:4], scalar1=1.0 / D, scalar2=EPS, op0=ALU.mult, op1=ALU.add), reads=[ss_r], writes=[ss_r])
                    E("act", lambda ss=ss: nc.scalar.activation(out=ss[:, 4:8], in_=ss[:, 4:8], func=AF.Sqrt), reads=[ss_r], writes=[ss_r])
                    E("dve", lambda ss=ss: nc.vector.reciprocal(out=ss[:, 4:8], in_=ss[:, 4:8]), reads=[ss_r], writes=[ss_r])
                    xnb, xnb_r = xnb_ring.next()
                    for n in range(4):
                        if n % 2 == 0:
                            E("dve", lambda xt=xt, xnb=xnb, ss=ss, n=n: nc.vector.tensor_scalar(out=xnb[:, n, :], in0=xt[:, n, :], scalar1=ss[:, 4 + n:5 + n], scalar2=None, op0=ALU.mult),
                              reads=[xt_r, ss_r], writes=[xnb_r])
                        else:
                            E("pool", lambda xt=xt, xnb=xnb, ss=ss, n=n: nc.gpsimd.tensor_scalar(out=xnb[:, n, :], in0=xt[:, n, :], scalar1=ss[:, 4 + n:5 + n], scalar2=None, op0=ALU.mult),
                              reads=[xt_r, ss_r], writes=[xnb_r])
                    xnT, xnT_r = xnT_ring.next()
                    for dc in range(8):
                        pT, pT_r = pT_ring.next()
                        for n in range(4):
                            E("pe", lambda pT=pT, xnb=xnb, n=n, dc=dc: nc.tensor.transpose(out=pT[:, n * 128:(n + 1) * 128], in_=xnb[:, n, dc * 128:(dc + 1) * 128], identity=ident_b[:]),
                              reads=[xnb_r, r_identb], writes=[pT_r])
                        if dc % 2 == 0:
                            E("act", lambda pT=pT, xnT=xnT, dc=dc: nc.scalar.copy(out=xnT[:, dc, :], in_=pT[:]), reads=[pT_r], writes=[xnT_r])
                        else:
                            E("dve", lambda pT=pT, xnT=xnT, dc=dc: nc.vector.tensor_copy(out=xnT[:, dc, :], in_=pT[:]), reads=[pT_r], writes=[xnT_r])
                    for (c0, dst, scale, isb, dres) in fm:
                        pa, pa_r = pacc.next()
                        for dc in range(8):
                            E("pe", lambda pa=pa, dc=dc, c0=c0, xnT=xnT: nc.tensor.matmul(pa[:], lhsT=Wg[:, dc, c0:c0 + 128], rhs=xnT[:, dc, :], start=(dc == 0), stop=(dc == 7)),
                              reads=[r_Wg, xnT_r], writes=[pa_r])
                        o_t, o_r = (ostb if isb else ostf).next()
                        ev += 1
                        if ev % 2 == 0:
                            E("act", lambda o_t=o_t, pa=pa, scale=scale: nc.scalar.activation(out=o_t[:], in_=pa[:], func=AF.Copy, scale=scale), reads=[pa_r], writes=[o_r])
                        else:
                            E("dve", lambda o_t=o_t, pa=pa, scale=scale: nc.vector.tensor_scalar(out=o_t[:], in0=pa[:], scalar1=scale, scalar2=None, op0=ALU.mult), reads=[pa_r], writes=[o_r])
                        if ev % 2 == 0:
                            E("dsp", lambda o_t=o_t, dst=dst, tcn=tcn: nc.sync.dma_start(out=dst[:, tcn * 512:(tcn + 1) * 512], in_=o_t[:]), reads=[o_r], writes=[dres])
                        else:
                            E("dpool", lambda o_t=o_t, dst=dst, tcn=tcn: nc.gpsimd.dma_start(out=dst[:, tcn * 512:(tcn + 1) * 512], in_=o_t[:]), reads=[o_r], writes=[dres])
                    for n in range(4):
                        t0 = tcn * 512 + n * 128
                        pa, pa_r = pacc.next()
                        for dc in range(8):
                            E("pe", lambda pa=pa, dc=dc, xnT=xnT, n=n: nc.tensor.matmul(pa[:, 0:128], lhsT=xnT[:, dc, n * 128:(n + 1) * 128], rhs=Wg[:, dc, 896:1024], start=(dc == 0), stop=(dc == 7)),
                              reads=[r_Wg, xnT_r], writes=[pa_r])
                        pb, pb_r = pacc.next()
                        for dc in range(8):
                            E("pe", lambda pb=pb, dc=dc, xnT=xnT, n=n: nc.tensor.matmul(pb[:, 0:152], lhsT=xnT[:, dc, n * 128:(n + 1) * 128], rhs=Wg[:, dc, 1152:1304], start=(dc == 0), stop=(dc == 7)),
                              reads=[r_Wg, xnT_r], writes=[pb_r])
                        ov, ov_r = osv.next()
                        og, og_r = osg.next()
                        E("act", lambda ov=ov, pa=pa: nc.scalar.copy(out=ov[:, 0:128], in_=pa[:, 0:128]), reads=[pa_r], writes=[ov_r])
                        E("dve", lambda ov=ov, pb=pb: nc.vector.tensor_copy(out=ov[:, 128:256], in_=pb[:, 0:128]), reads=[pb_r], writes=[ov_r])
                        E("dve", lambda og=og, pb=pb: nc.vector.tensor_copy(out=og[:], in_=pb[:, 128:152]), reads=[pb_r], writes=[og_r])
                        E("dsp", lambda ov=ov, t0=t0: nc.sync.dma_start(out=vs_s[t0:t0 + 128, :], in_=ov[:, 0:128]), reads=[ov_r], writes=[R["vs_s"]])
                        E("dpool", lambda ov=ov, t0=t0: nc.gpsimd.dma_start(out=vw_s[t0:t0 + 128, :], in_=ov[:, 128:256]), reads=[ov_r], writes=[R["vw_s"]])
                        E("dsp", lambda og=og, t0=t0: nc.sync.dma_start(out=gates_s[t0:t0 + 128, :], in_=og[:]), reads=[og_r], writes=[R["gates_s"]])

        if "B" in phases:
            with Scope(kb) as st:
                cw = sb(st, "cw", [128, 4, 4], F32); r_cw = Res()
                lv = sb(st, "lv", [128, 5, 4], F32); r_lv = Res()
                clc = sb(st, "clc", [128, 3, 4], F32); r_clc = Res()
                bdf = sb(st, "bdf", [128, 2, 4, 128], F32); r_bdf = Res()
                bdb = sb(st, "bdb", [128, 2, 4, 128], BF16); r_bdb = Res()
                ones_b = sb(st, "ones_b", [128, 128], BF16); r_ones = Res()
                E("dsp", lambda: nc.sync.dma_start(out=cw[:], in_=lru_cw), writes=[r_cw])
                E("dact", lambda: nc.scalar.dma_start(out=lv[:], in_=lru_vec), writes=[r_lv])
                E("dsp", lambda: nc.sync.dma_start(out=bdf[:, 0], in_=lru_bda), writes=[r_bdf])
                E("dact", lambda: nc.scalar.dma_start(out=bdf[:, 1], in_=lru_bdx), writes=[r_bdf])
                E("dve", lambda: nc.vector.tensor_copy(out=bdb[:], in_=bdf[:]), reads=[r_bdf], writes=[r_bdb])
                E("dve", lambda: nc.vector.memset(ones_b[:], 1.0), writes=[r_ones])
                E("act", lambda: nc.scalar.activation(out=clc[:, 0, :], in_=lv[:, 3, :], func=AF.Exp, scale=-1.0), reads=[r_lv], writes=[r_clc])
                E("act", lambda: nc.scalar.activation(out=clc[:, 0, :], in_=clc[:, 0, :], func=AF.Ln, bias=1.0), reads=[r_clc], writes=[r_clc])
                E("dve", lambda: nc.vector.tensor_scalar(out=clc[:, 1, :], in0=clc[:, 0, :], scalar1=-8.0, scalar2=None, op0=ALU.mult), reads=[r_clc], writes=[r_clc])
                E("dve", lambda: nc.vector.tensor_scalar(out=clc[:, 2, :], in0=clc[:, 0, :], scalar1=-16.0, scalar2=None, op0=ALU.mult), reads=[r_clc], writes=[r_clc])
                L = sb(st, "Lall", [128, 4, T], F32); r_L = Res()
                X = [sb(st, f"lruX{i}", [128, T], F32) for i in range(5)]
                rX = [Res() for _ in range(5)]
                xcb = sb(st, "xcb", [128, T], BF16); r_xcb = Res()
                pg = Ring([ps(st, f"pg{i}", [128, 512], F32) for i in range(4)])
                for cc in range(4):
                    X1, X2, X3, X4, X5 = X
                    r1, r2, r3, r4, r5 = rX
                    for hh in range(2):
                        E("dsp", lambda cc=cc, hh=hh: nc.sync.dma_start(out=X1[:, hh * 2048:(hh + 1) * 2048], in_=xrT_s[cc * 128:(cc + 1) * 128, hh * 2048:(hh + 1) * 2048]), reads=[R["xrT_s"]], writes=[r1])
                        E("dact", lambda cc=cc, hh=hh: nc.scalar.dma_start(out=X3[:, hh * 2048:(hh + 1) * 2048], in_=xgT_s[cc * 128:(cc + 1) * 128, hh * 2048:(hh + 1) * 2048]), reads=[R["xgT_s"]], writes=[r3])
                    E("dve", lambda cc=cc: nc.vector.tensor_scalar(out=X2[:], in0=X1[:], scalar1=cw[:, cc, 3:4], scalar2=lv[:, 0, cc:cc + 1], op0=ALU.mult, op1=ALU.add), reads=[r1, r_cw, r_lv], writes=[r2])
                    for sh in (1, 2, 3):
                        E("dve", lambda cc=cc, sh=sh: nc.vector.scalar_tensor_tensor(out=X2[:, sh:T], in0=X1[:, 0:T - sh], scalar=cw[:, cc, 3 - sh:4 - sh], in1=X2[:, sh:T], op0=ALU.mult, op1=ALU.add), reads=[r1, r2, r_cw], writes=[r2])
                    E("pool", lambda: nc.gpsimd.tensor_copy(out=xcb[:], in_=X2[:]), reads=[r2], writes=[r_xcb])
                    for gi, (Xo, ro, bi) in enumerate(((X4, r4, 1), (X5, r5, 2))):
                        for tcn in range(8):
                            pgt, pg_r = pg.next()
                            E("pe", lambda pgt=pgt, gi=gi, cc=cc, tcn=tcn: nc.tensor.matmul(pgt[:], lhsT=bdb[:, gi, cc, :], rhs=xcb[:, tcn * 512:(tcn + 1) * 512], start=True, stop=True), reads=[r_bdb, r_xcb], writes=[pg_r])
                            E("act", lambda pgt=pgt, Xo=Xo, bi=bi, cc=cc, tcn=tcn: nc.scalar.activation(out=Xo[:, tcn * 512:(tcn + 1) * 512], in_=pgt[:], func=AF.Sigmoid, bias=lv[:, bi, cc:cc + 1]), reads=[pg_r, r_lv], writes=[ro])
                    E("act", lambda cc=cc: nc.scalar.activation(out=X1[:], in_=X4[:], func=AF.Exp, scale=clc[:, 1, cc:cc + 1]), reads=[r4, r_clc], writes=[r1])
                    E("act", lambda cc=cc: nc.scalar.activation(out=X4[:], in_=X4[:], func=AF.Exp, scale=clc[:, 2, cc:cc + 1]), reads=[r4, r_clc], writes=[r4])
                    E("act", lambda: nc.scalar.activation(out=X4[:], in_=X4[:], func=AF.Sqrt, scale=-1.0, bias=1.0), reads=[r4], writes=[r4])
                    E("pool", lambda: nc.gpsimd.tensor_tensor(out=X5[:], in0=X5[:], in1=X2[:], op=ALU.mult), reads=[r5, r2], writes=[r5])
                    E("dve", lambda: nc.vector.tensor_tensor(out=X4[:], in0=X4[:], in1=X5[:], op=ALU.mult), reads=[r4, r5], writes=[r4])
                    E("dve", lambda: nc.vector.tensor_tensor_scan(out=X2[:], data0=X1[:], data1=X4[:], initial=0.0, op0=ALU.mult, op1=ALU.add), reads=[r1, r4], writes=[r2])
                    E("act", lambda: nc.scalar.activation(out=X3[:], in_=X3[:], func=AF.Gelu_apprx_tanh), reads=[r3], writes=[r3])
                    E("pool", lambda cc=cc: nc.gpsimd.tensor_tensor(out=L[:, cc, :], in0=X2[:], in1=X3[:], op=ALU.mult), reads=[r2, r3], writes=[r_L])
                sq = Ring([sb(st, f"lsq{i}", [128, 512], BF16) for i in range(2)])
                rs_ring = Ring([sb(st, f"lrs{i}", [128, 512], F32) for i in range(2)])
                lo = Ring([sb(st, f"lo{i}", [128, 512], BF16) for i in range(3)])
                for tcn in range(8):
                    pgt, pg_r = pg.next()
                    for cc in range(4):
                        sq_t, sq_r = sq.next()
                        E("act", lambda sq_t=sq_t, cc=cc, tcn=tcn: nc.scalar.activation(out=sq_t[:], in_=L[:, cc, tcn * 512:(tcn + 1) * 512], func=AF.Square), reads=[r_L], writes=[sq_r])
                        E("pe", lambda pgt=pgt, sq_t=sq_t, cc=cc: nc.tensor.matmul(pgt[:], lhsT=ones_b[:], rhs=sq_t[:], start=(cc == 0), stop=(cc == 3)), reads=[r_ones, sq_r], writes=[pg_r])
                    rs_t, rs_r = rs_ring.next()
                    E("dve", lambda rs_t=rs_t, pgt=pgt: nc.vector.tensor_scalar(out=rs_t[:], in0=pgt[:], scalar1=1.0 / 512, scalar2=EPS, op0=ALU.mult, op1=ALU.add), reads=[pg_r], writes=[rs_r])
                    E("act", lambda rs_t=rs_t: nc.scalar.activation(out=rs_t[:], in_=rs_t[:], func=AF.Sqrt), reads=[rs_r], writes=[rs_r])
                    E("dve", lambda rs_t=rs_t: nc.vector.reciprocal(out=rs_t[:], in_=rs_t[:]), reads=[rs_r], writes=[rs_r])
                    for cc in range(4):
                        lo_t, lo_r = lo.next()
                        E("dve", lambda lo_t=lo_t, rs_t=rs_t, cc=cc, tcn=tcn: nc.vector.scalar_tensor_tensor(out=lo_t[:], in0=L[:, cc, tcn * 512:(tcn + 1) * 512], scalar=lv[:, 4, cc:cc + 1], in1=rs_t[:], op0=ALU.mult, op1=ALU.mult), reads=[r_L, rs_r, r_lv], writes=[lo_r])
                        E("dsp", lambda lo_t=lo_t, cc=cc, tcn=tcn: nc.sync.dma_start(out=mixT_s[512 + cc * 128:512 + (cc + 1) * 128, tcn * 512:(tcn + 1) * 512], in_=lo_t[:]), reads=[lo_r], writes=[R["mixT_s"]])

        if "C" in phases:
            with Scope(kb) as st:
                Aout = sb(st, "Aout", [128, NT, 512], BF16)
                rA = [Res() for _ in range(NT)]
                sig = sb(st, "sig", [128, NT, 24], F32); r_sig = Res()
                force_t = sb(st, "force_t", [128, NT, 64], F32); r_force = Res()
                keep_t = sb(st, "keep_t", [128, NT, 64], F32); r_keep = Res()
                t31 = sb(st, "t31", [128, 8], F32); r_t31 = Res()
                BD = sb(st, "BD", [128, 8, 3, 128], BF16); r_BD = Res()
                ovl_t = sb(st, "ovl_t", [128, 2, 65], F32); r_ovl = Res()
                ga_t = sb(st, "ga_t", [128, 512], F32); r_ga = Res()
                bcm = sb(st, "bcm", [128, 5, 512], F32); r_bcm = Res()
                DMA("dsp", sig[:], gates_s.rearrange("(n p) c -> p n c", p=128), [R["gates_s"]], [r_sig])
                ACTF(sig[:], sig[:], AF.Sigmoid, [r_sig], [r_sig])
                DMA("dact", force_t[:], force_in, [], [r_force])
                DMA("dsp", keep_t[:], keep_in, [], [r_keep])
                DMA("dact", t31[:], t31_in, [], [r_t31])
                DMA("dsp", ovl_t[:], ovl_ext, [], [r_ovl])
                DMA("dact", ga_t[:], ga_in, [], [r_ga])
                DMA("dsp", bcm[:], bc_m, [], [r_bcm])
                psb = [ps(st, f"pC{i}", [128, 512], F32) for i in range(8)]
                pS = Ring(psb[0:2])
                pO = psb[2:6]; r_pO = [Res() for _ in range(4)]
                pX = Ring(psb[6:8])
                with Scope(kb) as st2:
                    bdg = sb(st2, "bdg", [128, 8, 2, 128], F32); r_bdg = Res()
                    bdm = sb(st2, "bdm", [128, 3, 128], F32); r_bdm = Res()
                    DMA("dsp", bdg[:], bd_g.rearrange("h p j t -> p h j t"), [], [r_bdg])
                    DMA("dact", bdm[:], bd_m, [], [r_bdm])
                    for hg in range(8):
                        for j in range(2):
                            STT(BD[:, hg, j, :], bdg[:, hg, j, :], t31[:, hg:hg + 1], bdm[:, j, :], ALU.subtract, ALU.add, [r_bdg, r_bdm, r_t31], [r_BD])
                        CP("dve", BD[:, hg, 2, :], bdm[:, 2, :], [r_bdm], [r_BD])
                P_ring = Ring([sb(st, f"Pt{i}", [128, 512], BF16) for i in range(3)])
                sm = Ring([sb(st, f"smC{i}", [128, 8], F32) for i in range(8)])

                def finish_tile(po, po_r, ncol, i, hg, br, first, imp=None):
                    s_t, s_r = sm.next()
                    TS("dve", s_t[:, 0:1], po[:, ncol:ncol + 1], 1e-30, None, ALU.max, None, [po_r], [s_r])
                    E("dve", lambda: nc.vector.reciprocal(out=s_t[:, 1:2], in_=s_t[:, 0:1]), [s_r], [s_r])
                    TT("dve", s_t[:, 2:3], s_t[:, 1:2], sig[:, i, hg * 3 + br:hg * 3 + br + 1], ALU.mult, [s_r, r_sig], [s_r])
                    dst = Aout[:, i, hg * 64:(hg + 1) * 64]
                    if first:
                        TS("dve", dst, po[:, 0:64], s_t[:, 2:3], None, ALU.mult, None, [po_r, s_r], [rA[i]])
                    else:
                        STT(dst, po[:, 0:64], s_t[:, 2:3], dst, ALU.mult, ALU.add, [po_r, s_r, rA[i]], [rA[i]])
                    if imp is not None:
                        imp_t, imp_r, imp_first = imp
                        if imp_first:
                            TS("dve", imp_t, po[:, 64:128], s_t[:, 1:2], None, ALU.mult, None, [po_r, s_r], [imp_r])
                        else:
                            STT(imp_t, po[:, 64:128], s_t[:, 1:2], imp_t, ALU.mult, ALU.add, [po_r, s_r, imp_r], [imp_r])

                for k in range(2):
                    with Scope(kb) as stg:
                        KcmpT = sb(stg, "KcmpT", [64, 256], BF16); r_Kc = Res()
                        Vco = sb(stg, "Vco", [128, 2, 129], BF16); r_Vco = Res()
                        with Scope(kb) as stc:
                            w1s = Ring([sb(stc, f"w1s{i}", [128, 8, 256], F32) for i in range(2)])
                            w1b = sb(stc, "w1b", [128, 2, 16, 256], BF16); r_w1b = Res()
                            pes = sb(stc, "pes", [128, 2, 16], F32); r_pes = Res()
                            peb = sb(stc, "peb", [128, 2, 16], BF16); r_peb = Res()
                            w2s = sb(stc, "w2s", [128, 2, 2, 64], F32); r_w2s = Res()
                            w2b = sb(stc, "w2b", [128, 2, 2, 64], BF16); r_w2b = Res()
                            stk = sb(stc, "stk", [128, 2, T], BF16); r_stk = Res()
                            hb = sb(stc, "hb", [128, 4], F32); r_hb = Res()
                            gh = sb(stc, "gh", [128, 2, 2, 256], BF16); r_gh = Res()
                            for kv in range(2):
                                for hh in range(2):
                                    w_t, w_r = w1s.next()
                                    DMA("dsp" if hh == 0 else "dact", w_t[:], cmp_w1[kv, :, hh * 8:(hh + 1) * 8, :], [], [w_r])
                                    CP("pool" if hh == 0 else "dve", w1b[:, kv, hh * 8:(hh + 1) * 8, :], w_t[:], [w_r], [r_w1b])
                                DMA("dsp", pes[:, kv, :], cmp_pe[kv], [], [r_pes])
                                DMA("dact", w2s[:, kv], cmp_w2[kv], [], [r_w2s])
                                src = kcT_s if kv == 0 else vcT_s
                                sres = R["kcT_s"] if kv == 0 else R["vcT_s"]
                                DMA("dsp", stk[0:64, kv, :], src[k * 64:(k + 1) * 64, :], [sres], [r_stk])
                                MSET("pool", stk[64:128, kv, T - 1:T], 0.0, [r_stk])
                                DMA("dact", stk[64:128, kv, 0:T - 1], src[k * 64:(k + 1) * 64, 1:T], [sres], [r_stk])
                            CP("dve", peb[:], pes[:], [r_pes], [r_peb])
                            CP("dve", w2b[:], w2s[:], [r_w2s], [r_w2b])
                            MSET("pool", gh[:], 0.0, [r_gh])
                            for kv in range(2):
                                for hh in range(2):
                                    px, px_r = pX.next()
                                    for m in range(16):
                                        MM(px[:, 0:1], w1b[:, kv, m, hh * 128:(hh + 1) * 128], peb[:, kv, m:m + 1], m == 0, m == 15, [r_w1b, r_peb], [px_r])
                                    CP("dve", hb[:, kv * 2 + hh:kv * 2 + hh + 1], px[:, 0:1], [px_r], [r_hb])
                                    p_s, p_r = pS.next()
                                    for m in range(16):
                                        MM(p_s[:, 0:255], w1b[:, kv, m, hh * 128:(hh + 1) * 128], stk[:, kv, 2 * m:2 * m + 16 * 254 + 1:16], m == 0, m == 15, [r_w1b, r_stk], [p_r])
                                    ACTF(gh[:, kv, hh, 0:255], p_s[:, 0:255], AF.Gelu_apprx_tanh, [p_r, r_hb], [r_gh], bias=hb[:, kv * 2 + hh:kv * 2 + hh + 1])
                            px, px_r = pX.next()
                            for hh in range(2):
                                MM(px[0:64, 0:256], w2b[:, 0, hh, :], gh[:, 0, hh, :], hh == 0, hh == 1, [r_w2b, r_gh], [px_r])
                            CP("dve", KcmpT[:], px[0:64, 0:256], [px_r], [r_Kc])
                            for ct in range(2):
                                px, px_r = pX.next()
                                for hh in range(2):
                                    MM(px[:, 0:64], gh[:, 1, hh, ct * 128:(ct + 1) * 128], w2b[:, 1, hh, :], hh == 0, hh == 1, [r_gh, r_w2b], [px_r])
                                CP("dve", Vco[:, ct, 0:64], px[:, 0:64], [px_r], [r_Vco])
                            CP("pool", Vco[:, :, 64:129], ovl_t[:], [r_ovl], [r_Vco])
                            if k == 0:
                                dump("d_kcmp", KcmpT[:], [64, 256], BF16, r_Kc)
                                dump("d_vco", Vco[:], [128, 2, 129], BF16, r_Vco)
                                dump("d_hb", hb[:], [128, 4], F32, r_hb)
                                dump("d_gh", gh[:], [128, 2, 2, 256], BF16, r_gh)

                        QT = sb(stg, "QT", [128, 4, T], BF16)
                        r_QT = [Res() for _ in range(4)]
                        r_QM = [[Res() for _ in range(NT)] for _ in range(4)]
                        KsT = sb(stg, "KsT", [128, T], BF16); r_KsT = Res()
                        KwT = sb(stg, "KwT", [64, T], BF16); r_KwT = Res()
                        Vs = sb(stg, "Vs", [128, NT, 65], BF16); r_Vs = Res()
                        Vw = sb(stg, "Vw", [128, NT, 65], BF16); r_Vw = Res()
                        imp_acc = sb(stg, "imp_acc", [128, NT, 64], F32)
                        r_imp = [Res() for _ in range(NT)]
                        for g in range(4):
                            hg = 4 * k + g
                            DMA("dsp" if g % 2 == 0 else "dact", QT[0:64, g, :], qT_s[hg * 64:(hg + 1) * 64, :], [R["qT_s"]], [r_QT[g]])
                        DMA("dsp", KsT[0:64, :], ksT_s[k * 64:(k + 1) * 64, :], [R["ksT_s"]], [r_KsT])
                        with Scope(kb) as ste:
                            ers = sb(ste, "ers", [128, T], F32); r_ers = Res()
                            DMA("dact", ers[64:128, :], erows, [], [r_ers])
                            CP("pool", KsT[64:128, :], ers[64:128, :], [r_ers], [r_KsT])
                        DMA("dact", KwT[:], kwT_s[k * 64:(k + 1) * 64, :], [R["kwT_s"]], [r_KwT])
                        DMA("dsp", Vs[:, :, 0:64], vs_s.rearrange("(n p) c -> p n c", p=128)[:, :, k * 64:(k + 1) * 64], [R["vs_s"]], [r_Vs])
                        DMA("dact", Vw[:, :, 0:64], vw_s.rearrange("(n p) c -> p n c", p=128)[:, :, k * 64:(k + 1) * 64], [R["vw_s"]], [r_Vw])
                        MSET("pool", Vs[:, :, 64:65], 1.0, [r_Vs])
                        MSET("pool", Vw[:, :, 64:65], 1.0, [r_Vw])

                        bcs = Ring([sb(stg, f"bcs{i}", [128, 5, 512], F32) for i in range(2)])
                        BC = Ring([sb(stg, f"BCb{i}", [128, 5, 512], BF16) for i in range(2)])
                        for g in range(4):
                            hg = 4 * k + g
                            bs_t, bs_r = bcs.next()
                            DMA("dsp", bs_t[:, 0:3], bc_g[hg, :, 0:3], [], [bs_r])
                            DMA("dact", bs_t[:, 3:5], bc_g[hg, :, 3:5], [], [bs_r])
                            bc_t, bc_r = BC.next()
                            for m in range(5):
                                STT(bc_t[:, m, :], bs_t[:, m, :], t31[:, hg:hg + 1], bcm[:, m, :], ALU.subtract, ALU.add, [bs_r, r_bcm, r_t31], [bc_r])
                            for tcn in range(8):
                                cts = [0] if tcn < 4 else [0, 1]
                                for ct in cts:
                                    mp = tcn - 4 * ct
                                    p_s, p_r = pS.next()
                                    MM(p_s[:], KcmpT[:, ct * 128:(ct + 1) * 128], QT[0:64, g, tcn * 512:(tcn + 1) * 512], True, mp >= 5, [r_Kc, r_QT[g]], [p_r])
                                    if mp < 5:
                                        MM(p_s[:], ident_b[:], bc_t[:, mp, :], False, True, [r_identb, bc_r], [p_r])
                                    P_t, P_r = P_ring.next()
                                    ACTF(P_t[:], p_s[:], AF.Exp, [p_r, r_t31], [P_r], bias=t31[:, hg:hg + 1])
                                    for q in range(4):
                                        MM(pO[q][:, 0:129], P_t[:, q * 128:(q + 1) * 128], Vco[:, ct, :], ct == 0, ct == cts[-1], [P_r, r_Vco], [r_pO[q]])
                                for q in range(4):
                                    i = 4 * tcn + q
                                    finish_tile(pO[q], r_pO[q], 128, i, hg, 0, True, imp=(imp_acc[:, i, :], r_imp[i], g == 0))

                        if k == 0:
                            dump("d_imp", imp_acc[:], [128, NT, 64], F32, r_imp)
                            dump("d_aout_c", Aout[:], [128, NT, 512], BF16, rA)
                        MBr = Ring([sb(stg, f"MB{i}", [128, 128], F32) for i in range(2)])
                        for (mb_t, mb_r) in zip(MBr.tiles, MBr.res):
                            MSET("dve", mb_t[:], 0.0, [mb_r])
                        tk = Ring([sb(stg, f"tk{i}", [128, 2, 64], F32) for i in range(2)])
                        mxr = Ring([sb(stg, f"mx{i}", [128, 16], F32) for i in range(2)])
                        mtr = Ring([sb(stg, f"mtr{i}", [128, 128], BF16) for i in range(2)])
                        for i in range(NT):
                            tk_t, tk_r = tk.next()
                            mx_t, mx_r = mxr.next()
                            TT("dve", tk_t[:, 0, :], imp_acc[:, i, :], keep_t[:, i, :], ALU.mult, [r_imp[i], r_keep], [tk_r])
                            TT("dve", tk_t[:, 0, :], tk_t[:, 0, :], force_t[:, i, :], ALU.add, [tk_r, r_force], [tk_r])
                            E("dve", lambda: nc.vector.max(out=mx_t[:, 0:8], in_=tk_t[:, 0, :]), [tk_r], [mx_r])
                            E("dve", lambda: nc.vector.match_replace(out=tk_t[:, 1, :], in_to_replace=mx_t[:, 0:8], in_values=tk_t[:, 0, :], imm_value=-1e30), [tk_r, mx_r], [tk_r])
                            E("dve", lambda: nc.vector.max(out=mx_t[:, 8:16], in_=tk_t[:, 1, :]), [tk_r], [mx_r])
                            mb_t, mb_r = MBr.next()
                            TS("dve", mb_t[:, 64:128], tk_t[:, 0, :], mx_t[:, 15:16], None, ALU.is_ge, None, [tk_r, mx_r], [mb_r])
                            TS("dve", mb_t[:, 64:128], mb_t[:, 64:128], 1.0, -NEGM, ALU.subtract, ALU.mult, [mb_r], [mb_r])
                            px, px_r = pX.next()
                            TR(px[:, 0:128], mb_t[:], ident_f[:], [mb_r, r_identf], [px_r])
                            mt_t, mt_r = mtr.next()
                            CP("act", mt_t[64:128, :], px[64:128, 0:128], [px_r], [mt_r])
                            for g in range(4):
                                CP("pool" if g % 2 == 0 else "dve", QT[64:128, g, i * 128:(i + 1) * 128], mt_t[64:128, :], [mt_r], [r_QM[g][i]])

                        if k == 0:
                            dump("d_qt0", QT[:, 0, :], [128, T], BF16, r_QT + [x for l in r_QM for x in l])
                        for g in range(4):
                            hg = 4 * k + g
                            for br in (1, 2):
                                for tcn in range(8):
                                    j_lo = 0 if br == 1 else max(0, 4 * tcn - 4)
                                    j_hi = 4 * tcn + 3
                                    for j in range(j_lo, j_hi + 1):
                                        qa = max(0, j - 4 * tcn)
                                        qb = 3 if br == 1 else min(3, j + 4 - 4 * tcn)
                                        c0, c1 = qa * 128, (qb + 1) * 128
                                        t0 = tcn * 512
                                        adds = []
                                        for q in range(qa, qb + 1):
                                            dlt = 4 * tcn + q - j
                                            if dlt == 0:
                                                adds.append((q, 0))
                                            elif dlt == 1:
                                                adds.append((q, 1))
                                            elif dlt == 4 and br == 2:
                                                adds.append((q, 2))
                                        p_s, p_r = pS.next()
                                        if br == 1:
                                            rd = [r_KsT, r_QT[g]] + [r_QM[g][4 * tcn + q] for q in range(qa, qb + 1)]
                                            MM(p_s[:, c0:c1], KsT[:, j * 128:(j + 1) * 128], QT[:, g, t0 + c0:t0 + c1], True, len(adds) == 0, rd, [p_r])
                                        else:
                                            MM(p_s[:, c0:c1], KwT[:, j * 128:(j + 1) * 128], QT[0:64, g, t0 + c0:t0 + c1], True, len(adds) == 0, [r_KwT, r_QT[g]], [p_r])
                                        for ai, (q, ty) in enumerate(adds):
                                            MM(p_s[:, q * 128:(q + 1) * 128], ident_b[:], BD[:, hg, ty, :], False, ai == len(adds) - 1, [r_identb, r_BD], [p_r])
                                        P_t, P_r = P_ring.next()
                                        ACTF(P_t[:, c0:c1], p_s[:, c0:c1], AF.Exp, [p_r, r_t31], [P_r], bias=t31[:, hg:hg + 1])
                                        Vx, r_Vx = (Vs, r_Vs) if br == 1 else (Vw, r_Vw)
                                        for q in range(qa, qb + 1):
                                            i = 4 * tcn + q
                                            first_j = 0 if br == 1 else max(0, i - 4)
                                            MM(pO[q][:, 0:65], P_t[:, q * 128:(q + 1) * 128], Vx[:, j, :], j == first_j, j == i, [P_r, r_Vx], [r_pO[q]])
                                    for q in range(4):
                                        i = 4 * tcn + q
                                        finish_tile(pO[q], r_pO[q], 64, i, hg, br, False)

                dump("d_aout", Aout[:], [128, NT, 512], BF16, rA)
                with Scope(kb) as stn:
                    junkC = sb(stn, "junkC", [128, 512], BF16); r_junkC = Res()
                    an = Ring([sb(stn, f"an{i}", [128, 512], BF16) for i in range(2)])
                    af = Ring([sb(stn, f"af{i}", [128, 512], F32) for i in range(2)])
                    ao = Ring([sb(stn, f"ao{i}", [128, 512], BF16) for i in range(2)])
                    pTb = Ring([ps(stn, f"pTC{i}", [128, 512], BF16) for i in range(2)]) if False else None
                    for i in range(NT):
                        s_t, s_r = sm.next()
                        ACTF(junkC[:], Aout[:, i, :], AF.Square, [rA[i]], [r_junkC, s_r], accum_out=s_t[:, 0:1])
                        TS("dve", s_t[:, 1:2], s_t[:, 0:1], 1.0 / 512, EPS, ALU.mult, ALU.add, [s_r], [s_r])
                        ACTF(s_t[:, 1:2], s_t[:, 1:2], AF.Sqrt, [s_r], [s_r])
                        E("dve", lambda: nc.vector.reciprocal(out=s_t[:, 2:3], in_=s_t[:, 1:2]), [s_r], [s_r])
                        af_t, af_r = af.next()
                        STT(af_t[:], Aout[:, i, :], s_t[:, 2:3], ga_t[:], ALU.mult, ALU.mult, [rA[i], s_r, r_ga], [af_r])
                        px, px_r = pX.next()
                        for fc in range(4):
                            TR(px[:, fc * 128:(fc + 1) * 128], af_t[:, fc * 128:(fc + 1) * 128], ident_f[:], [af_r, r_identf], [px_r])
                        ao_t, ao_r = ao.next()
                        CP("act", ao_t[:], px[:], [px_r], [ao_r])
                        DMA("dsp" if i % 2 == 0 else "dpool", mixT_s[0:512, i * 128:(i + 1) * 128].rearrange("(f p) t -> p f t", p=128),
                            ao_t[:].rearrange("p (f t) -> p f t", f=4), [ao_r], [R["mixT_s"]])

        if "D" in phases:
            NTL = TH // 128
            with Scope(kb) as st:
                Wo = sb(st, "Wo", [128, 8, D], BF16); r_Wo = Res()
                Wq = sb(st, "Wq", [128, 8, 2048], BF16); r_Wq = Res()
                skb = sb(st, "skb", [128, 2, 128], BF16); r_skb = Res()
                repf = sb(st, "repf", [128, D], F32); r_rep = Res()
                io16 = sb(st, "io16", [128, 16], F32); r_io = Res()
                io128 = sb(st, "io128", [128, 128], F32); r_io128 = Res()
                selt = sb(st, "selt", [128, 2], F32); r_sel = Res()
                DMA("dsp", repf[:], rep4[:, 0, :], [], [r_rep])
                DMA("dact", io16[:], iota16, [], [r_io])
                DMA("dact", io128[:], iota128, [], [r_io128])
                DMA("dact", selt[:], selc, [], [r_sel])
                with Scope(kb) as stw:
                    wst = Ring([sb(stw, f"wstD{i}", [128, 2048], F32) for i in range(3)])
                    n = 0
                    for (src, dstw, dres, ncol, nch) in ((w_out_in, Wo, r_Wo, D, 8), (peer_wq, Wq, r_Wq, 2048, 8)):
                        for dc in range(nch):
                            w_t, w_r = wst.next()
                            n += 1
                            DMA("dsp" if n % 2 == 0 else "dact", w_t[:, 0:ncol], src[dc * 128:(dc + 1) * 128, :], [], [w_r])
                            CP("dve" if n % 2 == 0 else "pool", dstw[:, dc, :], w_t[:, 0:ncol], [w_r], [dres])
                    w_t, w_r = wst.next()
                    DMA("dsp", w_t[:, 0:256].rearrange("p (a k) -> p a k", a=2), sk_T.rearrange("a p k -> p a k"), [], [w_r])
                    CP("dve", skb[:], w_t[:, 0:256].rearrange("p (a k) -> p a k", a=2), [w_r], [r_skb])

                pacc = Ring([ps(st, f"pD{i}", [128, 512], F32) for i in range(4)])
                pw_ring = Ring([ps(st, f"pDw{i}", [128, 512], F32) for i in range(2)])
                ptb = Ring([ps(st, f"pDb{i}", [128, 1024], BF16) for i in range(2)])
                mst = Ring([sb(st, f"mst{i}", [128, 8, 2, 128], BF16) for i in range(2)])
                mixh_ring = Ring([sb(st, f"mixh{i}", [128, 8, 128], BF16) for i in range(2)])
                xh_ring = Ring([sb(st, f"xhD{i}", [128, D], F32) for i in range(2)])
                H_ring = Ring([sb(st, f"HD{i}", [128, D], F32) for i in range(2)])
                xng_ring = Ring([sb(st, f"xng{i}", [128, D], F32) for i in range(1)])
                xnb_ring = Ring([sb(st, f"xnbD{i}", [128, D], BF16) for i in range(1)])
                xT_ring = Ring([sb(st, f"xTD{i}", [128, 8, 128], BF16) for i in range(2)])
                qTb = sb(st, "qTb", [128, 16, 128], BF16); r_qTb = Res()
                Ssc = sb(st, "Ssc", [128, 16, 128], F32); r_S = Res()
                Swk = sb(st, "Swk", [128, 128], F32); r_Swk = Res()
                v16 = sb(st, "v16", [128, 16, 16], F32); r_v16 = Res()
                i16 = sb(st, "i16", [128, 16, 16], U32); r_i16 = Res()
                i16f = sb(st, "i16f", [128, 16, 16], F32); r_i16f = Res()
                cand = sb(st, "cand", [128, 8, 256], F32); r_cand = Res()
                cwk = sb(st, "cwk", [128, 256], F32); r_cwk = Res()
                sc16 = sb(st, "sc16", [128, 8, 16], F32); r_sc = Res()
                ci16 = sb(st, "ci16", [128, 8, 16], U32); r_ci = Res()
                ab_u = sb(st, "ab_u", [128, 2, 8, 16], U32); r_abu = Res()
                ab_f = sb(st, "ab_f", [128, 2, 8, 16], F32); r_abf = Res()
                eq = sb(st, "eq", [128, 8, 16, 16], F32); r_eq = Res()
                isel = sb(st, "isel", [128, 3, 8, 16], F32); r_isel = Res()
                gz = sb(st, "gz", [128, 16], F32); r_gz = Res()
                junkB = sb(st, "junkDb", [128, D], BF16); r_junkB = Res()
                smD = Ring([sb(st, f"smD{i}", [128, 8], F32) for i in range(4)])
                ijgT = sb(st, "ijgT", [128, 3, 128], F32); r_ijgT = Res()
                OI = Ring([sb(st, f"OI{i}", [128, 128], BF16) for i in range(6)])
                OJ = Ring([sb(st, f"OJ{i}", [128, 128], BF16) for i in range(6)])
                Wst = sb(st, "Wst", [128, 128, 128], BF16); r_Wst = Res()

                def rms_scaled(src, src_r, gain, gain_r, dstf, dstf_r):
                    s_t, s_r = smD.next()
                    ACTF(junkB[:], src, AF.Square, [src_r], [r_junkB, s_r], accum_out=s_t[:, 0:1])
                    TS("dve", s_t[:, 1:2], s_t[:, 0:1], 1.0 / D, EPS, ALU.mult, ALU.add, [s_r], [s_r])
                    ACTF(s_t[:, 1:2], s_t[:, 1:2], AF.Sqrt, [s_r], [s_r])
                    E("dve", lambda: nc.vector.reciprocal(out=s_t[:, 2:3], in_=s_t[:, 1:2]), [s_r], [s_r])
                    STT(dstf, src, s_t[:, 2:3], gain, ALU.mult, ALU.mult, [src_r, s_r, gain_r], [dstf_r])

                def transpose8(srcb, srcb_r, dstT, dstT_r, nblk=8):
                    pt, pt_r = ptb.next()
                    for dc in range(nblk):
                        TR(pt[:, dc * 128:(dc + 1) * 128], srcb[:, dc * 128:(dc + 1) * 128], ident_b[:], [srcb_r, r_identb], [pt_r])
                    CP("act", dstT.rearrange("p a t -> p (a t)"), pt[:, 0:nblk * 128], [pt_r], [dstT_r])

                for it in range(NTL):
                    tsl = slice(it * 128, (it + 1) * 128)
                    xh_t, xh_r = xh_ring.next()
                    DMA("dsp", xh_t[:], xh[tsl, :], [], [xh_r])
                    H, H_r = H_ring.next()
                    m_t, m_r = mst.next()
                    for a in range(2):
                        DMA("dsp" if a == 0 else "dact", m_t[:, :, a, :], mixT_s[:, a * TH + it * 128:a * TH + (it + 1) * 128].rearrange("(f p) t -> p f t", p=128), [R["mixT_s"]], [m_r])
                    mixh, r_mixh = mixh_ring.next()
                    TS("pool", mixh[:], m_t[:, :, 0, :], selt[:, 0:1], None, ALU.mult, None, [m_r, r_sel], [r_mixh])
                    STT(mixh[:], m_t[:, :, 1, :], selt[:, 1:2], mixh[:], ALU.mult, ALU.add, [m_r, r_sel, r_mixh], [r_mixh])
                    for ch in range(2):
                        pa, pa_r = pacc.next()
                        for fc in range(8):
                            MM(pa[:], mixh[:, fc, :], Wo[:, fc, ch * 512:(ch + 1) * 512], fc == 0, fc == 7, [r_mixh, r_Wo], [pa_r])
                        TT("dve", H[:, ch * 512:(ch + 1) * 512], pa[:], xh_t[:, ch * 512:(ch + 1) * 512], ALU.add, [pa_r, xh_r], [H_r])
                    DMA("dpool", H1_s[tsl, :], H[:], [H_r], [R["H1_s"]])
                    xng, xng_r = xng_ring.next()
                    rms_scaled(H[:], H_r, repf[:], r_rep, xng[:], xng_r)
                    xnb, xnb_r = xnb_ring.next()
                    CP("pool", xnb[:], xng[:], [xng_r], [xnb_r])
                    xT, xT_r = xT_ring.next()
                    transpose8(xnb, xnb_r, xT[:], xT_r)
                    DMA("dact", xnT2_s[:, :, tsl], xT[:], [xT_r], [R["xnT2_s"]])
                    for grp in range(4):
                        pa, pa_r = pacc.next()
                        for j in range(4):
                            hp = grp * 4 + j
                            for dc in range(8):
                                MM(pa[:, j * 128:(j + 1) * 128], Wq[:, dc, hp * 128:(hp + 1) * 128], xT[:, dc, :], dc == 0, dc == 7, [r_Wq, xT_r], [pa_r])
                        CP("act" if grp % 2 == 0 else "dve", qTb[:, grp * 4:(grp + 1) * 4, :].rearrange("p a t -> p (a t)"), pa[:], [pa_r], [r_qTb])
                    for grp in range(4):
                        pa, pa_r = pacc.next()
                        for j in range(4):
                            hp = grp * 4 + j
                            MM(pa[:, j * 128:(j + 1) * 128], qTb[:, hp, :], skb[:, hp % 2, :], True, True, [r_qTb, r_skb], [pa_r])
                        CP("act" if grp % 2 == 0 else "dve", Ssc[:, grp * 4:(grp + 1) * 4, :].rearrange("p a t -> p (a t)"), pa[:], [pa_r], [r_S])
                    for hp in range(16):
                        E("dve", lambda: nc.vector.max(out=v16[:, hp, 0:8], in_=Ssc[:, hp, :]), [r_S], [r_v16])
                        E("dve", lambda: nc.vector.max_index(out=i16[:, hp, 0:8], in_max=v16[:, hp, 0:8], in_values=Ssc[:, hp, :]), [r_S, r_v16], [r_i16])
                        E("dve", lambda: nc.vector.match_replace(out=Swk[:], in_to_replace=v16[:, hp, 0:8], in_values=Ssc[:, hp, :], imm_value=-1e30), [r_S, r_v16], [r_Swk])
                        E("dve", lambda: nc.vector.max(out=v16[:, hp, 8:16], in_=Swk[:]), [r_Swk], [r_v16])
                        E("dve", lambda: nc.vector.max_index(out=i16[:, hp, 8:16], in_max=v16[:, hp, 8:16], in_values=Swk[:]), [r_Swk, r_v16], [r_i16])
                    CP("dve", i16f[:], i16[:], [r_i16], [r_i16f])
                    v4 = v16[:].rearrange("p (h two) k -> p h two k", two=2)
                    in0 = v4[:, :, 0, :].rearrange("p h (a o) -> p h a o", o=1).to_broadcast([128, 8, 16, 16])
                    in1 = v4[:, :, 1, :].rearrange("p h (o b) -> p h o b", o=1).to_broadcast([128, 8, 16, 16])
                    TT("dve", cand[:].rearrange("p h (a b) -> p h a b", a=16), in0, in1, ALU.add, [r_v16], [r_cand])
                    for h in range(8):
                        E("dve", lambda: nc.vector.max(out=sc16[:, h, 0:8], in_=cand[:, h, :]), [r_cand], [r_sc])
                        E("dve", lambda: nc.vector.max_index(out=ci16[:, h, 0:8], in_max=sc16[:, h, 0:8], in_values=cand[:, h, :]), [r_cand, r_sc], [r_ci])
                        E("dve", lambda: nc.vector.match_replace(out=cwk[:], in_to_replace=sc16[:, h, 0:8], in_values=cand[:, h, :], imm_value=-1e30), [r_cand, r_sc], [r_cwk])
                        E("dve", lambda: nc.vector.max(out=sc16[:, h, 8:16], in_=cwk[:]), [r_cwk], [r_sc])
                        E("dve", lambda: nc.vector.max_index(out=ci16[:, h, 8:16], in_max=sc16[:, h, 8:16], in_values=cwk[:]), [r_cwk, r_sc], [r_ci])
                    E("dve", lambda: nc.vector.tensor_single_scalar(out=ab_u[:, 0], in_=ci16[:], scalar=4, op=ALU.logical_shift_right), [r_ci], [r_abu])
                    E("dve", lambda: nc.vector.tensor_single_scalar(out=ab_u[:, 1], in_=ci16[:], scalar=15, op=ALU.bitwise_and), [r_ci], [r_abu])
                    CP("dve", ab_f[:], ab_u[:], [r_abu], [r_abf])
                    i4 = i16f[:].rearrange("p (h two) k -> p h two k", two=2)
                    for w in range(2):
                        a_b = ab_f[:, w].rearrange("p h (k o) -> p h k o", o=1).to_broadcast([128, 8, 16, 16])
                        io_b = io16[:].rearrange("p (o q a) -> p o q a", o=1, q=1).to_broadcast([128, 8, 16, 16])
                        TT("dve", eq[:], a_b, io_b, ALU.is_equal, [r_abf, r_io], [r_eq])
                        iv_b = i4[:, :, w, :].rearrange("p h (o a) -> p h o a", o=1).to_broadcast([128, 8, 16, 16])
                        TT("dve", eq[:], eq[:], iv_b, ALU.mult, [r_eq, r_i16f], [r_eq])
                        E("dve", lambda: nc.vector.tensor_reduce(out=isel[:, w], in_=eq[:], axis=AX.X, op=ALU.add), [r_eq], [r_isel])
                    TT("dve", isel[:, 2], sc16[:], sc16[:, :, 0:1].to_broadcast([128, 8, 16]), ALU.subtract, [r_sc], [r_isel])
                    ACTF(isel[:, 2], isel[:, 2], AF.Exp, [r_isel], [r_isel])
                    E("dve", lambda: nc.vector.tensor_reduce(out=gz[:, 0:8], in_=isel[:, 2], axis=AX.X, op=ALU.add), [r_isel], [r_gz])
                    E("dve", lambda: nc.vector.reciprocal(out=gz[:, 8:16], in_=gz[:, 0:8]), [r_gz], [r_gz])
                    TT("dve", isel[:, 2], isel[:, 2], gz[:, 8:16].rearrange("p (h o) -> p h o", o=1).to_broadcast([128, 8, 16]), ALU.mult, [r_isel, r_gz], [r_isel])
                    pa, pa_r = pacc.next()
                    for w in range(3):
                        TR(pa[:, w * 128:(w + 1) * 128], isel[:, w].rearrange("p h k -> p (h k)"), ident_f[:], [r_isel, r_identf], [pa_r])
                    CP("act", ijgT[:].rearrange("p a t -> p (a t)"), pa[:, 0:384], [pa_r], [r_ijgT])
                    for tq in range(32):
                        pw, pw_r = pw_ring.next()
                        for u in range(4):
                            t = tq * 4 + u
                            oi, oi_r = OI.next()
                            oj, oj_r = OJ.next()
                            TS("pool", oi[:], io128[:], ijgT[:, 0, t:t + 1], None, ALU.is_equal, None, [r_io128, r_ijgT], [oi_r])
                            TS("dve", oj[:], io128[:], ijgT[:, 1, t:t + 1], ijgT[:, 2, t:t + 1], ALU.is_equal, ALU.mult, [r_io128, r_ijgT], [oj_r])
                            MM(pw[:, u * 128:(u + 1) * 128], oj[:], oi[:], True, True, [oj_r, oi_r], [pw_r])
                        CP("act", Wst[:, :, tq * 4:(tq + 1) * 4].rearrange("p i t -> p t i"), pw[:].rearrange("p (t i) -> p t i", t=4), [pw_r], [r_Wst])
                    for qd in range(4):
                        DMA(("dsp", "dact", "dpool", "dsp")[qd], Wt_s[qd * 32:(qd + 1) * 32, :, tsl].rearrange("i j t -> j i t"), Wst[:, qd * 32:(qd + 1) * 32, :], [r_Wst], [R["Wt_s"]])

            with Scope(kb) as st:
                Yacc = sb(st, "Yacc", [128, NTL, D], F32)
                rY = [Res() for _ in range(NTL)]
                H1v = H1_s.rearrange("(n p) d -> p n d", p=128)
                for n4 in range(4):
                    DMA("dsp" if n4 % 2 == 0 else "dact", Yacc[:, n4 * 4:(n4 + 1) * 4, :], H1v[:, n4 * 4:(n4 + 1) * 4, :], [R["H1_s"]], rY[n4 * 4:(n4 + 1) * 4])
                p1 = Ring([ps(st, f"pE1{i}", [128, 512], F32) for i in range(3)])
                p2 = Ring([ps(st, f"pE2{i}", [128, 512], F32) for i in range(3)])
                ptb2 = Ring([ps(st, f"pEb{i}", [128, 1024], BF16) for i in range(2)])
                with Scope(kb) as st2:
                    xnTa = sb(st2, "xnTa", [128, 8, TH], BF16); r_xnTa = Res()
                    for dc in range(8):
                        DMA("dsp" if dc % 2 == 0 else "dact", xnTa[:, dc, :], xnT2_s[:, dc, :], [R["xnT2_s"]], [r_xnTa])
                    IB = 8
                    ust = Ring([sb(st2, f"ust{i}", [128, D], F32) for i in range(2)])
                    vst = Ring([sb(st2, f"vst{i}", [128, D], F32) for i in range(2)])
                    ub = Ring([sb(st2, f"ub{i}", [128, D], BF16) for i in range(2)])
                    uT = Ring([sb(st2, f"uT{i}", [128, 8, 128], BF16) for i in range(2)])
                    Vb = sb(st2, "Vb", [128, IB, D], BF16); r_Vb = [Res() for _ in range(IB)]
                    WA = sb(st2, "WA", [128, IB, TH], BF16); r_WA = [Res() for _ in range(IB)]
                    wt = Ring([sb(st2, f"wt{i}", [128, TH], BF16) for i in range(3)])
                    gl = Ring([sb(st2, f"gl{i}", [128, 512], BF16) for i in range(3)])
                    for ib0 in range(0, 128, IB):
                        for ib in range(IB):
                            i = ib0 + ib
                            u_t, u_r = ust.next()
                            v_t, v_r = vst.next()
                            w_t, w_r = wt.next()
                            DMA("dsp", u_t[:], peer_u[i * 128:(i + 1) * 128, :], [], [u_r])
                            DMA("dact", v_t[:], peer_v[i * 128:(i + 1) * 128, :], [], [v_r])
                            DMA("dpool", w_t[:], Wt_s[i], [R["Wt_s"]], [w_r])
                            ub_t, ub_r = ub.next()
                            CP("pool", ub_t[:], u_t[:], [u_r], [ub_r])
                            CP("pool", Vb[:, ib, :], v_t[:], [v_r], [r_Vb[ib]])
                            pt, pt_r = ptb2.next()
                            for dc in range(8):
                                TR(pt[:, dc * 128:(dc + 1) * 128], ub_t[:, dc * 128:(dc + 1) * 128], ident_b[:], [ub_r, r_identb], [pt_r])
                            uT_t, uT_r = uT.next()
                            CP("act", uT_t[:].rearrange("p a t -> p (a t)"), pt[:], [pt_r], [uT_r])
                            for tc4 in range(4):
                                pa, pa_r = p1.next()
                                for dc in range(8):
                                    MM(pa[:], uT_t[:, dc, :], xnTa[:, dc, tc4 * 512:(tc4 + 1) * 512], dc == 0, dc == 7, [uT_r, r_xnTa], [pa_r])
                                g_t, g_r = gl.next()
                                ACTF(g_t[:], pa[:], AF.Gelu_apprx_tanh, [pa_r], [g_r])
                                TT("dve", WA[:, ib, tc4 * 512:(tc4 + 1) * 512], g_t[:], w_t[:, tc4 * 512:(tc4 + 1) * 512], ALU.mult, [g_r, w_r], [r_WA[ib]])
                        for tt in range(NTL):
                            for ch in range(2):
                                pb, pb_r = p2.next()
                                for ib in range(IB):
                                    MM(pb[:], WA[:, ib, tt * 128:(tt + 1) * 128], Vb[:, ib, ch * 512:(ch + 1) * 512], ib == 0, ib == IB - 1, [r_WA[ib], r_Vb[ib]], [pb_r])
                                TT("dve", Yacc[:, tt, ch * 512:(ch + 1) * 512], Yacc[:, tt, ch * 512:(ch + 1) * 512], pb[:], ALU.add, [rY[tt], pb_r], [rY[tt]])


                with Scope(kb) as st3:
                    Wgt = sb(st3, "Wgt", [128, 8, D], BF16); r_Wgt = Res()
                    Wp = sb(st3, "Wp", [128, 2, D], BF16); r_Wp = Res()
                    rep3 = sb(st3, "rep3", [128, 3, D], F32); r_rep3 = Res()
                    DMA("dsp", rep3[:], rep4[:, 1:4, :], [], [r_rep3])
                    wst3 = Ring([sb(st3, f"wst3{i}", [128, D], F32) for i in range(2)])
                    for (src, dstw, dres, nch) in ((ple_wg, Wgt, r_Wgt, 8), (ple_pj, Wp, r_Wp, 2)):
                        for dc in range(nch):
                            w_t, w_r = wst3.next()
                            DMA("dsp" if dc % 2 == 0 else "dact", w_t[:], src[dc * 128:(dc + 1) * 128, :], [], [w_r])
                            CP("dve" if dc % 2 == 0 else "pool", dstw[:, dc, :], w_t[:], [w_r], [dres])
                    x3_ring = Ring([sb(st3, f"x3{i}", [128, D], F32) for i in range(2)])
                    x3b_ring = Ring([sb(st3, f"x3b{i}", [128, D], BF16) for i in range(2)])
                    x3T_ring = Ring([sb(st3, f"x3T{i}", [128, 8, 128], BF16) for i in range(2)])
                    pht = Ring([sb(st3, f"pht{i}", [128, 256], F32) for i in range(2)])
                    phb = Ring([sb(st3, f"phb{i}", [128, 256], BF16) for i in range(2)])
                    phT = Ring([sb(st3, f"phT{i}", [128, 2, 128], BF16) for i in range(2)])
                    gt_ring = Ring([sb(st3, f"gtD{i}", [128, D], F32) for i in range(2)])
                    ot_ring = Ring([sb(st3, f"otD{i}", [128, D], F32) for i in range(2)])
                    junk3 = sb(st3, "junk3", [128, D], BF16); r_junk3 = Res()
                    sm3 = Ring([sb(st3, f"sm3{i}", [128, 8], F32) for i in range(4)])

                    def rms3(src, src_r, gi, dstf, dstf_r):
                        s_t, s_r = sm3.next()
                        ACTF(junk3[:], src, AF.Square, [src_r], [r_junk3, s_r], accum_out=s_t[:, 0:1])
                        TS("dve", s_t[:, 1:2], s_t[:, 0:1], 1.0 / D, EPS, ALU.mult, ALU.add, [s_r], [s_r])
                        ACTF(s_t[:, 1:2], s_t[:, 1:2], AF.Sqrt, [s_r], [s_r])
                        E("dve", lambda: nc.vector.reciprocal(out=s_t[:, 2:3], in_=s_t[:, 1:2]), [s_r], [s_r])
                        STT(dstf, src, s_t[:, 2:3], rep3[:, gi, :], ALU.mult, ALU.mult, [src_r, s_r, r_rep3], [dstf_r])

                    def tr3(srcb, srcb_r, dstT, dstT_r, nblk):
                        pt, pt_r = ptb2.next()
                        for dc in range(nblk):
                            TR(pt[:, dc * 128:(dc + 1) * 128], srcb[:, dc * 128:(dc + 1) * 128], ident_b[:], [srcb_r, r_identb], [pt_r])
                        CP("act", dstT.rearrange("p a t -> p (a t)"), pt[:, 0:nblk * 128], [pt_r], [dstT_r])

                    for it in range(NTL):
                        tsl = slice(it * 128, (it + 1) * 128)
                        Hh = Yacc[:, it, :]; H_r = rY[it]
                        x3, x3_r = x3_ring.next()
                        rms3(Hh, H_r, 0, x3[:], x3_r)
                        x3b, x3b_r = x3b_ring.next()
                        CP("pool", x3b[:], x3[:], [x3_r], [x3b_r])
                        x3T, x3T_r = x3T_ring.next()
                        tr3(x3b, x3b_r, x3T[:], x3T_r, 8)
                        ph_t, ph_r = pht.next()
                        DMA("dact", ph_t[:], ph[tsl, :], [], [ph_r])
                        pb_t, pb_r = phb.next()
                        CP("pool", pb_t[:], ph_t[:], [ph_r], [pb_r])
                        pT_t, pT_r = phT.next()
                        tr3(pb_t, pb_r, pT_t[:], pT_r, 2)
                        gt, gt_r = gt_ring.next()
                        for ch in range(2):
                            csl = slice(ch * 512, (ch + 1) * 512)
                            pa, pa_r = p1.next()
                            for dc in range(8):
                                MM(pa[:], x3T[:, dc, :], Wgt[:, dc, csl], dc == 0, dc == 7, [x3T_r, r_Wgt], [pa_r])
                            TT("dve", gt[:, csl], pa[:], rep3[:, 2, csl], ALU.add, [pa_r, r_rep3], [gt_r])
                            ACTF(gt[:, csl], gt[:, csl], AF.Sigmoid, [gt_r], [gt_r])
                            pb2, pb2_r = p2.next()
                            for dc in range(2):
                                MM(pb2[:], pT_t[:, dc, :], Wp[:, dc, csl], dc == 0, dc == 1, [pT_r, r_Wp], [pb2_r])
                            TT("dve", gt[:, csl], gt[:, csl], pb2[:], ALU.mult, [gt_r, pb2_r], [gt_r])
                            TT("pool", Yacc[:, it, csl], Yacc[:, it, csl], gt[:, csl], ALU.add, [H_r, gt_r], [H_r])
                        ot, ot_r = ot_ring.next()
                        rms3(Hh, H_r, 1, ot[:], ot_r)
                        DMA("dsp", out[tsl, :], ot[:], [ot_r], [])

        kb.drain_all()
    return nc


def _blockdiag(w):
    o = np.zeros((128, 4, 128), np.float32)
    for n in range(8):
        cc, j = n // 2, n % 2
        o[j * 64:(j + 1) * 64, cc, j * 64:(j + 1) * 64] = w[n]
    return o


def _rel_bucket(dist):
    n = np.maximum(dist, 0)
    nf = np.maximum(n, 16).astype(np.float32)
    large = 16 + (np.log(nf / np.float32(16)) / np.float32(np.log(8.0)) * np.float32(16)).astype(np.int32)
    large = np.minimum(large, 31)
    return np.where(n < 16, n, large)


def _nsa_consts(rel_table):
    c = {}
    assert (_rel_bucket(np.arange(113, 8192)) == 31).all()
    cl = np.arange(128)[:, None, None]; mp = np.arange(5)[None, :, None]; tt = np.arange(512)[None, None, :]
    dist = 512 * mp + tt - 16 * cl - 31
    c["bc_g"] = np.ascontiguousarray(rel_table[_rel_bucket(dist)].transpose(3, 0, 1, 2))
    c["bc_m"] = np.where(dist >= 0, 0.0, NEGM).astype(np.float32)
    assert (512 * 5 - 16 * 127 - 31) >= 113
    sl = np.arange(128)[:, None]; tl = np.arange(128)[None, :]
    d0 = tl - sl; d1 = 128 + tl - sl
    g0 = rel_table[_rel_bucket(d0)]; g1 = rel_table[_rel_bucket(d1)]
    c["bd_g"] = np.ascontiguousarray(np.stack([g0, g1], 0).transpose(3, 1, 0, 2))
    m0 = np.where(d0 >= 0, 0.0, NEGM); m2 = np.where(tl < sl, 0.0, NEGM)
    c["bd_m"] = np.ascontiguousarray(np.stack([m0, np.zeros_like(m0), m2], 1)).astype(np.float32)
    c["t31"] = np.ascontiguousarray(np.broadcast_to(rel_table[31][None, :], (128, 8))).astype(np.float32)
    t = (np.arange(NT)[None, :, None] * 128 + np.arange(128)[:, None, None])
    blk = np.arange(64)[None, None, :]
    d = t // 64 - blk
    local = (d >= 0) & (d < 2)
    init = (blk == 0) & ~local
    past = (d >= 0) & ~local & ~init
    c["force_c"] = np.where(local, 2.0e4, np.where(init, 1.0e4, np.where(past, 0.0, -1.0))).astype(np.float32)
    c["keep_c"] = past.astype(np.float32)
    cs = np.arange(256)[:, None] * 16; ss = np.arange(64)[None, :] * 64
    ov = np.clip(np.minimum(cs + 32, ss + 64) - np.maximum(cs, ss), 0, None).astype(np.float32) / 32.0
    ove = np.concatenate([ov, np.ones((256, 1), np.float32)], 1)
    ove[255] = 0.0
    c["ovl_ext"] = np.ascontiguousarray(ove.reshape(2, 128, 65).transpose(1, 0, 2))
    c["erows"] = (np.arange(T)[None, :] // 64 == np.arange(64)[:, None]).astype(np.float32)
    return c


def _prep_inputs(inputs):
    x = np.ascontiguousarray(inputs["x"], dtype=np.float32)
    p = np.ascontiguousarray(inputs["p"], dtype=np.float32)
    shared = {
        "ident": np.eye(128, dtype=np.float32),
        "w_in": np.ascontiguousarray(inputs["w_in"][0]),
        "g_attn": np.ascontiguousarray(inputs["attn_norm"][0].reshape(8, 128).T),
        "lru_cw": np.ascontiguousarray(inputs["conv_w"][0][:, 0, :].reshape(4, 4, 128).transpose(2, 1, 0)),
        "lru_vec": np.ascontiguousarray(np.stack([inputs[k][0].reshape(4, 128) for k in
                                                  ("conv_b", "lru_ba", "lru_bx", "lru_lambda", "grp_norm_lru")], 0).transpose(2, 0, 1)),
        "cmp_w1": np.ascontiguousarray(np.stack([inputs[k][0].reshape(16, 128, 256).transpose(1, 0, 2) for k in ("cmp_k_w1", "cmp_v_w1")], 0)),
        "cmp_pe": np.ascontiguousarray(np.stack([inputs[k][0].reshape(16, 128).T for k in ("cmp_k_pe", "cmp_v_pe")], 0)),
        "cmp_w2": np.ascontiguousarray(np.stack([inputs[k][0].reshape(2, 128, 64).transpose(1, 0, 2) for k in ("cmp_k_w2", "cmp_v_w2")], 0)),
        "ga_rep": np.ascontiguousarray(np.broadcast_to(inputs["grp_norm_attn"][0][None, :], (128, 512))).astype(np.float32),
        "w_out": np.ascontiguousarray(inputs["w_out"][0]),
        "peer_wq": np.ascontiguousarray(inputs["peer_wq"][0]),
        "sk_T": np.ascontiguousarray(inputs["peer_subkeys"][0].transpose(0, 2, 1)),
        "peer_u": np.ascontiguousarray(inputs["peer_u"][0]),
        "peer_v": np.ascontiguousarray(inputs["peer_v"][0]),
        "ple_wgate": np.ascontiguousarray(inputs["ple_wgate"][0]),
        "ple_proj": np.ascontiguousarray(inputs["ple_proj"][0]),
        "rep4": np.ascontiguousarray(np.broadcast_to(np.stack([inputs["ffn_norm"][0], inputs["ple_norm"][0], inputs["final_norm"], inputs["ple_bgate"][0]], 0)[None], (128, 4, D))).astype(np.float32),
        "iota128": np.ascontiguousarray(np.broadcast_to(np.arange(128, dtype=np.float32)[None], (128, 128))),
        "iota16": np.ascontiguousarray(np.broadcast_to(np.arange(16, dtype=np.float32)[None], (128, 16))),
        "lru_bda": _blockdiag(inputs["lru_wa"][0]),
        "lru_bdx": _blockdiag(inputs["lru_wx"][0]),
    }
    shared.update(_nsa_consts(np.asarray(inputs["rel_table"], np.float32)))
    in_maps = []
    for c in range(8):
        b, hf = c // 2, c % 2
        m = dict(shared)
        m["xb"] = x[b]
        m["xh"] = np.ascontiguousarray(x[b, hf * TH:(hf + 1) * TH])
        m["ph"] = np.ascontiguousarray(p[0, b, hf * TH:(hf + 1) * TH])
        sel = np.zeros((128, 2), np.float32); sel[:, hf] = 1.0
        m["selc"] = sel
        in_maps.append(m)
    return in_maps


def kernel(**inputs):
    nc = build_program()
    in_maps = _prep_inputs(inputs)
    res = run_bass_kernel_spmd(nc, in_maps, core_ids=list(range(8)))
    outp = np.zeros((4, T, D), np.float32)
    for c in range(8):
        b, hf = c // 2, c % 2
        outp[b, hf * TH:(hf + 1) * TH] = res.results[c]["out"]
    return outp
```

```python
import numpy as np
from contextlib import ExitStack
import concourse.bass as bass
import concourse.mybir as mybir
from concourse.bass_utils import run_bass_kernel_spmd

F32 = mybir.dt.float32
BF16 = mybir.dt.bfloat16
U32 = mybir.dt.uint32
AF = mybir.ActivationFunctionType
ALU = mybir.AluOpType
AX = mybir.AxisListType

T = 4096
D = 1024
NT = T // 128
TH = 2048
IN_COLS = 2328
EPS = 1e-6
NEGM = -30000.0


class Res:
    __slots__ = ("name", "lw", "rd")

    def __init__(self, name=""):
        self.name = name
        self.lw = None
        self.rd = {}


class KB:
    NDMA = 4

    def __init__(self, nc, stack):
        self.nc = nc
        self.issue = {"pe": nc.tensor, "act": nc.scalar, "dve": nc.vector, "pool": nc.gpsimd,
                      "dsp": nc.sync, "dact": nc.scalar, "dpool": nc.gpsimd}
        self.stream = {"pe": "pe", "act": "act", "dve": "dve", "pool": "pool",
                       "dsp": "sp", "dact": "act", "dpool": "pool"}
        self.sems = {}
        self.cnt = {}
        for q in self.issue:
            n = self.NDMA if self.is_dma(q) else 1
            self.sems[q] = [stack.enter_context(nc.semaphore(f"s_{q}{i}")) for i in range(n)]
            self.cnt[q] = 0
        self.waited = {s: {} for s in ("pe", "act", "dve", "pool", "sp")}
        self.ninst = 0
        self._rr = 0

    @staticmethod
    def is_dma(q):
        return q in ("dsp", "dact", "dpool")

    @staticmethod
    def _need(need, dep):
        if dep is None:
            return
        q, c = dep
        if need.get(q, 0) < c:
            need[q] = c

    def _waits(self, st, eng, need, skip_q=None):
        for dq, c in need.items():
            if dq == "pe" and skip_q == "pe":
                continue
            if self.is_dma(dq):
                n = self.NDMA
                for si in range(n):
                    k = (c - 1 - si) // n + 1 if c - 1 >= si else 0
                    if k <= 0:
                        continue
                    key = (dq, si)
                    if self.waited[st].get(key, 0) >= k:
                        continue
                    eng.wait_ge(self.sems[dq][si], 16 * k)
                    self.waited[st][key] = k
            else:
                key = (dq, 0)
                if self.waited[st].get(key, 0) >= c:
                    continue
                eng.wait_ge(self.sems[dq][0], c)
                self.waited[st][key] = c

    def emit(self, q, fn, reads=(), writes=()):
        need = {}
        for r in reads:
            self._need(need, r.lw)
        for w in writes:
            self._need(need, w.lw)
            for rq, rc in w.rd.items():
                self._need(need, (rq, rc))
        st = self.stream[q]
        self._waits(st, self.issue[q], need, skip_q=q)
        inst = fn()
        self.cnt[q] += 1
        c = self.cnt[q]
        if self.is_dma(q):
            inst.then_inc(self.sems[q][(c - 1) % self.NDMA], 16)
        else:
            inst.then_inc(self.sems[q][0], 1)
        for r in reads:
            if r.rd.get(q, 0) < c:
                r.rd[q] = c
        for w in writes:
            w.lw = (q, c)
            w.rd = {}
        self.ninst += 1
        return inst

    def dmaq(self):
        self._rr ^= 1
        return "dsp" if self._rr else "dact"

    def barrier(self):
        need = {q: c for q, c in self.cnt.items() if c > 0}
        for st, eng in (("pe", self.nc.tensor), ("act", self.nc.scalar), ("dve", self.nc.vector),
                        ("pool", self.nc.gpsimd), ("sp", self.nc.sync)):
            self._waits(st, eng, dict(need))

    def drain_all(self):
        need = {q: c for q, c in self.cnt.items() if c > 0}
        self._waits("sp", self.nc.sync, need)


class Scope:
    def __init__(self, kb):
        self.kb = kb
        self.st = ExitStack()

    def __enter__(self):
        self.st.__enter__()
        return self.st

    def __exit__(self, *a):
        if a[0] is None:
            self.kb.barrier()
        return self.st.__exit__(*a)


class Ring:
    def __init__(self, tiles):
        self.tiles = tiles
        self.res = [Res() for _ in tiles]
        self.i = -1

    def next(self):
        self.i = (self.i + 1) % len(self.tiles)
        return self.tiles[self.i], self.res[self.i]


def build_program(dbg=None, phases=("A", "B", "C", "D")):
    nc = bass.Bass("TRN2", target_bir_lowering=False)

    def din(name, shape, dt=F32):
        return nc.dram_tensor(name, list(shape), dt, kind="ExternalInput").ap()

    dbg = dbg or ()

    def dscr(name, shape, dt):
        kind = "ExternalOutput" if name in dbg else "Internal"
        return nc.dram_tensor(name, list(shape), dt, kind=kind).ap()

    xb = din("xb", [T, D])
    xh = din("xh", [TH, D])
    ph = din("ph", [TH, 256])
    selc = din("selc", [128, 2])
    ident = din("ident", [128, 128])
    w_in = din("w_in", [D, IN_COLS])
    g_attn = din("g_attn", [128, 8])
    out = nc.dram_tensor("out", [TH, D], F32, kind="ExternalOutput").ap()
    lru_cw = din("lru_cw", [128, 4, 4])
    lru_vec = din("lru_vec", [128, 5, 4])
    lru_bda = din("lru_bda", [128, 4, 128])
    lru_bdx = din("lru_bdx", [128, 4, 128])

    qT_s = dscr("qT_s", [512, T], BF16)
    kcT_s = dscr("kcT_s", [128, T], BF16)
    vcT_s = dscr("vcT_s", [128, T], BF16)
    ksT_s = dscr("ksT_s", [128, T], BF16)
    kwT_s = dscr("kwT_s", [128, T], BF16)
    vs_s = dscr("vs_s", [T, 128], BF16)
    vw_s = dscr("vw_s", [T, 128], BF16)
    gates_s = dscr("gates_s", [T, 24], F32)
    xrT_s = dscr("xrT_s", [512, T], F32)
    xgT_s = dscr("xgT_s", [512, T], F32)

    cmp_w1 = din("cmp_w1", [2, 128, 16, 256])
    cmp_pe = din("cmp_pe", [2, 128, 16])
    cmp_w2 = din("cmp_w2", [2, 128, 2, 64])
    ovl_ext = din("ovl_ext", [128, 2, 65])
    bc_g = din("bc_g", [8, 128, 5, 512])
    bc_m = din("bc_m", [128, 5, 512])
    bd_g = din("bd_g", [8, 128, 2, 128])
    bd_m = din("bd_m", [128, 3, 128])
    t31_in = din("t31", [128, 8])
    force_in = din("force_c", [128, 32, 64])
    keep_in = din("keep_c", [128, 32, 64])
    erows = din("erows", [64, T])
    ga_in = din("ga_rep", [128, 512])
    w_out_in = din("w_out", [D, D])
    peer_wq = din("peer_wq", [D, 2048])
    sk_T = din("sk_T", [2, 128, 128])
    peer_u = din("peer_u", [16384, D])
    peer_v = din("peer_v", [16384, D])
    ple_wg = din("ple_wgate", [D, D])
    ple_pj = din("ple_proj", [256, D])
    rep4 = din("rep4", [128, 4, D])
    iota16 = din("iota16", [128, 16])
    iota128 = din("iota128", [128, 128])
    H1_s = dscr("H1_s", [TH, D], F32)
    xnT2_s = dscr("xnT2_s", [128, 8, TH], BF16)
    Wt_s = dscr("Wt_s", [128, 128, TH], BF16)
    mixT_s = dscr("mixT_s", [1024, T], BF16)
    R = {n: Res(n) for n in ("H1_s", "xnT2_s", "Wt_s", "mixT_s", "qT_s", "kcT_s", "vcT_s", "ksT_s", "kwT_s", "vs_s", "vw_s", "gates_s", "xrT_s", "xgT_s")}

    with ExitStack() as top:
        kb = KB(nc, top)
        E = kb.emit

        uniq = [0]

        def sb(st, name, shape, dt):
            uniq[0] += 1
            return st.enter_context(nc.sbuf_tensor(f"sb{uniq[0]}_{name}", list(shape), dt))

        def ps(st, name, shape, dt):
            uniq[0] += 1
            return st.enter_context(nc.psum_tensor(f"ps{uniq[0]}_{name}", list(shape), dt))


        def MM(out_, lhsT, rhs, start, stop, reads, writes):
            return E("pe", lambda: nc.tensor.matmul(out_, lhsT=lhsT, rhs=rhs, start=start, stop=stop), reads, writes)

        def TR(out_, in_, idt, reads, writes):
            return E("pe", lambda: nc.tensor.transpose(out=out_, in_=in_, identity=idt), reads, writes)

        def ACTF(out_, in_, func, reads, writes, **kw):
            return E("act", lambda: nc.scalar.activation(out=out_, in_=in_, func=func, **kw), reads, writes)

        def veng(q):
            return nc.vector if q == "dve" else nc.gpsimd

        def TS(q, out_, in0, s1, s2, op0, op1, reads, writes):
            if op1 is None:
                return E(q, lambda: veng(q).tensor_scalar(out=out_, in0=in0, scalar1=s1, scalar2=None, op0=op0), reads, writes)
            return E(q, lambda: veng(q).tensor_scalar(out=out_, in0=in0, scalar1=s1, scalar2=s2, op0=op0, op1=op1), reads, writes)

        def TT(q, out_, in0, in1, op, reads, writes):
            return E(q, lambda: veng(q).tensor_tensor(out=out_, in0=in0, in1=in1, op=op), reads, writes)

        def STT(out_, in0, scalar, in1, op0, op1, reads, writes, **kw):
            return E("dve", lambda: nc.vector.scalar_tensor_tensor(out=out_, in0=in0, scalar=scalar, in1=in1, op0=op0, op1=op1, **kw), reads, writes)

        def CP(q, out_, in_, reads, writes):
            if q == "act":
                return E("act", lambda: nc.scalar.copy(out=out_, in_=in_), reads, writes)
            return E(q, lambda: veng(q).tensor_copy(out=out_, in_=in_), reads, writes)

        def MSET(q, out_, val, writes):
            return E(q, lambda: veng(q).memset(out_, val), (), writes)

        def DMA(q, out_, in_, reads, writes):
            eng = {"dsp": nc.sync, "dact": nc.scalar, "dpool": nc.gpsimd}[q]
            return E(q, lambda: eng.dma_start(out=out_, in_=in_), reads, writes)

        def dump(name, ap, shape, dt, res):
            if name not in dbg:
                return
            d = nc.dram_tensor(name, list(shape), dt, kind="ExternalOutput").ap()
            DMA("dsp", d, ap, [res] if not isinstance(res, list) else res, [])

        ident_f = sb(top, "ident_f", [128, 128], F32); r_identf = Res()
        ident_b = sb(top, "ident_b", [128, 128], BF16); r_identb = Res()
        E("dsp", lambda: nc.sync.dma_start(out=ident_f[:], in_=ident), writes=[r_identf])
        E("dve", lambda: nc.vector.tensor_copy(out=ident_b[:], in_=ident_f[:]), reads=[r_identf], writes=[r_identb])

        if "A" in phases:
            with Scope(kb) as st:
                Wg = sb(st, "Wg", [128, 8, IN_COLS], BF16); r_Wg = Res()
                gcol = sb(st, "gcol", [128, 8], F32); r_gcol = Res()
                wst = Ring([sb(st, f"wst{i}", [128, IN_COLS], F32) for i in range(2)])
                E("dsp", lambda: nc.sync.dma_start(out=gcol[:], in_=g_attn), writes=[r_gcol])
                for dc in range(8):
                    w_t, w_r = wst.next()
                    E("dsp" if dc % 2 == 0 else "dact",
                      (lambda w_t=w_t, dc=dc: nc.sync.dma_start(out=w_t[:], in_=w_in[dc * 128:(dc + 1) * 128, :])) if dc % 2 == 0 else
                      (lambda w_t=w_t, dc=dc: nc.scalar.dma_start(out=w_t[:], in_=w_in[dc * 128:(dc + 1) * 128, :])),
                      writes=[w_r])
                    eng = "dve" if dc % 2 == 0 else "pool"
                    ve = nc.vector if dc % 2 == 0 else nc.gpsimd
                    E(eng, lambda ve=ve, w_t=w_t, dc=dc: ve.tensor_scalar(out=Wg[:, dc, :], in0=w_t[:], scalar1=gcol[:, dc:dc + 1], scalar2=None, op0=ALU.mult),
                      reads=[w_r, r_gcol], writes=[r_Wg])

                xt_ring = Ring([sb(st, f"xt{i}", [128, 4, D], F32) for i in range(2)])
                xnb_ring = Ring([sb(st, f"xnb{i}", [128, 4, D], BF16) for i in range(2)])
                xnT_ring = Ring([sb(st, f"xnT{i}", [128, 8, 512], BF16) for i in range(2)])
                junk = sb(st, "junkA", [128, D], BF16); r_junk = Res()
                ss_ring = Ring([sb(st, f"ss{i}", [128, 8], F32) for i in range(2)])
                pT_ring = Ring([ps(st, f"pT{i}", [128, 512], BF16) for i in range(2)])
                pacc = Ring([ps(st, f"pacc{i}", [128, 512], F32) for i in range(4)])
                ostf = Ring([sb(st, f"ostf{i}", [128, 512], F32) for i in range(3)])
                ostb = Ring([sb(st, f"ostb{i}", [128, 512], BF16) for i in range(3)])
                osv = Ring([sb(st, f"osv{i}", [128, 256], BF16) for i in range(2)])
                osg = Ring([sb(st, f"osg{i}", [128, 24], F32) for i in range(2)])
                xb_v = xb.rearrange("(n p) d -> p n d", p=128)
                fm = []
                for cc in range(4):
                    fm.append((cc * 128, qT_s[cc * 128:(cc + 1) * 128, :], 0.125, True, R["qT_s"]))
                fm.append((512, kcT_s, 1.0, True, R["kcT_s"]))
                fm.append((640, vcT_s, 1.0, True, R["vcT_s"]))
                fm.append((768, ksT_s, 1.0, True, R["ksT_s"]))
                fm.append((1024, kwT_s, 1.0, True, R["kwT_s"]))
                for cc in range(4):
                    fm.append((1304 + cc * 128, xrT_s[cc * 128:(cc + 1) * 128, :], 1.0, False, R["xrT_s"]))
                for cc in range(4):
                    fm.append((1816 + cc * 128, xgT_s[cc * 128:(cc + 1) * 128, :], 1.0, False, R["xgT_s"]))
                ev = 0
                for tcn in range(8):
                    xt, xt_r = xt_ring.next()
                    E("dsp", lambda xt=xt, tcn=tcn: nc.sync.dma_start(out=xt[:], in_=xb_v[:, tcn * 4:(tcn + 1) * 4, :]), writes=[xt_r])
                    ss, ss_r = ss_ring.next()
                    for n in range(4):
                        E("act", lambda xt=xt, ss=ss, n=n: nc.scalar.activation(out=junk[:], in_=xt[:, n, :], func=AF.Square, accum_out=ss[:, n:n + 1]),
                          reads=[xt_r], writes=[r_junk, ss_r])
                    E("dve", lambda ss=ss: nc.vector.tensor_scalar(out=ss[:, 4:8], in0=ss[:, 0:4], scalar1=1.0 / D, scalar2=EPS, op0=ALU.mult, op1=ALU.add), reads=[ss_r], writes=[ss_r])
                    E("act", lambda ss=ss: nc.scalar.activation(out=ss[:, 4:8], in_=ss[:, 4:8], func=AF.Sqrt), reads=[ss_r], writes=[ss_r])
                    E("dve", lambda ss=ss: nc.vector.reciprocal(out=ss[:, 4:8], in_=ss[:, 4:8]), reads=[ss_r], writes=[ss_r])
                    xnb, xnb_r = xnb_ring.next()
                    for n in range(4):
                        if n % 2 == 0:
                            E("dve", lambda xt=xt, xnb=xnb, ss=ss, n=n: nc.vector.tensor_scalar(out=xnb[:, n, :], in0=xt[:, n, :], scalar1=ss[:, 4 + n:5 + n], scalar2=None, op0=ALU.mult),
                              reads=[xt_r, ss_r], writes=[xnb_r])
                        else:
                            E("pool", lambda xt=xt, xnb=xnb, ss=ss, n=n: nc.gpsimd.tensor_scalar(out=xnb[:, n, :], in0=xt[:, n, :], scalar1=ss[:, 4 + n:5 + n], scalar2=None, op0=ALU.mult),
                              reads=[xt_r, ss_r], writes=[xnb_r])
                    xnT, xnT_r = xnT_ring.next()
                    for dc in range(8):
                        pT, pT_r = pT_ring.next()
                        for n in range(4):
                            E("pe", lambda pT=pT, xnb=xnb, n=n, dc=dc: nc.tensor.transpose(out=pT[:, n * 128:(n + 1) * 128], in_=xnb[:, n, dc * 128:(dc + 1) * 128], identity=ident_b[:]),
                              reads=[xnb_r, r_identb], writes=[pT_r])
                        if dc % 2 == 0:
                            E("act", lambda pT=pT, xnT=xnT, dc=dc: nc.scalar.copy(out=xnT[:, dc, :], in_=pT[:]), reads=[pT_r], writes=[xnT_r])
                        else:
                            E("dve", lambda pT=pT, xnT=xnT, dc=dc: nc.vector.tensor_copy(out=xnT[:, dc, :], in_=pT[:]), reads=[pT_r], writes=[xnT_r])
                    for (c0, dst, scale, isb, dres) in fm:
                        pa, pa_r = pacc.next()
                        for dc in range(8):
                            E("pe", lambda pa=pa, dc=dc, c0=c0, xnT=xnT: nc.tensor.matmul(pa[:], lhsT=Wg[:, dc, c0:c0 + 128], rhs=xnT[:, dc, :], start=(dc == 0), stop=(dc == 7)),
                              reads=[r_Wg, xnT_r], writes=[pa_r])
                        o_t, o_r = (ostb if isb else ostf).next()
                        ev += 1
                        if ev % 2 == 0:
                            E("act", lambda o_t=o_t, pa=pa, scale=scale: nc.scalar.activation(out=o_t[:], in_=pa[:], func=AF.Copy, scale=scale), reads=[pa_r], writes=[o_r])
                        else:
                            E("dve", lambda o_t=o_t, pa=pa, scale=scale: nc.vector.tensor_scalar(out=o_t[:], in0=pa[:], scalar1=scale, scalar2=None, op0=ALU.mult), reads=[pa_r], writes=[o_r])
                        if ev % 2 == 0:
                            E("dsp", lambda o_t=o_t, dst=dst, tcn=tcn: nc.sync.dma_start(out=dst[:, tcn * 512:(tcn + 1) * 512], in_=o_t[:]), reads=[o_r], writes=[dres])
                        else:
                            E("dpool", lambda o_t=o_t, dst=dst, tcn=tcn: nc.gpsimd.dma_start(out=dst[:, tcn * 512:(tcn + 1) * 512], in_=o_t[:]), reads=[o_r], writes=[dres])
                    for n in range(4):
                        t0 = tcn * 512 + n * 128
                        pa, pa_r = pacc.next()
                        for dc in range(8):
                            E("pe", lambda pa=pa, dc=dc, xnT=xnT, n=n: nc.tensor.matmul(pa[:, 0:128], lhsT=xnT[:, dc, n * 128:(n + 1) * 128], rhs=Wg[:, dc, 896:1024], start=(dc == 0), stop=(dc == 7)),
                              reads=[r_Wg, xnT_r], writes=[pa_r])
                        pb, pb_r = pacc.next()
                        for dc in range(8):
                            E("pe", lambda pb=pb, dc=dc, xnT=xnT, n=n: nc.tensor.matmul(pb[:, 0:152], lhsT=xnT[:, dc, n * 128:(n + 1) * 128], rhs=Wg[:, dc, 1152:1304], start=(dc == 0), stop=(dc == 7)),
                              reads=[r_Wg, xnT_r], writes=[pb_r])
                        ov, ov_r = osv.next()
                        og, og_r = osg.next()
                        E("act", lambda ov=ov, pa=pa: nc.scalar.copy(out=ov[:, 0:128], in_=pa[:, 0:128]), reads=[pa_r], writes=[ov_r])
                        E("dve", lambda ov=ov, pb=pb: nc.vector.tensor_copy(out=ov[:, 128:256], in_=pb[:, 0:128]), reads=[pb_r], writes=[ov_r])
                        E("dve", lambda og=og, pb=pb: nc.vector.tensor_copy(out=og[:], in_=pb[:, 128:152]), reads=[pb_r], writes=[og_r])
                        E("dsp", lambda ov=ov, t0=t0: nc.sync.dma_start(out=vs_s[t0:t0 + 128, :], in_=ov[:, 0:128]), reads=[ov_r], writes=[R["vs_s"]])
                        E("dpool", lambda ov=ov, t0=t0: nc.gpsimd.dma_start(out=vw_s[t0:t0 + 128, :], in_=ov[:, 128:256]), reads=[ov_r], writes=[R["vw_s"]])
                        E("dsp", lambda og=og, t0=t0: nc.sync.dma_start(out=gates_s[t0:t0 + 128, :], in_=og[:]), reads=[og_r], writes=[R["gates_s"]])

        if "B" in phases:
            with Scope(kb) as st:
                cw = sb(st, "cw", [128, 4, 4], F32); r_cw = Res()
                lv = sb(st, "lv", [128, 5, 4], F32); r_lv = Res()
                clc = sb(st, "clc", [128, 3, 4], F32); r_clc = Res()
                bdf = sb(st, "bdf", [128, 2, 4, 128], F32); r_bdf = Res()
                bdb = sb(st, "bdb", [128, 2, 4, 128], BF16); r_bdb = Res()
                ones_b = sb(st, "ones_b", [128, 128], BF16); r_ones = Res()
                E("dsp", lambda: nc.sync.dma_start(out=cw[:], in_=lru_cw), writes=[r_cw])
                E("dact", lambda: nc.scalar.dma_start(out=lv[:], in_=lru_vec), writes=[r_lv])
                E("dsp", lambda: nc.sync.dma_start(out=bdf[:, 0], in_=lru_bda), writes=[r_bdf])
                E("dact", lambda: nc.scalar.dma_start(out=bdf[:, 1], in_=lru_bdx), writes=[r_bdf])
                E("dve", lambda: nc.vector.tensor_copy(out=bdb[:], in_=bdf[:]), reads=[r_bdf], writes=[r_bdb])
                E("dve", lambda: nc.vector.memset(ones_b[:], 1.0), writes=[r_ones])
                E("act", lambda: nc.scalar.activation(out=clc[:, 0, :], in_=lv[:, 3, :], func=AF.Exp, scale=-1.0), reads=[r_lv], writes=[r_clc])
                E("act", lambda: nc.scalar.activation(out=clc[:, 0, :], in_=clc[:, 0, :], func=AF.Ln, bias=1.0), reads=[r_clc], writes=[r_clc])
                E("dve", lambda: nc.vector.tensor_scalar(out=clc[:, 1, :], in0=clc[:, 0, :], scalar1=-8.0, scalar2=None, op0=ALU.mult), reads=[r_clc], writes=[r_clc])
                E("dve", lambda: nc.vector.tensor_scalar(out=clc[:, 2, :], in0=clc[:, 0, :], scalar1=-16.0, scalar2=None, op0=ALU.mult), reads=[r_clc], writes=[r_clc])
                L = sb(st, "Lall", [128, 4, T], F32); r_L = Res()
                X = [sb(st, f"lruX{i}", [128, T], F32) for i in range(5)]
                rX = [Res() for _ in range(5)]
                xcb = sb(st, "xcb", [128, T], BF16); r_xcb = Res()
                pg = Ring([ps(st, f"pg{i}", [128, 512], F32) for i in range(4)])
                for cc in range(4):
                    X1, X2, X3, X4, X5 = X
                    r1, r2, r3, r4, r5 = rX
                    for hh in range(2):
                        E("dsp", lambda cc=cc, hh=hh: nc.sync.dma_start(out=X1[:, hh * 2048:(hh + 1) * 2048], in_=xrT_s[cc * 128:(cc + 1) * 128, hh * 2048:(hh + 1) * 2048]), reads=[R["xrT_s"]], writes=[r1])
                        E("dact", lambda cc=cc, hh=hh: nc.scalar.dma_start(out=X3[:, hh * 2048:(hh + 1) * 2048], in_=xgT_s[cc * 128:(cc + 1) * 128, hh * 2048:(hh + 1) * 2048]), reads=[R["xgT_s"]], writes=[r3])
                    E("dve", lambda cc=cc: nc.vector.tensor_scalar(out=X2[:], in0=X1[:], scalar1=cw[:, cc, 3:4], scalar2=lv[:, 0, cc:cc + 1], op0=ALU.mult, op1=ALU.add), reads=[r1, r_cw, r_lv], writes=[r2])
                    for sh in (1, 2, 3):
                        E("dve", lambda cc=cc, sh=sh: nc.vector.scalar_tensor_tensor(out=X2[:, sh:T], in0=X1[:, 0:T - sh], scalar=cw[:, cc, 3 - sh:4 - sh], in1=X2[:, sh:T], op0=ALU.mult, op1=ALU.add), reads=[r1, r2, r_cw], writes=[r2])
                    E("pool", lambda: nc.gpsimd.tensor_copy(out=xcb[:], in_=X2[:]), reads=[r2], writes=[r_xcb])
                    for gi, (Xo, ro, bi) in enumerate(((X4, r4, 1), (X5, r5, 2))):
                        for tcn in range(8):
                            pgt, pg_r = pg.next()
                            E("pe", lambda pgt=pgt, gi=gi, cc=cc, tcn=tcn: nc.tensor.matmul(pgt[:], lhsT=bdb[:, gi, cc, :], rhs=xcb[:, tcn * 512:(tcn + 1) * 512], start=True, stop=True), reads=[r_bdb, r_xcb], writes=[pg_r])
                            E("act", lambda pgt=pgt, Xo=Xo, bi=bi, cc=cc, tcn=tcn: nc.scalar.activation(out=Xo[:, tcn * 512:(tcn + 1) * 512], in_=pgt[:], func=AF.Sigmoid, bias=lv[:, bi, cc:cc + 1]), reads=[pg_r, r_lv], writes=[ro])
                    E("act", lambda cc=cc: nc.scalar.activation(out=X1[:], in_=X4[:], func=AF.Exp, scale=clc[:, 1, cc:cc + 1]), reads=[r4, r_clc], writes=[r1])
                    E("act", lambda cc=cc: nc.scalar.activation(out=X4[:], in_=X4[:], func=AF.Exp, scale=clc[:, 2, cc:cc + 1]), reads=[r4, r_clc], writes=[r4])
                    E("act", lambda: nc.scalar.activation(out=X4[:], in_=X4[:], func=AF.Sqrt, scale=-1.0, bias=1.0), reads=[r4], writes=[r4])
                    E("pool", lambda: nc.gpsimd.tensor_tensor(out=X5[:], in0=X5[:], in1=X2[:], op=ALU.mult), reads=[r5, r2], writes=[r5])
                    E("dve", lambda: nc.vector.tensor_tensor(out=X4[:], in0=X4[:], in1=X5[:], op=ALU.mult), reads=[r4, r5], writes=[r4])
                    E("dve", lambda: nc.vector.tensor_tensor_scan(out=X2[:], data0=X1[:], data1=X4[:], initial=0.0, op0=ALU.mult, op1=ALU.add), reads=[r1, r4], writes=[r2])
                    E("act", lambda: nc.scalar.activation(out=X3[:], in_=X3[:], func=AF.Gelu_apprx_tanh), reads=[r3], writes=[r3])
                    E("pool", lambda cc=cc: nc.gpsimd.tensor_tensor(out=L[:, cc, :], in0=X2[:], in1=X3[:], op=ALU.mult), reads=[r2, r3], writes=[r_L])
                sq = Ring([sb(st, f"lsq{i}", [128, 512], BF16) for i in range(2)])
                rs_ring = Ring([sb(st, f"lrs{i}", [128, 512], F32) for i in range(2)])
                lo = Ring([sb(st, f"lo{i}", [128, 512], BF16) for i in range(3)])
                for tcn in range(8):
                    pgt, pg_r = pg.next()
                    for cc in range(4):
                        sq_t, sq_r = sq.next()
                        E("act", lambda sq_t=sq_t, cc=cc, tcn=tcn: nc.scalar.activation(out=sq_t[:], in_=L[:, cc, tcn * 512:(tcn + 1) * 512], func=AF.Square), reads=[r_L], writes=[sq_r])
                        E("pe", lambda pgt=pgt, sq_t=sq_t, cc=cc: nc.tensor.matmul(pgt[:], lhsT=ones_b[:], rhs=sq_t[:], start=(cc == 0), stop=(cc == 3)), reads=[r_ones, sq_r], writes=[pg_r])
                    rs_t, rs_r = rs_ring.next()
                    E("dve", lambda rs_t=rs_t, pgt=pgt: nc.vector.tensor_scalar(out=rs_t[:], in0=pgt[:], scalar1=1.0 / 512, scalar2=EPS, op0=ALU.mult, op1=ALU.add), reads=[pg_r], writes=[rs_r])
                    E("act", lambda rs_t=rs_t: nc.scalar.activation(out=rs_t[:], in_=rs_t[:], func=AF.Sqrt), reads=[rs_r], writes=[rs_r])
                    E("dve", lambda rs_t=rs_t: nc.vector.reciprocal(out=rs_t[:], in_=rs_t[:]), reads=[rs_r], writes=[rs_r])
                    for cc in range(4):
                        lo_t, lo_r = lo.next()
                        E("dve", lambda lo_t=lo_t, rs_t=rs_t, cc=cc, tcn=tcn: nc.vector.scalar_tensor_tensor(out=lo_t[:], in0=L[:, cc, tcn * 512:(tcn + 1) * 512], scalar=lv[:, 4, cc:cc + 1], in1=rs_t[:], op0=ALU.mult, op1=ALU.mult), reads=[r_L, rs_r, r_lv], writes=[lo_r])
                        E("dsp", lambda lo_t=lo_t, cc=cc, tcn=tcn: nc.sync.dma_start(out=mixT_s[512 + cc * 128:512 + (cc + 1) * 128, tcn * 512:(tcn + 1) * 512], in_=lo_t[:]), reads=[lo_r], writes=[R["mixT_s"]])

        if "C" in phases:
            with Scope(kb) as st:
                Aout = sb(st, "Aout", [128, NT, 512], BF16)
                rA = [Res() for _ in range(NT)]
                sig = sb(st, "sig", [128, NT, 24], F32); r_sig = Res()
                force_t = sb(st, "force_t", [128, NT, 64], F32); r_force = Res()
                keep_t = sb(st, "keep_t", [128, NT, 64], F32); r_keep = Res()
                t31 = sb(st, "t31", [128, 8], F32); r_t31 = Res()
                BD = sb(st, "BD", [128, 8, 3, 128], BF16); r_BD = Res()
                ovl_t = sb(st, "ovl_t", [128, 2, 65], F32); r_ovl = Res()
                ga_t = sb(st, "ga_t", [128, 512], F32); r_ga = Res()
                bcm = sb(st, "bcm", [128, 5, 512], F32); r_bcm = Res()
                DMA("dsp", sig[:], gates_s.rearrange("(n p) c -> p n c", p=128), [R["gates_s"]], [r_sig])
                ACTF(sig[:], sig[:], AF.Sigmoid, [r_sig], [r_sig])
                DMA("dact", force_t[:], force_in, [], [r_force])
                DMA("dsp", keep_t[:], keep_in, [], [r_keep])
                DMA("dact", t31[:], t31_in, [], [r_t31])
                DMA("dsp", ovl_t[:], ovl_ext, [], [r_ovl])
                DMA("dact", ga_t[:], ga_in, [], [r_ga])
                DMA("dsp", bcm[:], bc_m, [], [r_bcm])
                psb = [ps(st, f"pC{i}", [128, 512], F32) for i in range(8)]
                pS = Ring(psb[0:2])
                pO = psb[2:6]; r_pO = [Res() for _ in range(4)]
                pX = Ring(psb[6:8])
                with Scope(kb) as st2:
                    bdg = sb(st2, "bdg", [128, 8, 2, 128], F32); r_bdg = Res()
                    bdm = sb(st2, "bdm", [128, 3, 128], F32); r_bdm = Res()
                    DMA("dsp", bdg[:], bd_g.rearrange("h p j t -> p h j t"), [], [r_bdg])
                    DMA("dact", bdm[:], bd_m, [], [r_bdm])
                    for hg in range(8):
                        for j in range(2):
                            STT(BD[:, hg, j, :], bdg[:, hg, j, :], t31[:, hg:hg + 1], bdm[:, j, :], ALU.subtract, ALU.add, [r_bdg, r_bdm, r_t31], [r_BD])
                        CP("dve", BD[:, hg, 2, :], bdm[:, 2, :], [r_bdm], [r_BD])
                P_ring = Ring([sb(st, f"Pt{i}", [128, 512], BF16) for i in range(3)])
                sm = Ring([sb(st, f"smC{i}", [128, 8], F32) for i in range(8)])

                def finish_tiles(items, ncol, hg, br, first, imp_first=None):
                    sts = [sm.next() for _ in items]
                    for (po, po_r, i, _, _), (s_t, s_r) in zip(items, sts):
                        TS("dve", s_t[:, 0:1], po[:, ncol:ncol + 1], 1e-30, None, ALU.max, None, [po_r], [s_r])
                    for (po, po_r, i, _, _), (s_t, s_r) in zip(items, sts):
                        E("dve", lambda: nc.vector.reciprocal(out=s_t[:, 1:2], in_=s_t[:, 0:1]), [s_r], [s_r])
                    for (po, po_r, i, _, _), (s_t, s_r) in zip(items, sts):
                        TT("dve", s_t[:, 2:3], s_t[:, 1:2], sig[:, i, hg * 3 + br:hg * 3 + br + 1], ALU.mult, [s_r, r_sig], [s_r])
                    for (po, po_r, i, _, _), (s_t, s_r) in zip(items, sts):
                        dst = Aout[:, i, hg * 64:(hg + 1) * 64]
                        if first:
                            TS("dve", dst, po[:, 0:64], s_t[:, 2:3], None, ALU.mult, None, [po_r, s_r], [rA[i]])
                        else:
                            STT(dst, po[:, 0:64], s_t[:, 2:3], dst, ALU.mult, ALU.add, [po_r, s_r, rA[i]], [rA[i]])
                    if imp_first is not None:
                        for (po, po_r, i, imp_t, imp_r), (s_t, s_r) in zip(items, sts):
                            if imp_first:
                                TS("dve", imp_t, po[:, 64:128], s_t[:, 1:2], None, ALU.mult, None, [po_r, s_r], [imp_r])
                            else:
                                STT(imp_t, po[:, 64:128], s_t[:, 1:2], imp_t, ALU.mult, ALU.add, [po_r, s_r, imp_r], [imp_r])

                for k in range(2):
                    with Scope(kb) as stg:
                        KcmpT = sb(stg, "KcmpT", [64, 256], BF16); r_Kc = Res()
                        Vco = sb(stg, "Vco", [128, 2, 129], BF16); r_Vco = Res()
                        with Scope(kb) as stc:
                            w1s = Ring([sb(stc, f"w1s{i}", [128, 8, 256], F32) for i in range(2)])
                            w1b = sb(stc, "w1b", [128, 2, 16, 256], BF16); r_w1b = Res()
                            pes = sb(stc, "pes", [128, 2, 16], F32); r_pes = Res()
                            peb = sb(stc, "peb", [128, 2, 16], BF16); r_peb = Res()
                            w2s = sb(stc, "w2s", [128, 2, 2, 64], F32); r_w2s = Res()
                            w2b = sb(stc, "w2b", [128, 2, 2, 64], BF16); r_w2b = Res()
                            stk = sb(stc, "stk", [128, 2, T], BF16); r_stk = Res()
                            hb = sb(stc, "hb", [128, 4], F32); r_hb = Res()
                            gh = sb(stc, "gh", [128, 2, 2, 256], BF16); r_gh = Res()
                            for kv in range(2):
                                for hh in range(2):
                                    w_t, w_r = w1s.next()
                                    DMA("dsp" if hh == 0 else "dact", w_t[:], cmp_w1[kv, :, hh * 8:(hh + 1) * 8, :], [], [w_r])
                                    CP("pool" if hh == 0 else "dve", w1b[:, kv, hh * 8:(hh + 1) * 8, :], w_t[:], [w_r], [r_w1b])
                                DMA("dsp", pes[:, kv, :], cmp_pe[kv], [], [r_pes])
                                DMA("dact", w2s[:, kv], cmp_w2[kv], [], [r_w2s])
                                src = kcT_s if kv == 0 else vcT_s
                                sres = R["kcT_s"] if kv == 0 else R["vcT_s"]
                                DMA("dsp", stk[0:64, kv, :], src[k * 64:(k + 1) * 64, :], [sres], [r_stk])
                                MSET("pool", stk[64:128, kv, T - 1:T], 0.0, [r_stk])
                                DMA("dact", stk[64:128, kv, 0:T - 1], src[k * 64:(k + 1) * 64, 1:T], [sres], [r_stk])
                            CP("dve", peb[:], pes[:], [r_pes], [r_peb])
                            CP("dve", w2b[:], w2s[:], [r_w2s], [r_w2b])
                            MSET("pool", gh[:], 0.0, [r_gh])
                            for kv in range(2):
                                for hh in range(2):
                                    px, px_r = pX.next()
                                    for m in range(16):
                                        MM(px[:, 0:1], w1b[:, kv, m, hh * 128:(hh + 1) * 128], peb[:, kv, m:m + 1], m == 0, m == 15, [r_w1b, r_peb], [px_r])
                                    CP("dve", hb[:, kv * 2 + hh:kv * 2 + hh + 1], px[:, 0:1], [px_r], [r_hb])
                                    p_s, p_r = pS.next()
                                    for m in range(16):
                                        MM(p_s[:, 0:255], w1b[:, kv, m, hh * 128:(hh + 1) * 128], stk[:, kv, 2 * m:2 * m + 16 * 254 + 1:16], m == 0, m == 15, [r_w1b, r_stk], [p_r])
                                    ACTF(gh[:, kv, hh, 0:255], p_s[:, 0:255], AF.Gelu_apprx_tanh, [p_r, r_hb], [r_gh], bias=hb[:, kv * 2 + hh:kv * 2 + hh + 1])
                            px, px_r = pX.next()
                            for hh in range(2):
                                MM(px[0:64, 0:256], w2b[:, 0, hh, :], gh[:, 0, hh, :], hh == 0, hh == 1, [r_w2b, r_gh], [px_r])
                            CP("dve", KcmpT[:], px[0:64, 0:256], [px_r], [r_Kc])
                            for ct in range(2):
                                px, px_r = pX.next()
                                for hh in range(2):
                                    MM(px[:, 0:64], gh[:, 1, hh, ct * 128:(ct + 1) * 128], w2b[:, 1, hh, :], hh == 0, hh == 1, [r_gh, r_w2b], [px_r])
                                CP("dve", Vco[:, ct, 0:64], px[:, 0:64], [px_r], [r_Vco])
                            CP("pool", Vco[:, :, 64:129], ovl_t[:], [r_ovl], [r_Vco])
                            if k == 0:
                                dump("d_kcmp", KcmpT[:], [64, 256], BF16, r_Kc)
                                dump("d_vco", Vco[:], [128, 2, 129], BF16, r_Vco)
                                dump("d_hb", hb[:], [128, 4], F32, r_hb)
                                dump("d_gh", gh[:], [128, 2, 2, 256], BF16, r_gh)

                        QT = sb(stg, "QT", [128, 4, T], BF16)
                        r_QT = [Res() for _ in range(4)]
                        r_QM = [[Res() for _ in range(NT)] for _ in range(4)]
                        KsT = sb(stg, "KsT", [128, T], BF16); r_KsT = Res()
                        KwT = sb(stg, "KwT", [64, T], BF16); r_KwT = Res()
                        Vs = sb(stg, "Vs", [128, NT, 65], BF16); r_Vs = Res()
                        Vw = sb(stg, "Vw", [128, NT, 65], BF16); r_Vw = Res()
                        imp_acc = sb(stg, "imp_acc", [128, NT, 64], F32)
                        r_imp = [Res() for _ in range(NT)]
                        for g in range(4):
                            hg = 4 * k + g
                            DMA("dsp" if g % 2 == 0 else "dact", QT[0:64, g, :], qT_s[hg * 64:(hg + 1) * 64, :], [R["qT_s"]], [r_QT[g]])
                        DMA("dsp", KsT[0:64, :], ksT_s[k * 64:(k + 1) * 64, :], [R["ksT_s"]], [r_KsT])
                        with Scope(kb) as ste:
                            ers = sb(ste, "ers", [128, T], F32); r_ers = Res()
                            DMA("dact", ers[64:128, :], erows, [], [r_ers])
                            CP("pool", KsT[64:128, :], ers[64:128, :], [r_ers], [r_KsT])
                        DMA("dact", KwT[:], kwT_s[k * 64:(k + 1) * 64, :], [R["kwT_s"]], [r_KwT])
                        DMA("dsp", Vs[:, :, 0:64], vs_s.rearrange("(n p) c -> p n c", p=128)[:, :, k * 64:(k + 1) * 64], [R["vs_s"]], [r_Vs])
                        DMA("dact", Vw[:, :, 0:64], vw_s.rearrange("(n p) c -> p n c", p=128)[:, :, k * 64:(k + 1) * 64], [R["vw_s"]], [r_Vw])
                        MSET("pool", Vs[:, :, 64:65], 1.0, [r_Vs])
                        MSET("pool", Vw[:, :, 64:65], 1.0, [r_Vw])

                        bcs = Ring([sb(stg, f"bcs{i}", [128, 5, 512], F32) for i in range(2)])
                        BC = Ring([sb(stg, f"BCb{i}", [128, 5, 512], BF16) for i in range(2)])
                        for g in range(4):
                            hg = 4 * k + g
                            bs_t, bs_r = bcs.next()
                            DMA("dsp", bs_t[:, 0:3], bc_g[hg, :, 0:3], [], [bs_r])
                            DMA("dact", bs_t[:, 3:5], bc_g[hg, :, 3:5], [], [bs_r])
                            bc_t, bc_r = BC.next()
                            for m in range(5):
                                STT(bc_t[:, m, :], bs_t[:, m, :], t31[:, hg:hg + 1], bcm[:, m, :], ALU.subtract, ALU.add, [bs_r, r_bcm, r_t31], [bc_r])
                            for tcn in range(8):
                                cts = [0] if tcn < 4 else [0, 1]
                                for ct in cts:
                                    mp = tcn - 4 * ct
                                    p_s, p_r = pS.next()
                                    MM(p_s[:], KcmpT[:, ct * 128:(ct + 1) * 128], QT[0:64, g, tcn * 512:(tcn + 1) * 512], True, mp >= 5, [r_Kc, r_QT[g]], [p_r])
                                    if mp < 5:
                                        MM(p_s[:], ident_b[:], bc_t[:, mp, :], False, True, [r_identb, bc_r], [p_r])
                                    P_t, P_r = P_ring.next()
                                    ACTF(P_t[:], p_s[:], AF.Exp, [p_r, r_t31], [P_r], bias=t31[:, hg:hg + 1])
                                    for q in range(4):
                                        MM(pO[q][:, 0:129], P_t[:, q * 128:(q + 1) * 128], Vco[:, ct, :], ct == 0, ct == cts[-1], [P_r, r_Vco], [r_pO[q]])
                                finish_tiles([(pO[q], r_pO[q], 4 * tcn + q, imp_acc[:, 4 * tcn + q, :], r_imp[4 * tcn + q]) for q in range(4)], 128, hg, 0, True, imp_first=(g == 0))

                        if k == 0:
                            dump("d_imp", imp_acc[:], [128, NT, 64], F32, r_imp)
                            dump("d_aout_c", Aout[:], [128, NT, 512], BF16, rA)
                        MBr = Ring([sb(stg, f"MB{i}", [128, 128], F32) for i in range(2)])
                        for (mb_t, mb_r) in zip(MBr.tiles, MBr.res):
                            MSET("dve", mb_t[:], 0.0, [mb_r])
                        tk = Ring([sb(stg, f"tk{i}", [128, 2, 64], F32) for i in range(2)])
                        mxr = Ring([sb(stg, f"mx{i}", [128, 16], F32) for i in range(2)])
                        mtr = Ring([sb(stg, f"mtr{i}", [128, 128], BF16) for i in range(2)])
                        for i in range(NT):
                            tk_t, tk_r = tk.next()
                            mx_t, mx_r = mxr.next()
                            TT("dve", tk_t[:, 0, :], imp_acc[:, i, :], keep_t[:, i, :], ALU.mult, [r_imp[i], r_keep], [tk_r])
                            TT("dve", tk_t[:, 0, :], tk_t[:, 0, :], force_t[:, i, :], ALU.add, [tk_r, r_force], [tk_r])
                            E("dve", lambda: nc.vector.max(out=mx_t[:, 0:8], in_=tk_t[:, 0, :]), [tk_r], [mx_r])
                            E("dve", lambda: nc.vector.match_replace(out=tk_t[:, 1, :], in_to_replace=mx_t[:, 0:8], in_values=tk_t[:, 0, :], imm_value=-1e30), [tk_r, mx_r], [tk_r])
                            E("dve", lambda: nc.vector.max(out=mx_t[:, 8:16], in_=tk_t[:, 1, :]), [tk_r], [mx_r])
                            mb_t, mb_r = MBr.next()
                            TS("dve", mb_t[:, 64:128], tk_t[:, 0, :], mx_t[:, 15:16], None, ALU.is_ge, None, [tk_r, mx_r], [mb_r])
                            TS("dve", mb_t[:, 64:128], mb_t[:, 64:128], 1.0, -NEGM, ALU.subtract, ALU.mult, [mb_r], [mb_r])
                            px, px_r = pX.next()
                            TR(px[:, 0:128], mb_t[:], ident_f[:], [mb_r, r_identf], [px_r])
                            mt_t, mt_r = mtr.next()
                            CP("act", mt_t[64:128, :], px[64:128, 0:128], [px_r], [mt_r])
                            for g in range(4):
                                CP("pool" if g % 2 == 0 else "dve", QT[64:128, g, i * 128:(i + 1) * 128], mt_t[64:128, :], [mt_r], [r_QM[g][i]])

                        if k == 0:
                            dump("d_qt0", QT[:, 0, :], [128, T], BF16, r_QT + [x for l in r_QM for x in l])
                        for g in range(4):
                            hg = 4 * k + g
                            for br in (1, 2):
                                for tcn in range(8):
                                    j_lo = 0 if br == 1 else max(0, 4 * tcn - 4)
                                    j_hi = 4 * tcn + 3
                                    for j in range(j_lo, j_hi + 1):
                                        qa = max(0, j - 4 * tcn)
                                        qb = 3 if br == 1 else min(3, j + 4 - 4 * tcn)
                                        c0, c1 = qa * 128, (qb + 1) * 128
                                        t0 = tcn * 512
                                        adds = []
                                        for q in range(qa, qb + 1):
                                            dlt = 4 * tcn + q - j
                                            if dlt == 0:
                                                adds.append((q, 0))
                                            elif dlt == 1:
                                                adds.append((q, 1))
                                            elif dlt == 4 and br == 2:
                                                adds.append((q, 2))
                                        p_s, p_r = pS.next()
                                        if br == 1:
                                            rd = [r_KsT, r_QT[g]] + [r_QM[g][4 * tcn + q] for q in range(qa, qb + 1)]
                                            MM(p_s[:, c0:c1], KsT[:, j * 128:(j + 1) * 128], QT[:, g, t0 + c0:t0 + c1], True, len(adds) == 0, rd, [p_r])
                                        else:
                                            MM(p_s[:, c0:c1], KwT[:, j * 128:(j + 1) * 128], QT[0:64, g, t0 + c0:t0 + c1], True, len(adds) == 0, [r_KwT, r_QT[g]], [p_r])
                                        for ai, (q, ty) in enumerate(adds):
                                            MM(p_s[:, q * 128:(q + 1) * 128], ident_b[:], BD[:, hg, ty, :], False, ai == len(adds) - 1, [r_identb, r_BD], [p_r])
                                        P_t, P_r = P_ring.next()
                                        ACTF(P_t[:, c0:c1], p_s[:, c0:c1], AF.Exp, [p_r, r_t31], [P_r], bias=t31[:, hg:hg + 1])
                                        Vx, r_Vx = (Vs, r_Vs) if br == 1 else (Vw, r_Vw)
                                        for q in range(qa, qb + 1):
                                            i = 4 * tcn + q
                                            first_j = 0 if br == 1 else max(0, i - 4)
                                            MM(pO[q][:, 0:65], P_t[:, q * 128:(q + 1) * 128], Vx[:, j, :], j == first_j, j == i, [P_r, r_Vx], [r_pO[q]])
                                    finish_tiles([(pO[q], r_pO[q], 4 * tcn + q, None, None) for q in range(4)], 64, hg, br, False)

                dump("d_aout", Aout[:], [128, NT, 512], BF16, rA)
                with Scope(kb) as stn:
                    junkC = sb(stn, "junkC", [128, 512], BF16); r_junkC = Res()
                    an = Ring([sb(stn, f"an{i}", [128, 512], BF16) for i in range(2)])
                    af = Ring([sb(stn, f"af{i}", [128, 512], F32) for i in range(2)])
                    ao = Ring([sb(stn, f"ao{i}", [128, 512], BF16) for i in range(2)])
                    pTb = Ring([ps(stn, f"pTC{i}", [128, 512], BF16) for i in range(2)]) if False else None
                    for i in range(NT):
                        s_t, s_r = sm.next()
                        ACTF(junkC[:], Aout[:, i, :], AF.Square, [rA[i]], [r_junkC, s_r], accum_out=s_t[:, 0:1])
                        TS("dve", s_t[:, 1:2], s_t[:, 0:1], 1.0 / 512, EPS, ALU.mult, ALU.add, [s_r], [s_r])
                        ACTF(s_t[:, 1:2], s_t[:, 1:2], AF.Sqrt, [s_r], [s_r])
                        E("dve", lambda: nc.vector.reciprocal(out=s_t[:, 2:3], in_=s_t[:, 1:2]), [s_r], [s_r])
                        af_t, af_r = af.next()
                        STT(af_t[:], Aout[:, i, :], s_t[:, 2:3], ga_t[:], ALU.mult, ALU.mult, [rA[i], s_r, r_ga], [af_r])
                        px, px_r = pX.next()
                        for fc in range(4):
                            TR(px[:, fc * 128:(fc + 1) * 128], af_t[:, fc * 128:(fc + 1) * 128], ident_f[:], [af_r, r_identf], [px_r])
                        ao_t, ao_r = ao.next()
                        CP("act", ao_t[:], px[:], [px_r], [ao_r])
                        DMA("dsp" if i % 2 == 0 else "dpool", mixT_s[0:512, i * 128:(i + 1) * 128].rearrange("(f p) t -> p f t", p=128),
                            ao_t[:].rearrange("p (f t) -> p f t", f=4), [ao_r], [R["mixT_s"]])

        if "D" in phases or "D1" in phases:
            NTL = TH // 128
            with Scope(kb) as st:
                Wo = sb(st, "Wo", [128, 8, D], BF16); r_Wo = Res()
                Wq = sb(st, "Wq", [128, 8, 2048], BF16); r_Wq = Res()
                skb = sb(st, "skb", [128, 2, 128], BF16); r_skb = Res()
                repf = sb(st, "repf", [128, D], F32); r_rep = Res()
                io16 = sb(st, "io16", [128, 16], F32); r_io = Res()
                io128 = sb(st, "io128", [128, 128], F32); r_io128 = Res()
                selt = sb(st, "selt", [128, 2], F32); r_sel = Res()
                DMA("dsp", repf[:], rep4[:, 0, :], [], [r_rep])
                DMA("dact", io16[:], iota16, [], [r_io])
                DMA("dact", io128[:], iota128, [], [r_io128])
                DMA("dact", selt[:], selc, [], [r_sel])
                with Scope(kb) as stw:
                    wst = Ring([sb(stw, f"wstD{i}", [128, 2048], F32) for i in range(3)])
                    n = 0
                    for (src, dstw, dres, ncol, nch) in ((w_out_in, Wo, r_Wo, D, 8), (peer_wq, Wq, r_Wq, 2048, 8)):
                        for dc in range(nch):
                            w_t, w_r = wst.next()
                            n += 1
                            DMA("dsp" if n % 2 == 0 else "dact", w_t[:, 0:ncol], src[dc * 128:(dc + 1) * 128, :], [], [w_r])
                            CP("dve" if n % 2 == 0 else "pool", dstw[:, dc, :], w_t[:, 0:ncol], [w_r], [dres])
                    w_t, w_r = wst.next()
                    DMA("dsp", w_t[:, 0:256].rearrange("p (a k) -> p a k", a=2), sk_T.rearrange("a p k -> p a k"), [], [w_r])
                    CP("dve", skb[:], w_t[:, 0:256].rearrange("p (a k) -> p a k", a=2), [w_r], [r_skb])

                pacc = Ring([ps(st, f"pD{i}", [128, 512], F32) for i in range(4)])
                pw_ring = Ring([ps(st, f"pDw{i}", [128, 512], F32) for i in range(2)])
                ptb = Ring([ps(st, f"pDb{i}", [128, 1024], BF16) for i in range(2)])
                mst = Ring([sb(st, f"mst{i}", [128, 8, 2, 128], BF16) for i in range(2)])
                mixh_ring = Ring([sb(st, f"mixh{i}", [128, 8, 128], BF16) for i in range(2)])
                xh_ring = Ring([sb(st, f"xhD{i}", [128, D], F32) for i in range(2)])
                H_ring = Ring([sb(st, f"HD{i}", [128, D], F32) for i in range(2)])
                xng_ring = Ring([sb(st, f"xng{i}", [128, D], F32) for i in range(1)])
                xnb_ring = Ring([sb(st, f"xnbD{i}", [128, D], BF16) for i in range(1)])
                xT_ring = Ring([sb(st, f"xTD{i}", [128, 8, 128], BF16) for i in range(2)])
                qTb = sb(st, "qTb", [128, 16, 128], BF16); r_qTb = Res()
                Ssc = sb(st, "Ssc", [128, 16, 128], F32); r_S = Res()
                Swk = sb(st, "Swk", [128, 16, 128], F32)
                rv = [Res() for _ in range(16)]; rv2 = [Res() for _ in range(16)]; ri = [Res() for _ in range(16)]; ri2 = [Res() for _ in range(16)]; rw = [Res() for _ in range(16)]
                v16 = sb(st, "v16", [128, 16, 16], F32); r_v16 = Res()
                i16 = sb(st, "i16", [128, 16, 16], U32); r_i16 = Res()
                i16f = sb(st, "i16f", [128, 16, 16], F32); r_i16f = Res()
                cand = sb(st, "cand", [128, 8, 256], F32); r_cand = Res()
                cwk = sb(st, "cwk", [128, 8, 256], F32)
                sc16 = sb(st, "sc16", [128, 8, 16], F32); r_sc = Res()
                ci16 = sb(st, "ci16", [128, 8, 16], U32); r_ci = Res()
                ab_u = sb(st, "ab_u", [128, 2, 8, 16], U32); r_abu = Res()
                ab_f = sb(st, "ab_f", [128, 2, 8, 16], F32); r_abf = Res()
                eq = sb(st, "eq", [128, 8, 16, 16], F32); r_eq = Res()
                isel = sb(st, "isel", [128, 3, 8, 16], F32); r_isel = Res()
                gz = sb(st, "gz", [128, 16], F32); r_gz = Res()
                junkB = sb(st, "junkDb", [128, D], BF16); r_junkB = Res()
                smD = Ring([sb(st, f"smD{i}", [128, 8], F32) for i in range(4)])
                ijgT = sb(st, "ijgT", [128, 3, 128], F32); r_ijgT = Res()
                OI = Ring([sb(st, f"OI{i}", [128, 16, 128], BF16) for i in range(2)])
                OJ = Ring([sb(st, f"OJ{i}", [128, 16, 128], BF16) for i in range(2)])
                OJf = Ring([sb(st, f"OJf{i}", [128, 16, 128], BF16) for i in range(2)])
                Wst = sb(st, "Wst", [128, 128, 128], BF16); r_Wst = Res()

                def rms_scaled(src, src_r, gain, gain_r, dstf, dstf_r):
                    s_t, s_r = smD.next()
                    ACTF(junkB[:], src, AF.Square, [src_r], [r_junkB, s_r], accum_out=s_t[:, 0:1])
                    TS("dve", s_t[:, 1:2], s_t[:, 0:1], 1.0 / D, EPS, ALU.mult, ALU.add, [s_r], [s_r])
                    ACTF(s_t[:, 1:2], s_t[:, 1:2], AF.Sqrt, [s_r], [s_r])
                    E("dve", lambda: nc.vector.reciprocal(out=s_t[:, 2:3], in_=s_t[:, 1:2]), [s_r], [s_r])
                    STT(dstf, src, s_t[:, 2:3], gain, ALU.mult, ALU.mult, [src_r, s_r, gain_r], [dstf_r])

                def transpose8(srcb, srcb_r, dstT, dstT_r, nblk=8):
                    pt, pt_r = ptb.next()
                    for dc in range(nblk):
                        TR(pt[:, dc * 128:(dc + 1) * 128], srcb[:, dc * 128:(dc + 1) * 128], ident_b[:], [srcb_r, r_identb], [pt_r])
                    CP("act", dstT.rearrange("p a t -> p (a t)"), pt[:, 0:nblk * 128], [pt_r], [dstT_r])

                for it in range(NTL):
                    tsl = slice(it * 128, (it + 1) * 128)
                    xh_t, xh_r = xh_ring.next()
                    DMA("dsp", xh_t[:], xh[tsl, :], [], [xh_r])
                    H, H_r = H_ring.next()
                    m_t, m_r = mst.next()
                    for a in range(2):
                        DMA("dsp" if a == 0 else "dact", m_t[:, :, a, :], mixT_s[:, a * TH + it * 128:a * TH + (it + 1) * 128].rearrange("(f p) t -> p f t", p=128), [R["mixT_s"]], [m_r])
                    mixh, r_mixh = mixh_ring.next()
                    TS("pool", mixh[:], m_t[:, :, 0, :], selt[:, 0:1], None, ALU.mult, None, [m_r, r_sel], [r_mixh])
                    STT(mixh[:], m_t[:, :, 1, :], selt[:, 1:2], mixh[:], ALU.mult, ALU.add, [m_r, r_sel, r_mixh], [r_mixh])
                    for ch in range(2):
                        pa, pa_r = pacc.next()
                        for fc in range(8):
                            MM(pa[:], mixh[:, fc, :], Wo[:, fc, ch * 512:(ch + 1) * 512], fc == 0, fc == 7, [r_mixh, r_Wo], [pa_r])
                        TT("dve", H[:, ch * 512:(ch + 1) * 512], pa[:], xh_t[:, ch * 512:(ch + 1) * 512], ALU.add, [pa_r, xh_r], [H_r])
                    DMA("dpool", H1_s[tsl, :], H[:], [H_r], [R["H1_s"]])
                    xng, xng_r = xng_ring.next()
                    rms_scaled(H[:], H_r, repf[:], r_rep, xng[:], xng_r)
                    xnb, xnb_r = xnb_ring.next()
                    CP("pool", xnb[:], xng[:], [xng_r], [xnb_r])
                    xT, xT_r = xT_ring.next()
                    transpose8(xnb, xnb_r, xT[:], xT_r)
                    DMA("dact", xnT2_s[:, :, tsl], xT[:], [xT_r], [R["xnT2_s"]])
                    for grp in range(4):
                        pa, pa_r = pacc.next()
                        for j in range(4):
                            hp = grp * 4 + j
                            for dc in range(8):
                                MM(pa[:, j * 128:(j + 1) * 128], Wq[:, dc, hp * 128:(hp + 1) * 128], xT[:, dc, :], dc == 0, dc == 7, [r_Wq, xT_r], [pa_r])
                        CP("act" if grp % 2 == 0 else "dve", qTb[:, grp * 4:(grp + 1) * 4, :].rearrange("p a t -> p (a t)"), pa[:], [pa_r], [r_qTb])
                    for grp in range(4):
                        pa, pa_r = pacc.next()
                        for j in range(4):
                            hp = grp * 4 + j
                            MM(pa[:, j * 128:(j + 1) * 128], qTb[:, hp, :], skb[:, hp % 2, :], True, True, [r_qTb, r_skb], [pa_r])
                        CP("act" if grp % 2 == 0 else "dve", Ssc[:, grp * 4:(grp + 1) * 4, :].rearrange("p a t -> p (a t)"), pa[:], [pa_r], [r_S])
                    for hp in range(16):
                        E("dve", lambda: nc.vector.max(out=v16[:, hp, 0:8], in_=Ssc[:, hp, :]), [r_S], [rv[hp]])
                    for hp in range(16):
                        E("dve", lambda: nc.vector.max_index(out=i16[:, hp, 0:8], in_max=v16[:, hp, 0:8], in_values=Ssc[:, hp, :]), [r_S, rv[hp]], [ri[hp]])
                    for hp in range(16):
                        E("dve", lambda: nc.vector.match_replace(out=Swk[:, hp, :], in_to_replace=v16[:, hp, 0:8], in_values=Ssc[:, hp, :], imm_value=-1e30), [r_S, rv[hp]], [rw[hp]])
                    for hp in range(16):
                        E("dve", lambda: nc.vector.max(out=v16[:, hp, 8:16], in_=Swk[:, hp, :]), [rw[hp]], [rv2[hp]])
                    for hp in range(16):
                        E("dve", lambda: nc.vector.max_index(out=i16[:, hp, 8:16], in_max=v16[:, hp, 8:16], in_values=Swk[:, hp, :]), [rw[hp], rv2[hp]], [ri2[hp]])
                    r_v16 = Res(); r_i16 = Res()
                    E("dve", lambda: nc.vector.tensor_copy(out=i16f[:], in_=i16[:]), ri + ri2, [r_i16f, r_i16])
                    v4 = v16[:].rearrange("p (h two) k -> p h two k", two=2)
                    in0 = v4[:, :, 0, :].rearrange("p h (a o) -> p h a o", o=1).to_broadcast([128, 8, 16, 16])
                    in1 = v4[:, :, 1, :].rearrange("p h (o b) -> p h o b", o=1).to_broadcast([128, 8, 16, 16])
                    TT("dve", cand[:].rearrange("p h (a b) -> p h a b", a=16), in0, in1, ALU.add, rv + rv2, [r_cand])
                    for h in range(8):
                        E("dve", lambda: nc.vector.max(out=sc16[:, h, 0:8], in_=cand[:, h, :]), [r_cand], [rv[h]])
                    for h in range(8):
                        E("dve", lambda: nc.vector.max_index(out=ci16[:, h, 0:8], in_max=sc16[:, h, 0:8], in_values=cand[:, h, :]), [r_cand, rv[h]], [ri[h]])
                    for h in range(8):
                        E("dve", lambda: nc.vector.match_replace(out=cwk[:, h, :], in_to_replace=sc16[:, h, 0:8], in_values=cand[:, h, :], imm_value=-1e30), [r_cand, rv[h]], [rw[h]])
                    for h in range(8):
                        E("dve", lambda: nc.vector.max(out=sc16[:, h, 8:16], in_=cwk[:, h, :]), [rw[h]], [rv2[h]])
                    for h in range(8):
                        E("dve", lambda: nc.vector.max_index(out=ci16[:, h, 8:16], in_max=sc16[:, h, 8:16], in_values=cwk[:, h, :]), [rw[h], rv2[h]], [ri2[h]])
                    r_sc = Res(); r_ci = Res()
                    E("dve", lambda: nc.vector.tensor_single_scalar(out=ab_u[:, 0], in_=ci16[:], scalar=4, op=ALU.logical_shift_right), ri[:8] + ri2[:8] + rv[:8] + rv2[:8], [r_abu, r_sc, r_ci])
                    E("dve", lambda: nc.vector.tensor_single_scalar(out=ab_u[:, 1], in_=ci16[:], scalar=15, op=ALU.bitwise_and), [r_ci], [r_abu])
                    CP("dve", ab_f[:], ab_u[:], [r_abu], [r_abf])
                    i4 = i16f[:].rearrange("p (h two) k -> p h two k", two=2)
                    for w in range(2):
                        a_b = ab_f[:, w].rearrange("p h (k o) -> p h k o", o=1).to_broadcast([128, 8, 16, 16])
                        io_b = io16[:].rearrange("p (o q a) -> p o q a", o=1, q=1).to_broadcast([128, 8, 16, 16])
                        TT("dve", eq[:], a_b, io_b, ALU.is_equal, [r_abf, r_io], [r_eq])
                        iv_b = i4[:, :, w, :].rearrange("p h (o a) -> p h o a", o=1).to_broadcast([128, 8, 16, 16])
                        TT("dve", eq[:], eq[:], iv_b, ALU.mult, [r_eq, r_i16f], [r_eq])
                        E("dve", lambda: nc.vector.tensor_reduce(out=isel[:, w], in_=eq[:], axis=AX.X, op=ALU.add), [r_eq], [r_isel])
                    TT("dve", isel[:, 2], sc16[:], sc16[:, :, 0:1].to_broadcast([128, 8, 16]), ALU.subtract, [r_sc], [r_isel])
                    ACTF(isel[:, 2], isel[:, 2], AF.Exp, [r_isel], [r_isel])
                    E("dve", lambda: nc.vector.tensor_reduce(out=gz[:, 0:8], in_=isel[:, 2], axis=AX.X, op=ALU.add), [r_isel], [r_gz])
                    E("dve", lambda: nc.vector.reciprocal(out=gz[:, 8:16], in_=gz[:, 0:8]), [r_gz], [r_gz])
                    TT("dve", isel[:, 2], isel[:, 2], gz[:, 8:16].rearrange("p (h o) -> p h o", o=1).to_broadcast([128, 8, 16]), ALU.mult, [r_isel, r_gz], [r_isel])
                    pa, pa_r = pacc.next()
                    for w in range(3):
                        TR(pa[:, w * 128:(w + 1) * 128], isel[:, w].rearrange("p h k -> p (h k)"), ident_f[:], [r_isel, r_identf], [pa_r])
                    CP("act", ijgT[:].rearrange("p a t -> p (a t)"), pa[:, 0:384], [pa_r], [r_ijgT])
                    TB = 16
                    for tb in range(128 // TB):
                        t0 = tb * TB
                        oi, oi_r = OI.next()
                        oj, oj_r = OJ.next()
                        ojf, ojf_r = OJf.next()
                        io_b = io128[:].rearrange("p (o i) -> p o i", o=1).to_broadcast([128, TB, 128])

                        def colb(w):
                            return ijgT[:, w, t0:t0 + TB].rearrange("p (t o) -> p t o", o=1).to_broadcast([128, TB, 128])
                        TT("dve", oi[:], io_b, colb(0), ALU.is_equal, [r_io128, r_ijgT], [oi_r])
                        TT("dve", ojf[:], io_b, colb(1), ALU.is_equal, [r_io128, r_ijgT], [ojf_r])
                        TT("pool", oj[:], ojf[:], colb(2), ALU.mult, [ojf_r, r_ijgT], [oj_r])
                        for tq in range(TB // 4):
                            pw, pw_r = pw_ring.next()
                            for u in range(4):
                                MM(pw[:, u * 128:(u + 1) * 128], oj[:, tq * 4 + u, :], oi[:, tq * 4 + u, :], True, True, [oj_r, oi_r], [pw_r])
                            tg = t0 + tq * 4
                            CP("act", Wst[:, :, tg:tg + 4].rearrange("p i t -> p t i"), pw[:].rearrange("p (t i) -> p t i", t=4), [pw_r], [r_Wst])
                    for qd in range(4 if "noWdma" not in phases else 0):
                        DMA(("dsp", "dact", "dpool", "dsp")[qd], Wt_s[qd * 32:(qd + 1) * 32, :, tsl].rearrange("i j t -> j i t"), Wst[:, qd * 32:(qd + 1) * 32, :], [r_Wst], [R["Wt_s"]])

            with Scope(kb) as st:
                Yacc = sb(st, "Yacc", [128, NTL, D], F32)
                rY = [Res() for _ in range(NTL)]
                H1v = H1_s.rearrange("(n p) d -> p n d", p=128)
                for n4 in range(4):
                    DMA("dsp" if n4 % 2 == 0 else "dact", Yacc[:, n4 * 4:(n4 + 1) * 4, :], H1v[:, n4 * 4:(n4 + 1) * 4, :], [R["H1_s"]], rY[n4 * 4:(n4 + 1) * 4])
                p1 = Ring([ps(st, f"pE1{i}", [128, 512], F32) for i in range(3)])
                p2 = Ring([ps(st, f"pE2{i}", [128, 512], F32) for i in range(3)])
                ptb2 = Ring([ps(st, f"pEb{i}", [128, 1024], BF16) for i in range(2)])
                with Scope(kb) as st2:
                  if "D" in phases or "D2" in phases:
                    xnTa = sb(st2, "xnTa", [128, 8, TH], BF16); r_xnTa = Res()
                    for dc in range(8):
                        DMA("dsp" if dc % 2 == 0 else "dact", xnTa[:, dc, :], xnT2_s[:, dc, :], [R["xnT2_s"]], [r_xnTa])
                    IB = 8
                    ust = Ring([sb(st2, f"ust{i}", [128, D], F32) for i in range(2)])
                    vst = Ring([sb(st2, f"vst{i}", [128, D], F32) for i in range(2)])
                    ub = Ring([sb(st2, f"ub{i}", [128, D], BF16) for i in range(2)])
                    uT = Ring([sb(st2, f"uT{i}", [128, 8, 128], BF16) for i in range(2)])
                    Vb = sb(st2, "Vb", [128, IB, D], BF16); r_Vb = [Res() for _ in range(IB)]
                    WA = sb(st2, "WA", [128, IB, TH], BF16); r_WA = [Res() for _ in range(IB)]
                    wt = Ring([sb(st2, f"wt{i}", [128, TH], BF16) for i in range(3)])
                    gl = Ring([sb(st2, f"gl{i}", [128, 512], BF16) for i in range(3)])
                    for ib0 in range(0, 128, IB):
                        for ib in range(IB):
                            i = ib0 + ib
                            u_t, u_r = ust.next()
                            v_t, v_r = vst.next()
                            w_t, w_r = wt.next()
                            DMA("dsp", u_t[:], peer_u[i * 128:(i + 1) * 128, :], [], [u_r])
                            DMA("dact", v_t[:], peer_v[i * 128:(i + 1) * 128, :], [], [v_r])
                            DMA("dpool", w_t[:], Wt_s[i], [R["Wt_s"]], [w_r])
                            ub_t, ub_r = ub.next()
                            CP("pool", ub_t[:], u_t[:], [u_r], [ub_r])
                            CP("pool", Vb[:, ib, :], v_t[:], [v_r], [r_Vb[ib]])
                            pt, pt_r = ptb2.next()
                            for dc in range(8):
                                TR(pt[:, dc * 128:(dc + 1) * 128], ub_t[:, dc * 128:(dc + 1) * 128], ident_b[:], [ub_r, r_identb], [pt_r])
                            uT_t, uT_r = uT.next()
                            CP("act", uT_t[:].rearrange("p a t -> p (a t)"), pt[:], [pt_r], [uT_r])
                            for tc4 in range(4):
                                pa, pa_r = p1.next()
                                for dc in range(8):
                                    MM(pa[:], uT_t[:, dc, :], xnTa[:, dc, tc4 * 512:(tc4 + 1) * 512], dc == 0, dc == 7, [uT_r, r_xnTa], [pa_r])
                                g_t, g_r = gl.next()
                                ACTF(g_t[:], pa[:], AF.Gelu_apprx_tanh, [pa_r], [g_r])
                                TT("dve", WA[:, ib, tc4 * 512:(tc4 + 1) * 512], g_t[:], w_t[:, tc4 * 512:(tc4 + 1) * 512], ALU.mult, [g_r, w_r], [r_WA[ib]])
                        for tt in range(NTL):
                            for ch in range(2):
                                pb, pb_r = p2.next()
                                for ib in range(IB):
                                    MM(pb[:], WA[:, ib, tt * 128:(tt + 1) * 128], Vb[:, ib, ch * 512:(ch + 1) * 512], ib == 0, ib == IB - 1, [r_WA[ib], r_Vb[ib]], [pb_r])
                                TT("dve", Yacc[:, tt, ch * 512:(ch + 1) * 512], Yacc[:, tt, ch * 512:(ch + 1) * 512], pb[:], ALU.add, [rY[tt], pb_r], [rY[tt]])


                with Scope(kb) as st3:
                    Wgt = sb(st3, "Wgt", [128, 8, D], BF16); r_Wgt = Res()
                    Wp = sb(st3, "Wp", [128, 2, D], BF16); r_Wp = Res()
                    rep3 = sb(st3, "rep3", [128, 3, D], F32); r_rep3 = Res()
                    DMA("dsp", rep3[:], rep4[:, 1:4, :], [], [r_rep3])
                    wst3 = Ring([sb(st3, f"wst3{i}", [128, D], F32) for i in range(2)])
                    for (src, dstw, dres, nch) in ((ple_wg, Wgt, r_Wgt, 8), (ple_pj, Wp, r_Wp, 2)):
                        for dc in range(nch):
                            w_t, w_r = wst3.next()
                            DMA("dsp" if dc % 2 == 0 else "dact", w_t[:], src[dc * 128:(dc + 1) * 128, :], [], [w_r])
                            CP("dve" if dc % 2 == 0 else "pool", dstw[:, dc, :], w_t[:], [w_r], [dres])
                    x3_ring = Ring([sb(st3, f"x3{i}", [128, D], F32) for i in range(2)])
                    x3b_ring = Ring([sb(st3, f"x3b{i}", [128, D], BF16) for i in range(2)])
                    x3T_ring = Ring([sb(st3, f"x3T{i}", [128, 8, 128], BF16) for i in range(2)])
                    pht = Ring([sb(st3, f"pht{i}", [128, 256], F32) for i in range(2)])
                    phb = Ring([sb(st3, f"phb{i}", [128, 256], BF16) for i in range(2)])
                    phT = Ring([sb(st3, f"phT{i}", [128, 2, 128], BF16) for i in range(2)])
                    gt_ring = Ring([sb(st3, f"gtD{i}", [128, D], F32) for i in range(2)])
                    ot_ring = Ring([sb(st3, f"otD{i}", [128, D], F32) for i in range(2)])
                    junk3 = sb(st3, "junk3", [128, D], BF16); r_junk3 = Res()
                    sm3 = Ring([sb(st3, f"sm3{i}", [128, 8], F32) for i in range(4)])

                    def rms3(src, src_r, gi, dstf, dstf_r):
                        s_t, s_r = sm3.next()
                        ACTF(junk3[:], src, AF.Square, [src_r], [r_junk3, s_r], accum_out=s_t[:, 0:1])
                        TS("dve", s_t[:, 1:2], s_t[:, 0:1], 1.0 / D, EPS, ALU.mult, ALU.add, [s_r], [s_r])
                        ACTF(s_t[:, 1:2], s_t[:, 1:2], AF.Sqrt, [s_r], [s_r])
                        E("dve", lambda: nc.vector.reciprocal(out=s_t[:, 2:3], in_=s_t[:, 1:2]), [s_r], [s_r])
                        STT(dstf, src, s_t[:, 2:3], rep3[:, gi, :], ALU.mult, ALU.mult, [src_r, s_r, r_rep3], [dstf_r])

                    def tr3(srcb, srcb_r, dstT, dstT_r, nblk):
                        pt, pt_r = ptb2.next()
                        for dc in range(nblk):
                            TR(pt[:, dc * 128:(dc + 1) * 128], srcb[:, dc * 128:(dc + 1) * 128], ident_b[:], [srcb_r, r_identb], [pt_r])
                        CP("act", dstT.rearrange("p a t -> p (a t)"), pt[:, 0:nblk * 128], [pt_r], [dstT_r])

                    for it in range(NTL):
                        tsl = slice(it * 128, (it + 1) * 128)
                        Hh = Yacc[:, it, :]; H_r = rY[it]
                        x3, x3_r = x3_ring.next()
                        rms3(Hh, H_r, 0, x3[:], x3_r)
                        x3b, x3b_r = x3b_ring.next()
                        CP("pool", x3b[:], x3[:], [x3_r], [x3b_r])
                        x3T, x3T_r = x3T_ring.next()
                        tr3(x3b, x3b_r, x3T[:], x3T_r, 8)
                        ph_t, ph_r = pht.next()
                        DMA("dact", ph_t[:], ph[tsl, :], [], [ph_r])
                        pb_t, pb_r = phb.next()
                        CP("pool", pb_t[:], ph_t[:], [ph_r], [pb_r])
                        pT_t, pT_r = phT.next()
                        tr3(pb_t, pb_r, pT_t[:], pT_r, 2)
                        gt, gt_r = gt_ring.next()
                        for ch in range(2):
                            csl = slice(ch * 512, (ch + 1) * 512)
                            pa, pa_r = p1.next()
                            for dc in range(8):
                                MM(pa[:], x3T[:, dc, :], Wgt[:, dc, csl], dc == 0, dc == 7, [x3T_r, r_Wgt], [pa_r])
                            TT("dve", gt[:, csl], pa[:], rep3[:, 2, csl], ALU.add, [pa_r, r_rep3], [gt_r])
                            ACTF(gt[:, csl], gt[:, csl], AF.Sigmoid, [gt_r], [gt_r])
                            pb2, pb2_r = p2.next()
                            for dc in range(2):
                                MM(pb2[:], pT_t[:, dc, :], Wp[:, dc, csl], dc == 0, dc == 1, [pT_r, r_Wp], [pb2_r])
                            TT("dve", gt[:, csl], gt[:, csl], pb2[:], ALU.mult, [gt_r, pb2_r], [gt_r])
                            TT("pool", Yacc[:, it, csl], Yacc[:, it, csl], gt[:, csl], ALU.add, [H_r, gt_r], [H_r])
                        ot, ot_r = ot_ring.next()
                        rms3(Hh, H_r, 1, ot[:], ot_r)
                        DMA("dsp", out[tsl, :], ot[:], [ot_r], [])

        kb.drain_all()
    return nc


def _blockdiag(w):
    o = np.zeros((128, 4, 128), np.float32)
    for n in range(8):
        cc, j = n // 2, n % 2
        o[j * 64:(j + 1) * 64, cc, j * 64:(j + 1) * 64] = w[n]
    return o


def _rel_bucket(dist):
    n = np.maximum(dist, 0)
    nf = np.maximum(n, 16).astype(np.float32)
    large = 16 + (np.log(nf / np.float32(16)) / np.float32(np.log(8.0)) * np.float32(16)).astype(np.int32)
    large = np.minimum(large, 31)
    return np.where(n < 16, n, large)


def _nsa_consts(rel_table):
    c = {}
    assert (_rel_bucket(np.arange(113, 8192)) == 31).all()
    cl = np.arange(128)[:, None, None]; mp = np.arange(5)[None, :, None]; tt = np.arange(512)[None, None, :]
    dist = 512 * mp + tt - 16 * cl - 31
    c["bc_g"] = np.ascontiguousarray(rel_table[_rel_bucket(dist)].transpose(3, 0, 1, 2))
    c["bc_m"] = np.where(dist >= 0, 0.0, NEGM).astype(np.float32)
    assert (512 * 5 - 16 * 127 - 31) >= 113
    sl = np.arange(128)[:, None]; tl = np.arange(128)[None, :]
    d0 = tl - sl; d1 = 128 + tl - sl
    g0 = rel_table[_rel_bucket(d0)]; g1 = rel_table[_rel_bucket(d1)]
    c["bd_g"] = np.ascontiguousarray(np.stack([g0, g1], 0).transpose(3, 1, 0, 2))
    m0 = np.where(d0 >= 0, 0.0, NEGM); m2 = np.where(tl < sl, 0.0, NEGM)
    c["bd_m"] = np.ascontiguousarray(np.stack([m0, np.zeros_like(m0), m2], 1)).astype(np.float32)
    c["t31"] = np.ascontiguousarray(np.broadcast_to(rel_table[31][None, :], (128, 8))).astype(np.float32)
    t = (np.arange(NT)[None, :, None] * 128 + np.arange(128)[:, None, None])
    blk = np.arange(64)[None, None, :]
    d = t // 64 - blk
    local = (d >= 0) & (d < 2)
    init = (blk == 0) & ~local
    past = (d >= 0) & ~local & ~init
    c["force_c"] = np.where(local, 2.0e4, np.where(init, 1.0e4, np.where(past, 0.0, -1.0))).astype(np.float32)
    c["keep_c"] = past.astype(np.float32)
    cs = np.arange(256)[:, None] * 16; ss = np.arange(64)[None, :] * 64
    ov = np.clip(np.minimum(cs + 32, ss + 64) - np.maximum(cs, ss), 0, None).astype(np.float32) / 32.0
    ove = np.concatenate([ov, np.ones((256, 1), np.float32)], 1)
    ove[255] = 0.0
    c["ovl_ext"] = np.ascontiguousarray(ove.reshape(2, 128, 65).transpose(1, 0, 2))
    c["erows"] = (np.arange(T)[None, :] // 64 == np.arange(64)[:, None]).astype(np.float32)
    return c


def _prep_inputs(inputs):
    x = np.ascontiguousarray(inputs["x"], dtype=np.float32)
    p = np.ascontiguousarray(inputs["p"], dtype=np.float32)
    shared = {
        "ident": np.eye(128, dtype=np.float32),
        "w_in": np.ascontiguousarray(inputs["w_in"][0]),
        "g_attn": np.ascontiguousarray(inputs["attn_norm"][0].reshape(8, 128).T),
        "lru_cw": np.ascontiguousarray(inputs["conv_w"][0][:, 0, :].reshape(4, 4, 128).transpose(2, 1, 0)),
        "lru_vec": np.ascontiguousarray(np.stack([inputs[k][0].reshape(4, 128) for k in
                                                  ("conv_b", "lru_ba", "lru_bx", "lru_lambda", "grp_norm_lru")], 0).transpose(2, 0, 1)),
        "cmp_w1": np.ascontiguousarray(np.stack([inputs[k][0].reshape(16, 128, 256).transpose(1, 0, 2) for k in ("cmp_k_w1", "cmp_v_w1")], 0)),
        "cmp_pe": np.ascontiguousarray(np.stack([inputs[k][0].reshape(16, 128).T for k in ("cmp_k_pe", "cmp_v_pe")], 0)),
        "cmp_w2": np.ascontiguousarray(np.stack([inputs[k][0].reshape(2, 128, 64).transpose(1, 0, 2) for k in ("cmp_k_w2", "cmp_v_w2")], 0)),
        "ga_rep": np.ascontiguousarray(np.broadcast_to(inputs["grp_norm_attn"][0][None, :], (128, 512))).astype(np.float32),
        "w_out": np.ascontiguousarray(inputs["w_out"][0]),
        "peer_wq": np.ascontiguousarray(inputs["peer_wq"][0]),
        "sk_T": np.ascontiguousarray(inputs["peer_subkeys"][0].transpose(0, 2, 1)),
        "peer_u": np.ascontiguousarray(inputs["peer_u"][0]),
        "peer_v": np.ascontiguousarray(inputs["peer_v"][0]),
        "ple_wgate": np.ascontiguousarray(inputs["ple_wgate"][0]),
        "ple_proj": np.ascontiguousarray(inputs["ple_proj"][0]),
        "rep4": np.ascontiguousarray(np.broadcast_to(np.stack([inputs["ffn_norm"][0], inputs["ple_norm"][0], inputs["final_norm"], inputs["ple_bgate"][0]], 0)[None], (128, 4, D))).astype(np.float32),
        "iota128": np.ascontiguousarray(np.broadcast_to(np.arange(128, dtype=np.float32)[None], (128, 128))),
        "iota16": np.ascontiguousarray(np.broadcast_to(np.arange(16, dtype=np.float32)[None], (128, 16))),
        "lru_bda": _blockdiag(inputs["lru_wa"][0]),
        "lru_bdx": _blockdiag(inputs["lru_wx"][0]),
    }
    shared.update(_nsa_consts(np.asarray(inputs["rel_table"], np.float32)))
    in_maps = []
    for c in range(8):
        b, hf = c // 2, c % 2
        m = dict(shared)
        m["xb"] = x[b]
        m["xh"] = np.ascontiguousarray(x[b, hf * TH:(hf + 1) * TH])
        m["ph"] = np.ascontiguousarray(p[0, b, hf * TH:(hf + 1) * TH])
        sel = np.zeros((128, 2), np.float32); sel[:, hf] = 1.0
        m["selc"] = sel
        in_maps.append(m)
    return in_maps


def kernel(**inputs):
    nc = build_program()
    in_maps = _prep_inputs(inputs)
    res = run_bass_kernel_spmd(nc, in_maps, core_ids=list(range(8)))
    outp = np.zeros((4, T, D), np.float32)
    for c in range(8):
        b, hf = c // 2, c % 2
        outp[b, hf * TH:(hf + 1) * TH] = res.results[c]["out"]
    return outp
```

```python
import numpy as np
from contextlib import ExitStack
import concourse.bass as bass
import concourse.mybir as mybir
from concourse.bass_utils import run_bass_kernel_spmd

F32 = mybir.dt.float32
BF16 = mybir.dt.bfloat16
U32 = mybir.dt.uint32
AF = mybir.ActivationFunctionType
ALU = mybir.AluOpType
AX = mybir.AxisListType

T = 4096
D = 1024
NT = T // 128
TH = 2048
IN_COLS = 2328
EPS = 1e-6
NEGM = -30000.0


class Res:
    __slots__ = ("name", "lw", "rd")

    def __init__(self, name=""):
        self.name = name
        self.lw = None
        self.rd = {}


class KB:
    NDMA = 4

    def __init__(self, nc, stack):
        self.nc = nc
        self.issue = {"pe": nc.tensor, "act": nc.scalar, "dve": nc.vector, "pool": nc.gpsimd,
                      "dsp": nc.sync, "dact": nc.scalar, "dpool": nc.gpsimd}
        self.stream = {"pe": "pe", "act": "act", "dve": "dve", "pool": "pool",
                       "dsp": "sp", "dact": "act", "dpool": "pool"}
        self.sems = {}
        self.cnt = {}
        for q in self.issue:
            n = self.NDMA if self.is_dma(q) else 1
            self.sems[q] = [stack.enter_context(nc.semaphore(f"s_{q}{i}")) for i in range(n)]
            self.cnt[q] = 0
        self.waited = {s: {} for s in ("pe", "act", "dve", "pool", "sp")}
        self.ninst = 0
        self._rr = 0

    @staticmethod
    def is_dma(q):
        return q in ("dsp", "dact", "dpool")

    @staticmethod
    def _need(need, dep):
        if dep is None:
            return
        q, c = dep
        if need.get(q, 0) < c:
            need[q] = c

    def _waits(self, st, eng, need, skip_q=None):
        for dq, c in need.items():
            if dq == "pe" and skip_q == "pe":
                continue
            if self.is_dma(dq):
                n = self.NDMA
                for si in range(n):
                    k = (c - 1 - si) // n + 1 if c - 1 >= si else 0
                    if k <= 0:
                        continue
                    key = (dq, si)
                    if self.waited[st].get(key, 0) >= k:
                        continue
                    eng.wait_ge(self.sems[dq][si], 16 * k)
                    self.waited[st][key] = k
            else:
                key = (dq, 0)
                if self.waited[st].get(key, 0) >= c:
                    continue
                eng.wait_ge(self.sems[dq][0], c)
                self.waited[st][key] = c

    def emit(self, q, fn, reads=(), writes=()):
        need = {}
        for r in reads:
            self._need(need, r.lw)
        for w in writes:
            self._need(need, w.lw)
            for rq, rc in w.rd.items():
                self._need(need, (rq, rc))
        st = self.stream[q]
        self._waits(st, self.issue[q], need, skip_q=q)
        inst = fn()
        self.cnt[q] += 1
        c = self.cnt[q]
        if self.is_dma(q):
            inst.then_inc(self.sems[q][(c - 1) % self.NDMA], 16)
        else:
            inst.then_inc(self.sems[q][0], 1)
        for r in reads:
            if r.rd.get(q, 0) < c:
                r.rd[q] = c
        for w in writes:
            w.lw = (q, c)
            w.rd = {}
        self.ninst += 1
        return inst

    def dmaq(self):
        self._rr ^= 1
        return "dsp" if self._rr else "dact"

    def barrier(self):
        need = {q: c for q, c in self.cnt.items() if c > 0}
        for st, eng in (("pe", self.nc.tensor), ("act", self.nc.scalar), ("dve", self.nc.vector),
                        ("pool", self.nc.gpsimd), ("sp", self.nc.sync)):
            self._waits(st, eng, dict(need))

    def drain_all(self):
        need = {q: c for q, c in self.cnt.items() if c > 0}
        self._waits("sp", self.nc.sync, need)


class Scope:
    def __init__(self, kb):
        self.kb = kb
        self.st = ExitStack()

    def __enter__(self):
        self.st.__enter__()
        return self.st

    def __exit__(self, *a):
        if a[0] is None:
            self.kb.barrier()
        return self.st.__exit__(*a)


class Ring:
    def __init__(self, tiles):
        self.tiles = tiles
        self.res = [Res() for _ in tiles]
        self.i = -1

    def next(self):
        self.i = (self.i + 1) % len(self.tiles)
        return self.tiles[self.i], self.res[self.i]


def build_program(dbg=None, phases=("A", "B", "C", "D")):
    nc = bass.Bass("TRN2", target_bir_lowering=False)

    def din(name, shape, dt=F32):
        return nc.dram_tensor(name, list(shape), dt, kind="ExternalInput").ap()

    dbg = dbg or ()

    def dscr(name, shape, dt):
        kind = "ExternalOutput" if name in dbg else "Internal"
        return nc.dram_tensor(name, list(shape), dt, kind=kind).ap()

    xb = din("xb", [T, D])
    xh = din("xh", [TH, D])
    ph = din("ph", [TH, 256])
    selc = din("selc", [128, 2])
    ident = din("ident", [128, 128])
    w_in = din("w_in", [D, IN_COLS])
    g_attn = din("g_attn", [128, 8])
    out = nc.dram_tensor("out", [TH, D], F32, kind="ExternalOutput").ap()
    lru_cw = din("lru_cw", [128, 4, 4])
    lru_vec = din("lru_vec", [128, 5, 4])
    lru_bda = din("lru_bda", [128, 4, 128])
    lru_bdx = din("lru_bdx", [128, 4, 128])

    qT_s = dscr("qT_s", [512, T], BF16)
    kcT_s = dscr("kcT_s", [128, T], BF16)
    vcT_s = dscr("vcT_s", [128, T], BF16)
    ksT_s = dscr("ksT_s", [128, T], BF16)
    kwT_s = dscr("kwT_s", [128, T], BF16)
    vs_s = dscr("vs_s", [T, 128], BF16)
    vw_s = dscr("vw_s", [T, 128], BF16)
    gates_s = dscr("gates_s", [T, 24], F32)
    xrT_s = dscr("xrT_s", [512, T], F32)
    xgT_s = dscr("xgT_s", [512, T], F32)

    cmp_w1 = din("cmp_w1", [2, 128, 16, 256])
    cmp_pe = din("cmp_pe", [2, 128, 16])
    cmp_w2 = din("cmp_w2", [2, 128, 2, 64])
    ovl_ext = din("ovl_ext", [128, 2, 65])
    bc_g = din("bc_g", [8, 128, 5, 512])
    bc_m = din("bc_m", [128, 5, 512])
    bd_g = din("bd_g", [8, 128, 2, 128])
    bd_m = din("bd_m", [128, 3, 128])
    t31_in = din("t31", [128, 8])
    force_in = din("force_c", [128, 32, 64])
    keep_in = din("keep_c", [128, 32, 64])
    erows = din("erows", [64, T])
    ga_in = din("ga_rep", [128, 512])
    w_out_in = din("w_out", [D, D])
    peer_wq = din("peer_wq", [D, 2048])
    sk_T = din("sk_T", [2, 128, 128])
    peer_u = din("peer_u", [16384, D])
    peer_v = din("peer_v", [16384, D])
    ple_wg = din("ple_wgate", [D, D])
    ple_pj = din("ple_proj", [256, D])
    rep4 = din("rep4", [128, 4, D])
    iota16 = din("iota16", [128, 16])
    iota128 = din("iota128", [128, 128])
    H1_s = dscr("H1_s", [TH, D], F32)
    xnT2_s = dscr("xnT2_s", [128, 8, TH], BF16)
    Wt_s = dscr("Wt_s", [TH // 128, 128, 128, 128], BF16)
    mixT_s = dscr("mixT_s", [1024, T], BF16)
    R = {n: Res(n) for n in ("H1_s", "xnT2_s", "Wt_s", "mixT_s", "qT_s", "kcT_s", "vcT_s", "ksT_s", "kwT_s", "vs_s", "vw_s", "gates_s", "xrT_s", "xgT_s")}

    with ExitStack() as top:
        kb = KB(nc, top)
        E = kb.emit

        uniq = [0]

        def sb(st, name, shape, dt):
            uniq[0] += 1
            return st.enter_context(nc.sbuf_tensor(f"sb{uniq[0]}_{name}", list(shape), dt))

        def ps(st, name, shape, dt):
            uniq[0] += 1
            return st.enter_context(nc.psum_tensor(f"ps{uniq[0]}_{name}", list(shape), dt))


        def MM(out_, lhsT, rhs, start, stop, reads, writes):
            return E("pe", lambda: nc.tensor.matmul(out_, lhsT=lhsT, rhs=rhs, start=start, stop=stop), reads, writes)

        def TR(out_, in_, idt, reads, writes):
            return E("pe", lambda: nc.tensor.transpose(out=out_, in_=in_, identity=idt), reads, writes)

        def ACTF(out_, in_, func, reads, writes, **kw):
            return E("act", lambda: nc.scalar.activation(out=out_, in_=in_, func=func, **kw), reads, writes)

        def veng(q):
            return nc.vector if q == "dve" else nc.gpsimd

        def TS(q, out_, in0, s1, s2, op0, op1, reads, writes):
            if op1 is None:
                return E(q, lambda: veng(q).tensor_scalar(out=out_, in0=in0, scalar1=s1, scalar2=None, op0=op0), reads, writes)
            return E(q, lambda: veng(q).tensor_scalar(out=out_, in0=in0, scalar1=s1, scalar2=s2, op0=op0, op1=op1), reads, writes)

        def TT(q, out_, in0, in1, op, reads, writes):
            return E(q, lambda: veng(q).tensor_tensor(out=out_, in0=in0, in1=in1, op=op), reads, writes)

        def STT(out_, in0, scalar, in1, op0, op1, reads, writes, **kw):
            return E("dve", lambda: nc.vector.scalar_tensor_tensor(out=out_, in0=in0, scalar=scalar, in1=in1, op0=op0, op1=op1, **kw), reads, writes)

        def CP(q, out_, in_, reads, writes):
            if q == "act":
                return E("act", lambda: nc.scalar.copy(out=out_, in_=in_), reads, writes)
            return E(q, lambda: veng(q).tensor_copy(out=out_, in_=in_), reads, writes)

        def MSET(q, out_, val, writes):
            return E(q, lambda: veng(q).memset(out_, val), (), writes)

        def DMA(q, out_, in_, reads, writes):
            eng = {"dsp": nc.sync, "dact": nc.scalar, "dpool": nc.gpsimd}[q]
            return E(q, lambda: eng.dma_start(out=out_, in_=in_), reads, writes)

        def dump(name, ap, shape, dt, res):
            if name not in dbg:
                return
            d = nc.dram_tensor(name, list(shape), dt, kind="ExternalOutput").ap()
            DMA("dsp", d, ap, [res] if not isinstance(res, list) else res, [])

        ident_f = sb(top, "ident_f", [128, 128], F32); r_identf = Res()
        ident_b = sb(top, "ident_b", [128, 128], BF16); r_identb = Res()
        E("dsp", lambda: nc.sync.dma_start(out=ident_f[:], in_=ident), writes=[r_identf])
        E("dve", lambda: nc.vector.tensor_copy(out=ident_b[:], in_=ident_f[:]), reads=[r_identf], writes=[r_identb])

        if "A" in phases:
            with Scope(kb) as st:
                Wg = sb(st, "Wg", [128, 8, IN_COLS], BF16); r_Wg = Res()
                gcol = sb(st, "gcol", [128, 8], F32); r_gcol = Res()
                wst = Ring([sb(st, f"wst{i}", [128, IN_COLS], F32) for i in range(2)])
                E("dsp", lambda: nc.sync.dma_start(out=gcol[:], in_=g_attn), writes=[r_gcol])
                for dc in range(8):
                    w_t, w_r = wst.next()
                    E("dsp" if dc % 2 == 0 else "dact",
                      (lambda w_t=w_t, dc=dc: nc.sync.dma_start(out=w_t[:], in_=w_in[dc * 128:(dc + 1) * 128, :])) if dc % 2 == 0 else
                      (lambda w_t=w_t, dc=dc: nc.scalar.dma_start(out=w_t[:], in_=w_in[dc * 128:(dc + 1) * 128, :])),
                      writes=[w_r])
                    eng = "dve" if dc % 2 == 0 else "pool"
                    ve = nc.vector if dc % 2 == 0 else nc.gpsimd
                    E(eng, lambda ve=ve, w_t=w_t, dc=dc: ve.tensor_scalar(out=Wg[:, dc, :], in0=w_t[:], scalar1=gcol[:, dc:dc + 1], scalar2=None, op0=ALU.mult),
                      reads=[w_r, r_gcol], writes=[r_Wg])

                xt_ring = Ring([sb(st, f"xt{i}", [128, 4, D], F32) for i in range(2)])
                xnb_ring = Ring([sb(st, f"xnb{i}", [128, 4, D], BF16) for i in range(2)])
                xnT_ring = Ring([sb(st, f"xnT{i}", [128, 8, 512], BF16) for i in range(2)])
                junk = sb(st, "junkA", [128, D], BF16); r_junk = Res()
                ss_ring = Ring([sb(st, f"ss{i}", [128, 8], F32) for i in range(2)])
                pT_ring = Ring([ps(st, f"pT{i}", [128, 512], BF16) for i in range(2)])
                pacc = Ring([ps(st, f"pacc{i}", [128, 512], F32) for i in range(4)])
                ostf = Ring([sb(st, f"ostf{i}", [128, 512], F32) for i in range(3)])
                ostb = Ring([sb(st, f"ostb{i}", [128, 512], BF16) for i in range(3)])
                osv = Ring([sb(st, f"osv{i}", [128, 256], BF16) for i in range(2)])
                osg = Ring([sb(st, f"osg{i}", [128, 24], F32) for i in range(2)])
                xb_v = xb.rearrange("(n p) d -> p n d", p=128)
                fm = []
                for cc in range(4):
                    fm.append((cc * 128, qT_s[cc * 128:(cc + 1) * 128, :], 0.125, True, R["qT_s"]))
                fm.append((512, kcT_s, 1.0, True, R["kcT_s"]))
                fm.append((640, vcT_s, 1.0, True, R["vcT_s"]))
                fm.append((768, ksT_s, 1.0, True, R["ksT_s"]))
                fm.append((1024, kwT_s, 1.0, True, R["kwT_s"]))
                for cc in range(4):
                    fm.append((1304 + cc * 128, xrT_s[cc * 128:(cc + 1) * 128, :], 1.0, False, R["xrT_s"]))
                for cc in range(4):
                    fm.append((1816 + cc * 128, xgT_s[cc * 128:(cc + 1) * 128, :], 1.0, False, R["xgT_s"]))
                ev = 0
                for tcn in range(8):
                    xt, xt_r = xt_ring.next()
                    E("dsp", lambda xt=xt, tcn=tcn: nc.sync.dma_start(out=xt[:], in_=xb_v[:, tcn * 4:(tcn + 1) * 4, :]), writes=[xt_r])
                    ss, ss_r = ss_ring.next()
                    for n in range(4):
                        E("act", lambda xt=xt, ss=ss, n=n: nc.scalar.activation(out=junk[:], in_=xt[:, n, :], func=AF.Square, accum_out=ss[:, n:n + 1]),
                          reads=[xt_r], writes=[r_junk, ss_r])
                    E("dve", lambda ss=ss: nc.vector.tensor_scalar(out=ss[:, 4:8], in0=ss[:, 0:4], scalar1=1.0 / D, scalar2=EPS, op0=ALU.mult, op1=ALU.add), reads=[ss_r], writes=[ss_r])
                    E("act", lambda ss=ss: nc.scalar.activation(out=ss[:, 4:8], in_=ss[:, 4:8], func=AF.Sqrt), reads=[ss_r], writes=[ss_r])
                    E("dve", lambda ss=ss: nc.vector.reciprocal(out=ss[:, 4:8], in_=ss[:, 4:8]), reads=[ss_r], writes=[ss_r])
                    xnb, xnb_r = xnb_ring.next()
                    for n in range(4):
                        if n % 2 == 0:
                            E("dve", lambda xt=xt, xnb=xnb, ss=ss, n=n: nc.vector.tensor_scalar(out=xnb[:, n, :], in0=xt[:, n, :], scalar1=ss[:, 4 + n:5 + n], scalar2=None, op0=ALU.mult),
                              reads=[xt_r, ss_r], writes=[xnb_r])
                        else:
                            E("pool", lambda xt=xt, xnb=xnb, ss=ss, n=n: nc.gpsimd.tensor_scalar(out=xnb[:, n, :], in0=xt[:, n, :], scalar1=ss[:, 4 + n:5 + n], scalar2=None, op0=ALU.mult),
                              reads=[xt_r, ss_r], writes=[xnb_r])
                    xnT, xnT_r = xnT_ring.next()
                    for dc in range(8):
                        pT, pT_r = pT_ring.next()
                        for n in range(4):
                            E("pe", lambda pT=pT, xnb=xnb, n=n, dc=dc: nc.tensor.transpose(out=pT[:, n * 128:(n + 1) * 128], in_=xnb[:, n, dc * 128:(dc + 1) * 128], identity=ident_b[:]),
                              reads=[xnb_r, r_identb], writes=[pT_r])
                        if dc % 2 == 0:
                            E("act", lambda pT=pT, xnT=xnT, dc=dc: nc.scalar.copy(out=xnT[:, dc, :], in_=pT[:]), reads=[pT_r], writes=[xnT_r])
                        else:
                            E("dve", lambda pT=pT, xnT=xnT, dc=dc: nc.vector.tensor_copy(out=xnT[:, dc, :], in_=pT[:]), reads=[pT_r], writes=[xnT_r])
                    for (c0, dst, scale, isb, dres) in fm:
                        pa, pa_r = pacc.next()
                        for dc in range(8):
                            E("pe", lambda pa=pa, dc=dc, c0=c0, xnT=xnT: nc.tensor.matmul(pa[:], lhsT=Wg[:, dc, c0:c0 + 128], rhs=xnT[:, dc, :], start=(dc == 0), stop=(dc == 7)),
                              reads=[r_Wg, xnT_r], writes=[pa_r])
                        o_t, o_r = (ostb if isb else ostf).next()
                        ev += 1
                        if ev % 2 == 0:
                            E("act", lambda o_t=o_t, pa=pa, scale=scale: nc.scalar.activation(out=o_t[:], in_=pa[:], func=AF.Copy, scale=scale), reads=[pa_r], writes=[o_r])
                        else:
                            E("dve", lambda o_t=o_t, pa=pa, scale=scale: nc.vector.tensor_scalar(out=o_t[:], in0=pa[:], scalar1=scale, scalar2=None, op0=ALU.mult), reads=[pa_r], writes=[o_r])
                        if ev % 2 == 0:
                            E("dsp", lambda o_t=o_t, dst=dst, tcn=tcn: nc.sync.dma_start(out=dst[:, tcn * 512:(tcn + 1) * 512], in_=o_t[:]), reads=[o_r], writes=[dres])
                        else:
                            E("dpool", lambda o_t=o_t, dst=dst, tcn=tcn: nc.gpsimd.dma_start(out=dst[:, tcn * 512:(tcn + 1) * 512], in_=o_t[:]), reads=[o_r], writes=[dres])
                    for n in range(4):
                        t0 = tcn * 512 + n * 128
                        pa, pa_r = pacc.next()
                        for dc in range(8):
                            E("pe", lambda pa=pa, dc=dc, xnT=xnT, n=n: nc.tensor.matmul(pa[:, 0:128], lhsT=xnT[:, dc, n * 128:(n + 1) * 128], rhs=Wg[:, dc, 896:1024], start=(dc == 0), stop=(dc == 7)),
                              reads=[r_Wg, xnT_r], writes=[pa_r])
                        pb, pb_r = pacc.next()
                        for dc in range(8):
                            E("pe", lambda pb=pb, dc=dc, xnT=xnT, n=n: nc.tensor.matmul(pb[:, 0:152], lhsT=xnT[:, dc, n * 128:(n + 1) * 128], rhs=Wg[:, dc, 1152:1304], start=(dc == 0), stop=(dc == 7)),
                              reads=[r_Wg, xnT_r], writes=[pb_r])
                        ov, ov_r = osv.next()
                        og, og_r = osg.next()
                        E("act", lambda ov=ov, pa=pa: nc.scalar.copy(out=ov[:, 0:128], in_=pa[:, 0:128]), reads=[pa_r], writes=[ov_r])
                        E("dve", lambda ov=ov, pb=pb: nc.vector.tensor_copy(out=ov[:, 128:256], in_=pb[:, 0:128]), reads=[pb_r], writes=[ov_r])
                        E("dve", lambda og=og, pb=pb: nc.vector.tensor_copy(out=og[:], in_=pb[:, 128:152]), reads=[pb_r], writes=[og_r])
                        E("dsp", lambda ov=ov, t0=t0: nc.sync.dma_start(out=vs_s[t0:t0 + 128, :], in_=ov[:, 0:128]), reads=[ov_r], writes=[R["vs_s"]])
                        E("dpool", lambda ov=ov, t0=t0: nc.gpsimd.dma_start(out=vw_s[t0:t0 + 128, :], in_=ov[:, 128:256]), reads=[ov_r], writes=[R["vw_s"]])
                        E("dsp", lambda og=og, t0=t0: nc.sync.dma_start(out=gates_s[t0:t0 + 128, :], in_=og[:]), reads=[og_r], writes=[R["gates_s"]])

        if "B" in phases:
            with Scope(kb) as st:
                cw = sb(st, "cw", [128, 4, 4], F32); r_cw = Res()
                lv = sb(st, "lv", [128, 5, 4], F32); r_lv = Res()
                clc = sb(st, "clc", [128, 3, 4], F32); r_clc = Res()
                bdf = sb(st, "bdf", [128, 2, 4, 128], F32); r_bdf = Res()
                bdb = sb(st, "bdb", [128, 2, 4, 128], BF16); r_bdb = Res()
                ones_b = sb(st, "ones_b", [128, 128], BF16); r_ones = Res()
                E("dsp", lambda: nc.sync.dma_start(out=cw[:], in_=lru_cw), writes=[r_cw])
                E("dact", lambda: nc.scalar.dma_start(out=lv[:], in_=lru_vec), writes=[r_lv])
                E("dsp", lambda: nc.sync.dma_start(out=bdf[:, 0], in_=lru_bda), writes=[r_bdf])
                E("dact", lambda: nc.scalar.dma_start(out=bdf[:, 1], in_=lru_bdx), writes=[r_bdf])
                E("dve", lambda: nc.vector.tensor_copy(out=bdb[:], in_=bdf[:]), reads=[r_bdf], writes=[r_bdb])
                E("dve", lambda: nc.vector.memset(ones_b[:], 1.0), writes=[r_ones])
                E("act", lambda: nc.scalar.activation(out=clc[:, 0, :], in_=lv[:, 3, :], func=AF.Exp, scale=-1.0), reads=[r_lv], writes=[r_clc])
                E("act", lambda: nc.scalar.activation(out=clc[:, 0, :], in_=clc[:, 0, :], func=AF.Ln, bias=1.0), reads=[r_clc], writes=[r_clc])
                E("dve", lambda: nc.vector.tensor_scalar(out=clc[:, 1, :], in0=clc[:, 0, :], scalar1=-8.0, scalar2=None, op0=ALU.mult), reads=[r_clc], writes=[r_clc])
                E("dve", lambda: nc.vector.tensor_scalar(out=clc[:, 2, :], in0=clc[:, 0, :], scalar1=-16.0, scalar2=None, op0=ALU.mult), reads=[r_clc], writes=[r_clc])
                L = sb(st, "Lall", [128, 4, T], F32); r_L = Res()
                X = [sb(st, f"lruX{i}", [128, T], F32) for i in range(5)]
                rX = [Res() for _ in range(5)]
                xcb = sb(st, "xcb", [128, T], BF16); r_xcb = Res()
                pg = Ring([ps(st, f"pg{i}", [128, 512], F32) for i in range(4)])
                for cc in range(4):
                    X1, X2, X3, X4, X5 = X
                    r1, r2, r3, r4, r5 = rX
                    for hh in range(2):
                        E("dsp", lambda cc=cc, hh=hh: nc.sync.dma_start(out=X1[:, hh * 2048:(hh + 1) * 2048], in_=xrT_s[cc * 128:(cc + 1) * 128, hh * 2048:(hh + 1) * 2048]), reads=[R["xrT_s"]], writes=[r1])
                        E("dact", lambda cc=cc, hh=hh: nc.scalar.dma_start(out=X3[:, hh * 2048:(hh + 1) * 2048], in_=xgT_s[cc * 128:(cc + 1) * 128, hh * 2048:(hh + 1) * 2048]), reads=[R["xgT_s"]], writes=[r3])
                    E("dve", lambda cc=cc: nc.vector.tensor_scalar(out=X2[:], in0=X1[:], scalar1=cw[:, cc, 3:4], scalar2=lv[:, 0, cc:cc + 1], op0=ALU.mult, op1=ALU.add), reads=[r1, r_cw, r_lv], writes=[r2])
                    for sh in (1, 2, 3):
                        E("dve", lambda cc=cc, sh=sh: nc.vector.scalar_tensor_tensor(out=X2[:, sh:T], in0=X1[:, 0:T - sh], scalar=cw[:, cc, 3 - sh:4 - sh], in1=X2[:, sh:T], op0=ALU.mult, op1=ALU.add), reads=[r1, r2, r_cw], writes=[r2])
                    E("pool", lambda: nc.gpsimd.tensor_copy(out=xcb[:], in_=X2[:]), reads=[r2], writes=[r_xcb])
                    for gi, (Xo, ro, bi) in enumerate(((X4, r4, 1), (X5, r5, 2))):
                        for tcn in range(8):
                            pgt, pg_r = pg.next()
                            E("pe", lambda pgt=pgt, gi=gi, cc=cc, tcn=tcn: nc.tensor.matmul(pgt[:], lhsT=bdb[:, gi, cc, :], rhs=xcb[:, tcn * 512:(tcn + 1) * 512], start=True, stop=True), reads=[r_bdb, r_xcb], writes=[pg_r])
                            E("act", lambda pgt=pgt, Xo=Xo, bi=bi, cc=cc, tcn=tcn: nc.scalar.activation(out=Xo[:, tcn * 512:(tcn + 1) * 512], in_=pgt[:], func=AF.Sigmoid, bias=lv[:, bi, cc:cc + 1]), reads=[pg_r, r_lv], writes=[ro])
                    E("act", lambda cc=cc: nc.scalar.activation(out=X1[:], in_=X4[:], func=AF.Exp, scale=clc[:, 1, cc:cc + 1]), reads=[r4, r_clc], writes=[r1])
                    E("act", lambda cc=cc: nc.scalar.activation(out=X4[:], in_=X4[:], func=AF.Exp, scale=clc[:, 2, cc:cc + 1]), reads=[r4, r_clc], writes=[r4])
                    E("act", lambda: nc.scalar.activation(out=X4[:], in_=X4[:], func=AF.Sqrt, scale=-1.0, bias=1.0), reads=[r4], writes=[r4])
                    E("pool", lambda: nc.gpsimd.tensor_tensor(out=X5[:], in0=X5[:], in1=X2[:], op=ALU.mult), reads=[r5, r2], writes=[r5])
                    E("dve", lambda: nc.vector.tensor_tensor(out=X4[:], in0=X4[:], in1=X5[:], op=ALU.mult), reads=[r4, r5], writes=[r4])
                    E("dve", lambda: nc.vector.tensor_tensor_scan(out=X2[:], data0=X1[:], data1=X4[:], initial=0.0, op0=ALU.mult, op1=ALU.add), reads=[r1, r4], writes=[r2])
                    E("act", lambda: nc.scalar.activation(out=X3[:], in_=X3[:], func=AF.Gelu_apprx_tanh), reads=[r3], writes=[r3])
                    E("pool", lambda cc=cc: nc.gpsimd.tensor_tensor(out=L[:, cc, :], in0=X2[:], in1=X3[:], op=ALU.mult), reads=[r2, r3], writes=[r_L])
                sq = Ring([sb(st, f"lsq{i}", [128, 512], BF16) for i in range(2)])
                rs_ring = Ring([sb(st, f"lrs{i}", [128, 512], F32) for i in range(2)])
                lo = Ring([sb(st, f"lo{i}", [128, 512], BF16) for i in range(3)])
                for tcn in range(8):
                    pgt, pg_r = pg.next()
                    for cc in range(4):
                        sq_t, sq_r = sq.next()
                        E("act", lambda sq_t=sq_t, cc=cc, tcn=tcn: nc.scalar.activation(out=sq_t[:], in_=L[:, cc, tcn * 512:(tcn + 1) * 512], func=AF.Square), reads=[r_L], writes=[sq_r])
                        E("pe", lambda pgt=pgt, sq_t=sq_t, cc=cc: nc.tensor.matmul(pgt[:], lhsT=ones_b[:], rhs=sq_t[:], start=(cc == 0), stop=(cc == 3)), reads=[r_ones, sq_r], writes=[pg_r])
                    rs_t, rs_r = rs_ring.next()
                    E("dve", lambda rs_t=rs_t, pgt=pgt: nc.vector.tensor_scalar(out=rs_t[:], in0=pgt[:], scalar1=1.0 / 512, scalar2=EPS, op0=ALU.mult, op1=ALU.add), reads=[pg_r], writes=[rs_r])
                    E("act", lambda rs_t=rs_t: nc.scalar.activation(out=rs_t[:], in_=rs_t[:], func=AF.Sqrt), reads=[rs_r], writes=[rs_r])
                    E("dve", lambda rs_t=rs_t: nc.vector.reciprocal(out=rs_t[:], in_=rs_t[:]), reads=[rs_r], writes=[rs_r])
                    for cc in range(4):
                        lo_t, lo_r = lo.next()
                        E("dve", lambda lo_t=lo_t, rs_t=rs_t, cc=cc, tcn=tcn: nc.vector.scalar_tensor_tensor(out=lo_t[:], in0=L[:, cc, tcn * 512:(tcn + 1) * 512], scalar=lv[:, 4, cc:cc + 1], in1=rs_t[:], op0=ALU.mult, op1=ALU.mult), reads=[r_L, rs_r, r_lv], writes=[lo_r])
                        E("dsp", lambda lo_t=lo_t, cc=cc, tcn=tcn: nc.sync.dma_start(out=mixT_s[512 + cc * 128:512 + (cc + 1) * 128, tcn * 512:(tcn + 1) * 512], in_=lo_t[:]), reads=[lo_r], writes=[R["mixT_s"]])

        if "C" in phases:
            with Scope(kb) as st:
                Aout = sb(st, "Aout", [128, NT, 512], BF16)
                rA = [Res() for _ in range(NT)]
                sig = sb(st, "sig", [128, NT, 24], F32); r_sig = Res()
                force_t = sb(st, "force_t", [128, NT, 64], F32); r_force = Res()
                keep_t = sb(st, "keep_t", [128, NT, 64], F32); r_keep = Res()
                t31 = sb(st, "t31", [128, 8], F32); r_t31 = Res()
                BD = sb(st, "BD", [128, 8, 3, 128], BF16); r_BD = Res()
                ovl_t = sb(st, "ovl_t", [128, 2, 65], F32); r_ovl = Res()
                ga_t = sb(st, "ga_t", [128, 512], F32); r_ga = Res()
                bcm = sb(st, "bcm", [128, 5, 512], F32); r_bcm = Res()
                DMA("dsp", sig[:], gates_s.rearrange("(n p) c -> p n c", p=128), [R["gates_s"]], [r_sig])
                ACTF(sig[:], sig[:], AF.Sigmoid, [r_sig], [r_sig])
                DMA("dact", force_t[:], force_in, [], [r_force])
                DMA("dsp", keep_t[:], keep_in, [], [r_keep])
                DMA("dact", t31[:], t31_in, [], [r_t31])
                DMA("dsp", ovl_t[:], ovl_ext, [], [r_ovl])
                DMA("dact", ga_t[:], ga_in, [], [r_ga])
                DMA("dsp", bcm[:], bc_m, [], [r_bcm])
                psb = [ps(st, f"pC{i}", [128, 512], F32) for i in range(8)]
                pS = Ring(psb[0:3])
                pO = psb[3:7]; r_pO = [Res() for _ in range(4)]
                pX = Ring(psb[7:8])
                with Scope(kb) as st2:
                    bdg = sb(st2, "bdg", [128, 8, 2, 128], F32); r_bdg = Res()
                    bdm = sb(st2, "bdm", [128, 3, 128], F32); r_bdm = Res()
                    DMA("dsp", bdg[:], bd_g.rearrange("h p j t -> p h j t"), [], [r_bdg])
                    DMA("dact", bdm[:], bd_m, [], [r_bdm])
                    for hg in range(8):
                        for j in range(2):
                            STT(BD[:, hg, j, :], bdg[:, hg, j, :], t31[:, hg:hg + 1], bdm[:, j, :], ALU.subtract, ALU.add, [r_bdg, r_bdm, r_t31], [r_BD])
                        CP("dve", BD[:, hg, 2, :], bdm[:, 2, :], [r_bdm], [r_BD])
                P_ring = Ring([sb(st, f"Pt{i}", [128, 512], BF16) for i in range(5)])
                sm = Ring([sb(st, f"smC{i}", [128, 8], F32) for i in range(8)])
                osb = Ring([sb(st, f"osb{i}", [128, 132], F32) for i in range(8)])

                def finish_tiles(items, ncol, hg, br, first, imp_first=None):
                    sts = [sm.next() for _ in items]
                    for (po, po_r, i, _, _), (s_t, s_r) in zip(items, sts):
                        TS("dve", s_t[:, 0:1], po[:, ncol:ncol + 1], 1e-30, None, ALU.max, None, [po_r], [s_r])
                    for (po, po_r, i, _, _), (s_t, s_r) in zip(items, sts):
                        E("dve", lambda: nc.vector.reciprocal(out=s_t[:, 1:2], in_=s_t[:, 0:1]), [s_r], [s_r])
                    for (po, po_r, i, _, _), (s_t, s_r) in zip(items, sts):
                        TT("dve", s_t[:, 2:3], s_t[:, 1:2], sig[:, i, hg * 3 + br:hg * 3 + br + 1], ALU.mult, [s_r, r_sig], [s_r])
                    for (po, po_r, i, _, _), (s_t, s_r) in zip(items, sts):
                        dst = Aout[:, i, hg * 64:(hg + 1) * 64]
                        if first:
                            TS("dve", dst, po[:, 0:64], s_t[:, 2:3], None, ALU.mult, None, [po_r, s_r], [rA[i]])
                        else:
                            STT(dst, po[:, 0:64], s_t[:, 2:3], dst, ALU.mult, ALU.add, [po_r, s_r, rA[i]], [rA[i]])
                    if imp_first is not None:
                        for (po, po_r, i, imp_t, imp_r), (s_t, s_r) in zip(items, sts):
                            if imp_first:
                                TS("dve", imp_t, po[:, 64:128], s_t[:, 1:2], None, ALU.mult, None, [po_r, s_r], [imp_r])
                            else:
                                STT(imp_t, po[:, 64:128], s_t[:, 1:2], imp_t, ALU.mult, ALU.add, [po_r, s_r, imp_r], [imp_r])

                for k in range(2):
                    with Scope(kb) as stg:
                        KcmpT = sb(stg, "KcmpT", [64, 256], BF16); r_Kc = Res()
                        Vco = sb(stg, "Vco", [128, 2, 129], BF16); r_Vco = Res()
                        with Scope(kb) as stc:
                            w1s = Ring([sb(stc, f"w1s{i}", [128, 8, 256], F32) for i in range(2)])
                            w1b = sb(stc, "w1b", [128, 2, 16, 256], BF16); r_w1b = Res()
                            pes = sb(stc, "pes", [128, 2, 16], F32); r_pes = Res()
                            peb = sb(stc, "peb", [128, 2, 16], BF16); r_peb = Res()
                            w2s = sb(stc, "w2s", [128, 2, 2, 64], F32); r_w2s = Res()
                            w2b = sb(stc, "w2b", [128, 2, 2, 64], BF16); r_w2b = Res()
                            stk = sb(stc, "stk", [128, 2, T], BF16); r_stk = Res()
                            hb = sb(stc, "hb", [128, 4], F32); r_hb = Res()
                            gh = sb(stc, "gh", [128, 2, 2, 256], BF16); r_gh = Res()
                            for kv in range(2):
                                for hh in range(2):
                                    w_t, w_r = w1s.next()
                                    DMA("dsp" if hh == 0 else "dact", w_t[:], cmp_w1[kv, :, hh * 8:(hh + 1) * 8, :], [], [w_r])
                                    CP("pool" if hh == 0 else "dve", w1b[:, kv, hh * 8:(hh + 1) * 8, :], w_t[:], [w_r], [r_w1b])
                                DMA("dsp", pes[:, kv, :], cmp_pe[kv], [], [r_pes])
                                DMA("dact", w2s[:, kv], cmp_w2[kv], [], [r_w2s])
                                src = kcT_s if kv == 0 else vcT_s
                                sres = R["kcT_s"] if kv == 0 else R["vcT_s"]
                                DMA("dsp", stk[0:64, kv, :], src[k * 64:(k + 1) * 64, :], [sres], [r_stk])
                                MSET("pool", stk[64:128, kv, T - 1:T], 0.0, [r_stk])
                                DMA("dact", stk[64:128, kv, 0:T - 1], src[k * 64:(k + 1) * 64, 1:T], [sres], [r_stk])
                            CP("dve", peb[:], pes[:], [r_pes], [r_peb])
                            CP("dve", w2b[:], w2s[:], [r_w2s], [r_w2b])
                            MSET("pool", gh[:], 0.0, [r_gh])
                            for kv in range(2):
                                for hh in range(2):
                                    px, px_r = pX.next()
                                    for m in range(16):
                                        MM(px[:, 0:1], w1b[:, kv, m, hh * 128:(hh + 1) * 128], peb[:, kv, m:m + 1], m == 0, m == 15, [r_w1b, r_peb], [px_r])
                                    CP("dve", hb[:, kv * 2 + hh:kv * 2 + hh + 1], px[:, 0:1], [px_r], [r_hb])
                                    p_s, p_r = pS.next()
                                    for m in range(16):
                                        MM(p_s[:, 0:255], w1b[:, kv, m, hh * 128:(hh + 1) * 128], stk[:, kv, 2 * m:2 * m + 16 * 254 + 1:16], m == 0, m == 15, [r_w1b, r_stk], [p_r])
                                    ACTF(gh[:, kv, hh, 0:255], p_s[:, 0:255], AF.Gelu_apprx_tanh, [p_r, r_hb], [r_gh], bias=hb[:, kv * 2 + hh:kv * 2 + hh + 1])
                            px, px_r = pX.next()
                            for hh in range(2):
                                MM(px[0:64, 0:256], w2b[:, 0, hh, :], gh[:, 0, hh, :], hh == 0, hh == 1, [r_w2b, r_gh], [px_r])
                            CP("dve", KcmpT[:], px[0:64, 0:256], [px_r], [r_Kc])
                            for ct in range(2):
                                px, px_r = pX.next()
                                for hh in range(2):
                                    MM(px[:, 0:64], gh[:, 1, hh, ct * 128:(ct + 1) * 128], w2b[:, 1, hh, :], hh == 0, hh == 1, [r_gh, r_w2b], [px_r])
                                CP("dve", Vco[:, ct, 0:64], px[:, 0:64], [px_r], [r_Vco])
                            CP("pool", Vco[:, :, 64:129], ovl_t[:], [r_ovl], [r_Vco])
                            if k == 0:
                                dump("d_kcmp", KcmpT[:], [64, 256], BF16, r_Kc)
                                dump("d_vco", Vco[:], [128, 2, 129], BF16, r_Vco)
                                dump("d_hb", hb[:], [128, 4], F32, r_hb)
                                dump("d_gh", gh[:], [128, 2, 2, 256], BF16, r_gh)

                        QT = sb(stg, "QT", [128, 4, T], BF16)
                        r_QT = [Res() for _ in range(4)]
                        r_QM = [[Res() for _ in range(NT)] for _ in range(4)]
                        KsT = sb(stg, "KsT", [128, T], BF16); r_KsT = Res()
                        KwT = sb(stg, "KwT", [64, T], BF16); r_KwT = Res()
                        Vs = sb(stg, "Vs", [128, NT, 65], BF16); r_Vs = Res()
                        Vw = sb(stg, "Vw", [128, NT, 65], BF16); r_Vw = Res()
                        imp_acc = sb(stg, "imp_acc", [128, NT, 64], F32)
                        r_imp = [Res() for _ in range(NT)]
                        for g in range(4):
                            hg = 4 * k + g
                            DMA("dsp" if g % 2 == 0 else "dact", QT[0:64, g, :], qT_s[hg * 64:(hg + 1) * 64, :], [R["qT_s"]], [r_QT[g]])
                        DMA("dsp", KsT[0:64, :], ksT_s[k * 64:(k + 1) * 64, :], [R["ksT_s"]], [r_KsT])
                        with Scope(kb) as ste:
                            ers = sb(ste, "ers", [128, T], F32); r_ers = Res()
                            DMA("dact", ers[64:128, :], erows, [], [r_ers])
                            CP("pool", KsT[64:128, :], ers[64:128, :], [r_ers], [r_KsT])
                        DMA("dact", KwT[:], kwT_s[k * 64:(k + 1) * 64, :], [R["kwT_s"]], [r_KwT])
                        DMA("dsp", Vs[:, :, 0:64], vs_s.rearrange("(n p) c -> p n c", p=128)[:, :, k * 64:(k + 1) * 64], [R["vs_s"]], [r_Vs])
                        DMA("dact", Vw[:, :, 0:64], vw_s.rearrange("(n p) c -> p n c", p=128)[:, :, k * 64:(k + 1) * 64], [R["vw_s"]], [r_Vw])
                        MSET("pool", Vs[:, :, 64:65], 1.0, [r_Vs])
                        MSET("pool", Vw[:, :, 64:65], 1.0, [r_Vw])

                        bcs = Ring([sb(stg, f"bcs{i}", [128, 5, 512], F32) for i in range(2)])
                        BC = Ring([sb(stg, f"BCb{i}", [128, 5, 512], BF16) for i in range(2)])
                        bc_cur = {}

                        def cmp_stage1(it):
                            g, tcn, ct, last = it
                            hg = 4 * k + g
                            if tcn == 0 and ct == 0:
                                bs_t, bs_r = bcs.next()
                                DMA("dsp", bs_t[:, 0:3], bc_g[hg, :, 0:3], [], [bs_r])
                                DMA("dact", bs_t[:, 3:5], bc_g[hg, :, 3:5], [], [bs_r])
                                bc_t, bc_r = BC.next()
                                for m in range(5):
                                    STT(bc_t[:, m, :], bs_t[:, m, :], t31[:, hg:hg + 1], bcm[:, m, :], ALU.subtract, ALU.add, [bs_r, r_bcm, r_t31], [bc_r])
                                bc_cur[g] = (bc_t, bc_r)
                            bc_t, bc_r = bc_cur[g]
                            mp = tcn - 4 * ct
                            p_s, p_r = pS.next()
                            MM(p_s[:], KcmpT[:, ct * 128:(ct + 1) * 128], QT[0:64, g, tcn * 512:(tcn + 1) * 512], True, mp >= 5, [r_Kc, r_QT[g]], [p_r])
                            if mp < 5:
                                MM(p_s[:], ident_b[:], bc_t[:, mp, :], False, True, [r_identb, bc_r], [p_r])
                            P_t, P_r = P_ring.next()
                            ACTF(P_t[:], p_s[:], AF.Exp, [p_r, r_t31], [P_r], bias=t31[:, hg:hg + 1])
                            return (P_t, P_r)

                        def cmp_stage2(it, st1):
                            g, tcn, ct, last = it
                            hg = 4 * k + g
                            P_t, P_r = st1
                            for q in range(4):
                                MM(pO[q][:, 0:129], P_t[:, q * 128:(q + 1) * 128], Vco[:, ct, :], ct == 0, last, [P_r, r_Vco], [r_pO[q]])
                            if last:
                                items = []
                                for q in range(4):
                                    i = 4 * tcn + q
                                    o_t, o_r = osb.next()
                                    CP("dve", o_t[:, 0:129], pO[q][:, 0:129], [r_pO[q]], [o_r])
                                    items.append((o_t, o_r, i, imp_acc[:, i, :], r_imp[i]))
                                finish_tiles(items, 128, hg, 0, True, imp_first=(g == 0))

                        its = []
                        for g in range(4):
                            for tcn in range(8):
                                cts = [0] if tcn < 4 else [0, 1]
                                for ct in cts:
                                    its.append((g, tcn, ct, ct == cts[-1]))
                        LAG = 2
                        pend = []
                        for n in range(len(its) + LAG):
                            if n < len(its):
                                pend.append((its[n], cmp_stage1(its[n])))
                            if n >= LAG:
                                it0, st0 = pend.pop(0)
                                cmp_stage2(it0, st0)

                        if k == 0:
                            dump("d_imp", imp_acc[:], [128, NT, 64], F32, r_imp)
                            dump("d_aout_c", Aout[:], [128, NT, 512], BF16, rA)
                        MBr = Ring([sb(stg, f"MB{i}", [128, 128], F32) for i in range(2)])
                        for (mb_t, mb_r) in zip(MBr.tiles, MBr.res):
                            MSET("dve", mb_t[:], 0.0, [mb_r])
                        tk = Ring([sb(stg, f"tk{i}", [128, 2, 64], F32) for i in range(2)])
                        mxr = Ring([sb(stg, f"mx{i}", [128, 16], F32) for i in range(2)])
                        mtr = Ring([sb(stg, f"mtr{i}", [128, 128], BF16) for i in range(2)])
                        for i in range(NT):
                            tk_t, tk_r = tk.next()
                            mx_t, mx_r = mxr.next()
                            TT("dve", tk_t[:, 0, :], imp_acc[:, i, :], keep_t[:, i, :], ALU.mult, [r_imp[i], r_keep], [tk_r])
                            TT("dve", tk_t[:, 0, :], tk_t[:, 0, :], force_t[:, i, :], ALU.add, [tk_r, r_force], [tk_r])
                            E("dve", lambda: nc.vector.max(out=mx_t[:, 0:8], in_=tk_t[:, 0, :]), [tk_r], [mx_r])
                            E("dve", lambda: nc.vector.match_replace(out=tk_t[:, 1, :], in_to_replace=mx_t[:, 0:8], in_values=tk_t[:, 0, :], imm_value=-1e30), [tk_r, mx_r], [tk_r])
                            E("dve", lambda: nc.vector.max(out=mx_t[:, 8:16], in_=tk_t[:, 1, :]), [tk_r], [mx_r])
                            mb_t, mb_r = MBr.next()
                            TS("dve", mb_t[:, 64:128], tk_t[:, 0, :], mx_t[:, 15:16], None, ALU.is_ge, None, [tk_r, mx_r], [mb_r])
                            TS("dve", mb_t[:, 64:128], mb_t[:, 64:128], 1.0, -NEGM, ALU.subtract, ALU.mult, [mb_r], [mb_r])
                            px, px_r = pX.next()
                            TR(px[:, 0:128], mb_t[:], ident_f[:], [mb_r, r_identf], [px_r])
                            mt_t, mt_r = mtr.next()
                            CP("act", mt_t[64:128, :], px[64:128, 0:128], [px_r], [mt_r])
                            for g in range(4):
                                CP("pool" if g % 2 == 0 else "dve", QT[64:128, g, i * 128:(i + 1) * 128], mt_t[64:128, :], [mt_r], [r_QM[g][i]])

                        if k == 0:
                            dump("d_qt0", QT[:, 0, :], [128, T], BF16, r_QT + [x for l in r_QM for x in l])
                        def sel_stage1(it):
                            g, br, tcn, j = it
                            hg = 4 * k + g
                            qa = max(0, j - 4 * tcn)
                            qb = 3 if br == 1 else min(3, j + 4 - 4 * tcn)
                            c0, c1 = qa * 128, (qb + 1) * 128
                            t0 = tcn * 512
                            adds = []
                            for q in range(qa, qb + 1):
                                dlt = 4 * tcn + q - j
                                if dlt == 0:
                                    adds.append((q, 0))
                                elif dlt == 1:
                                    adds.append((q, 1))
                                elif dlt == 4 and br == 2:
                                    adds.append((q, 2))
                            p_s, p_r = pS.next()
                            if br == 1:
                                rd = [r_KsT, r_QT[g]] + [r_QM[g][4 * tcn + q] for q in range(qa, qb + 1)]
                                MM(p_s[:, c0:c1], KsT[:, j * 128:(j + 1) * 128], QT[:, g, t0 + c0:t0 + c1], True, len(adds) == 0, rd, [p_r])
                            else:
                                MM(p_s[:, c0:c1], KwT[:, j * 128:(j + 1) * 128], QT[0:64, g, t0 + c0:t0 + c1], True, len(adds) == 0, [r_KwT, r_QT[g]], [p_r])
                            for ai, (q, ty) in enumerate(adds):
                                MM(p_s[:, q * 128:(q + 1) * 128], ident_b[:], BD[:, hg, ty, :], False, ai == len(adds) - 1, [r_identb, r_BD], [p_r])
                            P_t, P_r = P_ring.next()
                            ACTF(P_t[:, c0:c1], p_s[:, c0:c1], AF.Exp, [p_r, r_t31], [P_r], bias=t31[:, hg:hg + 1])
                            return (P_t, P_r, qa, qb)

                        def sel_stage2(it, st1):
                            g, br, tcn, j = it
                            hg = 4 * k + g
                            P_t, P_r, qa, qb = st1
                            Vx, r_Vx = (Vs, r_Vs) if br == 1 else (Vw, r_Vw)
                            for q in range(qa, qb + 1):
                                i = 4 * tcn + q
                                first_j = 0 if br == 1 else max(0, i - 4)
                                MM(pO[q][:, 0:65], P_t[:, q * 128:(q + 1) * 128], Vx[:, j, :], j == first_j, j == i, [P_r, r_Vx], [r_pO[q]])
                            if j == 4 * tcn + 3:
                                items = []
                                for q in range(4):
                                    o_t, o_r = osb.next()
                                    CP("dve", o_t[:, 0:65], pO[q][:, 0:65], [r_pO[q]], [o_r])
                                    items.append((o_t, o_r, 4 * tcn + q, None, None))
                                finish_tiles(items, 64, hg, br, False)

                        its = []
                        for g in range(4):
                            for br in (1, 2):
                                for tcn in range(8):
                                    j_lo = 0 if br == 1 else max(0, 4 * tcn - 4)
                                    for j in range(j_lo, 4 * tcn + 4):
                                        its.append((g, br, tcn, j))
                        LAG = 2
                        pend = []
                        for n in range(len(its) + LAG):
                            if n < len(its):
                                pend.append((its[n], sel_stage1(its[n])))
                            if n >= LAG:
                                it0, st0 = pend.pop(0)
                                sel_stage2(it0, st0)

                dump("d_aout", Aout[:], [128, NT, 512], BF16, rA)
                with Scope(kb) as stn:
                    junkC = sb(stn, "junkC", [128, 512], BF16); r_junkC = Res()
                    an = Ring([sb(stn, f"an{i}", [128, 512], BF16) for i in range(2)])
                    af = Ring([sb(stn, f"af{i}", [128, 512], F32) for i in range(2)])
                    ao = Ring([sb(stn, f"ao{i}", [128, 512], BF16) for i in range(2)])
                    pTb = Ring([ps(stn, f"pTC{i}", [128, 512], BF16) for i in range(2)]) if False else None
                    for i in range(NT):
                        s_t, s_r = sm.next()
                        ACTF(junkC[:], Aout[:, i, :], AF.Square, [rA[i]], [r_junkC, s_r], accum_out=s_t[:, 0:1])
                        TS("dve", s_t[:, 1:2], s_t[:, 0:1], 1.0 / 512, EPS, ALU.mult, ALU.add, [s_r], [s_r])
                        ACTF(s_t[:, 1:2], s_t[:, 1:2], AF.Sqrt, [s_r], [s_r])
                        E("dve", lambda: nc.vector.reciprocal(out=s_t[:, 2:3], in_=s_t[:, 1:2]), [s_r], [s_r])
                        af_t, af_r = af.next()
                        STT(af_t[:], Aout[:, i, :], s_t[:, 2:3], ga_t[:], ALU.mult, ALU.mult, [rA[i], s_r, r_ga], [af_r])
                        px, px_r = pX.next()
                        for fc in range(4):
                            TR(px[:, fc * 128:(fc + 1) * 128], af_t[:, fc * 128:(fc + 1) * 128], ident_f[:], [af_r, r_identf], [px_r])
                        ao_t, ao_r = ao.next()
                        CP("act", ao_t[:], px[:], [px_r], [ao_r])
                        DMA("dsp" if i % 2 == 0 else "dpool", mixT_s[0:512, i * 128:(i + 1) * 128].rearrange("(f p) t -> p f t", p=128),
                            ao_t[:].rearrange("p (f t) -> p f t", f=4), [ao_r], [R["mixT_s"]])

        if "D" in phases or "D1" in phases:
            NTL = TH // 128
            with Scope(kb) as st:
                Wo = sb(st, "Wo", [128, 8, D], BF16); r_Wo = Res()
                Wq = sb(st, "Wq", [128, 8, 2048], BF16); r_Wq = Res()
                skb = sb(st, "skb", [128, 2, 128], BF16); r_skb = Res()
                repf = sb(st, "repf", [128, D], F32); r_rep = Res()
                io16 = sb(st, "io16", [128, 16], F32); r_io = Res()
                io128 = sb(st, "io128", [128, 128], F32); r_io128 = Res()
                selt = sb(st, "selt", [128, 2], F32); r_sel = Res()
                DMA("dsp", repf[:], rep4[:, 0, :], [], [r_rep])
                DMA("dact", io16[:], iota16, [], [r_io])
                DMA("dact", io128[:], iota128, [], [r_io128])
                DMA("dact", selt[:], selc, [], [r_sel])
                with Scope(kb) as stw:
                    wst = Ring([sb(stw, f"wstD{i}", [128, 2048], F32) for i in range(3)])
                    n = 0
                    for (src, dstw, dres, ncol, nch) in ((w_out_in, Wo, r_Wo, D, 8), (peer_wq, Wq, r_Wq, 2048, 8)):
                        for dc in range(nch):
                            w_t, w_r = wst.next()
                            n += 1
                            DMA("dsp" if n % 2 == 0 else "dact", w_t[:, 0:ncol], src[dc * 128:(dc + 1) * 128, :], [], [w_r])
                            CP("dve" if n % 2 == 0 else "pool", dstw[:, dc, :], w_t[:, 0:ncol], [w_r], [dres])
                    w_t, w_r = wst.next()
                    DMA("dsp", w_t[:, 0:256].rearrange("p (a k) -> p a k", a=2), sk_T.rearrange("a p k -> p a k"), [], [w_r])
                    CP("dve", skb[:], w_t[:, 0:256].rearrange("p (a k) -> p a k", a=2), [w_r], [r_skb])

                pacc = Ring([ps(st, f"pD{i}", [128, 512], F32) for i in range(4)])
                pw_ring = Ring([ps(st, f"pDw{i}", [128, 512], F32) for i in range(2)])
                ptb = Ring([ps(st, f"pDb{i}", [128, 1024], BF16) for i in range(2)])
                mst = Ring([sb(st, f"mst{i}", [128, 8, 2, 128], BF16) for i in range(1)])
                mixh_ring = Ring([sb(st, f"mixh{i}", [128, 8, 128], BF16) for i in range(1)])
                xh_ring = Ring([sb(st, f"xhD{i}", [128, D], F32) for i in range(2)])
                H_ring = Ring([sb(st, f"HD{i}", [128, D], F32) for i in range(2)])
                xng_ring = Ring([sb(st, f"xng{i}", [128, D], F32) for i in range(1)])
                xnb_ring = Ring([sb(st, f"xnbD{i}", [128, D], BF16) for i in range(1)])
                xT_ring = Ring([sb(st, f"xTD{i}", [128, 8, 128], BF16) for i in range(2)])
                qTb = sb(st, "qTb", [128, 16, 128], BF16); r_qTb = Res()
                Ssc_ring = Ring([sb(st, f"Ssc{i}", [128, 16, 128], F32) for i in range(2)])
                Swk = sb(st, "Swk", [128, 8, 128], F32)
                rv = [Res() for _ in range(16)]; rv2 = [Res() for _ in range(16)]; ri = [Res() for _ in range(16)]; ri2 = [Res() for _ in range(16)]; rw = [Res() for _ in range(16)]
                v16 = sb(st, "v16", [128, 16, 16], F32); r_v16 = Res()
                i16 = sb(st, "i16", [128, 16, 16], U32); r_i16 = Res()
                i16f = sb(st, "i16f", [128, 16, 16], F32); r_i16f = Res()
                cand = sb(st, "cand", [128, 8, 256], F32); r_cand = Res()
                cwk = sb(st, "cwk", [128, 8, 256], F32)
                sc16 = sb(st, "sc16", [128, 8, 16], F32); r_sc = Res()
                ci16 = sb(st, "ci16", [128, 8, 16], U32); r_ci = Res()
                ab_u = sb(st, "ab_u", [128, 2, 8, 16], U32); r_abu = Res()
                ab_f = sb(st, "ab_f", [128, 2, 8, 16], F32); r_abf = Res()
                eq = sb(st, "eq", [128, 8, 16, 16], F32); r_eq = Res()
                isel_ring = Ring([sb(st, f"isel{i}", [128, 3, 8, 16], F32) for i in range(2)])
                gz = sb(st, "gz", [128, 16], F32); r_gz = Res()
                junkB = sb(st, "junkDb", [128, D], BF16); r_junkB = Res()
                smD = Ring([sb(st, f"smD{i}", [128, 8], F32) for i in range(4)])
                ijgT_ring = Ring([sb(st, f"ijgT{i}", [128, 3, 128], F32) for i in range(2)])
                OI = Ring([sb(st, f"OI{i}", [128, 16, 128], BF16) for i in range(2)])
                OJ = Ring([sb(st, f"OJ{i}", [128, 16, 128], BF16) for i in range(2)])
                OJf = Ring([sb(st, f"OJf{i}", [128, 16, 128], BF16) for i in range(2)])
                Wst = sb(st, "Wst", [128, 128, 128], BF16); r_Wst = Res()

                def rms_scaled(src, src_r, gain, gain_r, dstf, dstf_r):
                    s_t, s_r = smD.next()
                    ACTF(junkB[:], src, AF.Square, [src_r], [r_junkB, s_r], accum_out=s_t[:, 0:1])
                    TS("dve", s_t[:, 1:2], s_t[:, 0:1], 1.0 / D, EPS, ALU.mult, ALU.add, [s_r], [s_r])
                    ACTF(s_t[:, 1:2], s_t[:, 1:2], AF.Sqrt, [s_r], [s_r])
                    E("dve", lambda: nc.vector.reciprocal(out=s_t[:, 2:3], in_=s_t[:, 1:2]), [s_r], [s_r])
                    STT(dstf, src, s_t[:, 2:3], gain, ALU.mult, ALU.mult, [src_r, s_r, gain_r], [dstf_r])

                def transpose8(srcb, srcb_r, dstT, dstT_r, nblk=8):
                    pt, pt_r = ptb.next()
                    for dc in range(nblk):
                        TR(pt[:, dc * 128:(dc + 1) * 128], srcb[:, dc * 128:(dc + 1) * 128], ident_b[:], [srcb_r, r_identb], [pt_r])
                    CP("act", dstT.rearrange("p a t -> p (a t)"), pt[:, 0:nblk * 128], [pt_r], [dstT_r])

                def S1(it):
                    tsl = slice(it * 128, (it + 1) * 128)
                    xh_t, xh_r = xh_ring.next()
                    DMA("dsp", xh_t[:], xh[tsl, :], [], [xh_r])
                    H, H_r = H_ring.next()
                    m_t, m_r = mst.next()
                    for a in range(2):
                        DMA("dsp" if a == 0 else "dact", m_t[:, :, a, :], mixT_s[:, a * TH + it * 128:a * TH + (it + 1) * 128].rearrange("(f p) t -> p f t", p=128), [R["mixT_s"]], [m_r])
                    mixh, r_mixh = mixh_ring.next()
                    TS("pool", mixh[:], m_t[:, :, 0, :], selt[:, 0:1], None, ALU.mult, None, [m_r, r_sel], [r_mixh])
                    STT(mixh[:], m_t[:, :, 1, :], selt[:, 1:2], mixh[:], ALU.mult, ALU.add, [m_r, r_sel, r_mixh], [r_mixh])
                    for ch in range(2):
                        pa, pa_r = pacc.next()
                        for fc in range(8):
                            MM(pa[:], mixh[:, fc, :], Wo[:, fc, ch * 512:(ch + 1) * 512], fc == 0, fc == 7, [r_mixh, r_Wo], [pa_r])
                        TT("dve", H[:, ch * 512:(ch + 1) * 512], pa[:], xh_t[:, ch * 512:(ch + 1) * 512], ALU.add, [pa_r, xh_r], [H_r])
                    DMA("dpool", H1_s[tsl, :], H[:], [H_r], [R["H1_s"]])
                    xng, xng_r = xng_ring.next()
                    rms_scaled(H[:], H_r, repf[:], r_rep, xng[:], xng_r)
                    xnb, xnb_r = xnb_ring.next()
                    CP("pool", xnb[:], xng[:], [xng_r], [xnb_r])
                    xT, xT_r = xT_ring.next()
                    transpose8(xnb, xnb_r, xT[:], xT_r)
                    DMA("dact", xnT2_s[:, :, tsl], xT[:], [xT_r], [R["xnT2_s"]])
                    for grp in range(4):
                        pa, pa_r = pacc.next()
                        for j in range(4):
                            hp = grp * 4 + j
                            for dc in range(8):
                                MM(pa[:, j * 128:(j + 1) * 128], Wq[:, dc, hp * 128:(hp + 1) * 128], xT[:, dc, :], dc == 0, dc == 7, [r_Wq, xT_r], [pa_r])
                        CP("act", qTb[:, grp * 4:(grp + 1) * 4, :].rearrange("p a t -> p (a t)"), pa[:], [pa_r], [r_qTb])
                    Ssc, r_S = Ssc_ring.next()
                    for grp in range(4):
                        pa, pa_r = pacc.next()
                        for j in range(4):
                            hp = grp * 4 + j
                            MM(pa[:, j * 128:(j + 1) * 128], qTb[:, hp, :], skb[:, hp % 2, :], True, True, [r_qTb, r_skb], [pa_r])
                        CP("act", Ssc[:, grp * 4:(grp + 1) * 4, :].rearrange("p a t -> p (a t)"), pa[:], [pa_r], [r_S])
                    return (Ssc, r_S)

                def S2(it, st1):
                    Ssc, r_S = st1
                    for g0 in (0, 8):
                        hps = range(g0, g0 + 8)
                        for hp in hps:
                            E("dve", lambda: nc.vector.max(out=v16[:, hp, 0:8], in_=Ssc[:, hp, :]), [r_S], [rv[hp]])
                        for hp in hps:
                            E("dve", lambda: nc.vector.max_index(out=i16[:, hp, 0:8], in_max=v16[:, hp, 0:8], in_values=Ssc[:, hp, :]), [r_S, rv[hp]], [ri[hp]])
                        for hp in hps:
                            E("dve", lambda: nc.vector.match_replace(out=Swk[:, hp - g0, :], in_to_replace=v16[:, hp, 0:8], in_values=Ssc[:, hp, :], imm_value=-1e30), [r_S, rv[hp]], [rw[hp - g0]])
                        for hp in hps:
                            E("dve", lambda: nc.vector.max(out=v16[:, hp, 8:16], in_=Swk[:, hp - g0, :]), [rw[hp - g0]], [rv2[hp]])
                        for hp in hps:
                            E("dve", lambda: nc.vector.max_index(out=i16[:, hp, 8:16], in_max=v16[:, hp, 8:16], in_values=Swk[:, hp - g0, :]), [rw[hp - g0], rv2[hp]], [ri2[hp]])
                    r_i16 = Res()
                    E("dve", lambda: nc.vector.tensor_copy(out=i16f[:], in_=i16[:]), ri + ri2, [r_i16f, r_i16])
                    v4 = v16[:].rearrange("p (h two) k -> p h two k", two=2)
                    in0 = v4[:, :, 0, :].rearrange("p h (a o) -> p h a o", o=1).to_broadcast([128, 8, 16, 16])
                    in1 = v4[:, :, 1, :].rearrange("p h (o b) -> p h o b", o=1).to_broadcast([128, 8, 16, 16])
                    TT("dve", cand[:].rearrange("p h (a b) -> p h a b", a=16), in0, in1, ALU.add, rv + rv2, [r_cand])
                    for h in range(8):
                        E("dve", lambda: nc.vector.max(out=sc16[:, h, 0:8], in_=cand[:, h, :]), [r_cand], [rv[h]])
                    for h in range(8):
                        E("dve", lambda: nc.vector.max_index(out=ci16[:, h, 0:8], in_max=sc16[:, h, 0:8], in_values=cand[:, h, :]), [r_cand, rv[h]], [ri[h]])
                    for h in range(8):
                        E("dve", lambda: nc.vector.match_replace(out=cwk[:, h, :], in_to_replace=sc16[:, h, 0:8], in_values=cand[:, h, :], imm_value=-1e30), [r_cand, rv[h]], [rw[h]])
                    for h in range(8):
                        E("dve", lambda: nc.vector.max(out=sc16[:, h, 8:16], in_=cwk[:, h, :]), [rw[h]], [rv2[h]])
                    for h in range(8):
                        E("dve", lambda: nc.vector.max_index(out=ci16[:, h, 8:16], in_max=sc16[:, h, 8:16], in_values=cwk[:, h, :]), [rw[h], rv2[h]], [ri2[h]])
                    r_sc = Res(); r_ci = Res()
                    E("dve", lambda: nc.vector.tensor_single_scalar(out=ab_u[:, 0], in_=ci16[:], scalar=4, op=ALU.logical_shift_right), ri[:8] + ri2[:8] + rv[:8] + rv2[:8], [r_abu, r_sc, r_ci])
                    E("dve", lambda: nc.vector.tensor_single_scalar(out=ab_u[:, 1], in_=ci16[:], scalar=15, op=ALU.bitwise_and), [r_ci], [r_abu])
                    CP("dve", ab_f[:], ab_u[:], [r_abu], [r_abf])
                    isel, r_isel = isel_ring.next()
                    i4 = i16f[:].rearrange("p (h two) k -> p h two k", two=2)
                    for w in range(2):
                        a_b = ab_f[:, w].rearrange("p h (k o) -> p h k o", o=1).to_broadcast([128, 8, 16, 16])
                        io_b = io16[:].rearrange("p (o q a) -> p o q a", o=1, q=1).to_broadcast([128, 8, 16, 16])
                        TT("dve", eq[:], a_b, io_b, ALU.is_equal, [r_abf, r_io], [r_eq])
                        iv_b = i4[:, :, w, :].rearrange("p h (o a) -> p h o a", o=1).to_broadcast([128, 8, 16, 16])
                        TT("dve", eq[:], eq[:], iv_b, ALU.mult, [r_eq, r_i16f], [r_eq])
                        E("dve", lambda: nc.vector.tensor_reduce(out=isel[:, w], in_=eq[:], axis=AX.X, op=ALU.add), [r_eq], [r_isel])
                    TT("dve", isel[:, 2], sc16[:], sc16[:, :, 0:1].to_broadcast([128, 8, 16]), ALU.subtract, [r_sc], [r_isel])
                    ACTF(isel[:, 2], isel[:, 2], AF.Exp, [r_isel], [r_isel])
                    E("dve", lambda: nc.vector.tensor_reduce(out=gz[:, 0:8], in_=isel[:, 2], axis=AX.X, op=ALU.add), [r_isel], [r_gz])
                    E("dve", lambda: nc.vector.reciprocal(out=gz[:, 8:16], in_=gz[:, 0:8]), [r_gz], [r_gz])
                    TT("dve", isel[:, 2], isel[:, 2], gz[:, 8:16].rearrange("p (h o) -> p h o", o=1).to_broadcast([128, 8, 16]), ALU.mult, [r_isel, r_gz], [r_isel])
                    pa, pa_r = pacc.next()
                    for w in range(3):
                        TR(pa[:, w * 128:(w + 1) * 128], isel[:, w].rearrange("p h k -> p (h k)"), ident_f[:], [r_isel, r_identf], [pa_r])
                    ijgT, r_ijgT = ijgT_ring.next()
                    CP("act", ijgT[:].rearrange("p a t -> p (a t)"), pa[:, 0:384], [pa_r], [r_ijgT])
                    return (ijgT, r_ijgT)

                def S3(it, st2):
                    ijgT, r_ijgT = st2
                    TB = 16
                    for tb in range(128 // TB):
                        t0 = tb * TB
                        oi, oi_r = OI.next()
                        oj, oj_r = OJ.next()
                        ojf, ojf_r = OJf.next()
                        io_b = io128[:].rearrange("p (o i) -> p o i", o=1).to_broadcast([128, TB, 128])

                        def colb(w):
                            return ijgT[:, w, t0:t0 + TB].rearrange("p (t o) -> p t o", o=1).to_broadcast([128, TB, 128])
                        TT("dve", oi[:], io_b, colb(0), ALU.is_equal, [r_io128, r_ijgT], [oi_r])
                        TT("dve", ojf[:], io_b, colb(1), ALU.is_equal, [r_io128, r_ijgT], [ojf_r])
                        TT("pool", oj[:], ojf[:], colb(2), ALU.mult, [ojf_r, r_ijgT], [oj_r])
                        for tq in range(TB // 4):
                            pw, pw_r = pw_ring.next()
                            for u in range(4):
                                MM(pw[:, u * 128:(u + 1) * 128], oj[:, tq * 4 + u, :], oi[:, tq * 4 + u, :], True, True, [oj_r, oi_r], [pw_r])
                            tg = t0 + tq * 4
                            CP("act", Wst[:, :, tg:tg + 4].rearrange("p i t -> p t i"), pw[:].rearrange("p (t i) -> p t i", t=4), [pw_r], [r_Wst])
                    DMA("dsp" if it % 2 == 0 else "dact", Wt_s[it], Wst[:], [r_Wst], [R["Wt_s"]])

                st1s, st2s = {}, {}
                for n in range(NTL + 2):
                    if n >= 2:
                        S3(n - 2, st2s.pop(n - 2))
                    if n < NTL:
                        st1s[n] = S1(n)
                    if 1 <= n <= NTL:
                        st2s[n - 1] = S2(n - 1, st1s.pop(n - 1))

            with Scope(kb) as st:
                Yacc = sb(st, "Yacc", [128, NTL, D], F32)
                rY = [Res() for _ in range(NTL)]
                H1v = H1_s.rearrange("(n p) d -> p n d", p=128)
                for n4 in range(4):
                    DMA("dsp" if n4 % 2 == 0 else "dact", Yacc[:, n4 * 4:(n4 + 1) * 4, :], H1v[:, n4 * 4:(n4 + 1) * 4, :], [R["H1_s"]], rY[n4 * 4:(n4 + 1) * 4])
                p1 = Ring([ps(st, f"pE1{i}", [128, 512], F32) for i in range(3)])
                p2 = Ring([ps(st, f"pE2{i}", [128, 512], F32) for i in range(3)])
                ptb2 = Ring([ps(st, f"pEb{i}", [128, 1024], BF16) for i in range(2)])
                with Scope(kb) as st2:
                  if "D" in phases or "D2" in phases:
                    xnTa = sb(st2, "xnTa", [128, 8, TH], BF16); r_xnTa = Res()
                    for dc in range(8):
                        DMA("dsp" if dc % 2 == 0 else "dact", xnTa[:, dc, :], xnT2_s[:, dc, :], [R["xnT2_s"]], [r_xnTa])
                    IB = 8
                    ust = Ring([sb(st2, f"ust{i}", [128, D], F32) for i in range(2)])
                    vst = Ring([sb(st2, f"vst{i}", [128, D], F32) for i in range(2)])
                    ub = Ring([sb(st2, f"ub{i}", [128, D], BF16) for i in range(2)])
                    uT = Ring([sb(st2, f"uT{i}", [128, 8, 128], BF16) for i in range(2)])
                    Vb = sb(st2, "Vb", [128, IB, D], BF16); r_Vb = [Res() for _ in range(IB)]
                    WA = sb(st2, "WA", [128, IB, TH], BF16); r_WA = [Res() for _ in range(IB)]
                    wt = Ring([sb(st2, f"wt{i}", [128, TH], BF16) for i in range(3)])
                    gl = Ring([sb(st2, f"gl{i}", [128, 512], BF16) for i in range(3)])
                    for ib0 in range(0, 128, IB):
                        for ib in range(IB):
                            i = ib0 + ib
                            u_t, u_r = ust.next()
                            v_t, v_r = vst.next()
                            w_t, w_r = wt.next()
                            DMA("dsp", u_t[:], peer_u[i * 128:(i + 1) * 128, :], [], [u_r])
                            DMA("dact", v_t[:], peer_v[i * 128:(i + 1) * 128, :], [], [v_r])
                            for hw in range(2):
                                DMA("dpool" if hw == 0 else ("dsp" if i % 2 == 0 else "dact"), w_t[:, hw * 1024:(hw + 1) * 1024].rearrange("p (n t) -> p n t", t=128),
                                    Wt_s[hw * 8:(hw + 1) * 8, :, i, :].rearrange("n j t -> j n t"), [R["Wt_s"]], [w_r])
                            ub_t, ub_r = ub.next()
                            CP("pool", ub_t[:], u_t[:], [u_r], [ub_r])
                            CP("pool", Vb[:, ib, :], v_t[:], [v_r], [r_Vb[ib]])
                            pt, pt_r = ptb2.next()
                            for dc in range(8):
                                TR(pt[:, dc * 128:(dc + 1) * 128], ub_t[:, dc * 128:(dc + 1) * 128], ident_b[:], [ub_r, r_identb], [pt_r])
                            uT_t, uT_r = uT.next()
                            CP("act", uT_t[:].rearrange("p a t -> p (a t)"), pt[:], [pt_r], [uT_r])
                            for tc4 in range(4):
                                pa, pa_r = p1.next()
                                for dc in range(8):
                                    MM(pa[:], uT_t[:, dc, :], xnTa[:, dc, tc4 * 512:(tc4 + 1) * 512], dc == 0, dc == 7, [uT_r, r_xnTa], [pa_r])
                                g_t, g_r = gl.next()
                                ACTF(g_t[:], pa[:], AF.Gelu_apprx_tanh, [pa_r], [g_r])
                                TT("dve", WA[:, ib, tc4 * 512:(tc4 + 1) * 512], g_t[:], w_t[:, tc4 * 512:(tc4 + 1) * 512], ALU.mult, [g_r, w_r], [r_WA[ib]])
                        for tt in range(NTL):
                            for ch in range(2):
                                pb, pb_r = p2.next()
                                for ib in range(IB):
                                    MM(pb[:], WA[:, ib, tt * 128:(tt + 1) * 128], Vb[:, ib, ch * 512:(ch + 1) * 512], ib == 0, ib == IB - 1, [r_WA[ib], r_Vb[ib]], [pb_r])
                                TT("dve", Yacc[:, tt, ch * 512:(ch + 1) * 512], Yacc[:, tt, ch * 512:(ch + 1) * 512], pb[:], ALU.add, [rY[tt], pb_r], [rY[tt]])


                with Scope(kb) as st3:
                    Wgt = sb(st3, "Wgt", [128, 8, D], BF16); r_Wgt = Res()
                    Wp = sb(st3, "Wp", [128, 2, D], BF16); r_Wp = Res()
                    rep3 = sb(st3, "rep3", [128, 3, D], F32); r_rep3 = Res()
                    DMA("dsp", rep3[:], rep4[:, 1:4, :], [], [r_rep3])
                    wst3 = Ring([sb(st3, f"wst3{i}", [128, D], F32) for i in range(2)])
                    for (src, dstw, dres, nch) in ((ple_wg, Wgt, r_Wgt, 8), (ple_pj, Wp, r_Wp, 2)):
                        for dc in range(nch):
                            w_t, w_r = wst3.next()
                            DMA("dsp" if dc % 2 == 0 else "dact", w_t[:], src[dc * 128:(dc + 1) * 128, :], [], [w_r])
                            CP("dve" if dc % 2 == 0 else "pool", dstw[:, dc, :], w_t[:], [w_r], [dres])
                    x3_ring = Ring([sb(st3, f"x3{i}", [128, D], F32) for i in range(2)])
                    x3b_ring = Ring([sb(st3, f"x3b{i}", [128, D], BF16) for i in range(2)])
                    x3T_ring = Ring([sb(st3, f"x3T{i}", [128, 8, 128], BF16) for i in range(2)])
                    pht = Ring([sb(st3, f"pht{i}", [128, 256], F32) for i in range(2)])
                    phb = Ring([sb(st3, f"phb{i}", [128, 256], BF16) for i in range(2)])
                    phT = Ring([sb(st3, f"phT{i}", [128, 2, 128], BF16) for i in range(2)])
                    gt_ring = Ring([sb(st3, f"gtD{i}", [128, D], F32) for i in range(2)])
                    ot_ring = Ring([sb(st3, f"otD{i}", [128, D], F32) for i in range(2)])
                    junk3 = sb(st3, "junk3", [128, D], BF16); r_junk3 = Res()
                    sm3 = Ring([sb(st3, f"sm3{i}", [128, 8], F32) for i in range(4)])

                    def rms3(src, src_r, gi, dstf, dstf_r):
                        s_t, s_r = sm3.next()
                        ACTF(junk3[:], src, AF.Square, [src_r], [r_junk3, s_r], accum_out=s_t[:, 0:1])
                        TS("dve", s_t[:, 1:2], s_t[:, 0:1], 1.0 / D, EPS, ALU.mult, ALU.add, [s_r], [s_r])
                        ACTF(s_t[:, 1:2], s_t[:, 1:2], AF.Sqrt, [s_r], [s_r])
                        E("dve", lambda: nc.vector.reciprocal(out=s_t[:, 2:3], in_=s_t[:, 1:2]), [s_r], [s_r])
                        STT(dstf, src, s_t[:, 2:3], rep3[:, gi, :], ALU.mult, ALU.mult, [src_r, s_r, r_rep3], [dstf_r])

                    def tr3(srcb, srcb_r, dstT, dstT_r, nblk):
                        pt, pt_r = ptb2.next()
                        for dc in range(nblk):
                            TR(pt[:, dc * 128:(dc + 1) * 128], srcb[:, dc * 128:(dc + 1) * 128], ident_b[:], [srcb_r, r_identb], [pt_r])
                        CP("act", dstT.rearrange("p a t -> p (a t)"), pt[:, 0:nblk * 128], [pt_r], [dstT_r])

                    for it in range(NTL):
                        tsl = slice(it * 128, (it + 1) * 128)
                        Hh = Yacc[:, it, :]; H_r = rY[it]
                        x3, x3_r = x3_ring.next()
                        rms3(Hh, H_r, 0, x3[:], x3_r)
                        x3b, x3b_r = x3b_ring.next()
                        CP("pool", x3b[:], x3[:], [x3_r], [x3b_r])
                        x3T, x3T_r = x3T_ring.next()
                        tr3(x3b, x3b_r, x3T[:], x3T_r, 8)
                        ph_t, ph_r = pht.next()
                        DMA("dact", ph_t[:], ph[tsl, :], [], [ph_r])
                        pb_t, pb_r = phb.next()
                        CP("pool", pb_t[:], ph_t[:], [ph_r], [pb_r])
                        pT_t, pT_r = phT.next()
                        tr3(pb_t, pb_r, pT_t[:], pT_r, 2)
                        gt, gt_r = gt_ring.next()
                        for ch in range(2):
                            csl = slice(ch * 512, (ch + 1) * 512)
                            pa, pa_r = p1.next()
                            for dc in range(8):
                                MM(pa[:], x3T[:, dc, :], Wgt[:, dc, csl], dc == 0, dc == 7, [x3T_r, r_Wgt], [pa_r])
                            TT("dve", gt[:, csl], pa[:], rep3[:, 2, csl], ALU.add, [pa_r, r_rep3], [gt_r])
                            ACTF(gt[:, csl], gt[:, csl], AF.Sigmoid, [gt_r], [gt_r])
                            pb2, pb2_r = p2.next()
                            for dc in range(2):
                                MM(pb2[:], pT_t[:, dc, :], Wp[:, dc, csl], dc == 0, dc == 1, [pT_r, r_Wp], [pb2_r])
                            TT("dve", gt[:, csl], gt[:, csl], pb2[:], ALU.mult, [gt_r, pb2_r], [gt_r])
                            TT("pool", Yacc[:, it, csl], Yacc[:, it, csl], gt[:, csl], ALU.add, [H_r, gt_r], [H_r])
                        ot, ot_r = ot_ring.next()
                        rms3(Hh, H_r, 1, ot[:], ot_r)
                        DMA("dsp", out[tsl, :], ot[:], [ot_r], [])

        kb.drain_all()
    return nc


def _blockdiag(w):
    o = np.zeros((128, 4, 128), np.float32)
    for n in range(8):
        cc, j = n // 2, n % 2
        o[j * 64:(j + 1) * 64, cc, j * 64:(j + 1) * 64] = w[n]
    return o


def _rel_bucket(dist):
    n = np.maximum(dist, 0)
    nf = np.maximum(n, 16).astype(np.float32)
    large = 16 + (np.log(nf / np.float32(16)) / np.float32(np.log(8.0)) * np.float32(16)).astype(np.int32)
    large = np.minimum(large, 31)
    return np.where(n < 16, n, large)


def _nsa_consts(rel_table):
    c = {}
    assert (_rel_bucket(np.arange(113, 8192)) == 31).all()
    cl = np.arange(128)[:, None, None]; mp = np.arange(5)[None, :, None]; tt = np.arange(512)[None, None, :]
    dist = 512 * mp + tt - 16 * cl - 31
    c["bc_g"] = np.ascontiguousarray(rel_table[_rel_bucket(dist)].transpose(3, 0, 1, 2))
    c["bc_m"] = np.where(dist >= 0, 0.0, NEGM).astype(np.float32)
    assert (512 * 5 - 16 * 127 - 31) >= 113
    sl = np.arange(128)[:, None]; tl = np.arange(128)[None, :]
    d0 = tl - sl; d1 = 128 + tl - sl
    g0 = rel_table[_rel_bucket(d0)]; g1 = rel_table[_rel_bucket(d1)]
    c["bd_g"] = np.ascontiguousarray(np.stack([g0, g1], 0).transpose(3, 1, 0, 2))
    m0 = np.where(d0 >= 0, 0.0, NEGM); m2 = np.where(tl < sl, 0.0, NEGM)
    c["bd_m"] = np.ascontiguousarray(np.stack([m0, np.zeros_like(m0), m2], 1)).astype(np.float32)
    c["t31"] = np.ascontiguousarray(np.broadcast_to(rel_table[31][None, :], (128, 8))).astype(np.float32)
    t = (np.arange(NT)[None, :, None] * 128 + np.arange(128)[:, None, None])
    blk = np.arange(64)[None, None, :]
    d = t // 64 - blk
    local = (d >= 0) & (d < 2)
    init = (blk == 0) & ~local
    past = (d >= 0) & ~local & ~init
    c["force_c"] = np.where(local, 2.0e4, np.where(init, 1.0e4, np.where(past, 0.0, -1.0))).astype(np.float32)
    c["keep_c"] = past.astype(np.float32)
    cs = np.arange(256)[:, None] * 16; ss = np.arange(64)[None, :] * 64
    ov = np.clip(np.minimum(cs + 32, ss + 64) - np.maximum(cs, ss), 0, None).astype(np.float32) / 32.0
    ove = np.concatenate([ov, np.ones((256, 1), np.float32)], 1)
    ove[255] = 0.0
    c["ovl_ext"] = np.ascontiguousarray(ove.reshape(2, 128, 65).transpose(1, 0, 2))
    c["erows"] = (np.arange(T)[None, :] // 64 == np.arange(64)[:, None]).astype(np.float32)
    return c


def _prep_inputs(inputs):
    x = np.ascontiguousarray(inputs["x"], dtype=np.float32)
    p = np.ascontiguousarray(inputs["p"], dtype=np.float32)
    shared = {
        "ident": np.eye(128, dtype=np.float32),
        "w_in": np.ascontiguousarray(inputs["w_in"][0]),
        "g_attn": np.ascontiguousarray(inputs["attn_norm"][0].reshape(8, 128).T),
        "lru_cw": np.ascontiguousarray(inputs["conv_w"][0][:, 0, :].reshape(4, 4, 128).transpose(2, 1, 0)),
        "lru_vec": np.ascontiguousarray(np.stack([inputs[k][0].reshape(4, 128) for k in
                                                  ("conv_b", "lru_ba", "lru_bx", "lru_lambda", "grp_norm_lru")], 0).transpose(2, 0, 1)),
        "cmp_w1": np.ascontiguousarray(np.stack([inputs[k][0].reshape(16, 128, 256).transpose(1, 0, 2) for k in ("cmp_k_w1", "cmp_v_w1")], 0)),
        "cmp_pe": np.ascontiguousarray(np.stack([inputs[k][0].reshape(16, 128).T for k in ("cmp_k_pe", "cmp_v_pe")], 0)),
        "cmp_w2": np.ascontiguousarray(np.stack([inputs[k][0].reshape(2, 128, 64).transpose(1, 0, 2) for k in ("cmp_k_w2", "cmp_v_w2")], 0)),
        "ga_rep": np.ascontiguousarray(np.broadcast_to(inputs["grp_norm_attn"][0][None, :], (128, 512))).astype(np.float32),
        "w_out": np.ascontiguousarray(inputs["w_out"][0]),
        "peer_wq": np.ascontiguousarray(inputs["peer_wq"][0]),
        "sk_T": np.ascontiguousarray(inputs["peer_subkeys"][0].transpose(0, 2, 1)),
        "peer_u": np.ascontiguousarray(inputs["peer_u"][0]),
        "peer_v": np.ascontiguousarray(inputs["peer_v"][0]),
        "ple_wgate": np.ascontiguousarray(inputs["ple_wgate"][0]),
        "ple_proj": np.ascontiguousarray(inputs["ple_proj"][0]),
        "rep4": np.ascontiguousarray(np.broadcast_to(np.stack([inputs["ffn_norm"][0], inputs["ple_norm"][0], inputs["final_norm"], inputs["ple_bgate"][0]], 0)[None], (128, 4, D))).astype(np.float32),
        "iota128": np.ascontiguousarray(np.broadcast_to(np.arange(128, dtype=np.float32)[None], (128, 128))),
        "iota16": np.ascontiguousarray(np.broadcast_to(np.arange(16, dtype=np.float32)[None], (128, 16))),
        "lru_bda": _blockdiag(inputs["lru_wa"][0]),
        "lru_bdx": _blockdiag(inputs["lru_wx"][0]),
    }
    shared.update(_nsa_consts(np.asarray(inputs["rel_table"], np.float32)))
    in_maps = []
    for c in range(8):
        b, hf = c // 2, c % 2
        m = dict(shared)
        m["xb"] = x[b]
        m["xh"] = np.ascontiguousarray(x[b, hf * TH:(hf + 1) * TH])
        m["ph"] = np.ascontiguousarray(p[0, b, hf * TH:(hf + 1) * TH])
        sel = np.zeros((128, 2), np.float32); sel[:, hf] = 1.0
        m["selc"] = sel
        in_maps.append(m)
    return in_maps


def kernel(**inputs):
    nc = build_program()
    in_maps = _prep_inputs(inputs)
    res = run_bass_kernel_spmd(nc, in_maps, core_ids=list(range(8)))
    outp = np.zeros((4, T, D), np.float32)
    for c in range(8):
        b, hf = c // 2, c % 2
        outp[b, hf * TH:(hf + 1) * TH] = res.results[c]["out"]
    return outp
```

```python
import numpy as np
from contextlib import ExitStack
import concourse.bass as bass
import concourse.mybir as mybir
from concourse.bass_utils import run_bass_kernel_spmd

F32 = mybir.dt.float32
BF16 = mybir.dt.bfloat16
U32 = mybir.dt.uint32
AF = mybir.ActivationFunctionType
ALU = mybir.AluOpType
AX = mybir.AxisListType

T = 4096
D = 1024
NT = T // 128
TH = 2048
IN_COLS = 2328
EPS = 1e-6
NEGM = -30000.0


class Res:
    __slots__ = ("name", "lw", "rd")

    def __init__(self, name=""):
        self.name = name
        self.lw = None
        self.rd = {}


class KB:
    NDMA = 4

    def __init__(self, nc, stack):
        self.nc = nc
        self.issue = {"pe": nc.tensor, "act": nc.scalar, "dve": nc.vector, "pool": nc.gpsimd,
                      "dsp": nc.sync, "dact": nc.scalar, "dpool": nc.gpsimd}
        self.stream = {"pe": "pe", "act": "act", "dve": "dve", "pool": "pool",
                       "dsp": "sp", "dact": "act", "dpool": "pool"}
        self.sems = {}
        self.cnt = {}
        for q in self.issue:
            n = self.NDMA if self.is_dma(q) else 1
            self.sems[q] = [stack.enter_context(nc.semaphore(f"s_{q}{i}")) for i in range(n)]
            self.cnt[q] = 0
        self.waited = {s: {} for s in ("pe", "act", "dve", "pool", "sp")}
        self.ninst = 0
        self._rr = 0

    @staticmethod
    def is_dma(q):
        return q in ("dsp", "dact", "dpool")

    @staticmethod
    def _need(need, dep):
        if dep is None:
            return
        q, c = dep
        if need.get(q, 0) < c:
            need[q] = c

    def _waits(self, st, eng, need, skip_q=None):
        for dq, c in need.items():
            if dq == "pe" and skip_q == "pe":
                continue
            if self.is_dma(dq):
                n = self.NDMA
                for si in range(n):
                    k = (c - 1 - si) // n + 1 if c - 1 >= si else 0
                    if k <= 0:
                        continue
                    key = (dq, si)
                    if self.waited[st].get(key, 0) >= k:
                        continue
                    eng.wait_ge(self.sems[dq][si], 16 * k)
                    self.waited[st][key] = k
            else:
                key = (dq, 0)
                if self.waited[st].get(key, 0) >= c:
                    continue
                eng.wait_ge(self.sems[dq][0], c)
                self.waited[st][key] = c

    def emit(self, q, fn, reads=(), writes=()):
        need = {}
        for r in reads:
            self._need(need, r.lw)
        for w in writes:
            self._need(need, w.lw)
            for rq, rc in w.rd.items():
                self._need(need, (rq, rc))
        st = self.stream[q]
        self._waits(st, self.issue[q], need, skip_q=q)
        inst = fn()
        self.cnt[q] += 1
        c = self.cnt[q]
        if self.is_dma(q):
            inst.then_inc(self.sems[q][(c - 1) % self.NDMA], 16)
        else:
            inst.then_inc(self.sems[q][0], 1)
        for r in reads:
            if r.rd.get(q, 0) < c:
                r.rd[q] = c
        for w in writes:
            w.lw = (q, c)
            w.rd = {}
        self.ninst += 1
        return inst

    def dmaq(self):
        self._rr ^= 1
        return "dsp" if self._rr else "dact"

    def barrier(self):
        need = {q: c for q, c in self.cnt.items() if c > 0}
        for st, eng in (("pe", self.nc.tensor), ("act", self.nc.scalar), ("dve", self.nc.vector),
                        ("pool", self.nc.gpsimd), ("sp", self.nc.sync)):
            self._waits(st, eng, dict(need))

    def drain_all(self):
        need = {q: c for q, c in self.cnt.items() if c > 0}
        self._waits("sp", self.nc.sync, need)


class Scope:
    def __init__(self, kb):
        self.kb = kb
        self.st = ExitStack()

    def __enter__(self):
        self.st.__enter__()
        return self.st

    def __exit__(self, *a):
        if a[0] is None:
            self.kb.barrier()
        return self.st.__exit__(*a)


class Ring:
    def __init__(self, tiles):
        self.tiles = tiles
        self.res = [Res() for _ in tiles]
        self.i = -1

    def next(self):
        self.i = (self.i + 1) % len(self.tiles)
        return self.tiles[self.i], self.res[self.i]


def build_program(dbg=None, phases=("A", "B", "C", "D")):
    nc = bass.Bass("TRN2", target_bir_lowering=False)

    def din(name, shape, dt=F32):
        return nc.dram_tensor(name, list(shape), dt, kind="ExternalInput").ap()

    dbg = dbg or ()

    def dscr(name, shape, dt):
        kind = "ExternalOutput" if name in dbg else "Internal"
        return nc.dram_tensor(name, list(shape), dt, kind=kind).ap()

    xb = din("xb", [T, D])
    xh = din("xh", [TH, D])
    ph = din("ph", [TH, 256])
    selc = din("selc", [128, 2])
    ident = din("ident", [128, 128])
    w_in = din("w_in", [D, IN_COLS])
    g_attn = din("g_attn", [128, 8])
    out = nc.dram_tensor("out", [TH, D], F32, kind="ExternalOutput").ap()
    lru_cw = din("lru_cw", [128, 4, 4])
    lru_vec = din("lru_vec", [128, 5, 4])
    lru_bda = din("lru_bda", [128, 4, 128])
    lru_bdx = din("lru_bdx", [128, 4, 128])

    qT_s = dscr("qT_s", [512, T], BF16)
    kcT_s = dscr("kcT_s", [128, T], BF16)
    vcT_s = dscr("vcT_s", [128, T], BF16)
    ksT_s = dscr("ksT_s", [128, T], BF16)
    kwT_s = dscr("kwT_s", [128, T], BF16)
    vs_s = dscr("vs_s", [T, 128], BF16)
    vw_s = dscr("vw_s", [T, 128], BF16)
    gates_s = dscr("gates_s", [T, 24], F32)
    xrT_s = dscr("xrT_s", [512, T], F32)
    xgT_s = dscr("xgT_s", [512, T], F32)

    cmp_w1 = din("cmp_w1", [2, 128, 16, 256])
    cmp_pe = din("cmp_pe", [2, 128, 16])
    cmp_w2 = din("cmp_w2", [2, 128, 2, 64])
    ovl_ext = din("ovl_ext", [128, 2, 65])
    bc_g = din("bc_g", [8, 128, 5, 512])
    bc_m = din("bc_m", [128, 5, 512])
    bd_g = din("bd_g", [8, 128, 2, 128])
    bd_m = din("bd_m", [128, 3, 128])
    t31_in = din("t31", [128, 8])
    force_in = din("force_c", [128, 32, 64])
    keep_in = din("keep_c", [128, 32, 64])
    erows = din("erows", [64, T])
    ga_in = din("ga_rep", [128, 512])
    w_out_in = din("w_out", [D, D])
    peer_wq = din("peer_wq", [D, 2048])
    sk_T = din("sk_T", [2, 128, 128])
    peer_u = din("peer_u", [16384, D])
    peer_v = din("peer_v", [16384, D])
    ple_wg = din("ple_wgate", [D, D])
    ple_pj = din("ple_proj", [256, D])
    rep4 = din("rep4", [128, 4, D])
    iota16 = din("iota16", [128, 16])
    iota128 = din("iota128", [128, 128])
    H1_s = dscr("H1_s", [TH, D], F32)
    xnT2_s = dscr("xnT2_s", [128, 8, TH], BF16)
    Wt_s = dscr("Wt_s", [TH // 128, 128, 128, 128], BF16)
    mixT_s = dscr("mixT_s", [1024, T], BF16)
    R = {n: Res(n) for n in ("H1_s", "xnT2_s", "Wt_s", "mixT_s", "qT_s", "kcT_s", "vcT_s", "ksT_s", "kwT_s", "vs_s", "vw_s", "gates_s", "xrT_s", "xgT_s")}

    with ExitStack() as top:
        kb = KB(nc, top)
        E = kb.emit

        uniq = [0]

        def sb(st, name, shape, dt):
            uniq[0] += 1
            return st.enter_context(nc.sbuf_tensor(f"sb{uniq[0]}_{name}", list(shape), dt))

        def ps(st, name, shape, dt):
            uniq[0] += 1
            return st.enter_context(nc.psum_tensor(f"ps{uniq[0]}_{name}", list(shape), dt))


        def MM(out_, lhsT, rhs, start, stop, reads, writes):
            return E("pe", lambda: nc.tensor.matmul(out_, lhsT=lhsT, rhs=rhs, start=start, stop=stop), reads, writes)

        def TR(out_, in_, idt, reads, writes):
            return E("pe", lambda: nc.tensor.transpose(out=out_, in_=in_, identity=idt), reads, writes)

        def ACTF(out_, in_, func, reads, writes, **kw):
            return E("act", lambda: nc.scalar.activation(out=out_, in_=in_, func=func, **kw), reads, writes)

        def veng(q):
            return nc.vector if q == "dve" else nc.gpsimd

        def TS(q, out_, in0, s1, s2, op0, op1, reads, writes):
            if op1 is None:
                return E(q, lambda: veng(q).tensor_scalar(out=out_, in0=in0, scalar1=s1, scalar2=None, op0=op0), reads, writes)
            return E(q, lambda: veng(q).tensor_scalar(out=out_, in0=in0, scalar1=s1, scalar2=s2, op0=op0, op1=op1), reads, writes)

        def TT(q, out_, in0, in1, op, reads, writes):
            return E(q, lambda: veng(q).tensor_tensor(out=out_, in0=in0, in1=in1, op=op), reads, writes)

        def STT(out_, in0, scalar, in1, op0, op1, reads, writes, **kw):
            return E("dve", lambda: nc.vector.scalar_tensor_tensor(out=out_, in0=in0, scalar=scalar, in1=in1, op0=op0, op1=op1, **kw), reads, writes)

        def CP(q, out_, in_, reads, writes):
            if q == "act":
                return E("act", lambda: nc.scalar.copy(out=out_, in_=in_), reads, writes)
            return E(q, lambda: veng(q).tensor_copy(out=out_, in_=in_), reads, writes)

        def MSET(q, out_, val, writes):
            return E(q, lambda: veng(q).memset(out_, val), (), writes)

        def DMA(q, out_, in_, reads, writes):
            eng = {"dsp": nc.sync, "dact": nc.scalar, "dpool": nc.gpsimd}[q]
            return E(q, lambda: eng.dma_start(out=out_, in_=in_), reads, writes)

        def dump(name, ap, shape, dt, res):
            if name not in dbg:
                return
            d = nc.dram_tensor(name, list(shape), dt, kind="ExternalOutput").ap()
            DMA("dsp", d, ap, [res] if not isinstance(res, list) else res, [])

        ident_f = sb(top, "ident_f", [128, 128], F32); r_identf = Res()
        ident_b = sb(top, "ident_b", [128, 128], BF16); r_identb = Res()
        E("dsp", lambda: nc.sync.dma_start(out=ident_f[:], in_=ident), writes=[r_identf])
        E("dve", lambda: nc.vector.tensor_copy(out=ident_b[:], in_=ident_f[:]), reads=[r_identf], writes=[r_identb])

        if "A" in phases:
            with Scope(kb) as st:
                Wg = sb(st, "Wg", [128, 8, IN_COLS], BF16); r_Wg = Res()
                gcol = sb(st, "gcol", [128, 8], F32); r_gcol = Res()
                wst = Ring([sb(st, f"wst{i}", [128, IN_COLS], F32) for i in range(2)])
                E("dsp", lambda: nc.sync.dma_start(out=gcol[:], in_=g_attn), writes=[r_gcol])
                for dc in range(8):
                    w_t, w_r = wst.next()
                    E("dsp" if dc % 2 == 0 else "dact",
                      (lambda w_t=w_t, dc=dc: nc.sync.dma_start(out=w_t[:], in_=w_in[dc * 128:(dc + 1) * 128, :])) if dc % 2 == 0 else
                      (lambda w_t=w_t, dc=dc: nc.scalar.dma_start(out=w_t[:], in_=w_in[dc * 128:(dc + 1) * 128, :])),
                      writes=[w_r])
                    eng = "dve" if dc % 2 == 0 else "pool"
                    ve = nc.vector if dc % 2 == 0 else nc.gpsimd
                    E(eng, lambda ve=ve, w_t=w_t, dc=dc: ve.tensor_scalar(out=Wg[:, dc, :], in0=w_t[:], scalar1=gcol[:, dc:dc + 1], scalar2=None, op0=ALU.mult),
                      reads=[w_r, r_gcol], writes=[r_Wg])

                xt_ring = Ring([sb(st, f"xt{i}", [128, 4, D], F32) for i in range(2)])
                xnb_ring = Ring([sb(st, f"xnb{i}", [128, 4, D], BF16) for i in range(2)])
                xnT_ring = Ring([sb(st, f"xnT{i}", [128, 8, 512], BF16) for i in range(2)])
                junk = sb(st, "junkA", [128, D], BF16); r_junk = Res()
                ss_ring = Ring([sb(st, f"ss{i}", [128, 8], F32) for i in range(2)])
                pT_ring = Ring([ps(st, f"pT{i}", [128, 512], BF16) for i in range(2)])
                pacc = Ring([ps(st, f"pacc{i}", [128, 512], F32) for i in range(4)])
                ostf = Ring([sb(st, f"ostf{i}", [128, 512], F32) for i in range(3)])
                ostb = Ring([sb(st, f"ostb{i}", [128, 512], BF16) for i in range(3)])
                osv = Ring([sb(st, f"osv{i}", [128, 256], BF16) for i in range(2)])
                osg = Ring([sb(st, f"osg{i}", [128, 24], F32) for i in range(2)])
                xb_v = xb.rearrange("(n p) d -> p n d", p=128)
                fm = []
                for cc in range(4):
                    fm.append((cc * 128, qT_s[cc * 128:(cc + 1) * 128, :], 0.125, True, R["qT_s"]))
                fm.append((512, kcT_s, 1.0, True, R["kcT_s"]))
                fm.append((640, vcT_s, 1.0, True, R["vcT_s"]))
                fm.append((768, ksT_s, 1.0, True, R["ksT_s"]))
                fm.append((1024, kwT_s, 1.0, True, R["kwT_s"]))
                for cc in range(4):
                    fm.append((1304 + cc * 128, xrT_s[cc * 128:(cc + 1) * 128, :], 1.0, False, R["xrT_s"]))
                for cc in range(4):
                    fm.append((1816 + cc * 128, xgT_s[cc * 128:(cc + 1) * 128, :], 1.0, False, R["xgT_s"]))
                ev = 0
                for tcn in range(8):
                    xt, xt_r = xt_ring.next()
                    E("dsp", lambda xt=xt, tcn=tcn: nc.sync.dma_start(out=xt[:], in_=xb_v[:, tcn * 4:(tcn + 1) * 4, :]), writes=[xt_r])
                    ss, ss_r = ss_ring.next()
                    for n in range(4):
                        E("act", lambda xt=xt, ss=ss, n=n: nc.scalar.activation(out=junk[:], in_=xt[:, n, :], func=AF.Square, accum_out=ss[:, n:n + 1]),
                          reads=[xt_r], writes=[r_junk, ss_r])
                    E("dve", lambda ss=ss: nc.vector.tensor_scalar(out=ss[:, 4:8], in0=ss[:, 0:4], scalar1=1.0 / D, scalar2=EPS, op0=ALU.mult, op1=ALU.add), reads=[ss_r], writes=[ss_r])
                    E("act", lambda ss=ss: nc.scalar.activation(out=ss[:, 4:8], in_=ss[:, 4:8], func=AF.Sqrt), reads=[ss_r], writes=[ss_r])
                    E("dve", lambda ss=ss: nc.vector.reciprocal(out=ss[:, 4:8], in_=ss[:, 4:8]), reads=[ss_r], writes=[ss_r])
                    xnb, xnb_r = xnb_ring.next()
                    for n in range(4):
                        if n % 2 == 0:
                            E("dve", lambda xt=xt, xnb=xnb, ss=ss, n=n: nc.vector.tensor_scalar(out=xnb[:, n, :], in0=xt[:, n, :], scalar1=ss[:, 4 + n:5 + n], scalar2=None, op0=ALU.mult),
                              reads=[xt_r, ss_r], writes=[xnb_r])
                        else:
                            E("pool", lambda xt=xt, xnb=xnb, ss=ss, n=n: nc.gpsimd.tensor_scalar(out=xnb[:, n, :], in0=xt[:, n, :], scalar1=ss[:, 4 + n:5 + n], scalar2=None, op0=ALU.mult),
                              reads=[xt_r, ss_r], writes=[xnb_r])
                    xnT, xnT_r = xnT_ring.next()
                    for dc in range(8):
                        pT, pT_r = pT_ring.next()
                        for n in range(4):
                            E("pe", lambda pT=pT, xnb=xnb, n=n, dc=dc: nc.tensor.transpose(out=pT[:, n * 128:(n + 1) * 128], in_=xnb[:, n, dc * 128:(dc + 1) * 128], identity=ident_b[:]),
                              reads=[xnb_r, r_identb], writes=[pT_r])
                        if dc % 2 == 0:
                            E("act", lambda pT=pT, xnT=xnT, dc=dc: nc.scalar.copy(out=xnT[:, dc, :], in_=pT[:]), reads=[pT_r], writes=[xnT_r])
                        else:
                            E("dve", lambda pT=pT, xnT=xnT, dc=dc: nc.vector.tensor_copy(out=xnT[:, dc, :], in_=pT[:]), reads=[pT_r], writes=[xnT_r])
                    for (c0, dst, scale, isb, dres) in fm:
                        pa, pa_r = pacc.next()
                        for dc in range(8):
                            E("pe", lambda pa=pa, dc=dc, c0=c0, xnT=xnT: nc.tensor.matmul(pa[:], lhsT=Wg[:, dc, c0:c0 + 128], rhs=xnT[:, dc, :], start=(dc == 0), stop=(dc == 7)),
                              reads=[r_Wg, xnT_r], writes=[pa_r])
                        o_t, o_r = (ostb if isb else ostf).next()
                        ev += 1
                        if ev % 2 == 0:
                            E("act", lambda o_t=o_t, pa=pa, scale=scale: nc.scalar.activation(out=o_t[:], in_=pa[:], func=AF.Copy, scale=scale), reads=[pa_r], writes=[o_r])
                        else:
                            E("dve", lambda o_t=o_t, pa=pa, scale=scale: nc.vector.tensor_scalar(out=o_t[:], in0=pa[:], scalar1=scale, scalar2=None, op0=ALU.mult), reads=[pa_r], writes=[o_r])
                        if ev % 2 == 0:
                            E("dsp", lambda o_t=o_t, dst=dst, tcn=tcn: nc.sync.dma_start(out=dst[:, tcn * 512:(tcn + 1) * 512], in_=o_t[:]), reads=[o_r], writes=[dres])
                        else:
                            E("dpool", lambda o_t=o_t, dst=dst, tcn=tcn: nc.gpsimd.dma_start(out=dst[:, tcn * 512:(tcn + 1) * 512], in_=o_t[:]), reads=[o_r], writes=[dres])
                    for n in range(4):
                        t0 = tcn * 512 + n * 128
                        pa, pa_r = pacc.next()
                        for dc in range(8):
                            E("pe", lambda pa=pa, dc=dc, xnT=xnT, n=n: nc.tensor.matmul(pa[:, 0:128], lhsT=xnT[:, dc, n * 128:(n + 1) * 128], rhs=Wg[:, dc, 896:1024], start=(dc == 0), stop=(dc == 7)),
                              reads=[r_Wg, xnT_r], writes=[pa_r])
                        pb, pb_r = pacc.next()
                        for dc in range(8):
                            E("pe", lambda pb=pb, dc=dc, xnT=xnT, n=n: nc.tensor.matmul(pb[:, 0:152], lhsT=xnT[:, dc, n * 128:(n + 1) * 128], rhs=Wg[:, dc, 1152:1304], start=(dc == 0), stop=(dc == 7)),
                              reads=[r_Wg, xnT_r], writes=[pb_r])
                        ov, ov_r = osv.next()
                        og, og_r = osg.next()
                        E("act", lambda ov=ov, pa=pa: nc.scalar.copy(out=ov[:, 0:128], in_=pa[:, 0:128]), reads=[pa_r], writes=[ov_r])
                        E("dve", lambda ov=ov, pb=pb: nc.vector.tensor_copy(out=ov[:, 128:256], in_=pb[:, 0:128]), reads=[pb_r], writes=[ov_r])
                        E("dve", lambda og=og, pb=pb: nc.vector.tensor_copy(out=og[:], in_=pb[:, 128:152]), reads=[pb_r], writes=[og_r])
                        E("dsp", lambda ov=ov, t0=t0: nc.sync.dma_start(out=vs_s[t0:t0 + 128, :], in_=ov[:, 0:128]), reads=[ov_r], writes=[R["vs_s"]])
                        E("dpool", lambda ov=ov, t0=t0: nc.gpsimd.dma_start(out=vw_s[t0:t0 + 128, :], in_=ov[:, 128:256]), reads=[ov_r], writes=[R["vw_s"]])
                        E("dsp", lambda og=og, t0=t0: nc.sync.dma_start(out=gates_s[t0:t0 + 128, :], in_=og[:]), reads=[og_r], writes=[R["gates_s"]])

        if "B" in phases:
            with Scope(kb) as st:
                cw = sb(st, "cw", [128, 4, 4], F32); r_cw = Res()
                lv = sb(st, "lv", [128, 5, 4], F32); r_lv = Res()
                clc = sb(st, "clc", [128, 3, 4], F32); r_clc = Res()
                bdf = sb(st, "bdf", [128, 2, 4, 128], F32); r_bdf = Res()
                bdb = sb(st, "bdb", [128, 2, 4, 128], BF16); r_bdb = Res()
                ones_b = sb(st, "ones_b", [128, 128], BF16); r_ones = Res()
                E("dsp", lambda: nc.sync.dma_start(out=cw[:], in_=lru_cw), writes=[r_cw])
                E("dact", lambda: nc.scalar.dma_start(out=lv[:], in_=lru_vec), writes=[r_lv])
                E("dsp", lambda: nc.sync.dma_start(out=bdf[:, 0], in_=lru_bda), writes=[r_bdf])
                E("dact", lambda: nc.scalar.dma_start(out=bdf[:, 1], in_=lru_bdx), writes=[r_bdf])
                E("dve", lambda: nc.vector.tensor_copy(out=bdb[:], in_=bdf[:]), reads=[r_bdf], writes=[r_bdb])
                E("dve", lambda: nc.vector.memset(ones_b[:], 1.0), writes=[r_ones])
                E("act", lambda: nc.scalar.activation(out=clc[:, 0, :], in_=lv[:, 3, :], func=AF.Exp, scale=-1.0), reads=[r_lv], writes=[r_clc])
                E("act", lambda: nc.scalar.activation(out=clc[:, 0, :], in_=clc[:, 0, :], func=AF.Ln, bias=1.0), reads=[r_clc], writes=[r_clc])
                E("dve", lambda: nc.vector.tensor_scalar(out=clc[:, 1, :], in0=clc[:, 0, :], scalar1=-8.0, scalar2=None, op0=ALU.mult), reads=[r_clc], writes=[r_clc])
                E("dve", lambda: nc.vector.tensor_scalar(out=clc[:, 2, :], in0=clc[:, 0, :], scalar1=-16.0, scalar2=None, op0=ALU.mult), reads=[r_clc], writes=[r_clc])
                L = sb(st, "Lall", [128, 4, T], F32); r_L = Res()
                X = [sb(st, f"lruX{i}", [128, T], F32) for i in range(5)]
                rX = [Res() for _ in range(5)]
                xcb = sb(st, "xcb", [128, T], BF16); r_xcb = Res()
                pg = Ring([ps(st, f"pg{i}", [128, 512], F32) for i in range(4)])
                for cc in range(4):
                    X1, X2, X3, X4, X5 = X
                    r1, r2, r3, r4, r5 = rX
                    for hh in range(2):
                        E("dsp", lambda cc=cc, hh=hh: nc.sync.dma_start(out=X1[:, hh * 2048:(hh + 1) * 2048], in_=xrT_s[cc * 128:(cc + 1) * 128, hh * 2048:(hh + 1) * 2048]), reads=[R["xrT_s"]], writes=[r1])
                        E("dact", lambda cc=cc, hh=hh: nc.scalar.dma_start(out=X3[:, hh * 2048:(hh + 1) * 2048], in_=xgT_s[cc * 128:(cc + 1) * 128, hh * 2048:(hh + 1) * 2048]), reads=[R["xgT_s"]], writes=[r3])
                    E("dve", lambda cc=cc: nc.vector.tensor_scalar(out=X2[:], in0=X1[:], scalar1=cw[:, cc, 3:4], scalar2=lv[:, 0, cc:cc + 1], op0=ALU.mult, op1=ALU.add), reads=[r1, r_cw, r_lv], writes=[r2])
                    for sh in (1, 2, 3):
                        E("dve", lambda cc=cc, sh=sh: nc.vector.scalar_tensor_tensor(out=X2[:, sh:T], in0=X1[:, 0:T - sh], scalar=cw[:, cc, 3 - sh:4 - sh], in1=X2[:, sh:T], op0=ALU.mult, op1=ALU.add), reads=[r1, r2, r_cw], writes=[r2])
                    E("pool", lambda: nc.gpsimd.tensor_copy(out=xcb[:], in_=X2[:]), reads=[r2], writes=[r_xcb])
                    for gi, (Xo, ro, bi) in enumerate(((X4, r4, 1), (X5, r5, 2))):
                        for tcn in range(8):
                            pgt, pg_r = pg.next()
                            E("pe", lambda pgt=pgt, gi=gi, cc=cc, tcn=tcn: nc.tensor.matmul(pgt[:], lhsT=bdb[:, gi, cc, :], rhs=xcb[:, tcn * 512:(tcn + 1) * 512], start=True, stop=True), reads=[r_bdb, r_xcb], writes=[pg_r])
                            E("act", lambda pgt=pgt, Xo=Xo, bi=bi, cc=cc, tcn=tcn: nc.scalar.activation(out=Xo[:, tcn * 512:(tcn + 1) * 512], in_=pgt[:], func=AF.Sigmoid, bias=lv[:, bi, cc:cc + 1]), reads=[pg_r, r_lv], writes=[ro])
                    E("act", lambda cc=cc: nc.scalar.activation(out=X1[:], in_=X4[:], func=AF.Exp, scale=clc[:, 1, cc:cc + 1]), reads=[r4, r_clc], writes=[r1])
                    E("act", lambda cc=cc: nc.scalar.activation(out=X4[:], in_=X4[:], func=AF.Exp, scale=clc[:, 2, cc:cc + 1]), reads=[r4, r_clc], writes=[r4])
                    E("act", lambda: nc.scalar.activation(out=X4[:], in_=X4[:], func=AF.Sqrt, scale=-1.0, bias=1.0), reads=[r4], writes=[r4])
                    E("pool", lambda: nc.gpsimd.tensor_tensor(out=X5[:], in0=X5[:], in1=X2[:], op=ALU.mult), reads=[r5, r2], writes=[r5])
                    E("dve", lambda: nc.vector.tensor_tensor(out=X4[:], in0=X4[:], in1=X5[:], op=ALU.mult), reads=[r4, r5], writes=[r4])
                    E("dve", lambda: nc.vector.tensor_tensor_scan(out=X2[:], data0=X1[:], data1=X4[:], initial=0.0, op0=ALU.mult, op1=ALU.add), reads=[r1, r4], writes=[r2])
                    E("act", lambda: nc.scalar.activation(out=X3[:], in_=X3[:], func=AF.Gelu_apprx_tanh), reads=[r3], writes=[r3])
                    E("pool", lambda cc=cc: nc.gpsimd.tensor_tensor(out=L[:, cc, :], in0=X2[:], in1=X3[:], op=ALU.mult), reads=[r2, r3], writes=[r_L])
                sq = Ring([sb(st, f"lsq{i}", [128, 512], BF16) for i in range(2)])
                rs_ring = Ring([sb(st, f"lrs{i}", [128, 512], F32) for i in range(2)])
                lo = Ring([sb(st, f"lo{i}", [128, 512], BF16) for i in range(3)])
                for tcn in range(8):
                    pgt, pg_r = pg.next()
                    for cc in range(4):
                        sq_t, sq_r = sq.next()
                        E("act", lambda sq_t=sq_t, cc=cc, tcn=tcn: nc.scalar.activation(out=sq_t[:], in_=L[:, cc, tcn * 512:(tcn + 1) * 512], func=AF.Square), reads=[r_L], writes=[sq_r])
                        E("pe", lambda pgt=pgt, sq_t=sq_t, cc=cc: nc.tensor.matmul(pgt[:], lhsT=ones_b[:], rhs=sq_t[:], start=(cc == 0), stop=(cc == 3)), reads=[r_ones, sq_r], writes=[pg_r])
                    rs_t, rs_r = rs_ring.next()
                    E("dve", lambda rs_t=rs_t, pgt=pgt: nc.vector.tensor_scalar(out=rs_t[:], in0=pgt[:], scalar1=1.0 / 512, scalar2=EPS, op0=ALU.mult, op1=ALU.add), reads=[pg_r], writes=[rs_r])
                    E("act", lambda rs_t=rs_t: nc.scalar.activation(out=rs_t[:], in_=rs_t[:], func=AF.Sqrt), reads=[rs_r], writes=[rs_r])
                    E("dve", lambda rs_t=rs_t: nc.vector.reciprocal(out=rs_t[:], in_=rs_t[:]), reads=[rs_r], writes=[rs_r])
                    for cc in range(4):
                        lo_t, lo_r = lo.next()
                        E("dve", lambda lo_t=lo_t, rs_t=rs_t, cc=cc, tcn=tcn: nc.vector.scalar_tensor_tensor(out=lo_t[:], in0=L[:, cc, tcn * 512:(tcn + 1) * 512], scalar=lv[:, 4, cc:cc + 1], in1=rs_t[:], op0=ALU.mult, op1=ALU.mult), reads=[r_L, rs_r, r_lv], writes=[lo_r])
                        E("dsp", lambda lo_t=lo_t, cc=cc, tcn=tcn: nc.sync.dma_start(out=mixT_s[512 + cc * 128:512 + (cc + 1) * 128, tcn * 512:(tcn + 1) * 512], in_=lo_t[:]), reads=[lo_r], writes=[R["mixT_s"]])

        if "C" in phases:
            with Scope(kb) as st:
                Aout = sb(st, "Aout", [128, NT, 512], BF16)
                rA = [Res() for _ in range(NT)]
                sig = sb(st, "sig", [128, NT, 24], F32); r_sig = Res()
                force_t = sb(st, "force_t", [128, NT, 64], F32); r_force = Res()
                keep_t = sb(st, "keep_t", [128, NT, 64], F32); r_keep = Res()
                t31 = sb(st, "t31", [128, 8], F32); r_t31 = Res()
                BD = sb(st, "BD", [128, 8, 3, 128], BF16); r_BD = Res()
                ovl_t = sb(st, "ovl_t", [128, 2, 65], F32); r_ovl = Res()
                ga_t = sb(st, "ga_t", [128, 512], F32); r_ga = Res()
                bcm = sb(st, "bcm", [128, 5, 512], F32); r_bcm = Res()
                DMA("dsp", sig[:], gates_s.rearrange("(n p) c -> p n c", p=128), [R["gates_s"]], [r_sig])
                ACTF(sig[:], sig[:], AF.Sigmoid, [r_sig], [r_sig])
                DMA("dact", force_t[:], force_in, [], [r_force])
                DMA("dsp", keep_t[:], keep_in, [], [r_keep])
                DMA("dact", t31[:], t31_in, [], [r_t31])
                DMA("dsp", ovl_t[:], ovl_ext, [], [r_ovl])
                DMA("dact", ga_t[:], ga_in, [], [r_ga])
                DMA("dsp", bcm[:], bc_m, [], [r_bcm])
                psb = [ps(st, f"pC{i}", [128, 512], F32) for i in range(8)]
                pS = Ring(psb[0:3])
                pO = psb[3:7]; r_pO = [Res() for _ in range(4)]
                pX = Ring(psb[7:8])
                with Scope(kb) as st2:
                    bdg = sb(st2, "bdg", [128, 8, 2, 128], F32); r_bdg = Res()
                    bdm = sb(st2, "bdm", [128, 3, 128], F32); r_bdm = Res()
                    DMA("dsp", bdg[:], bd_g.rearrange("h p j t -> p h j t"), [], [r_bdg])
                    DMA("dact", bdm[:], bd_m, [], [r_bdm])
                    for hg in range(8):
                        for j in range(2):
                            STT(BD[:, hg, j, :], bdg[:, hg, j, :], t31[:, hg:hg + 1], bdm[:, j, :], ALU.subtract, ALU.add, [r_bdg, r_bdm, r_t31], [r_BD])
                        CP("dve", BD[:, hg, 2, :], bdm[:, 2, :], [r_bdm], [r_BD])
                P_ring = Ring([sb(st, f"Pt{i}", [128, 512], BF16) for i in range(5)])
                sm = Ring([sb(st, f"smC{i}", [128, 8], F32) for i in range(8)])
                osb = Ring([sb(st, f"osb{i}", [128, 132], F32) for i in range(8)])

                def finish_tiles(items, ncol, hg, br, first, imp_first=None):
                    sts = [sm.next() for _ in items]
                    for (po, po_r, i, _, _), (s_t, s_r) in zip(items, sts):
                        TS("dve", s_t[:, 0:1], po[:, ncol:ncol + 1], 1e-30, None, ALU.max, None, [po_r], [s_r])
                    for (po, po_r, i, _, _), (s_t, s_r) in zip(items, sts):
                        E("dve", lambda: nc.vector.reciprocal(out=s_t[:, 1:2], in_=s_t[:, 0:1]), [s_r], [s_r])
                    for (po, po_r, i, _, _), (s_t, s_r) in zip(items, sts):
                        TT("dve", s_t[:, 2:3], s_t[:, 1:2], sig[:, i, hg * 3 + br:hg * 3 + br + 1], ALU.mult, [s_r, r_sig], [s_r])
                    for (po, po_r, i, _, _), (s_t, s_r) in zip(items, sts):
                        dst = Aout[:, i, hg * 64:(hg + 1) * 64]
                        if first:
                            TS("dve", dst, po[:, 0:64], s_t[:, 2:3], None, ALU.mult, None, [po_r, s_r], [rA[i]])
                        else:
                            STT(dst, po[:, 0:64], s_t[:, 2:3], dst, ALU.mult, ALU.add, [po_r, s_r, rA[i]], [rA[i]])
                    if imp_first is not None:
                        for (po, po_r, i, imp_t, imp_r), (s_t, s_r) in zip(items, sts):
                            if imp_first:
                                TS("dve", imp_t, po[:, 64:128], s_t[:, 1:2], None, ALU.mult, None, [po_r, s_r], [imp_r])
                            else:
                                STT(imp_t, po[:, 64:128], s_t[:, 1:2], imp_t, ALU.mult, ALU.add, [po_r, s_r, imp_r], [imp_r])

                for k in range(2):
                    with Scope(kb) as stg:
                        KcmpT = sb(stg, "KcmpT", [64, 256], BF16); r_Kc = Res()
                        Vco = sb(stg, "Vco", [128, 2, 129], BF16); r_Vco = Res()
                        with Scope(kb) as stc:
                            w1s = Ring([sb(stc, f"w1s{i}", [128, 8, 256], F32) for i in range(2)])
                            w1b = sb(stc, "w1b", [128, 2, 16, 256], BF16); r_w1b = Res()
                            pes = sb(stc, "pes", [128, 2, 16], F32); r_pes = Res()
                            peb = sb(stc, "peb", [128, 2, 16], BF16); r_peb = Res()
                            w2s = sb(stc, "w2s", [128, 2, 2, 64], F32); r_w2s = Res()
                            w2b = sb(stc, "w2b", [128, 2, 2, 64], BF16); r_w2b = Res()
                            stk = sb(stc, "stk", [128, 2, T], BF16); r_stk = Res()
                            hb = sb(stc, "hb", [128, 4], F32); r_hb = Res()
                            gh = sb(stc, "gh", [128, 2, 2, 256], BF16); r_gh = Res()
                            for kv in range(2):
                                for hh in range(2):
                                    w_t, w_r = w1s.next()
                                    DMA("dsp" if hh == 0 else "dact", w_t[:], cmp_w1[kv, :, hh * 8:(hh + 1) * 8, :], [], [w_r])
                                    CP("pool" if hh == 0 else "dve", w1b[:, kv, hh * 8:(hh + 1) * 8, :], w_t[:], [w_r], [r_w1b])
                                DMA("dsp", pes[:, kv, :], cmp_pe[kv], [], [r_pes])
                                DMA("dact", w2s[:, kv], cmp_w2[kv], [], [r_w2s])
                                src = kcT_s if kv == 0 else vcT_s
                                sres = R["kcT_s"] if kv == 0 else R["vcT_s"]
                                DMA("dsp", stk[0:64, kv, :], src[k * 64:(k + 1) * 64, :], [sres], [r_stk])
                                MSET("pool", stk[64:128, kv, T - 1:T], 0.0, [r_stk])
                                DMA("dact", stk[64:128, kv, 0:T - 1], src[k * 64:(k + 1) * 64, 1:T], [sres], [r_stk])
                            CP("dve", peb[:], pes[:], [r_pes], [r_peb])
                            CP("dve", w2b[:], w2s[:], [r_w2s], [r_w2b])
                            MSET("pool", gh[:], 0.0, [r_gh])
                            for kv in range(2):
                                for hh in range(2):
                                    px, px_r = pX.next()
                                    for m in range(16):
                                        MM(px[:, 0:1], w1b[:, kv, m, hh * 128:(hh + 1) * 128], peb[:, kv, m:m + 1], m == 0, m == 15, [r_w1b, r_peb], [px_r])
                                    CP("dve", hb[:, kv * 2 + hh:kv * 2 + hh + 1], px[:, 0:1], [px_r], [r_hb])
                                    p_s, p_r = pS.next()
                                    for m in range(16):
                                        MM(p_s[:, 0:255], w1b[:, kv, m, hh * 128:(hh + 1) * 128], stk[:, kv, 2 * m:2 * m + 16 * 254 + 1:16], m == 0, m == 15, [r_w1b, r_stk], [p_r])
                                    ACTF(gh[:, kv, hh, 0:255], p_s[:, 0:255], AF.Gelu_apprx_tanh, [p_r, r_hb], [r_gh], bias=hb[:, kv * 2 + hh:kv * 2 + hh + 1])
                            px, px_r = pX.next()
                            for hh in range(2):
                                MM(px[0:64, 0:256], w2b[:, 0, hh, :], gh[:, 0, hh, :], hh == 0, hh == 1, [r_w2b, r_gh], [px_r])
                            CP("dve", KcmpT[:], px[0:64, 0:256], [px_r], [r_Kc])
                            for ct in range(2):
                                px, px_r = pX.next()
                                for hh in range(2):
                                    MM(px[:, 0:64], gh[:, 1, hh, ct * 128:(ct + 1) * 128], w2b[:, 1, hh, :], hh == 0, hh == 1, [r_gh, r_w2b], [px_r])
                                CP("dve", Vco[:, ct, 0:64], px[:, 0:64], [px_r], [r_Vco])
                            CP("pool", Vco[:, :, 64:129], ovl_t[:], [r_ovl], [r_Vco])
                            if k == 0:
                                dump("d_kcmp", KcmpT[:], [64, 256], BF16, r_Kc)
                                dump("d_vco", Vco[:], [128, 2, 129], BF16, r_Vco)
                                dump("d_hb", hb[:], [128, 4], F32, r_hb)
                                dump("d_gh", gh[:], [128, 2, 2, 256], BF16, r_gh)

                        QT = sb(stg, "QT", [128, 4, T], BF16)
                        r_QT = [Res() for _ in range(4)]
                        r_QM = [[Res() for _ in range(NT)] for _ in range(4)]
                        KsT = sb(stg, "KsT", [128, T], BF16); r_KsT = Res()
                        KwT = sb(stg, "KwT", [64, T], BF16); r_KwT = Res()
                        Vs = sb(stg, "Vs", [128, NT, 65], BF16); r_Vs = Res()
                        Vw = sb(stg, "Vw", [128, NT, 65], BF16); r_Vw = Res()
                        imp_acc = sb(stg, "imp_acc", [128, NT, 64], F32)
                        r_imp = [Res() for _ in range(NT)]
                        for g in range(4):
                            hg = 4 * k + g
                            DMA("dsp" if g % 2 == 0 else "dact", QT[0:64, g, :], qT_s[hg * 64:(hg + 1) * 64, :], [R["qT_s"]], [r_QT[g]])
                        DMA("dsp", KsT[0:64, :], ksT_s[k * 64:(k + 1) * 64, :], [R["ksT_s"]], [r_KsT])
                        with Scope(kb) as ste:
                            ers = sb(ste, "ers", [128, T], F32); r_ers = Res()
                            DMA("dact", ers[64:128, :], erows, [], [r_ers])
                            CP("pool", KsT[64:128, :], ers[64:128, :], [r_ers], [r_KsT])
                        DMA("dact", KwT[:], kwT_s[k * 64:(k + 1) * 64, :], [R["kwT_s"]], [r_KwT])
                        DMA("dsp", Vs[:, :, 0:64], vs_s.rearrange("(n p) c -> p n c", p=128)[:, :, k * 64:(k + 1) * 64], [R["vs_s"]], [r_Vs])
                        DMA("dact", Vw[:, :, 0:64], vw_s.rearrange("(n p) c -> p n c", p=128)[:, :, k * 64:(k + 1) * 64], [R["vw_s"]], [r_Vw])
                        MSET("pool", Vs[:, :, 64:65], 1.0, [r_Vs])
                        MSET("pool", Vw[:, :, 64:65], 1.0, [r_Vw])

                        bcs = Ring([sb(stg, f"bcs{i}", [128, 5, 512], F32) for i in range(2)])
                        BC = Ring([sb(stg, f"BCb{i}", [128, 5, 512], BF16) for i in range(2)])
                        bc_cur = {}

                        def cmp_stage1(it):
                            g, tcn, ct, last = it
                            hg = 4 * k + g
                            if tcn == 0 and ct == 0:
                                bs_t, bs_r = bcs.next()
                                DMA("dsp", bs_t[:, 0:3], bc_g[hg, :, 0:3], [], [bs_r])
                                DMA("dact", bs_t[:, 3:5], bc_g[hg, :, 3:5], [], [bs_r])
                                bc_t, bc_r = BC.next()
                                for m in range(5):
                                    STT(bc_t[:, m, :], bs_t[:, m, :], t31[:, hg:hg + 1], bcm[:, m, :], ALU.subtract, ALU.add, [bs_r, r_bcm, r_t31], [bc_r])
                                bc_cur[g] = (bc_t, bc_r)
                            bc_t, bc_r = bc_cur[g]
                            mp = tcn - 4 * ct
                            p_s, p_r = pS.next()
                            MM(p_s[:], KcmpT[:, ct * 128:(ct + 1) * 128], QT[0:64, g, tcn * 512:(tcn + 1) * 512], True, mp >= 5, [r_Kc, r_QT[g]], [p_r])
                            if mp < 5:
                                MM(p_s[:], ident_b[:], bc_t[:, mp, :], False, True, [r_identb, bc_r], [p_r])
                            P_t, P_r = P_ring.next()
                            ACTF(P_t[:], p_s[:], AF.Exp, [p_r, r_t31], [P_r], bias=t31[:, hg:hg + 1])
                            return (P_t, P_r)

                        def cmp_stage2(it, st1):
                            g, tcn, ct, last = it
                            hg = 4 * k + g
                            P_t, P_r = st1
                            for q in range(4):
                                MM(pO[q][:, 0:129], P_t[:, q * 128:(q + 1) * 128], Vco[:, ct, :], ct == 0, last, [P_r, r_Vco], [r_pO[q]])
                            if last:
                                items = []
                                for q in range(4):
                                    i = 4 * tcn + q
                                    o_t, o_r = osb.next()
                                    CP("dve", o_t[:, 0:129], pO[q][:, 0:129], [r_pO[q]], [o_r])
                                    items.append((o_t, o_r, i, imp_acc[:, i, :], r_imp[i]))
                                finish_tiles(items, 128, hg, 0, True, imp_first=(g == 0))

                        its = []
                        for g in range(4):
                            for tcn in range(8):
                                cts = [0] if tcn < 4 else [0, 1]
                                for ct in cts:
                                    its.append((g, tcn, ct, ct == cts[-1]))
                        LAG = 2
                        pend = []
                        for n in range(len(its) + LAG):
                            if n < len(its):
                                pend.append((its[n], cmp_stage1(its[n])))
                            if n >= LAG:
                                it0, st0 = pend.pop(0)
                                cmp_stage2(it0, st0)

                        if k == 0:
                            dump("d_imp", imp_acc[:], [128, NT, 64], F32, r_imp)
                            dump("d_aout_c", Aout[:], [128, NT, 512], BF16, rA)
                        MBr = Ring([sb(stg, f"MB{i}", [128, 128], F32) for i in range(2)])
                        for (mb_t, mb_r) in zip(MBr.tiles, MBr.res):
                            MSET("dve", mb_t[:], 0.0, [mb_r])
                        tk = Ring([sb(stg, f"tk{i}", [128, 2, 64], F32) for i in range(2)])
                        mxr = Ring([sb(stg, f"mx{i}", [128, 16], F32) for i in range(2)])
                        mtr = Ring([sb(stg, f"mtr{i}", [128, 128], BF16) for i in range(2)])
                        for i in range(NT):
                            tk_t, tk_r = tk.next()
                            mx_t, mx_r = mxr.next()
                            TT("dve", tk_t[:, 0, :], imp_acc[:, i, :], keep_t[:, i, :], ALU.mult, [r_imp[i], r_keep], [tk_r])
                            TT("dve", tk_t[:, 0, :], tk_t[:, 0, :], force_t[:, i, :], ALU.add, [tk_r, r_force], [tk_r])
                            E("dve", lambda: nc.vector.max(out=mx_t[:, 0:8], in_=tk_t[:, 0, :]), [tk_r], [mx_r])
                            E("dve", lambda: nc.vector.match_replace(out=tk_t[:, 1, :], in_to_replace=mx_t[:, 0:8], in_values=tk_t[:, 0, :], imm_value=-1e30), [tk_r, mx_r], [tk_r])
                            E("dve", lambda: nc.vector.max(out=mx_t[:, 8:16], in_=tk_t[:, 1, :]), [tk_r], [mx_r])
                            mb_t, mb_r = MBr.next()
                            TS("dve", mb_t[:, 64:128], tk_t[:, 0, :], mx_t[:, 15:16], None, ALU.is_ge, None, [tk_r, mx_r], [mb_r])
                            TS("dve", mb_t[:, 64:128], mb_t[:, 64:128], 1.0, -NEGM, ALU.subtract, ALU.mult, [mb_r], [mb_r])
                            px, px_r = pX.next()
                            TR(px[:, 0:128], mb_t[:], ident_f[:], [mb_r, r_identf], [px_r])
                            mt_t, mt_r = mtr.next()
                            CP("act", mt_t[64:128, :], px[64:128, 0:128], [px_r], [mt_r])
                            for g in range(4):
                                CP("pool" if g % 2 == 0 else "dve", QT[64:128, g, i * 128:(i + 1) * 128], mt_t[64:128, :], [mt_r], [r_QM[g][i]])

                        if k == 0:
                            dump("d_qt0", QT[:, 0, :], [128, T], BF16, r_QT + [x for l in r_QM for x in l])
                        def sel_stage1(it):
                            g, br, tcn, j = it
                            hg = 4 * k + g
                            qa = max(0, j - 4 * tcn)
                            qb = 3 if br == 1 else min(3, j + 4 - 4 * tcn)
                            c0, c1 = qa * 128, (qb + 1) * 128
                            t0 = tcn * 512
                            adds = []
                            for q in range(qa, qb + 1):
                                dlt = 4 * tcn + q - j
                                if dlt == 0:
                                    adds.append((q, 0))
                                elif dlt == 1:
                                    adds.append((q, 1))
                                elif dlt == 4 and br == 2:
                                    adds.append((q, 2))
                            p_s, p_r = pS.next()
                            if br == 1:
                                rd = [r_KsT, r_QT[g]] + [r_QM[g][4 * tcn + q] for q in range(qa, qb + 1)]
                                MM(p_s[:, c0:c1], KsT[:, j * 128:(j + 1) * 128], QT[:, g, t0 + c0:t0 + c1], True, len(adds) == 0, rd, [p_r])
                            else:
                                MM(p_s[:, c0:c1], KwT[:, j * 128:(j + 1) * 128], QT[0:64, g, t0 + c0:t0 + c1], True, len(adds) == 0, [r_KwT, r_QT[g]], [p_r])
                            for ai, (q, ty) in enumerate(adds):
                                MM(p_s[:, q * 128:(q + 1) * 128], ident_b[:], BD[:, hg, ty, :], False, ai == len(adds) - 1, [r_identb, r_BD], [p_r])
                            P_t, P_r = P_ring.next()
                            ACTF(P_t[:, c0:c1], p_s[:, c0:c1], AF.Exp, [p_r, r_t31], [P_r], bias=t31[:, hg:hg + 1])
                            return (P_t, P_r, qa, qb)

                        def sel_stage2(it, st1):
                            g, br, tcn, j = it
                            hg = 4 * k + g
                            P_t, P_r, qa, qb = st1
                            Vx, r_Vx = (Vs, r_Vs) if br == 1 else (Vw, r_Vw)
                            for q in range(qa, qb + 1):
                                i = 4 * tcn + q
                                first_j = 0 if br == 1 else max(0, i - 4)
                                MM(pO[q][:, 0:65], P_t[:, q * 128:(q + 1) * 128], Vx[:, j, :], j == first_j, j == i, [P_r, r_Vx], [r_pO[q]])
                            if j == 4 * tcn + 3:
                                items = []
                                for q in range(4):
                                    o_t, o_r = osb.next()
                                    CP("dve", o_t[:, 0:65], pO[q][:, 0:65], [r_pO[q]], [o_r])
                                    items.append((o_t, o_r, 4 * tcn + q, None, None))
                                finish_tiles(items, 64, hg, br, False)

                        its = []
                        for g in range(4):
                            for br in (1, 2):
                                for tcn in range(8):
                                    j_lo = 0 if br == 1 else max(0, 4 * tcn - 4)
                                    for j in range(j_lo, 4 * tcn + 4):
                                        its.append((g, br, tcn, j))
                        LAG = 2
                        pend = []
                        for n in range(len(its) + LAG):
                            if n < len(its):
                                pend.append((its[n], sel_stage1(its[n])))
                            if n >= LAG:
                                it0, st0 = pend.pop(0)
                                sel_stage2(it0, st0)

                dump("d_aout", Aout[:], [128, NT, 512], BF16, rA)
                with Scope(kb) as stn:
                    junkC = sb(stn, "junkC", [128, 512], BF16); r_junkC = Res()
                    an = Ring([sb(stn, f"an{i}", [128, 512], BF16) for i in range(2)])
                    af = Ring([sb(stn, f"af{i}", [128, 512], F32) for i in range(2)])
                    ao = Ring([sb(stn, f"ao{i}", [128, 512], BF16) for i in range(2)])
                    pTb = Ring([ps(stn, f"pTC{i}", [128, 512], BF16) for i in range(2)]) if False else None
                    for i in range(NT):
                        s_t, s_r = sm.next()
                        ACTF(junkC[:], Aout[:, i, :], AF.Square, [rA[i]], [r_junkC, s_r], accum_out=s_t[:, 0:1])
                        TS("dve", s_t[:, 1:2], s_t[:, 0:1], 1.0 / 512, EPS, ALU.mult, ALU.add, [s_r], [s_r])
                        ACTF(s_t[:, 1:2], s_t[:, 1:2], AF.Sqrt, [s_r], [s_r])
                        E("dve", lambda: nc.vector.reciprocal(out=s_t[:, 2:3], in_=s_t[:, 1:2]), [s_r], [s_r])
                        af_t, af_r = af.next()
                        STT(af_t[:], Aout[:, i, :], s_t[:, 2:3], ga_t[:], ALU.mult, ALU.mult, [rA[i], s_r, r_ga], [af_r])
                        px, px_r = pX.next()
                        for fc in range(4):
                            TR(px[:, fc * 128:(fc + 1) * 128], af_t[:, fc * 128:(fc + 1) * 128], ident_f[:], [af_r, r_identf], [px_r])
                        ao_t, ao_r = ao.next()
                        CP("act", ao_t[:], px[:], [px_r], [ao_r])
                        DMA("dsp" if i % 2 == 0 else "dpool", mixT_s[0:512, i * 128:(i + 1) * 128].rearrange("(f p) t -> p f t", p=128),
                            ao_t[:].rearrange("p (f t) -> p f t", f=4), [ao_r], [R["mixT_s"]])

        if "D" in phases or "D1" in phases:
            NTL = TH // 128
            with Scope(kb) as st:
                Wo = sb(st, "Wo", [128, 8, D], BF16); r_Wo = Res()
                Wq = sb(st, "Wq", [128, 8, 2048], BF16); r_Wq = Res()
                skb = sb(st, "skb", [128, 2, 128], BF16); r_skb = Res()
                repf = sb(st, "repf", [128, D], F32); r_rep = Res()
                io16 = sb(st, "io16", [128, 16], F32); r_io = Res()
                io128 = sb(st, "io128", [128, 128], F32); r_io128 = Res()
                selt = sb(st, "selt", [128, 2], F32); r_sel = Res()
                DMA("dsp", repf[:], rep4[:, 0, :], [], [r_rep])
                DMA("dact", io16[:], iota16, [], [r_io])
                DMA("dact", io128[:], iota128, [], [r_io128])
                DMA("dact", selt[:], selc, [], [r_sel])
                with Scope(kb) as stw:
                    wst = Ring([sb(stw, f"wstD{i}", [128, 2048], F32) for i in range(3)])
                    n = 0
                    for (src, dstw, dres, ncol, nch) in ((w_out_in, Wo, r_Wo, D, 8), (peer_wq, Wq, r_Wq, 2048, 8)):
                        for dc in range(nch):
                            w_t, w_r = wst.next()
                            n += 1
                            DMA("dsp" if n % 2 == 0 else "dact", w_t[:, 0:ncol], src[dc * 128:(dc + 1) * 128, :], [], [w_r])
                            CP("dve" if n % 2 == 0 else "pool", dstw[:, dc, :], w_t[:, 0:ncol], [w_r], [dres])
                    w_t, w_r = wst.next()
                    DMA("dsp", w_t[:, 0:256].rearrange("p (a k) -> p a k", a=2), sk_T.rearrange("a p k -> p a k"), [], [w_r])
                    CP("dve", skb[:], w_t[:, 0:256].rearrange("p (a k) -> p a k", a=2), [w_r], [r_skb])

                pacc = Ring([ps(st, f"pD{i}", [128, 512], F32) for i in range(4)])
                pw_ring = Ring([ps(st, f"pDw{i}", [128, 512], F32) for i in range(2)])
                ptb = Ring([ps(st, f"pDb{i}", [128, 1024], BF16) for i in range(2)])
                mst = Ring([sb(st, f"mst{i}", [128, 8, 2, 128], BF16) for i in range(1)])
                mixh_ring = Ring([sb(st, f"mixh{i}", [128, 8, 128], BF16) for i in range(1)])
                xh_ring = Ring([sb(st, f"xhD{i}", [128, D], F32) for i in range(2)])
                H_ring = Ring([sb(st, f"HD{i}", [128, D], F32) for i in range(2)])
                xng_ring = Ring([sb(st, f"xng{i}", [128, D], F32) for i in range(1)])
                xnb_ring = Ring([sb(st, f"xnbD{i}", [128, D], BF16) for i in range(1)])
                xT_ring = Ring([sb(st, f"xTD{i}", [128, 8, 128], BF16) for i in range(2)])
                qTb = sb(st, "qTb", [128, 16, 128], BF16); r_qTb = Res()
                Ssc_ring = Ring([sb(st, f"Ssc{i}", [128, 16, 128], F32) for i in range(2)])
                Swk = sb(st, "Swk", [128, 8, 128], F32)
                rv = [Res() for _ in range(16)]; rv2 = [Res() for _ in range(16)]; ri = [Res() for _ in range(16)]; ri2 = [Res() for _ in range(16)]; rw = [Res() for _ in range(16)]
                v16 = sb(st, "v16", [128, 16, 16], F32); r_v16 = Res()
                i16 = sb(st, "i16", [128, 16, 16], U32); r_i16 = Res()
                i16f = sb(st, "i16f", [128, 16, 16], F32); r_i16f = Res()
                cand = sb(st, "cand", [128, 8, 256], F32); r_cand = Res()
                cwk = sb(st, "cwk", [128, 8, 256], F32)
                sc16 = sb(st, "sc16", [128, 8, 16], F32); r_sc = Res()
                ci16 = sb(st, "ci16", [128, 8, 16], U32); r_ci = Res()
                ab_u = sb(st, "ab_u", [128, 2, 8, 16], U32); r_abu = Res()
                ab_f = sb(st, "ab_f", [128, 2, 8, 16], F32); r_abf = Res()
                eq = sb(st, "eq", [128, 8, 16, 16], F32); r_eq = Res()
                isel_ring = Ring([sb(st, f"isel{i}", [128, 3, 8, 16], F32) for i in range(2)])
                gz = sb(st, "gz", [128, 16], F32); r_gz = Res()
                junkB = sb(st, "junkDb", [128, D], BF16); r_junkB = Res()
                smD = Ring([sb(st, f"smD{i}", [128, 8], F32) for i in range(4)])
                ijgT_ring = Ring([sb(st, f"ijgT{i}", [128, 3, 128], F32) for i in range(2)])
                OI = Ring([sb(st, f"OI{i}", [128, 16, 128], BF16) for i in range(2)])
                OJ = Ring([sb(st, f"OJ{i}", [128, 16, 128], BF16) for i in range(2)])
                OJf = Ring([sb(st, f"OJf{i}", [128, 16, 128], BF16) for i in range(2)])
                Wst = sb(st, "Wst", [128, 128, 128], BF16); r_Wst = Res()

                def rms_scaled(src, src_r, gain, gain_r, dstf, dstf_r):
                    s_t, s_r = smD.next()
                    ACTF(junkB[:], src, AF.Square, [src_r], [r_junkB, s_r], accum_out=s_t[:, 0:1])
                    TS("dve", s_t[:, 1:2], s_t[:, 0:1], 1.0 / D, EPS, ALU.mult, ALU.add, [s_r], [s_r])
                    ACTF(s_t[:, 1:2], s_t[:, 1:2], AF.Sqrt, [s_r], [s_r])
                    E("dve", lambda: nc.vector.reciprocal(out=s_t[:, 2:3], in_=s_t[:, 1:2]), [s_r], [s_r])
                    STT(dstf, src, s_t[:, 2:3], gain, ALU.mult, ALU.mult, [src_r, s_r, gain_r], [dstf_r])

                def rms_scaled_g(src, src_r, gain, gain_r, dstf, dstf_r):
                    s_t, s_r = smD.next()
                    ACTF(junkB[:], src, AF.Square, [src_r], [r_junkB, s_r], accum_out=s_t[:, 0:1])
                    yield
                    TS("dve", s_t[:, 1:2], s_t[:, 0:1], 1.0 / D, EPS, ALU.mult, ALU.add, [s_r], [s_r])
                    ACTF(s_t[:, 1:2], s_t[:, 1:2], AF.Sqrt, [s_r], [s_r])
                    yield
                    E("dve", lambda: nc.vector.reciprocal(out=s_t[:, 2:3], in_=s_t[:, 1:2]), [s_r], [s_r])
                    STT(dstf, src, s_t[:, 2:3], gain, ALU.mult, ALU.mult, [src_r, s_r, gain_r], [dstf_r])

                def transpose8(srcb, srcb_r, dstT, dstT_r, nblk=8):
                    pt, pt_r = ptb.next()
                    for dc in range(nblk):
                        TR(pt[:, dc * 128:(dc + 1) * 128], srcb[:, dc * 128:(dc + 1) * 128], ident_b[:], [srcb_r, r_identb], [pt_r])
                    CP("act", dstT.rearrange("p a t -> p (a t)"), pt[:, 0:nblk * 128], [pt_r], [dstT_r])

                def S1(it):
                    tsl = slice(it * 128, (it + 1) * 128)
                    xh_t, xh_r = xh_ring.next()
                    DMA("dsp", xh_t[:], xh[tsl, :], [], [xh_r])
                    H, H_r = H_ring.next()
                    m_t, m_r = mst.next()
                    for a in range(2):
                        DMA("dsp" if a == 0 else "dact", m_t[:, :, a, :], mixT_s[:, a * TH + it * 128:a * TH + (it + 1) * 128].rearrange("(f p) t -> p f t", p=128), [R["mixT_s"]], [m_r])
                    mixh, r_mixh = mixh_ring.next()
                    TS("pool", mixh[:], m_t[:, :, 0, :], selt[:, 0:1], None, ALU.mult, None, [m_r, r_sel], [r_mixh])
                    STT(mixh[:], m_t[:, :, 1, :], selt[:, 1:2], mixh[:], ALU.mult, ALU.add, [m_r, r_sel, r_mixh], [r_mixh])
                    yield
                    for ch in range(2):
                        pa, pa_r = pacc.next()
                        for fc in range(8):
                            MM(pa[:], mixh[:, fc, :], Wo[:, fc, ch * 512:(ch + 1) * 512], fc == 0, fc == 7, [r_mixh, r_Wo], [pa_r])
                        TT("dve", H[:, ch * 512:(ch + 1) * 512], pa[:], xh_t[:, ch * 512:(ch + 1) * 512], ALU.add, [pa_r, xh_r], [H_r])
                        yield
                    DMA("dpool", H1_s[tsl, :], H[:], [H_r], [R["H1_s"]])
                    xng, xng_r = xng_ring.next()
                    yield from rms_scaled_g(H[:], H_r, repf[:], r_rep, xng[:], xng_r)
                    yield
                    xnb, xnb_r = xnb_ring.next()
                    CP("pool", xnb[:], xng[:], [xng_r], [xnb_r])
                    yield
                    xT, xT_r = xT_ring.next()
                    transpose8(xnb, xnb_r, xT[:], xT_r)
                    yield
                    DMA("dact", xnT2_s[:, :, tsl], xT[:], [xT_r], [R["xnT2_s"]])
                    for grp in range(4):
                        pa, pa_r = pacc.next()
                        for j in range(4):
                            hp = grp * 4 + j
                            for dc in range(8):
                                MM(pa[:, j * 128:(j + 1) * 128], Wq[:, dc, hp * 128:(hp + 1) * 128], xT[:, dc, :], dc == 0, dc == 7, [r_Wq, xT_r], [pa_r])
                        CP("act", qTb[:, grp * 4:(grp + 1) * 4, :].rearrange("p a t -> p (a t)"), pa[:], [pa_r], [r_qTb])
                        yield
                    Ssc, r_S = Ssc_ring.next()
                    for grp in range(4):
                        pa, pa_r = pacc.next()
                        for j in range(4):
                            hp = grp * 4 + j
                            MM(pa[:, j * 128:(j + 1) * 128], qTb[:, hp, :], skb[:, hp % 2, :], True, True, [r_qTb, r_skb], [pa_r])
                        CP("act", Ssc[:, grp * 4:(grp + 1) * 4, :].rearrange("p a t -> p (a t)"), pa[:], [pa_r], [r_S])
                        yield
                    return (Ssc, r_S)

                def S2(it, st1):
                    Ssc, r_S = st1
                    for g0 in (0, 8):
                        hps = range(g0, g0 + 8)
                        for hp in hps:
                            E("dve", lambda: nc.vector.max(out=v16[:, hp, 0:8], in_=Ssc[:, hp, :]), [r_S], [rv[hp]])
                        yield
                        for hp in hps:
                            E("dve", lambda: nc.vector.max_index(out=i16[:, hp, 0:8], in_max=v16[:, hp, 0:8], in_values=Ssc[:, hp, :]), [r_S, rv[hp]], [ri[hp]])
                        yield
                        for hp in hps:
                            E("dve", lambda: nc.vector.match_replace(out=Swk[:, hp - g0, :], in_to_replace=v16[:, hp, 0:8], in_values=Ssc[:, hp, :], imm_value=-1e30), [r_S, rv[hp]], [rw[hp - g0]])
                        yield
                        for hp in hps:
                            E("dve", lambda: nc.vector.max(out=v16[:, hp, 8:16], in_=Swk[:, hp - g0, :]), [rw[hp - g0]], [rv2[hp]])
                        yield
                        for hp in hps:
                            E("dve", lambda: nc.vector.max_index(out=i16[:, hp, 8:16], in_max=v16[:, hp, 8:16], in_values=Swk[:, hp - g0, :]), [rw[hp - g0], rv2[hp]], [ri2[hp]])
                        yield
                    r_i16 = Res()
                    E("dve", lambda: nc.vector.tensor_copy(out=i16f[:], in_=i16[:]), ri + ri2, [r_i16f, r_i16])
                    v4 = v16[:].rearrange("p (h two) k -> p h two k", two=2)
                    in0 = v4[:, :, 0, :].rearrange("p h (a o) -> p h a o", o=1).to_broadcast([128, 8, 16, 16])
                    in1 = v4[:, :, 1, :].rearrange("p h (o b) -> p h o b", o=1).to_broadcast([128, 8, 16, 16])
                    TT("dve", cand[:].rearrange("p h (a b) -> p h a b", a=16), in0, in1, ALU.add, rv + rv2, [r_cand])
                    for h in range(8):
                        E("dve", lambda: nc.vector.max(out=sc16[:, h, 0:8], in_=cand[:, h, :]), [r_cand], [rv[h]])
                    yield
                    for h in range(8):
                        E("dve", lambda: nc.vector.max_index(out=ci16[:, h, 0:8], in_max=sc16[:, h, 0:8], in_values=cand[:, h, :]), [r_cand, rv[h]], [ri[h]])
                    yield
                    for h in range(8):
                        E("dve", lambda: nc.vector.match_replace(out=cwk[:, h, :], in_to_replace=sc16[:, h, 0:8], in_values=cand[:, h, :], imm_value=-1e30), [r_cand, rv[h]], [rw[h]])
                    yield
                    for h in range(8):
                        E("dve", lambda: nc.vector.max(out=sc16[:, h, 8:16], in_=cwk[:, h, :]), [rw[h]], [rv2[h]])
                    yield
                    for h in range(8):
                        E("dve", lambda: nc.vector.max_index(out=ci16[:, h, 8:16], in_max=sc16[:, h, 8:16], in_values=cwk[:, h, :]), [rw[h], rv2[h]], [ri2[h]])
                    yield
                    r_sc = Res(); r_ci = Res()
                    E("dve", lambda: nc.vector.tensor_single_scalar(out=ab_u[:, 0], in_=ci16[:], scalar=4, op=ALU.logical_shift_right), ri[:8] + ri2[:8] + rv[:8] + rv2[:8], [r_abu, r_sc, r_ci])
                    E("dve", lambda: nc.vector.tensor_single_scalar(out=ab_u[:, 1], in_=ci16[:], scalar=15, op=ALU.bitwise_and), [r_ci], [r_abu])
                    CP("dve", ab_f[:], ab_u[:], [r_abu], [r_abf])
                    isel, r_isel = isel_ring.next()
                    i4 = i16f[:].rearrange("p (h two) k -> p h two k", two=2)
                    for w in range(2):
                        a_b = ab_f[:, w].rearrange("p h (k o) -> p h k o", o=1).to_broadcast([128, 8, 16, 16])
                        io_b = io16[:].rearrange("p (o q a) -> p o q a", o=1, q=1).to_broadcast([128, 8, 16, 16])
                        TT("dve", eq[:], a_b, io_b, ALU.is_equal, [r_abf, r_io], [r_eq])
                        iv_b = i4[:, :, w, :].rearrange("p h (o a) -> p h o a", o=1).to_broadcast([128, 8, 16, 16])
                        TT("dve", eq[:], eq[:], iv_b, ALU.mult, [r_eq, r_i16f], [r_eq])
                        E("dve", lambda: nc.vector.tensor_reduce(out=isel[:, w], in_=eq[:], axis=AX.X, op=ALU.add), [r_eq], [r_isel])
                        yield
                    TT("dve", isel[:, 2], sc16[:], sc16[:, :, 0:1].to_broadcast([128, 8, 16]), ALU.subtract, [r_sc], [r_isel])
                    ACTF(isel[:, 2], isel[:, 2], AF.Exp, [r_isel], [r_isel])
                    E("dve", lambda: nc.vector.tensor_reduce(out=gz[:, 0:8], in_=isel[:, 2], axis=AX.X, op=ALU.add), [r_isel], [r_gz])
                    E("dve", lambda: nc.vector.reciprocal(out=gz[:, 8:16], in_=gz[:, 0:8]), [r_gz], [r_gz])
                    TT("dve", isel[:, 2], isel[:, 2], gz[:, 8:16].rearrange("p (h o) -> p h o", o=1).to_broadcast([128, 8, 16]), ALU.mult, [r_isel, r_gz], [r_isel])
                    pa, pa_r = pacc.next()
                    for w in range(3):
                        TR(pa[:, w * 128:(w + 1) * 128], isel[:, w].rearrange("p h k -> p (h k)"), ident_f[:], [r_isel, r_identf], [pa_r])
                    ijgT, r_ijgT = ijgT_ring.next()
                    CP("act", ijgT[:].rearrange("p a t -> p (a t)"), pa[:, 0:384], [pa_r], [r_ijgT])
                    return (ijgT, r_ijgT)

                def S3(it, st2):
                    ijgT, r_ijgT = st2
                    TB = 16
                    for tb in range(128 // TB):
                        t0 = tb * TB
                        oi, oi_r = OI.next()
                        oj, oj_r = OJ.next()
                        ojf, ojf_r = OJf.next()
                        io_b = io128[:].rearrange("p (o i) -> p o i", o=1).to_broadcast([128, TB, 128])

                        def colb(w):
                            return ijgT[:, w, t0:t0 + TB].rearrange("p (t o) -> p t o", o=1).to_broadcast([128, TB, 128])
                        TT("dve", oi[:], io_b, colb(0), ALU.is_equal, [r_io128, r_ijgT], [oi_r])
                        TT("dve", ojf[:], io_b, colb(1), ALU.is_equal, [r_io128, r_ijgT], [ojf_r])
                        TT("pool", oj[:], ojf[:], colb(2), ALU.mult, [ojf_r, r_ijgT], [oj_r])
                        for tq in range(TB // 4):
                            pw, pw_r = pw_ring.next()
                            for u in range(4):
                                MM(pw[:, u:512:4], oj[:, tq * 4 + u, :], oi[:, tq * 4 + u, :], True, True, [oj_r, oi_r], [pw_r])
                            tg = t0 + tq * 4
                            CP("act", Wst[:, :, tg:tg + 4], pw[:].rearrange("p (i t) -> p i t", t=4), [pw_r], [r_Wst])
                            yield
                    DMA("dsp" if it % 2 == 0 else "dact", Wt_s[it], Wst[:], [r_Wst], [R["Wt_s"]])

                def drive(gens):
                    res = [None] * len(gens)
                    live = list(range(len(gens)))
                    while live:
                        for gi in list(live):
                            try:
                                next(gens[gi])
                            except StopIteration as e:
                                res[gi] = e.value
                                live.remove(gi)
                    return res

                st1s, st2s = {}, {}
                for n in range(NTL + 2):
                    gens, tags = [], []
                    if n < NTL:
                        gens.append(S1(n)); tags.append(("s1", n))
                    if n >= 2:
                        gens.append(S3(n - 2, st2s.pop(n - 2))); tags.append(("s3", n - 2))
                    if 1 <= n <= NTL:
                        gens.append(S2(n - 1, st1s.pop(n - 1))); tags.append(("s2", n - 1))
                    for (tg_, tn), rv_ in zip(tags, drive(gens)):
                        if tg_ == "s1":
                            st1s[tn] = rv_
                        elif tg_ == "s2":
                            st2s[tn] = rv_

            with Scope(kb) as st:
                Yacc = sb(st, "Yacc", [128, NTL, D], F32)
                rY = [Res() for _ in range(NTL)]
                H1v = H1_s.rearrange("(n p) d -> p n d", p=128)
                for n4 in range(4):
                    DMA("dsp" if n4 % 2 == 0 else "dact", Yacc[:, n4 * 4:(n4 + 1) * 4, :], H1v[:, n4 * 4:(n4 + 1) * 4, :], [R["H1_s"]], rY[n4 * 4:(n4 + 1) * 4])
                p1 = Ring([ps(st, f"pE1{i}", [128, 512], F32) for i in range(3)])
                p2 = Ring([ps(st, f"pE2{i}", [128, 512], F32) for i in range(3)])
                ptb2 = Ring([ps(st, f"pEb{i}", [128, 1024], BF16) for i in range(2)])
                with Scope(kb) as st2:
                  if "D" in phases or "D2" in phases:
                    xnTa = sb(st2, "xnTa", [128, 8, TH], BF16); r_xnTa = Res()
                    for dc in range(8):
                        DMA("dsp" if dc % 2 == 0 else "dact", xnTa[:, dc, :], xnT2_s[:, dc, :], [R["xnT2_s"]], [r_xnTa])
                    IB = 8
                    ust = Ring([sb(st2, f"ust{i}", [128, D], F32) for i in range(2)])
                    vst = Ring([sb(st2, f"vst{i}", [128, D], F32) for i in range(2)])
                    ub = Ring([sb(st2, f"ub{i}", [128, D], BF16) for i in range(2)])
                    uT = Ring([sb(st2, f"uT{i}", [128, 8, 128], BF16) for i in range(2)])
                    Vb = sb(st2, "Vb", [128, IB, D], BF16); r_Vb = [Res() for _ in range(IB)]
                    WA = sb(st2, "WA", [128, IB, TH], BF16); r_WA = [Res() for _ in range(IB)]
                    wt = Ring([sb(st2, f"wt{i}", [128, TH], BF16) for i in range(3)])
                    gl = Ring([sb(st2, f"gl{i}", [128, 512], BF16) for i in range(3)])
                    for ib0 in range(0, 128, IB):
                        for ib in range(IB):
                            i = ib0 + ib
                            u_t, u_r = ust.next()
                            v_t, v_r = vst.next()
                            w_t, w_r = wt.next()
                            DMA("dsp", u_t[:], peer_u[i * 128:(i + 1) * 128, :], [], [u_r])
                            DMA("dact", v_t[:], peer_v[i * 128:(i + 1) * 128, :], [], [v_r])
                            for hw in range(2):
                                DMA("dpool" if hw == 0 else ("dsp" if i % 2 == 0 else "dact"), w_t[:, hw * 1024:(hw + 1) * 1024].rearrange("p (n t) -> p n t", t=128),
                                    Wt_s[hw * 8:(hw + 1) * 8, :, i, :].rearrange("n j t -> j n t"), [R["Wt_s"]], [w_r])
                            ub_t, ub_r = ub.next()
                            CP("pool", ub_t[:], u_t[:], [u_r], [ub_r])
                            CP("pool", Vb[:, ib, :], v_t[:], [v_r], [r_Vb[ib]])
                            pt, pt_r = ptb2.next()
                            for dc in range(8):
                                TR(pt[:, dc * 128:(dc + 1) * 128], ub_t[:, dc * 128:(dc + 1) * 128], ident_b[:], [ub_r, r_identb], [pt_r])
                            uT_t, uT_r = uT.next()
                            CP("act", uT_t[:].rearrange("p a t -> p (a t)"), pt[:], [pt_r], [uT_r])
                            for tc4 in range(4):
                                pa, pa_r = p1.next()
                                for dc in range(8):
                                    MM(pa[:], uT_t[:, dc, :], xnTa[:, dc, tc4 * 512:(tc4 + 1) * 512], dc == 0, dc == 7, [uT_r, r_xnTa], [pa_r])
                                g_t, g_r = gl.next()
                                ACTF(g_t[:], pa[:], AF.Gelu_apprx_tanh, [pa_r], [g_r])
                                TT("dve", WA[:, ib, tc4 * 512:(tc4 + 1) * 512], g_t[:], w_t[:, tc4 * 512:(tc4 + 1) * 512], ALU.mult, [g_r, w_r], [r_WA[ib]])
                        for tt in range(NTL):
                            for ch in range(2):
                                pb, pb_r = p2.next()
                                for ib in range(IB):
                                    MM(pb[:], WA[:, ib, tt * 128:(tt + 1) * 128], Vb[:, ib, ch * 512:(ch + 1) * 512], ib == 0, ib == IB - 1, [r_WA[ib], r_Vb[ib]], [pb_r])
                                TT("dve", Yacc[:, tt, ch * 512:(ch + 1) * 512], Yacc[:, tt, ch * 512:(ch + 1) * 512], pb[:], ALU.add, [rY[tt], pb_r], [rY[tt]])


                with Scope(kb) as st3:
                    Wgt = sb(st3, "Wgt", [128, 8, D], BF16); r_Wgt = Res()
                    Wp = sb(st3, "Wp", [128, 2, D], BF16); r_Wp = Res()
                    rep3 = sb(st3, "rep3", [128, 3, D], F32); r_rep3 = Res()
                    DMA("dsp", rep3[:], rep4[:, 1:4, :], [], [r_rep3])
                    wst3 = Ring([sb(st3, f"wst3{i}", [128, D], F32) for i in range(2)])
                    for (src, dstw, dres, nch) in ((ple_wg, Wgt, r_Wgt, 8), (ple_pj, Wp, r_Wp, 2)):
                        for dc in range(nch):
                            w_t, w_r = wst3.next()
                            DMA("dsp" if dc % 2 == 0 else "dact", w_t[:], src[dc * 128:(dc + 1) * 128, :], [], [w_r])
                            CP("dve" if dc % 2 == 0 else "pool", dstw[:, dc, :], w_t[:], [w_r], [dres])
                    x3_ring = Ring([sb(st3, f"x3{i}", [128, D], F32) for i in range(2)])
                    x3b_ring = Ring([sb(st3, f"x3b{i}", [128, D], BF16) for i in range(2)])
                    x3T_ring = Ring([sb(st3, f"x3T{i}", [128, 8, 128], BF16) for i in range(2)])
                    pht = Ring([sb(st3, f"pht{i}", [128, 256], F32) for i in range(2)])
                    phb = Ring([sb(st3, f"phb{i}", [128, 256], BF16) for i in range(2)])
                    phT = Ring([sb(st3, f"phT{i}", [128, 2, 128], BF16) for i in range(2)])
                    gt_ring = Ring([sb(st3, f"gtD{i}", [128, D], F32) for i in range(2)])
                    ot_ring = Ring([sb(st3, f"otD{i}", [128, D], F32) for i in range(2)])
                    junk3 = sb(st3, "junk3", [128, D], BF16); r_junk3 = Res()
                    sm3 = Ring([sb(st3, f"sm3{i}", [128, 8], F32) for i in range(4)])

                    def rms3(src, src_r, gi, dstf, dstf_r):
                        s_t, s_r = sm3.next()
                        ACTF(junk3[:], src, AF.Square, [src_r], [r_junk3, s_r], accum_out=s_t[:, 0:1])
                        TS("dve", s_t[:, 1:2], s_t[:, 0:1], 1.0 / D, EPS, ALU.mult, ALU.add, [s_r], [s_r])
                        ACTF(s_t[:, 1:2], s_t[:, 1:2], AF.Sqrt, [s_r], [s_r])
                        E("dve", lambda: nc.vector.reciprocal(out=s_t[:, 2:3], in_=s_t[:, 1:2]), [s_r], [s_r])
                        STT(dstf, src, s_t[:, 2:3], rep3[:, gi, :], ALU.mult, ALU.mult, [src_r, s_r, r_rep3], [dstf_r])

                    def tr3(srcb, srcb_r, dstT, dstT_r, nblk):
                        pt, pt_r = ptb2.next()
                        for dc in range(nblk):
                            TR(pt[:, dc * 128:(dc + 1) * 128], srcb[:, dc * 128:(dc + 1) * 128], ident_b[:], [srcb_r, r_identb], [pt_r])
                        CP("act", dstT.rearrange("p a t -> p (a t)"), pt[:, 0:nblk * 128], [pt_r], [dstT_r])

                    for it in range(NTL):
                        tsl = slice(it * 128, (it + 1) * 128)
                        Hh = Yacc[:, it, :]; H_r = rY[it]
                        x3, x3_r = x3_ring.next()
                        rms3(Hh, H_r, 0, x3[:], x3_r)
                        x3b, x3b_r = x3b_ring.next()
                        CP("pool", x3b[:], x3[:], [x3_r], [x3b_r])
                        x3T, x3T_r = x3T_ring.next()
                        tr3(x3b, x3b_r, x3T[:], x3T_r, 8)
                        ph_t, ph_r = pht.next()
                        DMA("dact", ph_t[:], ph[tsl, :], [], [ph_r])
                        pb_t, pb_r = phb.next()
                        CP("pool", pb_t[:], ph_t[:], [ph_r], [pb_r])
                        pT_t, pT_r = phT.next()
                        tr3(pb_t, pb_r, pT_t[:], pT_r, 2)
                        gt, gt_r = gt_ring.next()
                        for ch in range(2):
                            csl = slice(ch * 512, (ch + 1) * 512)
                            pa, pa_r = p1.next()
                            for dc in range(8):
                                MM(pa[:], x3T[:, dc, :], Wgt[:, dc, csl], dc == 0, dc == 7, [x3T_r, r_Wgt], [pa_r])
                            TT("dve", gt[:, csl], pa[:], rep3[:, 2, csl], ALU.add, [pa_r, r_rep3], [gt_r])
                            ACTF(gt[:, csl], gt[:, csl], AF.Sigmoid, [gt_r], [gt_r])
                            pb2, pb2_r = p2.next()
                            for dc in range(2):
                                MM(pb2[:], pT_t[:, dc, :], Wp[:, dc, csl], dc == 0, dc == 1, [pT_r, r_Wp], [pb2_r])
                            TT("dve", gt[:, csl], gt[:, csl], pb2[:], ALU.mult, [gt_r, pb2_r], [gt_r])
                            TT("pool", Yacc[:, it, csl], Yacc[:, it, csl], gt[:, csl], ALU.add, [H_r, gt_r], [H_r])
                        ot, ot_r = ot_ring.next()
                        rms3(Hh, H_r, 1, ot[:], ot_r)
                        DMA("dsp", out[tsl, :], ot[:], [ot_r], [])

        kb.drain_all()
    return nc


def _blockdiag(w):
    o = np.zeros((128, 4, 128), np.float32)
    for n in range(8):
        cc, j = n // 2, n % 2
        o[j * 64:(j + 1) * 64, cc, j * 64:(j + 1) * 64] = w[n]
    return o


def _rel_bucket(dist):
    n = np.maximum(dist, 0)
    nf = np.maximum(n, 16).astype(np.float32)
    large = 16 + (np.log(nf / np.float32(16)) / np.float32(np.log(8.0)) * np.float32(16)).astype(np.int32)
    large = np.minimum(large, 31)
    return np.where(n < 16, n, large)


def _nsa_consts(rel_table):
    c = {}
    assert (_rel_bucket(np.arange(113, 8192)) == 31).all()
    cl = np.arange(128)[:, None, None]; mp = np.arange(5)[None, :, None]; tt = np.arange(512)[None, None, :]
    dist = 512 * mp + tt - 16 * cl - 31
    c["bc_g"] = np.ascontiguousarray(rel_table[_rel_bucket(dist)].transpose(3, 0, 1, 2))
    c["bc_m"] = np.where(dist >= 0, 0.0, NEGM).astype(np.float32)
    assert (512 * 5 - 16 * 127 - 31) >= 113
    sl = np.arange(128)[:, None]; tl = np.arange(128)[None, :]
    d0 = tl - sl; d1 = 128 + tl - sl
    g0 = rel_table[_rel_bucket(d0)]; g1 = rel_table[_rel_bucket(d1)]
    c["bd_g"] = np.ascontiguousarray(np.stack([g0, g1], 0).transpose(3, 1, 0, 2))
    m0 = np.where(d0 >= 0, 0.0, NEGM); m2 = np.where(tl < sl, 0.0, NEGM)
    c["bd_m"] = np.ascontiguousarray(np.stack([m0, np.zeros_like(m0), m2], 1)).astype(np.float32)
    c["t31"] = np.ascontiguousarray(np.broadcast_to(rel_table[31][None, :], (128, 8))).astype(np.float32)
    t = (np.arange(NT)[None, :, None] * 128 + np.arange(128)[:, None, None])
    blk = np.arange(64)[None, None, :]
    d = t // 64 - blk
    local = (d >= 0) & (d < 2)
    init = (blk == 0) & ~local
    past = (d >= 0) & ~local & ~init
    c["force_c"] = np.where(local, 2.0e4, np.where(init, 1.0e4, np.where(past, 0.0, -1.0))).astype(np.float32)
    c["keep_c"] = past.astype(np.float32)
    cs = np.arange(256)[:, None] * 16; ss = np.arange(64)[None, :] * 64
    ov = np.clip(np.minimum(cs + 32, ss + 64) - np.maximum(cs, ss), 0, None).astype(np.float32) / 32.0
    ove = np.concatenate([ov, np.ones((256, 1), np.float32)], 1)
    ove[255] = 0.0
    c["ovl_ext"] = np.ascontiguousarray(ove.reshape(2, 128, 65).transpose(1, 0, 2))
    c["erows"] = (np.arange(T)[None, :] // 64 == np.arange(64)[:, None]).astype(np.float32)
    return c


def _prep_inputs(inputs):
    x = np.ascontiguousarray(inputs["x"], dtype=np.float32)
    p = np.ascontiguousarray(inputs["p"], dtype=np.float32)
    shared = {
        "ident": np.eye(128, dtype=np.float32),
        "w_in": np.ascontiguousarray(inputs["w_in"][0]),
        "g_attn": np.ascontiguousarray(inputs["attn_norm"][0].reshape(8, 128).T),
        "lru_cw": np.ascontiguousarray(inputs["conv_w"][0][:, 0, :].reshape(4, 4, 128).transpose(2, 1, 0)),
        "lru_vec": np.ascontiguousarray(np.stack([inputs[k][0].reshape(4, 128) for k in
                                                  ("conv_b", "lru_ba", "lru_bx", "lru_lambda", "grp_norm_lru")], 0).transpose(2, 0, 1)),
        "cmp_w1": np.ascontiguousarray(np.stack([inputs[k][0].reshape(16, 128, 256).transpose(1, 0, 2) for k in ("cmp_k_w1", "cmp_v_w1")], 0)),
        "cmp_pe": np.ascontiguousarray(np.stack([inputs[k][0].reshape(16, 128).T for k in ("cmp_k_pe", "cmp_v_pe")], 0)),
        "cmp_w2": np.ascontiguousarray(np.stack([inputs[k][0].reshape(2, 128, 64).transpose(1, 0, 2) for k in ("cmp_k_w2", "cmp_v_w2")], 0)),
        "ga_rep": np.ascontiguousarray(np.broadcast_to(inputs["grp_norm_attn"][0][None, :], (128, 512))).astype(np.float32),
        "w_out": np.ascontiguousarray(inputs["w_out"][0]),
        "peer_wq": np.ascontiguousarray(inputs["peer_wq"][0]),
        "sk_T": np.ascontiguousarray(inputs["peer_subkeys"][0].transpose(0, 2, 1)),
        "peer_u": np.ascontiguousarray(inputs["peer_u"][0]),
        "peer_v": np.ascontiguousarray(inputs["peer_v"][0]),
        "ple_wgate": np.ascontiguousarray(inputs["ple_wgate"][0]),
        "ple_proj": np.ascontiguousarray(inputs["ple_proj"][0]),
        "rep4": np.ascontiguousarray(np.broadcast_to(np.stack([inputs["ffn_norm"][0], inputs["ple_norm"][0], inputs["final_norm"], inputs["ple_bgate"][0]], 0)[None], (128, 4, D))).astype(np.float32),
        "iota128": np.ascontiguousarray(np.broadcast_to(np.arange(128, dtype=np.float32)[None], (128, 128))),
        "iota16": np.ascontiguousarray(np.broadcast_to(np.arange(16, dtype=np.float32)[None], (128, 16))),
        "lru_bda": _blockdiag(inputs["lru_wa"][0]),
        "lru_bdx": _blockdiag(inputs["lru_wx"][0]),
    }
    shared.update(_nsa_consts(np.asarray(inputs["rel_table"], np.float32)))
    in_maps = []
    for c in range(8):
        b, hf = c // 2, c % 2
        m = dict(shared)
        m["xb"] = x[b]
        m["xh"] = np.ascontiguousarray(x[b, hf * TH:(hf + 1) * TH])
        m["ph"] = np.ascontiguousarray(p[0, b, hf * TH:(hf + 1) * TH])
        sel = np.zeros((128, 2), np.float32); sel[:, hf] = 1.0
        m["selc"] = sel
        in_maps.append(m)
    return in_maps


def kernel(**inputs):
    nc = build_program()
    in_maps = _prep_inputs(inputs)
    res = run_bass_kernel_spmd(nc, in_maps, core_ids=list(range(8)))
    outp = np.zeros((4, T, D), np.float32)
    for c in range(8):
        b, hf = c // 2, c % 2
        outp[b, hf * TH:(hf + 1) * TH] = res.results[c]["out"]
    return outp
```

```python
import numpy as np
from contextlib import ExitStack
import concourse.bass as bass
import concourse.mybir as mybir
from concourse.bass_utils import run_bass_kernel_spmd

F32 = mybir.dt.float32
BF16 = mybir.dt.bfloat16
U32 = mybir.dt.uint32
AF = mybir.ActivationFunctionType
ALU = mybir.AluOpType
AX = mybir.AxisListType

T = 4096
D = 1024
NT = T // 128
TH = 2048
IN_COLS = 2328
EPS = 1e-6
NEGM = -30000.0


class Res:
    __slots__ = ("name", "lw", "rd")

    def __init__(self, name=""):
        self.name = name
        self.lw = None
        self.rd = {}


class KB:
    NDMA = 4

    def __init__(self, nc, stack):
        self.nc = nc
        self.issue = {"pe": nc.tensor, "act": nc.scalar, "dve": nc.vector, "pool": nc.gpsimd,
                      "dsp": nc.sync, "dact": nc.scalar, "dpool": nc.gpsimd}
        self.stream = {"pe": "pe", "act": "act", "dve": "dve", "pool": "pool",
                       "dsp": "sp", "dact": "act", "dpool": "pool"}
        self.sems = {}
        self.cnt = {}
        for q in self.issue:
            n = self.NDMA if self.is_dma(q) else 1
            self.sems[q] = [stack.enter_context(nc.semaphore(f"s_{q}{i}")) for i in range(n)]
            self.cnt[q] = 0
        self.waited = {s: {} for s in ("pe", "act", "dve", "pool", "sp")}
        self.ninst = 0
        self._rr = 0

    @staticmethod
    def is_dma(q):
        return q in ("dsp", "dact", "dpool")

    @staticmethod
    def _need(need, dep):
        if dep is None:
            return
        q, c = dep
        if need.get(q, 0) < c:
            need[q] = c

    def _waits(self, st, eng, need, skip_q=None):
        for dq, c in need.items():
            if dq == "pe" and skip_q == "pe":
                continue
            if self.is_dma(dq):
                n = self.NDMA
                for si in range(n):
                    k = (c - 1 - si) // n + 1 if c - 1 >= si else 0
                    if k <= 0:
                        continue
                    key = (dq, si)
                    if self.waited[st].get(key, 0) >= k:
                        continue
                    eng.wait_ge(self.sems[dq][si], 16 * k)
                    self.waited[st][key] = k
            else:
                key = (dq, 0)
                if self.waited[st].get(key, 0) >= c:
                    continue
                eng.wait_ge(self.sems[dq][0], c)
                self.waited[st][key] = c

    def emit(self, q, fn, reads=(), writes=()):
        need = {}
        for r in reads:
            self._need(need, r.lw)
        for w in writes:
            self._need(need, w.lw)
            for rq, rc in w.rd.items():
                self._need(need, (rq, rc))
        st = self.stream[q]
        self._waits(st, self.issue[q], need, skip_q=q)
        inst = fn()
        self.cnt[q] += 1
        c = self.cnt[q]
        if self.is_dma(q):
            inst.then_inc(self.sems[q][(c - 1) % self.NDMA], 16)
        else:
            inst.then_inc(self.sems[q][0], 1)
        for r in reads:
            if r.rd.get(q, 0) < c:
                r.rd[q] = c
        for w in writes:
            w.lw = (q, c)
            w.rd = {}
        self.ninst += 1
        return inst

    def dmaq(self):
        self._rr ^= 1
        return "dsp" if self._rr else "dact"

    def barrier(self):
        need = {q: c for q, c in self.cnt.items() if c > 0}
        for st, eng in (("pe", self.nc.tensor), ("act", self.nc.scalar), ("dve", self.nc.vector),
                        ("pool", self.nc.gpsimd), ("sp", self.nc.sync)):
            self._waits(st, eng, dict(need))

    def drain_all(self):
        need = {q: c for q, c in self.cnt.items() if c > 0}
        self._waits("sp", self.nc.sync, need)


class Scope:
    def __init__(self, kb):
        self.kb = kb
        self.st = ExitStack()

    def __enter__(self):
        self.st.__enter__()
        return self.st

    def __exit__(self, *a):
        if a[0] is None:
            self.kb.barrier()
        return self.st.__exit__(*a)


class Ring:
    def __init__(self, tiles):
        self.tiles = tiles
        self.res = [Res() for _ in tiles]
        self.i = -1

    def next(self):
        self.i = (self.i + 1) % len(self.tiles)
        return self.tiles[self.i], self.res[self.i]


def build_program(dbg=None, phases=("A", "B", "C", "D")):
    nc = bass.Bass("TRN2", target_bir_lowering=False)

    def din(name, shape, dt=F32):
        return nc.dram_tensor(name, list(shape), dt, kind="ExternalInput").ap()

    dbg = dbg or ()

    def dscr(name, shape, dt):
        kind = "ExternalOutput" if name in dbg else "Internal"
        return nc.dram_tensor(name, list(shape), dt, kind=kind).ap()

    xb = din("xb", [T, D])
    xh = din("xh", [TH, D])
    ph = din("ph", [TH, 256])
    selc = din("selc", [128, 2])
    ident = din("ident", [128, 128])
    w_in = din("w_in", [D, IN_COLS])
    g_attn = din("g_attn", [128, 8])
    out = nc.dram_tensor("out", [TH, D], F32, kind="ExternalOutput").ap()
    lru_cw = din("lru_cw", [128, 4, 4])
    lru_vec = din("lru_vec", [128, 5, 4])
    lru_bda = din("lru_bda", [128, 4, 128])
    lru_bdx = din("lru_bdx", [128, 4, 128])

    qT_s = dscr("qT_s", [512, T], BF16)
    kcT_s = dscr("kcT_s", [128, T], BF16)
    vcT_s = dscr("vcT_s", [128, T], BF16)
    ksT_s = dscr("ksT_s", [128, T], BF16)
    kwT_s = dscr("kwT_s", [128, T], BF16)
    vs_s = dscr("vs_s", [T, 128], BF16)
    vw_s = dscr("vw_s", [T, 128], BF16)
    gates_s = dscr("gates_s", [T, 24], F32)
    xrT_s = dscr("xrT_s", [512, T], F32)
    xgT_s = dscr("xgT_s", [512, T], F32)

    cmp_w1 = din("cmp_w1", [2, 128, 16, 256])
    cmp_pe = din("cmp_pe", [2, 128, 16])
    cmp_w2 = din("cmp_w2", [2, 128, 2, 64])
    ovl_ext = din("ovl_ext", [128, 2, 65])
    bc_g = din("bc_g", [8, 128, 5, 512])
    bc_m = din("bc_m", [128, 5, 512])
    bd_g = din("bd_g", [8, 128, 2, 128])
    bd_m = din("bd_m", [128, 3, 128])
    t31_in = din("t31", [128, 8])
    force_in = din("force_c", [128, 32, 64])
    keep_in = din("keep_c", [128, 32, 64])
    erows = din("erows", [64, T])
    ga_in = din("ga_rep", [128, 512])
    w_out_in = din("w_out", [D, D])
    peer_wq = din("peer_wq", [D, 2048])
    sk_T = din("sk_T", [2, 128, 128])
    peer_u = din("peer_u", [16384, D])
    peer_v = din("peer_v", [16384, D])
    ple_wg = din("ple_wgate", [D, D])
    ple_pj = din("ple_proj", [256, D])
    rep4 = din("rep4", [128, 4, D])
    iota16 = din("iota16", [128, 16])
    iota128 = din("iota128", [128, 128])
    H1_s = dscr("H1_s", [TH, D], F32)
    xnT2_s = dscr("xnT2_s", [128, 8, TH], BF16)
    Wt_s = dscr("Wt_s", [TH // 128, 128, 128, 128], BF16)
    mixT_s = dscr("mixT_s", [1024, T], BF16)
    R = {n: Res(n) for n in ("H1_s", "xnT2_s", "Wt_s", "mixT_s", "qT_s", "kcT_s", "vcT_s", "ksT_s", "kwT_s", "vs_s", "vw_s", "gates_s", "xrT_s", "xgT_s")}

    with ExitStack() as top:
        kb = KB(nc, top)
        E = kb.emit

        uniq = [0]

        def sb(st, name, shape, dt):
            uniq[0] += 1
            return st.enter_context(nc.sbuf_tensor(f"sb{uniq[0]}_{name}", list(shape), dt))

        def ps(st, name, shape, dt):
            uniq[0] += 1
            return st.enter_context(nc.psum_tensor(f"ps{uniq[0]}_{name}", list(shape), dt))


        def MM(out_, lhsT, rhs, start, stop, reads, writes):
            return E("pe", lambda: nc.tensor.matmul(out_, lhsT=lhsT, rhs=rhs, start=start, stop=stop), reads, writes)

        def TR(out_, in_, idt, reads, writes):
            return E("pe", lambda: nc.tensor.transpose(out=out_, in_=in_, identity=idt), reads, writes)

        def ACTF(out_, in_, func, reads, writes, **kw):
            return E("act", lambda: nc.scalar.activation(out=out_, in_=in_, func=func, **kw), reads, writes)

        def veng(q):
            return nc.vector if q == "dve" else nc.gpsimd

        def TS(q, out_, in0, s1, s2, op0, op1, reads, writes):
            if op1 is None:
                return E(q, lambda: veng(q).tensor_scalar(out=out_, in0=in0, scalar1=s1, scalar2=None, op0=op0), reads, writes)
            return E(q, lambda: veng(q).tensor_scalar(out=out_, in0=in0, scalar1=s1, scalar2=s2, op0=op0, op1=op1), reads, writes)

        def TT(q, out_, in0, in1, op, reads, writes):
            return E(q, lambda: veng(q).tensor_tensor(out=out_, in0=in0, in1=in1, op=op), reads, writes)

        def STT(out_, in0, scalar, in1, op0, op1, reads, writes, **kw):
            return E("dve", lambda: nc.vector.scalar_tensor_tensor(out=out_, in0=in0, scalar=scalar, in1=in1, op0=op0, op1=op1, **kw), reads, writes)

        def CP(q, out_, in_, reads, writes):
            if q == "act":
                return E("act", lambda: nc.scalar.copy(out=out_, in_=in_), reads, writes)
            return E(q, lambda: veng(q).tensor_copy(out=out_, in_=in_), reads, writes)

        def MSET(q, out_, val, writes):
            return E(q, lambda: veng(q).memset(out_, val), (), writes)

        def DMA(q, out_, in_, reads, writes):
            eng = {"dsp": nc.sync, "dact": nc.scalar, "dpool": nc.gpsimd}[q]
            return E(q, lambda: eng.dma_start(out=out_, in_=in_), reads, writes)

        def dump(name, ap, shape, dt, res):
            if name not in dbg:
                return
            d = nc.dram_tensor(name, list(shape), dt, kind="ExternalOutput").ap()
            DMA("dsp", d, ap, [res] if not isinstance(res, list) else res, [])

        ident_f = sb(top, "ident_f", [128, 128], F32); r_identf = Res()
        ident_b = sb(top, "ident_b", [128, 128], BF16); r_identb = Res()
        E("dsp", lambda: nc.sync.dma_start(out=ident_f[:], in_=ident), writes=[r_identf])
        E("dve", lambda: nc.vector.tensor_copy(out=ident_b[:], in_=ident_f[:]), reads=[r_identf], writes=[r_identb])

        if "A" in phases:
            with Scope(kb) as st:
                Wg = sb(st, "Wg", [128, 8, IN_COLS], BF16); r_Wg = Res()
                gcol = sb(st, "gcol", [128, 8], F32); r_gcol = Res()
                wst = Ring([sb(st, f"wst{i}", [128, IN_COLS], F32) for i in range(2)])
                E("dsp", lambda: nc.sync.dma_start(out=gcol[:], in_=g_attn), writes=[r_gcol])
                for dc in range(8):
                    w_t, w_r = wst.next()
                    E("dsp" if dc % 2 == 0 else "dact",
                      (lambda w_t=w_t, dc=dc: nc.sync.dma_start(out=w_t[:], in_=w_in[dc * 128:(dc + 1) * 128, :])) if dc % 2 == 0 else
                      (lambda w_t=w_t, dc=dc: nc.scalar.dma_start(out=w_t[:], in_=w_in[dc * 128:(dc + 1) * 128, :])),
                      writes=[w_r])
                    eng = "dve" if dc % 2 == 0 else "pool"
                    ve = nc.vector if dc % 2 == 0 else nc.gpsimd
                    E(eng, lambda ve=ve, w_t=w_t, dc=dc: ve.tensor_scalar(out=Wg[:, dc, :], in0=w_t[:], scalar1=gcol[:, dc:dc + 1], scalar2=None, op0=ALU.mult),
                      reads=[w_r, r_gcol], writes=[r_Wg])

                xt_ring = Ring([sb(st, f"xt{i}", [128, 4, D], F32) for i in range(2)])
                xnb_ring = Ring([sb(st, f"xnb{i}", [128, 4, D], BF16) for i in range(2)])
                xnT_ring = Ring([sb(st, f"xnT{i}", [128, 8, 512], BF16) for i in range(2)])
                junk = sb(st, "junkA", [128, D], BF16); r_junk = Res()
                ss_ring = Ring([sb(st, f"ss{i}", [128, 8], F32) for i in range(2)])
                pT_ring = Ring([ps(st, f"pT{i}", [128, 512], BF16) for i in range(2)])
                pacc = Ring([ps(st, f"pacc{i}", [128, 512], F32) for i in range(4)])
                ostf = Ring([sb(st, f"ostf{i}", [128, 512], F32) for i in range(3)])
                ostb = Ring([sb(st, f"ostb{i}", [128, 512], BF16) for i in range(3)])
                osv = Ring([sb(st, f"osv{i}", [128, 256], BF16) for i in range(2)])
                osg = Ring([sb(st, f"osg{i}", [128, 24], F32) for i in range(2)])
                xb_v = xb.rearrange("(n p) d -> p n d", p=128)
                fm = []
                for cc in range(4):
                    fm.append((cc * 128, qT_s[cc * 128:(cc + 1) * 128, :], 0.125, True, R["qT_s"]))
                fm.append((512, kcT_s, 1.0, True, R["kcT_s"]))
                fm.append((640, vcT_s, 1.0, True, R["vcT_s"]))
                fm.append((768, ksT_s, 1.0, True, R["ksT_s"]))
                fm.append((1024, kwT_s, 1.0, True, R["kwT_s"]))
                for cc in range(4):
                    fm.append((1304 + cc * 128, xrT_s[cc * 128:(cc + 1) * 128, :], 1.0, False, R["xrT_s"]))
                for cc in range(4):
                    fm.append((1816 + cc * 128, xgT_s[cc * 128:(cc + 1) * 128, :], 1.0, False, R["xgT_s"]))
                ev = 0
                for tcn in range(8):
                    xt, xt_r = xt_ring.next()
                    E("dsp", lambda xt=xt, tcn=tcn: nc.sync.dma_start(out=xt[:], in_=xb_v[:, tcn * 4:(tcn + 1) * 4, :]), writes=[xt_r])
                    ss, ss_r = ss_ring.next()
                    for n in range(4):
                        E("act", lambda xt=xt, ss=ss, n=n: nc.scalar.activation(out=junk[:], in_=xt[:, n, :], func=AF.Square, accum_out=ss[:, n:n + 1]),
                          reads=[xt_r], writes=[r_junk, ss_r])
                    E("dve", lambda ss=ss: nc.vector.tensor_scalar(out=ss[:, 4:8], in0=ss[:, 0:4], scalar1=1.0 / D, scalar2=EPS, op0=ALU.mult, op1=ALU.add), reads=[ss_r], writes=[ss_r])
                    E("act", lambda ss=ss: nc.scalar.activation(out=ss[:, 4:8], in_=ss[:, 4:8], func=AF.Sqrt), reads=[ss_r], writes=[ss_r])
                    E("dve", lambda ss=ss: nc.vector.reciprocal(out=ss[:, 4:8], in_=ss[:, 4:8]), reads=[ss_r], writes=[ss_r])
                    xnb, xnb_r = xnb_ring.next()
                    for n in range(4):
                        if n % 2 == 0:
                            E("dve", lambda xt=xt, xnb=xnb, ss=ss, n=n: nc.vector.tensor_scalar(out=xnb[:, n, :], in0=xt[:, n, :], scalar1=ss[:, 4 + n:5 + n], scalar2=None, op0=ALU.mult),
                              reads=[xt_r, ss_r], writes=[xnb_r])
                        else:
                            E("pool", lambda xt=xt, xnb=xnb, ss=ss, n=n: nc.gpsimd.tensor_scalar(out=xnb[:, n, :], in0=xt[:, n, :], scalar1=ss[:, 4 + n:5 + n], scalar2=None, op0=ALU.mult),
                              reads=[xt_r, ss_r], writes=[xnb_r])
                    xnT, xnT_r = xnT_ring.next()
                    for dc in range(8):
                        pT, pT_r = pT_ring.next()
                        for n in range(4):
                            E("pe", lambda pT=pT, xnb=xnb, n=n, dc=dc: nc.tensor.transpose(out=pT[:, n * 128:(n + 1) * 128], in_=xnb[:, n, dc * 128:(dc + 1) * 128], identity=ident_b[:]),
                              reads=[xnb_r, r_identb], writes=[pT_r])
                        if dc % 2 == 0:
                            E("act", lambda pT=pT, xnT=xnT, dc=dc: nc.scalar.copy(out=xnT[:, dc, :], in_=pT[:]), reads=[pT_r], writes=[xnT_r])
                        else:
                            E("dve", lambda pT=pT, xnT=xnT, dc=dc: nc.vector.tensor_copy(out=xnT[:, dc, :], in_=pT[:]), reads=[pT_r], writes=[xnT_r])
                    for (c0, dst, scale, isb, dres) in fm:
                        pa, pa_r = pacc.next()
                        for dc in range(8):
                            E("pe", lambda pa=pa, dc=dc, c0=c0, xnT=xnT: nc.tensor.matmul(pa[:], lhsT=Wg[:, dc, c0:c0 + 128], rhs=xnT[:, dc, :], start=(dc == 0), stop=(dc == 7)),
                              reads=[r_Wg, xnT_r], writes=[pa_r])
                        o_t, o_r = (ostb if isb else ostf).next()
                        ev += 1
                        if ev % 2 == 0:
                            E("act", lambda o_t=o_t, pa=pa, scale=scale: nc.scalar.activation(out=o_t[:], in_=pa[:], func=AF.Copy, scale=scale), reads=[pa_r], writes=[o_r])
                        else:
                            E("dve", lambda o_t=o_t, pa=pa, scale=scale: nc.vector.tensor_scalar(out=o_t[:], in0=pa[:], scalar1=scale, scalar2=None, op0=ALU.mult), reads=[pa_r], writes=[o_r])
                        if ev % 2 == 0:
                            E("dsp", lambda o_t=o_t, dst=dst, tcn=tcn: nc.sync.dma_start(out=dst[:, tcn * 512:(tcn + 1) * 512], in_=o_t[:]), reads=[o_r], writes=[dres])
                        else:
                            E("dpool", lambda o_t=o_t, dst=dst, tcn=tcn: nc.gpsimd.dma_start(out=dst[:, tcn * 512:(tcn + 1) * 512], in_=o_t[:]), reads=[o_r], writes=[dres])
                    for n in range(4):
                        t0 = tcn * 512 + n * 128
                        pa, pa_r = pacc.next()
                        for dc in range(8):
                            E("pe", lambda pa=pa, dc=dc, xnT=xnT, n=n: nc.tensor.matmul(pa[:, 0:128], lhsT=xnT[:, dc, n * 128:(n + 1) * 128], rhs=Wg[:, dc, 896:1024], start=(dc == 0), stop=(dc == 7)),
                              reads=[r_Wg, xnT_r], writes=[pa_r])
                        pb, pb_r = pacc.next()
                        for dc in range(8):
                            E("pe", lambda pb=pb, dc=dc, xnT=xnT, n=n: nc.tensor.matmul(pb[:, 0:152], lhsT=xnT[:, dc, n * 128:(n + 1) * 128], rhs=Wg[:, dc, 1152:1304], start=(dc == 0), stop=(dc == 7)),
                              reads=[r_Wg, xnT_r], writes=[pb_r])
                        ov, ov_r = osv.next()
                        og, og_r = osg.next()
                        E("act", lambda ov=ov, pa=pa: nc.scalar.copy(out=ov[:, 0:128], in_=pa[:, 0:128]), reads=[pa_r], writes=[ov_r])
                        E("dve", lambda ov=ov, pb=pb: nc.vector.tensor_copy(out=ov[:, 128:256], in_=pb[:, 0:128]), reads=[pb_r], writes=[ov_r])
                        E("dve", lambda og=og, pb=pb: nc.vector.tensor_copy(out=og[:], in_=pb[:, 128:152]), reads=[pb_r], writes=[og_r])
                        E("dsp", lambda ov=ov, t0=t0: nc.sync.dma_start(out=vs_s[t0:t0 + 128, :], in_=ov[:, 0:128]), reads=[ov_r], writes=[R["vs_s"]])
                        E("dpool", lambda ov=ov, t0=t0: nc.gpsimd.dma_start(out=vw_s[t0:t0 + 128, :], in_=ov[:, 128:256]), reads=[ov_r], writes=[R["vw_s"]])
                        E("dsp", lambda og=og, t0=t0: nc.sync.dma_start(out=gates_s[t0:t0 + 128, :], in_=og[:]), reads=[og_r], writes=[R["gates_s"]])

        if "B" in phases:
            with Scope(kb) as st:
                cw = sb(st, "cw", [128, 4, 4], F32); r_cw = Res()
                lv = sb(st, "lv", [128, 5, 4], F32); r_lv = Res()
                clc = sb(st, "clc", [128, 3, 4], F32); r_clc = Res()
                bdf = sb(st, "bdf", [128, 2, 4, 128], F32); r_bdf = Res()
                bdb = sb(st, "bdb", [128, 2, 4, 128], BF16); r_bdb = Res()
                ones_b = sb(st, "ones_b", [128, 128], BF16); r_ones = Res()
                E("dsp", lambda: nc.sync.dma_start(out=cw[:], in_=lru_cw), writes=[r_cw])
                E("dact", lambda: nc.scalar.dma_start(out=lv[:], in_=lru_vec), writes=[r_lv])
                E("dsp", lambda: nc.sync.dma_start(out=bdf[:, 0], in_=lru_bda), writes=[r_bdf])
                E("dact", lambda: nc.scalar.dma_start(out=bdf[:, 1], in_=lru_bdx), writes=[r_bdf])
                E("dve", lambda: nc.vector.tensor_copy(out=bdb[:], in_=bdf[:]), reads=[r_bdf], writes=[r_bdb])
                E("dve", lambda: nc.vector.memset(ones_b[:], 1.0), writes=[r_ones])
                E("act", lambda: nc.scalar.activation(out=clc[:, 0, :], in_=lv[:, 3, :], func=AF.Exp, scale=-1.0), reads=[r_lv], writes=[r_clc])
                E("act", lambda: nc.scalar.activation(out=clc[:, 0, :], in_=clc[:, 0, :], func=AF.Ln, bias=1.0), reads=[r_clc], writes=[r_clc])
                E("dve", lambda: nc.vector.tensor_scalar(out=clc[:, 1, :], in0=clc[:, 0, :], scalar1=-8.0, scalar2=None, op0=ALU.mult), reads=[r_clc], writes=[r_clc])
                E("dve", lambda: nc.vector.tensor_scalar(out=clc[:, 2, :], in0=clc[:, 0, :], scalar1=-16.0, scalar2=None, op0=ALU.mult), reads=[r_clc], writes=[r_clc])
                L = sb(st, "Lall", [128, 4, T], F32); r_L = Res()
                X = [sb(st, f"lruX{i}", [128, T], F32) for i in range(5)]
                rX = [Res() for _ in range(5)]
                xcb = sb(st, "xcb", [128, T], BF16); r_xcb = Res()
                pg = Ring([ps(st, f"pg{i}", [128, 512], F32) for i in range(4)])
                for cc in range(4):
                    X1, X2, X3, X4, X5 = X
                    r1, r2, r3, r4, r5 = rX
                    for hh in range(2):
                        E("dsp", lambda cc=cc, hh=hh: nc.sync.dma_start(out=X1[:, hh * 2048:(hh + 1) * 2048], in_=xrT_s[cc * 128:(cc + 1) * 128, hh * 2048:(hh + 1) * 2048]), reads=[R["xrT_s"]], writes=[r1])
                        E("dact", lambda cc=cc, hh=hh: nc.scalar.dma_start(out=X3[:, hh * 2048:(hh + 1) * 2048], in_=xgT_s[cc * 128:(cc + 1) * 128, hh * 2048:(hh + 1) * 2048]), reads=[R["xgT_s"]], writes=[r3])
                    E("dve", lambda cc=cc: nc.vector.tensor_scalar(out=X2[:], in0=X1[:], scalar1=cw[:, cc, 3:4], scalar2=lv[:, 0, cc:cc + 1], op0=ALU.mult, op1=ALU.add), reads=[r1, r_cw, r_lv], writes=[r2])
                    for sh in (1, 2, 3):
                        E("dve", lambda cc=cc, sh=sh: nc.vector.scalar_tensor_tensor(out=X2[:, sh:T], in0=X1[:, 0:T - sh], scalar=cw[:, cc, 3 - sh:4 - sh], in1=X2[:, sh:T], op0=ALU.mult, op1=ALU.add), reads=[r1, r2, r_cw], writes=[r2])
                    E("pool", lambda: nc.gpsimd.tensor_copy(out=xcb[:], in_=X2[:]), reads=[r2], writes=[r_xcb])
                    for gi, (Xo, ro, bi) in enumerate(((X4, r4, 1), (X5, r5, 2))):
                        for tcn in range(8):
                            pgt, pg_r = pg.next()
                            E("pe", lambda pgt=pgt, gi=gi, cc=cc, tcn=tcn: nc.tensor.matmul(pgt[:], lhsT=bdb[:, gi, cc, :], rhs=xcb[:, tcn * 512:(tcn + 1) * 512], start=True, stop=True), reads=[r_bdb, r_xcb], writes=[pg_r])
                            E("act", lambda pgt=pgt, Xo=Xo, bi=bi, cc=cc, tcn=tcn: nc.scalar.activation(out=Xo[:, tcn * 512:(tcn + 1) * 512], in_=pgt[:], func=AF.Sigmoid, bias=lv[:, bi, cc:cc + 1]), reads=[pg_r, r_lv], writes=[ro])
                    E("act", lambda cc=cc: nc.scalar.activation(out=X1[:], in_=X4[:], func=AF.Exp, scale=clc[:, 1, cc:cc + 1]), reads=[r4, r_clc], writes=[r1])
                    E("act", lambda cc=cc: nc.scalar.activation(out=X4[:], in_=X4[:], func=AF.Exp, scale=clc[:, 2, cc:cc + 1]), reads=[r4, r_clc], writes=[r4])
                    E("act", lambda: nc.scalar.activation(out=X4[:], in_=X4[:], func=AF.Sqrt, scale=-1.0, bias=1.0), reads=[r4], writes=[r4])
                    E("pool", lambda: nc.gpsimd.tensor_tensor(out=X5[:], in0=X5[:], in1=X2[:], op=ALU.mult), reads=[r5, r2], writes=[r5])
                    E("dve", lambda: nc.vector.tensor_tensor(out=X4[:], in0=X4[:], in1=X5[:], op=ALU.mult), reads=[r4, r5], writes=[r4])
                    E("dve", lambda: nc.vector.tensor_tensor_scan(out=X2[:], data0=X1[:], data1=X4[:], initial=0.0, op0=ALU.mult, op1=ALU.add), reads=[r1, r4], writes=[r2])
                    E("act", lambda: nc.scalar.activation(out=X3[:], in_=X3[:], func=AF.Gelu_apprx_tanh), reads=[r3], writes=[r3])
                    E("pool", lambda cc=cc: nc.gpsimd.tensor_tensor(out=L[:, cc, :], in0=X2[:], in1=X3[:], op=ALU.mult), reads=[r2, r3], writes=[r_L])
                sq = Ring([sb(st, f"lsq{i}", [128, 512], BF16) for i in range(2)])
                rs_ring = Ring([sb(st, f"lrs{i}", [128, 512], F32) for i in range(2)])
                lo = Ring([sb(st, f"lo{i}", [128, 512], BF16) for i in range(3)])
                for tcn in range(8):
                    pgt, pg_r = pg.next()
                    for cc in range(4):
                        sq_t, sq_r = sq.next()
                        E("act", lambda sq_t=sq_t, cc=cc, tcn=tcn: nc.scalar.activation(out=sq_t[:], in_=L[:, cc, tcn * 512:(tcn + 1) * 512], func=AF.Square), reads=[r_L], writes=[sq_r])
                        E("pe", lambda pgt=pgt, sq_t=sq_t, cc=cc: nc.tensor.matmul(pgt[:], lhsT=ones_b[:], rhs=sq_t[:], start=(cc == 0), stop=(cc == 3)), reads=[r_ones, sq_r], writes=[pg_r])
                    rs_t, rs_r = rs_ring.next()
                    E("dve", lambda rs_t=rs_t, pgt=pgt: nc.vector.tensor_scalar(out=rs_t[:], in0=pgt[:], scalar1=1.0 / 512, scalar2=EPS, op0=ALU.mult, op1=ALU.add), reads=[pg_r], writes=[rs_r])
                    E("act", lambda rs_t=rs_t: nc.scalar.activation(out=rs_t[:], in_=rs_t[:], func=AF.Sqrt), reads=[rs_r], writes=[rs_r])
                    E("dve", lambda rs_t=rs_t: nc.vector.reciprocal(out=rs_t[:], in_=rs_t[:]), reads=[rs_r], writes=[rs_r])
                    for cc in range(4):
                        lo_t, lo_r = lo.next()
                        E("dve", lambda lo_t=lo_t, rs_t=rs_t, cc=cc, tcn=tcn: nc.vector.scalar_tensor_tensor(out=lo_t[:], in0=L[:, cc, tcn * 512:(tcn + 1) * 512], scalar=lv[:, 4, cc:cc + 1], in1=rs_t[:], op0=ALU.mult, op1=ALU.mult), reads=[r_L, rs_r, r_lv], writes=[lo_r])
                        E("dsp", lambda lo_t=lo_t, cc=cc, tcn=tcn: nc.sync.dma_start(out=mixT_s[512 + cc * 128:512 + (cc + 1) * 128, tcn * 512:(tcn + 1) * 512], in_=lo_t[:]), reads=[lo_r], writes=[R["mixT_s"]])

        if "C" in phases:
            with Scope(kb) as st:
                Aout = sb(st, "Aout", [128, NT, 512], BF16)
                rA = [Res() for _ in range(NT)]
                sig = sb(st, "sig", [128, NT, 24], F32); r_sig = Res()
                force_t = sb(st, "force_t", [128, NT, 64], F32); r_force = Res()
                keep_t = sb(st, "keep_t", [128, NT, 64], F32); r_keep = Res()
                t31 = sb(st, "t31", [128, 8], F32); r_t31 = Res()
                BD = sb(st, "BD", [128, 8, 3, 128], BF16); r_BD = Res()
                ovl_t = sb(st, "ovl_t", [128, 2, 65], F32); r_ovl = Res()
                ga_t = sb(st, "ga_t", [128, 512], F32); r_ga = Res()
                bcm = sb(st, "bcm", [128, 5, 512], F32); r_bcm = Res()
                DMA("dsp", sig[:], gates_s.rearrange("(n p) c -> p n c", p=128), [R["gates_s"]], [r_sig])
                ACTF(sig[:], sig[:], AF.Sigmoid, [r_sig], [r_sig])
                DMA("dact", force_t[:], force_in, [], [r_force])
                DMA("dsp", keep_t[:], keep_in, [], [r_keep])
                DMA("dact", t31[:], t31_in, [], [r_t31])
                DMA("dsp", ovl_t[:], ovl_ext, [], [r_ovl])
                DMA("dact", ga_t[:], ga_in, [], [r_ga])
                DMA("dsp", bcm[:], bc_m, [], [r_bcm])
                psb = [ps(st, f"pC{i}", [128, 512], F32) for i in range(8)]
                pS = Ring(psb[0:3])
                pO = psb[3:7]; r_pO = [Res() for _ in range(4)]
                pX = Ring(psb[7:8])
                with Scope(kb) as st2:
                    bdg = sb(st2, "bdg", [128, 8, 2, 128], F32); r_bdg = Res()
                    bdm = sb(st2, "bdm", [128, 3, 128], F32); r_bdm = Res()
                    DMA("dsp", bdg[:], bd_g.rearrange("h p j t -> p h j t"), [], [r_bdg])
                    DMA("dact", bdm[:], bd_m, [], [r_bdm])
                    for hg in range(8):
                        for j in range(2):
                            STT(BD[:, hg, j, :], bdg[:, hg, j, :], t31[:, hg:hg + 1], bdm[:, j, :], ALU.subtract, ALU.add, [r_bdg, r_bdm, r_t31], [r_BD])
                        CP("dve", BD[:, hg, 2, :], bdm[:, 2, :], [r_bdm], [r_BD])
                P_ring = Ring([sb(st, f"Pt{i}", [128, 512], BF16) for i in range(5)])
                sm = Ring([sb(st, f"smC{i}", [128, 8], F32) for i in range(8)])
                osb = Ring([sb(st, f"osb{i}", [128, 132], F32) for i in range(8)])

                def finish_tiles(items, ncol, hg, br, first, imp_first=None):
                    sts = [sm.next() for _ in items]
                    for (po, po_r, i, _, _), (s_t, s_r) in zip(items, sts):
                        TS("dve", s_t[:, 0:1], po[:, ncol:ncol + 1], 1e-30, None, ALU.max, None, [po_r], [s_r])
                    for (po, po_r, i, _, _), (s_t, s_r) in zip(items, sts):
                        E("dve", lambda: nc.vector.reciprocal(out=s_t[:, 1:2], in_=s_t[:, 0:1]), [s_r], [s_r])
                    for (po, po_r, i, _, _), (s_t, s_r) in zip(items, sts):
                        TT("dve", s_t[:, 2:3], s_t[:, 1:2], sig[:, i, hg * 3 + br:hg * 3 + br + 1], ALU.mult, [s_r, r_sig], [s_r])
                    for (po, po_r, i, _, _), (s_t, s_r) in zip(items, sts):
                        dst = Aout[:, i, hg * 64:(hg + 1) * 64]
                        if first:
                            TS("dve", dst, po[:, 0:64], s_t[:, 2:3], None, ALU.mult, None, [po_r, s_r], [rA[i]])
                        else:
                            STT(dst, po[:, 0:64], s_t[:, 2:3], dst, ALU.mult, ALU.add, [po_r, s_r, rA[i]], [rA[i]])
                    if imp_first is not None:
                        for (po, po_r, i, imp_t, imp_r), (s_t, s_r) in zip(items, sts):
                            if imp_first:
                                TS("dve", imp_t, po[:, 64:128], s_t[:, 1:2], None, ALU.mult, None, [po_r, s_r], [imp_r])
                            else:
                                STT(imp_t, po[:, 64:128], s_t[:, 1:2], imp_t, ALU.mult, ALU.add, [po_r, s_r, imp_r], [imp_r])

                for k in range(2):
                    with Scope(kb) as stg:
                        KcmpT = sb(stg, "KcmpT", [64, 256], BF16); r_Kc = Res()
                        Vco = sb(stg, "Vco", [128, 2, 129], BF16); r_Vco = Res()
                        with Scope(kb) as stc:
                            w1s = Ring([sb(stc, f"w1s{i}", [128, 8, 256], F32) for i in range(2)])
                            w1b = sb(stc, "w1b", [128, 2, 16, 256], BF16); r_w1b = Res()
                            pes = sb(stc, "pes", [128, 2, 16], F32); r_pes = Res()
                            peb = sb(stc, "peb", [128, 2, 16], BF16); r_peb = Res()
                            w2s = sb(stc, "w2s", [128, 2, 2, 64], F32); r_w2s = Res()
                            w2b = sb(stc, "w2b", [128, 2, 2, 64], BF16); r_w2b = Res()
                            stk = sb(stc, "stk", [128, 2, T], BF16); r_stk = Res()
                            hb = sb(stc, "hb", [128, 4], F32); r_hb = Res()
                            gh = sb(stc, "gh", [128, 2, 2, 256], BF16); r_gh = Res()
                            for kv in range(2):
                                for hh in range(2):
                                    w_t, w_r = w1s.next()
                                    DMA("dsp" if hh == 0 else "dact", w_t[:], cmp_w1[kv, :, hh * 8:(hh + 1) * 8, :], [], [w_r])
                                    CP("pool" if hh == 0 else "dve", w1b[:, kv, hh * 8:(hh + 1) * 8, :], w_t[:], [w_r], [r_w1b])
                                DMA("dsp", pes[:, kv, :], cmp_pe[kv], [], [r_pes])
                                DMA("dact", w2s[:, kv], cmp_w2[kv], [], [r_w2s])
                                src = kcT_s if kv == 0 else vcT_s
                                sres = R["kcT_s"] if kv == 0 else R["vcT_s"]
                                DMA("dsp", stk[0:64, kv, :], src[k * 64:(k + 1) * 64, :], [sres], [r_stk])
                                MSET("pool", stk[64:128, kv, T - 1:T], 0.0, [r_stk])
                                DMA("dact", stk[64:128, kv, 0:T - 1], src[k * 64:(k + 1) * 64, 1:T], [sres], [r_stk])
                            CP("dve", peb[:], pes[:], [r_pes], [r_peb])
                            CP("dve", w2b[:], w2s[:], [r_w2s], [r_w2b])
                            MSET("pool", gh[:], 0.0, [r_gh])
                            for kv in range(2):
                                for hh in range(2):
                                    px, px_r = pX.next()
                                    for m in range(16):
                                        MM(px[:, 0:1], w1b[:, kv, m, hh * 128:(hh + 1) * 128], peb[:, kv, m:m + 1], m == 0, m == 15, [r_w1b, r_peb], [px_r])
                                    CP("dve", hb[:, kv * 2 + hh:kv * 2 + hh + 1], px[:, 0:1], [px_r], [r_hb])
                                    p_s, p_r = pS.next()
                                    for m in range(16):
                                        MM(p_s[:, 0:255], w1b[:, kv, m, hh * 128:(hh + 1) * 128], stk[:, kv, 2 * m:2 * m + 16 * 254 + 1:16], m == 0, m == 15, [r_w1b, r_stk], [p_r])
                                    ACTF(gh[:, kv, hh, 0:255], p_s[:, 0:255], AF.Gelu_apprx_tanh, [p_r, r_hb], [r_gh], bias=hb[:, kv * 2 + hh:kv * 2 + hh + 1])
                            px, px_r = pX.next()
                            for hh in range(2):
                                MM(px[0:64, 0:256], w2b[:, 0, hh, :], gh[:, 0, hh, :], hh == 0, hh == 1, [r_w2b, r_gh], [px_r])
                            CP("dve", KcmpT[:], px[0:64, 0:256], [px_r], [r_Kc])
                            for ct in range(2):
                                px, px_r = pX.next()
                                for hh in range(2):
                                    MM(px[:, 0:64], gh[:, 1, hh, ct * 128:(ct + 1) * 128], w2b[:, 1, hh, :], hh == 0, hh == 1, [r_gh, r_w2b], [px_r])
                                CP("dve", Vco[:, ct, 0:64], px[:, 0:64], [px_r], [r_Vco])
                            CP("pool", Vco[:, :, 64:129], ovl_t[:], [r_ovl], [r_Vco])
                            if k == 0:
                                dump("d_kcmp", KcmpT[:], [64, 256], BF16, r_Kc)
                                dump("d_vco", Vco[:], [128, 2, 129], BF16, r_Vco)
                                dump("d_hb", hb[:], [128, 4], F32, r_hb)
                                dump("d_gh", gh[:], [128, 2, 2, 256], BF16, r_gh)

                        QT = sb(stg, "QT", [128, 4, T], BF16)
                        r_QT = [Res() for _ in range(4)]
                        r_QM = [[Res() for _ in range(NT)] for _ in range(4)]
                        KsT = sb(stg, "KsT", [128, T], BF16); r_KsT = Res()
                        KwT = sb(stg, "KwT", [64, T], BF16); r_KwT = Res()
                        Vs = sb(stg, "Vs", [128, NT, 65], BF16); r_Vs = Res()
                        Vw = sb(stg, "Vw", [128, NT, 65], BF16); r_Vw = Res()
                        imp_acc = sb(stg, "imp_acc", [128, NT, 64], F32)
                        r_imp = [Res() for _ in range(NT)]
                        for g in range(4):
                            hg = 4 * k + g
                            DMA("dsp" if g % 2 == 0 else "dact", QT[0:64, g, :], qT_s[hg * 64:(hg + 1) * 64, :], [R["qT_s"]], [r_QT[g]])
                        DMA("dsp", KsT[0:64, :], ksT_s[k * 64:(k + 1) * 64, :], [R["ksT_s"]], [r_KsT])
                        with Scope(kb) as ste:
                            ers = sb(ste, "ers", [128, T], F32); r_ers = Res()
                            DMA("dact", ers[64:128, :], erows, [], [r_ers])
                            CP("pool", KsT[64:128, :], ers[64:128, :], [r_ers], [r_KsT])
                        DMA("dact", KwT[:], kwT_s[k * 64:(k + 1) * 64, :], [R["kwT_s"]], [r_KwT])
                        DMA("dsp", Vs[:, :, 0:64], vs_s.rearrange("(n p) c -> p n c", p=128)[:, :, k * 64:(k + 1) * 64], [R["vs_s"]], [r_Vs])
                        DMA("dact", Vw[:, :, 0:64], vw_s.rearrange("(n p) c -> p n c", p=128)[:, :, k * 64:(k + 1) * 64], [R["vw_s"]], [r_Vw])
                        MSET("pool", Vs[:, :, 64:65], 1.0, [r_Vs])
                        MSET("pool", Vw[:, :, 64:65], 1.0, [r_Vw])

                        bcs = Ring([sb(stg, f"bcs{i}", [128, 5, 512], F32) for i in range(2)])
                        BC = Ring([sb(stg, f"BCb{i}", [128, 5, 512], BF16) for i in range(2)])
                        bc_cur = {}

                        def cmp_stage1(it):
                            g, tcn, ct, last = it
                            hg = 4 * k + g
                            if tcn == 0 and ct == 0:
                                bs_t, bs_r = bcs.next()
                                DMA("dsp", bs_t[:, 0:3], bc_g[hg, :, 0:3], [], [bs_r])
                                DMA("dact", bs_t[:, 3:5], bc_g[hg, :, 3:5], [], [bs_r])
                                bc_t, bc_r = BC.next()
                                for m in range(5):
                                    STT(bc_t[:, m, :], bs_t[:, m, :], t31[:, hg:hg + 1], bcm[:, m, :], ALU.subtract, ALU.add, [bs_r, r_bcm, r_t31], [bc_r])
                                bc_cur[g] = (bc_t, bc_r)
                            bc_t, bc_r = bc_cur[g]
                            mp = tcn - 4 * ct
                            p_s, p_r = pS.next()
                            MM(p_s[:], KcmpT[:, ct * 128:(ct + 1) * 128], QT[0:64, g, tcn * 512:(tcn + 1) * 512], True, mp >= 5, [r_Kc, r_QT[g]], [p_r])
                            if mp < 5:
                                MM(p_s[:], ident_b[:], bc_t[:, mp, :], False, True, [r_identb, bc_r], [p_r])
                            P_t, P_r = P_ring.next()
                            ACTF(P_t[:], p_s[:], AF.Exp, [p_r, r_t31], [P_r], bias=t31[:, hg:hg + 1])
                            return (P_t, P_r)

                        def cmp_stage2(it, st1):
                            g, tcn, ct, last = it
                            hg = 4 * k + g
                            P_t, P_r = st1
                            for q in range(4):
                                MM(pO[q][:, 0:129], P_t[:, q * 128:(q + 1) * 128], Vco[:, ct, :], ct == 0, last, [P_r, r_Vco], [r_pO[q]])
                            if last:
                                items = []
                                for q in range(4):
                                    i = 4 * tcn + q
                                    o_t, o_r = osb.next()
                                    CP("dve", o_t[:, 0:129], pO[q][:, 0:129], [r_pO[q]], [o_r])
                                    items.append((o_t, o_r, i, imp_acc[:, i, :], r_imp[i]))
                                finish_tiles(items, 128, hg, 0, True, imp_first=(g == 0))

                        its = []
                        for g in range(4):
                            for tcn in range(8):
                                cts = [0] if tcn < 4 else [0, 1]
                                for ct in cts:
                                    its.append((g, tcn, ct, ct == cts[-1]))
                        LAG = 2
                        pend = []
                        for n in range(len(its) + LAG):
                            if n < len(its):
                                pend.append((its[n], cmp_stage1(its[n])))
                            if n >= LAG:
                                it0, st0 = pend.pop(0)
                                cmp_stage2(it0, st0)

                        if k == 0:
                            dump("d_imp", imp_acc[:], [128, NT, 64], F32, r_imp)
                            dump("d_aout_c", Aout[:], [128, NT, 512], BF16, rA)
                        MBr = Ring([sb(stg, f"MB{i}", [128, 128], F32) for i in range(2)])
                        for (mb_t, mb_r) in zip(MBr.tiles, MBr.res):
                            MSET("dve", mb_t[:], 0.0, [mb_r])
                        tk = Ring([sb(stg, f"tk{i}", [128, 2, 64], F32) for i in range(2)])
                        mxr = Ring([sb(stg, f"mx{i}", [128, 16], F32) for i in range(2)])
                        mtr = Ring([sb(stg, f"mtr{i}", [128, 128], BF16) for i in range(2)])
                        for i in range(NT):
                            tk_t, tk_r = tk.next()
                            mx_t, mx_r = mxr.next()
                            TT("dve", tk_t[:, 0, :], imp_acc[:, i, :], keep_t[:, i, :], ALU.mult, [r_imp[i], r_keep], [tk_r])
                            TT("dve", tk_t[:, 0, :], tk_t[:, 0, :], force_t[:, i, :], ALU.add, [tk_r, r_force], [tk_r])
                            E("dve", lambda: nc.vector.max(out=mx_t[:, 0:8], in_=tk_t[:, 0, :]), [tk_r], [mx_r])
                            E("dve", lambda: nc.vector.match_replace(out=tk_t[:, 1, :], in_to_replace=mx_t[:, 0:8], in_values=tk_t[:, 0, :], imm_value=-1e30), [tk_r, mx_r], [tk_r])
                            E("dve", lambda: nc.vector.max(out=mx_t[:, 8:16], in_=tk_t[:, 1, :]), [tk_r], [mx_r])
                            mb_t, mb_r = MBr.next()
                            TS("dve", mb_t[:, 64:128], tk_t[:, 0, :], mx_t[:, 15:16], None, ALU.is_ge, None, [tk_r, mx_r], [mb_r])
                            TS("dve", mb_t[:, 64:128], mb_t[:, 64:128], 1.0, -NEGM, ALU.subtract, ALU.mult, [mb_r], [mb_r])
                            px, px_r = pX.next()
                            TR(px[:, 0:128], mb_t[:], ident_f[:], [mb_r, r_identf], [px_r])
                            mt_t, mt_r = mtr.next()
                            CP("act", mt_t[64:128, :], px[64:128, 0:128], [px_r], [mt_r])
                            for g in range(4):
                                CP("pool" if g % 2 == 0 else "dve", QT[64:128, g, i * 128:(i + 1) * 128], mt_t[64:128, :], [mt_r], [r_QM[g][i]])

                        if k == 0:
                            dump("d_qt0", QT[:, 0, :], [128, T], BF16, r_QT + [x for l in r_QM for x in l])
                        def sel_stage1(it):
                            g, br, tcn, j = it
                            hg = 4 * k + g
                            qa = max(0, j - 4 * tcn)
                            qb = 3 if br == 1 else min(3, j + 4 - 4 * tcn)
                            c0, c1 = qa * 128, (qb + 1) * 128
                            t0 = tcn * 512
                            adds = []
                            for q in range(qa, qb + 1):
                                dlt = 4 * tcn + q - j
                                if dlt == 0:
                                    adds.append((q, 0))
                                elif dlt == 1:
                                    adds.append((q, 1))
                                elif dlt == 4 and br == 2:
                                    adds.append((q, 2))
                            p_s, p_r = pS.next()
                            if br == 1:
                                rd = [r_KsT, r_QT[g]] + [r_QM[g][4 * tcn + q] for q in range(qa, qb + 1)]
                                MM(p_s[:, c0:c1], KsT[:, j * 128:(j + 1) * 128], QT[:, g, t0 + c0:t0 + c1], True, len(adds) == 0, rd, [p_r])
                            else:
                                MM(p_s[:, c0:c1], KwT[:, j * 128:(j + 1) * 128], QT[0:64, g, t0 + c0:t0 + c1], True, len(adds) == 0, [r_KwT, r_QT[g]], [p_r])
                            for ai, (q, ty) in enumerate(adds):
                                MM(p_s[:, q * 128:(q + 1) * 128], ident_b[:], BD[:, hg, ty, :], False, ai == len(adds) - 1, [r_identb, r_BD], [p_r])
                            P_t, P_r = P_ring.next()
                            ACTF(P_t[:, c0:c1], p_s[:, c0:c1], AF.Exp, [p_r, r_t31], [P_r], bias=t31[:, hg:hg + 1])
                            return (P_t, P_r, qa, qb)

                        def sel_stage2(it, st1):
                            g, br, tcn, j = it
                            hg = 4 * k + g
                            P_t, P_r, qa, qb = st1
                            Vx, r_Vx = (Vs, r_Vs) if br == 1 else (Vw, r_Vw)
                            for q in range(qa, qb + 1):
                                i = 4 * tcn + q
                                first_j = 0 if br == 1 else max(0, i - 4)
                                MM(pO[q][:, 0:65], P_t[:, q * 128:(q + 1) * 128], Vx[:, j, :], j == first_j, j == i, [P_r, r_Vx], [r_pO[q]])
                            if j == 4 * tcn + 3:
                                items = []
                                for q in range(4):
                                    o_t, o_r = osb.next()
                                    CP("dve", o_t[:, 0:65], pO[q][:, 0:65], [r_pO[q]], [o_r])
                                    items.append((o_t, o_r, 4 * tcn + q, None, None))
                                finish_tiles(items, 64, hg, br, False)

                        its = []
                        for g in range(4):
                            for br in (1, 2):
                                for tcn in range(8):
                                    j_lo = 0 if br == 1 else max(0, 4 * tcn - 4)
                                    for j in range(j_lo, 4 * tcn + 4):
                                        its.append((g, br, tcn, j))
                        LAG = 2
                        pend = []
                        for n in range(len(its) + LAG):
                            if n < len(its):
                                pend.append((its[n], sel_stage1(its[n])))
                            if n >= LAG:
                                it0, st0 = pend.pop(0)
                                sel_stage2(it0, st0)

                dump("d_aout", Aout[:], [128, NT, 512], BF16, rA)
                with Scope(kb) as stn:
                    junkC = sb(stn, "junkC", [128, 512], BF16); r_junkC = Res()
                    an = Ring([sb(stn, f"an{i}", [128, 512], BF16) for i in range(2)])
                    af = Ring([sb(stn, f"af{i}", [128, 512], F32) for i in range(2)])
                    ao = Ring([sb(stn, f"ao{i}", [128, 512], BF16) for i in range(2)])
                    pTb = Ring([ps(stn, f"pTC{i}", [128, 512], BF16) for i in range(2)]) if False else None
                    for i in range(NT):
                        s_t, s_r = sm.next()
                        ACTF(junkC[:], Aout[:, i, :], AF.Square, [rA[i]], [r_junkC, s_r], accum_out=s_t[:, 0:1])
                        TS("dve", s_t[:, 1:2], s_t[:, 0:1], 1.0 / 512, EPS, ALU.mult, ALU.add, [s_r], [s_r])
                        ACTF(s_t[:, 1:2], s_t[:, 1:2], AF.Sqrt, [s_r], [s_r])
                        E("dve", lambda: nc.vector.reciprocal(out=s_t[:, 2:3], in_=s_t[:, 1:2]), [s_r], [s_r])
                        af_t, af_r = af.next()
                        STT(af_t[:], Aout[:, i, :], s_t[:, 2:3], ga_t[:], ALU.mult, ALU.mult, [rA[i], s_r, r_ga], [af_r])
                        px, px_r = pX.next()
                        for fc in range(4):
                            TR(px[:, fc * 128:(fc + 1) * 128], af_t[:, fc * 128:(fc + 1) * 128], ident_f[:], [af_r, r_identf], [px_r])
                        ao_t, ao_r = ao.next()
                        CP("act", ao_t[:], px[:], [px_r], [ao_r])
                        DMA("dsp" if i % 2 == 0 else "dpool", mixT_s[0:512, i * 128:(i + 1) * 128].rearrange("(f p) t -> p f t", p=128),
                            ao_t[:].rearrange("p (f t) -> p f t", f=4), [ao_r], [R["mixT_s"]])

        if "D" in phases or "D1" in phases:
            NTL = TH // 128
            with Scope(kb) as st:
                Wo = sb(st, "Wo", [128, 8, D], BF16); r_Wo = Res()
                Wq = sb(st, "Wq", [128, 8, 2048], BF16); r_Wq = Res()
                skb = sb(st, "skb", [128, 2, 128], BF16); r_skb = Res()
                repf = sb(st, "repf", [128, D], F32); r_rep = Res()
                io16 = sb(st, "io16", [128, 16], F32); r_io = Res()
                io128 = sb(st, "io128", [128, 128], F32); r_io128 = Res()
                selt = sb(st, "selt", [128, 2], F32); r_sel = Res()
                DMA("dsp", repf[:], rep4[:, 0, :], [], [r_rep])
                DMA("dact", io16[:], iota16, [], [r_io])
                DMA("dact", io128[:], iota128, [], [r_io128])
                DMA("dact", selt[:], selc, [], [r_sel])
                with Scope(kb) as stw:
                    wst = Ring([sb(stw, f"wstD{i}", [128, 2048], F32) for i in range(3)])
                    n = 0
                    for (src, dstw, dres, ncol, nch) in ((w_out_in, Wo, r_Wo, D, 8), (peer_wq, Wq, r_Wq, 2048, 8)):
                        for dc in range(nch):
                            w_t, w_r = wst.next()
                            n += 1
                            DMA("dsp" if n % 2 == 0 else "dact", w_t[:, 0:ncol], src[dc * 128:(dc + 1) * 128, :], [], [w_r])
                            CP("dve" if n % 2 == 0 else "pool", dstw[:, dc, :], w_t[:, 0:ncol], [w_r], [dres])
                    w_t, w_r = wst.next()
                    DMA("dsp", w_t[:, 0:256].rearrange("p (a k) -> p a k", a=2), sk_T.rearrange("a p k -> p a k"), [], [w_r])
                    CP("dve", skb[:], w_t[:, 0:256].rearrange("p (a k) -> p a k", a=2), [w_r], [r_skb])

                pacc = Ring([ps(st, f"pD{i}", [128, 512], F32) for i in range(4)])
                pw_ring = Ring([ps(st, f"pDw{i}", [128, 512], F32) for i in range(2)])
                ptb = Ring([ps(st, f"pDb{i}", [128, 1024], BF16) for i in range(2)])
                mst = Ring([sb(st, f"mst{i}", [128, 8, 2, 128], BF16) for i in range(1)])
                mixh_ring = Ring([sb(st, f"mixh{i}", [128, 8, 128], BF16) for i in range(1)])
                xh_ring = Ring([sb(st, f"xhD{i}", [128, D], F32) for i in range(2)])
                H_ring = Ring([sb(st, f"HD{i}", [128, D], F32) for i in range(2)])
                xng_ring = Ring([sb(st, f"xng{i}", [128, D], F32) for i in range(1)])
                xnb_ring = Ring([sb(st, f"xnbD{i}", [128, D], BF16) for i in range(1)])
                xT_ring = Ring([sb(st, f"xTD{i}", [128, 8, 128], BF16) for i in range(2)])
                qTb = sb(st, "qTb", [128, 16, 128], BF16); r_qTb = Res()
                Ssc_ring = Ring([sb(st, f"Ssc{i}", [128, 16, 128], F32) for i in range(2)])
                Swk = sb(st, "Swk", [128, 8, 128], F32)
                rv = [Res() for _ in range(16)]; rv2 = [Res() for _ in range(16)]; ri = [Res() for _ in range(16)]; ri2 = [Res() for _ in range(16)]; rw = [Res() for _ in range(16)]
                v16 = sb(st, "v16", [128, 16, 16], F32); r_v16 = Res()
                i16 = sb(st, "i16", [128, 16, 16], U32); r_i16 = Res()
                i16f = sb(st, "i16f", [128, 16, 16], F32); r_i16f = Res()
                cand = sb(st, "cand", [128, 8, 256], F32); r_cand = Res()
                cwk = sb(st, "cwk", [128, 8, 256], F32)
                sc16 = sb(st, "sc16", [128, 8, 16], F32); r_sc = Res()
                ci16 = sb(st, "ci16", [128, 8, 16], U32); r_ci = Res()
                ab_u = sb(st, "ab_u", [128, 2, 8, 16], U32); r_abu = Res()
                ab_f = sb(st, "ab_f", [128, 2, 8, 16], F32); r_abf = Res()
                eq = sb(st, "eq", [128, 8, 16, 16], F32); r_eq = Res()
                isel_ring = Ring([sb(st, f"isel{i}", [128, 3, 8, 16], F32) for i in range(2)])
                gz = sb(st, "gz", [128, 16], F32); r_gz = Res()
                junkB = sb(st, "junkDb", [128, D], BF16); r_junkB = Res()
                smD = Ring([sb(st, f"smD{i}", [128, 8], F32) for i in range(4)])
                ijgT_ring = Ring([sb(st, f"ijgT{i}", [128, 3, 128], F32) for i in range(2)])
                OI = Ring([sb(st, f"OI{i}", [128, 16, 128], BF16) for i in range(2)])
                OJ = Ring([sb(st, f"OJ{i}", [128, 16, 128], BF16) for i in range(2)])
                OJf = Ring([sb(st, f"OJf{i}", [128, 16, 128], BF16) for i in range(2)])
                Wst = sb(st, "Wst", [128, 128, 128], BF16); r_Wst = Res()

                def rms_scaled(src, src_r, gain, gain_r, dstf, dstf_r):
                    s_t, s_r = smD.next()
                    ACTF(junkB[:], src, AF.Square, [src_r], [r_junkB, s_r], accum_out=s_t[:, 0:1])
                    TS("dve", s_t[:, 1:2], s_t[:, 0:1], 1.0 / D, EPS, ALU.mult, ALU.add, [s_r], [s_r])
                    ACTF(s_t[:, 1:2], s_t[:, 1:2], AF.Sqrt, [s_r], [s_r])
                    E("dve", lambda: nc.vector.reciprocal(out=s_t[:, 2:3], in_=s_t[:, 1:2]), [s_r], [s_r])
                    STT(dstf, src, s_t[:, 2:3], gain, ALU.mult, ALU.mult, [src_r, s_r, gain_r], [dstf_r])

                def rms_scaled_g(src, src_r, gain, gain_r, dstf, dstf_r):
                    s_t, s_r = smD.next()
                    ACTF(junkB[:], src, AF.Square, [src_r], [r_junkB, s_r], accum_out=s_t[:, 0:1])
                    yield
                    TS("dve", s_t[:, 1:2], s_t[:, 0:1], 1.0 / D, EPS, ALU.mult, ALU.add, [s_r], [s_r])
                    ACTF(s_t[:, 1:2], s_t[:, 1:2], AF.Sqrt, [s_r], [s_r])
                    yield
                    E("dve", lambda: nc.vector.reciprocal(out=s_t[:, 2:3], in_=s_t[:, 1:2]), [s_r], [s_r])
                    STT(dstf, src, s_t[:, 2:3], gain, ALU.mult, ALU.mult, [src_r, s_r, gain_r], [dstf_r])

                def transpose8(srcb, srcb_r, dstT, dstT_r, nblk=8):
                    pt, pt_r = ptb.next()
                    for dc in range(nblk):
                        TR(pt[:, dc * 128:(dc + 1) * 128], srcb[:, dc * 128:(dc + 1) * 128], ident_b[:], [srcb_r, r_identb], [pt_r])
                    CP("act", dstT.rearrange("p a t -> p (a t)"), pt[:, 0:nblk * 128], [pt_r], [dstT_r])

                def S1(it):
                    tsl = slice(it * 128, (it + 1) * 128)
                    xh_t, xh_r = xh_ring.next()
                    DMA("dsp", xh_t[:], xh[tsl, :], [], [xh_r])
                    H, H_r = H_ring.next()
                    m_t, m_r = mst.next()
                    for a in range(2):
                        DMA("dsp" if a == 0 else "dact", m_t[:, :, a, :], mixT_s[:, a * TH + it * 128:a * TH + (it + 1) * 128].rearrange("(f p) t -> p f t", p=128), [R["mixT_s"]], [m_r])
                    mixh, r_mixh = mixh_ring.next()
                    ACTF(mixh[:], m_t[:, :, 0, :], AF.Copy, [m_r, r_sel], [r_mixh], scale=selt[:, 0:1])
                    STT(mixh[:], m_t[:, :, 1, :], selt[:, 1:2], mixh[:], ALU.mult, ALU.add, [m_r, r_sel, r_mixh], [r_mixh])
                    yield
                    for ch in range(2):
                        pa, pa_r = pacc.next()
                        for fc in range(8):
                            MM(pa[:], mixh[:, fc, :], Wo[:, fc, ch * 512:(ch + 1) * 512], fc == 0, fc == 7, [r_mixh, r_Wo], [pa_r])
                        TT("dve", H[:, ch * 512:(ch + 1) * 512], pa[:], xh_t[:, ch * 512:(ch + 1) * 512], ALU.add, [pa_r, xh_r], [H_r])
                        yield
                    DMA("dpool", H1_s[tsl, :], H[:], [H_r], [R["H1_s"]])
                    xng, xng_r = xng_ring.next()
                    yield from rms_scaled_g(H[:], H_r, repf[:], r_rep, xng[:], xng_r)
                    yield
                    xnb, xnb_r = xnb_ring.next()
                    CP("pool", xnb[:], xng[:], [xng_r], [xnb_r])
                    yield
                    xT, xT_r = xT_ring.next()
                    transpose8(xnb, xnb_r, xT[:], xT_r)
                    yield
                    DMA("dact", xnT2_s[:, :, tsl], xT[:], [xT_r], [R["xnT2_s"]])
                    for grp in range(4):
                        pa, pa_r = pacc.next()
                        for j in range(4):
                            hp = grp * 4 + j
                            for dc in range(8):
                                MM(pa[:, j * 128:(j + 1) * 128], Wq[:, dc, hp * 128:(hp + 1) * 128], xT[:, dc, :], dc == 0, dc == 7, [r_Wq, xT_r], [pa_r])
                        CP("act", qTb[:, grp * 4:(grp + 1) * 4, :].rearrange("p a t -> p (a t)"), pa[:], [pa_r], [r_qTb])
                        yield
                    Ssc, r_S = Ssc_ring.next()
                    for grp in range(4):
                        pa, pa_r = pacc.next()
                        for j in range(4):
                            hp = grp * 4 + j
                            MM(pa[:, j * 128:(j + 1) * 128], qTb[:, hp, :], skb[:, hp % 2, :], True, True, [r_qTb, r_skb], [pa_r])
                        CP("act", Ssc[:, grp * 4:(grp + 1) * 4, :].rearrange("p a t -> p (a t)"), pa[:], [pa_r], [r_S])
                        yield
                    return (Ssc, r_S)

                def S2(it, st1):
                    Ssc, r_S = st1
                    for g0 in (0, 8):
                        hps = range(g0, g0 + 8)
                        for hp in hps:
                            E("dve", lambda: nc.vector.max(out=v16[:, hp, 0:8], in_=Ssc[:, hp, :]), [r_S], [rv[hp]])
                        yield
                        for hp in hps:
                            E("dve", lambda: nc.vector.max_index(out=i16[:, hp, 0:8], in_max=v16[:, hp, 0:8], in_values=Ssc[:, hp, :]), [r_S, rv[hp]], [ri[hp]])
                        yield
                        for hp in hps:
                            E("dve", lambda: nc.vector.match_replace(out=Swk[:, hp - g0, :], in_to_replace=v16[:, hp, 0:8], in_values=Ssc[:, hp, :], imm_value=-1e30), [r_S, rv[hp]], [rw[hp - g0]])
                        yield
                        for hp in hps:
                            E("dve", lambda: nc.vector.max(out=v16[:, hp, 8:16], in_=Swk[:, hp - g0, :]), [rw[hp - g0]], [rv2[hp]])
                        yield
                        for hp in hps:
                            E("dve", lambda: nc.vector.max_index(out=i16[:, hp, 8:16], in_max=v16[:, hp, 8:16], in_values=Swk[:, hp - g0, :]), [rw[hp - g0], rv2[hp]], [ri2[hp]])
                        yield
                    r_i16 = Res()
                    E("dve", lambda: nc.vector.tensor_copy(out=i16f[:], in_=i16[:]), ri + ri2, [r_i16f, r_i16])
                    v4 = v16[:].rearrange("p (h two) k -> p h two k", two=2)
                    in0 = v4[:, :, 0, :].rearrange("p h (a o) -> p h a o", o=1).to_broadcast([128, 8, 16, 16])
                    in1 = v4[:, :, 1, :].rearrange("p h (o b) -> p h o b", o=1).to_broadcast([128, 8, 16, 16])
                    TT("dve", cand[:].rearrange("p h (a b) -> p h a b", a=16), in0, in1, ALU.add, rv + rv2, [r_cand])
                    for h in range(8):
                        E("dve", lambda: nc.vector.max(out=sc16[:, h, 0:8], in_=cand[:, h, :]), [r_cand], [rv[h]])
                    yield
                    for h in range(8):
                        E("dve", lambda: nc.vector.max_index(out=ci16[:, h, 0:8], in_max=sc16[:, h, 0:8], in_values=cand[:, h, :]), [r_cand, rv[h]], [ri[h]])
                    yield
                    for h in range(8):
                        E("dve", lambda: nc.vector.match_replace(out=cwk[:, h, :], in_to_replace=sc16[:, h, 0:8], in_values=cand[:, h, :], imm_value=-1e30), [r_cand, rv[h]], [rw[h]])
                    yield
                    for h in range(8):
                        E("dve", lambda: nc.vector.max(out=sc16[:, h, 8:16], in_=cwk[:, h, :]), [rw[h]], [rv2[h]])
                    yield
                    for h in range(8):
                        E("dve", lambda: nc.vector.max_index(out=ci16[:, h, 8:16], in_max=sc16[:, h, 8:16], in_values=cwk[:, h, :]), [rw[h], rv2[h]], [ri2[h]])
                    yield
                    r_sc = Res(); r_ci = Res()
                    E("dve", lambda: nc.vector.tensor_single_scalar(out=ab_u[:, 0], in_=ci16[:], scalar=4, op=ALU.logical_shift_right), ri[:8] + ri2[:8] + rv[:8] + rv2[:8], [r_abu, r_sc, r_ci])
                    E("dve", lambda: nc.vector.tensor_single_scalar(out=ab_u[:, 1], in_=ci16[:], scalar=15, op=ALU.bitwise_and), [r_ci], [r_abu])
                    CP("dve", ab_f[:], ab_u[:], [r_abu], [r_abf])
                    isel, r_isel = isel_ring.next()
                    i4 = i16f[:].rearrange("p (h two) k -> p h two k", two=2)
                    for w in range(2):
                        a_b = ab_f[:, w].rearrange("p h (k o) -> p h k o", o=1).to_broadcast([128, 8, 16, 16])
                        io_b = io16[:].rearrange("p (o q a) -> p o q a", o=1, q=1).to_broadcast([128, 8, 16, 16])
                        TT("dve", eq[:], a_b, io_b, ALU.is_equal, [r_abf, r_io], [r_eq])
                        iv_b = i4[:, :, w, :].rearrange("p h (o a) -> p h o a", o=1).to_broadcast([128, 8, 16, 16])
                        TT("dve", eq[:], eq[:], iv_b, ALU.mult, [r_eq, r_i16f], [r_eq])
                        E("dve", lambda: nc.vector.tensor_reduce(out=isel[:, w], in_=eq[:], axis=AX.X, op=ALU.add), [r_eq], [r_isel])
                        yield
                    TT("dve", isel[:, 2], sc16[:], sc16[:, :, 0:1].to_broadcast([128, 8, 16]), ALU.subtract, [r_sc], [r_isel])
                    ACTF(isel[:, 2], isel[:, 2], AF.Exp, [r_isel], [r_isel])
                    E("dve", lambda: nc.vector.tensor_reduce(out=gz[:, 0:8], in_=isel[:, 2], axis=AX.X, op=ALU.add), [r_isel], [r_gz])
                    E("dve", lambda: nc.vector.reciprocal(out=gz[:, 8:16], in_=gz[:, 0:8]), [r_gz], [r_gz])
                    TT("dve", isel[:, 2], isel[:, 2], gz[:, 8:16].rearrange("p (h o) -> p h o", o=1).to_broadcast([128, 8, 16]), ALU.mult, [r_isel, r_gz], [r_isel])
                    pa, pa_r = pacc.next()
                    for w in range(3):
                        TR(pa[:, w * 128:(w + 1) * 128], isel[:, w].rearrange("p h k -> p (h k)"), ident_f[:], [r_isel, r_identf], [pa_r])
                    ijgT, r_ijgT = ijgT_ring.next()
                    CP("act", ijgT[:].rearrange("p a t -> p (a t)"), pa[:, 0:384], [pa_r], [r_ijgT])
                    return (ijgT, r_ijgT)

                def S3(it, st2):
                    ijgT, r_ijgT = st2
                    TB = 16
                    for tb in range(128 // TB):
                        t0 = tb * TB
                        oi, oi_r = OI.next()
                        oj, oj_r = OJ.next()
                        io_b = io128[:].rearrange("p (o i) -> p o i", o=1).to_broadcast([128, TB, 128])

                        def colb(w):
                            return ijgT[:, w, t0:t0 + TB].rearrange("p (t o) -> p t o", o=1).to_broadcast([128, TB, 128])
                        ojf, ojf_r = OJf.next()
                        TT("dve", oi[:], io_b, colb(0), ALU.is_equal, [r_io128, r_ijgT], [oi_r])
                        TT("dve", ojf[:], io_b, colb(1), ALU.is_equal, [r_io128, r_ijgT], [ojf_r])
                        TT("pool", oj[:], ojf[:], colb(2), ALU.mult, [ojf_r, r_ijgT], [oj_r])
                        for tq in range(TB // 4):
                            pw, pw_r = pw_ring.next()
                            for u in range(4):
                                MM(pw[:, u:512:4], oj[:, tq * 4 + u, :], oi[:, tq * 4 + u, :], True, True, [oj_r, oi_r], [pw_r])
                            tg = t0 + tq * 4
                            CP("act", Wst[:, :, tg:tg + 4], pw[:].rearrange("p (i t) -> p i t", t=4), [pw_r], [r_Wst])
                            yield
                    DMA("dsp" if it % 2 == 0 else "dact", Wt_s[it], Wst[:], [r_Wst], [R["Wt_s"]])

                def drive(gens):
                    res = [None] * len(gens)
                    live = list(range(len(gens)))
                    while live:
                        for gi in list(live):
                            try:
                                next(gens[gi])
                            except StopIteration as e:
                                res[gi] = e.value
                                live.remove(gi)
                    return res

                st1s, st2s = {}, {}
                for n in range(NTL + 2):
                    gens, tags = [], []
                    if n < NTL:
                        gens.append(S1(n)); tags.append(("s1", n))
                    if n >= 2:
                        gens.append(S3(n - 2, st2s.pop(n - 2))); tags.append(("s3", n - 2))
                    if 1 <= n <= NTL:
                        gens.append(S2(n - 1, st1s.pop(n - 1))); tags.append(("s2", n - 1))
                    for (tg_, tn), rv_ in zip(tags, drive(gens)):
                        if tg_ == "s1":
                            st1s[tn] = rv_
                        elif tg_ == "s2":
                            st2s[tn] = rv_

            with Scope(kb) as st:
                Yacc = sb(st, "Yacc", [128, NTL, D], F32)
                rY = [Res() for _ in range(NTL)]
                H1v = H1_s.rearrange("(n p) d -> p n d", p=128)
                for n4 in range(4):
                    DMA("dsp" if n4 % 2 == 0 else "dact", Yacc[:, n4 * 4:(n4 + 1) * 4, :], H1v[:, n4 * 4:(n4 + 1) * 4, :], [R["H1_s"]], rY[n4 * 4:(n4 + 1) * 4])
                p1 = Ring([ps(st, f"pE1{i}", [128, 512], F32) for i in range(3)])
                p2 = Ring([ps(st, f"pE2{i}", [128, 512], F32) for i in range(3)])
                ptb2 = Ring([ps(st, f"pEb{i}", [128, 1024], BF16) for i in range(2)])
                with Scope(kb) as st2:
                  if "D" in phases or "D2" in phases:
                    xnTa = sb(st2, "xnTa", [128, 8, TH], BF16); r_xnTa = Res()
                    for dc in range(8):
                        DMA("dsp" if dc % 2 == 0 else "dact", xnTa[:, dc, :], xnT2_s[:, dc, :], [R["xnT2_s"]], [r_xnTa])
                    IB = 8
                    ust = Ring([sb(st2, f"ust{i}", [128, D], F32) for i in range(2)])
                    vst = Ring([sb(st2, f"vst{i}", [128, D], F32) for i in range(2)])
                    ub = Ring([sb(st2, f"ub{i}", [128, D], BF16) for i in range(2)])
                    uT = Ring([sb(st2, f"uT{i}", [128, 8, 128], BF16) for i in range(2)])
                    Vb = sb(st2, "Vb", [128, IB, D], BF16); r_Vb = [Res() for _ in range(IB)]
                    WA = sb(st2, "WA", [128, IB, TH], BF16); r_WA = [Res() for _ in range(IB)]
                    wt = Ring([sb(st2, f"wt{i}", [128, TH], BF16) for i in range(3)])
                    gl = Ring([sb(st2, f"gl{i}", [128, 512], BF16) for i in range(3)])
                    for ib0 in range(0, 128, IB):
                        for ib in range(IB):
                            i = ib0 + ib
                            u_t, u_r = ust.next()
                            v_t, v_r = vst.next()
                            w_t, w_r = wt.next()
                            DMA("dsp", u_t[:], peer_u[i * 128:(i + 1) * 128, :], [], [u_r])
                            DMA("dact", v_t[:], peer_v[i * 128:(i + 1) * 128, :], [], [v_r])
                            for hw in range(2):
                                DMA("dpool" if hw == 0 else ("dsp" if i % 2 == 0 else "dact"), w_t[:, hw * 1024:(hw + 1) * 1024].rearrange("p (n t) -> p n t", t=128),
                                    Wt_s[hw * 8:(hw + 1) * 8, :, i, :].rearrange("n j t -> j n t"), [R["Wt_s"]], [w_r])
                            ub_t, ub_r = ub.next()
                            CP("pool", ub_t[:], u_t[:], [u_r], [ub_r])
                            CP("pool", Vb[:, ib, :], v_t[:], [v_r], [r_Vb[ib]])
                            pt, pt_r = ptb2.next()
                            for dc in range(8):
                                TR(pt[:, dc * 128:(dc + 1) * 128], ub_t[:, dc * 128:(dc + 1) * 128], ident_b[:], [ub_r, r_identb], [pt_r])
                            uT_t, uT_r = uT.next()
                            CP("act", uT_t[:].rearrange("p a t -> p (a t)"), pt[:], [pt_r], [uT_r])
                            for tc4 in range(4):
                                pa, pa_r = p1.next()
                                for dc in range(8):
                                    MM(pa[:], uT_t[:, dc, :], xnTa[:, dc, tc4 * 512:(tc4 + 1) * 512], dc == 0, dc == 7, [uT_r, r_xnTa], [pa_r])
                                g_t, g_r = gl.next()
                                ACTF(g_t[:], pa[:], AF.Gelu_apprx_tanh, [pa_r], [g_r])
                                TT("dve", WA[:, ib, tc4 * 512:(tc4 + 1) * 512], g_t[:], w_t[:, tc4 * 512:(tc4 + 1) * 512], ALU.mult, [g_r, w_r], [r_WA[ib]])
                        for tt in range(NTL):
                            for ch in range(2):
                                pb, pb_r = p2.next()
                                for ib in range(IB):
                                    MM(pb[:], WA[:, ib, tt * 128:(tt + 1) * 128], Vb[:, ib, ch * 512:(ch + 1) * 512], ib == 0, ib == IB - 1, [r_WA[ib], r_Vb[ib]], [pb_r])
                                TT("dve", Yacc[:, tt, ch * 512:(ch + 1) * 512], Yacc[:, tt, ch * 512:(ch + 1) * 512], pb[:], ALU.add, [rY[tt], pb_r], [rY[tt]])


                with Scope(kb) as st3:
                    Wgt = sb(st3, "Wgt", [128, 8, D], BF16); r_Wgt = Res()
                    Wp = sb(st3, "Wp", [128, 2, D], BF16); r_Wp = Res()
                    rep3 = sb(st3, "rep3", [128, 3, D], F32); r_rep3 = Res()
                    DMA("dsp", rep3[:], rep4[:, 1:4, :], [], [r_rep3])
                    wst3 = Ring([sb(st3, f"wst3{i}", [128, D], F32) for i in range(2)])
                    for (src, dstw, dres, nch) in ((ple_wg, Wgt, r_Wgt, 8), (ple_pj, Wp, r_Wp, 2)):
                        for dc in range(nch):
                            w_t, w_r = wst3.next()
                            DMA("dsp" if dc % 2 == 0 else "dact", w_t[:], src[dc * 128:(dc + 1) * 128, :], [], [w_r])
                            CP("dve" if dc % 2 == 0 else "pool", dstw[:, dc, :], w_t[:], [w_r], [dres])
                    x3_ring = Ring([sb(st3, f"x3{i}", [128, D], F32) for i in range(2)])
                    x3b_ring = Ring([sb(st3, f"x3b{i}", [128, D], BF16) for i in range(2)])
                    x3T_ring = Ring([sb(st3, f"x3T{i}", [128, 8, 128], BF16) for i in range(2)])
                    pht = Ring([sb(st3, f"pht{i}", [128, 256], F32) for i in range(2)])
                    phb = Ring([sb(st3, f"phb{i}", [128, 256], BF16) for i in range(2)])
                    phT = Ring([sb(st3, f"phT{i}", [128, 2, 128], BF16) for i in range(2)])
                    gt_ring = Ring([sb(st3, f"gtD{i}", [128, D], F32) for i in range(2)])
                    ot_ring = Ring([sb(st3, f"otD{i}", [128, D], F32) for i in range(2)])
                    junk3 = sb(st3, "junk3", [128, D], BF16); r_junk3 = Res()
                    sm3 = Ring([sb(st3, f"sm3{i}", [128, 8], F32) for i in range(4)])

                    def rms3(src, src_r, gi, dstf, dstf_r):
                        s_t, s_r = sm3.next()
                        ACTF(junk3[:], src, AF.Square, [src_r], [r_junk3, s_r], accum_out=s_t[:, 0:1])
                        TS("dve", s_t[:, 1:2], s_t[:, 0:1], 1.0 / D, EPS, ALU.mult, ALU.add, [s_r], [s_r])
                        ACTF(s_t[:, 1:2], s_t[:, 1:2], AF.Sqrt, [s_r], [s_r])
                        E("dve", lambda: nc.vector.reciprocal(out=s_t[:, 2:3], in_=s_t[:, 1:2]), [s_r], [s_r])
                        STT(dstf, src, s_t[:, 2:3], rep3[:, gi, :], ALU.mult, ALU.mult, [src_r, s_r, r_rep3], [dstf_r])

                    def tr3(srcb, srcb_r, dstT, dstT_r, nblk):
                        pt, pt_r = ptb2.next()
                        for dc in range(nblk):
                            TR(pt[:, dc * 128:(dc + 1) * 128], srcb[:, dc * 128:(dc + 1) * 128], ident_b[:], [srcb_r, r_identb], [pt_r])
                        CP("act", dstT.rearrange("p a t -> p (a t)"), pt[:, 0:nblk * 128], [pt_r], [dstT_r])

                    for it in range(NTL):
                        tsl = slice(it * 128, (it + 1) * 128)
                        Hh = Yacc[:, it, :]; H_r = rY[it]
                        x3, x3_r = x3_ring.next()
                        rms3(Hh, H_r, 0, x3[:], x3_r)
                        x3b, x3b_r = x3b_ring.next()
                        CP("pool", x3b[:], x3[:], [x3_r], [x3b_r])
                        x3T, x3T_r = x3T_ring.next()
                        tr3(x3b, x3b_r, x3T[:], x3T_r, 8)
                        ph_t, ph_r = pht.next()
                        DMA("dact", ph_t[:], ph[tsl, :], [], [ph_r])
                        pb_t, pb_r = phb.next()
                        CP("pool", pb_t[:], ph_t[:], [ph_r], [pb_r])
                        pT_t, pT_r = phT.next()
                        tr3(pb_t, pb_r, pT_t[:], pT_r, 2)
                        gt, gt_r = gt_ring.next()
                        for ch in range(2):
                            csl = slice(ch * 512, (ch + 1) * 512)
                            pa, pa_r = p1.next()
                            for dc in range(8):
                                MM(pa[:], x3T[:, dc, :], Wgt[:, dc, csl], dc == 0, dc == 7, [x3T_r, r_Wgt], [pa_r])
                            TT("dve", gt[:, csl], pa[:], rep3[:, 2, csl], ALU.add, [pa_r, r_rep3], [gt_r])
                            ACTF(gt[:, csl], gt[:, csl], AF.Sigmoid, [gt_r], [gt_r])
                            pb2, pb2_r = p2.next()
                            for dc in range(2):
                                MM(pb2[:], pT_t[:, dc, :], Wp[:, dc, csl], dc == 0, dc == 1, [pT_r, r_Wp], [pb2_r])
                            TT("dve", gt[:, csl], gt[:, csl], pb2[:], ALU.mult, [gt_r, pb2_r], [gt_r])
                            TT("pool", Yacc[:, it, csl], Yacc[:, it, csl], gt[:, csl], ALU.add, [H_r, gt_r], [H_r])
                        ot, ot_r = ot_ring.next()
                        rms3(Hh, H_r, 1, ot[:], ot_r)
                        DMA("dsp", out[tsl, :], ot[:], [ot_r], [])

        kb.drain_all()
    return nc


def _blockdiag(w):
    o = np.zeros((128, 4, 128), np.float32)
    for n in range(8):
        cc, j = n // 2, n % 2
        o[j * 64:(j + 1) * 64, cc, j * 64:(j + 1) * 64] = w[n]
    return o


def _rel_bucket(dist):
    n = np.maximum(dist, 0)
    nf = np.maximum(n, 16).astype(np.float32)
    large = 16 + (np.log(nf / np.float32(16)) / np.float32(np.log(8.0)) * np.float32(16)).astype(np.int32)
    large = np.minimum(large, 31)
    return np.where(n < 16, n, large)


def _nsa_consts(rel_table):
    c = {}
    assert (_rel_bucket(np.arange(113, 8192)) == 31).all()
    cl = np.arange(128)[:, None, None]; mp = np.arange(5)[None, :, None]; tt = np.arange(512)[None, None, :]
    dist = 512 * mp + tt - 16 * cl - 31
    c["bc_g"] = np.ascontiguousarray(rel_table[_rel_bucket(dist)].transpose(3, 0, 1, 2))
    c["bc_m"] = np.where(dist >= 0, 0.0, NEGM).astype(np.float32)
    assert (512 * 5 - 16 * 127 - 31) >= 113
    sl = np.arange(128)[:, None]; tl = np.arange(128)[None, :]
    d0 = tl - sl; d1 = 128 + tl - sl
    g0 = rel_table[_rel_bucket(d0)]; g1 = rel_table[_rel_bucket(d1)]
    c["bd_g"] = np.ascontiguousarray(np.stack([g0, g1], 0).transpose(3, 1, 0, 2))
    m0 = np.where(d0 >= 0, 0.0, NEGM); m2 = np.where(tl < sl, 0.0, NEGM)
    c["bd_m"] = np.ascontiguousarray(np.stack([m0, np.zeros_like(m0), m2], 1)).astype(np.float32)
    c["t31"] = np.ascontiguousarray(np.broadcast_to(rel_table[31][None, :], (128, 8))).astype(np.float32)
    t = (np.arange(NT)[None, :, None] * 128 + np.arange(128)[:, None, None])
    blk = np.arange(64)[None, None, :]
    d = t // 64 - blk
    local = (d >= 0) & (d < 2)
    init = (blk == 0) & ~local
    past = (d >= 0) & ~local & ~init
    c["force_c"] = np.where(local, 2.0e4, np.where(init, 1.0e4, np.where(past, 0.0, -1.0))).astype(np.float32)
    c["keep_c"] = past.astype(np.float32)
    cs = np.arange(256)[:, None] * 16; ss = np.arange(64)[None, :] * 64
    ov = np.clip(np.minimum(cs + 32, ss + 64) - np.maximum(cs, ss), 0, None).astype(np.float32) / 32.0
    ove = np.concatenate([ov, np.ones((256, 1), np.float32)], 1)
    ove[255] = 0.0
    c["ovl_ext"] = np.ascontiguousarray(ove.reshape(2, 128, 65).transpose(1, 0, 2))
    c["erows"] = (np.arange(T)[None, :] // 64 == np.arange(64)[:, None]).astype(np.float32)
    return c


def _prep_inputs(inputs):
    x = np.ascontiguousarray(inputs["x"], dtype=np.float32)
    p = np.ascontiguousarray(inputs["p"], dtype=np.float32)
    shared = {
        "ident": np.eye(128, dtype=np.float32),
        "w_in": np.ascontiguousarray(inputs["w_in"][0]),
        "g_attn": np.ascontiguousarray(inputs["attn_norm"][0].reshape(8, 128).T),
        "lru_cw": np.ascontiguousarray(inputs["conv_w"][0][:, 0, :].reshape(4, 4, 128).transpose(2, 1, 0)),
        "lru_vec": np.ascontiguousarray(np.stack([inputs[k][0].reshape(4, 128) for k in
                                                  ("conv_b", "lru_ba", "lru_bx", "lru_lambda", "grp_norm_lru")], 0).transpose(2, 0, 1)),
        "cmp_w1": np.ascontiguousarray(np.stack([inputs[k][0].reshape(16, 128, 256).transpose(1, 0, 2) for k in ("cmp_k_w1", "cmp_v_w1")], 0)),
        "cmp_pe": np.ascontiguousarray(np.stack([inputs[k][0].reshape(16, 128).T for k in ("cmp_k_pe", "cmp_v_pe")], 0)),
        "cmp_w2": np.ascontiguousarray(np.stack([inputs[k][0].reshape(2, 128, 64).transpose(1, 0, 2) for k in ("cmp_k_w2", "cmp_v_w2")], 0)),
        "ga_rep": np.ascontiguousarray(np.broadcast_to(inputs["grp_norm_attn"][0][None, :], (128, 512))).astype(np.float32),
        "w_out": np.ascontiguousarray(inputs["w_out"][0]),
        "peer_wq": np.ascontiguousarray(inputs["peer_wq"][0]),
        "sk_T": np.ascontiguousarray(inputs["peer_subkeys"][0].transpose(0, 2, 1)),
        "peer_u": np.ascontiguousarray(inputs["peer_u"][0]),
        "peer_v": np.ascontiguousarray(inputs["peer_v"][0]),
        "ple_wgate": np.ascontiguousarray(inputs["ple_wgate"][0]),
        "ple_proj": np.ascontiguousarray(inputs["ple_proj"][0]),
        "rep4": np.ascontiguousarray(np.broadcast_to(np.stack([inputs["ffn_norm"][0], inputs["ple_norm"][0], inputs["final_norm"], inputs["ple_bgate"][0]], 0)[None], (128, 4, D))).astype(np.float32),
        "iota128": np.ascontiguousarray(np.broadcast_to(np.arange(128, dtype=np.float32)[None], (128, 128))),
        "iota16": np.ascontiguousarray(np.broadcast_to(np.arange(16, dtype=np.float32)[None], (128, 16))),
        "lru_bda": _blockdiag(inputs["lru_wa"][0]),
        "lru_bdx": _blockdiag(inputs["lru_wx"][0]),
    }
    shared.update(_nsa_consts(np.asarray(inputs["rel_table"], np.float32)))
    in_maps = []
    for c in range(8):
        b, hf = c // 2, c % 2
        m = dict(shared)
        m["xb"] = x[b]
        m["xh"] = np.ascontiguousarray(x[b, hf * TH:(hf + 1) * TH])
        m["ph"] = np.ascontiguousarray(p[0, b, hf * TH:(hf + 1) * TH])
        sel = np.zeros((128, 2), np.float32); sel[:, hf] = 1.0
        m["selc"] = sel
        in_maps.append(m)
    return in_maps


def kernel(**inputs):
    nc = build_program()
    in_maps = _prep_inputs(inputs)
    res = run_bass_kernel_spmd(nc, in_maps, core_ids=list(range(8)))
    outp = np.zeros((4, T, D), np.float32)
    for c in range(8):
        b, hf = c // 2, c % 2
        outp[b, hf * TH:(hf + 1) * TH] = res.results[c]["out"]
    return outp
```

```python
import numpy as np
from contextlib import ExitStack
import concourse.bass as bass
import concourse.mybir as mybir
from concourse.bass_utils import run_bass_kernel_spmd

F32 = mybir.dt.float32
BF16 = mybir.dt.bfloat16
U32 = mybir.dt.uint32
AF = mybir.ActivationFunctionType
ALU = mybir.AluOpType
AX = mybir.AxisListType

T = 4096
D = 1024
NT = T // 128
TH = 2048
IN_COLS = 2328
EPS = 1e-6
NEGM = -30000.0


class Res:
    __slots__ = ("name", "lw", "rd")

    def __init__(self, name=""):
        self.name = name
        self.lw = None
        self.rd = {}


class KB:
    NDMA = 4

    def __init__(self, nc, stack):
        self.nc = nc
        self.issue = {"pe": nc.tensor, "act": nc.scalar, "dve": nc.vector, "pool": nc.gpsimd,
                      "dsp": nc.sync, "dact": nc.scalar, "dpool": nc.gpsimd}
        self.stream = {"pe": "pe", "act": "act", "dve": "dve", "pool": "pool",
                       "dsp": "sp", "dact": "act", "dpool": "pool"}
        self.sems = {}
        self.cnt = {}
        for q in self.issue:
            n = self.NDMA if self.is_dma(q) else 1
            self.sems[q] = [stack.enter_context(nc.semaphore(f"s_{q}{i}")) for i in range(n)]
            self.cnt[q] = 0
        self.waited = {s: {} for s in ("pe", "act", "dve", "pool", "sp")}
        self.ninst = 0
        self._rr = 0

    @staticmethod
    def is_dma(q):
        return q in ("dsp", "dact", "dpool")

    @staticmethod
    def _need(need, dep):
        if dep is None:
            return
        q, c = dep
        if need.get(q, 0) < c:
            need[q] = c

    def _waits(self, st, eng, need, skip_q=None):
        for dq, c in need.items():
            if dq == "pe" and skip_q == "pe":
                continue
            if self.is_dma(dq):
                n = self.NDMA
                for si in range(n):
                    k = (c - 1 - si) // n + 1 if c - 1 >= si else 0
                    if k <= 0:
                        continue
                    key = (dq, si)
                    if self.waited[st].get(key, 0) >= k:
                        continue
                    eng.wait_ge(self.sems[dq][si], 16 * k)
                    self.waited[st][key] = k
            else:
                key = (dq, 0)
                if self.waited[st].get(key, 0) >= c:
                    continue
                eng.wait_ge(self.sems[dq][0], c)
                self.waited[st][key] = c

    def emit(self, q, fn, reads=(), writes=()):
        need = {}
        for r in reads:
            self._need(need, r.lw)
        for w in writes:
            self._need(need, w.lw)
            for rq, rc in w.rd.items():
                self._need(need, (rq, rc))
        st = self.stream[q]
        self._waits(st, self.issue[q], need, skip_q=q)
        inst = fn()
        self.cnt[q] += 1
        c = self.cnt[q]
        if self.is_dma(q):
            inst.then_inc(self.sems[q][(c - 1) % self.NDMA], 16)
        else:
            inst.then_inc(self.sems[q][0], 1)
        for r in reads:
            if r.rd.get(q, 0) < c:
                r.rd[q] = c
        for w in writes:
            w.lw = (q, c)
            w.rd = {}
        self.ninst += 1
        return inst

    def dmaq(self):
        self._rr ^= 1
        return "dsp" if self._rr else "dact"

    def barrier(self):
        need = {q: c for q, c in self.cnt.items() if c > 0}
        for st, eng in (("pe", self.nc.tensor), ("act", self.nc.scalar), ("dve", self.nc.vector),
                        ("pool", self.nc.gpsimd), ("sp", self.nc.sync)):
            self._waits(st, eng, dict(need))

    def drain_all(self):
        need = {q: c for q, c in self.cnt.items() if c > 0}
        self._waits("sp", self.nc.sync, need)


class Scope:
    def __init__(self, kb):
        self.kb = kb
        self.st = ExitStack()

    def __enter__(self):
        self.st.__enter__()
        return self.st

    def __exit__(self, *a):
        if a[0] is None:
            self.kb.barrier()
        return self.st.__exit__(*a)


class Ring:
    def __init__(self, tiles):
        self.tiles = tiles
        self.res = [Res() for _ in tiles]
        self.i = -1

    def next(self):
        self.i = (self.i + 1) % len(self.tiles)
        return self.tiles[self.i], self.res[self.i]


def build_program(dbg=None, phases=("A", "B", "C", "D")):
    nc = bass.Bass("TRN2", target_bir_lowering=False)

    def din(name, shape, dt=F32):
        return nc.dram_tensor(name, list(shape), dt, kind="ExternalInput").ap()

    dbg = dbg or ()

    def dscr(name, shape, dt):
        kind = "ExternalOutput" if name in dbg else "Internal"
        return nc.dram_tensor(name, list(shape), dt, kind=kind).ap()

    xb = din("xb", [T, D])
    xh = din("xh", [TH, D])
    ph = din("ph", [TH, 256])
    selc = din("selc", [128, 2])
    ident = din("ident", [128, 128])
    w_in = din("w_in", [D, IN_COLS])
    g_attn = din("g_attn", [128, 8])
    out = nc.dram_tensor("out", [TH, D], F32, kind="ExternalOutput").ap()
    lru_cw = din("lru_cw", [128, 4, 4])
    lru_vec = din("lru_vec", [128, 5, 4])
    lru_bda = din("lru_bda", [128, 4, 128])
    lru_bdx = din("lru_bdx", [128, 4, 128])

    qT_s = dscr("qT_s", [512, T], BF16)
    kcT_s = dscr("kcT_s", [128, T], BF16)
    vcT_s = dscr("vcT_s", [128, T], BF16)
    ksT_s = dscr("ksT_s", [128, T], BF16)
    kwT_s = dscr("kwT_s", [128, T], BF16)
    vs_s = dscr("vs_s", [T, 128], BF16)
    vw_s = dscr("vw_s", [T, 128], BF16)
    gates_s = dscr("gates_s", [T, 24], F32)
    xrT_s = dscr("xrT_s", [512, T], F32)
    xgT_s = dscr("xgT_s", [512, T], F32)

    cmp_w1 = din("cmp_w1", [2, 128, 16, 256])
    cmp_pe = din("cmp_pe", [2, 128, 16])
    cmp_w2 = din("cmp_w2", [2, 128, 2, 64])
    ovl_ext = din("ovl_ext", [128, 2, 65])
    bc_g = din("bc_g", [8, 128, 5, 512])
    bc_m = din("bc_m", [128, 5, 512])
    bd_g = din("bd_g", [8, 128, 2, 128])
    bd_m = din("bd_m", [128, 3, 128])
    t31_in = din("t31", [128, 8])
    force_in = din("force_c", [128, 32, 64])
    keep_in = din("keep_c", [128, 32, 64])
    erows = din("erows", [64, T])
    ga_in = din("ga_rep", [128, 512])
    w_out_in = din("w_out", [D, D])
    peer_wq = din("peer_wq", [D, 2048])
    sk_T = din("sk_T", [2, 128, 128])
    peer_u = din("peer_u", [16384, D])
    peer_v = din("peer_v", [16384, D])
    ple_wg = din("ple_wgate", [D, D])
    ple_pj = din("ple_proj", [256, D])
    rep4 = din("rep4", [128, 4, D])
    iota16 = din("iota16", [128, 16])
    iota128 = din("iota128", [128, 128])
    H1_s = dscr("H1_s", [TH, D], F32)
    xnT2_s = dscr("xnT2_s", [128, 8, TH], BF16)
    Wt_s = dscr("Wt_s", [TH // 128, 128, 128, 128], BF16)
    mixT_s = dscr("mixT_s", [1024, T], BF16)
    R = {n: Res(n) for n in ("H1_s", "xnT2_s", "Wt_s", "mixT_s", "qT_s", "kcT_s", "vcT_s", "ksT_s", "kwT_s", "vs_s", "vw_s", "gates_s", "xrT_s", "xgT_s")}

    with ExitStack() as top:
        kb = KB(nc, top)
        E = kb.emit

        uniq = [0]

        def sb(st, name, shape, dt):
            uniq[0] += 1
            return st.enter_context(nc.sbuf_tensor(f"sb{uniq[0]}_{name}", list(shape), dt))

        def ps(st, name, shape, dt):
            uniq[0] += 1
            return st.enter_context(nc.psum_tensor(f"ps{uniq[0]}_{name}", list(shape), dt))


        def MM(out_, lhsT, rhs, start, stop, reads, writes):
            return E("pe", lambda: nc.tensor.matmul(out_, lhsT=lhsT, rhs=rhs, start=start, stop=stop), reads, writes)

        def TR(out_, in_, idt, reads, writes):
            return E("pe", lambda: nc.tensor.transpose(out=out_, in_=in_, identity=idt), reads, writes)

        def ACTF(out_, in_, func, reads, writes, **kw):
            return E("act", lambda: nc.scalar.activation(out=out_, in_=in_, func=func, **kw), reads, writes)

        def veng(q):
            return nc.vector if q == "dve" else nc.gpsimd

        def TS(q, out_, in0, s1, s2, op0, op1, reads, writes):
            if op1 is None:
                return E(q, lambda: veng(q).tensor_scalar(out=out_, in0=in0, scalar1=s1, scalar2=None, op0=op0), reads, writes)
            return E(q, lambda: veng(q).tensor_scalar(out=out_, in0=in0, scalar1=s1, scalar2=s2, op0=op0, op1=op1), reads, writes)

        def TT(q, out_, in0, in1, op, reads, writes):
            return E(q, lambda: veng(q).tensor_tensor(out=out_, in0=in0, in1=in1, op=op), reads, writes)

        def STT(out_, in0, scalar, in1, op0, op1, reads, writes, **kw):
            return E("dve", lambda: nc.vector.scalar_tensor_tensor(out=out_, in0=in0, scalar=scalar, in1=in1, op0=op0, op1=op1, **kw), reads, writes)

        def CP(q, out_, in_, reads, writes):
            if q == "act":
                return E("act", lambda: nc.scalar.copy(out=out_, in_=in_), reads, writes)
            return E(q, lambda: veng(q).tensor_copy(out=out_, in_=in_), reads, writes)

        def MSET(q, out_, val, writes):
            return E(q, lambda: veng(q).memset(out_, val), (), writes)

        def DMA(q, out_, in_, reads, writes):
            eng = {"dsp": nc.sync, "dact": nc.scalar, "dpool": nc.gpsimd}[q]
            return E(q, lambda: eng.dma_start(out=out_, in_=in_), reads, writes)

        def drive_rr(gens):
            res = [None] * len(gens)
            live = list(range(len(gens)))
            while live:
                for gi in list(live):
                    try:
                        next(gens[gi])
                    except StopIteration as e:
                        res[gi] = e.value
                        live.remove(gi)
            return res

        def dump(name, ap, shape, dt, res):
            if name not in dbg:
                return
            d = nc.dram_tensor(name, list(shape), dt, kind="ExternalOutput").ap()
            DMA("dsp", d, ap, [res] if not isinstance(res, list) else res, [])

        ident_f = sb(top, "ident_f", [128, 128], F32); r_identf = Res()
        ident_b = sb(top, "ident_b", [128, 128], BF16); r_identb = Res()
        E("dsp", lambda: nc.sync.dma_start(out=ident_f[:], in_=ident), writes=[r_identf])
        E("dve", lambda: nc.vector.tensor_copy(out=ident_b[:], in_=ident_f[:]), reads=[r_identf], writes=[r_identb])

        if "A" in phases:
            with Scope(kb) as st:
                Wg = sb(st, "Wg", [128, 8, IN_COLS], BF16); r_Wg = Res()
                gcol = sb(st, "gcol", [128, 8], F32); r_gcol = Res()
                wst = Ring([sb(st, f"wst{i}", [128, IN_COLS], F32) for i in range(2)])
                E("dsp", lambda: nc.sync.dma_start(out=gcol[:], in_=g_attn), writes=[r_gcol])
                for dc in range(8):
                    w_t, w_r = wst.next()
                    E("dsp" if dc % 2 == 0 else "dact",
                      (lambda w_t=w_t, dc=dc: nc.sync.dma_start(out=w_t[:], in_=w_in[dc * 128:(dc + 1) * 128, :])) if dc % 2 == 0 else
                      (lambda w_t=w_t, dc=dc: nc.scalar.dma_start(out=w_t[:], in_=w_in[dc * 128:(dc + 1) * 128, :])),
                      writes=[w_r])
                    eng = "dve" if dc % 2 == 0 else "pool"
                    ve = nc.vector if dc % 2 == 0 else nc.gpsimd
                    E(eng, lambda ve=ve, w_t=w_t, dc=dc: ve.tensor_scalar(out=Wg[:, dc, :], in0=w_t[:], scalar1=gcol[:, dc:dc + 1], scalar2=None, op0=ALU.mult),
                      reads=[w_r, r_gcol], writes=[r_Wg])

                xt_ring = Ring([sb(st, f"xt{i}", [128, 4, D], F32) for i in range(2)])
                xnb_ring = Ring([sb(st, f"xnb{i}", [128, 4, D], BF16) for i in range(2)])
                xnT_ring = Ring([sb(st, f"xnT{i}", [128, 8, 512], BF16) for i in range(2)])
                junk = sb(st, "junkA", [128, D], BF16); r_junk = Res()
                ss_ring = Ring([sb(st, f"ss{i}", [128, 8], F32) for i in range(2)])
                pT_ring = Ring([ps(st, f"pT{i}", [128, 512], BF16) for i in range(2)])
                pacc = Ring([ps(st, f"pacc{i}", [128, 512], F32) for i in range(4)])
                ostf = Ring([sb(st, f"ostf{i}", [128, 512], F32) for i in range(3)])
                ostb = Ring([sb(st, f"ostb{i}", [128, 512], BF16) for i in range(3)])
                osv = Ring([sb(st, f"osv{i}", [128, 256], BF16) for i in range(2)])
                osg = Ring([sb(st, f"osg{i}", [128, 24], F32) for i in range(2)])
                xb_v = xb.rearrange("(n p) d -> p n d", p=128)
                fm = []
                for cc in range(4):
                    fm.append((cc * 128, qT_s[cc * 128:(cc + 1) * 128, :], 0.125, True, R["qT_s"]))
                fm.append((512, kcT_s, 1.0, True, R["kcT_s"]))
                fm.append((640, vcT_s, 1.0, True, R["vcT_s"]))
                fm.append((768, ksT_s, 1.0, True, R["ksT_s"]))
                fm.append((1024, kwT_s, 1.0, True, R["kwT_s"]))
                for cc in range(4):
                    fm.append((1304 + cc * 128, xrT_s[cc * 128:(cc + 1) * 128, :], 1.0, False, R["xrT_s"]))
                for cc in range(4):
                    fm.append((1816 + cc * 128, xgT_s[cc * 128:(cc + 1) * 128, :], 1.0, False, R["xgT_s"]))
                evc = [0]

                def stageP(tcn):
                    xt, xt_r = xt_ring.next()
                    DMA("dsp", xt[:], xb_v[:, tcn * 4:(tcn + 1) * 4, :], [], [xt_r])
                    ss, ss_r = ss_ring.next()
                    for n in range(4):
                        ACTF(junk[:], xt[:, n, :], AF.Square, [xt_r], [r_junk, ss_r], accum_out=ss[:, n:n + 1])
                    yield
                    TS("dve", ss[:, 4:8], ss[:, 0:4], 1.0 / D, EPS, ALU.mult, ALU.add, [ss_r], [ss_r])
                    ACTF(ss[:, 4:8], ss[:, 4:8], AF.Sqrt, [ss_r], [ss_r])
                    yield
                    E("dve", lambda: nc.vector.reciprocal(out=ss[:, 4:8], in_=ss[:, 4:8]), [ss_r], [ss_r])
                    xnb, xnb_r = xnb_ring.next()
                    for n in range(4):
                        if n % 2 == 0:
                            TS("dve", xnb[:, n, :], xt[:, n, :], ss[:, 4 + n:5 + n], None, ALU.mult, None, [xt_r, ss_r], [xnb_r])
                        else:
                            ACTF(xnb[:, n, :], xt[:, n, :], AF.Copy, [xt_r, ss_r], [xnb_r], scale=ss[:, 4 + n:5 + n])
                    yield
                    xnT, xnT_r = xnT_ring.next()
                    for dc in range(8):
                        pT, pT_r = pT_ring.next()
                        for n in range(4):
                            TR(pT[:, n * 128:(n + 1) * 128], xnb[:, n, dc * 128:(dc + 1) * 128], ident_b[:], [xnb_r, r_identb], [pT_r])
                        CP("act" if dc % 2 == 0 else "dve", xnT[:, dc, :], pT[:], [pT_r], [xnT_r])
                        if dc % 2 == 1:
                            yield
                    return (xnT, xnT_r)

                def stageM(tcn, st):
                    xnT, xnT_r = st
                    for (c0, dst, scale, isb, dres) in fm:
                        pa, pa_r = pacc.next()
                        for dc in range(8):
                            MM(pa[:], Wg[:, dc, c0:c0 + 128], xnT[:, dc, :], dc == 0, dc == 7, [r_Wg, xnT_r], [pa_r])
                        o_t, o_r = (ostb if isb else ostf).next()
                        evc[0] += 1
                        if evc[0] % 2 == 0:
                            ACTF(o_t[:], pa[:], AF.Copy, [pa_r], [o_r], scale=scale)
                        else:
                            TS("dve", o_t[:], pa[:], scale, None, ALU.mult, None, [pa_r], [o_r])
                        DMA("dsp" if evc[0] % 2 == 0 else "dpool", dst[:, tcn * 512:(tcn + 1) * 512], o_t[:], [o_r], [dres])
                        yield
                    for n in range(4):
                        t0 = tcn * 512 + n * 128
                        pa, pa_r = pacc.next()
                        for dc in range(8):
                            MM(pa[:, 0:128], xnT[:, dc, n * 128:(n + 1) * 128], Wg[:, dc, 896:1024], dc == 0, dc == 7, [r_Wg, xnT_r], [pa_r])
                        pb, pb_r = pacc.next()
                        for dc in range(8):
                            MM(pb[:, 0:152], xnT[:, dc, n * 128:(n + 1) * 128], Wg[:, dc, 1152:1304], dc == 0, dc == 7, [r_Wg, xnT_r], [pb_r])
                        ov, ov_r = osv.next()
                        og, og_r = osg.next()
                        CP("act", ov[:, 0:128], pa[:, 0:128], [pa_r], [ov_r])
                        CP("dve", ov[:, 128:256], pb[:, 0:128], [pb_r], [ov_r])
                        CP("dve", og[:], pb[:, 128:152], [pb_r], [og_r])
                        DMA("dsp", vs_s[t0:t0 + 128, :], ov[:, 0:128], [ov_r], [R["vs_s"]])
                        DMA("dpool", vw_s[t0:t0 + 128, :], ov[:, 128:256], [ov_r], [R["vw_s"]])
                        DMA("dsp", gates_s[t0:t0 + 128, :], og[:], [og_r], [R["gates_s"]])
                        yield

                stP = drive_rr([stageP(0)])[0]
                for tcn in range(8):
                    gens = [stageM(tcn, stP)]
                    if tcn + 1 < 8:
                        gens.append(stageP(tcn + 1))
                    rr = drive_rr(gens)
                    if tcn + 1 < 8:
                        stP = rr[1]

        if "B" in phases:
            with Scope(kb) as st:
                cw = sb(st, "cw", [128, 4, 4], F32); r_cw = Res()
                lv = sb(st, "lv", [128, 5, 4], F32); r_lv = Res()
                clc = sb(st, "clc", [128, 3, 4], F32); r_clc = Res()
                bdf = sb(st, "bdf", [128, 2, 4, 128], F32); r_bdf = Res()
                bdb = sb(st, "bdb", [128, 2, 4, 128], BF16); r_bdb = Res()
                ones_b = sb(st, "ones_b", [128, 128], BF16); r_ones = Res()
                E("dsp", lambda: nc.sync.dma_start(out=cw[:], in_=lru_cw), writes=[r_cw])
                E("dact", lambda: nc.scalar.dma_start(out=lv[:], in_=lru_vec), writes=[r_lv])
                E("dsp", lambda: nc.sync.dma_start(out=bdf[:, 0], in_=lru_bda), writes=[r_bdf])
                E("dact", lambda: nc.scalar.dma_start(out=bdf[:, 1], in_=lru_bdx), writes=[r_bdf])
                E("dve", lambda: nc.vector.tensor_copy(out=bdb[:], in_=bdf[:]), reads=[r_bdf], writes=[r_bdb])
                E("dve", lambda: nc.vector.memset(ones_b[:], 1.0), writes=[r_ones])
                E("act", lambda: nc.scalar.activation(out=clc[:, 0, :], in_=lv[:, 3, :], func=AF.Exp, scale=-1.0), reads=[r_lv], writes=[r_clc])
                E("act", lambda: nc.scalar.activation(out=clc[:, 0, :], in_=clc[:, 0, :], func=AF.Ln, bias=1.0), reads=[r_clc], writes=[r_clc])
                E("dve", lambda: nc.vector.tensor_scalar(out=clc[:, 1, :], in0=clc[:, 0, :], scalar1=-8.0, scalar2=None, op0=ALU.mult), reads=[r_clc], writes=[r_clc])
                E("dve", lambda: nc.vector.tensor_scalar(out=clc[:, 2, :], in0=clc[:, 0, :], scalar1=-16.0, scalar2=None, op0=ALU.mult), reads=[r_clc], writes=[r_clc])
                L = sb(st, "Lall", [128, 4, T], F32); r_L = Res()
                X = [sb(st, f"lruX{i}", [128, T], F32) for i in range(5)]
                rX = [Res() for _ in range(5)]
                xcb = sb(st, "xcb", [128, T], BF16); r_xcb = Res()
                pg = Ring([ps(st, f"pg{i}", [128, 512], F32) for i in range(4)])
                for cc in range(4):
                    X1, X2, X3, X4, X5 = X
                    r1, r2, r3, r4, r5 = rX
                    for hh in range(2):
                        E("dsp", lambda cc=cc, hh=hh: nc.sync.dma_start(out=X1[:, hh * 2048:(hh + 1) * 2048], in_=xrT_s[cc * 128:(cc + 1) * 128, hh * 2048:(hh + 1) * 2048]), reads=[R["xrT_s"]], writes=[r1])
                        E("dact", lambda cc=cc, hh=hh: nc.scalar.dma_start(out=X3[:, hh * 2048:(hh + 1) * 2048], in_=xgT_s[cc * 128:(cc + 1) * 128, hh * 2048:(hh + 1) * 2048]), reads=[R["xgT_s"]], writes=[r3])
                    E("dve", lambda cc=cc: nc.vector.tensor_scalar(out=X2[:], in0=X1[:], scalar1=cw[:, cc, 3:4], scalar2=lv[:, 0, cc:cc + 1], op0=ALU.mult, op1=ALU.add), reads=[r1, r_cw, r_lv], writes=[r2])
                    for sh in (1, 2, 3):
                        E("dve", lambda cc=cc, sh=sh: nc.vector.scalar_tensor_tensor(out=X2[:, sh:T], in0=X1[:, 0:T - sh], scalar=cw[:, cc, 3 - sh:4 - sh], in1=X2[:, sh:T], op0=ALU.mult, op1=ALU.add), reads=[r1, r2, r_cw], writes=[r2])
                    E("pool", lambda: nc.gpsimd.tensor_copy(out=xcb[:], in_=X2[:]), reads=[r2], writes=[r_xcb])
                    for gi, (Xo, ro, bi) in enumerate(((X4, r4, 1), (X5, r5, 2))):
                        for tcn in range(8):
                            pgt, pg_r = pg.next()
                            E("pe", lambda pgt=pgt, gi=gi, cc=cc, tcn=tcn: nc.tensor.matmul(pgt[:], lhsT=bdb[:, gi, cc, :], rhs=xcb[:, tcn * 512:(tcn + 1) * 512], start=True, stop=True), reads=[r_bdb, r_xcb], writes=[pg_r])
                            E("act", lambda pgt=pgt, Xo=Xo, bi=bi, cc=cc, tcn=tcn: nc.scalar.activation(out=Xo[:, tcn * 512:(tcn + 1) * 512], in_=pgt[:], func=AF.Sigmoid, bias=lv[:, bi, cc:cc + 1]), reads=[pg_r, r_lv], writes=[ro])
                    E("act", lambda cc=cc: nc.scalar.activation(out=X1[:], in_=X4[:], func=AF.Exp, scale=clc[:, 1, cc:cc + 1]), reads=[r4, r_clc], writes=[r1])
                    E("act", lambda cc=cc: nc.scalar.activation(out=X4[:], in_=X4[:], func=AF.Exp, scale=clc[:, 2, cc:cc + 1]), reads=[r4, r_clc], writes=[r4])
                    E("act", lambda: nc.scalar.activation(out=X4[:], in_=X4[:], func=AF.Sqrt, scale=-1.0, bias=1.0), reads=[r4], writes=[r4])
                    E("pool", lambda: nc.gpsimd.tensor_tensor(out=X5[:], in0=X5[:], in1=X2[:], op=ALU.mult), reads=[r5, r2], writes=[r5])
                    E("dve", lambda: nc.vector.tensor_tensor(out=X4[:], in0=X4[:], in1=X5[:], op=ALU.mult), reads=[r4, r5], writes=[r4])
                    E("dve", lambda: nc.vector.tensor_tensor_scan(out=X2[:], data0=X1[:], data1=X4[:], initial=0.0, op0=ALU.mult, op1=ALU.add), reads=[r1, r4], writes=[r2])
                    E("act", lambda: nc.scalar.activation(out=X3[:], in_=X3[:], func=AF.Gelu_apprx_tanh), reads=[r3], writes=[r3])
                    E("pool", lambda cc=cc: nc.gpsimd.tensor_tensor(out=L[:, cc, :], in0=X2[:], in1=X3[:], op=ALU.mult), reads=[r2, r3], writes=[r_L])
                sq = Ring([sb(st, f"lsq{i}", [128, 512], BF16) for i in range(2)])
                rs_ring = Ring([sb(st, f"lrs{i}", [128, 512], F32) for i in range(2)])
                lo = Ring([sb(st, f"lo{i}", [128, 512], BF16) for i in range(3)])
                for tcn in range(8):
                    pgt, pg_r = pg.next()
                    for cc in range(4):
                        sq_t, sq_r = sq.next()
                        E("act", lambda sq_t=sq_t, cc=cc, tcn=tcn: nc.scalar.activation(out=sq_t[:], in_=L[:, cc, tcn * 512:(tcn + 1) * 512], func=AF.Square), reads=[r_L], writes=[sq_r])
                        E("pe", lambda pgt=pgt, sq_t=sq_t, cc=cc: nc.tensor.matmul(pgt[:], lhsT=ones_b[:], rhs=sq_t[:], start=(cc == 0), stop=(cc == 3)), reads=[r_ones, sq_r], writes=[pg_r])
                    rs_t, rs_r = rs_ring.next()
                    E("dve", lambda rs_t=rs_t, pgt=pgt: nc.vector.tensor_scalar(out=rs_t[:], in0=pgt[:], scalar1=1.0 / 512, scalar2=EPS, op0=ALU.mult, op1=ALU.add), reads=[pg_r], writes=[rs_r])
                    E("act", lambda rs_t=rs_t: nc.scalar.activation(out=rs_t[:], in_=rs_t[:], func=AF.Sqrt), reads=[rs_r], writes=[rs_r])
                    E("dve", lambda rs_t=rs_t: nc.vector.reciprocal(out=rs_t[:], in_=rs_t[:]), reads=[rs_r], writes=[rs_r])
                    for cc in range(4):
                        lo_t, lo_r = lo.next()
                        E("dve", lambda lo_t=lo_t, rs_t=rs_t, cc=cc, tcn=tcn: nc.vector.scalar_tensor_tensor(out=lo_t[:], in0=L[:, cc, tcn * 512:(tcn + 1) * 512], scalar=lv[:, 4, cc:cc + 1], in1=rs_t[:], op0=ALU.mult, op1=ALU.mult), reads=[r_L, rs_r, r_lv], writes=[lo_r])
                        E("dsp", lambda lo_t=lo_t, cc=cc, tcn=tcn: nc.sync.dma_start(out=mixT_s[512 + cc * 128:512 + (cc + 1) * 128, tcn * 512:(tcn + 1) * 512], in_=lo_t[:]), reads=[lo_r], writes=[R["mixT_s"]])

        if "C" in phases:
            with Scope(kb) as st:
                Aout = sb(st, "Aout", [128, NT, 512], BF16)
                rA = [Res() for _ in range(NT)]
                sig = sb(st, "sig", [128, NT, 24], F32); r_sig = Res()
                force_t = sb(st, "force_t", [128, NT, 64], F32); r_force = Res()
                keep_t = sb(st, "keep_t", [128, NT, 64], F32); r_keep = Res()
                t31 = sb(st, "t31", [128, 8], F32); r_t31 = Res()
                BD = sb(st, "BD", [128, 8, 3, 128], BF16); r_BD = Res()
                ovl_t = sb(st, "ovl_t", [128, 2, 65], F32); r_ovl = Res()
                ga_t = sb(st, "ga_t", [128, 512], F32); r_ga = Res()
                bcm = sb(st, "bcm", [128, 5, 512], F32); r_bcm = Res()
                DMA("dsp", sig[:], gates_s.rearrange("(n p) c -> p n c", p=128), [R["gates_s"]], [r_sig])
                ACTF(sig[:], sig[:], AF.Sigmoid, [r_sig], [r_sig])
                DMA("dact", force_t[:], force_in, [], [r_force])
                DMA("dsp", keep_t[:], keep_in, [], [r_keep])
                DMA("dact", t31[:], t31_in, [], [r_t31])
                DMA("dsp", ovl_t[:], ovl_ext, [], [r_ovl])
                DMA("dact", ga_t[:], ga_in, [], [r_ga])
                DMA("dsp", bcm[:], bc_m, [], [r_bcm])
                psb = [ps(st, f"pC{i}", [128, 512], F32) for i in range(8)]
                pS = Ring(psb[0:3])
                pO = psb[3:7]; r_pO = [Res() for _ in range(4)]
                pX = Ring(psb[7:8])
                with Scope(kb) as st2:
                    bdg = sb(st2, "bdg", [128, 8, 2, 128], F32); r_bdg = Res()
                    bdm = sb(st2, "bdm", [128, 3, 128], F32); r_bdm = Res()
                    DMA("dsp", bdg[:], bd_g.rearrange("h p j t -> p h j t"), [], [r_bdg])
                    DMA("dact", bdm[:], bd_m, [], [r_bdm])
                    for hg in range(8):
                        for j in range(2):
                            STT(BD[:, hg, j, :], bdg[:, hg, j, :], t31[:, hg:hg + 1], bdm[:, j, :], ALU.subtract, ALU.add, [r_bdg, r_bdm, r_t31], [r_BD])
                        CP("dve", BD[:, hg, 2, :], bdm[:, 2, :], [r_bdm], [r_BD])
                P_ring = Ring([sb(st, f"Pt{i}", [128, 512], BF16) for i in range(5)])
                sm = Ring([sb(st, f"smC{i}", [128, 8], F32) for i in range(8)])
                osb = Ring([sb(st, f"osb{i}", [128, 132], F32) for i in range(8)])

                def finish_tiles(items, ncol, hg, br, first, imp_first=None):
                    sts = [sm.next() for _ in items]
                    for (po, po_r, i, _, _), (s_t, s_r) in zip(items, sts):
                        TS("dve", s_t[:, 0:1], po[:, ncol:ncol + 1], 1e-30, None, ALU.max, None, [po_r], [s_r])
                    for (po, po_r, i, _, _), (s_t, s_r) in zip(items, sts):
                        E("dve", lambda: nc.vector.reciprocal(out=s_t[:, 1:2], in_=s_t[:, 0:1]), [s_r], [s_r])
                    for (po, po_r, i, _, _), (s_t, s_r) in zip(items, sts):
                        TT("dve", s_t[:, 2:3], s_t[:, 1:2], sig[:, i, hg * 3 + br:hg * 3 + br + 1], ALU.mult, [s_r, r_sig], [s_r])
                    for (po, po_r, i, _, _), (s_t, s_r) in zip(items, sts):
                        dst = Aout[:, i, hg * 64:(hg + 1) * 64]
                        if first:
                            TS("dve", dst, po[:, 0:64], s_t[:, 2:3], None, ALU.mult, None, [po_r, s_r], [rA[i]])
                        else:
                            STT(dst, po[:, 0:64], s_t[:, 2:3], dst, ALU.mult, ALU.add, [po_r, s_r, rA[i]], [rA[i]])
                    if imp_first is not None:
                        for (po, po_r, i, imp_t, imp_r), (s_t, s_r) in zip(items, sts):
                            if imp_first:
                                TS("dve", imp_t, po[:, 64:128], s_t[:, 1:2], None, ALU.mult, None, [po_r, s_r], [imp_r])
                            else:
                                STT(imp_t, po[:, 64:128], s_t[:, 1:2], imp_t, ALU.mult, ALU.add, [po_r, s_r, imp_r], [imp_r])

                for k in range(2):
                    with Scope(kb) as stg:
                        KcmpT = sb(stg, "KcmpT", [64, 256], BF16); r_Kc = Res()
                        Vco = sb(stg, "Vco", [128, 2, 129], BF16); r_Vco = Res()
                        with Scope(kb) as stc:
                            w1s = Ring([sb(stc, f"w1s{i}", [128, 8, 256], F32) for i in range(2)])
                            w1b = sb(stc, "w1b", [128, 2, 16, 256], BF16); r_w1b = Res()
                            pes = sb(stc, "pes", [128, 2, 16], F32); r_pes = Res()
                            peb = sb(stc, "peb", [128, 2, 16], BF16); r_peb = Res()
                            w2s = sb(stc, "w2s", [128, 2, 2, 64], F32); r_w2s = Res()
                            w2b = sb(stc, "w2b", [128, 2, 2, 64], BF16); r_w2b = Res()
                            stk = sb(stc, "stk", [128, 2, T], BF16); r_stk = Res()
                            hb = sb(stc, "hb", [128, 4], F32); r_hb = Res()
                            gh = sb(stc, "gh", [128, 2, 2, 256], BF16); r_gh = Res()
                            for kv in range(2):
                                for hh in range(2):
                                    w_t, w_r = w1s.next()
                                    DMA("dsp" if hh == 0 else "dact", w_t[:], cmp_w1[kv, :, hh * 8:(hh + 1) * 8, :], [], [w_r])
                                    CP("pool" if hh == 0 else "dve", w1b[:, kv, hh * 8:(hh + 1) * 8, :], w_t[:], [w_r], [r_w1b])
                                DMA("dsp", pes[:, kv, :], cmp_pe[kv], [], [r_pes])
                                DMA("dact", w2s[:, kv], cmp_w2[kv], [], [r_w2s])
                                src = kcT_s if kv == 0 else vcT_s
                                sres = R["kcT_s"] if kv == 0 else R["vcT_s"]
                                DMA("dsp", stk[0:64, kv, :], src[k * 64:(k + 1) * 64, :], [sres], [r_stk])
                                MSET("pool", stk[64:128, kv, T - 1:T], 0.0, [r_stk])
                                DMA("dact", stk[64:128, kv, 0:T - 1], src[k * 64:(k + 1) * 64, 1:T], [sres], [r_stk])
                            CP("dve", peb[:], pes[:], [r_pes], [r_peb])
                            CP("dve", w2b[:], w2s[:], [r_w2s], [r_w2b])
                            MSET("pool", gh[:], 0.0, [r_gh])
                            for kv in range(2):
                                for hh in range(2):
                                    px, px_r = pX.next()
                                    for m in range(16):
                                        MM(px[:, 0:1], w1b[:, kv, m, hh * 128:(hh + 1) * 128], peb[:, kv, m:m + 1], m == 0, m == 15, [r_w1b, r_peb], [px_r])
                                    CP("dve", hb[:, kv * 2 + hh:kv * 2 + hh + 1], px[:, 0:1], [px_r], [r_hb])
                                    p_s, p_r = pS.next()
                                    for m in range(16):
                                        MM(p_s[:, 0:255], w1b[:, kv, m, hh * 128:(hh + 1) * 128], stk[:, kv, 2 * m:2 * m + 16 * 254 + 1:16], m == 0, m == 15, [r_w1b, r_stk], [p_r])
                                    ACTF(gh[:, kv, hh, 0:255], p_s[:, 0:255], AF.Gelu_apprx_tanh, [p_r, r_hb], [r_gh], bias=hb[:, kv * 2 + hh:kv * 2 + hh + 1])
                            px, px_r = pX.next()
                            for hh in range(2):
                                MM(px[0:64, 0:256], w2b[:, 0, hh, :], gh[:, 0, hh, :], hh == 0, hh == 1, [r_w2b, r_gh], [px_r])
                            CP("dve", KcmpT[:], px[0:64, 0:256], [px_r], [r_Kc])
                            for ct in range(2):
                                px, px_r = pX.next()
                                for hh in range(2):
                                    MM(px[:, 0:64], gh[:, 1, hh, ct * 128:(ct + 1) * 128], w2b[:, 1, hh, :], hh == 0, hh == 1, [r_gh, r_w2b], [px_r])
                                CP("dve", Vco[:, ct, 0:64], px[:, 0:64], [px_r], [r_Vco])
                            CP("pool", Vco[:, :, 64:129], ovl_t[:], [r_ovl], [r_Vco])
                            if k == 0:
                                dump("d_kcmp", KcmpT[:], [64, 256], BF16, r_Kc)
                                dump("d_vco", Vco[:], [128, 2, 129], BF16, r_Vco)
                                dump("d_hb", hb[:], [128, 4], F32, r_hb)
                                dump("d_gh", gh[:], [128, 2, 2, 256], BF16, r_gh)

                        QT = sb(stg, "QT", [128, 4, T], BF16)
                        r_QT = [Res() for _ in range(4)]
                        r_QM = [[Res() for _ in range(NT)] for _ in range(4)]
                        KsT = sb(stg, "KsT", [128, T], BF16); r_KsT = Res()
                        KwT = sb(stg, "KwT", [64, T], BF16); r_KwT = Res()
                        Vs = sb(stg, "Vs", [128, NT, 65], BF16); r_Vs = Res()
                        Vw = sb(stg, "Vw", [128, NT, 65], BF16); r_Vw = Res()
                        imp_acc = sb(stg, "imp_acc", [128, NT, 64], F32)
                        r_imp = [Res() for _ in range(NT)]
                        for g in range(4):
                            hg = 4 * k + g
                            DMA("dsp" if g % 2 == 0 else "dact", QT[0:64, g, :], qT_s[hg * 64:(hg + 1) * 64, :], [R["qT_s"]], [r_QT[g]])
                        DMA("dsp", KsT[0:64, :], ksT_s[k * 64:(k + 1) * 64, :], [R["ksT_s"]], [r_KsT])
                        with Scope(kb) as ste:
                            ers = sb(ste, "ers", [128, T], F32); r_ers = Res()
                            DMA("dact", ers[64:128, :], erows, [], [r_ers])
                            CP("pool", KsT[64:128, :], ers[64:128, :], [r_ers], [r_KsT])
                        DMA("dact", KwT[:], kwT_s[k * 64:(k + 1) * 64, :], [R["kwT_s"]], [r_KwT])
                        DMA("dsp", Vs[:, :, 0:64], vs_s.rearrange("(n p) c -> p n c", p=128)[:, :, k * 64:(k + 1) * 64], [R["vs_s"]], [r_Vs])
                        DMA("dact", Vw[:, :, 0:64], vw_s.rearrange("(n p) c -> p n c", p=128)[:, :, k * 64:(k + 1) * 64], [R["vw_s"]], [r_Vw])
                        MSET("pool", Vs[:, :, 64:65], 1.0, [r_Vs])
                        MSET("pool", Vw[:, :, 64:65], 1.0, [r_Vw])

                        bcs = Ring([sb(stg, f"bcs{i}", [128, 5, 512], F32) for i in range(2)])
                        BC = Ring([sb(stg, f"BCb{i}", [128, 5, 512], BF16) for i in range(2)])
                        bc_cur = {}

                        def cmp_stage1(it):
                            g, tcn, ct, last = it
                            hg = 4 * k + g
                            if tcn == 0 and ct == 0:
                                bs_t, bs_r = bcs.next()
                                DMA("dsp", bs_t[:, 0:3], bc_g[hg, :, 0:3], [], [bs_r])
                                DMA("dact", bs_t[:, 3:5], bc_g[hg, :, 3:5], [], [bs_r])
                                bc_t, bc_r = BC.next()
                                for m in range(5):
                                    STT(bc_t[:, m, :], bs_t[:, m, :], t31[:, hg:hg + 1], bcm[:, m, :], ALU.subtract, ALU.add, [bs_r, r_bcm, r_t31], [bc_r])
                                bc_cur[g] = (bc_t, bc_r)
                            bc_t, bc_r = bc_cur[g]
                            mp = tcn - 4 * ct
                            p_s, p_r = pS.next()
                            MM(p_s[:], KcmpT[:, ct * 128:(ct + 1) * 128], QT[0:64, g, tcn * 512:(tcn + 1) * 512], True, mp >= 5, [r_Kc, r_QT[g]], [p_r])
                            if mp < 5:
                                MM(p_s[:], ident_b[:], bc_t[:, mp, :], False, True, [r_identb, bc_r], [p_r])
                            P_t, P_r = P_ring.next()
                            ACTF(P_t[:], p_s[:], AF.Exp, [p_r, r_t31], [P_r], bias=t31[:, hg:hg + 1])
                            return (P_t, P_r)

                        def cmp_stage2(it, st1):
                            g, tcn, ct, last = it
                            hg = 4 * k + g
                            P_t, P_r = st1
                            for q in range(4):
                                MM(pO[q][:, 0:129], P_t[:, q * 128:(q + 1) * 128], Vco[:, ct, :], ct == 0, last, [P_r, r_Vco], [r_pO[q]])
                            if last:
                                items = []
                                for q in range(4):
                                    i = 4 * tcn + q
                                    o_t, o_r = osb.next()
                                    CP("dve", o_t[:, 0:129], pO[q][:, 0:129], [r_pO[q]], [o_r])
                                    items.append((o_t, o_r, i, imp_acc[:, i, :], r_imp[i]))
                                finish_tiles(items, 128, hg, 0, True, imp_first=(g == 0))

                        its = []
                        for g in range(4):
                            for tcn in range(8):
                                cts = [0] if tcn < 4 else [0, 1]
                                for ct in cts:
                                    its.append((g, tcn, ct, ct == cts[-1]))
                        LAG = 2
                        pend = []
                        for n in range(len(its) + LAG):
                            if n < len(its):
                                pend.append((its[n], cmp_stage1(its[n])))
                            if n >= LAG:
                                it0, st0 = pend.pop(0)
                                cmp_stage2(it0, st0)

                        if k == 0:
                            dump("d_imp", imp_acc[:], [128, NT, 64], F32, r_imp)
                            dump("d_aout_c", Aout[:], [128, NT, 512], BF16, rA)
                        MBr = Ring([sb(stg, f"MB{i}", [128, 128], F32) for i in range(2)])
                        for (mb_t, mb_r) in zip(MBr.tiles, MBr.res):
                            MSET("dve", mb_t[:], 0.0, [mb_r])
                        tk = Ring([sb(stg, f"tk{i}", [128, 2, 64], F32) for i in range(2)])
                        mxr = Ring([sb(stg, f"mx{i}", [128, 16], F32) for i in range(2)])
                        mtr = Ring([sb(stg, f"mtr{i}", [128, 128], BF16) for i in range(2)])
                        def c3_gen():
                          for i in range(NT):
                            tk_t, tk_r = tk.next()
                            mx_t, mx_r = mxr.next()
                            TT("dve", tk_t[:, 0, :], imp_acc[:, i, :], keep_t[:, i, :], ALU.mult, [r_imp[i], r_keep], [tk_r])
                            TT("dve", tk_t[:, 0, :], tk_t[:, 0, :], force_t[:, i, :], ALU.add, [tk_r, r_force], [tk_r])
                            E("dve", lambda: nc.vector.max(out=mx_t[:, 0:8], in_=tk_t[:, 0, :]), [tk_r], [mx_r])
                            E("dve", lambda: nc.vector.match_replace(out=tk_t[:, 1, :], in_to_replace=mx_t[:, 0:8], in_values=tk_t[:, 0, :], imm_value=-1e30), [tk_r, mx_r], [tk_r])
                            E("dve", lambda: nc.vector.max(out=mx_t[:, 8:16], in_=tk_t[:, 1, :]), [tk_r], [mx_r])
                            mb_t, mb_r = MBr.next()
                            TS("dve", mb_t[:, 64:128], tk_t[:, 0, :], mx_t[:, 15:16], None, ALU.is_ge, None, [tk_r, mx_r], [mb_r])
                            TS("dve", mb_t[:, 64:128], mb_t[:, 64:128], 1.0, -NEGM, ALU.subtract, ALU.mult, [mb_r], [mb_r])
                            px, px_r = pX.next()
                            TR(px[:, 0:128], mb_t[:], ident_f[:], [mb_r, r_identf], [px_r])
                            mt_t, mt_r = mtr.next()
                            CP("act", mt_t[64:128, :], px[64:128, 0:128], [px_r], [mt_r])
                            for g in range(4):
                                CP("pool" if g % 2 == 0 else "dve", QT[64:128, g, i * 128:(i + 1) * 128], mt_t[64:128, :], [mt_r], [r_QM[g][i]])
                            yield

                        if k == 0:
                            dump("d_qt0", QT[:, 0, :], [128, T], BF16, r_QT + [x for l in r_QM for x in l])
                        def sel_stage1(it):
                            g, br, tcn, j = it
                            hg = 4 * k + g
                            qa = max(0, j - 4 * tcn)
                            qb = 3 if br == 1 else min(3, j + 4 - 4 * tcn)
                            c0, c1 = qa * 128, (qb + 1) * 128
                            t0 = tcn * 512
                            adds = []
                            for q in range(qa, qb + 1):
                                dlt = 4 * tcn + q - j
                                if dlt == 0:
                                    adds.append((q, 0))
                                elif dlt == 1:
                                    adds.append((q, 1))
                                elif dlt == 4 and br == 2:
                                    adds.append((q, 2))
                            p_s, p_r = pS.next()
                            if br == 1:
                                rd = [r_KsT, r_QT[g]] + [r_QM[g][4 * tcn + q] for q in range(qa, qb + 1)]
                                MM(p_s[:, c0:c1], KsT[:, j * 128:(j + 1) * 128], QT[:, g, t0 + c0:t0 + c1], True, len(adds) == 0, rd, [p_r])
                            else:
                                MM(p_s[:, c0:c1], KwT[:, j * 128:(j + 1) * 128], QT[0:64, g, t0 + c0:t0 + c1], True, len(adds) == 0, [r_KwT, r_QT[g]], [p_r])
                            for ai, (q, ty) in enumerate(adds):
                                MM(p_s[:, q * 128:(q + 1) * 128], ident_b[:], BD[:, hg, ty, :], False, ai == len(adds) - 1, [r_identb, r_BD], [p_r])
                            P_t, P_r = P_ring.next()
                            ACTF(P_t[:, c0:c1], p_s[:, c0:c1], AF.Exp, [p_r, r_t31], [P_r], bias=t31[:, hg:hg + 1])
                            return (P_t, P_r, qa, qb)

                        def sel_stage2(it, st1):
                            g, br, tcn, j = it
                            hg = 4 * k + g
                            P_t, P_r, qa, qb = st1
                            Vx, r_Vx = (Vs, r_Vs) if br == 1 else (Vw, r_Vw)
                            for q in range(qa, qb + 1):
                                i = 4 * tcn + q
                                first_j = 0 if br == 1 else max(0, i - 4)
                                MM(pO[q][:, 0:65], P_t[:, q * 128:(q + 1) * 128], Vx[:, j, :], j == first_j, j == i, [P_r, r_Vx], [r_pO[q]])
                            if j == 4 * tcn + 3:
                                items = []
                                for q in range(4):
                                    o_t, o_r = osb.next()
                                    CP("dve", o_t[:, 0:65], pO[q][:, 0:65], [r_pO[q]], [o_r])
                                    items.append((o_t, o_r, 4 * tcn + q, None, None))
                                finish_tiles(items, 64, hg, br, False)

                        def branch_gen(br):
                            its = []
                            for g in range(4):
                                for tcn in range(8):
                                    j_lo = 0 if br == 1 else max(0, 4 * tcn - 4)
                                    for j in range(j_lo, 4 * tcn + 4):
                                        its.append((g, br, tcn, j))
                            LAG = 2
                            pend = []
                            for n in range(len(its) + LAG):
                                if n < len(its):
                                    pend.append((its[n], sel_stage1(its[n])))
                                if n >= LAG:
                                    it0, st0 = pend.pop(0)
                                    sel_stage2(it0, st0)
                                if n % 4 == 3:
                                    yield

                        drive_rr([branch_gen(2), c3_gen()])
                        drive_rr([branch_gen(1)])

                dump("d_aout", Aout[:], [128, NT, 512], BF16, rA)
                with Scope(kb) as stn:
                    junkC = sb(stn, "junkC", [128, 512], BF16); r_junkC = Res()
                    an = Ring([sb(stn, f"an{i}", [128, 512], BF16) for i in range(2)])
                    af = Ring([sb(stn, f"af{i}", [128, 512], F32) for i in range(2)])
                    ao = Ring([sb(stn, f"ao{i}", [128, 512], BF16) for i in range(2)])
                    pTb = Ring([ps(stn, f"pTC{i}", [128, 512], BF16) for i in range(2)]) if False else None
                    for i in range(NT):
                        s_t, s_r = sm.next()
                        ACTF(junkC[:], Aout[:, i, :], AF.Square, [rA[i]], [r_junkC, s_r], accum_out=s_t[:, 0:1])
                        TS("dve", s_t[:, 1:2], s_t[:, 0:1], 1.0 / 512, EPS, ALU.mult, ALU.add, [s_r], [s_r])
                        ACTF(s_t[:, 1:2], s_t[:, 1:2], AF.Sqrt, [s_r], [s_r])
                        E("dve", lambda: nc.vector.reciprocal(out=s_t[:, 2:3], in_=s_t[:, 1:2]), [s_r], [s_r])
                        af_t, af_r = af.next()
                        STT(af_t[:], Aout[:, i, :], s_t[:, 2:3], ga_t[:], ALU.mult, ALU.mult, [rA[i], s_r, r_ga], [af_r])
                        px, px_r = pX.next()
                        for fc in range(4):
                            TR(px[:, fc * 128:(fc + 1) * 128], af_t[:, fc * 128:(fc + 1) * 128], ident_f[:], [af_r, r_identf], [px_r])
                        ao_t, ao_r = ao.next()
                        CP("act", ao_t[:], px[:], [px_r], [ao_r])
                        DMA("dsp" if i % 2 == 0 else "dpool", mixT_s[0:512, i * 128:(i + 1) * 128].rearrange("(f p) t -> p f t", p=128),
                            ao_t[:].rearrange("p (f t) -> p f t", f=4), [ao_r], [R["mixT_s"]])

        if "D" in phases or "D1" in phases:
            NTL = TH // 128
            with Scope(kb) as st:
                Wo = sb(st, "Wo", [128, 8, D], BF16); r_Wo = Res()
                Wq = sb(st, "Wq", [128, 8, 2048], BF16); r_Wq = Res()
                skb = sb(st, "skb", [128, 2, 128], BF16); r_skb = Res()
                repf = sb(st, "repf", [128, D], F32); r_rep = Res()
                io16 = sb(st, "io16", [128, 16], F32); r_io = Res()
                io128 = sb(st, "io128", [128, 128], F32); r_io128 = Res()
                selt = sb(st, "selt", [128, 2], F32); r_sel = Res()
                DMA("dsp", repf[:], rep4[:, 0, :], [], [r_rep])
                DMA("dact", io16[:], iota16, [], [r_io])
                DMA("dact", io128[:], iota128, [], [r_io128])
                DMA("dact", selt[:], selc, [], [r_sel])
                with Scope(kb) as stw:
                    wst = Ring([sb(stw, f"wstD{i}", [128, 2048], F32) for i in range(3)])
                    n = 0
                    for (src, dstw, dres, ncol, nch) in ((w_out_in, Wo, r_Wo, D, 8), (peer_wq, Wq, r_Wq, 2048, 8)):
                        for dc in range(nch):
                            w_t, w_r = wst.next()
                            n += 1
                            DMA("dsp" if n % 2 == 0 else "dact", w_t[:, 0:ncol], src[dc * 128:(dc + 1) * 128, :], [], [w_r])
                            CP("dve" if n % 2 == 0 else "pool", dstw[:, dc, :], w_t[:, 0:ncol], [w_r], [dres])
                    w_t, w_r = wst.next()
                    DMA("dsp", w_t[:, 0:256].rearrange("p (a k) -> p a k", a=2), sk_T.rearrange("a p k -> p a k"), [], [w_r])
                    CP("dve", skb[:], w_t[:, 0:256].rearrange("p (a k) -> p a k", a=2), [w_r], [r_skb])

                pacc = Ring([ps(st, f"pD{i}", [128, 512], F32) for i in range(4)])
                pw_ring = Ring([ps(st, f"pDw{i}", [128, 512], F32) for i in range(2)])
                ptb = Ring([ps(st, f"pDb{i}", [128, 1024], BF16) for i in range(2)])
                mst = Ring([sb(st, f"mst{i}", [128, 8, 2, 128], BF16) for i in range(1)])
                mixh_ring = Ring([sb(st, f"mixh{i}", [128, 8, 128], BF16) for i in range(1)])
                xh_ring = Ring([sb(st, f"xhD{i}", [128, D], F32) for i in range(2)])
                H_ring = Ring([sb(st, f"HD{i}", [128, D], F32) for i in range(2)])
                xng_ring = Ring([sb(st, f"xng{i}", [128, D], F32) for i in range(1)])
                xnb_ring = Ring([sb(st, f"xnbD{i}", [128, D], BF16) for i in range(1)])
                xT_ring = Ring([sb(st, f"xTD{i}", [128, 8, 128], BF16) for i in range(2)])
                qTb = sb(st, "qTb", [128, 16, 128], BF16); r_qTb = Res()
                Ssc_ring = Ring([sb(st, f"Ssc{i}", [128, 16, 128], F32) for i in range(2)])
                Swk = sb(st, "Swk", [128, 8, 128], F32)
                rv = [Res() for _ in range(16)]; rv2 = [Res() for _ in range(16)]; ri = [Res() for _ in range(16)]; ri2 = [Res() for _ in range(16)]; rw = [Res() for _ in range(16)]
                v16 = sb(st, "v16", [128, 16, 16], F32); r_v16 = Res()
                i16 = sb(st, "i16", [128, 16, 16], U32); r_i16 = Res()
                i16f = sb(st, "i16f", [128, 16, 16], F32); r_i16f = Res()
                cand = sb(st, "cand", [128, 8, 256], F32); r_cand = Res()
                cwk = sb(st, "cwk", [128, 8, 256], F32)
                sc16 = sb(st, "sc16", [128, 8, 16], F32); r_sc = Res()
                ci16 = sb(st, "ci16", [128, 8, 16], U32); r_ci = Res()
                ab_u = sb(st, "ab_u", [128, 2, 8, 16], U32); r_abu = Res()
                ab_f = sb(st, "ab_f", [128, 2, 8, 16], F32); r_abf = Res()
                eq = sb(st, "eq", [128, 8, 16, 16], F32); r_eq = Res()
                isel_ring = Ring([sb(st, f"isel{i}", [128, 3, 8, 16], F32) for i in range(2)])
                gz = sb(st, "gz", [128, 16], F32); r_gz = Res()
                junkB = sb(st, "junkDb", [128, D], BF16); r_junkB = Res()
                smD = Ring([sb(st, f"smD{i}", [128, 8], F32) for i in range(4)])
                ijgT_ring = Ring([sb(st, f"ijgT{i}", [128, 3, 128], F32) for i in range(2)])
                OI = Ring([sb(st, f"OI{i}", [128, 16, 128], BF16) for i in range(2)])
                OJ = Ring([sb(st, f"OJ{i}", [128, 16, 128], BF16) for i in range(2)])
                OJf = Ring([sb(st, f"OJf{i}", [128, 16, 128], BF16) for i in range(2)])
                Wst = sb(st, "Wst", [128, 128, 128], BF16); r_Wst = Res()

                def rms_scaled(src, src_r, gain, gain_r, dstf, dstf_r):
                    s_t, s_r = smD.next()
                    ACTF(junkB[:], src, AF.Square, [src_r], [r_junkB, s_r], accum_out=s_t[:, 0:1])
                    TS("dve", s_t[:, 1:2], s_t[:, 0:1], 1.0 / D, EPS, ALU.mult, ALU.add, [s_r], [s_r])
                    ACTF(s_t[:, 1:2], s_t[:, 1:2], AF.Sqrt, [s_r], [s_r])
                    E("dve", lambda: nc.vector.reciprocal(out=s_t[:, 2:3], in_=s_t[:, 1:2]), [s_r], [s_r])
                    STT(dstf, src, s_t[:, 2:3], gain, ALU.mult, ALU.mult, [src_r, s_r, gain_r], [dstf_r])

                def rms_scaled_g(src, src_r, gain, gain_r, dstf, dstf_r):
                    s_t, s_r = smD.next()
                    ACTF(junkB[:], src, AF.Square, [src_r], [r_junkB, s_r], accum_out=s_t[:, 0:1])
                    yield
                    TS("dve", s_t[:, 1:2], s_t[:, 0:1], 1.0 / D, EPS, ALU.mult, ALU.add, [s_r], [s_r])
                    ACTF(s_t[:, 1:2], s_t[:, 1:2], AF.Sqrt, [s_r], [s_r])
                    yield
                    E("dve", lambda: nc.vector.reciprocal(out=s_t[:, 2:3], in_=s_t[:, 1:2]), [s_r], [s_r])
                    STT(dstf, src, s_t[:, 2:3], gain, ALU.mult, ALU.mult, [src_r, s_r, gain_r], [dstf_r])

                def transpose8(srcb, srcb_r, dstT, dstT_r, nblk=8):
                    pt, pt_r = ptb.next()
                    for dc in range(nblk):
                        TR(pt[:, dc * 128:(dc + 1) * 128], srcb[:, dc * 128:(dc + 1) * 128], ident_b[:], [srcb_r, r_identb], [pt_r])
                    CP("act", dstT.rearrange("p a t -> p (a t)"), pt[:, 0:nblk * 128], [pt_r], [dstT_r])

                def S1(it):
                    tsl = slice(it * 128, (it + 1) * 128)
                    xh_t, xh_r = xh_ring.next()
                    DMA("dsp", xh_t[:], xh[tsl, :], [], [xh_r])
                    H, H_r = H_ring.next()
                    m_t, m_r = mst.next()
                    for a in range(2):
                        DMA("dsp" if a == 0 else "dact", m_t[:, :, a, :], mixT_s[:, a * TH + it * 128:a * TH + (it + 1) * 128].rearrange("(f p) t -> p f t", p=128), [R["mixT_s"]], [m_r])
                    mixh, r_mixh = mixh_ring.next()
                    ACTF(mixh[:], m_t[:, :, 0, :], AF.Copy, [m_r, r_sel], [r_mixh], scale=selt[:, 0:1])
                    STT(mixh[:], m_t[:, :, 1, :], selt[:, 1:2], mixh[:], ALU.mult, ALU.add, [m_r, r_sel, r_mixh], [r_mixh])
                    yield
                    for ch in range(2):
                        pa, pa_r = pacc.next()
                        for fc in range(8):
                            MM(pa[:], mixh[:, fc, :], Wo[:, fc, ch * 512:(ch + 1) * 512], fc == 0, fc == 7, [r_mixh, r_Wo], [pa_r])
                        TT("dve", H[:, ch * 512:(ch + 1) * 512], pa[:], xh_t[:, ch * 512:(ch + 1) * 512], ALU.add, [pa_r, xh_r], [H_r])
                        yield
                    DMA("dpool", H1_s[tsl, :], H[:], [H_r], [R["H1_s"]])
                    xng, xng_r = xng_ring.next()
                    yield from rms_scaled_g(H[:], H_r, repf[:], r_rep, xng[:], xng_r)
                    yield
                    xnb, xnb_r = xnb_ring.next()
                    CP("pool", xnb[:], xng[:], [xng_r], [xnb_r])
                    yield
                    xT, xT_r = xT_ring.next()
                    transpose8(xnb, xnb_r, xT[:], xT_r)
                    yield
                    DMA("dact", xnT2_s[:, :, tsl], xT[:], [xT_r], [R["xnT2_s"]])
                    for grp in range(4):
                        pa, pa_r = pacc.next()
                        for j in range(4):
                            hp = grp * 4 + j
                            for dc in range(8):
                                MM(pa[:, j * 128:(j + 1) * 128], Wq[:, dc, hp * 128:(hp + 1) * 128], xT[:, dc, :], dc == 0, dc == 7, [r_Wq, xT_r], [pa_r])
                        CP("act", qTb[:, grp * 4:(grp + 1) * 4, :].rearrange("p a t -> p (a t)"), pa[:], [pa_r], [r_qTb])
                        yield
                    Ssc, r_S = Ssc_ring.next()
                    for grp in range(4):
                        pa, pa_r = pacc.next()
                        for j in range(4):
                            hp = grp * 4 + j
                            MM(pa[:, j * 128:(j + 1) * 128], qTb[:, hp, :], skb[:, hp % 2, :], True, True, [r_qTb, r_skb], [pa_r])
                        CP("act", Ssc[:, grp * 4:(grp + 1) * 4, :].rearrange("p a t -> p (a t)"), pa[:], [pa_r], [r_S])
                        yield
                    return (Ssc, r_S)

                def S2(it, st1):
                    Ssc, r_S = st1
                    for g0 in (0, 8):
                        hps = range(g0, g0 + 8)
                        for hp in hps:
                            E("dve", lambda: nc.vector.max(out=v16[:, hp, 0:8], in_=Ssc[:, hp, :]), [r_S], [rv[hp]])
                        yield
                        for hp in hps:
                            E("dve", lambda: nc.vector.max_index(out=i16[:, hp, 0:8], in_max=v16[:, hp, 0:8], in_values=Ssc[:, hp, :]), [r_S, rv[hp]], [ri[hp]])
                        yield
                        for hp in hps:
                            E("dve", lambda: nc.vector.match_replace(out=Swk[:, hp - g0, :], in_to_replace=v16[:, hp, 0:8], in_values=Ssc[:, hp, :], imm_value=-1e30), [r_S, rv[hp]], [rw[hp - g0]])
                        yield
                        for hp in hps:
                            E("dve", lambda: nc.vector.max(out=v16[:, hp, 8:16], in_=Swk[:, hp - g0, :]), [rw[hp - g0]], [rv2[hp]])
                        yield
                        for hp in hps:
                            E("dve", lambda: nc.vector.max_index(out=i16[:, hp, 8:16], in_max=v16[:, hp, 8:16], in_values=Swk[:, hp - g0, :]), [rw[hp - g0], rv2[hp]], [ri2[hp]])
                        yield
                    r_i16 = Res()
                    E("dve", lambda: nc.vector.tensor_copy(out=i16f[:], in_=i16[:]), ri + ri2, [r_i16f, r_i16])
                    v4 = v16[:].rearrange("p (h two) k -> p h two k", two=2)
                    in0 = v4[:, :, 0, :].rearrange("p h (a o) -> p h a o", o=1).to_broadcast([128, 8, 16, 16])
                    in1 = v4[:, :, 1, :].rearrange("p h (o b) -> p h o b", o=1).to_broadcast([128, 8, 16, 16])
                    TT("dve", cand[:].rearrange("p h (a b) -> p h a b", a=16), in0, in1, ALU.add, rv + rv2, [r_cand])
                    for h in range(8):
                        E("dve", lambda: nc.vector.max(out=sc16[:, h, 0:8], in_=cand[:, h, :]), [r_cand], [rv[h]])
                    yield
                    for h in range(8):
                        E("dve", lambda: nc.vector.max_index(out=ci16[:, h, 0:8], in_max=sc16[:, h, 0:8], in_values=cand[:, h, :]), [r_cand, rv[h]], [ri[h]])
                    yield
                    for h in range(8):
                        E("dve", lambda: nc.vector.match_replace(out=cwk[:, h, :], in_to_replace=sc16[:, h, 0:8], in_values=cand[:, h, :], imm_value=-1e30), [r_cand, rv[h]], [rw[h]])
                    yield
                    for h in range(8):
                        E("dve", lambda: nc.vector.max(out=sc16[:, h, 8:16], in_=cwk[:, h, :]), [rw[h]], [rv2[h]])
                    yield
                    for h in range(8):
                        E("dve", lambda: nc.vector.max_index(out=ci16[:, h, 8:16], in_max=sc16[:, h, 8:16], in_values=cwk[:, h, :]), [rw[h], rv2[h]], [ri2[h]])
                    yield
                    r_sc = Res(); r_ci = Res()
                    E("dve", lambda: nc.vector.tensor_single_scalar(out=ab_u[:, 0], in_=ci16[:], scalar=4, op=ALU.logical_shift_right), ri[:8] + ri2[:8] + rv[:8] + rv2[:8], [r_abu, r_sc, r_ci])
                    E("dve", lambda: nc.vector.tensor_single_scalar(out=ab_u[:, 1], in_=ci16[:], scalar=15, op=ALU.bitwise_and), [r_ci], [r_abu])
                    CP("dve", ab_f[:], ab_u[:], [r_abu], [r_abf])
                    isel, r_isel = isel_ring.next()
                    i4 = i16f[:].rearrange("p (h two) k -> p h two k", two=2)
                    for w in range(2):
                        a_b = ab_f[:, w].rearrange("p h (k o) -> p h k o", o=1).to_broadcast([128, 8, 16, 16])
                        io_b = io16[:].rearrange("p (o q a) -> p o q a", o=1, q=1).to_broadcast([128, 8, 16, 16])
                        TT("dve", eq[:], a_b, io_b, ALU.is_equal, [r_abf, r_io], [r_eq])
                        iv_b = i4[:, :, w, :].rearrange("p h (o a) -> p h o a", o=1).to_broadcast([128, 8, 16, 16])
                        TT("dve", eq[:], eq[:], iv_b, ALU.mult, [r_eq, r_i16f], [r_eq])
                        E("dve", lambda: nc.vector.tensor_reduce(out=isel[:, w], in_=eq[:], axis=AX.X, op=ALU.add), [r_eq], [r_isel])
                        yield
                    TT("dve", isel[:, 2], sc16[:], sc16[:, :, 0:1].to_broadcast([128, 8, 16]), ALU.subtract, [r_sc], [r_isel])
                    ACTF(isel[:, 2], isel[:, 2], AF.Exp, [r_isel], [r_isel])
                    E("dve", lambda: nc.vector.tensor_reduce(out=gz[:, 0:8], in_=isel[:, 2], axis=AX.X, op=ALU.add), [r_isel], [r_gz])
                    E("dve", lambda: nc.vector.reciprocal(out=gz[:, 8:16], in_=gz[:, 0:8]), [r_gz], [r_gz])
                    TT("dve", isel[:, 2], isel[:, 2], gz[:, 8:16].rearrange("p (h o) -> p h o", o=1).to_broadcast([128, 8, 16]), ALU.mult, [r_isel, r_gz], [r_isel])
                    pa, pa_r = pacc.next()
                    for w in range(3):
                        TR(pa[:, w * 128:(w + 1) * 128], isel[:, w].rearrange("p h k -> p (h k)"), ident_f[:], [r_isel, r_identf], [pa_r])
                    ijgT, r_ijgT = ijgT_ring.next()
                    CP("act", ijgT[:].rearrange("p a t -> p (a t)"), pa[:, 0:384], [pa_r], [r_ijgT])
                    return (ijgT, r_ijgT)

                def S3(it, st2):
                    ijgT, r_ijgT = st2
                    TB = 16
                    for tb in range(128 // TB):
                        t0 = tb * TB
                        oi, oi_r = OI.next()
                        oj, oj_r = OJ.next()
                        io_b = io128[:].rearrange("p (o i) -> p o i", o=1).to_broadcast([128, TB, 128])

                        def colb(w):
                            return ijgT[:, w, t0:t0 + TB].rearrange("p (t o) -> p t o", o=1).to_broadcast([128, TB, 128])
                        ojf, ojf_r = OJf.next()
                        TT("dve", oi[:], io_b, colb(0), ALU.is_equal, [r_io128, r_ijgT], [oi_r])
                        TT("dve", ojf[:], io_b, colb(1), ALU.is_equal, [r_io128, r_ijgT], [ojf_r])
                        TT("pool", oj[:], ojf[:], colb(2), ALU.mult, [ojf_r, r_ijgT], [oj_r])
                        for tq in range(TB // 4):
                            pw, pw_r = pw_ring.next()
                            for u in range(4):
                                MM(pw[:, u:512:4], oj[:, tq * 4 + u, :], oi[:, tq * 4 + u, :], True, True, [oj_r, oi_r], [pw_r])
                            tg = t0 + tq * 4
                            CP("act", Wst[:, :, tg:tg + 4], pw[:].rearrange("p (i t) -> p i t", t=4), [pw_r], [r_Wst])
                            yield
                    DMA("dsp" if it % 2 == 0 else "dact", Wt_s[it], Wst[:], [r_Wst], [R["Wt_s"]])

                def drive(gens):
                    res = [None] * len(gens)
                    live = list(range(len(gens)))
                    while live:
                        for gi in list(live):
                            try:
                                next(gens[gi])
                            except StopIteration as e:
                                res[gi] = e.value
                                live.remove(gi)
                    return res

                st1s, st2s = {}, {}
                for n in range(NTL + 2):
                    gens, tags = [], []
                    if n < NTL:
                        gens.append(S1(n)); tags.append(("s1", n))
                    if n >= 2:
                        gens.append(S3(n - 2, st2s.pop(n - 2))); tags.append(("s3", n - 2))
                    if 1 <= n <= NTL:
                        gens.append(S2(n - 1, st1s.pop(n - 1))); tags.append(("s2", n - 1))
                    for (tg_, tn), rv_ in zip(tags, drive(gens)):
                        if tg_ == "s1":
                            st1s[tn] = rv_
                        elif tg_ == "s2":
                            st2s[tn] = rv_

            with Scope(kb) as st:
                Yacc = sb(st, "Yacc", [128, NTL, D], F32)
                rY = [Res() for _ in range(NTL)]
                H1v = H1_s.rearrange("(n p) d -> p n d", p=128)
                for n4 in range(4):
                    DMA("dsp" if n4 % 2 == 0 else "dact", Yacc[:, n4 * 4:(n4 + 1) * 4, :], H1v[:, n4 * 4:(n4 + 1) * 4, :], [R["H1_s"]], rY[n4 * 4:(n4 + 1) * 4])
                p1 = Ring([ps(st, f"pE1{i}", [128, 512], F32) for i in range(3)])
                p2 = Ring([ps(st, f"pE2{i}", [128, 512], F32) for i in range(3)])
                ptb2 = Ring([ps(st, f"pEb{i}", [128, 1024], BF16) for i in range(2)])
                with Scope(kb) as st2:
                  if "D" in phases or "D2" in phases:
                    xnTa = sb(st2, "xnTa", [128, 8, TH], BF16); r_xnTa = Res()
                    for dc in range(8):
                        DMA("dsp" if dc % 2 == 0 else "dact", xnTa[:, dc, :], xnT2_s[:, dc, :], [R["xnT2_s"]], [r_xnTa])
                    IB = 8
                    ust = Ring([sb(st2, f"ust{i}", [128, D], F32) for i in range(2)])
                    vst = Ring([sb(st2, f"vst{i}", [128, D], F32) for i in range(2)])
                    ub = Ring([sb(st2, f"ub{i}", [128, D], BF16) for i in range(2)])
                    uT = Ring([sb(st2, f"uT{i}", [128, 8, 128], BF16) for i in range(2)])
                    Vb = sb(st2, "Vb", [128, IB, D], BF16); r_Vb = [Res() for _ in range(IB)]
                    WA = sb(st2, "WA", [128, IB, TH], BF16); r_WA = [Res() for _ in range(IB)]
                    wt = Ring([sb(st2, f"wt{i}", [128, TH], BF16) for i in range(3)])
                    gl = Ring([sb(st2, f"gl{i}", [128, 512], BF16) for i in range(3)])
                    for ib0 in range(0, 128, IB):
                        for ib in range(IB):
                            i = ib0 + ib
                            u_t, u_r = ust.next()
                            v_t, v_r = vst.next()
                            w_t, w_r = wt.next()
                            DMA("dsp", u_t[:], peer_u[i * 128:(i + 1) * 128, :], [], [u_r])
                            DMA("dact", v_t[:], peer_v[i * 128:(i + 1) * 128, :], [], [v_r])
                            for hw in range(2):
                                DMA("dpool" if hw == 0 else ("dsp" if i % 2 == 0 else "dact"), w_t[:, hw * 1024:(hw + 1) * 1024].rearrange("p (n t) -> p n t", t=128),
                                    Wt_s[hw * 8:(hw + 1) * 8, :, i, :].rearrange("n j t -> j n t"), [R["Wt_s"]], [w_r])
                            ub_t, ub_r = ub.next()
                            CP("pool", ub_t[:], u_t[:], [u_r], [ub_r])
                            CP("pool", Vb[:, ib, :], v_t[:], [v_r], [r_Vb[ib]])
                            pt, pt_r = ptb2.next()
                            for dc in range(8):
                                TR(pt[:, dc * 128:(dc + 1) * 128], ub_t[:, dc * 128:(dc + 1) * 128], ident_b[:], [ub_r, r_identb], [pt_r])
                            uT_t, uT_r = uT.next()
                            CP("act", uT_t[:].rearrange("p a t -> p (a t)"), pt[:], [pt_r], [uT_r])
                            for tc4 in range(4):
                                pa, pa_r = p1.next()
                                for dc in range(8):
                                    MM(pa[:], uT_t[:, dc, :], xnTa[:, dc, tc4 * 512:(tc4 + 1) * 512], dc == 0, dc == 7, [uT_r, r_xnTa], [pa_r])
                                g_t, g_r = gl.next()
                                ACTF(g_t[:], pa[:], AF.Gelu_apprx_tanh, [pa_r], [g_r])
                                TT("dve", WA[:, ib, tc4 * 512:(tc4 + 1) * 512], g_t[:], w_t[:, tc4 * 512:(tc4 + 1) * 512], ALU.mult, [g_r, w_r], [r_WA[ib]])
                        for tt in range(NTL):
                            for ch in range(2):
                                pb, pb_r = p2.next()
                                for ib in range(IB):
                                    MM(pb[:], WA[:, ib, tt * 128:(tt + 1) * 128], Vb[:, ib, ch * 512:(ch + 1) * 512], ib == 0, ib == IB - 1, [r_WA[ib], r_Vb[ib]], [pb_r])
                                TT("dve", Yacc[:, tt, ch * 512:(ch + 1) * 512], Yacc[:, tt, ch * 512:(ch + 1) * 512], pb[:], ALU.add, [rY[tt], pb_r], [rY[tt]])


                with Scope(kb) as st3:
                    Wgt = sb(st3, "Wgt", [128, 8, D], BF16); r_Wgt = Res()
                    Wp = sb(st3, "Wp", [128, 2, D], BF16); r_Wp = Res()
                    rep3 = sb(st3, "rep3", [128, 3, D], F32); r_rep3 = Res()
                    DMA("dsp", rep3[:], rep4[:, 1:4, :], [], [r_rep3])
                    wst3 = Ring([sb(st3, f"wst3{i}", [128, D], F32) for i in range(2)])
                    for (src, dstw, dres, nch) in ((ple_wg, Wgt, r_Wgt, 8), (ple_pj, Wp, r_Wp, 2)):
                        for dc in range(nch):
                            w_t, w_r = wst3.next()
                            DMA("dsp" if dc % 2 == 0 else "dact", w_t[:], src[dc * 128:(dc + 1) * 128, :], [], [w_r])
                            CP("dve" if dc % 2 == 0 else "pool", dstw[:, dc, :], w_t[:], [w_r], [dres])
                    x3_ring = Ring([sb(st3, f"x3{i}", [128, D], F32) for i in range(2)])
                    x3b_ring = Ring([sb(st3, f"x3b{i}", [128, D], BF16) for i in range(2)])
                    x3T_ring = Ring([sb(st3, f"x3T{i}", [128, 8, 128], BF16) for i in range(2)])
                    pht = Ring([sb(st3, f"pht{i}", [128, 256], F32) for i in range(2)])
                    phb = Ring([sb(st3, f"phb{i}", [128, 256], BF16) for i in range(2)])
                    phT = Ring([sb(st3, f"phT{i}", [128, 2, 128], BF16) for i in range(2)])
                    gt_ring = Ring([sb(st3, f"gtD{i}", [128, D], F32) for i in range(2)])
                    ot_ring = Ring([sb(st3, f"otD{i}", [128, D], F32) for i in range(2)])
                    junk3 = sb(st3, "junk3", [128, D], BF16); r_junk3 = Res()
                    sm3 = Ring([sb(st3, f"sm3{i}", [128, 8], F32) for i in range(4)])

                    def rms3(src, src_r, gi, dstf, dstf_r):
                        s_t, s_r = sm3.next()
                        ACTF(junk3[:], src, AF.Square, [src_r], [r_junk3, s_r], accum_out=s_t[:, 0:1])
                        TS("dve", s_t[:, 1:2], s_t[:, 0:1], 1.0 / D, EPS, ALU.mult, ALU.add, [s_r], [s_r])
                        ACTF(s_t[:, 1:2], s_t[:, 1:2], AF.Sqrt, [s_r], [s_r])
                        E("dve", lambda: nc.vector.reciprocal(out=s_t[:, 2:3], in_=s_t[:, 1:2]), [s_r], [s_r])
                        STT(dstf, src, s_t[:, 2:3], rep3[:, gi, :], ALU.mult, ALU.mult, [src_r, s_r, r_rep3], [dstf_r])

                    def tr3(srcb, srcb_r, dstT, dstT_r, nblk):
                        pt, pt_r = ptb2.next()
                        for dc in range(nblk):
                            TR(pt[:, dc * 128:(dc + 1) * 128], srcb[:, dc * 128:(dc + 1) * 128], ident_b[:], [srcb_r, r_identb], [pt_r])
                        CP("act", dstT.rearrange("p a t -> p (a t)"), pt[:, 0:nblk * 128], [pt_r], [dstT_r])

                    for it in range(NTL):
                        tsl = slice(it * 128, (it + 1) * 128)
                        Hh = Yacc[:, it, :]; H_r = rY[it]
                        x3, x3_r = x3_ring.next()
                        rms3(Hh, H_r, 0, x3[:], x3_r)
                        x3b, x3b_r = x3b_ring.next()
                        CP("pool", x3b[:], x3[:], [x3_r], [x3b_r])
                        x3T, x3T_r = x3T_ring.next()
                        tr3(x3b, x3b_r, x3T[:], x3T_r, 8)
                        ph_t, ph_r = pht.next()
                        DMA("dact", ph_t[:], ph[tsl, :], [], [ph_r])
                        pb_t, pb_r = phb.next()
                        CP("pool", pb_t[:], ph_t[:], [ph_r], [pb_r])
                        pT_t, pT_r = phT.next()
                        tr3(pb_t, pb_r, pT_t[:], pT_r, 2)
                        gt, gt_r = gt_ring.next()
                        for ch in range(2):
                            csl = slice(ch * 512, (ch + 1) * 512)
                            pa, pa_r = p1.next()
                            for dc in range(8):
                                MM(pa[:], x3T[:, dc, :], Wgt[:, dc, csl], dc == 0, dc == 7, [x3T_r, r_Wgt], [pa_r])
                            TT("dve", gt[:, csl], pa[:], rep3[:, 2, csl], ALU.add, [pa_r, r_rep3], [gt_r])
                            ACTF(gt[:, csl], gt[:, csl], AF.Sigmoid, [gt_r], [gt_r])
                            pb2, pb2_r = p2.next()
                            for dc in range(2):
                                MM(pb2[:], pT_t[:, dc, :], Wp[:, dc, csl], dc == 0, dc == 1, [pT_r, r_Wp], [pb2_r])
                            TT("dve", gt[:, csl], gt[:, csl], pb2[:], ALU.mult, [gt_r, pb2_r], [gt_r])
                            TT("pool", Yacc[:, it, csl], Yacc[:, it, csl], gt[:, csl], ALU.add, [H_r, gt_r], [H_r])
                        ot, ot_r = ot_ring.next()
                        rms3(Hh, H_r, 1, ot[:], ot_r)
                        DMA("dsp", out[tsl, :], ot[:], [ot_r], [])

        kb.drain_all()
    return nc


def _blockdiag(w):
    o = np.zeros((128, 4, 128), np.float32)
    for n in range(8):
        cc, j = n // 2, n % 2
        o[j * 64:(j + 1) * 64, cc, j * 64:(j + 1) * 64] = w[n]
    return o


def _rel_bucket(dist):
    n = np.maximum(dist, 0)
    nf = np.maximum(n, 16).astype(np.float32)
    large = 16 + (np.log(nf / np.float32(16)) / np.float32(np.log(8.0)) * np.float32(16)).astype(np.int32)
    large = np.minimum(large, 31)
    return np.where(n < 16, n, large)


def _nsa_consts(rel_table):
    c = {}
    assert (_rel_bucket(np.arange(113, 8192)) == 31).all()
    cl = np.arange(128)[:, None, None]; mp = np.arange(5)[None, :, None]; tt = np.arange(512)[None, None, :]
    dist = 512 * mp + tt - 16 * cl - 31
    c["bc_g"] = np.ascontiguousarray(rel_table[_rel_bucket(dist)].transpose(3, 0, 1, 2))
    c["bc_m"] = np.where(dist >= 0, 0.0, NEGM).astype(np.float32)
    assert (512 * 5 - 16 * 127 - 31) >= 113
    sl = np.arange(128)[:, None]; tl = np.arange(128)[None, :]
    d0 = tl - sl; d1 = 128 + tl - sl
    g0 = rel_table[_rel_bucket(d0)]; g1 = rel_table[_rel_bucket(d1)]
    c["bd_g"] = np.ascontiguousarray(np.stack([g0, g1], 0).transpose(3, 1, 0, 2))
    m0 = np.where(d0 >= 0, 0.0, NEGM); m2 = np.where(tl < sl, 0.0, NEGM)
    c["bd_m"] = np.ascontiguousarray(np.stack([m0, np.zeros_like(m0), m2], 1)).astype(np.float32)
    c["t31"] = np.ascontiguousarray(np.broadcast_to(rel_table[31][None, :], (128, 8))).astype(np.float32)
    t = (np.arange(NT)[None, :, None] * 128 + np.arange(128)[:, None, None])
    blk = np.arange(64)[None, None, :]
    d = t // 64 - blk
    local = (d >= 0) & (d < 2)
    init = (blk == 0) & ~local
    past = (d >= 0) & ~local & ~init
    c["force_c"] = np.where(local, 2.0e4, np.where(init, 1.0e4, np.where(past, 0.0, -1.0))).astype(np.float32)
    c["keep_c"] = past.astype(np.float32)
    cs = np.arange(256)[:, None] * 16; ss = np.arange(64)[None, :] * 64
    ov = np.clip(np.minimum(cs + 32, ss + 64) - np.maximum(cs, ss), 0, None).astype(np.float32) / 32.0
    ove = np.concatenate([ov, np.ones((256, 1), np.float32)], 1)
    ove[255] = 0.0
    c["ovl_ext"] = np.ascontiguousarray(ove.reshape(2, 128, 65).transpose(1, 0, 2))
    c["erows"] = (np.arange(T)[None, :] // 64 == np.arange(64)[:, None]).astype(np.float32)
    return c


def _prep_inputs(inputs):
    x = np.ascontiguousarray(inputs["x"], dtype=np.float32)
    p = np.ascontiguousarray(inputs["p"], dtype=np.float32)
    shared = {
        "ident": np.eye(128, dtype=np.float32),
        "w_in": np.ascontiguousarray(inputs["w_in"][0]),
        "g_attn": np.ascontiguousarray(inputs["attn_norm"][0].reshape(8, 128).T),
        "lru_cw": np.ascontiguousarray(inputs["conv_w"][0][:, 0, :].reshape(4, 4, 128).transpose(2, 1, 0)),
        "lru_vec": np.ascontiguousarray(np.stack([inputs[k][0].reshape(4, 128) for k in
                                                  ("conv_b", "lru_ba", "lru_bx", "lru_lambda", "grp_norm_lru")], 0).transpose(2, 0, 1)),
        "cmp_w1": np.ascontiguousarray(np.stack([inputs[k][0].reshape(16, 128, 256).transpose(1, 0, 2) for k in ("cmp_k_w1", "cmp_v_w1")], 0)),
        "cmp_pe": np.ascontiguousarray(np.stack([inputs[k][0].reshape(16, 128).T for k in ("cmp_k_pe", "cmp_v_pe")], 0)),
        "cmp_w2": np.ascontiguousarray(np.stack([inputs[k][0].reshape(2, 128, 64).transpose(1, 0, 2) for k in ("cmp_k_w2", "cmp_v_w2")], 0)),
        "ga_rep": np.ascontiguousarray(np.broadcast_to(inputs["grp_norm_attn"][0][None, :], (128, 512))).astype(np.float32),
        "w_out": np.ascontiguousarray(inputs["w_out"][0]),
        "peer_wq": np.ascontiguousarray(inputs["peer_wq"][0]),
        "sk_T": np.ascontiguousarray(inputs["peer_subkeys"][0].transpose(0, 2, 1)),
        "peer_u": np.ascontiguousarray(inputs["peer_u"][0]),
        "peer_v": np.ascontiguousarray(inputs["peer_v"][0]),
        "ple_wgate": np.ascontiguousarray(inputs["ple_wgate"][0]),
        "ple_proj": np.ascontiguousarray(inputs["ple_proj"][0]),
        "rep4": np.ascontiguousarray(np.broadcast_to(np.stack([inputs["ffn_norm"][0], inputs["ple_norm"][0], inputs["final_norm"], inputs["ple_bgate"][0]], 0)[None], (128, 4, D))).astype(np.float32),
        "iota128": np.ascontiguousarray(np.broadcast_to(np.arange(128, dtype=np.float32)[None], (128, 128))),
        "iota16": np.ascontiguousarray(np.broadcast_to(np.arange(16, dtype=np.float32)[None], (128, 16))),
        "lru_bda": _blockdiag(inputs["lru_wa"][0]),
        "lru_bdx": _blockdiag(inputs["lru_wx"][0]),
    }
    shared.update(_nsa_consts(np.asarray(inputs["rel_table"], np.float32)))
    in_maps = []
    for c in range(8):
        b, hf = c // 2, c % 2
        m = dict(shared)
        m["xb"] = x[b]
        m["xh"] = np.ascontiguousarray(x[b, hf * TH:(hf + 1) * TH])
        m["ph"] = np.ascontiguousarray(p[0, b, hf * TH:(hf + 1) * TH])
        sel = np.zeros((128, 2), np.float32); sel[:, hf] = 1.0
        m["selc"] = sel
        in_maps.append(m)
    return in_maps


def kernel(**inputs):
    nc = build_program()
    in_maps = _prep_inputs(inputs)
    res = run_bass_kernel_spmd(nc, in_maps, core_ids=list(range(8)))
    outp = np.zeros((4, T, D), np.float32)
    for c in range(8):
        b, hf = c // 2, c % 2
        outp[b, hf * TH:(hf + 1) * TH] = res.results[c]["out"]
    return outp
```

```python
import numpy as np
from contextlib import ExitStack
import concourse.bass as bass
import concourse.mybir as mybir
from concourse.bass_utils import run_bass_kernel_spmd

F32 = mybir.dt.float32
BF16 = mybir.dt.bfloat16
U32 = mybir.dt.uint32
AF = mybir.ActivationFunctionType
ALU = mybir.AluOpType
AX = mybir.AxisListType

T = 4096
D = 1024
NT = T // 128
TH = 2048
IN_COLS = 2328
EPS = 1e-6
NEGM = -30000.0


class Res:
    __slots__ = ("name", "lw", "rd")

    def __init__(self, name=""):
        self.name = name
        self.lw = None
        self.rd = {}


class KB:
    NDMA = 4

    def __init__(self, nc, stack):
        self.nc = nc
        self.issue = {"pe": nc.tensor, "act": nc.scalar, "dve": nc.vector, "pool": nc.gpsimd,
                      "dsp": nc.sync, "dact": nc.scalar, "dpool": nc.gpsimd}
        self.stream = {"pe": "pe", "act": "act", "dve": "dve", "pool": "pool",
                       "dsp": "sp", "dact": "act", "dpool": "pool"}
        self.sems = {}
        self.cnt = {}
        for q in self.issue:
            n = self.NDMA if self.is_dma(q) else 1
            self.sems[q] = [stack.enter_context(nc.semaphore(f"s_{q}{i}")) for i in range(n)]
            self.cnt[q] = 0
        self.waited = {s: {} for s in ("pe", "act", "dve", "pool", "sp")}
        self.ninst = 0
        self._rr = 0

    @staticmethod
    def is_dma(q):
        return q in ("dsp", "dact", "dpool")

    @staticmethod
    def _need(need, dep):
        if dep is None:
            return
        q, c = dep
        if need.get(q, 0) < c:
            need[q] = c

    def _waits(self, st, eng, need, skip_q=None):
        for dq, c in need.items():
            if dq == "pe" and skip_q == "pe":
                continue
            if self.is_dma(dq):
                n = self.NDMA
                for si in range(n):
                    k = (c - 1 - si) // n + 1 if c - 1 >= si else 0
                    if k <= 0:
                        continue
                    key = (dq, si)
                    if self.waited[st].get(key, 0) >= k:
                        continue
                    eng.wait_ge(self.sems[dq][si], 16 * k)
                    self.waited[st][key] = k
            else:
                key = (dq, 0)
                if self.waited[st].get(key, 0) >= c:
                    continue
                eng.wait_ge(self.sems[dq][0], c)
                self.waited[st][key] = c

    def emit(self, q, fn, reads=(), writes=()):
        need = {}
        for r in reads:
            self._need(need, r.lw)
        for w in writes:
            self._need(need, w.lw)
            for rq, rc in w.rd.items():
                self._need(need, (rq, rc))
        st = self.stream[q]
        self._waits(st, self.issue[q], need, skip_q=q)
        inst = fn()
        self.cnt[q] += 1
        c = self.cnt[q]
        if self.is_dma(q):
            inst.then_inc(self.sems[q][(c - 1) % self.NDMA], 16)
        else:
            inst.then_inc(self.sems[q][0], 1)
        for r in reads:
            if r.rd.get(q, 0) < c:
                r.rd[q] = c
        for w in writes:
            w.lw = (q, c)
            w.rd = {}
        self.ninst += 1
        return inst

    def dmaq(self):
        self._rr ^= 1
        return "dsp" if self._rr else "dact"

    def barrier(self):
        need = {q: c for q, c in self.cnt.items() if c > 0}
        for st, eng in (("pe", self.nc.tensor), ("act", self.nc.scalar), ("dve", self.nc.vector),
                        ("pool", self.nc.gpsimd), ("sp", self.nc.sync)):
            self._waits(st, eng, dict(need))

    def drain_all(self):
        need = {q: c for q, c in self.cnt.items() if c > 0}
        self._waits("sp", self.nc.sync, need)


class Scope:
    def __init__(self, kb):
        self.kb = kb
        self.st = ExitStack()

    def __enter__(self):
        self.st.__enter__()
        return self.st

    def __exit__(self, *a):
        if a[0] is None:
            self.kb.barrier()
        return self.st.__exit__(*a)


class Ring:
    def __init__(self, tiles):
        self.tiles = tiles
        self.res = [Res() for _ in tiles]
        self.i = -1

    def next(self):
        self.i = (self.i + 1) % len(self.tiles)
        return self.tiles[self.i], self.res[self.i]


def build_program(dbg=None, phases=("A", "B", "C", "D")):
    nc = bass.Bass("TRN2", target_bir_lowering=False)

    def din(name, shape, dt=F32):
        return nc.dram_tensor(name, list(shape), dt, kind="ExternalInput").ap()

    dbg = dbg or ()

    def dscr(name, shape, dt):
        kind = "ExternalOutput" if name in dbg else "Internal"
        return nc.dram_tensor(name, list(shape), dt, kind=kind).ap()

    xb = din("xb", [T, D])
    xh = din("xh", [TH, D])
    ph = din("ph", [TH, 256])
    selc = din("selc", [128, 2])
    ident = din("ident", [128, 128])
    w_in = din("w_in", [D, IN_COLS])
    g_attn = din("g_attn", [128, 8])
    out = nc.dram_tensor("out", [TH, D], F32, kind="ExternalOutput").ap()
    lru_cw = din("lru_cw", [128, 4, 4])
    lru_vec = din("lru_vec", [128, 5, 4])
    lru_bda = din("lru_bda", [128, 4, 128])
    lru_bdx = din("lru_bdx", [128, 4, 128])

    qT_s = dscr("qT_s", [512, T], BF16)
    kcT_s = dscr("kcT_s", [128, T], BF16)
    vcT_s = dscr("vcT_s", [128, T], BF16)
    ksT_s = dscr("ksT_s", [128, T], BF16)
    kwT_s = dscr("kwT_s", [128, T], BF16)
    vs_s = dscr("vs_s", [T, 128], BF16)
    vw_s = dscr("vw_s", [T, 128], BF16)
    gates_s = dscr("gates_s", [T, 24], F32)
    xrT_s = dscr("xrT_s", [512, T], F32)
    xgT_s = dscr("xgT_s", [512, T], F32)

    cmp_w1 = din("cmp_w1", [2, 128, 16, 256])
    cmp_pe = din("cmp_pe", [2, 128, 16])
    cmp_w2 = din("cmp_w2", [2, 128, 2, 64])
    ovl_ext = din("ovl_ext", [128, 2, 65])
    bc_g = din("bc_g", [8, 128, 5, 512])
    bc_m = din("bc_m", [128, 5, 512])
    bd_g = din("bd_g", [8, 128, 2, 128])
    bd_m = din("bd_m", [128, 3, 128])
    t31_in = din("t31", [128, 8])
    force_in = din("force_c", [128, 32, 64])
    keep_in = din("keep_c", [128, 32, 64])
    erows = din("erows", [64, T])
    ga_in = din("ga_rep", [128, 512])
    w_out_in = din("w_out", [D, D])
    peer_wq = din("peer_wq", [D, 2048])
    sk_T = din("sk_T", [2, 128, 128])
    peer_u = din("peer_u", [16384, D])
    peer_v = din("peer_v", [16384, D])
    ple_wg = din("ple_wgate", [D, D])
    ple_pj = din("ple_proj", [256, D])
    rep4 = din("rep4", [128, 4, D])
    iota16 = din("iota16", [128, 16])
    iota128 = din("iota128", [128, 128])
    H1_s = dscr("H1_s", [TH, D], F32)
    xnT2_s = dscr("xnT2_s", [128, 8, TH], BF16)
    Wt_s = dscr("Wt_s", [TH // 128, 128, 128, 128], BF16)
    mixT_s = dscr("mixT_s", [1024, T], BF16)
    R = {n: Res(n) for n in ("H1_s", "xnT2_s", "Wt_s", "mixT_s", "qT_s", "kcT_s", "vcT_s", "ksT_s", "kwT_s", "vs_s", "vw_s", "gates_s", "xrT_s", "xgT_s")}

    with ExitStack() as top:
        kb = KB(nc, top)
        E = kb.emit

        uniq = [0]

        def sb(st, name, shape, dt):
            uniq[0] += 1
            return st.enter_context(nc.sbuf_tensor(f"sb{uniq[0]}_{name}", list(shape), dt))

        def ps(st, name, shape, dt):
            uniq[0] += 1
            return st.enter_context(nc.psum_tensor(f"ps{uniq[0]}_{name}", list(shape), dt))


        def MM(out_, lhsT, rhs, start, stop, reads, writes):
            return E("pe", lambda: nc.tensor.matmul(out_, lhsT=lhsT, rhs=rhs, start=start, stop=stop), reads, writes)

        def TR(out_, in_, idt, reads, writes):
            return E("pe", lambda: nc.tensor.transpose(out=out_, in_=in_, identity=idt), reads, writes)

        def ACTF(out_, in_, func, reads, writes, **kw):
            return E("act", lambda: nc.scalar.activation(out=out_, in_=in_, func=func, **kw), reads, writes)

        def veng(q):
            return nc.vector if q == "dve" else nc.gpsimd

        def TS(q, out_, in0, s1, s2, op0, op1, reads, writes):
            if op1 is None:
                return E(q, lambda: veng(q).tensor_scalar(out=out_, in0=in0, scalar1=s1, scalar2=None, op0=op0), reads, writes)
            return E(q, lambda: veng(q).tensor_scalar(out=out_, in0=in0, scalar1=s1, scalar2=s2, op0=op0, op1=op1), reads, writes)

        def TT(q, out_, in0, in1, op, reads, writes):
            return E(q, lambda: veng(q).tensor_tensor(out=out_, in0=in0, in1=in1, op=op), reads, writes)

        def STT(out_, in0, scalar, in1, op0, op1, reads, writes, **kw):
            return E("dve", lambda: nc.vector.scalar_tensor_tensor(out=out_, in0=in0, scalar=scalar, in1=in1, op0=op0, op1=op1, **kw), reads, writes)

        def CP(q, out_, in_, reads, writes):
            if q == "act":
                return E("act", lambda: nc.scalar.copy(out=out_, in_=in_), reads, writes)
            return E(q, lambda: veng(q).tensor_copy(out=out_, in_=in_), reads, writes)

        def MSET(q, out_, val, writes):
            return E(q, lambda: veng(q).memset(out_, val), (), writes)

        def DMA(q, out_, in_, reads, writes):
            eng = {"dsp": nc.sync, "dact": nc.scalar, "dpool": nc.gpsimd}[q]
            return E(q, lambda: eng.dma_start(out=out_, in_=in_), reads, writes)

        def drive_rr(gens):
            res = [None] * len(gens)
            live = list(range(len(gens)))
            while live:
                for gi in list(live):
                    try:
                        next(gens[gi])
                    except StopIteration as e:
                        res[gi] = e.value
                        live.remove(gi)
            return res

        def dump(name, ap, shape, dt, res):
            if name not in dbg:
                return
            d = nc.dram_tensor(name, list(shape), dt, kind="ExternalOutput").ap()
            DMA("dsp", d, ap, [res] if not isinstance(res, list) else res, [])

        ident_f = sb(top, "ident_f", [128, 128], F32); r_identf = Res()
        ident_b = sb(top, "ident_b", [128, 128], BF16); r_identb = Res()
        E("dsp", lambda: nc.sync.dma_start(out=ident_f[:], in_=ident), writes=[r_identf])
        E("dve", lambda: nc.vector.tensor_copy(out=ident_b[:], in_=ident_f[:]), reads=[r_identf], writes=[r_identb])

        if "A" in phases:
            with Scope(kb) as st:
                Wg = sb(st, "Wg", [128, 8, IN_COLS], BF16); r_Wg = Res()
                gcol = sb(st, "gcol", [128, 8], F32); r_gcol = Res()
                wst = Ring([sb(st, f"wst{i}", [128, IN_COLS], F32) for i in range(2)])
                E("dsp", lambda: nc.sync.dma_start(out=gcol[:], in_=g_attn), writes=[r_gcol])
                for dc in range(8):
                    w_t, w_r = wst.next()
                    E("dsp" if dc % 2 == 0 else "dact",
                      (lambda w_t=w_t, dc=dc: nc.sync.dma_start(out=w_t[:], in_=w_in[dc * 128:(dc + 1) * 128, :])) if dc % 2 == 0 else
                      (lambda w_t=w_t, dc=dc: nc.scalar.dma_start(out=w_t[:], in_=w_in[dc * 128:(dc + 1) * 128, :])),
                      writes=[w_r])
                    eng = "dve" if dc % 2 == 0 else "pool"
                    ve = nc.vector if dc % 2 == 0 else nc.gpsimd
                    E(eng, lambda ve=ve, w_t=w_t, dc=dc: ve.tensor_scalar(out=Wg[:, dc, :], in0=w_t[:], scalar1=gcol[:, dc:dc + 1], scalar2=None, op0=ALU.mult),
                      reads=[w_r, r_gcol], writes=[r_Wg])

                xt_ring = Ring([sb(st, f"xt{i}", [128, 4, D], F32) for i in range(2)])
                xnb_ring = Ring([sb(st, f"xnb{i}", [128, 4, D], BF16) for i in range(2)])
                xnT_ring = Ring([sb(st, f"xnT{i}", [128, 8, 512], BF16) for i in range(2)])
                junk = sb(st, "junkA", [128, D], BF16); r_junk = Res()
                ss_ring = Ring([sb(st, f"ss{i}", [128, 8], F32) for i in range(2)])
                pT_ring = Ring([ps(st, f"pT{i}", [128, 512], BF16) for i in range(2)])
                pacc = Ring([ps(st, f"pacc{i}", [128, 512], F32) for i in range(4)])
                ostf = Ring([sb(st, f"ostf{i}", [128, 512], F32) for i in range(3)])
                ostb = Ring([sb(st, f"ostb{i}", [128, 512], BF16) for i in range(3)])
                osv = Ring([sb(st, f"osv{i}", [128, 256], BF16) for i in range(2)])
                osg = Ring([sb(st, f"osg{i}", [128, 24], F32) for i in range(2)])
                xb_v = xb.rearrange("(n p) d -> p n d", p=128)
                fm = []
                for cc in range(4):
                    fm.append((cc * 128, qT_s[cc * 128:(cc + 1) * 128, :], 0.125, True, R["qT_s"]))
                fm.append((512, kcT_s, 1.0, True, R["kcT_s"]))
                fm.append((640, vcT_s, 1.0, True, R["vcT_s"]))
                fm.append((768, ksT_s, 1.0, True, R["ksT_s"]))
                fm.append((1024, kwT_s, 1.0, True, R["kwT_s"]))
                for cc in range(4):
                    fm.append((1304 + cc * 128, xrT_s[cc * 128:(cc + 1) * 128, :], 1.0, False, R["xrT_s"]))
                for cc in range(4):
                    fm.append((1816 + cc * 128, xgT_s[cc * 128:(cc + 1) * 128, :], 1.0, False, R["xgT_s"]))
                evc = [0]

                def stageP(tcn):
                    xt, xt_r = xt_ring.next()
                    DMA("dsp", xt[:], xb_v[:, tcn * 4:(tcn + 1) * 4, :], [], [xt_r])
                    ss, ss_r = ss_ring.next()
                    for n in range(4):
                        ACTF(junk[:], xt[:, n, :], AF.Square, [xt_r], [r_junk, ss_r], accum_out=ss[:, n:n + 1])
                    yield
                    TS("dve", ss[:, 4:8], ss[:, 0:4], 1.0 / D, EPS, ALU.mult, ALU.add, [ss_r], [ss_r])
                    ACTF(ss[:, 4:8], ss[:, 4:8], AF.Sqrt, [ss_r], [ss_r])
                    yield
                    E("dve", lambda: nc.vector.reciprocal(out=ss[:, 4:8], in_=ss[:, 4:8]), [ss_r], [ss_r])
                    xnb, xnb_r = xnb_ring.next()
                    for n in range(4):
                        if n % 2 == 0:
                            TS("dve", xnb[:, n, :], xt[:, n, :], ss[:, 4 + n:5 + n], None, ALU.mult, None, [xt_r, ss_r], [xnb_r])
                        else:
                            ACTF(xnb[:, n, :], xt[:, n, :], AF.Copy, [xt_r, ss_r], [xnb_r], scale=ss[:, 4 + n:5 + n])
                    yield
                    xnT, xnT_r = xnT_ring.next()
                    for dc in range(8):
                        pT, pT_r = pT_ring.next()
                        for n in range(4):
                            TR(pT[:, n * 128:(n + 1) * 128], xnb[:, n, dc * 128:(dc + 1) * 128], ident_b[:], [xnb_r, r_identb], [pT_r])
                        CP("act" if dc % 2 == 0 else "dve", xnT[:, dc, :], pT[:], [pT_r], [xnT_r])
                        if dc % 2 == 1:
                            yield
                    return (xnT, xnT_r)

                def stageM(tcn, st):
                    xnT, xnT_r = st
                    for (c0, dst, scale, isb, dres) in fm:
                        pa, pa_r = pacc.next()
                        for dc in range(8):
                            MM(pa[:], Wg[:, dc, c0:c0 + 128], xnT[:, dc, :], dc == 0, dc == 7, [r_Wg, xnT_r], [pa_r])
                        o_t, o_r = (ostb if isb else ostf).next()
                        evc[0] += 1
                        if evc[0] % 2 == 0:
                            ACTF(o_t[:], pa[:], AF.Copy, [pa_r], [o_r], scale=scale)
                        else:
                            TS("dve", o_t[:], pa[:], scale, None, ALU.mult, None, [pa_r], [o_r])
                        DMA("dsp" if evc[0] % 2 == 0 else "dpool", dst[:, tcn * 512:(tcn + 1) * 512], o_t[:], [o_r], [dres])
                        yield
                    for n in range(4):
                        t0 = tcn * 512 + n * 128
                        pa, pa_r = pacc.next()
                        for dc in range(8):
                            MM(pa[:, 0:128], xnT[:, dc, n * 128:(n + 1) * 128], Wg[:, dc, 896:1024], dc == 0, dc == 7, [r_Wg, xnT_r], [pa_r])
                        pb, pb_r = pacc.next()
                        for dc in range(8):
                            MM(pb[:, 0:152], xnT[:, dc, n * 128:(n + 1) * 128], Wg[:, dc, 1152:1304], dc == 0, dc == 7, [r_Wg, xnT_r], [pb_r])
                        ov, ov_r = osv.next()
                        og, og_r = osg.next()
                        CP("act", ov[:, 0:128], pa[:, 0:128], [pa_r], [ov_r])
                        CP("dve", ov[:, 128:256], pb[:, 0:128], [pb_r], [ov_r])
                        CP("dve", og[:], pb[:, 128:152], [pb_r], [og_r])
                        DMA("dsp", vs_s[t0:t0 + 128, :], ov[:, 0:128], [ov_r], [R["vs_s"]])
                        DMA("dpool", vw_s[t0:t0 + 128, :], ov[:, 128:256], [ov_r], [R["vw_s"]])
                        DMA("dsp", gates_s[t0:t0 + 128, :], og[:], [og_r], [R["gates_s"]])
                        yield

                stP = drive_rr([stageP(0)])[0]
                for tcn in range(8):
                    gens = [stageM(tcn, stP)]
                    if tcn + 1 < 8:
                        gens.append(stageP(tcn + 1))
                    rr = drive_rr(gens)
                    if tcn + 1 < 8:
                        stP = rr[1]

        if "B" in phases:
            with Scope(kb) as st:
                cw = sb(st, "cw", [128, 4, 4], F32); r_cw = Res()
                lv = sb(st, "lv", [128, 5, 4], F32); r_lv = Res()
                clc = sb(st, "clc", [128, 3, 4], F32); r_clc = Res()
                bdf = sb(st, "bdf", [128, 2, 4, 128], F32); r_bdf = Res()
                bdb = sb(st, "bdb", [128, 2, 4, 128], BF16); r_bdb = Res()
                ones_b = sb(st, "ones_b", [128, 128], BF16); r_ones = Res()
                E("dsp", lambda: nc.sync.dma_start(out=cw[:], in_=lru_cw), writes=[r_cw])
                E("dact", lambda: nc.scalar.dma_start(out=lv[:], in_=lru_vec), writes=[r_lv])
                E("dsp", lambda: nc.sync.dma_start(out=bdf[:, 0], in_=lru_bda), writes=[r_bdf])
                E("dact", lambda: nc.scalar.dma_start(out=bdf[:, 1], in_=lru_bdx), writes=[r_bdf])
                E("dve", lambda: nc.vector.tensor_copy(out=bdb[:], in_=bdf[:]), reads=[r_bdf], writes=[r_bdb])
                E("dve", lambda: nc.vector.memset(ones_b[:], 1.0), writes=[r_ones])
                E("act", lambda: nc.scalar.activation(out=clc[:, 0, :], in_=lv[:, 3, :], func=AF.Exp, scale=-1.0), reads=[r_lv], writes=[r_clc])
                E("act", lambda: nc.scalar.activation(out=clc[:, 0, :], in_=clc[:, 0, :], func=AF.Ln, bias=1.0), reads=[r_clc], writes=[r_clc])
                E("dve", lambda: nc.vector.tensor_scalar(out=clc[:, 1, :], in0=clc[:, 0, :], scalar1=-8.0, scalar2=None, op0=ALU.mult), reads=[r_clc], writes=[r_clc])
                E("dve", lambda: nc.vector.tensor_scalar(out=clc[:, 2, :], in0=clc[:, 0, :], scalar1=-16.0, scalar2=None, op0=ALU.mult), reads=[r_clc], writes=[r_clc])
                L = sb(st, "Lall", [128, 4, T], F32); r_L = Res()
                X = [sb(st, f"lruX{i}", [128, T], F32) for i in range(5)]
                rX = [Res() for _ in range(5)]
                xcb = sb(st, "xcb", [128, T], BF16); r_xcb = Res()
                pg = Ring([ps(st, f"pg{i}", [128, 512], F32) for i in range(4)])
                for cc in range(4):
                    X1, X2, X3, X4, X5 = X
                    r1, r2, r3, r4, r5 = rX
                    for hh in range(2):
                        E("dsp", lambda cc=cc, hh=hh: nc.sync.dma_start(out=X1[:, hh * 2048:(hh + 1) * 2048], in_=xrT_s[cc * 128:(cc + 1) * 128, hh * 2048:(hh + 1) * 2048]), reads=[R["xrT_s"]], writes=[r1])
                        E("dact", lambda cc=cc, hh=hh: nc.scalar.dma_start(out=X3[:, hh * 2048:(hh + 1) * 2048], in_=xgT_s[cc * 128:(cc + 1) * 128, hh * 2048:(hh + 1) * 2048]), reads=[R["xgT_s"]], writes=[r3])
                    E("dve", lambda cc=cc: nc.vector.tensor_scalar(out=X2[:], in0=X1[:], scalar1=cw[:, cc, 3:4], scalar2=lv[:, 0, cc:cc + 1], op0=ALU.mult, op1=ALU.add), reads=[r1, r_cw, r_lv], writes=[r2])
                    for sh in (1, 2, 3):
                        E("dve", lambda cc=cc, sh=sh: nc.vector.scalar_tensor_tensor(out=X2[:, sh:T], in0=X1[:, 0:T - sh], scalar=cw[:, cc, 3 - sh:4 - sh], in1=X2[:, sh:T], op0=ALU.mult, op1=ALU.add), reads=[r1, r2, r_cw], writes=[r2])
                    E("pool", lambda: nc.gpsimd.tensor_copy(out=xcb[:], in_=X2[:]), reads=[r2], writes=[r_xcb])
                    for gi, (Xo, ro, bi) in enumerate(((X4, r4, 1), (X5, r5, 2))):
                        for tcn in range(8):
                            pgt, pg_r = pg.next()
                            E("pe", lambda pgt=pgt, gi=gi, cc=cc, tcn=tcn: nc.tensor.matmul(pgt[:], lhsT=bdb[:, gi, cc, :], rhs=xcb[:, tcn * 512:(tcn + 1) * 512], start=True, stop=True), reads=[r_bdb, r_xcb], writes=[pg_r])
                            E("act", lambda pgt=pgt, Xo=Xo, bi=bi, cc=cc, tcn=tcn: nc.scalar.activation(out=Xo[:, tcn * 512:(tcn + 1) * 512], in_=pgt[:], func=AF.Sigmoid, bias=lv[:, bi, cc:cc + 1]), reads=[pg_r, r_lv], writes=[ro])
                    E("act", lambda cc=cc: nc.scalar.activation(out=X1[:], in_=X4[:], func=AF.Exp, scale=clc[:, 1, cc:cc + 1]), reads=[r4, r_clc], writes=[r1])
                    E("act", lambda cc=cc: nc.scalar.activation(out=X4[:], in_=X4[:], func=AF.Exp, scale=clc[:, 2, cc:cc + 1]), reads=[r4, r_clc], writes=[r4])
                    E("act", lambda: nc.scalar.activation(out=X4[:], in_=X4[:], func=AF.Sqrt, scale=-1.0, bias=1.0), reads=[r4], writes=[r4])
                    E("pool", lambda: nc.gpsimd.tensor_tensor(out=X5[:], in0=X5[:], in1=X2[:], op=ALU.mult), reads=[r5, r2], writes=[r5])
                    E("dve", lambda: nc.vector.tensor_tensor(out=X4[:], in0=X4[:], in1=X5[:], op=ALU.mult), reads=[r4, r5], writes=[r4])
                    E("dve", lambda: nc.vector.tensor_tensor_scan(out=X2[:], data0=X1[:], data1=X4[:], initial=0.0, op0=ALU.mult, op1=ALU.add), reads=[r1, r4], writes=[r2])
                    E("act", lambda: nc.scalar.activation(out=X3[:], in_=X3[:], func=AF.Gelu_apprx_tanh), reads=[r3], writes=[r3])
                    E("pool", lambda cc=cc: nc.gpsimd.tensor_tensor(out=L[:, cc, :], in0=X2[:], in1=X3[:], op=ALU.mult), reads=[r2, r3], writes=[r_L])
                sq = Ring([sb(st, f"lsq{i}", [128, 512], BF16) for i in range(2)])
                rs_ring = Ring([sb(st, f"lrs{i}", [128, 512], F32) for i in range(2)])
                lo = Ring([sb(st, f"lo{i}", [128, 512], BF16) for i in range(3)])
                for tcn in range(8):
                    pgt, pg_r = pg.next()
                    for cc in range(4):
                        sq_t, sq_r = sq.next()
                        E("act", lambda sq_t=sq_t, cc=cc, tcn=tcn: nc.scalar.activation(out=sq_t[:], in_=L[:, cc, tcn * 512:(tcn + 1) * 512], func=AF.Square), reads=[r_L], writes=[sq_r])
                        E("pe", lambda pgt=pgt, sq_t=sq_t, cc=cc: nc.tensor.matmul(pgt[:], lhsT=ones_b[:], rhs=sq_t[:], start=(cc == 0), stop=(cc == 3)), reads=[r_ones, sq_r], writes=[pg_r])
                    rs_t, rs_r = rs_ring.next()
                    E("dve", lambda rs_t=rs_t, pgt=pgt: nc.vector.tensor_scalar(out=rs_t[:], in0=pgt[:], scalar1=1.0 / 512, scalar2=EPS, op0=ALU.mult, op1=ALU.add), reads=[pg_r], writes=[rs_r])
                    E("act", lambda rs_t=rs_t: nc.scalar.activation(out=rs_t[:], in_=rs_t[:], func=AF.Sqrt), reads=[rs_r], writes=[rs_r])
                    E("dve", lambda rs_t=rs_t: nc.vector.reciprocal(out=rs_t[:], in_=rs_t[:]), reads=[rs_r], writes=[rs_r])
                    for cc in range(4):
                        lo_t, lo_r = lo.next()
                        E("dve", lambda lo_t=lo_t, rs_t=rs_t, cc=cc, tcn=tcn: nc.vector.scalar_tensor_tensor(out=lo_t[:], in0=L[:, cc, tcn * 512:(tcn + 1) * 512], scalar=lv[:, 4, cc:cc + 1], in1=rs_t[:], op0=ALU.mult, op1=ALU.mult), reads=[r_L, rs_r, r_lv], writes=[lo_r])
                        E("dsp", lambda lo_t=lo_t, cc=cc, tcn=tcn: nc.sync.dma_start(out=mixT_s[512 + cc * 128:512 + (cc + 1) * 128, tcn * 512:(tcn + 1) * 512], in_=lo_t[:]), reads=[lo_r], writes=[R["mixT_s"]])

        if "C" in phases:
            with Scope(kb) as st:
                Aout = sb(st, "Aout", [128, NT, 512], BF16)
                rA = [Res() for _ in range(NT)]
                sig = sb(st, "sig", [128, NT, 24], F32); r_sig = Res()
                force_t = sb(st, "force_t", [128, NT, 64], F32); r_force = Res()
                keep_t = sb(st, "keep_t", [128, NT, 64], F32); r_keep = Res()
                t31 = sb(st, "t31", [128, 8], F32); r_t31 = Res()
                BD = sb(st, "BD", [128, 8, 3, 128], BF16); r_BD = Res()
                ovl_t = sb(st, "ovl_t", [128, 2, 65], F32); r_ovl = Res()
                ga_t = sb(st, "ga_t", [128, 512], F32); r_ga = Res()
                bcm = sb(st, "bcm", [128, 5, 512], F32); r_bcm = Res()
                DMA("dsp", sig[:], gates_s.rearrange("(n p) c -> p n c", p=128), [R["gates_s"]], [r_sig])
                ACTF(sig[:], sig[:], AF.Sigmoid, [r_sig], [r_sig])
                DMA("dact", force_t[:], force_in, [], [r_force])
                DMA("dsp", keep_t[:], keep_in, [], [r_keep])
                DMA("dact", t31[:], t31_in, [], [r_t31])
                DMA("dsp", ovl_t[:], ovl_ext, [], [r_ovl])
                DMA("dact", ga_t[:], ga_in, [], [r_ga])
                DMA("dsp", bcm[:], bc_m, [], [r_bcm])
                psb = [ps(st, f"pC{i}", [128, 512], F32) for i in range(8)]
                pS = Ring(psb[0:3])
                pO = psb[3:7]; r_pO = [Res() for _ in range(4)]
                pX = Ring(psb[7:8])
                with Scope(kb) as st2:
                    bdg = sb(st2, "bdg", [128, 8, 2, 128], F32); r_bdg = Res()
                    bdm = sb(st2, "bdm", [128, 3, 128], F32); r_bdm = Res()
                    DMA("dsp", bdg[:], bd_g.rearrange("h p j t -> p h j t"), [], [r_bdg])
                    DMA("dact", bdm[:], bd_m, [], [r_bdm])
                    for hg in range(8):
                        for j in range(2):
                            STT(BD[:, hg, j, :], bdg[:, hg, j, :], t31[:, hg:hg + 1], bdm[:, j, :], ALU.subtract, ALU.add, [r_bdg, r_bdm, r_t31], [r_BD])
                        CP("dve", BD[:, hg, 2, :], bdm[:, 2, :], [r_bdm], [r_BD])
                P_ring = Ring([sb(st, f"Pt{i}", [128, 512], BF16) for i in range(5)])
                sm = Ring([sb(st, f"smC{i}", [128, 8], F32) for i in range(8)])
                osb = Ring([sb(st, f"osb{i}", [128, 132], F32) for i in range(8)])

                def finish_tiles(items, ncol, hg, br, first, imp_first=None):
                    sts = [sm.next() for _ in items]
                    for (po, po_r, i, _, _), (s_t, s_r) in zip(items, sts):
                        TS("dve", s_t[:, 0:1], po[:, ncol:ncol + 1], 1e-30, None, ALU.max, None, [po_r], [s_r])
                    for (po, po_r, i, _, _), (s_t, s_r) in zip(items, sts):
                        E("dve", lambda: nc.vector.reciprocal(out=s_t[:, 1:2], in_=s_t[:, 0:1]), [s_r], [s_r])
                    for (po, po_r, i, _, _), (s_t, s_r) in zip(items, sts):
                        TT("dve", s_t[:, 2:3], s_t[:, 1:2], sig[:, i, hg * 3 + br:hg * 3 + br + 1], ALU.mult, [s_r, r_sig], [s_r])
                    for (po, po_r, i, _, _), (s_t, s_r) in zip(items, sts):
                        dst = Aout[:, i, hg * 64:(hg + 1) * 64]
                        if first:
                            TS("dve", dst, po[:, 0:64], s_t[:, 2:3], None, ALU.mult, None, [po_r, s_r], [rA[i]])
                        else:
                            STT(dst, po[:, 0:64], s_t[:, 2:3], dst, ALU.mult, ALU.add, [po_r, s_r, rA[i]], [rA[i]])
                    if imp_first is not None:
                        for (po, po_r, i, imp_t, imp_r), (s_t, s_r) in zip(items, sts):
                            if imp_first:
                                TS("dve", imp_t, po[:, 64:128], s_t[:, 1:2], None, ALU.mult, None, [po_r, s_r], [imp_r])
                            else:
                                STT(imp_t, po[:, 64:128], s_t[:, 1:2], imp_t, ALU.mult, ALU.add, [po_r, s_r, imp_r], [imp_r])

                for k in range(2):
                    with Scope(kb) as stg:
                        KcmpT = sb(stg, "KcmpT", [64, 256], BF16); r_Kc = Res()
                        Vco = sb(stg, "Vco", [128, 2, 129], BF16); r_Vco = Res()
                        with Scope(kb) as stc:
                            w1s = Ring([sb(stc, f"w1s{i}", [128, 8, 256], F32) for i in range(2)])
                            w1b = sb(stc, "w1b", [128, 2, 16, 256], BF16); r_w1b = Res()
                            pes = sb(stc, "pes", [128, 2, 16], F32); r_pes = Res()
                            peb = sb(stc, "peb", [128, 2, 16], BF16); r_peb = Res()
                            w2s = sb(stc, "w2s", [128, 2, 2, 64], F32); r_w2s = Res()
                            w2b = sb(stc, "w2b", [128, 2, 2, 64], BF16); r_w2b = Res()
                            stk = sb(stc, "stk", [128, 2, T], BF16); r_stk = Res()
                            hb = sb(stc, "hb", [128, 4], F32); r_hb = Res()
                            gh = sb(stc, "gh", [128, 2, 2, 256], BF16); r_gh = Res()
                            for kv in range(2):
                                for hh in range(2):
                                    w_t, w_r = w1s.next()
                                    DMA("dsp" if hh == 0 else "dact", w_t[:], cmp_w1[kv, :, hh * 8:(hh + 1) * 8, :], [], [w_r])
                                    CP("pool" if hh == 0 else "dve", w1b[:, kv, hh * 8:(hh + 1) * 8, :], w_t[:], [w_r], [r_w1b])
                                DMA("dsp", pes[:, kv, :], cmp_pe[kv], [], [r_pes])
                                DMA("dact", w2s[:, kv], cmp_w2[kv], [], [r_w2s])
                                src = kcT_s if kv == 0 else vcT_s
                                sres = R["kcT_s"] if kv == 0 else R["vcT_s"]
                                DMA("dsp", stk[0:64, kv, :], src[k * 64:(k + 1) * 64, :], [sres], [r_stk])
                                MSET("pool", stk[64:128, kv, T - 1:T], 0.0, [r_stk])
                                DMA("dact", stk[64:128, kv, 0:T - 1], src[k * 64:(k + 1) * 64, 1:T], [sres], [r_stk])
                            CP("dve", peb[:], pes[:], [r_pes], [r_peb])
                            CP("dve", w2b[:], w2s[:], [r_w2s], [r_w2b])
                            MSET("pool", gh[:], 0.0, [r_gh])
                            for kv in range(2):
                                for hh in range(2):
                                    px, px_r = pX.next()
                                    for m in range(16):
                                        MM(px[:, 0:1], w1b[:, kv, m, hh * 128:(hh + 1) * 128], peb[:, kv, m:m + 1], m == 0, m == 15, [r_w1b, r_peb], [px_r])
                                    CP("dve", hb[:, kv * 2 + hh:kv * 2 + hh + 1], px[:, 0:1], [px_r], [r_hb])
                                    p_s, p_r = pS.next()
                                    for m in range(16):
                                        MM(p_s[:, 0:255], w1b[:, kv, m, hh * 128:(hh + 1) * 128], stk[:, kv, 2 * m:2 * m + 16 * 254 + 1:16], m == 0, m == 15, [r_w1b, r_stk], [p_r])
                                    ACTF(gh[:, kv, hh, 0:255], p_s[:, 0:255], AF.Gelu_apprx_tanh, [p_r, r_hb], [r_gh], bias=hb[:, kv * 2 + hh:kv * 2 + hh + 1])
                            px, px_r = pX.next()
                            for hh in range(2):
                                MM(px[0:64, 0:256], w2b[:, 0, hh, :], gh[:, 0, hh, :], hh == 0, hh == 1, [r_w2b, r_gh], [px_r])
                            CP("dve", KcmpT[:], px[0:64, 0:256], [px_r], [r_Kc])
                            for ct in range(2):
                                px, px_r = pX.next()
                                for hh in range(2):
                                    MM(px[:, 0:64], gh[:, 1, hh, ct * 128:(ct + 1) * 128], w2b[:, 1, hh, :], hh == 0, hh == 1, [r_gh, r_w2b], [px_r])
                                CP("dve", Vco[:, ct, 0:64], px[:, 0:64], [px_r], [r_Vco])
                            CP("pool", Vco[:, :, 64:129], ovl_t[:], [r_ovl], [r_Vco])
                            if k == 0:
                                dump("d_kcmp", KcmpT[:], [64, 256], BF16, r_Kc)
                                dump("d_vco", Vco[:], [128, 2, 129], BF16, r_Vco)
                                dump("d_hb", hb[:], [128, 4], F32, r_hb)
                                dump("d_gh", gh[:], [128, 2, 2, 256], BF16, r_gh)

                        QT = sb(stg, "QT", [128, 4, T], BF16)
                        r_QT = [Res() for _ in range(4)]
                        r_QM = [[Res() for _ in range(NT)] for _ in range(4)]
                        KsT = sb(stg, "KsT", [128, T], BF16); r_KsT = Res()
                        KwT = sb(stg, "KwT", [64, T], BF16); r_KwT = Res()
                        Vs = sb(stg, "Vs", [128, NT, 65], BF16); r_Vs = Res()
                        Vw = sb(stg, "Vw", [128, NT, 65], BF16); r_Vw = Res()
                        imp_acc = sb(stg, "imp_acc", [128, NT, 64], F32)
                        r_imp = [Res() for _ in range(NT)]
                        for g in range(4):
                            hg = 4 * k + g
                            DMA("dsp" if g % 2 == 0 else "dact", QT[0:64, g, :], qT_s[hg * 64:(hg + 1) * 64, :], [R["qT_s"]], [r_QT[g]])
                        DMA("dsp", KsT[0:64, :], ksT_s[k * 64:(k + 1) * 64, :], [R["ksT_s"]], [r_KsT])
                        with Scope(kb) as ste:
                            ers = sb(ste, "ers", [128, T], F32); r_ers = Res()
                            DMA("dact", ers[64:128, :], erows, [], [r_ers])
                            CP("pool", KsT[64:128, :], ers[64:128, :], [r_ers], [r_KsT])
                        DMA("dact", KwT[:], kwT_s[k * 64:(k + 1) * 64, :], [R["kwT_s"]], [r_KwT])
                        DMA("dsp", Vs[:, :, 0:64], vs_s.rearrange("(n p) c -> p n c", p=128)[:, :, k * 64:(k + 1) * 64], [R["vs_s"]], [r_Vs])
                        DMA("dact", Vw[:, :, 0:64], vw_s.rearrange("(n p) c -> p n c", p=128)[:, :, k * 64:(k + 1) * 64], [R["vw_s"]], [r_Vw])
                        MSET("pool", Vs[:, :, 64:65], 1.0, [r_Vs])
                        MSET("pool", Vw[:, :, 64:65], 1.0, [r_Vw])

                        bcs = Ring([sb(stg, f"bcs{i}", [128, 5, 512], F32) for i in range(2)])
                        BC = Ring([sb(stg, f"BCb{i}", [128, 5, 512], BF16) for i in range(2)])
                        bc_cur = {}

                        def cmp_stage1(it):
                            g, tcn, ct, last = it
                            hg = 4 * k + g
                            if tcn == 0 and ct == 0:
                                bs_t, bs_r = bcs.next()
                                DMA("dsp", bs_t[:, 0:3], bc_g[hg, :, 0:3], [], [bs_r])
                                DMA("dact", bs_t[:, 3:5], bc_g[hg, :, 3:5], [], [bs_r])
                                bc_t, bc_r = BC.next()
                                for m in range(5):
                                    STT(bc_t[:, m, :], bs_t[:, m, :], t31[:, hg:hg + 1], bcm[:, m, :], ALU.subtract, ALU.add, [bs_r, r_bcm, r_t31], [bc_r])
                                bc_cur[g] = (bc_t, bc_r)
                            bc_t, bc_r = bc_cur[g]
                            mp = tcn - 4 * ct
                            p_s, p_r = pS.next()
                            MM(p_s[:], KcmpT[:, ct * 128:(ct + 1) * 128], QT[0:64, g, tcn * 512:(tcn + 1) * 512], True, mp >= 5, [r_Kc, r_QT[g]], [p_r])
                            if mp < 5:
                                MM(p_s[:], ident_b[:], bc_t[:, mp, :], False, True, [r_identb, bc_r], [p_r])
                            P_t, P_r = P_ring.next()
                            ACTF(P_t[:], p_s[:], AF.Exp, [p_r, r_t31], [P_r], bias=t31[:, hg:hg + 1])
                            return (P_t, P_r)

                        def cmp_stage2(it, st1):
                            g, tcn, ct, last = it
                            hg = 4 * k + g
                            P_t, P_r = st1
                            for q in range(4):
                                MM(pO[q][:, 0:129], P_t[:, q * 128:(q + 1) * 128], Vco[:, ct, :], ct == 0, last, [P_r, r_Vco], [r_pO[q]])
                            if last:
                                items = []
                                for q in range(4):
                                    i = 4 * tcn + q
                                    o_t, o_r = osb.next()
                                    CP("dve", o_t[:, 0:129], pO[q][:, 0:129], [r_pO[q]], [o_r])
                                    items.append((o_t, o_r, i, imp_acc[:, i, :], r_imp[i]))
                                finish_tiles(items, 128, hg, 0, True, imp_first=(g == 0))

                        its = []
                        for g in range(4):
                            for tcn in range(8):
                                cts = [0] if tcn < 4 else [0, 1]
                                for ct in cts:
                                    its.append((g, tcn, ct, ct == cts[-1]))
                        LAG = 2
                        pend = []
                        for n in range(len(its) + LAG):
                            if n < len(its):
                                pend.append((its[n], cmp_stage1(its[n])))
                            if n >= LAG:
                                it0, st0 = pend.pop(0)
                                cmp_stage2(it0, st0)

                        if k == 0:
                            dump("d_imp", imp_acc[:], [128, NT, 64], F32, r_imp)
                            dump("d_aout_c", Aout[:], [128, NT, 512], BF16, rA)
                        MBr = Ring([sb(stg, f"MB{i}", [128, 128], F32) for i in range(2)])
                        for (mb_t, mb_r) in zip(MBr.tiles, MBr.res):
                            MSET("dve", mb_t[:], 0.0, [mb_r])
                        tk = Ring([sb(stg, f"tk{i}", [128, 2, 64], F32) for i in range(2)])
                        mxr = Ring([sb(stg, f"mx{i}", [128, 16], F32) for i in range(2)])
                        mtr = Ring([sb(stg, f"mtr{i}", [128, 128], BF16) for i in range(2)])
                        def c3_gen():
                          for i in range(NT):
                            tk_t, tk_r = tk.next()
                            mx_t, mx_r = mxr.next()
                            TT("dve", tk_t[:, 0, :], imp_acc[:, i, :], keep_t[:, i, :], ALU.mult, [r_imp[i], r_keep], [tk_r])
                            TT("dve", tk_t[:, 0, :], tk_t[:, 0, :], force_t[:, i, :], ALU.add, [tk_r, r_force], [tk_r])
                            E("dve", lambda: nc.vector.max(out=mx_t[:, 0:8], in_=tk_t[:, 0, :]), [tk_r], [mx_r])
                            E("dve", lambda: nc.vector.match_replace(out=tk_t[:, 1, :], in_to_replace=mx_t[:, 0:8], in_values=tk_t[:, 0, :], imm_value=-1e30), [tk_r, mx_r], [tk_r])
                            E("dve", lambda: nc.vector.max(out=mx_t[:, 8:16], in_=tk_t[:, 1, :]), [tk_r], [mx_r])
                            mb_t, mb_r = MBr.next()
                            TS("dve", mb_t[:, 64:128], tk_t[:, 0, :], mx_t[:, 15:16], None, ALU.is_ge, None, [tk_r, mx_r], [mb_r])
                            TS("dve", mb_t[:, 64:128], mb_t[:, 64:128], 1.0, -NEGM, ALU.subtract, ALU.mult, [mb_r], [mb_r])
                            px, px_r = pX.next()
                            TR(px[:, 0:128], mb_t[:], ident_f[:], [mb_r, r_identf], [px_r])
                            mt_t, mt_r = mtr.next()
                            CP("act", mt_t[64:128, :], px[64:128, 0:128], [px_r], [mt_r])
                            for g in range(4):
                                CP("pool" if g % 2 == 0 else "dve", QT[64:128, g, i * 128:(i + 1) * 128], mt_t[64:128, :], [mt_r], [r_QM[g][i]])
                            yield

                        if k == 0:
                            dump("d_qt0", QT[:, 0, :], [128, T], BF16, r_QT + [x for l in r_QM for x in l])
                        def sel_stage1(it):
                            g, br, tcn, j = it
                            hg = 4 * k + g
                            qa = max(0, j - 4 * tcn)
                            qb = 3 if br == 1 else min(3, j + 4 - 4 * tcn)
                            c0, c1 = qa * 128, (qb + 1) * 128
                            t0 = tcn * 512
                            adds = []
                            for q in range(qa, qb + 1):
                                dlt = 4 * tcn + q - j
                                if dlt == 0:
                                    adds.append((q, 0))
                                elif dlt == 1:
                                    adds.append((q, 1))
                                elif dlt == 4 and br == 2:
                                    adds.append((q, 2))
                            p_s, p_r = pS.next()
                            if br == 1:
                                rd = [r_KsT, r_QT[g]] + [r_QM[g][4 * tcn + q] for q in range(qa, qb + 1)]
                                MM(p_s[:, c0:c1], KsT[:, j * 128:(j + 1) * 128], QT[:, g, t0 + c0:t0 + c1], True, len(adds) == 0, rd, [p_r])
                            else:
                                MM(p_s[:, c0:c1], KwT[:, j * 128:(j + 1) * 128], QT[0:64, g, t0 + c0:t0 + c1], True, len(adds) == 0, [r_KwT, r_QT[g]], [p_r])
                            for ai, (q, ty) in enumerate(adds):
                                MM(p_s[:, q * 128:(q + 1) * 128], ident_b[:], BD[:, hg, ty, :], False, ai == len(adds) - 1, [r_identb, r_BD], [p_r])
                            P_t, P_r = P_ring.next()
                            ACTF(P_t[:, c0:c1], p_s[:, c0:c1], AF.Exp, [p_r, r_t31], [P_r], bias=t31[:, hg:hg + 1])
                            return (P_t, P_r, qa, qb)

                        def sel_stage2(it, st1):
                            g, br, tcn, j = it
                            hg = 4 * k + g
                            P_t, P_r, qa, qb = st1
                            Vx, r_Vx = (Vs, r_Vs) if br == 1 else (Vw, r_Vw)
                            for q in range(qa, qb + 1):
                                i = 4 * tcn + q
                                first_j = 0 if br == 1 else max(0, i - 4)
                                MM(pO[q][:, 0:65], P_t[:, q * 128:(q + 1) * 128], Vx[:, j, :], j == first_j, j == i, [P_r, r_Vx], [r_pO[q]])
                            if j == 4 * tcn + 3:
                                items = []
                                for q in range(4):
                                    o_t, o_r = osb.next()
                                    CP("dve", o_t[:, 0:65], pO[q][:, 0:65], [r_pO[q]], [o_r])
                                    items.append((o_t, o_r, 4 * tcn + q, None, None))
                                finish_tiles(items, 64, hg, br, False)

                        def branch_gen(br):
                            its = []
                            for g in range(4):
                                for tcn in range(8):
                                    j_lo = 0 if br == 1 else max(0, 4 * tcn - 4)
                                    for j in range(j_lo, 4 * tcn + 4):
                                        its.append((g, br, tcn, j))
                            LAG = 2
                            pend = []
                            for n in range(len(its) + LAG):
                                if n < len(its):
                                    pend.append((its[n], sel_stage1(its[n])))
                                if n >= LAG:
                                    it0, st0 = pend.pop(0)
                                    sel_stage2(it0, st0)
                                if n % 4 == 3:
                                    yield

                        drive_rr([branch_gen(2), c3_gen()])
                        drive_rr([branch_gen(1)])

                dump("d_aout", Aout[:], [128, NT, 512], BF16, rA)
                with Scope(kb) as stn:
                    junkC = sb(stn, "junkC", [128, 512], BF16); r_junkC = Res()
                    an = Ring([sb(stn, f"an{i}", [128, 512], BF16) for i in range(2)])
                    af = Ring([sb(stn, f"af{i}", [128, 512], F32) for i in range(2)])
                    ao = Ring([sb(stn, f"ao{i}", [128, 512], BF16) for i in range(2)])
                    pTb = Ring([ps(stn, f"pTC{i}", [128, 512], BF16) for i in range(2)]) if False else None
                    for i in range(NT):
                        s_t, s_r = sm.next()
                        ACTF(junkC[:], Aout[:, i, :], AF.Square, [rA[i]], [r_junkC, s_r], accum_out=s_t[:, 0:1])
                        TS("dve", s_t[:, 1:2], s_t[:, 0:1], 1.0 / 512, EPS, ALU.mult, ALU.add, [s_r], [s_r])
                        ACTF(s_t[:, 1:2], s_t[:, 1:2], AF.Sqrt, [s_r], [s_r])
                        E("dve", lambda: nc.vector.reciprocal(out=s_t[:, 2:3], in_=s_t[:, 1:2]), [s_r], [s_r])
                        af_t, af_r = af.next()
                        STT(af_t[:], Aout[:, i, :], s_t[:, 2:3], ga_t[:], ALU.mult, ALU.mult, [rA[i], s_r, r_ga], [af_r])
                        px, px_r = pX.next()
                        for fc in range(4):
                            TR(px[:, fc * 128:(fc + 1) * 128], af_t[:, fc * 128:(fc + 1) * 128], ident_f[:], [af_r, r_identf], [px_r])
                        ao_t, ao_r = ao.next()
                        CP("act", ao_t[:], px[:], [px_r], [ao_r])
                        DMA("dsp" if i % 2 == 0 else "dpool", mixT_s[0:512, i * 128:(i + 1) * 128].rearrange("(f p) t -> p f t", p=128),
                            ao_t[:].rearrange("p (f t) -> p f t", f=4), [ao_r], [R["mixT_s"]])

        if "D" in phases or "D1" in phases:
            NTL = TH // 128
            with Scope(kb) as st:
                Wo = sb(st, "Wo", [128, 8, D], BF16); r_Wo = Res()
                Wq = sb(st, "Wq", [128, 8, 2048], BF16); r_Wq = Res()
                skb = sb(st, "skb", [128, 2, 128], BF16); r_skb = Res()
                repf = sb(st, "repf", [128, D], F32); r_rep = Res()
                io16 = sb(st, "io16", [128, 16], F32); r_io = Res()
                io128 = sb(st, "io128", [128, 128], F32); r_io128 = Res()
                selt = sb(st, "selt", [128, 2], F32); r_sel = Res()
                DMA("dsp", repf[:], rep4[:, 0, :], [], [r_rep])
                DMA("dact", io16[:], iota16, [], [r_io])
                DMA("dact", io128[:], iota128, [], [r_io128])
                DMA("dact", selt[:], selc, [], [r_sel])
                with Scope(kb) as stw:
                    wst = Ring([sb(stw, f"wstD{i}", [128, 2048], F32) for i in range(3)])
                    n = 0
                    for (src, dstw, dres, ncol, nch) in ((w_out_in, Wo, r_Wo, D, 8), (peer_wq, Wq, r_Wq, 2048, 8)):
                        for dc in range(nch):
                            w_t, w_r = wst.next()
                            n += 1
                            DMA("dsp" if n % 2 == 0 else "dact", w_t[:, 0:ncol], src[dc * 128:(dc + 1) * 128, :], [], [w_r])
                            CP("dve" if n % 2 == 0 else "pool", dstw[:, dc, :], w_t[:, 0:ncol], [w_r], [dres])
                    w_t, w_r = wst.next()
                    DMA("dsp", w_t[:, 0:256].rearrange("p (a k) -> p a k", a=2), sk_T.rearrange("a p k -> p a k"), [], [w_r])
                    CP("dve", skb[:], w_t[:, 0:256].rearrange("p (a k) -> p a k", a=2), [w_r], [r_skb])

                pacc = Ring([ps(st, f"pD{i}", [128, 512], F32) for i in range(4)])
                pw_ring = Ring([ps(st, f"pDw{i}", [128, 512], F32) for i in range(2)])
                ptb = Ring([ps(st, f"pDb{i}", [128, 1024], BF16) for i in range(2)])
                mst = Ring([sb(st, f"mst{i}", [128, 8, 2, 128], BF16) for i in range(1)])
                mixh_ring = Ring([sb(st, f"mixh{i}", [128, 8, 128], BF16) for i in range(1)])
                xh_ring = Ring([sb(st, f"xhD{i}", [128, D], F32) for i in range(2)])
                H_ring = Ring([sb(st, f"HD{i}", [128, D], F32) for i in range(2)])
                xng_ring = Ring([sb(st, f"xng{i}", [128, D], F32) for i in range(1)])
                xnb_ring = Ring([sb(st, f"xnbD{i}", [128, D], BF16) for i in range(1)])
                xT_ring = Ring([sb(st, f"xTD{i}", [128, 8, 128], BF16) for i in range(2)])
                qTb = sb(st, "qTb", [128, 16, 128], BF16); r_qTb = Res()
                Ssc_ring = Ring([sb(st, f"Ssc{i}", [128, 16, 128], F32) for i in range(2)])
                Swk = sb(st, "Swk", [128, 8, 128], F32)
                rv = [Res() for _ in range(16)]; rv2 = [Res() for _ in range(16)]; ri = [Res() for _ in range(16)]; ri2 = [Res() for _ in range(16)]; rw = [Res() for _ in range(16)]
                v16 = sb(st, "v16", [128, 16, 16], F32); r_v16 = Res()
                i16 = sb(st, "i16", [128, 16, 16], U32); r_i16 = Res()
                i16f = sb(st, "i16f", [128, 16, 16], F32); r_i16f = Res()
                cand = sb(st, "cand", [128, 8, 256], F32); r_cand = Res()
                cwk = sb(st, "cwk", [128, 8, 256], F32)
                sc16 = sb(st, "sc16", [128, 8, 16], F32); r_sc = Res()
                ci16 = sb(st, "ci16", [128, 8, 16], U32); r_ci = Res()
                ab_u = sb(st, "ab_u", [128, 2, 8, 16], U32); r_abu = Res()
                ab_f = sb(st, "ab_f", [128, 2, 8, 16], F32); r_abf = Res()
                eq = sb(st, "eq", [128, 8, 16, 16], F32); r_eq = Res()
                isel_ring = Ring([sb(st, f"isel{i}", [128, 3, 8, 16], F32) for i in range(2)])
                gz = sb(st, "gz", [128, 16], F32); r_gz = Res()
                junkB = sb(st, "junkDb", [128, D], BF16); r_junkB = Res()
                smD = Ring([sb(st, f"smD{i}", [128, 8], F32) for i in range(4)])
                ijgT_ring = Ring([sb(st, f"ijgT{i}", [128, 3, 128], F32) for i in range(2)])
                OI = Ring([sb(st, f"OI{i}", [128, 16, 128], BF16) for i in range(2)])
                OJ = Ring([sb(st, f"OJ{i}", [128, 16, 128], BF16) for i in range(2)])
                OJf = Ring([sb(st, f"OJf{i}", [128, 16, 128], BF16) for i in range(2)])
                Wst = sb(st, "Wst", [128, 128, 128], BF16); r_Wst = Res()

                def rms_scaled(src, src_r, gain, gain_r, dstf, dstf_r):
                    s_t, s_r = smD.next()
                    ACTF(junkB[:], src, AF.Square, [src_r], [r_junkB, s_r], accum_out=s_t[:, 0:1])
                    TS("dve", s_t[:, 1:2], s_t[:, 0:1], 1.0 / D, EPS, ALU.mult, ALU.add, [s_r], [s_r])
                    ACTF(s_t[:, 1:2], s_t[:, 1:2], AF.Sqrt, [s_r], [s_r])
                    E("dve", lambda: nc.vector.reciprocal(out=s_t[:, 2:3], in_=s_t[:, 1:2]), [s_r], [s_r])
                    STT(dstf, src, s_t[:, 2:3], gain, ALU.mult, ALU.mult, [src_r, s_r, gain_r], [dstf_r])

                def rms_scaled_g(src, src_r, gain, gain_r, dstf, dstf_r):
                    s_t, s_r = smD.next()
                    ACTF(junkB[:], src, AF.Square, [src_r], [r_junkB, s_r], accum_out=s_t[:, 0:1])
                    yield
                    TS("dve", s_t[:, 1:2], s_t[:, 0:1], 1.0 / D, EPS, ALU.mult, ALU.add, [s_r], [s_r])
                    ACTF(s_t[:, 1:2], s_t[:, 1:2], AF.Sqrt, [s_r], [s_r])
                    yield
                    E("dve", lambda: nc.vector.reciprocal(out=s_t[:, 2:3], in_=s_t[:, 1:2]), [s_r], [s_r])
                    STT(dstf, src, s_t[:, 2:3], gain, ALU.mult, ALU.mult, [src_r, s_r, gain_r], [dstf_r])

                def transpose8(srcb, srcb_r, dstT, dstT_r, nblk=8):
                    pt, pt_r = ptb.next()
                    for dc in range(nblk):
                        TR(pt[:, dc * 128:(dc + 1) * 128], srcb[:, dc * 128:(dc + 1) * 128], ident_b[:], [srcb_r, r_identb], [pt_r])
                    CP("act", dstT.rearrange("p a t -> p (a t)"), pt[:, 0:nblk * 128], [pt_r], [dstT_r])

                def S1(it):
                    tsl = slice(it * 128, (it + 1) * 128)
                    xh_t, xh_r = xh_ring.next()
                    DMA("dsp", xh_t[:], xh[tsl, :], [], [xh_r])
                    H, H_r = H_ring.next()
                    m_t, m_r = mst.next()
                    for a in range(2):
                        DMA("dsp" if a == 0 else "dact", m_t[:, :, a, :], mixT_s[:, a * TH + it * 128:a * TH + (it + 1) * 128].rearrange("(f p) t -> p f t", p=128), [R["mixT_s"]], [m_r])
                    mixh, r_mixh = mixh_ring.next()
                    ACTF(mixh[:], m_t[:, :, 0, :], AF.Copy, [m_r, r_sel], [r_mixh], scale=selt[:, 0:1])
                    STT(mixh[:], m_t[:, :, 1, :], selt[:, 1:2], mixh[:], ALU.mult, ALU.add, [m_r, r_sel, r_mixh], [r_mixh])
                    yield
                    for ch in range(2):
                        pa, pa_r = pacc.next()
                        for fc in range(8):
                            MM(pa[:], mixh[:, fc, :], Wo[:, fc, ch * 512:(ch + 1) * 512], fc == 0, fc == 7, [r_mixh, r_Wo], [pa_r])
                        TT("dve", H[:, ch * 512:(ch + 1) * 512], pa[:], xh_t[:, ch * 512:(ch + 1) * 512], ALU.add, [pa_r, xh_r], [H_r])
                        yield
                    DMA("dpool", H1_s[tsl, :], H[:], [H_r], [R["H1_s"]])
                    xng, xng_r = xng_ring.next()
                    yield from rms_scaled_g(H[:], H_r, repf[:], r_rep, xng[:], xng_r)
                    yield
                    xnb, xnb_r = xnb_ring.next()
                    CP("pool", xnb[:], xng[:], [xng_r], [xnb_r])
                    yield
                    xT, xT_r = xT_ring.next()
                    transpose8(xnb, xnb_r, xT[:], xT_r)
                    yield
                    DMA("dact", xnT2_s[:, :, tsl], xT[:], [xT_r], [R["xnT2_s"]])
                    for grp in range(4):
                        pa, pa_r = pacc.next()
                        for j in range(4):
                            hp = grp * 4 + j
                            for dc in range(8):
                                MM(pa[:, j * 128:(j + 1) * 128], Wq[:, dc, hp * 128:(hp + 1) * 128], xT[:, dc, :], dc == 0, dc == 7, [r_Wq, xT_r], [pa_r])
                        CP("act", qTb[:, grp * 4:(grp + 1) * 4, :].rearrange("p a t -> p (a t)"), pa[:], [pa_r], [r_qTb])
                        yield
                    Ssc, r_S = Ssc_ring.next()
                    for grp in range(4):
                        pa, pa_r = pacc.next()
                        for j in range(4):
                            hp = grp * 4 + j
                            MM(pa[:, j * 128:(j + 1) * 128], qTb[:, hp, :], skb[:, hp % 2, :], True, True, [r_qTb, r_skb], [pa_r])
                        CP("act", Ssc[:, grp * 4:(grp + 1) * 4, :].rearrange("p a t -> p (a t)"), pa[:], [pa_r], [r_S])
                        yield
                    return (Ssc, r_S)

                def S2(it, st1):
                    Ssc, r_S = st1
                    for g0 in (0, 8):
                        hps = range(g0, g0 + 8)
                        for hp in hps:
                            E("dve", lambda: nc.vector.max(out=v16[:, hp, 0:8], in_=Ssc[:, hp, :]), [r_S], [rv[hp]])
                        yield
                        for hp in hps:
                            E("dve", lambda: nc.vector.max_index(out=i16[:, hp, 0:8], in_max=v16[:, hp, 0:8], in_values=Ssc[:, hp, :]), [r_S, rv[hp]], [ri[hp]])
                        yield
                        for hp in hps:
                            E("dve", lambda: nc.vector.match_replace(out=Swk[:, hp - g0, :], in_to_replace=v16[:, hp, 0:8], in_values=Ssc[:, hp, :], imm_value=-1e30), [r_S, rv[hp]], [rw[hp - g0]])
                        yield
                        for hp in hps:
                            E("dve", lambda: nc.vector.max(out=v16[:, hp, 8:16], in_=Swk[:, hp - g0, :]), [rw[hp - g0]], [rv2[hp]])
                        yield
                        for hp in hps:
                            E("dve", lambda: nc.vector.max_index(out=i16[:, hp, 8:16], in_max=v16[:, hp, 8:16], in_values=Swk[:, hp - g0, :]), [rw[hp - g0], rv2[hp]], [ri2[hp]])
                        yield
                    r_i16 = Res()
                    E("dve", lambda: nc.vector.tensor_copy(out=i16f[:], in_=i16[:]), ri + ri2, [r_i16f, r_i16])
                    v4 = v16[:].rearrange("p (h two) k -> p h two k", two=2)
                    in0 = v4[:, :, 0, :].rearrange("p h (a o) -> p h a o", o=1).to_broadcast([128, 8, 16, 16])
                    in1 = v4[:, :, 1, :].rearrange("p h (o b) -> p h o b", o=1).to_broadcast([128, 8, 16, 16])
                    TT("dve", cand[:].rearrange("p h (a b) -> p h a b", a=16), in0, in1, ALU.add, rv + rv2, [r_cand])
                    for h in range(8):
                        E("dve", lambda: nc.vector.max(out=sc16[:, h, 0:8], in_=cand[:, h, :]), [r_cand], [rv[h]])
                    yield
                    for h in range(8):
                        E("dve", lambda: nc.vector.max_index(out=ci16[:, h, 0:8], in_max=sc16[:, h, 0:8], in_values=cand[:, h, :]), [r_cand, rv[h]], [ri[h]])
                    yield
                    for h in range(8):
                        E("dve", lambda: nc.vector.match_replace(out=cwk[:, h, :], in_to_replace=sc16[:, h, 0:8], in_values=cand[:, h, :], imm_value=-1e30), [r_cand, rv[h]], [rw[h]])
                    yield
                    for h in range(8):
                        E("dve", lambda: nc.vector.max(out=sc16[:, h, 8:16], in_=cwk[:, h, :]), [rw[h]], [rv2[h]])
                    yield
                    for h in range(8):
                        E("dve", lambda: nc.vector.max_index(out=ci16[:, h, 8:16], in_max=sc16[:, h, 8:16], in_values=cwk[:, h, :]), [rw[h], rv2[h]], [ri2[h]])
                    yield
                    r_sc = Res(); r_ci = Res()
                    E("dve", lambda: nc.vector.tensor_single_scalar(out=ab_u[:, 0], in_=ci16[:], scalar=4, op=ALU.logical_shift_right), ri[:8] + ri2[:8] + rv[:8] + rv2[:8], [r_abu, r_sc, r_ci])
                    E("dve", lambda: nc.vector.tensor_single_scalar(out=ab_u[:, 1], in_=ci16[:], scalar=15, op=ALU.bitwise_and), [r_ci], [r_abu])
                    CP("dve", ab_f[:], ab_u[:], [r_abu], [r_abf])
                    isel, r_isel = isel_ring.next()
                    i4 = i16f[:].rearrange("p (h two) k -> p h two k", two=2)
                    for w in range(2):
                        a_b = ab_f[:, w].rearrange("p h (k o) -> p h k o", o=1).to_broadcast([128, 8, 16, 16])
                        io_b = io16[:].rearrange("p (o q a) -> p o q a", o=1, q=1).to_broadcast([128, 8, 16, 16])
                        TT("dve", eq[:], a_b, io_b, ALU.is_equal, [r_abf, r_io], [r_eq])
                        iv_b = i4[:, :, w, :].rearrange("p h (o a) -> p h o a", o=1).to_broadcast([128, 8, 16, 16])
                        TT("dve", eq[:], eq[:], iv_b, ALU.mult, [r_eq, r_i16f], [r_eq])
                        E("dve", lambda: nc.vector.tensor_reduce(out=isel[:, w], in_=eq[:], axis=AX.X, op=ALU.add), [r_eq], [r_isel])
                        yield
                    TT("dve", isel[:, 2], sc16[:], sc16[:, :, 0:1].to_broadcast([128, 8, 16]), ALU.subtract, [r_sc], [r_isel])
                    ACTF(isel[:, 2], isel[:, 2], AF.Exp, [r_isel], [r_isel])
                    E("dve", lambda: nc.vector.tensor_reduce(out=gz[:, 0:8], in_=isel[:, 2], axis=AX.X, op=ALU.add), [r_isel], [r_gz])
                    E("dve", lambda: nc.vector.reciprocal(out=gz[:, 8:16], in_=gz[:, 0:8]), [r_gz], [r_gz])
                    TT("dve", isel[:, 2], isel[:, 2], gz[:, 8:16].rearrange("p (h o) -> p h o", o=1).to_broadcast([128, 8, 16]), ALU.mult, [r_isel, r_gz], [r_isel])
                    pa, pa_r = pacc.next()
                    for w in range(3):
                        TR(pa[:, w * 128:(w + 1) * 128], isel[:, w].rearrange("p h k -> p (h k)"), ident_f[:], [r_isel, r_identf], [pa_r])
                    ijgT, r_ijgT = ijgT_ring.next()
                    CP("act", ijgT[:].rearrange("p a t -> p (a t)"), pa[:, 0:384], [pa_r], [r_ijgT])
                    return (ijgT, r_ijgT)

                def S3(it, st2):
                    ijgT, r_ijgT = st2
                    TB = 16
                    for tb in range(128 // TB):
                        t0 = tb * TB
                        oi, oi_r = OI.next()
                        oj, oj_r = OJ.next()
                        io_b = io128[:].rearrange("p (o i) -> p o i", o=1).to_broadcast([128, TB, 128])

                        def colb(w):
                            return ijgT[:, w, t0:t0 + TB].rearrange("p (t o) -> p t o", o=1).to_broadcast([128, TB, 128])
                        ojf, ojf_r = OJf.next()
                        TT("dve", oi[:], io_b, colb(0), ALU.is_equal, [r_io128, r_ijgT], [oi_r])
                        TT("dve", ojf[:], io_b, colb(1), ALU.is_equal, [r_io128, r_ijgT], [ojf_r])
                        TT("pool", oj[:], ojf[:], colb(2), ALU.mult, [ojf_r, r_ijgT], [oj_r])
                        for tq in range(TB // 4):
                            pw, pw_r = pw_ring.next()
                            for u in range(4):
                                MM(pw[:, u:512:4], oj[:, tq * 4 + u, :], oi[:, tq * 4 + u, :], True, True, [oj_r, oi_r], [pw_r])
                            tg = t0 + tq * 4
                            CP("act", Wst[:, :, tg:tg + 4], pw[:].rearrange("p (i t) -> p i t", t=4), [pw_r], [r_Wst])
                            yield
                    DMA("dsp" if it % 2 == 0 else "dact", Wt_s[it], Wst[:], [r_Wst], [R["Wt_s"]])

                def drive(gens):
                    res = [None] * len(gens)
                    live = list(range(len(gens)))
                    while live:
                        for gi in list(live):
                            try:
                                next(gens[gi])
                            except StopIteration as e:
                                res[gi] = e.value
                                live.remove(gi)
                    return res

                st1s, st2s = {}, {}
                for n in range(NTL + 2):
                    gens, tags = [], []
                    if n < NTL:
                        gens.append(S1(n)); tags.append(("s1", n))
                    if n >= 2:
                        gens.append(S3(n - 2, st2s.pop(n - 2))); tags.append(("s3", n - 2))
                    if 1 <= n <= NTL:
                        gens.append(S2(n - 1, st1s.pop(n - 1))); tags.append(("s2", n - 1))
                    for (tg_, tn), rv_ in zip(tags, drive(gens)):
                        if tg_ == "s1":
                            st1s[tn] = rv_
                        elif tg_ == "s2":
                            st2s[tn] = rv_

            with Scope(kb) as st:
                Yacc = sb(st, "Yacc", [128, NTL, D], F32)
                rY = [Res() for _ in range(NTL)]
                H1v = H1_s.rearrange("(n p) d -> p n d", p=128)
                for n4 in range(4):
                    DMA("dsp" if n4 % 2 == 0 else "dact", Yacc[:, n4 * 4:(n4 + 1) * 4, :], H1v[:, n4 * 4:(n4 + 1) * 4, :], [R["H1_s"]], rY[n4 * 4:(n4 + 1) * 4])
                p1 = Ring([ps(st, f"pE1{i}", [128, 512], F32) for i in range(3)])
                p2 = Ring([ps(st, f"pE2{i}", [128, 512], F32) for i in range(3)])
                ptb2 = Ring([ps(st, f"pEb{i}", [128, 1024], BF16) for i in range(2)])
                with Scope(kb) as st2:
                  if "D" in phases or "D2" in phases:
                    xnTa = sb(st2, "xnTa", [128, 8, TH], BF16); r_xnTa = Res()
                    for dc in range(8):
                        DMA("dsp" if dc % 2 == 0 else "dact", xnTa[:, dc, :], xnT2_s[:, dc, :], [R["xnT2_s"]], [r_xnTa])
                    IB = 4
                    NB = 128 // IB
                    ust = Ring([sb(st2, f"ust{i}", [128, D], F32) for i in range(2)])
                    vst = Ring([sb(st2, f"vst{i}", [128, D], F32) for i in range(2)])
                    ub = Ring([sb(st2, f"ub{i}", [128, D], BF16) for i in range(2)])
                    uT = Ring([sb(st2, f"uT{i}", [128, 8, 128], BF16) for i in range(2)])
                    Vbs = [sb(st2, f"Vb{i}", [128, IB, D], BF16) for i in range(2)]
                    WAs = [sb(st2, f"WA{i}", [128, IB, TH], BF16) for i in range(2)]
                    r_Vbs = [[Res() for _ in range(IB)] for _ in range(2)]
                    r_WAs = [[Res() for _ in range(IB)] for _ in range(2)]
                    wt = Ring([sb(st2, f"wt{i}", [128, TH], BF16) for i in range(3)])
                    gl = Ring([sb(st2, f"gl{i}", [128, 512], BF16) for i in range(3)])

                    def genS1(blk):
                        Vb, WA, r_Vb, r_WA = Vbs[blk % 2], WAs[blk % 2], r_Vbs[blk % 2], r_WAs[blk % 2]
                        for ib in range(IB):
                            i = blk * IB + ib
                            u_t, u_r = ust.next()
                            v_t, v_r = vst.next()
                            w_t, w_r = wt.next()
                            DMA("dsp", u_t[:], peer_u[i * 128:(i + 1) * 128, :], [], [u_r])
                            DMA("dact", v_t[:], peer_v[i * 128:(i + 1) * 128, :], [], [v_r])
                            for hw in range(2):
                                DMA("dpool" if hw == 0 else ("dsp" if i % 2 == 0 else "dact"), w_t[:, hw * 1024:(hw + 1) * 1024].rearrange("p (n t) -> p n t", t=128),
                                    Wt_s[hw * 8:(hw + 1) * 8, :, i, :].rearrange("n j t -> j n t"), [R["Wt_s"]], [w_r])
                            ub_t, ub_r = ub.next()
                            CP("pool", ub_t[:], u_t[:], [u_r], [ub_r])
                            CP("pool", Vb[:, ib, :], v_t[:], [v_r], [r_Vb[ib]])
                            pt, pt_r = ptb2.next()
                            for dc in range(8):
                                TR(pt[:, dc * 128:(dc + 1) * 128], ub_t[:, dc * 128:(dc + 1) * 128], ident_b[:], [ub_r, r_identb], [pt_r])
                            uT_t, uT_r = uT.next()
                            CP("act", uT_t[:].rearrange("p a t -> p (a t)"), pt[:], [pt_r], [uT_r])
                            yield
                            for tc4 in range(4):
                                pa, pa_r = p1.next()
                                for dc in range(8):
                                    MM(pa[:], uT_t[:, dc, :], xnTa[:, dc, tc4 * 512:(tc4 + 1) * 512], dc == 0, dc == 7, [uT_r, r_xnTa], [pa_r])
                                g_t, g_r = gl.next()
                                ACTF(g_t[:], pa[:], AF.Gelu_apprx_tanh, [pa_r], [g_r])
                                TT("dve", WA[:, ib, tc4 * 512:(tc4 + 1) * 512], g_t[:], w_t[:, tc4 * 512:(tc4 + 1) * 512], ALU.mult, [g_r, w_r], [r_WA[ib]])
                                yield

                    def genS2(blk):
                        Vb, WA, r_Vb, r_WA = Vbs[blk % 2], WAs[blk % 2], r_Vbs[blk % 2], r_WAs[blk % 2]
                        for tt in range(NTL):
                            for ch in range(2):
                                pb, pb_r = p2.next()
                                for ib in range(IB):
                                    MM(pb[:], WA[:, ib, tt * 128:(tt + 1) * 128], Vb[:, ib, ch * 512:(ch + 1) * 512], ib == 0, ib == IB - 1, [r_WA[ib], r_Vb[ib]], [pb_r])
                                TT("dve", Yacc[:, tt, ch * 512:(ch + 1) * 512], Yacc[:, tt, ch * 512:(ch + 1) * 512], pb[:], ALU.add, [rY[tt], pb_r], [rY[tt]])
                            yield

                    drive_rr([genS1(0)])
                    for blk in range(NB):
                        gens = []
                        if blk + 1 < NB:
                            gens.append(genS1(blk + 1))
                        gens.append(genS2(blk))
                        drive_rr(gens)

                with Scope(kb) as st3:
                    Wgt = sb(st3, "Wgt", [128, 8, D], BF16); r_Wgt = Res()
                    Wp = sb(st3, "Wp", [128, 2, D], BF16); r_Wp = Res()
                    rep3 = sb(st3, "rep3", [128, 3, D], F32); r_rep3 = Res()
                    DMA("dsp", rep3[:], rep4[:, 1:4, :], [], [r_rep3])
                    wst3 = Ring([sb(st3, f"wst3{i}", [128, D], F32) for i in range(2)])
                    for (src, dstw, dres, nch) in ((ple_wg, Wgt, r_Wgt, 8), (ple_pj, Wp, r_Wp, 2)):
                        for dc in range(nch):
                            w_t, w_r = wst3.next()
                            DMA("dsp" if dc % 2 == 0 else "dact", w_t[:], src[dc * 128:(dc + 1) * 128, :], [], [w_r])
                            CP("dve" if dc % 2 == 0 else "pool", dstw[:, dc, :], w_t[:], [w_r], [dres])
                    x3_ring = Ring([sb(st3, f"x3{i}", [128, D], F32) for i in range(2)])
                    x3b_ring = Ring([sb(st3, f"x3b{i}", [128, D], BF16) for i in range(2)])
                    x3T_ring = Ring([sb(st3, f"x3T{i}", [128, 8, 128], BF16) for i in range(2)])
                    pht = Ring([sb(st3, f"pht{i}", [128, 256], F32) for i in range(2)])
                    phb = Ring([sb(st3, f"phb{i}", [128, 256], BF16) for i in range(2)])
                    phT = Ring([sb(st3, f"phT{i}", [128, 2, 128], BF16) for i in range(2)])
                    gt_ring = Ring([sb(st3, f"gtD{i}", [128, D], F32) for i in range(2)])
                    ot_ring = Ring([sb(st3, f"otD{i}", [128, D], F32) for i in range(2)])
                    junk3 = sb(st3, "junk3", [128, D], BF16); r_junk3 = Res()
                    sm3 = Ring([sb(st3, f"sm3{i}", [128, 8], F32) for i in range(4)])

                    def rms3(src, src_r, gi, dstf, dstf_r):
                        s_t, s_r = sm3.next()
                        ACTF(junk3[:], src, AF.Square, [src_r], [r_junk3, s_r], accum_out=s_t[:, 0:1])
                        TS("dve", s_t[:, 1:2], s_t[:, 0:1], 1.0 / D, EPS, ALU.mult, ALU.add, [s_r], [s_r])
                        ACTF(s_t[:, 1:2], s_t[:, 1:2], AF.Sqrt, [s_r], [s_r])
                        E("dve", lambda: nc.vector.reciprocal(out=s_t[:, 2:3], in_=s_t[:, 1:2]), [s_r], [s_r])
                        STT(dstf, src, s_t[:, 2:3], rep3[:, gi, :], ALU.mult, ALU.mult, [src_r, s_r, r_rep3], [dstf_r])

                    def tr3(srcb, srcb_r, dstT, dstT_r, nblk):
                        pt, pt_r = ptb2.next()
                        for dc in range(nblk):
                            TR(pt[:, dc * 128:(dc + 1) * 128], srcb[:, dc * 128:(dc + 1) * 128], ident_b[:], [srcb_r, r_identb], [pt_r])
                        CP("act", dstT.rearrange("p a t -> p (a t)"), pt[:, 0:nblk * 128], [pt_r], [dstT_r])

                    for it in range(NTL):
                        tsl = slice(it * 128, (it + 1) * 128)
                        Hh = Yacc[:, it, :]; H_r = rY[it]
                        x3, x3_r = x3_ring.next()
                        rms3(Hh, H_r, 0, x3[:], x3_r)
                        x3b, x3b_r = x3b_ring.next()
                        CP("pool", x3b[:], x3[:], [x3_r], [x3b_r])
                        x3T, x3T_r = x3T_ring.next()
                        tr3(x3b, x3b_r, x3T[:], x3T_r, 8)
                        ph_t, ph_r = pht.next()
                        DMA("dact", ph_t[:], ph[tsl, :], [], [ph_r])
                        pb_t, pb_r = phb.next()
                        CP("pool", pb_t[:], ph_t[:], [ph_r], [pb_r])
                        pT_t, pT_r = phT.next()
                        tr3(pb_t, pb_r, pT_t[:], pT_r, 2)
                        gt, gt_r = gt_ring.next()
                        for ch in range(2):
                            csl = slice(ch * 512, (ch + 1) * 512)
                            pa, pa_r = p1.next()
                            for dc in range(8):
                                MM(pa[:], x3T[:, dc, :], Wgt[:, dc, csl], dc == 0, dc == 7, [x3T_r, r_Wgt], [pa_r])
                            TT("dve", gt[:, csl], pa[:], rep3[:, 2, csl], ALU.add, [pa_r, r_rep3], [gt_r])
                            ACTF(gt[:, csl], gt[:, csl], AF.Sigmoid, [gt_r], [gt_r])
                            pb2, pb2_r = p2.next()
                            for dc in range(2):
                                MM(pb2[:], pT_t[:, dc, :], Wp[:, dc, csl], dc == 0, dc == 1, [pT_r, r_Wp], [pb2_r])
                            TT("dve", gt[:, csl], gt[:, csl], pb2[:], ALU.mult, [gt_r, pb2_r], [gt_r])
                            TT("pool", Yacc[:, it, csl], Yacc[:, it, csl], gt[:, csl], ALU.add, [H_r, gt_r], [H_r])
                        ot, ot_r = ot_ring.next()
                        rms3(Hh, H_r, 1, ot[:], ot_r)
                        DMA("dsp", out[tsl, :], ot[:], [ot_r], [])

        kb.drain_all()
    return nc


def _blockdiag(w):
    o = np.zeros((128, 4, 128), np.float32)
    for n in range(8):
        cc, j = n // 2, n % 2
        o[j * 64:(j + 1) * 64, cc, j * 64:(j + 1) * 64] = w[n]
    return o


def _rel_bucket(dist):
    n = np.maximum(dist, 0)
    nf = np.maximum(n, 16).astype(np.float32)
    large = 16 + (np.log(nf / np.float32(16)) / np.float32(np.log(8.0)) * np.float32(16)).astype(np.int32)
    large = np.minimum(large, 31)
    return np.where(n < 16, n, large)


def _nsa_consts(rel_table):
    c = {}
    assert (_rel_bucket(np.arange(113, 8192)) == 31).all()
    cl = np.arange(128)[:, None, None]; mp = np.arange(5)[None, :, None]; tt = np.arange(512)[None, None, :]
    dist = 512 * mp + tt - 16 * cl - 31
    c["bc_g"] = np.ascontiguousarray(rel_table[_rel_bucket(dist)].transpose(3, 0, 1, 2))
    c["bc_m"] = np.where(dist >= 0, 0.0, NEGM).astype(np.float32)
    assert (512 * 5 - 16 * 127 - 31) >= 113
    sl = np.arange(128)[:, None]; tl = np.arange(128)[None, :]
    d0 = tl - sl; d1 = 128 + tl - sl
    g0 = rel_table[_rel_bucket(d0)]; g1 = rel_table[_rel_bucket(d1)]
    c["bd_g"] = np.ascontiguousarray(np.stack([g0, g1], 0).transpose(3, 1, 0, 2))
    m0 = np.where(d0 >= 0, 0.0, NEGM); m2 = np.where(tl < sl, 0.0, NEGM)
    c["bd_m"] = np.ascontiguousarray(np.stack([m0, np.zeros_like(m0), m2], 1)).astype(np.float32)
    c["t31"] = np.ascontiguousarray(np.broadcast_to(rel_table[31][None, :], (128, 8))).astype(np.float32)
    t = (np.arange(NT)[None, :, None] * 128 + np.arange(128)[:, None, None])
    blk = np.arange(64)[None, None, :]
    d = t // 64 - blk
    local = (d >= 0) & (d < 2)
    init = (blk == 0) & ~local
    past = (d >= 0) & ~local & ~init
    c["force_c"] = np.where(local, 2.0e4, np.where(init, 1.0e4, np.where(past, 0.0, -1.0))).astype(np.float32)
    c["keep_c"] = past.astype(np.float32)
    cs = np.arange(256)[:, None] * 16; ss = np.arange(64)[None, :] * 64
    ov = np.clip(np.minimum(cs + 32, ss + 64) - np.maximum(cs, ss), 0, None).astype(np.float32) / 32.0
    ove = np.concatenate([ov, np.ones((256, 1), np.float32)], 1)
    ove[255] = 0.0
    c["ovl_ext"] = np.ascontiguousarray(ove.reshape(2, 128, 65).transpose(1, 0, 2))
    c["erows"] = (np.arange(T)[None, :] // 64 == np.arange(64)[:, None]).astype(np.float32)
    return c


def _prep_inputs(inputs):
    x = np.ascontiguousarray(inputs["x"], dtype=np.float32)
    p = np.ascontiguousarray(inputs["p"], dtype=np.float32)
    shared = {
        "ident": np.eye(128, dtype=np.float32),
        "w_in": np.ascontiguousarray(inputs["w_in"][0]),
        "g_attn": np.ascontiguousarray(inputs["attn_norm"][0].reshape(8, 128).T),
        "lru_cw": np.ascontiguousarray(inputs["conv_w"][0][:, 0, :].reshape(4, 4, 128).transpose(2, 1, 0)),
        "lru_vec": np.ascontiguousarray(np.stack([inputs[k][0].reshape(4, 128) for k in
                                                  ("conv_b", "lru_ba", "lru_bx", "lru_lambda", "grp_norm_lru")], 0).transpose(2, 0, 1)),
        "cmp_w1": np.ascontiguousarray(np.stack([inputs[k][0].reshape(16, 128, 256).transpose(1, 0, 2) for k in ("cmp_k_w1", "cmp_v_w1")], 0)),
        "cmp_pe": np.ascontiguousarray(np.stack([inputs[k][0].reshape(16, 128).T for k in ("cmp_k_pe", "cmp_v_pe")], 0)),
        "cmp_w2": np.ascontiguousarray(np.stack([inputs[k][0].reshape(2, 128, 64).transpose(1, 0, 2) for k in ("cmp_k_w2", "cmp_v_w2")], 0)),
        "ga_rep": np.ascontiguousarray(np.broadcast_to(inputs["grp_norm_attn"][0][None, :], (128, 512))).astype(np.float32),
        "w_out": np.ascontiguousarray(inputs["w_out"][0]),
        "peer_wq": np.ascontiguousarray(inputs["peer_wq"][0]),
        "sk_T": np.ascontiguousarray(inputs["peer_subkeys"][0].transpose(0, 2, 1)),
        "peer_u": np.ascontiguousarray(inputs["peer_u"][0]),
        "peer_v": np.ascontiguousarray(inputs["peer_v"][0]),
        "ple_wgate": np.ascontiguousarray(inputs["ple_wgate"][0]),
        "ple_proj": np.ascontiguousarray(inputs["ple_proj"][0]),
        "rep4": np.ascontiguousarray(np.broadcast_to(np.stack([inputs["ffn_norm"][0], inputs["ple_norm"][0], inputs["final_norm"], inputs["ple_bgate"][0]], 0)[None], (128, 4, D))).astype(np.float32),
        "iota128": np.ascontiguousarray(np.broadcast_to(np.arange(128, dtype=np.float32)[None], (128, 128))),
        "iota16": np.ascontiguousarray(np.broadcast_to(np.arange(16, dtype=np.float32)[None], (128, 16))),
        "lru_bda": _blockdiag(inputs["lru_wa"][0]),
        "lru_bdx": _blockdiag(inputs["lru_wx"][0]),
    }
    shared.update(_nsa_consts(np.asarray(inputs["rel_table"], np.float32)))
    in_maps = []
    for c in range(8):
        b, hf = c // 2, c % 2
        m = dict(shared)
        m["xb"] = x[b]
        m["xh"] = np.ascontiguousarray(x[b, hf * TH:(hf + 1) * TH])
        m["ph"] = np.ascontiguousarray(p[0, b, hf * TH:(hf + 1) * TH])
        sel = np.zeros((128, 2), np.float32); sel[:, hf] = 1.0
        m["selc"] = sel
        in_maps.append(m)
    return in_maps


def kernel(**inputs):
    nc = build_program()
    in_maps = _prep_inputs(inputs)
    res = run_bass_kernel_spmd(nc, in_maps, core_ids=list(range(8)))
    outp = np.zeros((4, T, D), np.float32)
    for c in range(8):
        b, hf = c // 2, c % 2
        outp[b, hf * TH:(hf + 1) * TH] = res.results[c]["out"]
    return outp
```

```python
import numpy as np
from contextlib import ExitStack
import concourse.bass as bass
import concourse.mybir as mybir
from concourse.bass_utils import run_bass_kernel_spmd

F32 = mybir.dt.float32
BF16 = mybir.dt.bfloat16
U32 = mybir.dt.uint32
AF = mybir.ActivationFunctionType
ALU = mybir.AluOpType
AX = mybir.AxisListType

T = 4096
D = 1024
NT = T // 128
TH = 2048
IN_COLS = 2328
EPS = 1e-6
NEGM = -30000.0


class Res:
    __slots__ = ("name", "lw", "rd")

    def __init__(self, name=""):
        self.name = name
        self.lw = None
        self.rd = {}


class KB:
    NDMA = 4

    def __init__(self, nc, stack):
        self.nc = nc
        self.issue = {"pe": nc.tensor, "act": nc.scalar, "dve": nc.vector, "pool": nc.gpsimd,
                      "dsp": nc.sync, "dact": nc.scalar, "dpool": nc.gpsimd}
        self.stream = {"pe": "pe", "act": "act", "dve": "dve", "pool": "pool",
                       "dsp": "sp", "dact": "act", "dpool": "pool"}
        self.sems = {}
        self.cnt = {}
        for q in self.issue:
            n = self.NDMA if self.is_dma(q) else 1
            self.sems[q] = [stack.enter_context(nc.semaphore(f"s_{q}{i}")) for i in range(n)]
            self.cnt[q] = 0
        self.waited = {s: {} for s in ("pe", "act", "dve", "pool", "sp")}
        self.ninst = 0
        self._rr = 0

    @staticmethod
    def is_dma(q):
        return q in ("dsp", "dact", "dpool")

    @staticmethod
    def _need(need, dep):
        if dep is None:
            return
        q, c = dep
        if need.get(q, 0) < c:
            need[q] = c

    def _waits(self, st, eng, need, skip_q=None):
        for dq, c in need.items():
            if dq == "pe" and skip_q == "pe":
                continue
            if self.is_dma(dq):
                n = self.NDMA
                for si in range(n):
                    k = (c - 1 - si) // n + 1 if c - 1 >= si else 0
                    if k <= 0:
                        continue
                    key = (dq, si)
                    if self.waited[st].get(key, 0) >= k:
                        continue
                    eng.wait_ge(self.sems[dq][si], 16 * k)
                    self.waited[st][key] = k
            else:
                key = (dq, 0)
                if self.waited[st].get(key, 0) >= c:
                    continue
                eng.wait_ge(self.sems[dq][0], c)
                self.waited[st][key] = c

    def emit(self, q, fn, reads=(), writes=()):
        need = {}
        for r in reads:
            self._need(need, r.lw)
        for w in writes:
            self._need(need, w.lw)
            for rq, rc in w.rd.items():
                self._need(need, (rq, rc))
        st = self.stream[q]
        self._waits(st, self.issue[q], need, skip_q=q)
        inst = fn()
        self.cnt[q] += 1
        c = self.cnt[q]
        if self.is_dma(q):
            inst.then_inc(self.sems[q][(c - 1) % self.NDMA], 16)
        else:
            inst.then_inc(self.sems[q][0], 1)
        for r in reads:
            if r.rd.get(q, 0) < c:
                r.rd[q] = c
        for w in writes:
            w.lw = (q, c)
            w.rd = {}
        self.ninst += 1
        return inst

    def dmaq(self):
        self._rr ^= 1
        return "dsp" if self._rr else "dact"

    def barrier(self):
        need = {q: c for q, c in self.cnt.items() if c > 0}
        for st, eng in (("pe", self.nc.tensor), ("act", self.nc.scalar), ("dve", self.nc.vector),
                        ("pool", self.nc.gpsimd), ("sp", self.nc.sync)):
            self._waits(st, eng, dict(need))

    def drain_all(self):
        need = {q: c for q, c in self.cnt.items() if c > 0}
        self._waits("sp", self.nc.sync, need)


class Scope:
    def __init__(self, kb):
        self.kb = kb
        self.st = ExitStack()

    def __enter__(self):
        self.st.__enter__()
        return self.st

    def __exit__(self, *a):
        if a[0] is None:
            self.kb.barrier()
        return self.st.__exit__(*a)


class Ring:
    def __init__(self, tiles):
        self.tiles = tiles
        self.res = [Res() for _ in tiles]
        self.i = -1

    def next(self):
        self.i = (self.i + 1) % len(self.tiles)
        return self.tiles[self.i], self.res[self.i]


def build_program(dbg=None, phases=("A", "B", "C", "D")):
    nc = bass.Bass("TRN2", target_bir_lowering=False)

    def din(name, shape, dt=F32):
        return nc.dram_tensor(name, list(shape), dt, kind="ExternalInput").ap()

    dbg = dbg or ()

    def dscr(name, shape, dt):
        kind = "ExternalOutput" if name in dbg else "Internal"
        return nc.dram_tensor(name, list(shape), dt, kind=kind).ap()

    xb = din("xb", [T, D])
    xh = din("xh", [TH, D])
    ph = din("ph", [TH, 256])
    selc = din("selc", [128, 2])
    ident = din("ident", [128, 128])
    w_in = din("w_in", [D, IN_COLS])
    g_attn = din("g_attn", [128, 8])
    out = nc.dram_tensor("out", [TH, D], F32, kind="ExternalOutput").ap()
    lru_cw = din("lru_cw", [128, 4, 4])
    lru_vec = din("lru_vec", [128, 5, 4])
    lru_bda = din("lru_bda", [128, 4, 128])
    lru_bdx = din("lru_bdx", [128, 4, 128])

    qT_s = dscr("qT_s", [512, T], BF16)
    kcT_s = dscr("kcT_s", [128, T], BF16)
    vcT_s = dscr("vcT_s", [128, T], BF16)
    ksT_s = dscr("ksT_s", [128, T], BF16)
    kwT_s = dscr("kwT_s", [128, T], BF16)
    vs_s = dscr("vs_s", [T, 128], BF16)
    vw_s = dscr("vw_s", [T, 128], BF16)
    gates_s = dscr("gates_s", [T, 24], F32)
    xrT_s = dscr("xrT_s", [512, T], F32)
    xgT_s = dscr("xgT_s", [512, T], F32)

    cmp_w1 = din("cmp_w1", [2, 128, 16, 256])
    cmp_pe = din("cmp_pe", [2, 128, 16])
    cmp_w2 = din("cmp_w2", [2, 128, 2, 64])
    ovl_ext = din("ovl_ext", [128, 2, 65])
    bc_g = din("bc_g", [8, 128, 5, 512])
    bc_m = din("bc_m", [128, 5, 512])
    bd_g = din("bd_g", [8, 128, 2, 128])
    bd_m = din("bd_m", [128, 3, 128])
    t31_in = din("t31", [128, 8])
    force_in = din("force_c", [128, 32, 64])
    keep_in = din("keep_c", [128, 32, 64])
    erows = din("erows", [64, T])
    ga_in = din("ga_rep", [128, 512])
    w_out_in = din("w_out", [D, D])
    peer_wq = din("peer_wq", [D, 2048])
    sk_T = din("sk_T", [2, 128, 128])
    peer_u = din("peer_u", [16384, D])
    peer_v = din("peer_v", [16384, D])
    ple_wg = din("ple_wgate", [D, D])
    ple_pj = din("ple_proj", [256, D])
    rep4 = din("rep4", [128, 4, D])
    iota16 = din("iota16", [128, 16])
    iota128 = din("iota128", [128, 128])
    H1_s = dscr("H1_s", [TH, D], F32)
    xnT2_s = dscr("xnT2_s", [128, 8, TH], BF16)
    Wt_s = dscr("Wt_s", [TH // 128, 128, 128, 128], BF16)
    mixT_s = dscr("mixT_s", [1024, T], BF16)
    R = {n: Res(n) for n in ("H1_s", "xnT2_s", "Wt_s", "mixT_s", "qT_s", "kcT_s", "vcT_s", "ksT_s", "kwT_s", "vs_s", "vw_s", "gates_s", "xrT_s", "xgT_s")}

    with ExitStack() as top:
        kb = KB(nc, top)
        E = kb.emit

        uniq = [0]

        def sb(st, name, shape, dt):
            uniq[0] += 1
            return st.enter_context(nc.sbuf_tensor(f"sb{uniq[0]}_{name}", list(shape), dt))

        def ps(st, name, shape, dt):
            uniq[0] += 1
            return st.enter_context(nc.psum_tensor(f"ps{uniq[0]}_{name}", list(shape), dt))


        def MM(out_, lhsT, rhs, start, stop, reads, writes):
            return E("pe", lambda: nc.tensor.matmul(out_, lhsT=lhsT, rhs=rhs, start=start, stop=stop), reads, writes)

        def TR(out_, in_, idt, reads, writes):
            return E("pe", lambda: nc.tensor.transpose(out=out_, in_=in_, identity=idt), reads, writes)

        def ACTF(out_, in_, func, reads, writes, **kw):
            return E("act", lambda: nc.scalar.activation(out=out_, in_=in_, func=func, **kw), reads, writes)

        def veng(q):
            return nc.vector if q == "dve" else nc.gpsimd

        def TS(q, out_, in0, s1, s2, op0, op1, reads, writes):
            if op1 is None:
                return E(q, lambda: veng(q).tensor_scalar(out=out_, in0=in0, scalar1=s1, scalar2=None, op0=op0), reads, writes)
            return E(q, lambda: veng(q).tensor_scalar(out=out_, in0=in0, scalar1=s1, scalar2=s2, op0=op0, op1=op1), reads, writes)

        def TT(q, out_, in0, in1, op, reads, writes):
            return E(q, lambda: veng(q).tensor_tensor(out=out_, in0=in0, in1=in1, op=op), reads, writes)

        def STT(out_, in0, scalar, in1, op0, op1, reads, writes, **kw):
            return E("dve", lambda: nc.vector.scalar_tensor_tensor(out=out_, in0=in0, scalar=scalar, in1=in1, op0=op0, op1=op1, **kw), reads, writes)

        def CP(q, out_, in_, reads, writes):
            if q == "act":
                return E("act", lambda: nc.scalar.copy(out=out_, in_=in_), reads, writes)
            return E(q, lambda: veng(q).tensor_copy(out=out_, in_=in_), reads, writes)

        def MSET(q, out_, val, writes):
            return E(q, lambda: veng(q).memset(out_, val), (), writes)

        def DMA(q, out_, in_, reads, writes):
            eng = {"dsp": nc.sync, "dact": nc.scalar, "dpool": nc.gpsimd}[q]
            return E(q, lambda: eng.dma_start(out=out_, in_=in_), reads, writes)

        def drive_rr(gens, weights=None):
            res = [None] * len(gens)
            live = list(range(len(gens)))
            weights = weights or [1] * len(gens)
            while live:
                for gi in list(live):
                    for _ in range(weights[gi]):
                        try:
                            next(gens[gi])
                        except StopIteration as e:
                            res[gi] = e.value
                            live.remove(gi)
                            break
            return res

        def dump(name, ap, shape, dt, res):
            if name not in dbg:
                return
            d = nc.dram_tensor(name, list(shape), dt, kind="ExternalOutput").ap()
            DMA("dsp", d, ap, [res] if not isinstance(res, list) else res, [])

        ident_f = sb(top, "ident_f", [128, 128], F32); r_identf = Res()
        ident_b = sb(top, "ident_b", [128, 128], BF16); r_identb = Res()
        E("dsp", lambda: nc.sync.dma_start(out=ident_f[:], in_=ident), writes=[r_identf])
        E("dve", lambda: nc.vector.tensor_copy(out=ident_b[:], in_=ident_f[:]), reads=[r_identf], writes=[r_identb])

        if "A" in phases:
            with Scope(kb) as st:
                Wg = sb(st, "Wg", [128, 8, IN_COLS], BF16); r_Wg = Res()
                gcol = sb(st, "gcol", [128, 8], F32); r_gcol = Res()
                wst = Ring([sb(st, f"wst{i}", [128, IN_COLS], F32) for i in range(2)])
                E("dsp", lambda: nc.sync.dma_start(out=gcol[:], in_=g_attn), writes=[r_gcol])
                for dc in range(8):
                    w_t, w_r = wst.next()
                    E("dsp" if dc % 2 == 0 else "dact",
                      (lambda w_t=w_t, dc=dc: nc.sync.dma_start(out=w_t[:], in_=w_in[dc * 128:(dc + 1) * 128, :])) if dc % 2 == 0 else
                      (lambda w_t=w_t, dc=dc: nc.scalar.dma_start(out=w_t[:], in_=w_in[dc * 128:(dc + 1) * 128, :])),
                      writes=[w_r])
                    eng = "dve" if dc % 2 == 0 else "pool"
                    ve = nc.vector if dc % 2 == 0 else nc.gpsimd
                    E(eng, lambda ve=ve, w_t=w_t, dc=dc: ve.tensor_scalar(out=Wg[:, dc, :], in0=w_t[:], scalar1=gcol[:, dc:dc + 1], scalar2=None, op0=ALU.mult),
                      reads=[w_r, r_gcol], writes=[r_Wg])

                xt_ring = Ring([sb(st, f"xt{i}", [128, 4, D], F32) for i in range(2)])
                xnb_ring = Ring([sb(st, f"xnb{i}", [128, 4, D], BF16) for i in range(2)])
                xnT_ring = Ring([sb(st, f"xnT{i}", [128, 8, 512], BF16) for i in range(2)])
                junk = sb(st, "junkA", [128, D], BF16); r_junk = Res()
                ss_ring = Ring([sb(st, f"ss{i}", [128, 8], F32) for i in range(2)])
                pT_ring = Ring([ps(st, f"pT{i}", [128, 512], BF16) for i in range(2)])
                pacc = Ring([ps(st, f"pacc{i}", [128, 512], F32) for i in range(4)])
                ostf = Ring([sb(st, f"ostf{i}", [128, 512], F32) for i in range(3)])
                ostb = Ring([sb(st, f"ostb{i}", [128, 512], BF16) for i in range(3)])
                osv = Ring([sb(st, f"osv{i}", [128, 256], BF16) for i in range(2)])
                osg = Ring([sb(st, f"osg{i}", [128, 24], F32) for i in range(2)])
                xb_v = xb.rearrange("(n p) d -> p n d", p=128)
                fm = []
                for cc in range(4):
                    fm.append((cc * 128, qT_s[cc * 128:(cc + 1) * 128, :], 0.125, True, R["qT_s"]))
                fm.append((512, kcT_s, 1.0, True, R["kcT_s"]))
                fm.append((640, vcT_s, 1.0, True, R["vcT_s"]))
                fm.append((768, ksT_s, 1.0, True, R["ksT_s"]))
                fm.append((1024, kwT_s, 1.0, True, R["kwT_s"]))
                for cc in range(4):
                    fm.append((1304 + cc * 128, xrT_s[cc * 128:(cc + 1) * 128, :], 1.0, False, R["xrT_s"]))
                for cc in range(4):
                    fm.append((1816 + cc * 128, xgT_s[cc * 128:(cc + 1) * 128, :], 1.0, False, R["xgT_s"]))
                evc = [0]

                def stageP(tcn):
                    xt, xt_r = xt_ring.next()
                    DMA("dsp", xt[:], xb_v[:, tcn * 4:(tcn + 1) * 4, :], [], [xt_r])
                    ss, ss_r = ss_ring.next()
                    for n in range(4):
                        ACTF(junk[:], xt[:, n, :], AF.Square, [xt_r], [r_junk, ss_r], accum_out=ss[:, n:n + 1])
                    yield
                    TS("dve", ss[:, 4:8], ss[:, 0:4], 1.0 / D, EPS, ALU.mult, ALU.add, [ss_r], [ss_r])
                    ACTF(ss[:, 4:8], ss[:, 4:8], AF.Sqrt, [ss_r], [ss_r])
                    yield
                    E("dve", lambda: nc.vector.reciprocal(out=ss[:, 4:8], in_=ss[:, 4:8]), [ss_r], [ss_r])
                    xnb, xnb_r = xnb_ring.next()
                    for n in range(4):
                        if n % 2 == 0:
                            TS("dve", xnb[:, n, :], xt[:, n, :], ss[:, 4 + n:5 + n], None, ALU.mult, None, [xt_r, ss_r], [xnb_r])
                        else:
                            ACTF(xnb[:, n, :], xt[:, n, :], AF.Copy, [xt_r, ss_r], [xnb_r], scale=ss[:, 4 + n:5 + n])
                    yield
                    xnT, xnT_r = xnT_ring.next()
                    for dc in range(8):
                        pT, pT_r = pT_ring.next()
                        for n in range(4):
                            TR(pT[:, n * 128:(n + 1) * 128], xnb[:, n, dc * 128:(dc + 1) * 128], ident_b[:], [xnb_r, r_identb], [pT_r])
                        CP("act" if dc % 2 == 0 else "dve", xnT[:, dc, :], pT[:], [pT_r], [xnT_r])
                        if dc % 2 == 1:
                            yield
                    return (xnT, xnT_r)

                def stageM(tcn, st):
                    xnT, xnT_r = st
                    for (c0, dst, scale, isb, dres) in fm:
                        pa, pa_r = pacc.next()
                        for dc in range(8):
                            MM(pa[:], Wg[:, dc, c0:c0 + 128], xnT[:, dc, :], dc == 0, dc == 7, [r_Wg, xnT_r], [pa_r])
                        o_t, o_r = (ostb if isb else ostf).next()
                        evc[0] += 1
                        if evc[0] % 2 == 0:
                            ACTF(o_t[:], pa[:], AF.Copy, [pa_r], [o_r], scale=scale)
                        else:
                            TS("dve", o_t[:], pa[:], scale, None, ALU.mult, None, [pa_r], [o_r])
                        DMA("dsp" if evc[0] % 2 == 0 else "dpool", dst[:, tcn * 512:(tcn + 1) * 512], o_t[:], [o_r], [dres])
                        yield
                    for n in range(4):
                        t0 = tcn * 512 + n * 128
                        pa, pa_r = pacc.next()
                        for dc in range(8):
                            MM(pa[:, 0:128], xnT[:, dc, n * 128:(n + 1) * 128], Wg[:, dc, 896:1024], dc == 0, dc == 7, [r_Wg, xnT_r], [pa_r])
                        pb, pb_r = pacc.next()
                        for dc in range(8):
                            MM(pb[:, 0:152], xnT[:, dc, n * 128:(n + 1) * 128], Wg[:, dc, 1152:1304], dc == 0, dc == 7, [r_Wg, xnT_r], [pb_r])
                        ov, ov_r = osv.next()
                        og, og_r = osg.next()
                        CP("act", ov[:, 0:128], pa[:, 0:128], [pa_r], [ov_r])
                        CP("dve", ov[:, 128:256], pb[:, 0:128], [pb_r], [ov_r])
                        CP("dve", og[:], pb[:, 128:152], [pb_r], [og_r])
                        DMA("dsp", vs_s[t0:t0 + 128, :], ov[:, 0:128], [ov_r], [R["vs_s"]])
                        DMA("dpool", vw_s[t0:t0 + 128, :], ov[:, 128:256], [ov_r], [R["vw_s"]])
                        DMA("dsp", gates_s[t0:t0 + 128, :], og[:], [og_r], [R["gates_s"]])
                        yield

                stP = drive_rr([stageP(0)])[0]
                for tcn in range(8):
                    gens = [stageM(tcn, stP)]
                    if tcn + 1 < 8:
                        gens.append(stageP(tcn + 1))
                    rr = drive_rr(gens)
                    if tcn + 1 < 8:
                        stP = rr[1]

        if "B" in phases:
            with Scope(kb) as st:
                cw = sb(st, "cw", [128, 4, 4], F32); r_cw = Res()
                lv = sb(st, "lv", [128, 5, 4], F32); r_lv = Res()
                clc = sb(st, "clc", [128, 3, 4], F32); r_clc = Res()
                bdf = sb(st, "bdf", [128, 2, 4, 128], F32); r_bdf = Res()
                bdb = sb(st, "bdb", [128, 2, 4, 128], BF16); r_bdb = Res()
                ones_b = sb(st, "ones_b", [128, 128], BF16); r_ones = Res()
                E("dsp", lambda: nc.sync.dma_start(out=cw[:], in_=lru_cw), writes=[r_cw])
                E("dact", lambda: nc.scalar.dma_start(out=lv[:], in_=lru_vec), writes=[r_lv])
                E("dsp", lambda: nc.sync.dma_start(out=bdf[:, 0], in_=lru_bda), writes=[r_bdf])
                E("dact", lambda: nc.scalar.dma_start(out=bdf[:, 1], in_=lru_bdx), writes=[r_bdf])
                E("dve", lambda: nc.vector.tensor_copy(out=bdb[:], in_=bdf[:]), reads=[r_bdf], writes=[r_bdb])
                E("dve", lambda: nc.vector.memset(ones_b[:], 1.0), writes=[r_ones])
                E("act", lambda: nc.scalar.activation(out=clc[:, 0, :], in_=lv[:, 3, :], func=AF.Exp, scale=-1.0), reads=[r_lv], writes=[r_clc])
                E("act", lambda: nc.scalar.activation(out=clc[:, 0, :], in_=clc[:, 0, :], func=AF.Ln, bias=1.0), reads=[r_clc], writes=[r_clc])
                E("dve", lambda: nc.vector.tensor_scalar(out=clc[:, 1, :], in0=clc[:, 0, :], scalar1=-8.0, scalar2=None, op0=ALU.mult), reads=[r_clc], writes=[r_clc])
                E("dve", lambda: nc.vector.tensor_scalar(out=clc[:, 2, :], in0=clc[:, 0, :], scalar1=-16.0, scalar2=None, op0=ALU.mult), reads=[r_clc], writes=[r_clc])
                L = sb(st, "Lall", [128, 4, T], F32); r_L = Res()
                X = [sb(st, f"lruX{i}", [128, T], F32) for i in range(5)]
                rX = [Res() for _ in range(5)]
                xcb = sb(st, "xcb", [128, T], BF16); r_xcb = Res()
                pg = Ring([ps(st, f"pg{i}", [128, 512], F32) for i in range(4)])
                for cc in range(4):
                    X1, X2, X3, X4, X5 = X
                    r1, r2, r3, r4, r5 = rX
                    for hh in range(2):
                        E("dsp", lambda cc=cc, hh=hh: nc.sync.dma_start(out=X1[:, hh * 2048:(hh + 1) * 2048], in_=xrT_s[cc * 128:(cc + 1) * 128, hh * 2048:(hh + 1) * 2048]), reads=[R["xrT_s"]], writes=[r1])
                        E("dact", lambda cc=cc, hh=hh: nc.scalar.dma_start(out=X3[:, hh * 2048:(hh + 1) * 2048], in_=xgT_s[cc * 128:(cc + 1) * 128, hh * 2048:(hh + 1) * 2048]), reads=[R["xgT_s"]], writes=[r3])
                    E("dve", lambda cc=cc: nc.vector.tensor_scalar(out=X2[:], in0=X1[:], scalar1=cw[:, cc, 3:4], scalar2=lv[:, 0, cc:cc + 1], op0=ALU.mult, op1=ALU.add), reads=[r1, r_cw, r_lv], writes=[r2])
                    for sh in (1, 2, 3):
                        E("dve", lambda cc=cc, sh=sh: nc.vector.scalar_tensor_tensor(out=X2[:, sh:T], in0=X1[:, 0:T - sh], scalar=cw[:, cc, 3 - sh:4 - sh], in1=X2[:, sh:T], op0=ALU.mult, op1=ALU.add), reads=[r1, r2, r_cw], writes=[r2])
                    E("pool", lambda: nc.gpsimd.tensor_copy(out=xcb[:], in_=X2[:]), reads=[r2], writes=[r_xcb])
                    for gi, (Xo, ro, bi) in enumerate(((X4, r4, 1), (X5, r5, 2))):
                        for tcn in range(8):
                            pgt, pg_r = pg.next()
                            E("pe", lambda pgt=pgt, gi=gi, cc=cc, tcn=tcn: nc.tensor.matmul(pgt[:], lhsT=bdb[:, gi, cc, :], rhs=xcb[:, tcn * 512:(tcn + 1) * 512], start=True, stop=True), reads=[r_bdb, r_xcb], writes=[pg_r])
                            E("act", lambda pgt=pgt, Xo=Xo, bi=bi, cc=cc, tcn=tcn: nc.scalar.activation(out=Xo[:, tcn * 512:(tcn + 1) * 512], in_=pgt[:], func=AF.Sigmoid, bias=lv[:, bi, cc:cc + 1]), reads=[pg_r, r_lv], writes=[ro])
                    E("act", lambda cc=cc: nc.scalar.activation(out=X1[:], in_=X4[:], func=AF.Exp, scale=clc[:, 1, cc:cc + 1]), reads=[r4, r_clc], writes=[r1])
                    E("act", lambda cc=cc: nc.scalar.activation(out=X4[:], in_=X4[:], func=AF.Exp, scale=clc[:, 2, cc:cc + 1]), reads=[r4, r_clc], writes=[r4])
                    E("act", lambda: nc.scalar.activation(out=X4[:], in_=X4[:], func=AF.Sqrt, scale=-1.0, bias=1.0), reads=[r4], writes=[r4])
                    E("pool", lambda: nc.gpsimd.tensor_tensor(out=X5[:], in0=X5[:], in1=X2[:], op=ALU.mult), reads=[r5, r2], writes=[r5])
                    E("dve", lambda: nc.vector.tensor_tensor(out=X4[:], in0=X4[:], in1=X5[:], op=ALU.mult), reads=[r4, r5], writes=[r4])
                    E("dve", lambda: nc.vector.tensor_tensor_scan(out=X2[:], data0=X1[:], data1=X4[:], initial=0.0, op0=ALU.mult, op1=ALU.add), reads=[r1, r4], writes=[r2])
                    E("act", lambda: nc.scalar.activation(out=X3[:], in_=X3[:], func=AF.Gelu_apprx_tanh), reads=[r3], writes=[r3])
                    E("pool", lambda cc=cc: nc.gpsimd.tensor_tensor(out=L[:, cc, :], in0=X2[:], in1=X3[:], op=ALU.mult), reads=[r2, r3], writes=[r_L])
                sq = Ring([sb(st, f"lsq{i}", [128, 512], BF16) for i in range(2)])
                rs_ring = Ring([sb(st, f"lrs{i}", [128, 512], F32) for i in range(2)])
                lo = Ring([sb(st, f"lo{i}", [128, 512], BF16) for i in range(3)])
                for tcn in range(8):
                    pgt, pg_r = pg.next()
                    for cc in range(4):
                        sq_t, sq_r = sq.next()
                        E("act", lambda sq_t=sq_t, cc=cc, tcn=tcn: nc.scalar.activation(out=sq_t[:], in_=L[:, cc, tcn * 512:(tcn + 1) * 512], func=AF.Square), reads=[r_L], writes=[sq_r])
                        E("pe", lambda pgt=pgt, sq_t=sq_t, cc=cc: nc.tensor.matmul(pgt[:], lhsT=ones_b[:], rhs=sq_t[:], start=(cc == 0), stop=(cc == 3)), reads=[r_ones, sq_r], writes=[pg_r])
                    rs_t, rs_r = rs_ring.next()
                    E("dve", lambda rs_t=rs_t, pgt=pgt: nc.vector.tensor_scalar(out=rs_t[:], in0=pgt[:], scalar1=1.0 / 512, scalar2=EPS, op0=ALU.mult, op1=ALU.add), reads=[pg_r], writes=[rs_r])
                    E("act", lambda rs_t=rs_t: nc.scalar.activation(out=rs_t[:], in_=rs_t[:], func=AF.Sqrt), reads=[rs_r], writes=[rs_r])
                    E("dve", lambda rs_t=rs_t: nc.vector.reciprocal(out=rs_t[:], in_=rs_t[:]), reads=[rs_r], writes=[rs_r])
                    for cc in range(4):
                        lo_t, lo_r = lo.next()
                        E("dve", lambda lo_t=lo_t, rs_t=rs_t, cc=cc, tcn=tcn: nc.vector.scalar_tensor_tensor(out=lo_t[:], in0=L[:, cc, tcn * 512:(tcn + 1) * 512], scalar=lv[:, 4, cc:cc + 1], in1=rs_t[:], op0=ALU.mult, op1=ALU.mult), reads=[r_L, rs_r, r_lv], writes=[lo_r])
                        E("dsp", lambda lo_t=lo_t, cc=cc, tcn=tcn: nc.sync.dma_start(out=mixT_s[512 + cc * 128:512 + (cc + 1) * 128, tcn * 512:(tcn + 1) * 512], in_=lo_t[:]), reads=[lo_r], writes=[R["mixT_s"]])

        if "C" in phases:
            with Scope(kb) as st:
                Aout = sb(st, "Aout", [128, NT, 512], BF16)
                rA = [Res() for _ in range(NT)]
                sig = sb(st, "sig", [128, NT, 24], F32); r_sig = Res()
                force_t = sb(st, "force_t", [128, NT, 64], F32); r_force = Res()
                keep_t = sb(st, "keep_t", [128, NT, 64], F32); r_keep = Res()
                t31 = sb(st, "t31", [128, 8], F32); r_t31 = Res()
                BD = sb(st, "BD", [128, 8, 3, 128], BF16); r_BD = Res()
                ovl_t = sb(st, "ovl_t", [128, 2, 65], F32); r_ovl = Res()
                ga_t = sb(st, "ga_t", [128, 512], F32); r_ga = Res()
                bcm = sb(st, "bcm", [128, 5, 512], F32); r_bcm = Res()
                DMA("dsp", sig[:], gates_s.rearrange("(n p) c -> p n c", p=128), [R["gates_s"]], [r_sig])
                ACTF(sig[:], sig[:], AF.Sigmoid, [r_sig], [r_sig])
                DMA("dact", force_t[:], force_in, [], [r_force])
                DMA("dsp", keep_t[:], keep_in, [], [r_keep])
                DMA("dact", t31[:], t31_in, [], [r_t31])
                DMA("dsp", ovl_t[:], ovl_ext, [], [r_ovl])
                DMA("dact", ga_t[:], ga_in, [], [r_ga])
                DMA("dsp", bcm[:], bc_m, [], [r_bcm])
                psb = [ps(st, f"pC{i}", [128, 512], F32) for i in range(8)]
                pS = Ring(psb[0:3])
                pO = psb[3:7]; r_pO = [Res() for _ in range(4)]
                pX = Ring(psb[7:8])
                with Scope(kb) as st2:
                    bdg = sb(st2, "bdg", [128, 8, 2, 128], F32); r_bdg = Res()
                    bdm = sb(st2, "bdm", [128, 3, 128], F32); r_bdm = Res()
                    DMA("dsp", bdg[:], bd_g.rearrange("h p j t -> p h j t"), [], [r_bdg])
                    DMA("dact", bdm[:], bd_m, [], [r_bdm])
                    for hg in range(8):
                        for j in range(2):
                            STT(BD[:, hg, j, :], bdg[:, hg, j, :], t31[:, hg:hg + 1], bdm[:, j, :], ALU.subtract, ALU.add, [r_bdg, r_bdm, r_t31], [r_BD])
                        CP("dve", BD[:, hg, 2, :], bdm[:, 2, :], [r_bdm], [r_BD])
                P_ring = Ring([sb(st, f"Pt{i}", [128, 512], BF16) for i in range(5)])
                sm = Ring([sb(st, f"smC{i}", [128, 8], F32) for i in range(8)])
                osb = Ring([sb(st, f"osb{i}", [128, 132], F32) for i in range(8)])

                def finish_tiles(items, ncol, hg, br, first, imp_first=None):
                    sts = [sm.next() for _ in items]
                    for (po, po_r, i, _, _), (s_t, s_r) in zip(items, sts):
                        TS("dve", s_t[:, 0:1], po[:, ncol:ncol + 1], 1e-30, None, ALU.max, None, [po_r], [s_r])
                    for (po, po_r, i, _, _), (s_t, s_r) in zip(items, sts):
                        E("dve", lambda: nc.vector.reciprocal(out=s_t[:, 1:2], in_=s_t[:, 0:1]), [s_r], [s_r])
                    for (po, po_r, i, _, _), (s_t, s_r) in zip(items, sts):
                        TT("dve", s_t[:, 2:3], s_t[:, 1:2], sig[:, i, hg * 3 + br:hg * 3 + br + 1], ALU.mult, [s_r, r_sig], [s_r])
                    for (po, po_r, i, _, _), (s_t, s_r) in zip(items, sts):
                        dst = Aout[:, i, hg * 64:(hg + 1) * 64]
                        if first:
                            TS("dve", dst, po[:, 0:64], s_t[:, 2:3], None, ALU.mult, None, [po_r, s_r], [rA[i]])
                        else:
                            STT(dst, po[:, 0:64], s_t[:, 2:3], dst, ALU.mult, ALU.add, [po_r, s_r, rA[i]], [rA[i]])
                    if imp_first is not None:
                        for (po, po_r, i, imp_t, imp_r), (s_t, s_r) in zip(items, sts):
                            if imp_first:
                                TS("dve", imp_t, po[:, 64:128], s_t[:, 1:2], None, ALU.mult, None, [po_r, s_r], [imp_r])
                            else:
                                STT(imp_t, po[:, 64:128], s_t[:, 1:2], imp_t, ALU.mult, ALU.add, [po_r, s_r, imp_r], [imp_r])

                for k in range(2):
                    with Scope(kb) as stg:
                        KcmpT = sb(stg, "KcmpT", [64, 256], BF16); r_Kc = Res()
                        Vco = sb(stg, "Vco", [128, 2, 129], BF16); r_Vco = Res()
                        with Scope(kb) as stc:
                            w1s = Ring([sb(stc, f"w1s{i}", [128, 8, 256], F32) for i in range(2)])
                            w1b = sb(stc, "w1b", [128, 2, 16, 256], BF16); r_w1b = Res()
                            pes = sb(stc, "pes", [128, 2, 16], F32); r_pes = Res()
                            peb = sb(stc, "peb", [128, 2, 16], BF16); r_peb = Res()
                            w2s = sb(stc, "w2s", [128, 2, 2, 64], F32); r_w2s = Res()
                            w2b = sb(stc, "w2b", [128, 2, 2, 64], BF16); r_w2b = Res()
                            stk = sb(stc, "stk", [128, 2, T], BF16); r_stk = Res()
                            hb = sb(stc, "hb", [128, 4], F32); r_hb = Res()
                            gh = sb(stc, "gh", [128, 2, 2, 256], BF16); r_gh = Res()
                            for kv in range(2):
                                for hh in range(2):
                                    w_t, w_r = w1s.next()
                                    DMA("dsp" if hh == 0 else "dact", w_t[:], cmp_w1[kv, :, hh * 8:(hh + 1) * 8, :], [], [w_r])
                                    CP("pool" if hh == 0 else "dve", w1b[:, kv, hh * 8:(hh + 1) * 8, :], w_t[:], [w_r], [r_w1b])
                                DMA("dsp", pes[:, kv, :], cmp_pe[kv], [], [r_pes])
                                DMA("dact", w2s[:, kv], cmp_w2[kv], [], [r_w2s])
                                src = kcT_s if kv == 0 else vcT_s
                                sres = R["kcT_s"] if kv == 0 else R["vcT_s"]
                                DMA("dsp", stk[0:64, kv, :], src[k * 64:(k + 1) * 64, :], [sres], [r_stk])
                                MSET("pool", stk[64:128, kv, T - 1:T], 0.0, [r_stk])
                                DMA("dact", stk[64:128, kv, 0:T - 1], src[k * 64:(k + 1) * 64, 1:T], [sres], [r_stk])
                            CP("dve", peb[:], pes[:], [r_pes], [r_peb])
                            CP("dve", w2b[:], w2s[:], [r_w2s], [r_w2b])
                            MSET("pool", gh[:], 0.0, [r_gh])
                            for kv in range(2):
                                for hh in range(2):
                                    px, px_r = pX.next()
                                    for m in range(16):
                                        MM(px[:, 0:1], w1b[:, kv, m, hh * 128:(hh + 1) * 128], peb[:, kv, m:m + 1], m == 0, m == 15, [r_w1b, r_peb], [px_r])
                                    CP("dve", hb[:, kv * 2 + hh:kv * 2 + hh + 1], px[:, 0:1], [px_r], [r_hb])
                                    p_s, p_r = pS.next()
                                    for m in range(16):
                                        MM(p_s[:, 0:255], w1b[:, kv, m, hh * 128:(hh + 1) * 128], stk[:, kv, 2 * m:2 * m + 16 * 254 + 1:16], m == 0, m == 15, [r_w1b, r_stk], [p_r])
                                    ACTF(gh[:, kv, hh, 0:255], p_s[:, 0:255], AF.Gelu_apprx_tanh, [p_r, r_hb], [r_gh], bias=hb[:, kv * 2 + hh:kv * 2 + hh + 1])
                            px, px_r = pX.next()
                            for hh in range(2):
                                MM(px[0:64, 0:256], w2b[:, 0, hh, :], gh[:, 0, hh, :], hh == 0, hh == 1, [r_w2b, r_gh], [px_r])
                            CP("dve", KcmpT[:], px[0:64, 0:256], [px_r], [r_Kc])
                            for ct in range(2):
                                px, px_r = pX.next()
                                for hh in range(2):
                                    MM(px[:, 0:64], gh[:, 1, hh, ct * 128:(ct + 1) * 128], w2b[:, 1, hh, :], hh == 0, hh == 1, [r_gh, r_w2b], [px_r])
                                CP("dve", Vco[:, ct, 0:64], px[:, 0:64], [px_r], [r_Vco])
                            CP("pool", Vco[:, :, 64:129], ovl_t[:], [r_ovl], [r_Vco])
                            if k == 0:
                                dump("d_kcmp", KcmpT[:], [64, 256], BF16, r_Kc)
                                dump("d_vco", Vco[:], [128, 2, 129], BF16, r_Vco)
                                dump("d_hb", hb[:], [128, 4], F32, r_hb)
                                dump("d_gh", gh[:], [128, 2, 2, 256], BF16, r_gh)

                        QT = sb(stg, "QT", [128, 4, T], BF16)
                        r_QT = [Res() for _ in range(4)]
                        r_QM = [[Res() for _ in range(NT)] for _ in range(4)]
                        KsT = sb(stg, "KsT", [128, T], BF16); r_KsT = Res()
                        KwT = sb(stg, "KwT", [64, T], BF16); r_KwT = Res()
                        Vs = sb(stg, "Vs", [128, NT, 65], BF16); r_Vs = Res()
                        Vw = sb(stg, "Vw", [128, NT, 65], BF16); r_Vw = Res()
                        imp_acc = sb(stg, "imp_acc", [128, NT, 64], F32)
                        r_imp = [Res() for _ in range(NT)]
                        for g in range(4):
                            hg = 4 * k + g
                            DMA("dsp" if g % 2 == 0 else "dact", QT[0:64, g, :], qT_s[hg * 64:(hg + 1) * 64, :], [R["qT_s"]], [r_QT[g]])
                        DMA("dsp", KsT[0:64, :], ksT_s[k * 64:(k + 1) * 64, :], [R["ksT_s"]], [r_KsT])
                        with Scope(kb) as ste:
                            ers = sb(ste, "ers", [128, T], F32); r_ers = Res()
                            DMA("dact", ers[64:128, :], erows, [], [r_ers])
                            CP("pool", KsT[64:128, :], ers[64:128, :], [r_ers], [r_KsT])
                        DMA("dact", KwT[:], kwT_s[k * 64:(k + 1) * 64, :], [R["kwT_s"]], [r_KwT])
                        DMA("dsp", Vs[:, :, 0:64], vs_s.rearrange("(n p) c -> p n c", p=128)[:, :, k * 64:(k + 1) * 64], [R["vs_s"]], [r_Vs])
                        DMA("dact", Vw[:, :, 0:64], vw_s.rearrange("(n p) c -> p n c", p=128)[:, :, k * 64:(k + 1) * 64], [R["vw_s"]], [r_Vw])
                        MSET("pool", Vs[:, :, 64:65], 1.0, [r_Vs])
                        MSET("pool", Vw[:, :, 64:65], 1.0, [r_Vw])

                        bcs = Ring([sb(stg, f"bcs{i}", [128, 5, 512], F32) for i in range(2)])
                        BC = Ring([sb(stg, f"BCb{i}", [128, 5, 512], BF16) for i in range(2)])
                        bc_cur = {}

                        def cmp_stage1(it):
                            g, tcn, ct, last = it
                            hg = 4 * k + g
                            if tcn == 0 and ct == 0:
                                bs_t, bs_r = bcs.next()
                                DMA("dsp", bs_t[:, 0:3], bc_g[hg, :, 0:3], [], [bs_r])
                                DMA("dact", bs_t[:, 3:5], bc_g[hg, :, 3:5], [], [bs_r])
                                bc_t, bc_r = BC.next()
                                for m in range(5):
                                    STT(bc_t[:, m, :], bs_t[:, m, :], t31[:, hg:hg + 1], bcm[:, m, :], ALU.subtract, ALU.add, [bs_r, r_bcm, r_t31], [bc_r])
                                bc_cur[g] = (bc_t, bc_r)
                            bc_t, bc_r = bc_cur[g]
                            mp = tcn - 4 * ct
                            p_s, p_r = pS.next()
                            MM(p_s[:], KcmpT[:, ct * 128:(ct + 1) * 128], QT[0:64, g, tcn * 512:(tcn + 1) * 512], True, mp >= 5, [r_Kc, r_QT[g]], [p_r])
                            if mp < 5:
                                MM(p_s[:], ident_b[:], bc_t[:, mp, :], False, True, [r_identb, bc_r], [p_r])
                            P_t, P_r = P_ring.next()
                            ACTF(P_t[:], p_s[:], AF.Exp, [p_r, r_t31], [P_r], bias=t31[:, hg:hg + 1])
                            return (P_t, P_r)

                        def cmp_stage2(it, st1):
                            g, tcn, ct, last = it
                            hg = 4 * k + g
                            P_t, P_r = st1
                            for q in range(4):
                                MM(pO[q][:, 0:129], P_t[:, q * 128:(q + 1) * 128], Vco[:, ct, :], ct == 0, last, [P_r, r_Vco], [r_pO[q]])
                            if last:
                                items = []
                                for q in range(4):
                                    i = 4 * tcn + q
                                    o_t, o_r = osb.next()
                                    CP("dve", o_t[:, 0:129], pO[q][:, 0:129], [r_pO[q]], [o_r])
                                    items.append((o_t, o_r, i, imp_acc[:, i, :], r_imp[i]))
                                finish_tiles(items, 128, hg, 0, True, imp_first=(g == 0))

                        its = []
                        for g in range(4):
                            for tcn in range(8):
                                cts = [0] if tcn < 4 else [0, 1]
                                for ct in cts:
                                    its.append((g, tcn, ct, ct == cts[-1]))
                        LAG = 2
                        pend = []
                        for n in range(len(its) + LAG):
                            if n < len(its):
                                pend.append((its[n], cmp_stage1(its[n])))
                            if n >= LAG:
                                it0, st0 = pend.pop(0)
                                cmp_stage2(it0, st0)

                        if k == 0:
                            dump("d_imp", imp_acc[:], [128, NT, 64], F32, r_imp)
                            dump("d_aout_c", Aout[:], [128, NT, 512], BF16, rA)
                        MBr = Ring([sb(stg, f"MB{i}", [128, 128], F32) for i in range(2)])
                        for (mb_t, mb_r) in zip(MBr.tiles, MBr.res):
                            MSET("dve", mb_t[:], 0.0, [mb_r])
                        tk = Ring([sb(stg, f"tk{i}", [128, 2, 64], F32) for i in range(2)])
                        mxr = Ring([sb(stg, f"mx{i}", [128, 16], F32) for i in range(2)])
                        mtr = Ring([sb(stg, f"mtr{i}", [128, 128], BF16) for i in range(2)])
                        def c3_gen():
                          for i in range(NT):
                            tk_t, tk_r = tk.next()
                            mx_t, mx_r = mxr.next()
                            TT("dve", tk_t[:, 0, :], imp_acc[:, i, :], keep_t[:, i, :], ALU.mult, [r_imp[i], r_keep], [tk_r])
                            TT("dve", tk_t[:, 0, :], tk_t[:, 0, :], force_t[:, i, :], ALU.add, [tk_r, r_force], [tk_r])
                            E("dve", lambda: nc.vector.max(out=mx_t[:, 0:8], in_=tk_t[:, 0, :]), [tk_r], [mx_r])
                            E("dve", lambda: nc.vector.match_replace(out=tk_t[:, 1, :], in_to_replace=mx_t[:, 0:8], in_values=tk_t[:, 0, :], imm_value=-1e30), [tk_r, mx_r], [tk_r])
                            E("dve", lambda: nc.vector.max(out=mx_t[:, 8:16], in_=tk_t[:, 1, :]), [tk_r], [mx_r])
                            mb_t, mb_r = MBr.next()
                            TS("dve", mb_t[:, 64:128], tk_t[:, 0, :], mx_t[:, 15:16], None, ALU.is_ge, None, [tk_r, mx_r], [mb_r])
                            TS("dve", mb_t[:, 64:128], mb_t[:, 64:128], 1.0, -NEGM, ALU.subtract, ALU.mult, [mb_r], [mb_r])
                            px, px_r = pX.next()
                            TR(px[:, 0:128], mb_t[:], ident_f[:], [mb_r, r_identf], [px_r])
                            mt_t, mt_r = mtr.next()
                            CP("act", mt_t[64:128, :], px[64:128, 0:128], [px_r], [mt_r])
                            for g in range(4):
                                CP("pool" if g % 2 == 0 else "dve", QT[64:128, g, i * 128:(i + 1) * 128], mt_t[64:128, :], [mt_r], [r_QM[g][i]])
                            yield

                        if k == 0:
                            dump("d_qt0", QT[:, 0, :], [128, T], BF16, r_QT + [x for l in r_QM for x in l])
                        def sel_stage1(it):
                            g, br, tcn, j = it
                            hg = 4 * k + g
                            qa = max(0, j - 4 * tcn)
                            qb = 3 if br == 1 else min(3, j + 4 - 4 * tcn)
                            c0, c1 = qa * 128, (qb + 1) * 128
                            t0 = tcn * 512
                            adds = []
                            for q in range(qa, qb + 1):
                                dlt = 4 * tcn + q - j
                                if dlt == 0:
                                    adds.append((q, 0))
                                elif dlt == 1:
                                    adds.append((q, 1))
                                elif dlt == 4 and br == 2:
                                    adds.append((q, 2))
                            p_s, p_r = pS.next()
                            if br == 1:
                                rd = [r_KsT, r_QT[g]] + [r_QM[g][4 * tcn + q] for q in range(qa, qb + 1)]
                                MM(p_s[:, c0:c1], KsT[:, j * 128:(j + 1) * 128], QT[:, g, t0 + c0:t0 + c1], True, len(adds) == 0, rd, [p_r])
                            else:
                                MM(p_s[:, c0:c1], KwT[:, j * 128:(j + 1) * 128], QT[0:64, g, t0 + c0:t0 + c1], True, len(adds) == 0, [r_KwT, r_QT[g]], [p_r])
                            for ai, (q, ty) in enumerate(adds):
                                MM(p_s[:, q * 128:(q + 1) * 128], ident_b[:], BD[:, hg, ty, :], False, ai == len(adds) - 1, [r_identb, r_BD], [p_r])
                            P_t, P_r = P_ring.next()
                            ACTF(P_t[:, c0:c1], p_s[:, c0:c1], AF.Exp, [p_r, r_t31], [P_r], bias=t31[:, hg:hg + 1])
                            return (P_t, P_r, qa, qb)

                        def sel_stage2(it, st1):
                            g, br, tcn, j = it
                            hg = 4 * k + g
                            P_t, P_r, qa, qb = st1
                            Vx, r_Vx = (Vs, r_Vs) if br == 1 else (Vw, r_Vw)
                            for q in range(qa, qb + 1):
                                i = 4 * tcn + q
                                first_j = 0 if br == 1 else max(0, i - 4)
                                MM(pO[q][:, 0:65], P_t[:, q * 128:(q + 1) * 128], Vx[:, j, :], j == first_j, j == i, [P_r, r_Vx], [r_pO[q]])
                            if j == 4 * tcn + 3:
                                items = []
                                for q in range(4):
                                    o_t, o_r = osb.next()
                                    CP("dve", o_t[:, 0:65], pO[q][:, 0:65], [r_pO[q]], [o_r])
                                    items.append((o_t, o_r, 4 * tcn + q, None, None))
                                finish_tiles(items, 64, hg, br, False)

                        def branch_gen(br):
                            its = []
                            for g in range(4):
                                for tcn in range(8):
                                    j_lo = 0 if br == 1 else max(0, 4 * tcn - 4)
                                    for j in range(j_lo, 4 * tcn + 4):
                                        its.append((g, br, tcn, j))
                            LAG = 2
                            pend = []
                            for n in range(len(its) + LAG):
                                if n < len(its):
                                    pend.append((its[n], sel_stage1(its[n])))
                                if n >= LAG:
                                    it0, st0 = pend.pop(0)
                                    sel_stage2(it0, st0)
                                if n % 4 == 3:
                                    yield

                        drive_rr([branch_gen(2), c3_gen()])
                        drive_rr([branch_gen(1)])

                dump("d_aout", Aout[:], [128, NT, 512], BF16, rA)
                with Scope(kb) as stn:
                    junkC = sb(stn, "junkC", [128, 512], BF16); r_junkC = Res()
                    an = Ring([sb(stn, f"an{i}", [128, 512], BF16) for i in range(2)])
                    af = Ring([sb(stn, f"af{i}", [128, 512], F32) for i in range(2)])
                    ao = Ring([sb(stn, f"ao{i}", [128, 512], BF16) for i in range(2)])
                    pTb = Ring([ps(stn, f"pTC{i}", [128, 512], BF16) for i in range(2)]) if False else None
                    for i in range(NT):
                        s_t, s_r = sm.next()
                        ACTF(junkC[:], Aout[:, i, :], AF.Square, [rA[i]], [r_junkC, s_r], accum_out=s_t[:, 0:1])
                        TS("dve", s_t[:, 1:2], s_t[:, 0:1], 1.0 / 512, EPS, ALU.mult, ALU.add, [s_r], [s_r])
                        ACTF(s_t[:, 1:2], s_t[:, 1:2], AF.Sqrt, [s_r], [s_r])
                        E("dve", lambda: nc.vector.reciprocal(out=s_t[:, 2:3], in_=s_t[:, 1:2]), [s_r], [s_r])
                        af_t, af_r = af.next()
                        STT(af_t[:], Aout[:, i, :], s_t[:, 2:3], ga_t[:], ALU.mult, ALU.mult, [rA[i], s_r, r_ga], [af_r])
                        px, px_r = pX.next()
                        for fc in range(4):
                            TR(px[:, fc * 128:(fc + 1) * 128], af_t[:, fc * 128:(fc + 1) * 128], ident_f[:], [af_r, r_identf], [px_r])
                        ao_t, ao_r = ao.next()
                        CP("act", ao_t[:], px[:], [px_r], [ao_r])
                        DMA("dsp" if i % 2 == 0 else "dpool", mixT_s[0:512, i * 128:(i + 1) * 128].rearrange("(f p) t -> p f t", p=128),
                            ao_t[:].rearrange("p (f t) -> p f t", f=4), [ao_r], [R["mixT_s"]])

        if "D" in phases or "D1" in phases:
            NTL = TH // 128
            with Scope(kb) as st:
                Wo = sb(st, "Wo", [128, 8, D], BF16); r_Wo = Res()
                Wq = sb(st, "Wq", [128, 8, 2048], BF16); r_Wq = Res()
                skb = sb(st, "skb", [128, 2, 128], BF16); r_skb = Res()
                repf = sb(st, "repf", [128, D], F32); r_rep = Res()
                io16 = sb(st, "io16", [128, 16], F32); r_io = Res()
                io128 = sb(st, "io128", [128, 128], F32); r_io128 = Res()
                selt = sb(st, "selt", [128, 2], F32); r_sel = Res()
                DMA("dsp", repf[:], rep4[:, 0, :], [], [r_rep])
                DMA("dact", io16[:], iota16, [], [r_io])
                DMA("dact", io128[:], iota128, [], [r_io128])
                DMA("dact", selt[:], selc, [], [r_sel])
                with Scope(kb) as stw:
                    wst = Ring([sb(stw, f"wstD{i}", [128, 2048], F32) for i in range(3)])
                    n = 0
                    for (src, dstw, dres, ncol, nch) in ((w_out_in, Wo, r_Wo, D, 8), (peer_wq, Wq, r_Wq, 2048, 8)):
                        for dc in range(nch):
                            w_t, w_r = wst.next()
                            n += 1
                            DMA("dsp" if n % 2 == 0 else "dact", w_t[:, 0:ncol], src[dc * 128:(dc + 1) * 128, :], [], [w_r])
                            CP("dve" if n % 2 == 0 else "pool", dstw[:, dc, :], w_t[:, 0:ncol], [w_r], [dres])
                    w_t, w_r = wst.next()
                    DMA("dsp", w_t[:, 0:256].rearrange("p (a k) -> p a k", a=2), sk_T.rearrange("a p k -> p a k"), [], [w_r])
                    CP("dve", skb[:], w_t[:, 0:256].rearrange("p (a k) -> p a k", a=2), [w_r], [r_skb])

                pacc = Ring([ps(st, f"pD{i}", [128, 512], F32) for i in range(4)])
                pw_ring = Ring([ps(st, f"pDw{i}", [128, 512], F32) for i in range(2)])
                ptb = Ring([ps(st, f"pDb{i}", [128, 1024], BF16) for i in range(2)])
                mst = Ring([sb(st, f"mst{i}", [128, 8, 2, 128], BF16) for i in range(2)])
                mixh_ring = Ring([sb(st, f"mixh{i}", [128, 8, 128], BF16) for i in range(1)])
                xh_ring = Ring([sb(st, f"xhD{i}", [128, D], F32) for i in range(2)])
                H_ring = Ring([sb(st, f"HD{i}", [128, D], F32) for i in range(2)])
                xnb_ring = Ring([sb(st, f"xnbD{i}", [128, D], BF16) for i in range(1)])
                xT_ring = Ring([sb(st, f"xTD{i}", [128, 8, 128], BF16) for i in range(2)])
                qTb = sb(st, "qTb", [128, 16, 128], BF16); r_qTb = Res()
                Ssc_ring = Ring([sb(st, f"Ssc{i}", [128, 16, 128], F32) for i in range(2)])
                Swk = sb(st, "Swk", [128, 8, 128], F32)
                rv = [Res() for _ in range(16)]; rv2 = [Res() for _ in range(16)]; ri = [Res() for _ in range(16)]; ri2 = [Res() for _ in range(16)]; rw = [Res() for _ in range(16)]
                v16 = sb(st, "v16", [128, 16, 16], F32); r_v16 = Res()
                i16 = sb(st, "i16", [128, 16, 16], U32); r_i16 = Res()
                i16f = sb(st, "i16f", [128, 16, 16], F32); r_i16f = Res()
                cand = sb(st, "cand", [128, 8, 256], F32); r_cand = Res()
                cwk = sb(st, "cwk", [128, 8, 256], F32)
                sc16 = sb(st, "sc16", [128, 8, 16], F32); r_sc = Res()
                ci16 = sb(st, "ci16", [128, 8, 16], U32); r_ci = Res()
                ab_u = sb(st, "ab_u", [128, 2, 8, 16], U32); r_abu = Res()
                ab_f = sb(st, "ab_f", [128, 2, 8, 16], F32); r_abf = Res()
                eq = sb(st, "eq", [128, 8, 16, 16], F32); r_eq = Res()
                isel_ring = Ring([sb(st, f"isel{i}", [128, 3, 8, 16], F32) for i in range(2)])
                gz = sb(st, "gz", [128, 16], F32); r_gz = Res()
                junkB = sb(st, "junkDb", [128, D], BF16); r_junkB = Res()
                smD = Ring([sb(st, f"smD{i}", [128, 8], F32) for i in range(4)])
                ijgT_ring = Ring([sb(st, f"ijgT{i}", [128, 3, 128], F32) for i in range(2)])
                OI = Ring([sb(st, f"OI{i}", [128, 16, 128], BF16) for i in range(2)])
                OJ = Ring([sb(st, f"OJ{i}", [128, 16, 128], BF16) for i in range(2)])
                OJf = Ring([sb(st, f"OJf{i}", [128, 16, 128], BF16) for i in range(2)])
                Wst = sb(st, "Wst", [128, 128, 128], BF16); r_Wst = Res()

                def rms_scaled(src, src_r, gain, gain_r, dstf, dstf_r):
                    s_t, s_r = smD.next()
                    ACTF(junkB[:], src, AF.Square, [src_r], [r_junkB, s_r], accum_out=s_t[:, 0:1])
                    TS("dve", s_t[:, 1:2], s_t[:, 0:1], 1.0 / D, EPS, ALU.mult, ALU.add, [s_r], [s_r])
                    ACTF(s_t[:, 1:2], s_t[:, 1:2], AF.Sqrt, [s_r], [s_r])
                    E("dve", lambda: nc.vector.reciprocal(out=s_t[:, 2:3], in_=s_t[:, 1:2]), [s_r], [s_r])
                    STT(dstf, src, s_t[:, 2:3], gain, ALU.mult, ALU.mult, [src_r, s_r, gain_r], [dstf_r])

                def rms_scaled_g(src, src_r, gain, gain_r, dstf, dstf_r):
                    s_t, s_r = smD.next()
                    ACTF(junkB[:], src, AF.Square, [src_r], [r_junkB, s_r], accum_out=s_t[:, 0:1])
                    yield
                    TS("dve", s_t[:, 1:2], s_t[:, 0:1], 1.0 / D, EPS, ALU.mult, ALU.add, [s_r], [s_r])
                    ACTF(s_t[:, 1:2], s_t[:, 1:2], AF.Sqrt, [s_r], [s_r])
                    yield
                    E("dve", lambda: nc.vector.reciprocal(out=s_t[:, 2:3], in_=s_t[:, 1:2]), [s_r], [s_r])
                    STT(dstf, src, s_t[:, 2:3], gain, ALU.mult, ALU.mult, [src_r, s_r, gain_r], [dstf_r])

                def transpose8(srcb, srcb_r, dstT, dstT_r, nblk=8):
                    pt, pt_r = ptb.next()
                    for dc in range(nblk):
                        TR(pt[:, dc * 128:(dc + 1) * 128], srcb[:, dc * 128:(dc + 1) * 128], ident_b[:], [srcb_r, r_identb], [pt_r])
                    CP("act", dstT.rearrange("p a t -> p (a t)"), pt[:, 0:nblk * 128], [pt_r], [dstT_r])

                def S1_load(it):
                    tsl = slice(it * 128, (it + 1) * 128)
                    xh_t, xh_r = xh_ring.next()
                    DMA("dsp", xh_t[:], xh[tsl, :], [], [xh_r])
                    m_t, m_r = mst.next()
                    for a in range(2):
                        DMA("dsp" if a == 0 else "dact", m_t[:, :, a, :], mixT_s[:, a * TH + it * 128:a * TH + (it + 1) * 128].rearrange("(f p) t -> p f t", p=128), [R["mixT_s"]], [m_r])
                    return (xh_t, xh_r, m_t, m_r)

                def S1(it, ld):
                    tsl = slice(it * 128, (it + 1) * 128)
                    xh_t, xh_r, m_t, m_r = ld
                    H, H_r = H_ring.next()
                    mixh, r_mixh = mixh_ring.next()
                    ACTF(mixh[:], m_t[:, :, 0, :], AF.Copy, [m_r, r_sel], [r_mixh], scale=selt[:, 0:1])
                    STT(mixh[:], m_t[:, :, 1, :], selt[:, 1:2], mixh[:], ALU.mult, ALU.add, [m_r, r_sel, r_mixh], [r_mixh])
                    yield
                    for ch in range(2):
                        pa, pa_r = pacc.next()
                        for fc in range(8):
                            MM(pa[:], mixh[:, fc, :], Wo[:, fc, ch * 512:(ch + 1) * 512], fc == 0, fc == 7, [r_mixh, r_Wo], [pa_r])
                        TT("dve", H[:, ch * 512:(ch + 1) * 512], pa[:], xh_t[:, ch * 512:(ch + 1) * 512], ALU.add, [pa_r, xh_r], [H_r])
                        yield
                    DMA("dpool", H1_s[tsl, :], H[:], [H_r], [R["H1_s"]])
                    xnb, xnb_r = xnb_ring.next()
                    yield from rms_scaled_g(H[:], H_r, repf[:], r_rep, xnb[:], xnb_r)
                    yield
                    xT, xT_r = xT_ring.next()
                    transpose8(xnb, xnb_r, xT[:], xT_r)
                    yield
                    DMA("dact", xnT2_s[:, :, tsl], xT[:], [xT_r], [R["xnT2_s"]])
                    for grp in range(4):
                        pa, pa_r = pacc.next()
                        for j in range(4):
                            hp = grp * 4 + j
                            for dc in range(8):
                                MM(pa[:, j * 128:(j + 1) * 128], Wq[:, dc, hp * 128:(hp + 1) * 128], xT[:, dc, :], dc == 0, dc == 7, [r_Wq, xT_r], [pa_r])
                        CP("act", qTb[:, grp * 4:(grp + 1) * 4, :].rearrange("p a t -> p (a t)"), pa[:], [pa_r], [r_qTb])
                        yield
                    Ssc, r_S = Ssc_ring.next()
                    for grp in range(4):
                        pa, pa_r = pacc.next()
                        for j in range(4):
                            hp = grp * 4 + j
                            MM(pa[:, j * 128:(j + 1) * 128], qTb[:, hp, :], skb[:, hp % 2, :], True, True, [r_qTb, r_skb], [pa_r])
                        CP("act", Ssc[:, grp * 4:(grp + 1) * 4, :].rearrange("p a t -> p (a t)"), pa[:], [pa_r], [r_S])
                        yield
                    return (Ssc, r_S)

                def S2(it, st1):
                    Ssc, r_S = st1
                    for g0 in (0, 8):
                        hps = range(g0, g0 + 8)
                        for hp in hps:
                            E("dve", lambda: nc.vector.max(out=v16[:, hp, 0:8], in_=Ssc[:, hp, :]), [r_S], [rv[hp]])
                        yield
                        for hp in hps:
                            E("dve", lambda: nc.vector.max_index(out=i16[:, hp, 0:8], in_max=v16[:, hp, 0:8], in_values=Ssc[:, hp, :]), [r_S, rv[hp]], [ri[hp]])
                        yield
                        for hp in hps:
                            E("dve", lambda: nc.vector.match_replace(out=Swk[:, hp - g0, :], in_to_replace=v16[:, hp, 0:8], in_values=Ssc[:, hp, :], imm_value=-1e30), [r_S, rv[hp]], [rw[hp - g0]])
                        yield
                        for hp in hps:
                            E("dve", lambda: nc.vector.max(out=v16[:, hp, 8:16], in_=Swk[:, hp - g0, :]), [rw[hp - g0]], [rv2[hp]])
                        yield
                        for hp in hps:
                            E("dve", lambda: nc.vector.max_index(out=i16[:, hp, 8:16], in_max=v16[:, hp, 8:16], in_values=Swk[:, hp - g0, :]), [rw[hp - g0], rv2[hp]], [ri2[hp]])
                        yield
                    r_i16 = Res()
                    E("dve", lambda: nc.vector.tensor_copy(out=i16f[:], in_=i16[:]), ri + ri2, [r_i16f, r_i16])
                    v4 = v16[:].rearrange("p (h two) k -> p h two k", two=2)
                    in0 = v4[:, :, 0, :].rearrange("p h (a o) -> p h a o", o=1).to_broadcast([128, 8, 16, 16])
                    in1 = v4[:, :, 1, :].rearrange("p h (o b) -> p h o b", o=1).to_broadcast([128, 8, 16, 16])
                    TT("dve", cand[:].rearrange("p h (a b) -> p h a b", a=16), in0, in1, ALU.add, rv + rv2, [r_cand])
                    for h in range(8):
                        E("dve", lambda: nc.vector.max(out=sc16[:, h, 0:8], in_=cand[:, h, :]), [r_cand], [rv[h]])
                    yield
                    for h in range(8):
                        E("dve", lambda: nc.vector.max_index(out=ci16[:, h, 0:8], in_max=sc16[:, h, 0:8], in_values=cand[:, h, :]), [r_cand, rv[h]], [ri[h]])
                    yield
                    for h in range(8):
                        E("dve", lambda: nc.vector.match_replace(out=cwk[:, h, :], in_to_replace=sc16[:, h, 0:8], in_values=cand[:, h, :], imm_value=-1e30), [r_cand, rv[h]], [rw[h]])
                    yield
                    for h in range(8):
                        E("dve", lambda: nc.vector.max(out=sc16[:, h, 8:16], in_=cwk[:, h, :]), [rw[h]], [rv2[h]])
                    yield
                    for h in range(8):
                        E("dve", lambda: nc.vector.max_index(out=ci16[:, h, 8:16], in_max=sc16[:, h, 8:16], in_values=cwk[:, h, :]), [rw[h], rv2[h]], [ri2[h]])
                    yield
                    r_sc = Res(); r_ci = Res()
                    E("dve", lambda: nc.vector.tensor_single_scalar(out=ab_u[:, 0], in_=ci16[:], scalar=4, op=ALU.logical_shift_right), ri[:8] + ri2[:8] + rv[:8] + rv2[:8], [r_abu, r_sc, r_ci])
                    E("dve", lambda: nc.vector.tensor_single_scalar(out=ab_u[:, 1], in_=ci16[:], scalar=15, op=ALU.bitwise_and), [r_ci], [r_abu])
                    CP("dve", ab_f[:], ab_u[:], [r_abu], [r_abf])
                    isel, r_isel = isel_ring.next()
                    i4 = i16f[:].rearrange("p (h two) k -> p h two k", two=2)
                    for w in range(2):
                        a_b = ab_f[:, w].rearrange("p h (k o) -> p h k o", o=1).to_broadcast([128, 8, 16, 16])
                        io_b = io16[:].rearrange("p (o q a) -> p o q a", o=1, q=1).to_broadcast([128, 8, 16, 16])
                        TT("dve", eq[:], a_b, io_b, ALU.is_equal, [r_abf, r_io], [r_eq])
                        iv_b = i4[:, :, w, :].rearrange("p h (o a) -> p h o a", o=1).to_broadcast([128, 8, 16, 16])
                        TT("dve", eq[:], eq[:], iv_b, ALU.mult, [r_eq, r_i16f], [r_eq])
                        E("dve", lambda: nc.vector.tensor_reduce(out=isel[:, w], in_=eq[:], axis=AX.X, op=ALU.add), [r_eq], [r_isel])
                        yield
                    TT("dve", isel[:, 2], sc16[:], sc16[:, :, 0:1].to_broadcast([128, 8, 16]), ALU.subtract, [r_sc], [r_isel])
                    ACTF(isel[:, 2], isel[:, 2], AF.Exp, [r_isel], [r_isel])
                    E("dve", lambda: nc.vector.tensor_reduce(out=gz[:, 0:8], in_=isel[:, 2], axis=AX.X, op=ALU.add), [r_isel], [r_gz])
                    E("dve", lambda: nc.vector.reciprocal(out=gz[:, 8:16], in_=gz[:, 0:8]), [r_gz], [r_gz])
                    TT("dve", isel[:, 2], isel[:, 2], gz[:, 8:16].rearrange("p (h o) -> p h o", o=1).to_broadcast([128, 8, 16]), ALU.mult, [r_isel, r_gz], [r_isel])
                    pa, pa_r = pacc.next()
                    for w in range(3):
                        TR(pa[:, w * 128:(w + 1) * 128], isel[:, w].rearrange("p h k -> p (h k)"), ident_f[:], [r_isel, r_identf], [pa_r])
                    ijgT, r_ijgT = ijgT_ring.next()
                    CP("act", ijgT[:].rearrange("p a t -> p (a t)"), pa[:, 0:384], [pa_r], [r_ijgT])
                    return (ijgT, r_ijgT)

                def S3(it, st2):
                    ijgT, r_ijgT = st2
                    TB = 16
                    io_b = io128[:].rearrange("p (o i) -> p o i", o=1).to_broadcast([128, TB, 128])

                    def onehots(tb):
                        t0 = tb * TB
                        oi, oi_r = OI.next()
                        oj, oj_r = OJ.next()
                        ojf, ojf_r = OJf.next()

                        def colb(w):
                            return ijgT[:, w, t0:t0 + TB].rearrange("p (t o) -> p t o", o=1).to_broadcast([128, TB, 128])
                        TT("dve", oi[:], io_b, colb(0), ALU.is_equal, [r_io128, r_ijgT], [oi_r])
                        TT("dve", ojf[:], io_b, colb(1), ALU.is_equal, [r_io128, r_ijgT], [ojf_r])
                        TT("pool", oj[:], ojf[:], colb(2), ALU.mult, [ojf_r, r_ijgT], [oj_r])
                        return (oi, oi_r, oj, oj_r)

                    nxt = onehots(0)
                    yield
                    for tb in range(128 // TB):
                        t0 = tb * TB
                        oi, oi_r, oj, oj_r = nxt
                        if tb + 1 < 128 // TB:
                            nxt = onehots(tb + 1)
                        for tq in range(TB // 4):
                            pw, pw_r = pw_ring.next()
                            for u in range(4):
                                MM(pw[:, u:512:4], oj[:, tq * 4 + u, :], oi[:, tq * 4 + u, :], True, True, [oj_r, oi_r], [pw_r])
                            tg = t0 + tq * 4
                            CP("act", Wst[:, :, tg:tg + 4], pw[:].rearrange("p (i t) -> p i t", t=4), [pw_r], [r_Wst])
                            yield
                    DMA("dsp" if it % 2 == 0 else "dact", Wt_s[it], Wst[:], [r_Wst], [R["Wt_s"]])

                def drive(gens):
                    res = [None] * len(gens)
                    live = list(range(len(gens)))
                    while live:
                        for gi in list(live):
                            try:
                                next(gens[gi])
                            except StopIteration as e:
                                res[gi] = e.value
                                live.remove(gi)
                    return res

                st1s, st2s = {}, {}
                lds = {0: S1_load(0)}
                for n in range(NTL + 2):
                    gens, tags, wts = [], [], []
                    if n + 1 < NTL:
                        lds[n + 1] = S1_load(n + 1)
                    if n < NTL:
                        gens.append(S1(n, lds.pop(n))); tags.append(("s1", n)); wts.append(1)
                    if n >= 2:
                        gens.append(S3(n - 2, st2s.pop(n - 2))); tags.append(("s3", n - 2)); wts.append(2)
                    if 1 <= n <= NTL:
                        gens.append(S2(n - 1, st1s.pop(n - 1))); tags.append(("s2", n - 1)); wts.append(2)
                    for (tg_, tn), rv_ in zip(tags, drive_rr(gens, wts)):
                        if tg_ == "s1":
                            st1s[tn] = rv_
                        elif tg_ == "s2":
                            st2s[tn] = rv_

            with Scope(kb) as st:
                Yacc = sb(st, "Yacc", [128, NTL, D], F32)
                rY = [Res() for _ in range(NTL)]
                H1v = H1_s.rearrange("(n p) d -> p n d", p=128)
                for n4 in range(4):
                    DMA("dsp" if n4 % 2 == 0 else "dact", Yacc[:, n4 * 4:(n4 + 1) * 4, :], H1v[:, n4 * 4:(n4 + 1) * 4, :], [R["H1_s"]], rY[n4 * 4:(n4 + 1) * 4])
                p1 = Ring([ps(st, f"pE1{i}", [128, 512], F32) for i in range(3)])
                p2 = Ring([ps(st, f"pE2{i}", [128, 512], F32) for i in range(3)])
                ptb2 = Ring([ps(st, f"pEb{i}", [128, 1024], BF16) for i in range(2)])
                with Scope(kb) as st2:
                  if "D" in phases or "D2" in phases:
                    xnTa = sb(st2, "xnTa", [128, 8, TH], BF16); r_xnTa = Res()
                    for dc in range(8):
                        DMA("dsp" if dc % 2 == 0 else "dact", xnTa[:, dc, :], xnT2_s[:, dc, :], [R["xnT2_s"]], [r_xnTa])
                    IB = 4
                    NB = 128 // IB
                    ust = Ring([sb(st2, f"ust{i}", [128, D], F32) for i in range(2)])
                    vst = Ring([sb(st2, f"vst{i}", [128, D], F32) for i in range(2)])
                    ub = Ring([sb(st2, f"ub{i}", [128, D], BF16) for i in range(2)])
                    uT = Ring([sb(st2, f"uT{i}", [128, 8, 128], BF16) for i in range(2)])
                    Vbs = [sb(st2, f"Vb{i}", [128, IB, D], BF16) for i in range(2)]
                    WAs = [sb(st2, f"WA{i}", [128, IB, TH], BF16) for i in range(2)]
                    r_Vbs = [[Res() for _ in range(IB)] for _ in range(2)]
                    r_WAs = [[Res() for _ in range(IB)] for _ in range(2)]
                    wt = Ring([sb(st2, f"wt{i}", [128, TH], BF16) for i in range(3)])
                    gl = Ring([sb(st2, f"gl{i}", [128, 512], BF16) for i in range(3)])

                    def genS1(blk):
                        Vb, WA, r_Vb, r_WA = Vbs[blk % 2], WAs[blk % 2], r_Vbs[blk % 2], r_WAs[blk % 2]
                        for ib in range(IB):
                            i = blk * IB + ib
                            u_t, u_r = ust.next()
                            v_t, v_r = vst.next()
                            w_t, w_r = wt.next()
                            DMA("dsp", u_t[:], peer_u[i * 128:(i + 1) * 128, :], [], [u_r])
                            DMA("dact", v_t[:], peer_v[i * 128:(i + 1) * 128, :], [], [v_r])
                            for hw in range(2):
                                DMA("dpool" if hw == 0 else ("dsp" if i % 2 == 0 else "dact"), w_t[:, hw * 1024:(hw + 1) * 1024].rearrange("p (n t) -> p n t", t=128),
                                    Wt_s[hw * 8:(hw + 1) * 8, :, i, :].rearrange("n j t -> j n t"), [R["Wt_s"]], [w_r])
                            ub_t, ub_r = ub.next()
                            CP("pool", ub_t[:], u_t[:], [u_r], [ub_r])
                            CP("pool", Vb[:, ib, :], v_t[:], [v_r], [r_Vb[ib]])
                            pt, pt_r = ptb2.next()
                            for dc in range(8):
                                TR(pt[:, dc * 128:(dc + 1) * 128], ub_t[:, dc * 128:(dc + 1) * 128], ident_b[:], [ub_r, r_identb], [pt_r])
                            uT_t, uT_r = uT.next()
                            CP("act", uT_t[:].rearrange("p a t -> p (a t)"), pt[:], [pt_r], [uT_r])
                            yield
                            for tc4 in range(4):
                                pa, pa_r = p1.next()
                                for dc in range(8):
                                    MM(pa[:], uT_t[:, dc, :], xnTa[:, dc, tc4 * 512:(tc4 + 1) * 512], dc == 0, dc == 7, [uT_r, r_xnTa], [pa_r])
                                g_t, g_r = gl.next()
                                ACTF(g_t[:], pa[:], AF.Gelu_apprx_tanh, [pa_r], [g_r])
                                TT("dve", WA[:, ib, tc4 * 512:(tc4 + 1) * 512], g_t[:], w_t[:, tc4 * 512:(tc4 + 1) * 512], ALU.mult, [g_r, w_r], [r_WA[ib]])
                                yield

                    def genS2(blk):
                        Vb, WA, r_Vb, r_WA = Vbs[blk % 2], WAs[blk % 2], r_Vbs[blk % 2], r_WAs[blk % 2]
                        for tt in range(NTL):
                            for ch in range(2):
                                pb, pb_r = p2.next()
                                for ib in range(IB):
                                    MM(pb[:], WA[:, ib, tt * 128:(tt + 1) * 128], Vb[:, ib, ch * 512:(ch + 1) * 512], ib == 0, ib == IB - 1, [r_WA[ib], r_Vb[ib]], [pb_r])
                                TT("dve", Yacc[:, tt, ch * 512:(ch + 1) * 512], Yacc[:, tt, ch * 512:(ch + 1) * 512], pb[:], ALU.add, [rY[tt], pb_r], [rY[tt]])
                            yield

                    drive_rr([genS1(0)])
                    for blk in range(NB):
                        gens = []
                        if blk + 1 < NB:
                            gens.append(genS1(blk + 1))
                        gens.append(genS2(blk))
                        drive_rr(gens)

                with Scope(kb) as st3:
                    Wgt = sb(st3, "Wgt", [128, 8, D], BF16); r_Wgt = Res()
                    Wp = sb(st3, "Wp", [128, 2, D], BF16); r_Wp = Res()
                    rep3 = sb(st3, "rep3", [128, 3, D], F32); r_rep3 = Res()
                    DMA("dsp", rep3[:], rep4[:, 1:4, :], [], [r_rep3])
                    wst3 = Ring([sb(st3, f"wst3{i}", [128, D], F32) for i in range(2)])
                    for (src, dstw, dres, nch) in ((ple_wg, Wgt, r_Wgt, 8), (ple_pj, Wp, r_Wp, 2)):
                        for dc in range(nch):
                            w_t, w_r = wst3.next()
                            DMA("dsp" if dc % 2 == 0 else "dact", w_t[:], src[dc * 128:(dc + 1) * 128, :], [], [w_r])
                            CP("dve" if dc % 2 == 0 else "pool", dstw[:, dc, :], w_t[:], [w_r], [dres])
                    x3_ring = Ring([sb(st3, f"x3{i}", [128, D], F32) for i in range(2)])
                    x3b_ring = Ring([sb(st3, f"x3b{i}", [128, D], BF16) for i in range(2)])
                    x3T_ring = Ring([sb(st3, f"x3T{i}", [128, 8, 128], BF16) for i in range(2)])
                    pht = Ring([sb(st3, f"pht{i}", [128, 256], F32) for i in range(2)])
                    phb = Ring([sb(st3, f"phb{i}", [128, 256], BF16) for i in range(2)])
                    phT = Ring([sb(st3, f"phT{i}", [128, 2, 128], BF16) for i in range(2)])
                    gt_ring = Ring([sb(st3, f"gtD{i}", [128, D], F32) for i in range(2)])
                    ot_ring = Ring([sb(st3, f"otD{i}", [128, D], F32) for i in range(2)])
                    junk3 = sb(st3, "junk3", [128, D], BF16); r_junk3 = Res()
                    sm3 = Ring([sb(st3, f"sm3{i}", [128, 8], F32) for i in range(4)])

                    def rms3(src, src_r, gi, dstf, dstf_r):
                        s_t, s_r = sm3.next()
                        ACTF(junk3[:], src, AF.Square, [src_r], [r_junk3, s_r], accum_out=s_t[:, 0:1])
                        TS("dve", s_t[:, 1:2], s_t[:, 0:1], 1.0 / D, EPS, ALU.mult, ALU.add, [s_r], [s_r])
                        ACTF(s_t[:, 1:2], s_t[:, 1:2], AF.Sqrt, [s_r], [s_r])
                        E("dve", lambda: nc.vector.reciprocal(out=s_t[:, 2:3], in_=s_t[:, 1:2]), [s_r], [s_r])
                        STT(dstf, src, s_t[:, 2:3], rep3[:, gi, :], ALU.mult, ALU.mult, [src_r, s_r, r_rep3], [dstf_r])

                    def tr3(srcb, srcb_r, dstT, dstT_r, nblk):
                        pt, pt_r = ptb2.next()
                        for dc in range(nblk):
                            TR(pt[:, dc * 128:(dc + 1) * 128], srcb[:, dc * 128:(dc + 1) * 128], ident_b[:], [srcb_r, r_identb], [pt_r])
                        CP("act", dstT.rearrange("p a t -> p (a t)"), pt[:, 0:nblk * 128], [pt_r], [dstT_r])

                    def genD3(it):
                        tsl = slice(it * 128, (it + 1) * 128)
                        Hh = Yacc[:, it, :]; H_r = rY[it]
                        ph_t, ph_r = pht.next()
                        DMA("dact", ph_t[:], ph[tsl, :], [], [ph_r])
                        x3, x3_r = x3_ring.next()
                        rms3(Hh, H_r, 0, x3[:], x3_r)
                        yield
                        x3b, x3b_r = x3b_ring.next()
                        CP("pool", x3b[:], x3[:], [x3_r], [x3b_r])
                        pb_t, pb_r = phb.next()
                        CP("pool", pb_t[:], ph_t[:], [ph_r], [pb_r])
                        yield
                        x3T, x3T_r = x3T_ring.next()
                        tr3(x3b, x3b_r, x3T[:], x3T_r, 8)
                        pT_t, pT_r = phT.next()
                        tr3(pb_t, pb_r, pT_t[:], pT_r, 2)
                        yield
                        gt, gt_r = gt_ring.next()
                        for ch in range(2):
                            csl = slice(ch * 512, (ch + 1) * 512)
                            pa, pa_r = p1.next()
                            for dc in range(8):
                                MM(pa[:], x3T[:, dc, :], Wgt[:, dc, csl], dc == 0, dc == 7, [x3T_r, r_Wgt], [pa_r])
                            TT("dve", gt[:, csl], pa[:], rep3[:, 2, csl], ALU.add, [pa_r, r_rep3], [gt_r])
                            ACTF(gt[:, csl], gt[:, csl], AF.Sigmoid, [gt_r], [gt_r])
                            pb2, pb2_r = p2.next()
                            for dc in range(2):
                                MM(pb2[:], pT_t[:, dc, :], Wp[:, dc, csl], dc == 0, dc == 1, [pT_r, r_Wp], [pb2_r])
                            yield
                            TT("dve", gt[:, csl], gt[:, csl], pb2[:], ALU.mult, [gt_r, pb2_r], [gt_r])
                            TT("pool", Yacc[:, it, csl], Yacc[:, it, csl], gt[:, csl], ALU.add, [H_r, gt_r], [H_r])
                            yield
                        ot, ot_r = ot_ring.next()
                        rms3(Hh, H_r, 1, ot[:], ot_r)
                        DMA("dsp", out[tsl, :], ot[:], [ot_r], [])

                    for it in range(0, NTL, 2):
                        drive_rr([genD3(it), genD3(it + 1)])

        kb.drain_all()
    return nc


def _blockdiag(w):
    o = np.zeros((128, 4, 128), np.float32)
    for n in range(8):
        cc, j = n // 2, n % 2
        o[j * 64:(j + 1) * 64, cc, j * 64:(j + 1) * 64] = w[n]
    return o


def _rel_bucket(dist):
    n = np.maximum(dist, 0)
    nf = np.maximum(n, 16).astype(np.float32)
    large = 16 + (np.log(nf / np.float32(16)) / np.float32(np.log(8.0)) * np.float32(16)).astype(np.int32)
    large = np.minimum(large, 31)
    return np.where(n < 16, n, large)


def _nsa_consts(rel_table):
    c = {}
    assert (_rel_bucket(np.arange(113, 8192)) == 31).all()
    cl = np.arange(128)[:, None, None]; mp = np.arange(5)[None, :, None]; tt = np.arange(512)[None, None, :]
    dist = 512 * mp + tt - 16 * cl - 31
    c["bc_g"] = np.ascontiguousarray(rel_table[_rel_bucket(dist)].transpose(3, 0, 1, 2))
    c["bc_m"] = np.where(dist >= 0, 0.0, NEGM).astype(np.float32)
    assert (512 * 5 - 16 * 127 - 31) >= 113
    sl = np.arange(128)[:, None]; tl = np.arange(128)[None, :]
    d0 = tl - sl; d1 = 128 + tl - sl
    g0 = rel_table[_rel_bucket(d0)]; g1 = rel_table[_rel_bucket(d1)]
    c["bd_g"] = np.ascontiguousarray(np.stack([g0, g1], 0).transpose(3, 1, 0, 2))
    m0 = np.where(d0 >= 0, 0.0, NEGM); m2 = np.where(tl < sl, 0.0, NEGM)
    c["bd_m"] = np.ascontiguousarray(np.stack([m0, np.zeros_like(m0), m2], 1)).astype(np.float32)
    c["t31"] = np.ascontiguousarray(np.broadcast_to(rel_table[31][None, :], (128, 8))).astype(np.float32)
    t = (np.arange(NT)[None, :, None] * 128 + np.arange(128)[:, None, None])
    blk = np.arange(64)[None, None, :]
    d = t // 64 - blk
    local = (d >= 0) & (d < 2)
    init = (blk == 0) & ~local
    past = (d >= 0) & ~local & ~init
    c["force_c"] = np.where(local, 2.0e4, np.where(init, 1.0e4, np.where(past, 0.0, -1.0))).astype(np.float32)
    c["keep_c"] = past.astype(np.float32)
    cs = np.arange(256)[:, None] * 16; ss = np.arange(64)[None, :] * 64
    ov = np.clip(np.minimum(cs + 32, ss + 64) - np.maximum(cs, ss), 0, None).astype(np.float32) / 32.0
    ove = np.concatenate([ov, np.ones((256, 1), np.float32)], 1)
    ove[255] = 0.0
    c["ovl_ext"] = np.ascontiguousarray(ove.reshape(2, 128, 65).transpose(1, 0, 2))
    c["erows"] = (np.arange(T)[None, :] // 64 == np.arange(64)[:, None]).astype(np.float32)
    return c


def _prep_inputs(inputs):
    x = np.ascontiguousarray(inputs["x"], dtype=np.float32)
    p = np.ascontiguousarray(inputs["p"], dtype=np.float32)
    shared = {
        "ident": np.eye(128, dtype=np.float32),
        "w_in": np.ascontiguousarray(inputs["w_in"][0]),
        "g_attn": np.ascontiguousarray(inputs["attn_norm"][0].reshape(8, 128).T),
        "lru_cw": np.ascontiguousarray(inputs["conv_w"][0][:, 0, :].reshape(4, 4, 128).transpose(2, 1, 0)),
        "lru_vec": np.ascontiguousarray(np.stack([inputs[k][0].reshape(4, 128) for k in
                                                  ("conv_b", "lru_ba", "lru_bx", "lru_lambda", "grp_norm_lru")], 0).transpose(2, 0, 1)),
        "cmp_w1": np.ascontiguousarray(np.stack([inputs[k][0].reshape(16, 128, 256).transpose(1, 0, 2) for k in ("cmp_k_w1", "cmp_v_w1")], 0)),
        "cmp_pe": np.ascontiguousarray(np.stack([inputs[k][0].reshape(16, 128).T for k in ("cmp_k_pe", "cmp_v_pe")], 0)),
        "cmp_w2": np.ascontiguousarray(np.stack([inputs[k][0].reshape(2, 128, 64).transpose(1, 0, 2) for k in ("cmp_k_w2", "cmp_v_w2")], 0)),
        "ga_rep": np.ascontiguousarray(np.broadcast_to(inputs["grp_norm_attn"][0][None, :], (128, 512))).astype(np.float32),
        "w_out": np.ascontiguousarray(inputs["w_out"][0]),
        "peer_wq": np.ascontiguousarray(inputs["peer_wq"][0]),
        "sk_T": np.ascontiguousarray(inputs["peer_subkeys"][0].transpose(0, 2, 1)),
        "peer_u": np.ascontiguousarray(inputs["peer_u"][0]),
        "peer_v": np.ascontiguousarray(inputs["peer_v"][0]),
        "ple_wgate": np.ascontiguousarray(inputs["ple_wgate"][0]),
        "ple_proj": np.ascontiguousarray(inputs["ple_proj"][0]),
        "rep4": np.ascontiguousarray(np.broadcast_to(np.stack([inputs["ffn_norm"][0], inputs["ple_norm"][0], inputs["final_norm"], inputs["ple_bgate"][0]], 0)[None], (128, 4, D))).astype(np.float32),
        "iota128": np.ascontiguousarray(np.broadcast_to(np.arange(128, dtype=np.float32)[None], (128, 128))),
        "iota16": np.ascontiguousarray(np.broadcast_to(np.arange(16, dtype=np.float32)[None], (128, 16))),
        "lru_bda": _blockdiag(inputs["lru_wa"][0]),
        "lru_bdx": _blockdiag(inputs["lru_wx"][0]),
    }
    shared.update(_nsa_consts(np.asarray(inputs["rel_table"], np.float32)))
    in_maps = []
    for c in range(8):
        b, hf = c // 2, c % 2
        m = dict(shared)
        m["xb"] = x[b]
        m["xh"] = np.ascontiguousarray(x[b, hf * TH:(hf + 1) * TH])
        m["ph"] = np.ascontiguousarray(p[0, b, hf * TH:(hf + 1) * TH])
        sel = np.zeros((128, 2), np.float32); sel[:, hf] = 1.0
        m["selc"] = sel
        in_maps.append(m)
    return in_maps


def kernel(**inputs):
    nc = build_program()
    in_maps = _prep_inputs(inputs)
    res = run_bass_kernel_spmd(nc, in_maps, core_ids=list(range(8)))
    outp = np.zeros((4, T, D), np.float32)
    for c in range(8):
        b, hf = c // 2, c % 2
        outp[b, hf * TH:(hf + 1) * TH] = res.results[c]["out"]
    return outp
```

```python
import numpy as np
from contextlib import ExitStack
import concourse.bass as bass
import concourse.mybir as mybir
from concourse.bass_utils import run_bass_kernel_spmd

F32 = mybir.dt.float32
BF16 = mybir.dt.bfloat16
U32 = mybir.dt.uint32
AF = mybir.ActivationFunctionType
ALU = mybir.AluOpType
AX = mybir.AxisListType

T = 4096
D = 1024
NT = T // 128
TH = 2048
IN_COLS = 2328
EPS = 1e-6
NEGM = -30000.0


class Res:
    __slots__ = ("name", "lw", "rd")

    def __init__(self, name=""):
        self.name = name
        self.lw = None
        self.rd = {}


class KB:
    NDMA = 4

    def __init__(self, nc, stack):
        self.nc = nc
        self.issue = {"pe": nc.tensor, "act": nc.scalar, "dve": nc.vector, "pool": nc.gpsimd,
                      "dsp": nc.sync, "dact": nc.scalar, "dpool": nc.gpsimd}
        self.stream = {"pe": "pe", "act": "act", "dve": "dve", "pool": "pool",
                       "dsp": "sp", "dact": "act", "dpool": "pool"}
        self.sems = {}
        self.cnt = {}
        for q in self.issue:
            n = self.NDMA if self.is_dma(q) else 1
            self.sems[q] = [stack.enter_context(nc.semaphore(f"s_{q}{i}")) for i in range(n)]
            self.cnt[q] = 0
        self.waited = {s: {} for s in ("pe", "act", "dve", "pool", "sp")}
        self.ninst = 0
        self._rr = 0

    @staticmethod
    def is_dma(q):
        return q in ("dsp", "dact", "dpool")

    @staticmethod
    def _need(need, dep):
        if dep is None:
            return
        q, c = dep
        if need.get(q, 0) < c:
            need[q] = c

    def _waits(self, st, eng, need, skip_q=None):
        for dq, c in need.items():
            if dq == "pe" and skip_q == "pe":
                continue
            if self.is_dma(dq):
                n = self.NDMA
                for si in range(n):
                    k = (c - 1 - si) // n + 1 if c - 1 >= si else 0
                    if k <= 0:
                        continue
                    key = (dq, si)
                    if self.waited[st].get(key, 0) >= k:
                        continue
                    eng.wait_ge(self.sems[dq][si], 16 * k)
                    self.waited[st][key] = k
            else:
                key = (dq, 0)
                if self.waited[st].get(key, 0) >= c:
                    continue
                eng.wait_ge(self.sems[dq][0], c)
                self.waited[st][key] = c

    def emit(self, q, fn, reads=(), writes=()):
        need = {}
        for r in reads:
            self._need(need, r.lw)
        for w in writes:
            self._need(need, w.lw)
            for rq, rc in w.rd.items():
                self._need(need, (rq, rc))
        st = self.stream[q]
        self._waits(st, self.issue[q], need, skip_q=q)
        inst = fn()
        self.cnt[q] += 1
        c = self.cnt[q]
        if self.is_dma(q):
            inst.then_inc(self.sems[q][(c - 1) % self.NDMA], 16)
        else:
            inst.then_inc(self.sems[q][0], 1)
        for r in reads:
            if r.rd.get(q, 0) < c:
                r.rd[q] = c
        for w in writes:
            w.lw = (q, c)
            w.rd = {}
        self.ninst += 1
        return inst

    def dmaq(self):
        self._rr ^= 1
        return "dsp" if self._rr else "dact"

    def barrier(self):
        need = {q: c for q, c in self.cnt.items() if c > 0}
        for st, eng in (("pe", self.nc.tensor), ("act", self.nc.scalar), ("dve", self.nc.vector),
                        ("pool", self.nc.gpsimd), ("sp", self.nc.sync)):
            self._waits(st, eng, dict(need))

    def drain_all(self):
        need = {q: c for q, c in self.cnt.items() if c > 0}
        self._waits("sp", self.nc.sync, need)


class Scope:
    def __init__(self, kb):
        self.kb = kb
        self.st = ExitStack()

    def __enter__(self):
        self.st.__enter__()
        return self.st

    def __exit__(self, *a):
        if a[0] is None:
            self.kb.barrier()
        return self.st.__exit__(*a)


class Ring:
    def __init__(self, tiles):
        self.tiles = tiles
        self.res = [Res() for _ in tiles]
        self.i = -1

    def next(self):
        self.i = (self.i + 1) % len(self.tiles)
        return self.tiles[self.i], self.res[self.i]


def build_program(dbg=None, phases=("A", "B", "C", "D")):
    nc = bass.Bass("TRN2", target_bir_lowering=False)

    def din(name, shape, dt=F32):
        return nc.dram_tensor(name, list(shape), dt, kind="ExternalInput").ap()

    dbg = dbg or ()

    def dscr(name, shape, dt):
        kind = "ExternalOutput" if name in dbg else "Internal"
        return nc.dram_tensor(name, list(shape), dt, kind=kind).ap()

    xb = din("xb", [T, D])
    xh = din("xh", [TH, D])
    ph = din("ph", [TH, 256])
    selc = din("selc", [128, 2])
    ident = din("ident", [128, 128])
    w_in = din("w_in", [D, IN_COLS])
    g_attn = din("g_attn", [128, 8])
    out = nc.dram_tensor("out", [TH, D], F32, kind="ExternalOutput").ap()
    lru_cw = din("lru_cw", [128, 4, 4])
    lru_vec = din("lru_vec", [128, 5, 4])
    lru_bda = din("lru_bda", [128, 4, 128])
    lru_bdx = din("lru_bdx", [128, 4, 128])

    qT_s = dscr("qT_s", [512, T], BF16)
    kcT_s = dscr("kcT_s", [128, T], BF16)
    vcT_s = dscr("vcT_s", [128, T], BF16)
    ksT_s = dscr("ksT_s", [128, T], BF16)
    kwT_s = dscr("kwT_s", [128, T], BF16)
    vs_s = dscr("vs_s", [T, 128], BF16)
    vw_s = dscr("vw_s", [T, 128], BF16)
    gates_s = dscr("gates_s", [T, 24], F32)
    xrT_s = dscr("xrT_s", [512, T], F32)
    xgT_s = dscr("xgT_s", [512, T], F32)

    cmp_w1 = din("cmp_w1", [2, 128, 16, 256])
    cmp_pe = din("cmp_pe", [2, 128, 16])
    cmp_w2 = din("cmp_w2", [2, 128, 2, 64])
    ovl_ext = din("ovl_ext", [128, 2, 65])
    bc_g = din("bc_g", [8, 128, 5, 512])
    bc_m = din("bc_m", [128, 5, 512])
    bd_g = din("bd_g", [8, 128, 2, 128])
    bd_m = din("bd_m", [128, 3, 128])
    t31_in = din("t31", [128, 8])
    force_in = din("force_c", [128, 32, 64])
    keep_in = din("keep_c", [128, 32, 64])
    erows = din("erows", [64, T])
    ga_in = din("ga_rep", [128, 512])
    w_out_in = din("w_out", [D, D])
    peer_wq = din("peer_wq", [D, 2048])
    sk_T = din("sk_T", [2, 128, 128])
    peer_u = din("peer_u", [16384, D])
    peer_v = din("peer_v", [16384, D])
    ple_wg = din("ple_wgate", [D, D])
    ple_pj = din("ple_proj", [256, D])
    rep4 = din("rep4", [128, 4, D])
    iota16 = din("iota16", [128, 16])
    iota128 = din("iota128", [128, 128])
    H1_s = dscr("H1_s", [TH, D], F32)
    xnT2_s = dscr("xnT2_s", [128, 8, TH], BF16)
    Wt_s = dscr("Wt_s", [TH // 128, 128, 128, 128], BF16)
    mixT_s = dscr("mixT_s", [1024, T], BF16)
    R = {n: Res(n) for n in ("H1_s", "xnT2_s", "Wt_s", "mixT_s", "qT_s", "kcT_s", "vcT_s", "ksT_s", "kwT_s", "vs_s", "vw_s", "gates_s", "xrT_s", "xgT_s")}

    with ExitStack() as top:
        kb = KB(nc, top)
        E = kb.emit

        uniq = [0]

        def sb(st, name, shape, dt):
            uniq[0] += 1
            return st.enter_context(nc.sbuf_tensor(f"sb{uniq[0]}_{name}", list(shape), dt))

        def ps(st, name, shape, dt):
            uniq[0] += 1
            return st.enter_context(nc.psum_tensor(f"ps{uniq[0]}_{name}", list(shape), dt))


        def MM(out_, lhsT, rhs, start, stop, reads, writes):
            return E("pe", lambda: nc.tensor.matmul(out_, lhsT=lhsT, rhs=rhs, start=start, stop=stop), reads, writes)

        def TR(out_, in_, idt, reads, writes):
            return E("pe", lambda: nc.tensor.transpose(out=out_, in_=in_, identity=idt), reads, writes)

        def ACTF(out_, in_, func, reads, writes, **kw):
            return E("act", lambda: nc.scalar.activation(out=out_, in_=in_, func=func, **kw), reads, writes)

        def veng(q):
            return nc.vector if q == "dve" else nc.gpsimd

        def TS(q, out_, in0, s1, s2, op0, op1, reads, writes):
            if op1 is None:
                return E(q, lambda: veng(q).tensor_scalar(out=out_, in0=in0, scalar1=s1, scalar2=None, op0=op0), reads, writes)
            return E(q, lambda: veng(q).tensor_scalar(out=out_, in0=in0, scalar1=s1, scalar2=s2, op0=op0, op1=op1), reads, writes)

        def TT(q, out_, in0, in1, op, reads, writes):
            return E(q, lambda: veng(q).tensor_tensor(out=out_, in0=in0, in1=in1, op=op), reads, writes)

        def STT(out_, in0, scalar, in1, op0, op1, reads, writes, **kw):
            return E("dve", lambda: nc.vector.scalar_tensor_tensor(out=out_, in0=in0, scalar=scalar, in1=in1, op0=op0, op1=op1, **kw), reads, writes)

        def CP(q, out_, in_, reads, writes):
            if q == "act":
                return E("act", lambda: nc.scalar.copy(out=out_, in_=in_), reads, writes)
            return E(q, lambda: veng(q).tensor_copy(out=out_, in_=in_), reads, writes)

        def MSET(q, out_, val, writes):
            return E(q, lambda: veng(q).memset(out_, val), (), writes)

        def DMA(q, out_, in_, reads, writes):
            eng = {"dsp": nc.sync, "dact": nc.scalar, "dpool": nc.gpsimd}[q]
            return E(q, lambda: eng.dma_start(out=out_, in_=in_), reads, writes)

        def drive_rr(gens, weights=None):
            res = [None] * len(gens)
            live = list(range(len(gens)))
            weights = weights or [1] * len(gens)
            while live:
                for gi in list(live):
                    for _ in range(weights[gi]):
                        try:
                            next(gens[gi])
                        except StopIteration as e:
                            res[gi] = e.value
                            live.remove(gi)
                            break
            return res

        def dump(name, ap, shape, dt, res):
            if name not in dbg:
                return
            d = nc.dram_tensor(name, list(shape), dt, kind="ExternalOutput").ap()
            DMA("dsp", d, ap, [res] if not isinstance(res, list) else res, [])

        ident_f = sb(top, "ident_f", [128, 128], F32); r_identf = Res()
        ident_b = sb(top, "ident_b", [128, 128], BF16); r_identb = Res()
        E("dsp", lambda: nc.sync.dma_start(out=ident_f[:], in_=ident), writes=[r_identf])
        E("dve", lambda: nc.vector.tensor_copy(out=ident_b[:], in_=ident_f[:]), reads=[r_identf], writes=[r_identb])

        if "A" in phases:
            with Scope(kb) as st:
                Wg = sb(st, "Wg", [128, 8, IN_COLS], BF16); r_Wg = Res()
                gcol = sb(st, "gcol", [128, 8], F32); r_gcol = Res()
                wst = Ring([sb(st, f"wst{i}", [128, IN_COLS], F32) for i in range(2)])
                E("dsp", lambda: nc.sync.dma_start(out=gcol[:], in_=g_attn), writes=[r_gcol])
                for dc in range(8):
                    w_t, w_r = wst.next()
                    E("dsp" if dc % 2 == 0 else "dact",
                      (lambda w_t=w_t, dc=dc: nc.sync.dma_start(out=w_t[:], in_=w_in[dc * 128:(dc + 1) * 128, :])) if dc % 2 == 0 else
                      (lambda w_t=w_t, dc=dc: nc.scalar.dma_start(out=w_t[:], in_=w_in[dc * 128:(dc + 1) * 128, :])),
                      writes=[w_r])
                    eng = "dve" if dc % 2 == 0 else "pool"
                    ve = nc.vector if dc % 2 == 0 else nc.gpsimd
                    E(eng, lambda ve=ve, w_t=w_t, dc=dc: ve.tensor_scalar(out=Wg[:, dc, :], in0=w_t[:], scalar1=gcol[:, dc:dc + 1], scalar2=None, op0=ALU.mult),
                      reads=[w_r, r_gcol], writes=[r_Wg])

                xt_ring = Ring([sb(st, f"xt{i}", [128, 4, D], F32) for i in range(2)])
                xnb_ring = Ring([sb(st, f"xnb{i}", [128, 4, D], BF16) for i in range(2)])
                xnT_ring = Ring([sb(st, f"xnT{i}", [128, 8, 512], BF16) for i in range(2)])
                junk = sb(st, "junkA", [128, D], BF16); r_junk = Res()
                ss_ring = Ring([sb(st, f"ss{i}", [128, 8], F32) for i in range(2)])
                pT_ring = Ring([ps(st, f"pT{i}", [128, 512], BF16) for i in range(2)])
                pacc = Ring([ps(st, f"pacc{i}", [128, 512], F32) for i in range(4)])
                ostf = Ring([sb(st, f"ostf{i}", [128, 512], F32) for i in range(3)])
                ostb = Ring([sb(st, f"ostb{i}", [128, 512], BF16) for i in range(3)])
                osv = Ring([sb(st, f"osv{i}", [128, 256], BF16) for i in range(2)])
                osg = Ring([sb(st, f"osg{i}", [128, 24], F32) for i in range(2)])
                xb_v = xb.rearrange("(n p) d -> p n d", p=128)
                fm = []
                for cc in range(4):
                    fm.append((cc * 128, qT_s[cc * 128:(cc + 1) * 128, :], 0.125, True, R["qT_s"]))
                fm.append((512, kcT_s, 1.0, True, R["kcT_s"]))
                fm.append((640, vcT_s, 1.0, True, R["vcT_s"]))
                fm.append((768, ksT_s, 1.0, True, R["ksT_s"]))
                fm.append((1024, kwT_s, 1.0, True, R["kwT_s"]))
                for cc in range(4):
                    fm.append((1304 + cc * 128, xrT_s[cc * 128:(cc + 1) * 128, :], 1.0, False, R["xrT_s"]))
                for cc in range(4):
                    fm.append((1816 + cc * 128, xgT_s[cc * 128:(cc + 1) * 128, :], 1.0, False, R["xgT_s"]))
                evc = [0]

                def stageP(tcn):
                    xt, xt_r = xt_ring.next()
                    DMA("dsp", xt[:], xb_v[:, tcn * 4:(tcn + 1) * 4, :], [], [xt_r])
                    ss, ss_r = ss_ring.next()
                    for n in range(4):
                        ACTF(junk[:], xt[:, n, :], AF.Square, [xt_r], [r_junk, ss_r], accum_out=ss[:, n:n + 1])
                    yield
                    TS("dve", ss[:, 4:8], ss[:, 0:4], 1.0 / D, EPS, ALU.mult, ALU.add, [ss_r], [ss_r])
                    ACTF(ss[:, 4:8], ss[:, 4:8], AF.Sqrt, [ss_r], [ss_r])
                    yield
                    E("dve", lambda: nc.vector.reciprocal(out=ss[:, 4:8], in_=ss[:, 4:8]), [ss_r], [ss_r])
                    xnb, xnb_r = xnb_ring.next()
                    for n in range(4):
                        if n % 2 == 0:
                            TS("dve", xnb[:, n, :], xt[:, n, :], ss[:, 4 + n:5 + n], None, ALU.mult, None, [xt_r, ss_r], [xnb_r])
                        else:
                            ACTF(xnb[:, n, :], xt[:, n, :], AF.Copy, [xt_r, ss_r], [xnb_r], scale=ss[:, 4 + n:5 + n])
                    yield
                    xnT, xnT_r = xnT_ring.next()
                    for dc in range(8):
                        pT, pT_r = pT_ring.next()
                        for n in range(4):
                            TR(pT[:, n * 128:(n + 1) * 128], xnb[:, n, dc * 128:(dc + 1) * 128], ident_b[:], [xnb_r, r_identb], [pT_r])
                        CP("act" if dc % 2 == 0 else "dve", xnT[:, dc, :], pT[:], [pT_r], [xnT_r])
                        if dc % 2 == 1:
                            yield
                    return (xnT, xnT_r)

                def stageM(tcn, st):
                    xnT, xnT_r = st
                    for (c0, dst, scale, isb, dres) in fm:
                        pa, pa_r = pacc.next()
                        for dc in range(8):
                            MM(pa[:], Wg[:, dc, c0:c0 + 128], xnT[:, dc, :], dc == 0, dc == 7, [r_Wg, xnT_r], [pa_r])
                        o_t, o_r = (ostb if isb else ostf).next()
                        evc[0] += 1
                        if evc[0] % 2 == 0:
                            ACTF(o_t[:], pa[:], AF.Copy, [pa_r], [o_r], scale=scale)
                        else:
                            TS("dve", o_t[:], pa[:], scale, None, ALU.mult, None, [pa_r], [o_r])
                        DMA("dsp" if evc[0] % 2 == 0 else "dpool", dst[:, tcn * 512:(tcn + 1) * 512], o_t[:], [o_r], [dres])
                        yield
                    for n in range(4):
                        t0 = tcn * 512 + n * 128
                        pa, pa_r = pacc.next()
                        for dc in range(8):
                            MM(pa[:, 0:128], xnT[:, dc, n * 128:(n + 1) * 128], Wg[:, dc, 896:1024], dc == 0, dc == 7, [r_Wg, xnT_r], [pa_r])
                        pb, pb_r = pacc.next()
                        for dc in range(8):
                            MM(pb[:, 0:152], xnT[:, dc, n * 128:(n + 1) * 128], Wg[:, dc, 1152:1304], dc == 0, dc == 7, [r_Wg, xnT_r], [pb_r])
                        ov, ov_r = osv.next()
                        og, og_r = osg.next()
                        CP("act", ov[:, 0:128], pa[:, 0:128], [pa_r], [ov_r])
                        CP("dve", ov[:, 128:256], pb[:, 0:128], [pb_r], [ov_r])
                        CP("dve", og[:], pb[:, 128:152], [pb_r], [og_r])
                        DMA("dsp", vs_s[t0:t0 + 128, :], ov[:, 0:128], [ov_r], [R["vs_s"]])
                        DMA("dpool", vw_s[t0:t0 + 128, :], ov[:, 128:256], [ov_r], [R["vw_s"]])
                        DMA("dsp", gates_s[t0:t0 + 128, :], og[:], [og_r], [R["gates_s"]])
                        yield

                stP = drive_rr([stageP(0)])[0]
                for tcn in range(8):
                    gens = [stageM(tcn, stP)]
                    if tcn + 1 < 8:
                        gens.append(stageP(tcn + 1))
                    rr = drive_rr(gens)
                    if tcn + 1 < 8:
                        stP = rr[1]

        if "B" in phases:
            with Scope(kb) as st:
                cw = sb(st, "cw", [128, 4, 4], F32); r_cw = Res()
                lv = sb(st, "lv", [128, 5, 4], F32); r_lv = Res()
                clc = sb(st, "clc", [128, 3, 4], F32); r_clc = Res()
                bdf = sb(st, "bdf", [128, 2, 4, 128], F32); r_bdf = Res()
                bdb = sb(st, "bdb", [128, 2, 4, 128], BF16); r_bdb = Res()
                ones_b = sb(st, "ones_b", [128, 128], BF16); r_ones = Res()
                E("dsp", lambda: nc.sync.dma_start(out=cw[:], in_=lru_cw), writes=[r_cw])
                E("dact", lambda: nc.scalar.dma_start(out=lv[:], in_=lru_vec), writes=[r_lv])
                E("dsp", lambda: nc.sync.dma_start(out=bdf[:, 0], in_=lru_bda), writes=[r_bdf])
                E("dact", lambda: nc.scalar.dma_start(out=bdf[:, 1], in_=lru_bdx), writes=[r_bdf])
                E("dve", lambda: nc.vector.tensor_copy(out=bdb[:], in_=bdf[:]), reads=[r_bdf], writes=[r_bdb])
                E("dve", lambda: nc.vector.memset(ones_b[:], 1.0), writes=[r_ones])
                E("act", lambda: nc.scalar.activation(out=clc[:, 0, :], in_=lv[:, 3, :], func=AF.Exp, scale=-1.0), reads=[r_lv], writes=[r_clc])
                E("act", lambda: nc.scalar.activation(out=clc[:, 0, :], in_=clc[:, 0, :], func=AF.Ln, bias=1.0), reads=[r_clc], writes=[r_clc])
                E("dve", lambda: nc.vector.tensor_scalar(out=clc[:, 1, :], in0=clc[:, 0, :], scalar1=-8.0, scalar2=None, op0=ALU.mult), reads=[r_clc], writes=[r_clc])
                E("dve", lambda: nc.vector.tensor_scalar(out=clc[:, 2, :], in0=clc[:, 0, :], scalar1=-16.0, scalar2=None, op0=ALU.mult), reads=[r_clc], writes=[r_clc])
                L = sb(st, "Lall", [128, 4, T], F32); r_L = Res()
                X = [sb(st, f"lruX{i}", [128, T], F32) for i in range(5)]
                rX = [Res() for _ in range(5)]
                xcb = sb(st, "xcb", [128, T], BF16); r_xcb = Res()
                pg = Ring([ps(st, f"pg{i}", [128, 512], F32) for i in range(4)])
                for cc in range(4):
                    X1, X2, X3, X4, X5 = X
                    r1, r2, r3, r4, r5 = rX
                    for hh in range(2):
                        E("dsp", lambda cc=cc, hh=hh: nc.sync.dma_start(out=X1[:, hh * 2048:(hh + 1) * 2048], in_=xrT_s[cc * 128:(cc + 1) * 128, hh * 2048:(hh + 1) * 2048]), reads=[R["xrT_s"]], writes=[r1])
                        E("dact", lambda cc=cc, hh=hh: nc.scalar.dma_start(out=X3[:, hh * 2048:(hh + 1) * 2048], in_=xgT_s[cc * 128:(cc + 1) * 128, hh * 2048:(hh + 1) * 2048]), reads=[R["xgT_s"]], writes=[r3])
                    E("dve", lambda cc=cc: nc.vector.tensor_scalar(out=X2[:], in0=X1[:], scalar1=cw[:, cc, 3:4], scalar2=lv[:, 0, cc:cc + 1], op0=ALU.mult, op1=ALU.add), reads=[r1, r_cw, r_lv], writes=[r2])
                    for sh in (1, 2, 3):
                        E("dve", lambda cc=cc, sh=sh: nc.vector.scalar_tensor_tensor(out=X2[:, sh:T], in0=X1[:, 0:T - sh], scalar=cw[:, cc, 3 - sh:4 - sh], in1=X2[:, sh:T], op0=ALU.mult, op1=ALU.add), reads=[r1, r2, r_cw], writes=[r2])
                    E("pool", lambda: nc.gpsimd.tensor_copy(out=xcb[:], in_=X2[:]), reads=[r2], writes=[r_xcb])
                    for gi, (Xo, ro, bi) in enumerate(((X4, r4, 1), (X5, r5, 2))):
                        for tcn in range(8):
                            pgt, pg_r = pg.next()
                            E("pe", lambda pgt=pgt, gi=gi, cc=cc, tcn=tcn: nc.tensor.matmul(pgt[:], lhsT=bdb[:, gi, cc, :], rhs=xcb[:, tcn * 512:(tcn + 1) * 512], start=True, stop=True), reads=[r_bdb, r_xcb], writes=[pg_r])
                            E("act", lambda pgt=pgt, Xo=Xo, bi=bi, cc=cc, tcn=tcn: nc.scalar.activation(out=Xo[:, tcn * 512:(tcn + 1) * 512], in_=pgt[:], func=AF.Sigmoid, bias=lv[:, bi, cc:cc + 1]), reads=[pg_r, r_lv], writes=[ro])
                    E("act", lambda cc=cc: nc.scalar.activation(out=X1[:], in_=X4[:], func=AF.Exp, scale=clc[:, 1, cc:cc + 1]), reads=[r4, r_clc], writes=[r1])
                    E("act", lambda cc=cc: nc.scalar.activation(out=X4[:], in_=X4[:], func=AF.Exp, scale=clc[:, 2, cc:cc + 1]), reads=[r4, r_clc], writes=[r4])
                    E("act", lambda: nc.scalar.activation(out=X4[:], in_=X4[:], func=AF.Sqrt, scale=-1.0, bias=1.0), reads=[r4], writes=[r4])
                    E("pool", lambda: nc.gpsimd.tensor_tensor(out=X5[:], in0=X5[:], in1=X2[:], op=ALU.mult), reads=[r5, r2], writes=[r5])
                    E("dve", lambda: nc.vector.tensor_tensor(out=X4[:], in0=X4[:], in1=X5[:], op=ALU.mult), reads=[r4, r5], writes=[r4])
                    E("dve", lambda: nc.vector.tensor_tensor_scan(out=X2[:], data0=X1[:], data1=X4[:], initial=0.0, op0=ALU.mult, op1=ALU.add), reads=[r1, r4], writes=[r2])
                    E("act", lambda: nc.scalar.activation(out=X3[:], in_=X3[:], func=AF.Gelu_apprx_tanh), reads=[r3], writes=[r3])
                    E("pool", lambda cc=cc: nc.gpsimd.tensor_tensor(out=L[:, cc, :], in0=X2[:], in1=X3[:], op=ALU.mult), reads=[r2, r3], writes=[r_L])
                sq = Ring([sb(st, f"lsq{i}", [128, 512], BF16) for i in range(2)])
                rs_ring = Ring([sb(st, f"lrs{i}", [128, 512], F32) for i in range(2)])
                lo = Ring([sb(st, f"lo{i}", [128, 512], BF16) for i in range(3)])
                for tcn in range(8):
                    pgt, pg_r = pg.next()
                    for cc in range(4):
                        sq_t, sq_r = sq.next()
                        E("act", lambda sq_t=sq_t, cc=cc, tcn=tcn: nc.scalar.activation(out=sq_t[:], in_=L[:, cc, tcn * 512:(tcn + 1) * 512], func=AF.Square), reads=[r_L], writes=[sq_r])
                        E("pe", lambda pgt=pgt, sq_t=sq_t, cc=cc: nc.tensor.matmul(pgt[:], lhsT=ones_b[:], rhs=sq_t[:], start=(cc == 0), stop=(cc == 3)), reads=[r_ones, sq_r], writes=[pg_r])
                    rs_t, rs_r = rs_ring.next()
                    E("dve", lambda rs_t=rs_t, pgt=pgt: nc.vector.tensor_scalar(out=rs_t[:], in0=pgt[:], scalar1=1.0 / 512, scalar2=EPS, op0=ALU.mult, op1=ALU.add), reads=[pg_r], writes=[rs_r])
                    E("act", lambda rs_t=rs_t: nc.scalar.activation(out=rs_t[:], in_=rs_t[:], func=AF.Sqrt), reads=[rs_r], writes=[rs_r])
                    E("dve", lambda rs_t=rs_t: nc.vector.reciprocal(out=rs_t[:], in_=rs_t[:]), reads=[rs_r], writes=[rs_r])
                    for cc in range(4):
                        lo_t, lo_r = lo.next()
                        E("dve", lambda lo_t=lo_t, rs_t=rs_t, cc=cc, tcn=tcn: nc.vector.scalar_tensor_tensor(out=lo_t[:], in0=L[:, cc, tcn * 512:(tcn + 1) * 512], scalar=lv[:, 4, cc:cc + 1], in1=rs_t[:], op0=ALU.mult, op1=ALU.mult), reads=[r_L, rs_r, r_lv], writes=[lo_r])
                        E("dsp", lambda lo_t=lo_t, cc=cc, tcn=tcn: nc.sync.dma_start(out=mixT_s[512 + cc * 128:512 + (cc + 1) * 128, tcn * 512:(tcn + 1) * 512], in_=lo_t[:]), reads=[lo_r], writes=[R["mixT_s"]])

        if "C" in phases:
            with Scope(kb) as st:
                Aout = sb(st, "Aout", [128, NT, 512], BF16)
                rA = [Res() for _ in range(NT)]
                sig = sb(st, "sig", [128, NT, 24], F32); r_sig = Res()
                force_t = sb(st, "force_t", [128, NT, 64], F32); r_force = Res()
                keep_t = sb(st, "keep_t", [128, NT, 64], F32); r_keep = Res()
                t31 = sb(st, "t31", [128, 8], F32); r_t31 = Res()
                BD = sb(st, "BD", [128, 8, 3, 128], BF16); r_BD = Res()
                ovl_t = sb(st, "ovl_t", [128, 2, 65], F32); r_ovl = Res()
                ga_t = sb(st, "ga_t", [128, 512], F32); r_ga = Res()
                bcm = sb(st, "bcm", [128, 5, 512], F32); r_bcm = Res()
                DMA("dsp", sig[:], gates_s.rearrange("(n p) c -> p n c", p=128), [R["gates_s"]], [r_sig])
                ACTF(sig[:], sig[:], AF.Sigmoid, [r_sig], [r_sig])
                DMA("dact", force_t[:], force_in, [], [r_force])
                DMA("dsp", keep_t[:], keep_in, [], [r_keep])
                DMA("dact", t31[:], t31_in, [], [r_t31])
                DMA("dsp", ovl_t[:], ovl_ext, [], [r_ovl])
                DMA("dact", ga_t[:], ga_in, [], [r_ga])
                DMA("dsp", bcm[:], bc_m, [], [r_bcm])
                psb = [ps(st, f"pC{i}", [128, 512], F32) for i in range(8)]
                pS = Ring(psb[0:3])
                pO = psb[3:7]; r_pO = [Res() for _ in range(4)]
                pX = Ring(psb[7:8])
                with Scope(kb) as st2:
                    bdg = sb(st2, "bdg", [128, 8, 2, 128], F32); r_bdg = Res()
                    bdm = sb(st2, "bdm", [128, 3, 128], F32); r_bdm = Res()
                    DMA("dsp", bdg[:], bd_g.rearrange("h p j t -> p h j t"), [], [r_bdg])
                    DMA("dact", bdm[:], bd_m, [], [r_bdm])
                    for hg in range(8):
                        for j in range(2):
                            STT(BD[:, hg, j, :], bdg[:, hg, j, :], t31[:, hg:hg + 1], bdm[:, j, :], ALU.subtract, ALU.add, [r_bdg, r_bdm, r_t31], [r_BD])
                        CP("dve", BD[:, hg, 2, :], bdm[:, 2, :], [r_bdm], [r_BD])
                P_ring = Ring([sb(st, f"Pt{i}", [128, 512], BF16) for i in range(5)])
                sm = Ring([sb(st, f"smC{i}", [128, 8], F32) for i in range(8)])
                osb = Ring([sb(st, f"osb{i}", [128, 132], F32) for i in range(8)])

                def finish_tiles(items, ncol, hg, br, first, imp_first=None):
                    sts = [sm.next() for _ in items]
                    for (po, po_r, i, _, _), (s_t, s_r) in zip(items, sts):
                        TS("dve", s_t[:, 0:1], po[:, ncol:ncol + 1], 1e-30, None, ALU.max, None, [po_r], [s_r])
                    for (po, po_r, i, _, _), (s_t, s_r) in zip(items, sts):
                        E("dve", lambda: nc.vector.reciprocal(out=s_t[:, 1:2], in_=s_t[:, 0:1]), [s_r], [s_r])
                    for (po, po_r, i, _, _), (s_t, s_r) in zip(items, sts):
                        TT("dve", s_t[:, 2:3], s_t[:, 1:2], sig[:, i, hg * 3 + br:hg * 3 + br + 1], ALU.mult, [s_r, r_sig], [s_r])
                    for (po, po_r, i, _, _), (s_t, s_r) in zip(items, sts):
                        dst = Aout[:, i, hg * 64:(hg + 1) * 64]
                        if first:
                            TS("dve", dst, po[:, 0:64], s_t[:, 2:3], None, ALU.mult, None, [po_r, s_r], [rA[i]])
                        else:
                            STT(dst, po[:, 0:64], s_t[:, 2:3], dst, ALU.mult, ALU.add, [po_r, s_r, rA[i]], [rA[i]])
                    if imp_first is not None:
                        for (po, po_r, i, imp_t, imp_r), (s_t, s_r) in zip(items, sts):
                            if imp_first:
                                TS("dve", imp_t, po[:, 64:128], s_t[:, 1:2], None, ALU.mult, None, [po_r, s_r], [imp_r])
                            else:
                                STT(imp_t, po[:, 64:128], s_t[:, 1:2], imp_t, ALU.mult, ALU.add, [po_r, s_r, imp_r], [imp_r])

                w1b = sb(st, "w1b", [128, 2, 16, 256], BF16); r_w1b = Res()
                peb = sb(st, "peb", [128, 2, 16], BF16); r_peb = Res()
                w2b = sb(st, "w2b", [128, 2, 2, 64], BF16); r_w2b = Res()
                with Scope(kb) as stw1:
                    w1s = Ring([sb(stw1, f"w1s{i}", [128, 8, 256], F32) for i in range(2)])
                    pes = sb(stw1, "pes", [128, 2, 16], F32); r_pes = Res()
                    w2s = sb(stw1, "w2s", [128, 2, 2, 64], F32); r_w2s = Res()
                    for kv in range(2):
                        for hh in range(2):
                            w_t, w_r = w1s.next()
                            DMA("dsp" if hh == 0 else "dact", w_t[:], cmp_w1[kv, :, hh * 8:(hh + 1) * 8, :], [], [w_r])
                            CP("pool" if hh == 0 else "dve", w1b[:, kv, hh * 8:(hh + 1) * 8, :], w_t[:], [w_r], [r_w1b])
                        DMA("dsp", pes[:, kv, :], cmp_pe[kv], [], [r_pes])
                        DMA("dact", w2s[:, kv], cmp_w2[kv], [], [r_w2s])
                    CP("dve", peb[:], pes[:], [r_pes], [r_peb])
                    CP("dve", w2b[:], w2s[:], [r_w2s], [r_w2b])
                for k in range(2):
                    with Scope(kb) as stg:
                        KcmpT = sb(stg, "KcmpT", [64, 256], BF16); r_Kc = Res()
                        Vco = sb(stg, "Vco", [128, 2, 129], BF16); r_Vco = Res()
                        with Scope(kb) as stc:
                            stk = sb(stc, "stk", [128, 2, T], BF16); r_stk = Res()
                            hb = sb(stc, "hb", [128, 4], F32); r_hb = Res()
                            gh = sb(stc, "gh", [128, 2, 2, 256], BF16); r_gh = Res()
                            for kv in range(2):
                                src = kcT_s if kv == 0 else vcT_s
                                sres = R["kcT_s"] if kv == 0 else R["vcT_s"]
                                DMA("dsp", stk[0:64, kv, :], src[k * 64:(k + 1) * 64, :], [sres], [r_stk])
                                MSET("pool", stk[64:128, kv, T - 1:T], 0.0, [r_stk])
                                DMA("dact", stk[64:128, kv, 0:T - 1], src[k * 64:(k + 1) * 64, 1:T], [sres], [r_stk])
                            MSET("pool", gh[:], 0.0, [r_gh])
                            for kv in range(2):
                                for hh in range(2):
                                    px, px_r = pX.next()
                                    for m in range(16):
                                        MM(px[:, 0:1], w1b[:, kv, m, hh * 128:(hh + 1) * 128], peb[:, kv, m:m + 1], m == 0, m == 15, [r_w1b, r_peb], [px_r])
                                    CP("dve", hb[:, kv * 2 + hh:kv * 2 + hh + 1], px[:, 0:1], [px_r], [r_hb])
                                    p_s, p_r = pS.next()
                                    for m in range(16):
                                        MM(p_s[:, 0:255], w1b[:, kv, m, hh * 128:(hh + 1) * 128], stk[:, kv, 2 * m:2 * m + 16 * 254 + 1:16], m == 0, m == 15, [r_w1b, r_stk], [p_r])
                                    ACTF(gh[:, kv, hh, 0:255], p_s[:, 0:255], AF.Gelu_apprx_tanh, [p_r, r_hb], [r_gh], bias=hb[:, kv * 2 + hh:kv * 2 + hh + 1])
                            px, px_r = pX.next()
                            for hh in range(2):
                                MM(px[0:64, 0:256], w2b[:, 0, hh, :], gh[:, 0, hh, :], hh == 0, hh == 1, [r_w2b, r_gh], [px_r])
                            CP("dve", KcmpT[:], px[0:64, 0:256], [px_r], [r_Kc])
                            for ct in range(2):
                                px, px_r = pX.next()
                                for hh in range(2):
                                    MM(px[:, 0:64], gh[:, 1, hh, ct * 128:(ct + 1) * 128], w2b[:, 1, hh, :], hh == 0, hh == 1, [r_gh, r_w2b], [px_r])
                                CP("dve", Vco[:, ct, 0:64], px[:, 0:64], [px_r], [r_Vco])
                            CP("pool", Vco[:, :, 64:129], ovl_t[:], [r_ovl], [r_Vco])
                            if k == 0:
                                dump("d_kcmp", KcmpT[:], [64, 256], BF16, r_Kc)
                                dump("d_vco", Vco[:], [128, 2, 129], BF16, r_Vco)
                                dump("d_hb", hb[:], [128, 4], F32, r_hb)
                                dump("d_gh", gh[:], [128, 2, 2, 256], BF16, r_gh)

                        QT = sb(stg, "QT", [128, 4, T], BF16)
                        r_QT = [Res() for _ in range(4)]
                        r_QM = [[Res() for _ in range(NT)] for _ in range(4)]
                        KsT = sb(stg, "KsT", [128, T], BF16); r_KsT = Res()
                        KwT = sb(stg, "KwT", [64, T], BF16); r_KwT = Res()
                        Vs = sb(stg, "Vs", [128, NT, 65], BF16); r_Vs = Res()
                        Vw = sb(stg, "Vw", [128, NT, 65], BF16); r_Vw = Res()
                        imp_acc = sb(stg, "imp_acc", [128, NT, 64], F32)
                        r_imp = [Res() for _ in range(NT)]
                        for g in range(4):
                            hg = 4 * k + g
                            DMA("dsp" if g % 2 == 0 else "dact", QT[0:64, g, :], qT_s[hg * 64:(hg + 1) * 64, :], [R["qT_s"]], [r_QT[g]])
                        DMA("dsp", KsT[0:64, :], ksT_s[k * 64:(k + 1) * 64, :], [R["ksT_s"]], [r_KsT])
                        with Scope(kb) as ste:
                            ers = sb(ste, "ers", [128, T], F32); r_ers = Res()
                            DMA("dact", ers[64:128, :], erows, [], [r_ers])
                            CP("pool", KsT[64:128, :], ers[64:128, :], [r_ers], [r_KsT])
                        DMA("dact", KwT[:], kwT_s[k * 64:(k + 1) * 64, :], [R["kwT_s"]], [r_KwT])
                        DMA("dsp", Vs[:, :, 0:64], vs_s.rearrange("(n p) c -> p n c", p=128)[:, :, k * 64:(k + 1) * 64], [R["vs_s"]], [r_Vs])
                        DMA("dact", Vw[:, :, 0:64], vw_s.rearrange("(n p) c -> p n c", p=128)[:, :, k * 64:(k + 1) * 64], [R["vw_s"]], [r_Vw])
                        MSET("pool", Vs[:, :, 64:65], 1.0, [r_Vs])
                        MSET("pool", Vw[:, :, 64:65], 1.0, [r_Vw])

                        bcs = Ring([sb(stg, f"bcs{i}", [128, 5, 512], F32) for i in range(1)])
                        BC = Ring([sb(stg, f"BCb{i}", [128, 5, 512], BF16) for i in range(2)])
                        bc_cur = {}

                        def cmp_stage1(it):
                            g, tcn, ct, last = it
                            hg = 4 * k + g
                            if tcn == 0 and ct == 0:
                                bs_t, bs_r = bcs.next()
                                DMA("dsp", bs_t[:, 0:3], bc_g[hg, :, 0:3], [], [bs_r])
                                DMA("dact", bs_t[:, 3:5], bc_g[hg, :, 3:5], [], [bs_r])
                                bc_t, bc_r = BC.next()
                                for m in range(5):
                                    STT(bc_t[:, m, :], bs_t[:, m, :], t31[:, hg:hg + 1], bcm[:, m, :], ALU.subtract, ALU.add, [bs_r, r_bcm, r_t31], [bc_r])
                                bc_cur[g] = (bc_t, bc_r)
                            bc_t, bc_r = bc_cur[g]
                            mp = tcn - 4 * ct
                            p_s, p_r = pS.next()
                            MM(p_s[:], KcmpT[:, ct * 128:(ct + 1) * 128], QT[0:64, g, tcn * 512:(tcn + 1) * 512], True, mp >= 5, [r_Kc, r_QT[g]], [p_r])
                            if mp < 5:
                                MM(p_s[:], ident_b[:], bc_t[:, mp, :], False, True, [r_identb, bc_r], [p_r])
                            P_t, P_r = P_ring.next()
                            ACTF(P_t[:], p_s[:], AF.Exp, [p_r, r_t31], [P_r], bias=t31[:, hg:hg + 1])
                            return (P_t, P_r)

                        def cmp_stage2(it, st1):
                            g, tcn, ct, last = it
                            hg = 4 * k + g
                            P_t, P_r = st1
                            for q in range(4):
                                MM(pO[q][:, 0:129], P_t[:, q * 128:(q + 1) * 128], Vco[:, ct, :], ct == 0, last, [P_r, r_Vco], [r_pO[q]])
                            if last:
                                items = []
                                for q in range(4):
                                    i = 4 * tcn + q
                                    o_t, o_r = osb.next()
                                    CP("dve", o_t[:, 0:129], pO[q][:, 0:129], [r_pO[q]], [o_r])
                                    items.append((o_t, o_r, i, imp_acc[:, i, :], r_imp[i]))
                                finish_tiles(items, 128, hg, 0, True, imp_first=(g == 0))

                        its = []
                        for g in range(4):
                            for tcn in range(8):
                                cts = [0] if tcn < 4 else [0, 1]
                                for ct in cts:
                                    its.append((g, tcn, ct, ct == cts[-1]))
                        LAG = 2
                        pend = []
                        for n in range(len(its) + LAG):
                            if n < len(its):
                                pend.append((its[n], cmp_stage1(its[n])))
                            if n >= LAG:
                                it0, st0 = pend.pop(0)
                                cmp_stage2(it0, st0)

                        if k == 0:
                            dump("d_imp", imp_acc[:], [128, NT, 64], F32, r_imp)
                            dump("d_aout_c", Aout[:], [128, NT, 512], BF16, rA)
                        MBr = Ring([sb(stg, f"MB{i}", [128, 128], F32) for i in range(2)])
                        for (mb_t, mb_r) in zip(MBr.tiles, MBr.res):
                            MSET("dve", mb_t[:], 0.0, [mb_r])
                        tk = Ring([sb(stg, f"tk{i}", [128, 2, 64], F32) for i in range(2)])
                        mxr = Ring([sb(stg, f"mx{i}", [128, 16], F32) for i in range(2)])
                        mtr = Ring([sb(stg, f"mtr{i}", [128, 128], BF16) for i in range(2)])
                        def c3_gen():
                          for i in range(NT):
                            tk_t, tk_r = tk.next()
                            mx_t, mx_r = mxr.next()
                            TT("dve", tk_t[:, 0, :], imp_acc[:, i, :], keep_t[:, i, :], ALU.mult, [r_imp[i], r_keep], [tk_r])
                            TT("dve", tk_t[:, 0, :], tk_t[:, 0, :], force_t[:, i, :], ALU.add, [tk_r, r_force], [tk_r])
                            E("dve", lambda: nc.vector.max(out=mx_t[:, 0:8], in_=tk_t[:, 0, :]), [tk_r], [mx_r])
                            E("dve", lambda: nc.vector.match_replace(out=tk_t[:, 1, :], in_to_replace=mx_t[:, 0:8], in_values=tk_t[:, 0, :], imm_value=-1e30), [tk_r, mx_r], [tk_r])
                            E("dve", lambda: nc.vector.max(out=mx_t[:, 8:16], in_=tk_t[:, 1, :]), [tk_r], [mx_r])
                            mb_t, mb_r = MBr.next()
                            TS("dve", mb_t[:, 64:128], tk_t[:, 0, :], mx_t[:, 15:16], None, ALU.is_ge, None, [tk_r, mx_r], [mb_r])
                            TS("dve", mb_t[:, 64:128], mb_t[:, 64:128], 1.0, -NEGM, ALU.subtract, ALU.mult, [mb_r], [mb_r])
                            px, px_r = pX.next()
                            TR(px[:, 0:128], mb_t[:], ident_f[:], [mb_r, r_identf], [px_r])
                            mt_t, mt_r = mtr.next()
                            CP("act", mt_t[64:128, :], px[64:128, 0:128], [px_r], [mt_r])
                            for g in range(4):
                                CP("pool" if g % 2 == 0 else "dve", QT[64:128, g, i * 128:(i + 1) * 128], mt_t[64:128, :], [mt_r], [r_QM[g][i]])
                            yield

                        if k == 0:
                            dump("d_qt0", QT[:, 0, :], [128, T], BF16, r_QT + [x for l in r_QM for x in l])
                        def sel_stage1(it):
                            g, br, tcn, j = it
                            hg = 4 * k + g
                            qa = max(0, j - 4 * tcn)
                            qb = 3 if br == 1 else min(3, j + 4 - 4 * tcn)
                            c0, c1 = qa * 128, (qb + 1) * 128
                            t0 = tcn * 512
                            adds = []
                            for q in range(qa, qb + 1):
                                dlt = 4 * tcn + q - j
                                if dlt == 0:
                                    adds.append((q, 0))
                                elif dlt == 1:
                                    adds.append((q, 1))
                                elif dlt == 4 and br == 2:
                                    adds.append((q, 2))
                            p_s, p_r = pS.next()
                            if br == 1:
                                rd = [r_KsT, r_QT[g]] + [r_QM[g][4 * tcn + q] for q in range(qa, qb + 1)]
                                MM(p_s[:, c0:c1], KsT[:, j * 128:(j + 1) * 128], QT[:, g, t0 + c0:t0 + c1], True, len(adds) == 0, rd, [p_r])
                            else:
                                MM(p_s[:, c0:c1], KwT[:, j * 128:(j + 1) * 128], QT[0:64, g, t0 + c0:t0 + c1], True, len(adds) == 0, [r_KwT, r_QT[g]], [p_r])
                            for ai, (q, ty) in enumerate(adds):
                                MM(p_s[:, q * 128:(q + 1) * 128], ident_b[:], BD[:, hg, ty, :], False, ai == len(adds) - 1, [r_identb, r_BD], [p_r])
                            P_t, P_r = P_ring.next()
                            ACTF(P_t[:, c0:c1], p_s[:, c0:c1], AF.Exp, [p_r, r_t31], [P_r], bias=t31[:, hg:hg + 1])
                            return (P_t, P_r, qa, qb)

                        def sel_stage2(it, st1):
                            g, br, tcn, j = it
                            hg = 4 * k + g
                            P_t, P_r, qa, qb = st1
                            Vx, r_Vx = (Vs, r_Vs) if br == 1 else (Vw, r_Vw)
                            for q in range(qa, qb + 1):
                                i = 4 * tcn + q
                                first_j = 0 if br == 1 else max(0, i - 4)
                                MM(pO[q][:, 0:65], P_t[:, q * 128:(q + 1) * 128], Vx[:, j, :], j == first_j, j == i, [P_r, r_Vx], [r_pO[q]])
                            if j == 4 * tcn + 3:
                                items = []
                                for q in range(4):
                                    o_t, o_r = osb.next()
                                    CP("dve", o_t[:, 0:65], pO[q][:, 0:65], [r_pO[q]], [o_r])
                                    items.append((o_t, o_r, 4 * tcn + q, None, None))
                                finish_tiles(items, 64, hg, br, False)

                        def branch_gen(br):
                            its = []
                            for g in range(4):
                                for tcn in range(8):
                                    j_lo = 0 if br == 1 else max(0, 4 * tcn - 4)
                                    for j in range(j_lo, 4 * tcn + 4):
                                        its.append((g, br, tcn, j))
                            LAG = 2
                            pend = []
                            for n in range(len(its) + LAG):
                                if n < len(its):
                                    pend.append((its[n], sel_stage1(its[n])))
                                if n >= LAG:
                                    it0, st0 = pend.pop(0)
                                    sel_stage2(it0, st0)
                                if n % 4 == 3:
                                    yield

                        drive_rr([branch_gen(2), c3_gen()])
                        drive_rr([branch_gen(1)])

                dump("d_aout", Aout[:], [128, NT, 512], BF16, rA)
                with Scope(kb) as stn:
                    junkC = sb(stn, "junkC", [128, 512], BF16); r_junkC = Res()
                    an = Ring([sb(stn, f"an{i}", [128, 512], BF16) for i in range(2)])
                    af = Ring([sb(stn, f"af{i}", [128, 512], F32) for i in range(2)])
                    ao = Ring([sb(stn, f"ao{i}", [128, 512], BF16) for i in range(2)])
                    pTb = Ring([ps(stn, f"pTC{i}", [128, 512], BF16) for i in range(2)]) if False else None
                    for i in range(NT):
                        s_t, s_r = sm.next()
                        ACTF(junkC[:], Aout[:, i, :], AF.Square, [rA[i]], [r_junkC, s_r], accum_out=s_t[:, 0:1])
                        TS("dve", s_t[:, 1:2], s_t[:, 0:1], 1.0 / 512, EPS, ALU.mult, ALU.add, [s_r], [s_r])
                        ACTF(s_t[:, 1:2], s_t[:, 1:2], AF.Sqrt, [s_r], [s_r])
                        E("dve", lambda: nc.vector.reciprocal(out=s_t[:, 2:3], in_=s_t[:, 1:2]), [s_r], [s_r])
                        af_t, af_r = af.next()
                        STT(af_t[:], Aout[:, i, :], s_t[:, 2:3], ga_t[:], ALU.mult, ALU.mult, [rA[i], s_r, r_ga], [af_r])
                        px, px_r = pX.next()
                        for fc in range(4):
                            TR(px[:, fc * 128:(fc + 1) * 128], af_t[:, fc * 128:(fc + 1) * 128], ident_f[:], [af_r, r_identf], [px_r])
                        ao_t, ao_r = ao.next()
                        CP("act", ao_t[:], px[:], [px_r], [ao_r])
                        DMA("dsp" if i % 2 == 0 else "dpool", mixT_s[0:512, i * 128:(i + 1) * 128].rearrange("(f p) t -> p f t", p=128),
                            ao_t[:].rearrange("p (f t) -> p f t", f=4), [ao_r], [R["mixT_s"]])

        if "D" in phases or "D1" in phases:
            NTL = TH // 128
            with Scope(kb) as st:
                Wo = sb(st, "Wo", [128, 8, D], BF16); r_Wo = Res()
                Wq = sb(st, "Wq", [128, 8, 2048], BF16); r_Wq = Res()
                skb = sb(st, "skb", [128, 2, 128], BF16); r_skb = Res()
                repf = sb(st, "repf", [128, D], F32); r_rep = Res()
                io16 = sb(st, "io16", [128, 16], F32); r_io = Res()
                io128 = sb(st, "io128", [128, 128], F32); r_io128 = Res()
                selt = sb(st, "selt", [128, 2], F32); r_sel = Res()
                DMA("dsp", repf[:], rep4[:, 0, :], [], [r_rep])
                DMA("dact", io16[:], iota16, [], [r_io])
                DMA("dact", io128[:], iota128, [], [r_io128])
                DMA("dact", selt[:], selc, [], [r_sel])
                with Scope(kb) as stw:
                    wst = Ring([sb(stw, f"wstD{i}", [128, 2048], F32) for i in range(3)])
                    n = 0
                    for (src, dstw, dres, ncol, nch) in ((w_out_in, Wo, r_Wo, D, 8), (peer_wq, Wq, r_Wq, 2048, 8)):
                        for dc in range(nch):
                            w_t, w_r = wst.next()
                            n += 1
                            DMA("dsp" if n % 2 == 0 else "dact", w_t[:, 0:ncol], src[dc * 128:(dc + 1) * 128, :], [], [w_r])
                            CP("dve" if n % 2 == 0 else "pool", dstw[:, dc, :], w_t[:, 0:ncol], [w_r], [dres])
                    w_t, w_r = wst.next()
                    DMA("dsp", w_t[:, 0:256].rearrange("p (a k) -> p a k", a=2), sk_T.rearrange("a p k -> p a k"), [], [w_r])
                    CP("dve", skb[:], w_t[:, 0:256].rearrange("p (a k) -> p a k", a=2), [w_r], [r_skb])

                pacc = Ring([ps(st, f"pD{i}", [128, 512], F32) for i in range(4)])
                pw_ring = Ring([ps(st, f"pDw{i}", [128, 512], F32) for i in range(2)])
                ptb = Ring([ps(st, f"pDb{i}", [128, 1024], BF16) for i in range(2)])
                mst = Ring([sb(st, f"mst{i}", [128, 8, 2, 128], BF16) for i in range(2)])
                mixh_ring = Ring([sb(st, f"mixh{i}", [128, 8, 128], BF16) for i in range(1)])
                xh_ring = Ring([sb(st, f"xhD{i}", [128, D], F32) for i in range(2)])
                H_ring = Ring([sb(st, f"HD{i}", [128, D], F32) for i in range(2)])
                xnb_ring = Ring([sb(st, f"xnbD{i}", [128, D], BF16) for i in range(1)])
                xT_ring = Ring([sb(st, f"xTD{i}", [128, 8, 128], BF16) for i in range(2)])
                qTb = sb(st, "qTb", [128, 16, 128], BF16); r_qTb = Res()
                Ssc_ring = Ring([sb(st, f"Ssc{i}", [128, 16, 128], F32) for i in range(2)])
                Swk = sb(st, "Swk", [128, 8, 128], F32)
                rv = [Res() for _ in range(16)]; rv2 = [Res() for _ in range(16)]; ri = [Res() for _ in range(16)]; ri2 = [Res() for _ in range(16)]; rw = [Res() for _ in range(16)]
                v16 = sb(st, "v16", [128, 16, 16], F32); r_v16 = Res()
                i16 = sb(st, "i16", [128, 16, 16], U32); r_i16 = Res()
                i16f = sb(st, "i16f", [128, 16, 16], F32); r_i16f = Res()
                cand = sb(st, "cand", [128, 8, 256], F32); r_cand = Res()
                cwk = sb(st, "cwk", [128, 8, 256], F32)
                sc16 = sb(st, "sc16", [128, 8, 16], F32); r_sc = Res()
                ci16 = sb(st, "ci16", [128, 8, 16], U32); r_ci = Res()
                ab_u = sb(st, "ab_u", [128, 2, 8, 16], U32); r_abu = Res()
                ab_f = sb(st, "ab_f", [128, 2, 8, 16], F32); r_abf = Res()
                eq = sb(st, "eq", [128, 8, 16, 16], F32); r_eq = Res()
                isel_ring = Ring([sb(st, f"isel{i}", [128, 3, 8, 16], F32) for i in range(2)])
                gz = sb(st, "gz", [128, 16], F32); r_gz = Res()
                junkB = sb(st, "junkDb", [128, D], BF16); r_junkB = Res()
                smD = Ring([sb(st, f"smD{i}", [128, 8], F32) for i in range(4)])
                ijgT_ring = Ring([sb(st, f"ijgT{i}", [128, 3, 128], F32) for i in range(2)])
                OI = Ring([sb(st, f"OI{i}", [128, 16, 128], BF16) for i in range(2)])
                OJ = Ring([sb(st, f"OJ{i}", [128, 16, 128], BF16) for i in range(2)])
                OJf = Ring([sb(st, f"OJf{i}", [128, 16, 128], BF16) for i in range(2)])
                Wst = sb(st, "Wst", [128, 128, 128], BF16); r_Wst = Res()

                def rms_scaled(src, src_r, gain, gain_r, dstf, dstf_r):
                    s_t, s_r = smD.next()
                    ACTF(junkB[:], src, AF.Square, [src_r], [r_junkB, s_r], accum_out=s_t[:, 0:1])
                    TS("dve", s_t[:, 1:2], s_t[:, 0:1], 1.0 / D, EPS, ALU.mult, ALU.add, [s_r], [s_r])
                    ACTF(s_t[:, 1:2], s_t[:, 1:2], AF.Sqrt, [s_r], [s_r])
                    E("dve", lambda: nc.vector.reciprocal(out=s_t[:, 2:3], in_=s_t[:, 1:2]), [s_r], [s_r])
                    STT(dstf, src, s_t[:, 2:3], gain, ALU.mult, ALU.mult, [src_r, s_r, gain_r], [dstf_r])

                def rms_scaled_g(src, src_r, gain, gain_r, dstf, dstf_r):
                    s_t, s_r = smD.next()
                    ACTF(junkB[:], src, AF.Square, [src_r], [r_junkB, s_r], accum_out=s_t[:, 0:1])
                    yield
                    TS("dve", s_t[:, 1:2], s_t[:, 0:1], 1.0 / D, EPS, ALU.mult, ALU.add, [s_r], [s_r])
                    ACTF(s_t[:, 1:2], s_t[:, 1:2], AF.Sqrt, [s_r], [s_r])
                    yield
                    E("dve", lambda: nc.vector.reciprocal(out=s_t[:, 2:3], in_=s_t[:, 1:2]), [s_r], [s_r])
                    STT(dstf, src, s_t[:, 2:3], gain, ALU.mult, ALU.mult, [src_r, s_r, gain_r], [dstf_r])

                def transpose8(srcb, srcb_r, dstT, dstT_r, nblk=8):
                    pt, pt_r = ptb.next()
                    for dc in range(nblk):
                        TR(pt[:, dc * 128:(dc + 1) * 128], srcb[:, dc * 128:(dc + 1) * 128], ident_b[:], [srcb_r, r_identb], [pt_r])
                    CP("act", dstT.rearrange("p a t -> p (a t)"), pt[:, 0:nblk * 128], [pt_r], [dstT_r])

                def S1_load(it):
                    tsl = slice(it * 128, (it + 1) * 128)
                    xh_t, xh_r = xh_ring.next()
                    DMA("dsp", xh_t[:], xh[tsl, :], [], [xh_r])
                    m_t, m_r = mst.next()
                    for a in range(2):
                        DMA("dsp" if a == 0 else "dact", m_t[:, :, a, :], mixT_s[:, a * TH + it * 128:a * TH + (it + 1) * 128].rearrange("(f p) t -> p f t", p=128), [R["mixT_s"]], [m_r])
                    return (xh_t, xh_r, m_t, m_r)

                def S1(it, ld):
                    tsl = slice(it * 128, (it + 1) * 128)
                    xh_t, xh_r, m_t, m_r = ld
                    H, H_r = H_ring.next()
                    mixh, r_mixh = mixh_ring.next()
                    ACTF(mixh[:], m_t[:, :, 0, :], AF.Copy, [m_r, r_sel], [r_mixh], scale=selt[:, 0:1])
                    STT(mixh[:], m_t[:, :, 1, :], selt[:, 1:2], mixh[:], ALU.mult, ALU.add, [m_r, r_sel, r_mixh], [r_mixh])
                    yield
                    for ch in range(2):
                        pa, pa_r = pacc.next()
                        for fc in range(8):
                            MM(pa[:], mixh[:, fc, :], Wo[:, fc, ch * 512:(ch + 1) * 512], fc == 0, fc == 7, [r_mixh, r_Wo], [pa_r])
                        TT("dve", H[:, ch * 512:(ch + 1) * 512], pa[:], xh_t[:, ch * 512:(ch + 1) * 512], ALU.add, [pa_r, xh_r], [H_r])
                        yield
                    DMA("dpool", H1_s[tsl, :], H[:], [H_r], [R["H1_s"]])
                    xnb, xnb_r = xnb_ring.next()
                    yield from rms_scaled_g(H[:], H_r, repf[:], r_rep, xnb[:], xnb_r)
                    yield
                    xT, xT_r = xT_ring.next()
                    transpose8(xnb, xnb_r, xT[:], xT_r)
                    yield
                    DMA("dact", xnT2_s[:, :, tsl], xT[:], [xT_r], [R["xnT2_s"]])
                    for grp in range(4):
                        pa, pa_r = pacc.next()
                        for j in range(4):
                            hp = grp * 4 + j
                            for dc in range(8):
                                MM(pa[:, j * 128:(j + 1) * 128], Wq[:, dc, hp * 128:(hp + 1) * 128], xT[:, dc, :], dc == 0, dc == 7, [r_Wq, xT_r], [pa_r])
                        CP("act", qTb[:, grp * 4:(grp + 1) * 4, :].rearrange("p a t -> p (a t)"), pa[:], [pa_r], [r_qTb])
                        yield
                    Ssc, r_S = Ssc_ring.next()
                    for grp in range(4):
                        pa, pa_r = pacc.next()
                        for j in range(4):
                            hp = grp * 4 + j
                            MM(pa[:, j * 128:(j + 1) * 128], qTb[:, hp, :], skb[:, hp % 2, :], True, True, [r_qTb, r_skb], [pa_r])
                        CP("act", Ssc[:, grp * 4:(grp + 1) * 4, :].rearrange("p a t -> p (a t)"), pa[:], [pa_r], [r_S])
                        yield
                    return (Ssc, r_S)

                def S2(it, st1):
                    Ssc, r_S = st1
                    for g0 in (0, 8):
                        hps = range(g0, g0 + 8)
                        for hp in hps:
                            E("dve", lambda: nc.vector.max(out=v16[:, hp, 0:8], in_=Ssc[:, hp, :]), [r_S], [rv[hp]])
                        yield
                        for hp in hps:
                            E("dve", lambda: nc.vector.max_index(out=i16[:, hp, 0:8], in_max=v16[:, hp, 0:8], in_values=Ssc[:, hp, :]), [r_S, rv[hp]], [ri[hp]])
                        yield
                        for hp in hps:
                            E("dve", lambda: nc.vector.match_replace(out=Swk[:, hp - g0, :], in_to_replace=v16[:, hp, 0:8], in_values=Ssc[:, hp, :], imm_value=-1e30), [r_S, rv[hp]], [rw[hp - g0]])
                        yield
                        for hp in hps:
                            E("dve", lambda: nc.vector.max(out=v16[:, hp, 8:16], in_=Swk[:, hp - g0, :]), [rw[hp - g0]], [rv2[hp]])
                        yield
                        for hp in hps:
                            E("dve", lambda: nc.vector.max_index(out=i16[:, hp, 8:16], in_max=v16[:, hp, 8:16], in_values=Swk[:, hp - g0, :]), [rw[hp - g0], rv2[hp]], [ri2[hp]])
                        yield
                    r_i16 = Res()
                    E("dve", lambda: nc.vector.tensor_copy(out=i16f[:], in_=i16[:]), ri + ri2, [r_i16f, r_i16])
                    v4 = v16[:].rearrange("p (h two) k -> p h two k", two=2)
                    in0 = v4[:, :, 0, :].rearrange("p h (a o) -> p h a o", o=1).to_broadcast([128, 8, 16, 16])
                    in1 = v4[:, :, 1, :].rearrange("p h (o b) -> p h o b", o=1).to_broadcast([128, 8, 16, 16])
                    TT("dve", cand[:].rearrange("p h (a b) -> p h a b", a=16), in0, in1, ALU.add, rv + rv2, [r_cand])
                    for h in range(8):
                        E("dve", lambda: nc.vector.max(out=sc16[:, h, 0:8], in_=cand[:, h, :]), [r_cand], [rv[h]])
                    yield
                    for h in range(8):
                        E("dve", lambda: nc.vector.max_index(out=ci16[:, h, 0:8], in_max=sc16[:, h, 0:8], in_values=cand[:, h, :]), [r_cand, rv[h]], [ri[h]])
                    yield
                    for h in range(8):
                        E("dve", lambda: nc.vector.match_replace(out=cwk[:, h, :], in_to_replace=sc16[:, h, 0:8], in_values=cand[:, h, :], imm_value=-1e30), [r_cand, rv[h]], [rw[h]])
                    yield
                    for h in range(8):
                        E("dve", lambda: nc.vector.max(out=sc16[:, h, 8:16], in_=cwk[:, h, :]), [rw[h]], [rv2[h]])
                    yield
                    for h in range(8):
                        E("dve", lambda: nc.vector.max_index(out=ci16[:, h, 8:16], in_max=sc16[:, h, 8:16], in_values=cwk[:, h, :]), [rw[h], rv2[h]], [ri2[h]])
                    yield
                    r_sc = Res(); r_ci = Res()
                    E("dve", lambda: nc.vector.tensor_single_scalar(out=ab_u[:, 0], in_=ci16[:], scalar=4, op=ALU.logical_shift_right), ri[:8] + ri2[:8] + rv[:8] + rv2[:8], [r_abu, r_sc, r_ci])
                    E("dve", lambda: nc.vector.tensor_single_scalar(out=ab_u[:, 1], in_=ci16[:], scalar=15, op=ALU.bitwise_and), [r_ci], [r_abu])
                    CP("dve", ab_f[:], ab_u[:], [r_abu], [r_abf])
                    isel, r_isel = isel_ring.next()
                    i4 = i16f[:].rearrange("p (h two) k -> p h two k", two=2)
                    for w in range(2):
                        a_b = ab_f[:, w].rearrange("p h (k o) -> p h k o", o=1).to_broadcast([128, 8, 16, 16])
                        io_b = io16[:].rearrange("p (o q a) -> p o q a", o=1, q=1).to_broadcast([128, 8, 16, 16])
                        TT("dve", eq[:], a_b, io_b, ALU.is_equal, [r_abf, r_io], [r_eq])
                        iv_b = i4[:, :, w, :].rearrange("p h (o a) -> p h o a", o=1).to_broadcast([128, 8, 16, 16])
                        TT("dve", eq[:], eq[:], iv_b, ALU.mult, [r_eq, r_i16f], [r_eq])
                        E("dve", lambda: nc.vector.tensor_reduce(out=isel[:, w], in_=eq[:], axis=AX.X, op=ALU.add), [r_eq], [r_isel])
                        yield
                    TT("dve", isel[:, 2], sc16[:], sc16[:, :, 0:1].to_broadcast([128, 8, 16]), ALU.subtract, [r_sc], [r_isel])
                    ACTF(isel[:, 2], isel[:, 2], AF.Exp, [r_isel], [r_isel])
                    E("dve", lambda: nc.vector.tensor_reduce(out=gz[:, 0:8], in_=isel[:, 2], axis=AX.X, op=ALU.add), [r_isel], [r_gz])
                    E("dve", lambda: nc.vector.reciprocal(out=gz[:, 8:16], in_=gz[:, 0:8]), [r_gz], [r_gz])
                    TT("dve", isel[:, 2], isel[:, 2], gz[:, 8:16].rearrange("p (h o) -> p h o", o=1).to_broadcast([128, 8, 16]), ALU.mult, [r_isel, r_gz], [r_isel])
                    pa, pa_r = pacc.next()
                    for w in range(3):
                        TR(pa[:, w * 128:(w + 1) * 128], isel[:, w].rearrange("p h k -> p (h k)"), ident_f[:], [r_isel, r_identf], [pa_r])
                    ijgT, r_ijgT = ijgT_ring.next()
                    CP("act", ijgT[:].rearrange("p a t -> p (a t)"), pa[:, 0:384], [pa_r], [r_ijgT])
                    return (ijgT, r_ijgT)

                def S3(it, st2):
                    ijgT, r_ijgT = st2
                    TB = 16
                    io_b = io128[:].rearrange("p (o i) -> p o i", o=1).to_broadcast([128, TB, 128])

                    def onehots(tb):
                        t0 = tb * TB
                        oi, oi_r = OI.next()
                        oj, oj_r = OJ.next()
                        ojf, ojf_r = OJf.next()

                        def colb(w):
                            return ijgT[:, w, t0:t0 + TB].rearrange("p (t o) -> p t o", o=1).to_broadcast([128, TB, 128])
                        TT("dve", oi[:], io_b, colb(0), ALU.is_equal, [r_io128, r_ijgT], [oi_r])
                        TT("dve", ojf[:], io_b, colb(1), ALU.is_equal, [r_io128, r_ijgT], [ojf_r])
                        TT("pool", oj[:], ojf[:], colb(2), ALU.mult, [ojf_r, r_ijgT], [oj_r])
                        return (oi, oi_r, oj, oj_r)

                    nxt = onehots(0)
                    yield
                    for tb in range(128 // TB):
                        t0 = tb * TB
                        oi, oi_r, oj, oj_r = nxt
                        if tb + 1 < 128 // TB:
                            nxt = onehots(tb + 1)
                        for tq in range(TB // 4):
                            pw, pw_r = pw_ring.next()
                            for u in range(4):
                                MM(pw[:, u:512:4], oj[:, tq * 4 + u, :], oi[:, tq * 4 + u, :], True, True, [oj_r, oi_r], [pw_r])
                            tg = t0 + tq * 4
                            CP("act", Wst[:, :, tg:tg + 4], pw[:].rearrange("p (i t) -> p i t", t=4), [pw_r], [r_Wst])
                            yield
                    DMA("dsp" if it % 2 == 0 else "dact", Wt_s[it], Wst[:], [r_Wst], [R["Wt_s"]])

                def drive(gens):
                    res = [None] * len(gens)
                    live = list(range(len(gens)))
                    while live:
                        for gi in list(live):
                            try:
                                next(gens[gi])
                            except StopIteration as e:
                                res[gi] = e.value
                                live.remove(gi)
                    return res

                st1s, st2s = {}, {}
                lds = {0: S1_load(0)}
                for n in range(NTL + 2):
                    gens, tags, wts = [], [], []
                    if n + 1 < NTL:
                        lds[n + 1] = S1_load(n + 1)
                    if n < NTL:
                        gens.append(S1(n, lds.pop(n))); tags.append(("s1", n)); wts.append(1)
                    if n >= 2:
                        gens.append(S3(n - 2, st2s.pop(n - 2))); tags.append(("s3", n - 2)); wts.append(4)
                    if 1 <= n <= NTL:
                        gens.append(S2(n - 1, st1s.pop(n - 1))); tags.append(("s2", n - 1)); wts.append(2)
                    for (tg_, tn), rv_ in zip(tags, drive_rr(gens, wts)):
                        if tg_ == "s1":
                            st1s[tn] = rv_
                        elif tg_ == "s2":
                            st2s[tn] = rv_

            with Scope(kb) as st:
                Yacc = sb(st, "Yacc", [128, NTL, D], F32)
                rY = [Res() for _ in range(NTL)]
                p1 = Ring([ps(st, f"pE1{i}", [128, 512], F32) for i in range(3)])
                p2 = Ring([ps(st, f"pE2{i}", [128, 512], F32) for i in range(3)])
                ptb2 = Ring([ps(st, f"pEb{i}", [128, 1024], BF16) for i in range(2)])
                with Scope(kb) as st2:
                  if "D" in phases or "D2" in phases:
                    xnTa = sb(st2, "xnTa", [128, 8, TH], BF16); r_xnTa = Res()
                    for dc in range(8):
                        DMA("dsp" if dc % 2 == 0 else "dact", xnTa[:, dc, :], xnT2_s[:, dc, :], [R["xnT2_s"]], [r_xnTa])
                    H1v = H1_s.rearrange("(n p) d -> p n d", p=128)
                    for n4 in range(4):
                        DMA("dsp" if n4 % 2 == 0 else "dact", Yacc[:, n4 * 4:(n4 + 1) * 4, :], H1v[:, n4 * 4:(n4 + 1) * 4, :], [R["H1_s"]], rY[n4 * 4:(n4 + 1) * 4])
                    IB = 4
                    NB = 128 // IB
                    ust = Ring([sb(st2, f"ust{i}", [128, D], F32) for i in range(2)])
                    vst = Ring([sb(st2, f"vst{i}", [128, D], F32) for i in range(2)])
                    ub = Ring([sb(st2, f"ub{i}", [128, D], BF16) for i in range(2)])
                    uT = Ring([sb(st2, f"uT{i}", [128, 8, 128], BF16) for i in range(2)])
                    Vbs = [sb(st2, f"Vb{i}", [128, IB, D], BF16) for i in range(2)]
                    WAs = [sb(st2, f"WA{i}", [128, IB, TH], BF16) for i in range(2)]
                    r_Vbs = [[Res() for _ in range(IB)] for _ in range(2)]
                    r_WAs = [[Res() for _ in range(IB)] for _ in range(2)]
                    wt = Ring([sb(st2, f"wt{i}", [128, TH], BF16) for i in range(3)])
                    gl = Ring([sb(st2, f"gl{i}", [128, 512], BF16) for i in range(3)])

                    def genS1(blk):
                        Vb, WA, r_Vb, r_WA = Vbs[blk % 2], WAs[blk % 2], r_Vbs[blk % 2], r_WAs[blk % 2]
                        for ib in range(IB):
                            i = blk * IB + ib
                            u_t, u_r = ust.next()
                            v_t, v_r = vst.next()
                            w_t, w_r = wt.next()
                            DMA("dsp", u_t[:], peer_u[i * 128:(i + 1) * 128, :], [], [u_r])
                            DMA("dact", v_t[:], peer_v[i * 128:(i + 1) * 128, :], [], [v_r])
                            for hw in range(2):
                                DMA("dpool" if hw == 0 else ("dsp" if i % 2 == 0 else "dact"), w_t[:, hw * 1024:(hw + 1) * 1024].rearrange("p (n t) -> p n t", t=128),
                                    Wt_s[hw * 8:(hw + 1) * 8, :, i, :].rearrange("n j t -> j n t"), [R["Wt_s"]], [w_r])
                            ub_t, ub_r = ub.next()
                            CP("pool", ub_t[:], u_t[:], [u_r], [ub_r])
                            CP("pool", Vb[:, ib, :], v_t[:], [v_r], [r_Vb[ib]])
                            pt, pt_r = ptb2.next()
                            for dc in range(8):
                                TR(pt[:, dc * 128:(dc + 1) * 128], ub_t[:, dc * 128:(dc + 1) * 128], ident_b[:], [ub_r, r_identb], [pt_r])
                            uT_t, uT_r = uT.next()
                            CP("act", uT_t[:].rearrange("p a t -> p (a t)"), pt[:], [pt_r], [uT_r])
                            yield
                            for tc4 in range(4):
                                pa, pa_r = p1.next()
                                for dc in range(8):
                                    MM(pa[:], uT_t[:, dc, :], xnTa[:, dc, tc4 * 512:(tc4 + 1) * 512], dc == 0, dc == 7, [uT_r, r_xnTa], [pa_r])
                                g_t, g_r = gl.next()
                                ACTF(g_t[:], pa[:], AF.Gelu_apprx_tanh, [pa_r], [g_r])
                                TT("dve", WA[:, ib, tc4 * 512:(tc4 + 1) * 512], g_t[:], w_t[:, tc4 * 512:(tc4 + 1) * 512], ALU.mult, [g_r, w_r], [r_WA[ib]])
                                yield

                    def genS2(blk):
                        Vb, WA, r_Vb, r_WA = Vbs[blk % 2], WAs[blk % 2], r_Vbs[blk % 2], r_WAs[blk % 2]
                        for tt in range(NTL):
                            for ch in range(2):
                                pb, pb_r = p2.next()
                                for ib in range(IB):
                                    MM(pb[:], WA[:, ib, tt * 128:(tt + 1) * 128], Vb[:, ib, ch * 512:(ch + 1) * 512], ib == 0, ib == IB - 1, [r_WA[ib], r_Vb[ib]], [pb_r])
                                TT("dve", Yacc[:, tt, ch * 512:(ch + 1) * 512], Yacc[:, tt, ch * 512:(ch + 1) * 512], pb[:], ALU.add, [rY[tt], pb_r], [rY[tt]])
                            yield

                    drive_rr([genS1(0)])
                    for blk in range(NB):
                        gens = []
                        if blk + 1 < NB:
                            gens.append(genS1(blk + 1))
                        gens.append(genS2(blk))
                        drive_rr(gens)

                with Scope(kb) as st3:
                    Wgt = sb(st3, "Wgt", [128, 8, D], BF16); r_Wgt = Res()
                    Wp = sb(st3, "Wp", [128, 2, D], BF16); r_Wp = Res()
                    rep3 = sb(st3, "rep3", [128, 3, D], F32); r_rep3 = Res()
                    DMA("dsp", rep3[:], rep4[:, 1:4, :], [], [r_rep3])
                    wst3 = Ring([sb(st3, f"wst3{i}", [128, D], F32) for i in range(2)])
                    for (src, dstw, dres, nch) in ((ple_wg, Wgt, r_Wgt, 8), (ple_pj, Wp, r_Wp, 2)):
                        for dc in range(nch):
                            w_t, w_r = wst3.next()
                            DMA("dsp" if dc % 2 == 0 else "dact", w_t[:], src[dc * 128:(dc + 1) * 128, :], [], [w_r])
                            CP("dve" if dc % 2 == 0 else "pool", dstw[:, dc, :], w_t[:], [w_r], [dres])
                    x3_ring = Ring([sb(st3, f"x3{i}", [128, D], F32) for i in range(2)])
                    x3b_ring = Ring([sb(st3, f"x3b{i}", [128, D], BF16) for i in range(2)])
                    x3T_ring = Ring([sb(st3, f"x3T{i}", [128, 8, 128], BF16) for i in range(2)])
                    pht = Ring([sb(st3, f"pht{i}", [128, 256], F32) for i in range(2)])
                    phb = Ring([sb(st3, f"phb{i}", [128, 256], BF16) for i in range(2)])
                    phT = Ring([sb(st3, f"phT{i}", [128, 2, 128], BF16) for i in range(2)])
                    gt_ring = Ring([sb(st3, f"gtD{i}", [128, D], F32) for i in range(2)])
                    ot_ring = Ring([sb(st3, f"otD{i}", [128, D], F32) for i in range(2)])
                    junk3 = sb(st3, "junk3", [128, D], BF16); r_junk3 = Res()
                    sm3 = Ring([sb(st3, f"sm3{i}", [128, 8], F32) for i in range(4)])

                    def rms3(src, src_r, gi, dstf, dstf_r):
                        s_t, s_r = sm3.next()
                        ACTF(junk3[:], src, AF.Square, [src_r], [r_junk3, s_r], accum_out=s_t[:, 0:1])
                        TS("dve", s_t[:, 1:2], s_t[:, 0:1], 1.0 / D, EPS, ALU.mult, ALU.add, [s_r], [s_r])
                        ACTF(s_t[:, 1:2], s_t[:, 1:2], AF.Sqrt, [s_r], [s_r])
                        E("dve", lambda: nc.vector.reciprocal(out=s_t[:, 2:3], in_=s_t[:, 1:2]), [s_r], [s_r])
                        STT(dstf, src, s_t[:, 2:3], rep3[:, gi, :], ALU.mult, ALU.mult, [src_r, s_r, r_rep3], [dstf_r])

                    def tr3(srcb, srcb_r, dstT, dstT_r, nblk):
                        pt, pt_r = ptb2.next()
                        for dc in range(nblk):
                            TR(pt[:, dc * 128:(dc + 1) * 128], srcb[:, dc * 128:(dc + 1) * 128], ident_b[:], [srcb_r, r_identb], [pt_r])
                        CP("act", dstT.rearrange("p a t -> p (a t)"), pt[:, 0:nblk * 128], [pt_r], [dstT_r])

                    def genD3(it):
                        tsl = slice(it * 128, (it + 1) * 128)
                        Hh = Yacc[:, it, :]; H_r = rY[it]
                        ph_t, ph_r = pht.next()
                        DMA("dact", ph_t[:], ph[tsl, :], [], [ph_r])
                        x3, x3_r = x3_ring.next()
                        rms3(Hh, H_r, 0, x3[:], x3_r)
                        yield
                        x3b, x3b_r = x3b_ring.next()
                        CP("pool", x3b[:], x3[:], [x3_r], [x3b_r])
                        pb_t, pb_r = phb.next()
                        CP("pool", pb_t[:], ph_t[:], [ph_r], [pb_r])
                        yield
                        x3T, x3T_r = x3T_ring.next()
                        tr3(x3b, x3b_r, x3T[:], x3T_r, 8)
                        pT_t, pT_r = phT.next()
                        tr3(pb_t, pb_r, pT_t[:], pT_r, 2)
                        yield
                        gt, gt_r = gt_ring.next()
                        for ch in range(2):
                            csl = slice(ch * 512, (ch + 1) * 512)
                            pa, pa_r = p1.next()
                            for dc in range(8):
                                MM(pa[:], x3T[:, dc, :], Wgt[:, dc, csl], dc == 0, dc == 7, [x3T_r, r_Wgt], [pa_r])
                            TT("dve", gt[:, csl], pa[:], rep3[:, 2, csl], ALU.add, [pa_r, r_rep3], [gt_r])
                            ACTF(gt[:, csl], gt[:, csl], AF.Sigmoid, [gt_r], [gt_r])
                            pb2, pb2_r = p2.next()
                            for dc in range(2):
                                MM(pb2[:], pT_t[:, dc, :], Wp[:, dc, csl], dc == 0, dc == 1, [pT_r, r_Wp], [pb2_r])
                            yield
                            TT("dve", gt[:, csl], gt[:, csl], pb2[:], ALU.mult, [gt_r, pb2_r], [gt_r])
                            TT("pool", Yacc[:, it, csl], Yacc[:, it, csl], gt[:, csl], ALU.add, [H_r, gt_r], [H_r])
                            yield
                        ot, ot_r = ot_ring.next()
                        rms3(Hh, H_r, 1, ot[:], ot_r)
                        DMA("dsp", out[tsl, :], ot[:], [ot_r], [])

                    for it in range(0, NTL, 2):
                        drive_rr([genD3(it), genD3(it + 1)])

        kb.drain_all()
    return nc


def _blockdiag(w):
    o = np.zeros((128, 4, 128), np.float32)
    for n in range(8):
        cc, j = n // 2, n % 2
        o[j * 64:(j + 1) * 64, cc, j * 64:(j + 1) * 64] = w[n]
    return o


def _rel_bucket(dist):
    n = np.maximum(dist, 0)
    nf = np.maximum(n, 16).astype(np.float32)
    large = 16 + (np.log(nf / np.float32(16)) / np.float32(np.log(8.0)) * np.float32(16)).astype(np.int32)
    large = np.minimum(large, 31)
    return np.where(n < 16, n, large)


def _nsa_consts(rel_table):
    c = {}
    assert (_rel_bucket(np.arange(113, 8192)) == 31).all()
    cl = np.arange(128)[:, None, None]; mp = np.arange(5)[None, :, None]; tt = np.arange(512)[None, None, :]
    dist = 512 * mp + tt - 16 * cl - 31
    c["bc_g"] = np.ascontiguousarray(rel_table[_rel_bucket(dist)].transpose(3, 0, 1, 2))
    c["bc_m"] = np.where(dist >= 0, 0.0, NEGM).astype(np.float32)
    assert (512 * 5 - 16 * 127 - 31) >= 113
    sl = np.arange(128)[:, None]; tl = np.arange(128)[None, :]
    d0 = tl - sl; d1 = 128 + tl - sl
    g0 = rel_table[_rel_bucket(d0)]; g1 = rel_table[_rel_bucket(d1)]
    c["bd_g"] = np.ascontiguousarray(np.stack([g0, g1], 0).transpose(3, 1, 0, 2))
    m0 = np.where(d0 >= 0, 0.0, NEGM); m2 = np.where(tl < sl, 0.0, NEGM)
    c["bd_m"] = np.ascontiguousarray(np.stack([m0, np.zeros_like(m0), m2], 1)).astype(np.float32)
    c["t31"] = np.ascontiguousarray(np.broadcast_to(rel_table[31][None, :], (128, 8))).astype(np.float32)
    t = (np.arange(NT)[None, :, None] * 128 + np.arange(128)[:, None, None])
    blk = np.arange(64)[None, None, :]
    d = t // 64 - blk
    local = (d >= 0) & (d < 2)
    init = (blk == 0) & ~local
    past = (d >= 0) & ~local & ~init
    c["force_c"] = np.where(local, 2.0e4, np.where(init, 1.0e4, np.where(past, 0.0, -1.0))).astype(np.float32)
    c["keep_c"] = past.astype(np.float32)
    cs = np.arange(256)[:, None] * 16; ss = np.arange(64)[None, :] * 64
    ov = np.clip(np.minimum(cs + 32, ss + 64) - np.maximum(cs, ss), 0, None).astype(np.float32) / 32.0
    ove = np.concatenate([ov, np.ones((256, 1), np.float32)], 1)
    ove[255] = 0.0
    c["ovl_ext"] = np.ascontiguousarray(ove.reshape(2, 128, 65).transpose(1, 0, 2))
    c["erows"] = (np.arange(T)[None, :] // 64 == np.arange(64)[:, None]).astype(np.float32)
    return c


def _prep_inputs(inputs):
    x = np.ascontiguousarray(inputs["x"], dtype=np.float32)
    p = np.ascontiguousarray(inputs["p"], dtype=np.float32)
    shared = {
        "ident": np.eye(128, dtype=np.float32),
        "w_in": np.ascontiguousarray(inputs["w_in"][0]),
        "g_attn": np.ascontiguousarray(inputs["attn_norm"][0].reshape(8, 128).T),
        "lru_cw": np.ascontiguousarray(inputs["conv_w"][0][:, 0, :].reshape(4, 4, 128).transpose(2, 1, 0)),
        "lru_vec": np.ascontiguousarray(np.stack([inputs[k][0].reshape(4, 128) for k in
                                                  ("conv_b", "lru_ba", "lru_bx", "lru_lambda", "grp_norm_lru")], 0).transpose(2, 0, 1)),
        "cmp_w1": np.ascontiguousarray(np.stack([inputs[k][0].reshape(16, 128, 256).transpose(1, 0, 2) for k in ("cmp_k_w1", "cmp_v_w1")], 0)),
        "cmp_pe": np.ascontiguousarray(np.stack([inputs[k][0].reshape(16, 128).T for k in ("cmp_k_pe", "cmp_v_pe")], 0)),
        "cmp_w2": np.ascontiguousarray(np.stack([inputs[k][0].reshape(2, 128, 64).transpose(1, 0, 2) for k in ("cmp_k_w2", "cmp_v_w2")], 0)),
        "ga_rep": np.ascontiguousarray(np.broadcast_to(inputs["grp_norm_attn"][0][None, :], (128, 512))).astype(np.float32),
        "w_out": np.ascontiguousarray(inputs["w_out"][0]),
        "peer_wq": np.ascontiguousarray(inputs["peer_wq"][0]),
        "sk_T": np.ascontiguousarray(inputs["peer_subkeys"][0].transpose(0, 2, 1)),
        "peer_u": np.ascontiguousarray(inputs["peer_u"][0]),
        "peer_v": np.ascontiguousarray(inputs["peer_v"][0]),
        "ple_wgate": np.ascontiguousarray(inputs["ple_wgate"][0]),
        "ple_proj": np.ascontiguousarray(inputs["ple_proj"][0]),
        "rep4": np.ascontiguousarray(np.broadcast_to(np.stack([inputs["ffn_norm"][0], inputs["ple_norm"][0], inputs["final_norm"], inputs["ple_bgate"][0]], 0)[None], (128, 4, D))).astype(np.float32),
        "iota128": np.ascontiguousarray(np.broadcast_to(np.arange(128, dtype=np.float32)[None], (128, 128))),
        "iota16": np.ascontiguousarray(np.broadcast_to(np.arange(16, dtype=np.float32)[None], (128, 16))),
        "lru_bda": _blockdiag(inputs["lru_wa"][0]),
        "lru_bdx": _blockdiag(inputs["lru_wx"][0]),
    }
    shared.update(_nsa_consts(np.asarray(inputs["rel_table"], np.float32)))
    in_maps = []
    for c in range(8):
        b, hf = c // 2, c % 2
        m = dict(shared)
        m["xb"] = x[b]
        m["xh"] = np.ascontiguousarray(x[b, hf * TH:(hf + 1) * TH])
        m["ph"] = np.ascontiguousarray(p[0, b, hf * TH:(hf + 1) * TH])
        sel = np.zeros((128, 2), np.float32); sel[:, hf] = 1.0
        m["selc"] = sel
        in_maps.append(m)
    return in_maps


def kernel(**inputs):
    nc = build_program()
    in_maps = _prep_inputs(inputs)
    res = run_bass_kernel_spmd(nc, in_maps, core_ids=list(range(8)))
    outp = np.zeros((4, T, D), np.float32)
    for c in range(8):
        b, hf = c // 2, c % 2
        outp[b, hf * TH:(hf + 1) * TH] = res.results[c]["out"]
    return outp
```
